# Optimizing a Trainium2 kernel written in Bass

```python
import math
import jax
import jax.numpy as jnp
from jax import lax
import numpy as np

D_MODEL = 1024
BATCH = 4
SEQ = 4096
DEPTH = 1

GRID_W = 64
CTX_LEN = 256
EPS = 1e-6

GLA_HEADS = 4
GLA_DK = 128
GLA_DV = 256
GLA_KEY = GLA_HEADS * GLA_DK
GLA_VAL = GLA_HEADS * GLA_DV
GLA_RANK = 16
GLA_GATE_NORM = 16.0
GLA_CHUNK = 16

SSD_HEADS = 16
SSD_P = 64
SSD_INNER = SSD_HEADS * SSD_P
SSD_GROUPS = 2
SSD_HPG = SSD_HEADS // SSD_GROUPS
SSD_N = 128
SSD_CONV = 3
SSD_CHUNK = 64
SSD_XBC = SSD_INNER + 2 * SSD_GROUPS * SSD_N

MIX_WIDTH = GLA_VAL + SSD_INNER

OFF_K = GLA_KEY
OFF_V = 2 * GLA_KEY
OFF_R = OFF_V + GLA_VAL
OFF_G = OFF_R + GLA_VAL
OFF_Z = OFF_G + 2 * GLA_RANK
OFF_XBC = OFF_Z + SSD_INNER
OFF_DT = OFF_XBC + SSD_XBC
IN_WIDTH = OFF_DT + 2 * SSD_HEADS
IN_SPLIT_POINTS = (OFF_K, OFF_V, OFF_R, OFF_G, OFF_Z, OFF_XBC, OFF_DT)

N_GROUPS = 4
EXPERTS_PER_GROUP = 8
N_EXPERTS = N_GROUPS * EXPERTS_PER_GROUP
TOP_K = 2
D_FF = 512

kernel_name = 'hybrid_gla_ssd_hmoe_dit_block'


def rmsnorm(x, g):
    x32 = x.astype(jnp.float32)
    y = x32 * lax.rsqrt(jnp.mean(x32 * x32, axis=-1, keepdims=True) + EPS)
    return (y * g.astype(jnp.float32)).astype(x.dtype)


def modulate(h, shift, scale):
    return h * (1.0 + scale) + shift


def adaln(cond, w, b):
    return jnp.split((jax.nn.silu(cond) @ w + b)[:, None, :], 6, axis=-1)


def dwconv2d(u, w, b, rows, cols):
    bn, l, ch = u.shape
    img = u.reshape(bn, rows, cols, ch)
    out = lax.conv_general_dilated(img, w[:, :, None, :].astype(u.dtype), (1, 1), 'SAME',
                                   dimension_numbers=('NHWC', 'HWIO', 'NHWC'), feature_group_count=ch)
    return out.reshape(bn, l, ch) + b


def gla_chunk_scan(q, k, v, log_a, s0):
    bn, l, nh, _ = q.shape
    dv = v.shape[-1]
    n = l // GLA_CHUNK

    def chunks(t):
        return jnp.moveaxis(t.astype(jnp.float32).reshape(bn, n, GLA_CHUNK, nh, t.shape[-1]), 1, 0)

    mask = jnp.tril(jnp.ones((GLA_CHUNK, GLA_CHUNK), dtype=bool))[None, :, :, None, None]

    def step(s, inp):
        qc, kc, vc, ac = inp
        b = jnp.cumsum(ac, axis=1)
        decay = jnp.exp(jnp.where(mask, b[:, :, None] - b[:, None, :], -jnp.inf))
        scores = jnp.einsum('bihd,bjhd,bijhd->bhij', qc, kc, decay)
        o = (jnp.einsum('bhij,bjhv->bihv', scores, vc)
             + jnp.einsum('bihd,bhdv->bihv', qc * jnp.exp(b), s))
        b_last = b[:, -1]
        s = (jnp.exp(b_last)[..., None] * s
             + jnp.einsum('bjhd,bjhv->bhdv', kc * jnp.exp(b_last[:, None] - b), vc))
        return s, o

    s_fin, o = lax.scan(step, s0, (chunks(q), chunks(k), chunks(v), chunks(log_a)))
    o = jnp.moveaxis(o, 0, 1).reshape(bn, l, nh, dv)
    return o.astype(q.dtype), s_fin


def ssd_chunk_scan(xs, dt, a_neg, bm, cm, s0):
    bn, l, g, hg, p = xs.shape
    n = l // SSD_CHUNK
    f32 = jnp.float32

    def chunks(t):
        return t.astype(f32).reshape((bn, n, SSD_CHUNK) + t.shape[2:])

    xc, dtc, bc, cc = chunks(xs), chunks(dt), chunks(bm), chunks(cm)
    a_cum = jnp.cumsum(dtc * a_neg.astype(f32), axis=2)
    xdt = xc * dtc[..., None]
    mask = jnp.tril(jnp.ones((SSD_CHUNK, SSD_CHUNK), dtype=bool))[None, None, :, :, None, None]
    seg = jnp.exp(jnp.where(mask, a_cum[:, :, :, None] - a_cum[:, :, None, :], -jnp.inf))
    cb = jnp.einsum('bcign,bcjgn->bcijg', cc, bc)
    y_diag = jnp.einsum('bcijgh,bcjghp->bcighp', cb[..., None] * seg, xdt)
    decay_end = jnp.exp(a_cum[:, :, -1:] - a_cum)
    chunk_states = jnp.einsum('bcjgn,bcjghp->bcghpn', bc, xdt * decay_end[..., None])
    chunk_decay = jnp.exp(a_cum[:, :, -1])

    def step(s, inp):
        st, dec = inp
        return dec[..., None, None] * s + st, s

    s_fin, s_in = lax.scan(step, s0, (jnp.moveaxis(chunk_states, 1, 0), jnp.moveaxis(chunk_decay, 1, 0)))
    s_in = jnp.moveaxis(s_in, 0, 1)
    y_off = jnp.einsum('bcign,bcghpn->bcighp', cc, s_in) * jnp.exp(a_cum)[..., None]
    y = (y_diag + y_off).reshape(bn, l, g, hg, p)
    return y.astype(xs.dtype), s_fin


def token_mixer(h, rows, cols, states, w_in, gla_w_up, gla_b, gla_norm, conv_w, conv_b,
                dt_bias, a_log, d_skip, ssd_norm, with_output):
    bn, l, _ = h.shape
    proj = h @ w_in
    q, k, v, r, g_lr, z, xbc, dt_raw = jnp.split(proj, IN_SPLIT_POINTS, axis=-1)
    gs_f, gs_b, ss_f, ss_b = states
    flip = lambda t: jnp.flip(t, axis=1)

    q = q.reshape(bn, l, GLA_HEADS, GLA_DK) * (GLA_DK ** -0.5)
    k = k.reshape(bn, l, GLA_HEADS, GLA_DK)
    v = v.reshape(bn, l, GLA_HEADS, GLA_DV)
    g_f, g_b = jnp.split(g_lr, 2, axis=-1)
    la_f = (jax.nn.log_sigmoid(g_f @ gla_w_up[0] + gla_b[0]) / GLA_GATE_NORM).reshape(bn, l, GLA_HEADS, GLA_DK)
    la_b = (jax.nn.log_sigmoid(g_b @ gla_w_up[1] + gla_b[1]) / GLA_GATE_NORM).reshape(bn, l, GLA_HEADS, GLA_DK)
    o_f, gs_f = gla_chunk_scan(q, k, v, la_f, gs_f)
    o_b, gs_b = gla_chunk_scan(flip(q), flip(k), flip(v), flip(la_b), gs_b)

    xbc = jax.nn.silu(dwconv2d(xbc, conv_w, conv_b, rows, cols))
    xs, bm, cm = jnp.split(xbc, (SSD_INNER, SSD_INNER + SSD_GROUPS * SSD_N), axis=-1)
    xs = xs.reshape(bn, l, SSD_GROUPS, SSD_HPG, SSD_P)
    bm = bm.reshape(bn, l, SSD_GROUPS, SSD_N)
    cm = cm.reshape(bn, l, SSD_GROUPS, SSD_N)
    dt_f, dt_b = jnp.split(dt_raw, 2, axis=-1)
    dt_f = jax.nn.softplus(dt_f + dt_bias[0]).reshape(bn, l, SSD_GROUPS, SSD_HPG)
    dt_b = jax.nn.softplus(dt_b + dt_bias[1]).reshape(bn, l, SSD_GROUPS, SSD_HPG)
    a_f = -jnp.exp(a_log[0]).reshape(SSD_GROUPS, SSD_HPG)
    a_b = -jnp.exp(a_log[1]).reshape(SSD_GROUPS, SSD_HPG)
    y_f, ss_f = ssd_chunk_scan(xs, dt_f, a_f, bm, cm, ss_f)
    y_b, ss_b = ssd_chunk_scan(flip(xs), flip(dt_b), a_b, flip(bm), flip(cm), ss_b)
    new_states = (gs_f, gs_b, ss_f, ss_b)
    if not with_output:
        return None, new_states

    o = rmsnorm(o_f + flip(o_b), gla_norm).reshape(bn, l, GLA_VAL) * jax.nn.silu(r)
    y = y_f + flip(y_b) + xs * d_skip.reshape(SSD_GROUPS, SSD_HPG)[..., None]
    y = y.reshape(bn, l, SSD_INNER) * jax.nn.silu(z)
    y = rmsnorm(y.reshape(bn, l, SSD_GROUPS, SSD_INNER // SSD_GROUPS),
                ssd_norm.reshape(SSD_GROUPS, SSD_INNER // SSD_GROUPS)).reshape(bn, l, SSD_INNER)
    return jnp.concatenate([o, y], axis=-1), new_states


def hier_moe(h, wg, bg, we, be, w_gate, w_up, w_down):
    bn, l, d = h.shape
    t = h.reshape(bn * l, d)
    g_logits = (t @ wg + bg).astype(jnp.float32)
    g_sel = jnp.argmax(g_logits, axis=-1)
    p_group = jnp.take_along_axis(jax.nn.softmax(g_logits, axis=-1), g_sel[:, None], axis=-1)
    e_logits = (t @ we + be).astype(jnp.float32).reshape(-1, N_GROUPS, EXPERTS_PER_GROUP)
    e_in = jnp.take_along_axis(e_logits, g_sel[:, None, None], axis=1)[:, 0]
    top_v, top_i = lax.top_k(e_in, TOP_K)
    w = jax.nn.softmax(top_v, axis=-1) * p_group
    idx = g_sel[:, None] * EXPERTS_PER_GROUP + top_i
    combine = jnp.einsum('tk,tke->te', w, jax.nn.one_hot(idx, N_EXPERTS, dtype=jnp.float32))
    out = jnp.zeros((t.shape[0], d), jnp.float32)
    for e in range(N_EXPERTS):
        he = jax.nn.silu(t @ w_gate[e]) * (t @ w_up[e])
        out = out + combine[:, e:e + 1] * (he @ w_down[e]).astype(jnp.float32)
    return out.reshape(bn, l, d).astype(h.dtype)


def setup_inputs(seed: int = 0) -> dict:
    key = jax.random.key(seed)
    ks = jax.random.split(key, 32)
    f32 = jnp.float32

    def nrm(k, shape, s):
        return jax.random.normal(k, shape, f32) * s

    x = nrm(ks[0], (BATCH, SEQ, D_MODEL), 1.0)
    c = nrm(ks[1], (BATCH, D_MODEL), 1.0)
    ctx = nrm(ks[2], (BATCH, CTX_LEN, D_MODEL), 1.0)
    c_ctx = nrm(ks[3], (D_MODEL,), 1.0)
    w_ada = nrm(ks[4], (DEPTH, D_MODEL, 6 * D_MODEL), 0.5 * D_MODEL ** -0.5)
    b_ada = nrm(ks[5], (DEPTH, 6 * D_MODEL), 0.02)
    norm_mix = 1.0 + nrm(ks[6], (DEPTH, D_MODEL), 0.02)
    norm_ffn = 1.0 + nrm(ks[7], (DEPTH, D_MODEL), 0.02)
    w_in = nrm(ks[8], (DEPTH, D_MODEL, IN_WIDTH), D_MODEL ** -0.5)
    gla_w_up = nrm(ks[9], (DEPTH, 2, GLA_RANK, GLA_KEY), GLA_RANK ** -0.5)
    gla_b = nrm(ks[10], (DEPTH, 2, GLA_KEY), 0.1)
    gla_norm = 1.0 + nrm(ks[11], (DEPTH, GLA_DV), 0.02)
    ssd_conv_w = nrm(ks[12], (DEPTH, SSD_CONV, SSD_CONV, SSD_XBC), 1.0 / SSD_CONV)
    ssd_conv_b = nrm(ks[13], (DEPTH, SSD_XBC), 0.02)
    dt0 = jnp.exp(jax.random.uniform(ks[14], (DEPTH, 2, SSD_HEADS), f32, math.log(1e-3), math.log(1e-1)))
    ssd_dt_bias = dt0 + jnp.log(-jnp.expm1(-dt0))
    ssd_a_log = jnp.log(jax.random.uniform(ks[15], (DEPTH, 2, SSD_HEADS), f32, 1.0, 16.0))
    ssd_d = 1.0 + nrm(ks[16], (DEPTH, SSD_HEADS), 0.1)
    ssd_norm = 1.0 + nrm(ks[17], (DEPTH, SSD_INNER), 0.02)
    w_out = nrm(ks[18], (DEPTH, MIX_WIDTH, D_MODEL), MIX_WIDTH ** -0.5)
    router_group_w = nrm(ks[19], (DEPTH, D_MODEL, N_GROUPS), D_MODEL ** -0.5)
    router_group_b = nrm(ks[20], (DEPTH, N_GROUPS), 0.01)
    router_expert_w = nrm(ks[21], (DEPTH, D_MODEL, N_EXPERTS), D_MODEL ** -0.5)
    router_expert_b = nrm(ks[22], (DEPTH, N_EXPERTS), 0.01)
    expert_w_gate = nrm(ks[23], (DEPTH, N_EXPERTS, D_MODEL, D_FF), D_MODEL ** -0.5)
    expert_w_up = nrm(ks[24], (DEPTH, N_EXPERTS, D_MODEL, D_FF), D_MODEL ** -0.5)
    expert_w_down = nrm(ks[25], (DEPTH, N_EXPERTS, D_FF, D_MODEL), D_FF ** -0.5)
    final_norm = 1.0 + nrm(ks[26], (D_MODEL,), 0.02)
    return {'x': x, 'c': c, 'ctx': ctx, 'c_ctx': c_ctx, 'w_ada': w_ada, 'b_ada': b_ada,
            'norm_mix': norm_mix, 'norm_ffn': norm_ffn, 'w_in': w_in, 'gla_w_up': gla_w_up,
            'gla_b': gla_b, 'gla_norm': gla_norm, 'ssd_conv_w': ssd_conv_w, 'ssd_conv_b': ssd_conv_b,
            'ssd_dt_bias': ssd_dt_bias, 'ssd_a_log': ssd_a_log, 'ssd_d': ssd_d, 'ssd_norm': ssd_norm,
            'w_out': w_out, 'router_group_w': router_group_w, 'router_group_b': router_group_b,
            'router_expert_w': router_expert_w, 'router_expert_b': router_expert_b,
            'expert_w_gate': expert_w_gate, 'expert_w_up': expert_w_up, 'expert_w_down': expert_w_down,
            'final_norm': final_norm}


def reference(x, c, ctx, c_ctx, w_ada, b_ada, norm_mix, norm_ffn, w_in, gla_w_up, gla_b, gla_norm,
              ssd_conv_w, ssd_conv_b, ssd_dt_bias, ssd_a_log, ssd_d, ssd_norm, w_out,
              router_group_w, router_group_b, router_expert_w, router_expert_b,
              expert_w_gate, expert_w_up, expert_w_down, final_norm):
    bn, seq_len, _ = x.shape
    rows = seq_len // GRID_W
    ctx_len = ctx.shape[1]
    zero_states = (jnp.zeros((bn, GLA_HEADS, GLA_DK, GLA_DV), jnp.float32),
                   jnp.zeros((bn, GLA_HEADS, GLA_DK, GLA_DV), jnp.float32),
                   jnp.zeros((bn, SSD_GROUPS, SSD_HPG, SSD_P, SSD_N), jnp.float32),
                   jnp.zeros((bn, SSD_GROUPS, SSD_HPG, SSD_P, SSD_N), jnp.float32))
    h_lat, h_ctx = x, ctx
    for layer in range(DEPTH):
        last = layer == DEPTH - 1
        sh1, sc1, g1, sh2, sc2, g2 = adaln(c, w_ada[layer], b_ada[layer])
        csh1, csc1, cg1, csh2, csc2, cg2 = adaln(c_ctx[None, :], w_ada[layer], b_ada[layer])
        mix_w = (w_in[layer], gla_w_up[layer], gla_b[layer], gla_norm[layer], ssd_conv_w[layer],
                 ssd_conv_b[layer], ssd_dt_bias[layer], ssd_a_log[layer], ssd_d[layer], ssd_norm[layer])
        moe_w = (router_group_w[layer], router_group_b[layer], router_expert_w[layer], router_expert_b[layer],
                 expert_w_gate[layer], expert_w_up[layer], expert_w_down[layer])
        hc = modulate(rmsnorm(h_ctx, norm_mix[layer]), csh1, csc1)
        yc, ctx_states = token_mixer(hc, 1, ctx_len, zero_states, *mix_w, with_output=not last)
        hx = modulate(rmsnorm(h_lat, norm_mix[layer]), sh1, sc1)
        yx, _ = token_mixer(hx, rows, GRID_W, ctx_states, *mix_w, with_output=True)
        h_lat = h_lat + g1 * (yx @ w_out[layer])
        h_lat = h_lat + g2 * hier_moe(modulate(rmsnorm(h_lat, norm_ffn[layer]), sh2, sc2), *moe_w)
        if not last:
            h_ctx = h_ctx + cg1 * (yc @ w_out[layer])
            h_ctx = h_ctx + cg2 * hier_moe(modulate(rmsnorm(h_ctx, norm_ffn[layer]), csh2, csc2), *moe_w)
    return rmsnorm(h_lat, final_norm)
```

```python
import math
from contextlib import ExitStack

import numpy as np
import concourse.bass as bass
import concourse.mybir as mybir
from concourse.bass_utils import run_bass_kernel_spmd

F32 = mybir.dt.float32
BF16 = mybir.dt.bfloat16
AF = mybir.ActivationFunctionType
ALU = mybir.AluOpType
AX = mybir.AxisListType

D = 1024
NCTX, NOWN, NOTH = 256, 2048, 2048
TOK = NCTX + NOWN + NOTH
NT = TOK // 128
T_CTX0, T_OWN0, T_OTH0 = 0, 2, 18
EPS = 1e-6
IN_W = 5696
OFF_K, OFF_V, OFF_R, OFF_G, OFF_Z, OFF_XBC, OFF_DT = 512, 1024, 2048, 3072, 3104, 4128, 5664
NEXP, DFF = 32, 512


class Res:
    __slots__ = ("name", "lw", "rd")

    def __init__(self, name=""):
        self.name = name
        self.lw = None
        self.rd = {}


class Tile:
    def __init__(self, t, name):
        self.t = t
        self.r = Res(name)

    def __getitem__(self, idx):
        return self.t[idx]


class Tracker:
    ENG = ("pe", "act", "dve", "pool", "sp")
    CH = 2000

    def __init__(self, nc, sems, dma_sems, same_engine_sync=True):
        self.nc = nc
        self.eng = {"pe": nc.tensor, "act": nc.scalar, "dve": nc.vector, "pool": nc.gpsimd, "sp": nc.sync}
        self.cnt = {e: 0 for e in self.ENG}
        self.waited = {e: {} for e in self.ENG}
        self.sems = {e: [sems[e]] for e in sems}
        self.free_dma = list(dma_sems)
        self.same = same_engine_sync
        self.ninst = 0

    def new_dma_sem(self, group=0):
        d = self.free_dma.pop()
        self._uid = getattr(self, "_uid", 0) + 1
        d = d if isinstance(d, list) else [d, 0, 0, 0, "dma%d" % self._uid]
        if group:
            d[2] = group
            d[3] = d[1] + 16 * group
        return d

    @staticmethod
    def regroup(d, n):
        assert d[2] == 0
        d[2] = n
        d[3] = d[1] + 16 * n

    def release_dma_sem(self, d):
        self.free_dma.append(d)

    def _wait(self, e, ev):
        if ev is None:
            return
        if ev[0] == "dma":
            _, s, v, key = ev
            if self.waited[e].get(key, 0) >= v:
                return
            self.waited[e][key] = v
            self.eng[e].wait_ge(s, v)
        else:
            pe, n = ev
            if pe == e and (not self.same or e in ("pe", "sp")):
                return
            if self.waited[e].get(pe, 0) >= n:
                return
            self.waited[e][pe] = n
            self.eng[e].wait_ge(self.sems[pe][(n - 1) // self.CH], (n - 1) % self.CH + 1)

    def _deps(self, e, reads, writes):
        for r in reads:
            self._wait(e, r.lw)
        for w in writes:
            self._wait(e, w.lw)
            for ev in w.rd.values():
                self._wait(e, ev)

    @staticmethod
    def _note_read(r, ev):
        key = ev[3] if ev[0] == "dma" else ev[0]
        old = r.rd.get(key)
        if old is None or (old[2] if old[0] == "dma" else old[1]) < (ev[2] if ev[0] == "dma" else ev[1]):
            r.rd[key] = ev

    def op(self, e, fn, reads=(), writes=()):
        reads = [x.r if isinstance(x, Tile) else x for x in reads]
        writes = [x.r if isinstance(x, Tile) else x for x in writes]
        self._deps(e, reads, writes)
        self.cnt[e] += 1
        ev = (e, self.cnt[e])
        k = (self.cnt[e] - 1) // self.CH
        if k >= len(self.sems[e]):
            self.sems[e].append(self.free_dma.pop(0))
        fn(self.eng[e]).then_inc(self.sems[e][k], 1)
        self.ninst += 1
        for r in reads:
            self._note_read(r, ev)
        for w in writes:
            w.lw = ev
            w.rd = {}
        return ev

    def dma(self, e, dsem, out, in_, reads=(), writes=()):
        reads = [x.r if isinstance(x, Tile) else x for x in reads]
        writes = [x.r if isinstance(x, Tile) else x for x in writes]
        self._deps(e, reads, writes)
        if dsem[2] == 0:
            dsem[2] = 1
            dsem[3] = dsem[1] + 16
        dsem[1] += 16
        dsem[2] -= 1
        ev = ("dma", dsem[0], dsem[3], dsem[4])
        self.eng[e].dma_start(out=out, in_=in_).then_inc(dsem[0], 16)
        self.ninst += 1
        for r in reads:
            self._note_read(r, ev)
        for w in writes:
            w.lw = ev
            w.rd = {}
        return ev

    def wait_all(self, e, resources):
        for r in resources:
            r = r.r if isinstance(r, Tile) else r
            self._wait(e, r.lw)
            for ev in r.rd.values():
                self._wait(e, ev)


class Builder:
    def __init__(self, debug=None, stop_after=None):
        self.debug = debug or ()
        self.stop_after = stop_after
        self.nc = bass.Bass("TRN2", target_bir_lowering=False)
        self.dbg_out = {}

    def sb(self, st, name, shape, dt):
        self._uid = getattr(self, "_uid", 0) + 1
        return Tile(st.enter_context(self.nc.sbuf_tensor("sb%d_%s" % (self._uid, name), list(shape), dt)), name)

    def ps(self, st, name, shape=(128, 512), dt=F32):
        self._uid = getattr(self, "_uid", 0) + 1
        return Tile(st.enter_context(self.nc.psum_tensor("ps%d_%s" % (self._uid, name), list(shape), dt)), name)

    def dram_in(self, name, shape, dt=F32):
        return Tile(self.nc.dram_tensor(name, list(shape), dt, kind="ExternalInput").ap(), name)

    def dram_out(self, name, shape, dt=F32):
        return Tile(self.nc.dram_tensor(name, list(shape), dt, kind="ExternalOutput").ap(), name)

    def dram_scr(self, name, shape, dt):
        return Tile(self.nc.dram_tensor(name, list(shape), dt, kind="Internal").ap(), name)

    def dsem(self, group=0):
        return self.tr.new_dma_sem(group)

    def build(self):
        nc = self.nc
        I = {}
        I["xs"] = self.dram_in("xs", [TOK, D])
        I["cT"] = self.dram_in("cT", [128, 16])
        I["w_ada"] = self.dram_in("w_ada", [D, 6 * D])
        I["b_ada"] = self.dram_in("b_ada", [1, 6 * D])
        I["norm_mix_fm"] = self.dram_in("norm_mix_fm", [128, 8])
        I["norm_ffn_fm"] = self.dram_in("norm_ffn_fm", [128, 8])
        I["w_in"] = self.dram_in("w_in", [D, IN_W])
        I["w_up_aug"] = self.dram_in("w_up_aug", [2, 17, 512])
        I["gla_norm"] = self.dram_in("gla_norm", [1, 256])
        I["ssd_norm"] = self.dram_in("ssd_norm", [1, 1024])
        I["final_norm"] = self.dram_in("final_norm", [1, 1024])
        I["conv_w_fm"] = self.dram_in("conv_w_fm", [128, 12, 9])
        I["conv_b_fm"] = self.dram_in("conv_b_fm", [128, 12])
        I["dt_bias"] = self.dram_in("dt_bias", [1, 32])
        I["a_log"] = self.dram_in("a_log", [1, 32])
        I["ssd_d"] = self.dram_in("ssd_d", [1, 16])
        I["w_out"] = self.dram_in("w_out", [2048, D])
        I["w_router"] = self.dram_in("w_router", [128, 8 * 36])
        I["b_router"] = self.dram_in("b_router", [1, 36])
        I["w_gate"] = self.dram_in("w_gate", [NEXP, D, DFF])
        I["w_up"] = self.dram_in("w_up", [NEXP, D, DFF])
        I["w_down"] = self.dram_in("w_down", [NEXP, DFF, D])
        self.I = I
        self.out = self.dram_out("out", [NOWN, D])

        with ExitStack() as st:
            sems = {e: st.enter_context(nc.semaphore("s_" + e)) for e in Tracker.ENG}
            dsems = [st.enter_context(nc.semaphore("d%d" % i)) for i in range(90)]
            self.tr = Tracker(nc, sems, dsems)
            self.program(st)
        return nc

    def tap(self, name, tile_ap, shape, dt, reads):
        if name not in self.debug:
            return
        o = self.dram_out("dbg_" + name, shape, dt)
        self.dbg_out[name] = o
        n = shape[1]
        step = 2048
        d = self.dsem(len(range(0, n, step)))
        for c0 in range(0, n, step):
            c1 = min(n, c0 + step)
            self.tr.dma("sp", d, out=o.t[:, c0:c1], in_=tile_ap[:, c0:c1], reads=reads, writes=[o])
        self.final.append(o)

    def program(self, st):
        tr = self.tr
        self.final = []
        self.consts(st)
        self.stage_adaln(st)
        with ExitStack() as mst:
            self.stage_hT(mst)
            if self.stop_after == "hT":
                return self.finish()
            self.stage_gla(mst)
            if self.stop_after == "gla":
                return self.finish()
            self.stage_conv(mst)
            if self.stop_after == "conv":
                return self.finish()
            self.stage_ssd(mst)
            if self.stop_after == "ssd":
                return self.finish()
            self.barrier_release([self.c["hT"], self.c["BT"], self.c["CT"]] + self.c["hT_r"])
        self.stage_post(st)
        if self.stop_after == "post":
            return self.finish()
        self.stage_moe(st)
        if self.stop_after == "moe":
            return self.finish()
        self.stage_final(st)
        return self.finish()

    def finish(self):
        self.tr.wait_all("sp", self.final)

    def consts(self, st):
        tr = self.tr
        c = {}
        self.c = c
        c["ident_f"] = self.sb(st, "ident_f", [128, 128], F32)
        c["ident_b"] = self.sb(st, "ident_b", [128, 128], BF16)
        c["ones_f"] = self.sb(st, "ones_f", [128, 128], F32)
        for nm in ("tri_le", "tri_ge", "tri_gt", "tri_lt"):
            c[nm] = self.sb(st, nm, [128, 128], F32)
        idf = c["ident_f"]
        tr.op("pool", lambda e: e.memset(idf[:], 0.0), writes=[idf])
        tr.op("pool", lambda e: e.affine_select(out=idf[:], in_=idf[:], pattern=[[-1, 128]], compare_op=ALU.not_equal,
                                               fill=1.0, base=0, channel_multiplier=1), reads=[idf], writes=[idf])
        tr.op("pool", lambda e: e.tensor_copy(out=c["ident_b"][:], in_=idf[:]), reads=[idf], writes=[c["ident_b"]])
        tr.op("pool", lambda e: e.memset(c["ones_f"][:], 1.0), writes=[c["ones_f"]])
        specs = {"tri_le": (ALU.is_gt, 0), "tri_ge": (ALU.is_gt, 0), "tri_gt": (ALU.is_gt, 0), "tri_lt": (ALU.is_gt, 0)}
        t = c["tri_le"]
        tr.op("pool", lambda e: e.memset(t[:], 1.0), writes=[t])
        tr.op("pool", lambda e: e.affine_select(out=t[:], in_=t[:], pattern=[[1, 128]], compare_op=ALU.is_ge,
                                               fill=0.0, base=0, channel_multiplier=-1), reads=[t], writes=[t])
        t2 = c["tri_ge"]
        tr.op("pool", lambda e: e.memset(t2[:], 1.0), writes=[t2])
        tr.op("pool", lambda e: e.affine_select(out=t2[:], in_=t2[:], pattern=[[-1, 128]], compare_op=ALU.is_ge,
                                               fill=0.0, base=0, channel_multiplier=1), reads=[t2], writes=[t2])
        t3 = c["tri_gt"]
        tr.op("pool", lambda e: e.memset(t3[:], 1.0), writes=[t3])
        tr.op("pool", lambda e: e.affine_select(out=t3[:], in_=t3[:], pattern=[[-1, 128]], compare_op=ALU.is_gt,
                                               fill=0.0, base=0, channel_multiplier=1), reads=[t3], writes=[t3])
        t4 = c["tri_lt"]
        tr.op("pool", lambda e: e.memset(t4[:], 1.0), writes=[t4])
        tr.op("pool", lambda e: e.affine_select(out=t4[:], in_=t4[:], pattern=[[1, 128]], compare_op=ALU.is_gt,
                                               fill=0.0, base=0, channel_multiplier=-1), reads=[t4], writes=[t4])
        self.tap("tri_le", c["tri_le"][:], [128, 128], F32, [c["tri_le"]])
        self.tap("tri_gt", c["tri_gt"][:], [128, 128], F32, [c["tri_gt"]])

    def stage_adaln(self, st):
        tr, c, I = self.tr, self.c, self.I
        c["mod_fm"] = self.sb(st, "mod_fm", [128, 6, 8, 2], F32)
        c["g1_bc"] = self.sb(st, "g1_bc", [128, D], F32)
        c["g2_bc"] = self.sb(st, "g2_bc", [128, D], F32)
        c["s1"] = self.sb(st, "s1", [128, 8], F32)
        c["s1c"] = self.sb(st, "s1c", [128, 8], F32)
        c["b1"] = self.sb(st, "b1", [128, 8], F32)
        c["b1c"] = self.sb(st, "b1c", [128, 8], F32)
        c["s2"] = self.sb(st, "s2", [128, 8], F32)
        c["b2"] = self.sb(st, "b2", [128, 8], F32)
        with ExitStack() as s2:
            cT = self.sb(s2, "cT", [128, 16], F32)
            scT = self.sb(s2, "scT", [128, 16], F32)
            sc_rep = self.sb(s2, "sc_rep", [128, 8, 128], F32)
            brow = self.sb(s2, "brow", [1, 6 * D], F32)
            nm = self.sb(s2, "nm", [128, 8], F32)
            nf = self.sb(s2, "nf", [128, 8], F32)
            wblk = [self.sb(s2, "wblk%d" % i, [128, 8, D], F32) for i in range(2)]
            wsem = [self.dsem() for _ in range(2)]
            modps = self.ps(s2, "modps", [128, 512], F32)
            gps = [self.ps(s2, "gps%d" % i, [128, 512], F32) for i in range(2)]
            d = self.dsem(4)
            tr.dma("sp", d, out=cT[:], in_=I["cT"].t, writes=[cT])
            tr.dma("sp", d, out=brow[:], in_=I["b_ada"].t, writes=[brow])
            tr.dma("sp", d, out=nm[:], in_=I["norm_mix_fm"].t, writes=[nm])
            tr.dma("sp", d, out=nf[:], in_=I["norm_ffn_fm"].t, writes=[nf])
            tr.op("act", lambda e: e.activation(out=scT[:], in_=cT[:], func=AF.Silu), reads=[cT], writes=[scT])
            tr.op("dve", lambda e: e.tensor_copy(out=sc_rep[:], in_=scT[:].rearrange("p (k j) -> p k j", j=2)[:, :, 0:1].to_broadcast([128, 8, 128])),
                  reads=[scT], writes=[sc_rep])
            w_ada = I["w_ada"].t.rearrange("(kc p) n -> p kc n", p=128)
            mview = modps[:, 0:96].rearrange("p (b f t) -> p b f t", b=6, f=8)
            for blk in range(6):
                wb = wblk[blk % 2]
                tr.dma("sp", wsem[blk % 2], out=wb[:], in_=w_ada[:, :, blk * D:(blk + 1) * D], writes=[wb])
                if blk in (0, 1, 3, 4):
                    for fc in range(8):
                        for kc in range(8):
                            tr.op("pe", lambda e, fc=fc, kc=kc, wb=wb, blk=blk: e.matmul(
                                out=mview[:, blk, fc, :], lhsT=wb[:, kc, fc * 128:(fc + 1) * 128],
                                rhs=scT[:, 2 * kc:2 * kc + 2], start=(kc == 0), stop=False),
                                reads=[wb, scT], writes=[modps])
                        tr.op("pe", lambda e, fc=fc, blk=blk: e.matmul(
                            out=mview[:, blk, fc, :], lhsT=brow[0:1, blk * D + fc * 128: blk * D + (fc + 1) * 128],
                            rhs=c["ones_f"][0:1, 0:2], start=False, stop=True),
                            reads=[brow, c["ones_f"]], writes=[modps])
                else:
                    gdst = c["g1_bc"] if blk == 2 else c["g2_bc"]
                    for hh in range(2):
                        for kc in range(8):
                            tr.op("pe", lambda e, hh=hh, kc=kc, wb=wb: e.matmul(
                                out=gps[hh][:], lhsT=sc_rep[:, kc, :], rhs=wb[:, kc, hh * 512:(hh + 1) * 512],
                                start=(kc == 0), stop=False), reads=[wb, sc_rep], writes=[gps[hh]])
                        tr.op("pe", lambda e, hh=hh, blk=blk: e.matmul(
                            out=gps[hh][:], lhsT=c["ones_f"][0:1, :], rhs=brow[0:1, blk * D + hh * 512: blk * D + (hh + 1) * 512],
                            start=False, stop=True), reads=[brow, c["ones_f"]], writes=[gps[hh]])
                        tr.op("act", lambda e, hh=hh, gdst=gdst: e.activation(out=gdst[:, hh * 512:(hh + 1) * 512], in_=gps[hh][:], func=AF.Copy),
                              reads=[gps[hh]], writes=[gdst])
            mf = c["mod_fm"]
            tr.op("dve", lambda e: e.tensor_copy(out=mf[:].rearrange("p b f t -> p (b f t)"), in_=modps[:, 0:96]), reads=[modps], writes=[mf])
            tr.op("dve", lambda e: e.scalar_tensor_tensor(out=c["s1"][:], in0=mf[:, 1, :, 0], scalar=1.0, in1=nm[:], op0=ALU.add, op1=ALU.mult),
                  reads=[mf, nm], writes=[c["s1"]])
            tr.op("dve", lambda e: e.scalar_tensor_tensor(out=c["s1c"][:], in0=mf[:, 1, :, 1], scalar=1.0, in1=nm[:], op0=ALU.add, op1=ALU.mult),
                  reads=[mf, nm], writes=[c["s1c"]])
            tr.op("dve", lambda e: e.scalar_tensor_tensor(out=c["s2"][:], in0=mf[:, 4, :, 0], scalar=1.0, in1=nf[:], op0=ALU.add, op1=ALU.mult),
                  reads=[mf, nf], writes=[c["s2"]])
            tr.op("dve", lambda e: e.tensor_copy(out=c["b1"][:], in_=mf[:, 0, :, 0]), reads=[mf], writes=[c["b1"]])
            tr.op("dve", lambda e: e.tensor_copy(out=c["b1c"][:], in_=mf[:, 0, :, 1]), reads=[mf], writes=[c["b1c"]])
            tr.op("dve", lambda e: e.tensor_copy(out=c["b2"][:], in_=mf[:, 3, :, 0]), reads=[mf], writes=[c["b2"]])
            self.tap("mod_fm", mf[:].rearrange("p b f t -> p (b f t)"), [128, 96], F32, [mf])
            self.tap("g1_bc", c["g1_bc"][:], [128, D], F32, [c["g1_bc"]])
            self.barrier_release([cT, scT, sc_rep, brow, nm, nf, wblk[0], wblk[1], modps, gps[0], gps[1]])

    def barrier_release(self, tiles):
        self.pending = getattr(self, "pending", [])
        for t in tiles:
            self.pending.append(t.r if isinstance(t, Tile) else t)

    def fence(self):
        pend = getattr(self, "pending", [])
        for e in Tracker.ENG:
            self.tr.wait_all(e, pend)
        self.pending = []

    def stage_hT(self, st):
        tr, c, I = self.tr, self.c, self.I
        self.fence()
        c["hT"] = self.sb(st, "hT", [128, 8, TOK], BF16)
        c["hT_r"] = [Res("hT%d" % t) for t in range(NT)]
        with ExitStack() as s2:
            xr = [self.sb(s2, "xr%d" % i, [128, D], F32) for i in range(3)]
            xsem = [self.dsem() for _ in range(3)]
            junk = self.sb(s2, "junk", [128, D], BF16)
            ss = [self.sb(s2, "ss%d" % i, [128, 1], F32) for i in range(3)]
            rstd = [self.sb(s2, "rstd%d" % i, [128, 1], F32) for i in range(3)]
            xn = [self.sb(s2, "xn%d" % i, [128, D], BF16) for i in range(2)]
            tmp = [self.sb(s2, "tmp%d" % i, [128, 8, 128], F32) for i in range(2)]
            tps = [self.ps(s2, "tps%d" % i, [128, 1024], BF16) for i in range(2)]
            epst = self.sb(s2, "epst", [128, 1], F32)
            tr.op("pool", lambda e: e.memset(epst[:], EPS), writes=[epst])
            rel = xr + ss + rstd + xn + tmp + tps + [junk, epst]
            for t in range(NT):
                x_t, ss_t, rs_t, xn_t, tmp_t, ps_t = xr[t % 3], ss[t % 3], rstd[t % 3], xn[t % 2], tmp[t % 2], tps[t % 2]
                hT_ap = c["hT"][:, :, t * 128:(t + 1) * 128]
                hT_r = c["hT_r"][t]
                isctx = t < T_OWN0
                sc, sh = (c["s1c"], c["b1c"]) if isctx else (c["s1"], c["b1"])
                tr.dma("sp", xsem[t % 3], out=x_t[:], in_=I["xs"].t[t * 128:(t + 1) * 128, :], writes=[x_t])
                tr.op("act", lambda e, x_t=x_t, ss_t=ss_t: e.activation(out=junk[:], in_=x_t[:], func=AF.Square, accum_out=ss_t[:]),
                      reads=[x_t], writes=[junk, ss_t])
                tr.op("act", lambda e, ss_t=ss_t, rs_t=rs_t: e.activation(out=rs_t[:], in_=ss_t[:], func=AF.Ln, scale=1.0 / D, bias=epst[:]),
                      reads=[ss_t, epst], writes=[rs_t])
                tr.op("act", lambda e, rs_t=rs_t: e.activation(out=rs_t[:], in_=rs_t[:], func=AF.Exp, scale=-0.5),
                      reads=[rs_t], writes=[rs_t])
                tr.op("dve", lambda e, x_t=x_t, rs_t=rs_t, xn_t=xn_t: e.tensor_scalar(out=xn_t[:], in0=x_t[:], scalar1=rs_t[:], scalar2=None, op0=ALU.mult),
                      reads=[x_t, rs_t], writes=[xn_t])
                for kc in range(8):
                    tr.op("pe", lambda e, kc=kc, xn_t=xn_t, ps_t=ps_t: e.transpose(out=ps_t[:, kc * 128:(kc + 1) * 128], in_=xn_t[:, kc * 128:(kc + 1) * 128], identity=c["ident_b"][:]),
                          reads=[xn_t, c["ident_b"]], writes=[ps_t])
                tr.op("dve", lambda e, ps_t=ps_t, tmp_t=tmp_t, sc=sc: e.tensor_tensor(
                    out=tmp_t[:], in0=ps_t[:].rearrange("p (k t) -> p k t", k=8), in1=sc[:].unsqueeze(2).to_broadcast([128, 8, 128]), op=ALU.mult),
                    reads=[ps_t, sc], writes=[tmp_t])
                tr.op("pool", lambda e, tmp_t=tmp_t, hT_ap=hT_ap, sh=sh: e.tensor_tensor(
                    out=hT_ap, in0=tmp_t[:], in1=sh[:].unsqueeze(2).to_broadcast([128, 8, 128]), op=ALU.add),
                    reads=[tmp_t, sh], writes=[hT_r])
            for t in (0, 2, 17, 33):
                if ("hT%d" % t) in self.debug:
                    o = self.dram_out("dbg_hT%d" % t, [128, 8, 128], BF16)
                    tr.dma("sp", self.dsem(), out=o.t, in_=c["hT"][:, :, t * 128:(t + 1) * 128], reads=[c["hT_r"][t]], writes=[o])
                    self.final.append(o)
            self.barrier_release(rel)

    def scratch(self, name, shape, dt):
        if name in self.debug:
            o = self.dram_out("dbg_" + name, shape, dt)
            self.final.append(o)
            return o
        return self.dram_scr(name, shape, dt)

    def stage_conv(self, st):
        tr, c, I = self.tr, self.c, self.I
        self.fence()
        c["x_tok"] = self.scratch("x_tok", [TOK, 1024], BF16)
        c["B_tok"] = self.scratch("B_tok", [TOK, 256], BF16)
        c["BT"] = self.sb(st, "BT", [128, 2, NOWN], BF16)
        c["CT"] = self.sb(st, "CT", [128, 2, NOWN], BF16)
        xtok_v = c["x_tok"].t.rearrange("(n p) c -> p n c", p=128)
        btok_v = c["B_tok"].t.rearrange("(n p) c -> p n c", p=128)
        w_in_v = I["w_in"].t.rearrange("(kc p) n -> p kc n", p=128)
        with ExitStack() as s2:
            wx = [self.sb(s2, "wx%d" % i, [128, 8, 128], BF16) for i in range(2)]
            wxs = [self.dsem() for _ in range(2)]
            cw = self.sb(s2, "cw", [128, 12, 9], F32)
            cb = self.sb(s2, "cb", [128, 12], F32)
            diag = [self.sb(s2, "diag%d" % i, [128, 9, 128], BF16) for i in range(2)]
            pre = [self.sb(s2, "pre%d" % i, [128, 66, 66], BF16) for i in range(2)]
            prec = [self.sb(s2, "prec%d" % i, [128, 258], BF16) for i in range(2)]
            post = [self.sb(s2, "post%d" % i, [128, 512], BF16) for i in range(3)]
            tst = [self.sb(s2, "tst%d" % i, [128, 4, 128], BF16) for i in range(3)]
            tsem = [self.dsem() for _ in range(3)]
            pp = [self.ps(s2, "pp%d" % i) for i in range(2)]
            pc = [self.ps(s2, "pc%d" % i) for i in range(2)]
            pt = [self.ps(s2, "pt%d" % i, [128, 1024], BF16) for i in range(2)]
            rel = wx + diag + pre + prec + post + tst + pp + pc + pt + [cw, cb]
            d0 = self.dsem(2)
            tr.dma("sp", d0, out=cw[:], in_=I["conv_w_fm"].t, writes=[cw])
            tr.dma("sp", d0, out=cb[:], in_=I["conv_b_fm"].t, writes=[cb])
            for i in range(2):
                tr.op("pool", lambda e, i=i: e.memset(pre[i][:], 0.0), writes=[pre[i]])
                tr.op("pool", lambda e, i=i: e.memset(prec[i][:], 0.0), writes=[prec[i]])
            nev = 0
            npost = 0
            for ct in range(12):
                w, dg, pr, prc = wx[ct % 2], diag[ct % 2], pre[ct % 2], prec[ct % 2]
                tr.dma("pool", wxs[ct % 2], out=w[:], in_=w_in_v[:, :, OFF_XBC + ct * 128: OFF_XBC + (ct + 1) * 128], writes=[w])
                for tap in range(9):
                    tr.op("pool", lambda e, tap=tap, dg=dg, ct=ct: e.tensor_scalar(out=dg[:, tap, :], in0=c["ident_f"][:], scalar1=cw[:, ct, tap:tap + 1], scalar2=None, op0=ALU.mult),
                          reads=[c["ident_f"], cw], writes=[dg])
                for blk in range(9):
                    p_t = pp[nev % 2]
                    if blk == 0:
                        n, tok0, trs = 256, 0, [0, 1]
                    else:
                        n, tok0 = 512, NCTX + (blk - 1) * 512
                        trs = list(range(T_OWN0 + (blk - 1) * 4, T_OWN0 + blk * 4))
                    for kc in range(8):
                        tr.op("pe", lambda e, kc=kc, p_t=p_t, w=w, n=n, tok0=tok0: e.matmul(
                            out=p_t[:, 0:n], lhsT=w[:, kc, :], rhs=c["hT"][:, kc, tok0:tok0 + n], start=(kc == 0), stop=(kc == 7)),
                            reads=[w] + [c["hT_r"][t] for t in trs], writes=[p_t])
                    if blk == 0:
                        dst = prc[:, 1:257]
                        src = p_t[:, 0:256]
                        wr = prc
                    else:
                        r0 = (blk - 1) * 8
                        dst = pr[:, r0 + 1:r0 + 9, 1:65]
                        src = p_t[:, 0:512].rearrange("p (r q) -> p r q", q=64)
                        wr = pr
                    eng = "act" if nev % 2 == 0 else "dve"
                    if eng == "act":
                        tr.op("act", lambda e, dst=dst, src=src: e.activation(out=dst, in_=src, func=AF.Copy), reads=[p_t], writes=[wr])
                    else:
                        tr.op("dve", lambda e, dst=dst, src=src: e.tensor_copy(out=dst, in_=src), reads=[p_t], writes=[wr])
                    nev += 1
                for blk in range(9):
                    if ct >= 10 and (blk == 0 or blk >= 5):
                        continue
                    c_t = pc[blk % 2]
                    if blk == 0:
                        n = 256
                        for kw in range(3):
                            tr.op("pe", lambda e, kw=kw, c_t=c_t, dg=dg, prc=prc: e.matmul(
                                out=c_t[:, 0:256], lhsT=dg[:, 3 + kw, :], rhs=prc[:, kw:kw + 256], start=(kw == 0), stop=(kw == 2)),
                                reads=[dg, prc], writes=[c_t])
                    else:
                        n = 512
                        r0 = (blk - 1) * 8
                        for tap in range(9):
                            kh, kw = tap // 3, tap % 3
                            tr.op("pe", lambda e, tap=tap, kh=kh, kw=kw, c_t=c_t, dg=dg, pr=pr, r0=r0: e.matmul(
                                out=c_t[:, 0:512], lhsT=dg[:, tap, :], rhs=pr[:, r0 + kh:r0 + kh + 8, kw:kw + 64], start=(tap == 0), stop=(tap == 8)),
                                reads=[dg, pr], writes=[c_t])
                    own_blk = 1 <= blk <= 4
                    if ct >= 8 and own_blk:
                        g = (ct - 8) % 2
                        dstT = (c["BT"] if ct < 10 else c["CT"])
                        o0 = (blk - 1) * 512
                        tr.op("act", lambda e, dstT=dstT, g=g, o0=o0, c_t=c_t, ct=ct: e.activation(
                            out=dstT[:, g, o0:o0 + 512], in_=c_t[:, 0:512], func=AF.Silu, bias=cb[:, ct:ct + 1]),
                            reads=[c_t, cb], writes=[dstT])
                        if ct >= 10:
                            continue
                        src_post, src_r = dstT[:, g, o0:o0 + 512], dstT
                    else:
                        po = post[npost % 3]
                        tr.op("act", lambda e, po=po, c_t=c_t, ct=ct, n=n: e.activation(
                            out=po[:, 0:n], in_=c_t[:, 0:n], func=AF.Silu, bias=cb[:, ct:ct + 1]),
                            reads=[c_t, cb], writes=[po])
                        src_post, src_r = po[:, 0:n], po
                    ntl = n // 128
                    t_t = pt[npost % 2]
                    ts_t = tst[npost % 3]
                    for i in range(ntl):
                        tr.op("pe", lambda e, i=i, t_t=t_t, src_post=src_post: e.transpose(
                            out=t_t[:, i * 128:(i + 1) * 128], in_=src_post[:, i * 128:(i + 1) * 128], identity=c["ident_b"][:]),
                            reads=[src_r, c["ident_b"]], writes=[t_t])
                    tr.op("dve", lambda e, t_t=t_t, ts_t=ts_t, ntl=ntl: e.tensor_copy(
                        out=ts_t[:, 0:ntl, :], in_=t_t[:, 0:ntl * 128].rearrange("p (a b) -> p a b", b=128)),
                        reads=[t_t], writes=[ts_t])
                    tile0 = 0 if blk == 0 else T_OWN0 + (blk - 1) * 4
                    if ct < 8:
                        dst_d, dst_r = xtok_v[:, tile0:tile0 + ntl, ct * 128:(ct + 1) * 128], c["x_tok"]
                    else:
                        dst_d, dst_r = btok_v[:, tile0:tile0 + ntl, (ct - 8) * 128:(ct - 7) * 128], c["B_tok"]
                    tr.dma("sp", tsem[npost % 3], out=dst_d, in_=ts_t[:, 0:ntl, :], reads=[ts_t], writes=[])
                    c.setdefault("scr_ev", []).append(ts_t)
                    npost += 1
            self.conv_store_tiles = tst
            self.tap("BT", c["BT"][:].rearrange("p g t -> p (g t)"), [128, 2 * NOWN], BF16, [c["BT"]])
            self.tap("CT", c["CT"][:].rearrange("p g t -> p (g t)"), [128, 2 * NOWN], BF16, [c["CT"]])
            for e in Tracker.ENG:
                tr.wait_all(e, tst)
            self.barrier_release(rel)

    def stage_gla(self, st):
        tr, c, I = self.tr, self.c, self.I
        self.fence()
        c["oB"] = self.scratch("oB", [NOWN, 1024], F32)
        c["yx"] = self.scratch("yx", [NOWN, 2048], BF16)
        oB_v = c["oB"].t.rearrange("(n p) c -> n p c", p=128)
        yx_v = c["yx"].t.rearrange("(n p) c -> n p c", p=128)
        w_in_v = I["w_in"].t.rearrange("(kc p) n -> p kc n", p=128)
        LNQ = math.log(128.0 ** -0.5)
        with ExitStack() as s2:
            wg = self.sb(s2, "wgla", [128, 8, 3072], BF16)
            wgs = [Res("wgla%d" % i) for i in range(6)]
            wgg = self.sb(s2, "wgg", [128, 8, 32], BF16)
            wup = self.sb(s2, "wup", [17, 2, 512], F32)
            gn = self.sb(s2, "gn_bc", [128, 256], F32)
            c_one = self.sb(s2, "c_one", [128, 1], F32)
            c_lnq = self.sb(s2, "c_lnq", [128, 1], F32)
            c_eps = self.sb(s2, "c_eps", [128, 1], F32)
            negcol = self.sb(s2, "negcol", [128, 2], F32)
            Tm = [self.sb(s2, "TmA", [128, 128], F32), self.sb(s2, "TmB", [128, 128], F32)]
            S = [self.sb(s2, "S_A", [128, 4, 256], F32), self.sb(s2, "S_B", [128, 4, 256], F32)]
            Sbf = self.sb(s2, "Sbf", [128, 4, 256], BF16)
            g_aug = self.sb(s2, "g_aug", [32, 128], F32)
            v_bf = self.sb(s2, "v_bf", [128, 1024], BF16)
            lap = self.sb(s2, "lap", [128, 512], F32)
            e1 = lap
            Einv = self.sb(s2, "Einv", [128, 512], F32)
            Eq = self.sb(s2, "Eq", [128, 512], F32)
            kt_ = self.sb(s2, "kt_", [128, 512], BF16)
            qt_ = self.sb(s2, "qt_", [128, 512], BF16)
            kqT = self.sb(s2, "kqT", [128, 8, 128], BF16)
            PT = self.sb(s2, "PT", [128, 4, 128], BF16)
            dcol = self.sb(s2, "dcol", [128, 4], F32)
            silr = self.sb(s2, "silr", [128, 1024], F32)
            o_sb = self.sb(s2, "o_sb", [128, 1024], F32)
            oB_sb = [self.sb(s2, "oB_sb%d" % i, [128, 1024], F32) for i in range(2)]
            oBs = [self.dsem() for _ in range(2)]
            ost = [self.sb(s2, "ost%d" % i, [128, 1024], F32) for i in range(2)]
            osts = [self.dsem() for _ in range(2)]
            yst = [self.sb(s2, "yst%d" % i, [128, 1024], BF16) for i in range(2)]
            ysts = [self.dsem() for _ in range(2)]
            ss4 = self.sb(s2, "ss4", [128, 4], F32)
            rs4 = self.sb(s2, "rs4", [128, 4], F32)
            junk = self.sb(s2, "junkg", [128, 256], BF16)
            t1 = o_sb
            pK = self.ps(s2, "pK"); pQ = self.ps(s2, "pQ")
            pV = [self.ps(s2, "pV0"), self.ps(s2, "pV1")]
            pO = [self.ps(s2, "pO0"), self.ps(s2, "pO1")]
            pL = self.ps(s2, "pL")
            pT = self.ps(s2, "pT", [128, 1024], BF16)
            rel = [wg, wgg, wup, gn, c_one, c_lnq, c_eps, negcol, Tm[0], Tm[1], S[0], S[1], Sbf, g_aug, v_bf, lap, Einv, Eq, kt_, qt_,
                   kqT, PT, dcol, silr, o_sb, ss4, rs4, junk, pK, pQ, pL, pT] + pV + pO + oB_sb + ost + yst + wgs
            d0 = self.dsem(9)
            for i in range(6):
                tr.dma("pool", d0, out=wg[:, :, i * 512:(i + 1) * 512], in_=w_in_v[:, :, i * 512:(i + 1) * 512], writes=[wgs[i]])
            tr.dma("pool", d0, out=wgg[:], in_=w_in_v[:, :, OFF_G:OFF_G + 32], writes=[wgg])
            tr.dma("sp", d0, out=wup[:], in_=I["w_up_aug"].t.rearrange("d k n -> k d n"), writes=[wup])
            tr.dma("sp", d0, out=gn[:], in_=I["gla_norm"].t.partition_broadcast(128), writes=[gn])
            tr.op("pool", lambda e: e.memset(c_one[:], 1.0), writes=[c_one])
            tr.op("pool", lambda e: e.memset(c_lnq[:], LNQ), writes=[c_lnq])
            tr.op("pool", lambda e: e.memset(c_eps[:], EPS), writes=[c_eps])
            tr.op("pool", lambda e: e.memset(negcol[:], -1.0 / 16.0), writes=[negcol])
            tr.op("pool", lambda e: e.tensor_scalar(out=Tm[0][:], in0=c["tri_le"][:], scalar1=-1.0 / 16.0, scalar2=None, op0=ALU.mult), reads=[c["tri_le"]], writes=[Tm[0]])
            tr.op("pool", lambda e: e.tensor_scalar(out=Tm[1][:], in0=c["tri_ge"][:], scalar1=-1.0 / 16.0, scalar2=None, op0=ALU.mult), reads=[c["tri_ge"]], writes=[Tm[1]])
            tr.op("pool", lambda e: e.memset(g_aug[:], 1.0), writes=[g_aug])
            for dd in range(2):
                tr.op("pool", lambda e, dd=dd: e.memset(S[dd][:], 0.0), writes=[S[dd]])
            masks = [c["tri_le"], c["tri_ge"]]

            def mm_tok(ps_t, t, c0, n, wres):
                for kc in range(8):
                    tr.op("pe", lambda e, kc=kc: e.matmul(out=ps_t[:, 0:n], lhsT=c["hT"][:, kc, t * 128:(t + 1) * 128], rhs=wg[:, kc, c0:c0 + n],
                                                         start=(kc == 0), stop=(kc == 7)), reads=[c["hT_r"][t]] + wres, writes=[ps_t])

            def gla_tile(t, dd, full, sweepA, own_idx):
                Sd = S[dd]
                mm_tok(pK, t, 512, 512, [wgs[1]])
                mm_tok(pV[0], t, 1024, 512, [wgs[2]])
                mm_tok(pV[1], t, 1536, 512, [wgs[3]])
                for kc in range(8):
                    tr.op("pe", lambda e, kc=kc: e.matmul(out=pT[0:16, 0:256].bitcast(F32) if False else pL[0:16, 0:128], lhsT=wgg[:, kc, dd * 16:(dd + 1) * 16],
                                                         rhs=c["hT"][:, kc, t * 128:(t + 1) * 128], start=(kc == 0), stop=(kc == 7)),
                          reads=[c["hT_r"][t], wgg], writes=[pL])
                tr.op("act", lambda e: e.activation(out=g_aug[0:16, :], in_=pL[0:16, 0:128], func=AF.Copy), reads=[pL], writes=[g_aug])
                tr.op("act", lambda e: e.activation(out=v_bf[:, 0:512], in_=pV[0][:], func=AF.Copy), reads=[pV[0]], writes=[v_bf])
                tr.op("act", lambda e: e.activation(out=v_bf[:, 512:1024], in_=pV[1][:], func=AF.Copy), reads=[pV[1]], writes=[v_bf])
                if full:
                    mm_tok(pQ, t, 0, 512, [wgs[0]])
                tr.op("pe", lambda e: e.matmul(out=pL[:, 0:512], lhsT=g_aug[0:17, :], rhs=wup[:, dd, :], start=True, stop=True), reads=[g_aug, wup], writes=[pL])
                tr.op("act", lambda e: e.activation(out=e1[:], in_=pL[:, 0:512], func=AF.Exp, scale=-1.0), reads=[pL], writes=[e1])
                tr.op("act", lambda e: e.activation(out=lap[:], in_=e1[:], func=AF.Ln, bias=c_one[:]), reads=[e1, c_one], writes=[lap])
                tr.op("pe", lambda e: e.matmul(out=pL[:, 0:512], lhsT=Tm[dd][:], rhs=lap[:], start=True, stop=True), reads=[Tm[dd], lap], writes=[pL])
                tr.op("act", lambda e: e.activation(out=Einv[:], in_=pL[:, 0:512], func=AF.Exp, scale=-1.0), reads=[pL], writes=[Einv])
                if full:
                    tr.op("act", lambda e: e.activation(out=Eq[:], in_=pL[:, 0:512], func=AF.Exp, bias=c_lnq[:]), reads=[pL, c_lnq], writes=[Eq])
                tr.op("dve", lambda e: e.tensor_tensor(out=kt_[:], in0=pK[:], in1=Einv[:], op=ALU.mult), reads=[pK, Einv], writes=[kt_])
                for h in range(4):
                    tr.op("pe", lambda e, h=h: e.matmul(out=pL[:, 2 * h:2 * h + 2], lhsT=lap[:, h * 128:(h + 1) * 128], rhs=negcol[:], start=True, stop=True),
                          reads=[lap, negcol], writes=[pL])
                tr.op("act", lambda e: e.activation(out=dcol[:], in_=pL[:, 0:8:2], func=AF.Exp), reads=[pL], writes=[dcol])
                if full:
                    tr.op("dve", lambda e: e.tensor_tensor(out=qt_[:], in0=pQ[:], in1=Eq[:], op=ALU.mult), reads=[pQ, Eq], writes=[qt_])
                    for h in range(4):
                        tr.op("pe", lambda e, h=h: e.transpose(out=pT[:, h * 128:(h + 1) * 128], in_=kt_[:, h * 128:(h + 1) * 128], identity=c["ident_b"][:]),
                              reads=[kt_, c["ident_b"]], writes=[pT])
                    for h in range(4):
                        tr.op("pe", lambda e, h=h: e.transpose(out=pT[:, (4 + h) * 128:(5 + h) * 128], in_=qt_[:, h * 128:(h + 1) * 128], identity=c["ident_b"][:]),
                              reads=[qt_, c["ident_b"]], writes=[pT])
                    tr.op("act", lambda e: e.activation(out=kqT[:].rearrange("p a b -> p (a b)"), in_=pT[:], func=AF.Copy), reads=[pT], writes=[kqT])
                    for h in range(4):
                        tr.op("pe", lambda e, h=h: e.matmul(out=pK[:, h * 128:(h + 1) * 128], lhsT=kqT[:, h, :], rhs=kqT[:, 4 + h, :], start=True, stop=True),
                              reads=[kqT], writes=[pK])
                    tr.op("dve", lambda e: e.tensor_tensor(out=PT[:], in0=pK[:].rearrange("p (h i) -> p h i", h=4),
                                                          in1=masks[dd][:].unsqueeze(1).to_broadcast([128, 4, 128]), op=ALU.mult),
                          reads=[pK, masks[dd]], writes=[PT])
                    tr.op("act", lambda e: e.activation(out=Sbf[:].rearrange("p a b -> p (a b)"), in_=Sd[:].rearrange("p a b -> p (a b)"), func=AF.Copy), reads=[Sd], writes=[Sbf])
                    for h in range(4):
                        po = pO[h // 2]
                        cs = (h % 2) * 256
                        tr.op("pe", lambda e, h=h, po=po, cs=cs: e.matmul(out=po[:, cs:cs + 256], lhsT=PT[:, h, :], rhs=v_bf[:, h * 256:(h + 1) * 256], start=True, stop=False),
                              reads=[PT, v_bf], writes=[po])
                        tr.op("pe", lambda e, h=h, po=po, cs=cs: e.matmul(out=po[:, cs:cs + 256], lhsT=kqT[:, 4 + h, :], rhs=Sbf[:, h, :], start=False, stop=True),
                              reads=[kqT, Sbf], writes=[po])
                for h in range(4):
                    pv = pV[h // 2]
                    cs = (h % 2) * 256
                    tr.op("pe", lambda e, h=h, pv=pv, cs=cs: e.matmul(out=pv[:, cs:cs + 256], lhsT=kt_[:, h * 128:(h + 1) * 128], rhs=v_bf[:, h * 256:(h + 1) * 256], start=True, stop=True),
                          reads=[kt_, v_bf], writes=[pv])
                for h in range(4):
                    pv = pV[h // 2]
                    cs = (h % 2) * 256
                    tr.op("pool", lambda e, h=h: e.tensor_scalar(out=Sd[:, h, :], in0=Sd[:, h, :], scalar1=dcol[:, h:h + 1], scalar2=None, op0=ALU.mult), reads=[Sd, dcol], writes=[Sd])
                    tr.op("dve", lambda e, h=h, pv=pv, cs=cs: e.scalar_tensor_tensor(out=Sd[:, h, :], in0=pv[:, cs:cs + 256], scalar=dcol[:, h:h + 1], in1=Sd[:, h, :], op0=ALU.mult, op1=ALU.add),
                          reads=[pv, dcol, Sd], writes=[Sd])
                if not full:
                    return
                if not sweepA:
                    os_ = ost[own_idx % 2]
                    tr.op("act", lambda e: e.activation(out=os_[:, 0:512], in_=pO[0][:], func=AF.Copy), reads=[pO[0]], writes=[os_])
                    tr.op("dve", lambda e: e.tensor_copy(out=os_[:, 512:1024], in_=pO[1][:]), reads=[pO[1]], writes=[os_])
                    tr.dma("sp", osts[own_idx % 2], out=oB_v[own_idx], in_=os_[:], reads=[os_], writes=[])
                    return
                ob = oB_sb[own_idx % 2]
                tr.dma("sp", oBs[own_idx % 2], out=ob[:], in_=oB_v[own_idx], writes=[ob])
                mm_tok(pQ, t, 2048, 512, [wgs[4]])
                tr.op("act", lambda e: e.activation(out=silr[:, 0:512], in_=pQ[:], func=AF.Silu), reads=[pQ], writes=[silr])
                mm_tok(pQ, t, 2560, 512, [wgs[5]])
                tr.op("act", lambda e: e.activation(out=silr[:, 512:1024], in_=pQ[:], func=AF.Silu), reads=[pQ], writes=[silr])
                tr.op("pool", lambda e: e.tensor_tensor(out=silr[:].rearrange("p (h v) -> p h v", h=4), in0=silr[:].rearrange("p (h v) -> p h v", h=4),
                                                       in1=gn[:].unsqueeze(1).to_broadcast([128, 4, 256]), op=ALU.mult), reads=[silr, gn], writes=[silr])
                for hh in range(2):
                    tr.op("dve", lambda e, hh=hh: e.tensor_tensor(out=o_sb[:, hh * 512:(hh + 1) * 512], in0=pO[hh][:], in1=ob[:, hh * 512:(hh + 1) * 512], op=ALU.add),
                          reads=[pO[hh], ob], writes=[o_sb])
                for h in range(4):
                    tr.op("act", lambda e, h=h: e.activation(out=junk[:], in_=o_sb[:, h * 256:(h + 1) * 256], func=AF.Square, accum_out=ss4[:, h:h + 1]),
                          reads=[o_sb], writes=[junk, ss4])
                tr.op("act", lambda e: e.activation(out=rs4[:], in_=ss4[:], func=AF.Ln, scale=1.0 / 256.0, bias=c_eps[:]), reads=[ss4, c_eps], writes=[rs4])
                tr.op("act", lambda e: e.activation(out=rs4[:], in_=rs4[:], func=AF.Exp, scale=-0.5), reads=[rs4], writes=[rs4])
                tr.op("dve", lambda e: e.tensor_tensor(out=t1[:].rearrange("p (h v) -> p h v", h=4), in0=o_sb[:].rearrange("p (h v) -> p h v", h=4),
                                                      in1=rs4[:].unsqueeze(2).to_broadcast([128, 4, 256]), op=ALU.mult), reads=[o_sb, rs4], writes=[t1])
                ys = yst[own_idx % 2]
                tr.op("pool", lambda e: e.tensor_tensor(out=ys[:], in0=t1[:], in1=silr[:], op=ALU.mult), reads=[t1, silr], writes=[ys])
                tr.dma("sp", ysts[own_idx % 2], out=yx_v[own_idx][:, 0:1024], in_=ys[:], reads=[ys], writes=[])

            for t in (1, 0):
                gla_tile(t, 1, False, False, None)
            self.tap("gS_B", S[1][:].rearrange("p a b -> p (a b)"), [128, 1024], F32, [S[1]])
            for t in range(NT - 1, T_OTH0 - 1, -1):
                gla_tile(t, 1, False, False, None)
            for t in range(T_OTH0 - 1, T_OWN0 - 1, -1):
                gla_tile(t, 1, True, False, t - T_OWN0)
            for e in Tracker.ENG:
                tr.wait_all(e, ost)
            for t in (0, 1):
                gla_tile(t, 0, False, True, None)
            self.tap("gS_A", S[0][:].rearrange("p a b -> p (a b)"), [128, 1024], F32, [S[0]])
            for t in range(T_OWN0, T_OTH0):
                gla_tile(t, 0, True, True, t - T_OWN0)
            for e in Tracker.ENG:
                tr.wait_all(e, yst)
            self.barrier_release(rel)

    def stage_ssd(self, st):
        tr, c, I = self.tr, self.c, self.I
        self.fence()
        c["yB"] = self.scratch("yB", [NOWN, 1024], F32)
        yB_v = c["yB"].t.rearrange("(n p) c -> n p c", p=128)
        yx_v = c["yx"].t.rearrange("(n p) c -> n p c", p=128)
        xtok_v = c["x_tok"].t.rearrange("(n p) c -> n p c", p=128)
        btok_v = c["B_tok"].t.rearrange("(n p) c -> n p c", p=128)
        w_in_v = I["w_in"].t.rearrange("(kc p) n -> p kc n", p=128)
        BT, CT = c["BT"], c["CT"]
        with ExitStack() as s2:
            wz = self.sb(s2, "wz", [128, 8, 1024], BF16)
            wdt = self.sb(s2, "wdt", [128, 8, 32], BF16)
            Abc = self.sb(s2, "Abc", [128, 32], F32)
            dtb = self.sb(s2, "dtb", [128, 32], F32)
            Dsk = self.sb(s2, "Dsk", [128, 16], F32)
            snb = self.sb(s2, "snb", [128, 1024], F32)
            c_one = self.sb(s2, "c_one2", [128, 1], F32)
            c_eps = self.sb(s2, "c_eps2", [128, 1], F32)
            ST = [self.sb(s2, "ST_A", [128, 2, 512], F32), self.sb(s2, "ST_B", [128, 2, 512], F32)]
            STbf = self.sb(s2, "STbf", [128, 2, 512], BF16)
            xt = [self.sb(s2, "xt%d" % i, [128, 1024], BF16) for i in range(2)]
            bt = [self.sb(s2, "bt%d" % i, [128, 256], BF16) for i in range(2)]
            xts = [self.dsem() for _ in range(2)]
            dt_ = self.sb(s2, "dt_", [128, 16], F32)
            dtA = self.sb(s2, "dtA", [128, 16], F32)
            acs = self.sb(s2, "acs", [128, 16], F32)
            ea = self.sb(s2, "ea", [128, 16], F32)
            dend = self.sb(s2, "dend", [128, 16], F32)
            dtot = self.sb(s2, "dtot", [128, 16], F32)
            R1 = self.sb(s2, "R1", [128, 16, 128], F32)
            E = self.sb(s2, "E", [128, 16, 128], BF16)
            M = self.sb(s2, "M", [128, 16, 128], BF16)
            CBm = self.sb(s2, "CBm", [128, 2, 128], F32)
            xdt = self.sb(s2, "xdt", [128, 1024], BF16)
            xdd = self.sb(s2, "xdd", [128, 1024], BF16)
            silz = self.sb(s2, "silz", [128, 1024], F32)
            y_sb = self.sb(s2, "y_sb", [128, 1024], F32)
            tmp = self.sb(s2, "ytmp", [128, 1024], F32)
            yB_sb = [self.sb(s2, "yB_sb%d" % i, [128, 1024], F32) for i in range(2)]
            yBs = [self.dsem() for _ in range(2)]
            yst = [self.sb(s2, "ysst%d" % i, [128, 1024], F32) for i in range(2)]
            ysts = [self.dsem() for _ in range(2)]
            yxs = [self.sb(s2, "yxs%d" % i, [128, 1024], BF16) for i in range(2)]
            yxss = [self.dsem() for _ in range(2)]
            ss2 = self.sb(s2, "ss2", [128, 2], F32)
            rs2 = self.sb(s2, "rs2", [128, 2], F32)
            junk = self.sb(s2, "junks", [128, 512], BF16)
            pS = self.ps(s2, "pS")
            pD = [self.ps(s2, "pD%d" % i) for i in range(4)]
            pCB = self.ps(s2, "pCB")
            pY = [self.ps(s2, "pY%d" % i) for i in range(2)]
            rel = [wz, wdt, Abc, dtb, Dsk, snb, c_one, c_eps, ST[0], ST[1], STbf, dt_, dtA, acs, ea, dend, dtot, R1, E, M, CBm, xdt, xdd,
                   silz, y_sb, tmp, ss2, rs2, junk, pS, pCB] + xt + bt + yB_sb + yst + yxs + pD + pY
            d0 = self.dsem(6)
            tr.dma("pool", d0, out=wz[:], in_=w_in_v[:, :, OFF_Z:OFF_Z + 1024], writes=[wz])
            tr.dma("pool", d0, out=wdt[:], in_=w_in_v[:, :, OFF_DT:OFF_DT + 32], writes=[wdt])
            tr.dma("sp", d0, out=Abc[:], in_=I["a_log"].t.partition_broadcast(128), writes=[Abc])
            tr.dma("sp", d0, out=dtb[:], in_=I["dt_bias"].t.partition_broadcast(128), writes=[dtb])
            tr.dma("sp", d0, out=Dsk[:], in_=I["ssd_d"].t.partition_broadcast(128), writes=[Dsk])
            tr.dma("sp", d0, out=snb[:], in_=I["ssd_norm"].t.partition_broadcast(128), writes=[snb])
            tr.op("pool", lambda e: e.memset(c_one[:], 1.0), writes=[c_one])
            tr.op("pool", lambda e: e.memset(c_eps[:], EPS), writes=[c_eps])
            tr.op("act", lambda e: e.activation(out=Abc[:], in_=Abc[:], func=AF.Exp), reads=[Abc], writes=[Abc])
            tr.op("dve", lambda e: e.tensor_scalar(out=Abc[:], in0=Abc[:], scalar1=-1.0, scalar2=None, op0=ALU.mult), reads=[Abc], writes=[Abc])
            for dd in range(2):
                tr.op("pool", lambda e, dd=dd: e.memset(ST[dd][:], 0.0), writes=[ST[dd]])
            Lm = [c["tri_gt"], c["tri_lt"]]
            Tc = [c["tri_le"], c["tri_ge"]]
            cnt = [0]

            def ssd_tile(t, dd, full, sweepA, own_idx):
                STd = ST[dd]
                k = cnt[0] % 2
                cnt[0] += 1
                x_t, b_t = xt[k], bt[k]
                tr.regroup(xts[k], 2)
                tr.dma("sp", xts[k], out=x_t[:], in_=xtok_v[t], writes=[x_t])
                tr.dma("sp", xts[k], out=b_t[:], in_=btok_v[t], writes=[b_t])
                hres = [c["hT_r"][t]]
                lhs = lambda kc: c["hT"][:, kc, t * 128:(t + 1) * 128]
                if full and sweepA:
                    for hh in range(2):
                        for kc in range(8):
                            tr.op("pe", lambda e, kc=kc, hh=hh: e.matmul(out=pD[2 + hh][:], lhsT=lhs(kc), rhs=wz[:, kc, hh * 512:(hh + 1) * 512], start=(kc == 0), stop=(kc == 7)),
                                  reads=hres + [wz], writes=[pD[2 + hh]])
                        tr.op("act", lambda e, hh=hh: e.activation(out=silz[:, hh * 512:(hh + 1) * 512], in_=pD[2 + hh][:], func=AF.Silu), reads=[pD[2 + hh]], writes=[silz])
                for kc in range(8):
                    tr.op("pe", lambda e, kc=kc: e.matmul(out=pS[:, 0:16], lhsT=lhs(kc), rhs=wdt[:, kc, dd * 16:(dd + 1) * 16], start=(kc == 0), stop=(kc == 7)),
                          reads=hres + [wdt], writes=[pS])
                tr.op("dve", lambda e: e.tensor_tensor(out=dt_[:], in0=pS[:, 0:16], in1=dtb[:, dd * 16:(dd + 1) * 16], op=ALU.add), reads=[pS, dtb], writes=[dt_])
                tr.op("act", lambda e: e.activation(out=dt_[:], in_=dt_[:], func=AF.Exp), reads=[dt_], writes=[dt_])
                tr.op("act", lambda e: e.activation(out=dt_[:], in_=dt_[:], func=AF.Ln, bias=c_one[:]), reads=[dt_, c_one], writes=[dt_])
                tr.op("dve", lambda e: e.tensor_tensor(out=dtA[:], in0=dt_[:], in1=Abc[:, dd * 16:(dd + 1) * 16], op=ALU.mult), reads=[dt_, Abc], writes=[dtA])
                tr.op("pe", lambda e: e.matmul(out=pS[:, 16:32], lhsT=Tc[dd][:], rhs=dtA[:], start=True, stop=True), reads=[Tc[dd], dtA], writes=[pS])
                tr.op("pe", lambda e: e.matmul(out=pS[:, 32:48], lhsT=c["ones_f"][:], rhs=dtA[:], start=True, stop=True), reads=[c["ones_f"], dtA], writes=[pS])
                tr.op("dve", lambda e: e.tensor_copy(out=acs[:], in_=pS[:, 16:32]), reads=[pS], writes=[acs])
                tr.op("dve", lambda e: e.tensor_tensor(out=dend[:], in0=pS[:, 32:48], in1=acs[:], op=ALU.subtract), reads=[pS, acs], writes=[dend])
                tr.op("act", lambda e: e.activation(out=dend[:], in_=dend[:], func=AF.Exp), reads=[dend], writes=[dend])
                tr.op("act", lambda e: e.activation(out=dtot[:], in_=pS[:, 32:48], func=AF.Exp), reads=[pS], writes=[dtot])
                tr.op("dve", lambda e: e.tensor_tensor(out=xdt[:].rearrange("p (h q) -> p h q", h=16), in0=x_t[:].rearrange("p (h q) -> p h q", h=16),
                                                      in1=dt_[:].unsqueeze(2).to_broadcast([128, 16, 64]), op=ALU.mult), reads=[x_t, dt_], writes=[xdt])
                if full:
                    tok0 = (t - T_OWN0) * 128
                    tr.op("act", lambda e: e.activation(out=ea[:], in_=acs[:], func=AF.Exp), reads=[acs], writes=[ea])
                    tr.op("act", lambda e: e.activation(out=STbf[:].rearrange("p a b -> p (a b)"), in_=STd[:].rearrange("p a b -> p (a b)"), func=AF.Copy), reads=[STd], writes=[STbf])
                    tr.op("dve", lambda e: e.tensor_tensor(out=R1[:], in0=Tc[dd][:].unsqueeze(1).to_broadcast([128, 16, 128]),
                                                          in1=dtA[:].unsqueeze(2).to_broadcast([128, 16, 128]), op=ALU.mult), reads=[Tc[dd], dtA], writes=[R1])
                    for b4 in range(4):
                        tr.op("pe", lambda e, b4=b4: e.matmul(out=pD[b4][:], lhsT=Lm[dd][:], rhs=R1[:, 4 * b4:4 * b4 + 4, :], start=True, stop=True),
                              reads=[Lm[dd], R1], writes=[pD[b4]])
                        tr.op("act", lambda e, b4=b4: e.activation(out=E[:, 4 * b4:4 * b4 + 4, :], in_=pD[b4][:].rearrange("p (h i) -> p h i", h=4), func=AF.Exp), reads=[pD[b4]], writes=[E])
                    for g in range(2):
                        tr.op("pe", lambda e, g=g: e.matmul(out=pCB[:, g * 128:(g + 1) * 128], lhsT=BT[:, g, tok0:tok0 + 128], rhs=CT[:, g, tok0:tok0 + 128], start=True, stop=True),
                              reads=[BT, CT], writes=[pCB])
                    tr.op("dve", lambda e: e.tensor_tensor(out=CBm[:], in0=pCB[:, 0:256].rearrange("p (g i) -> p g i", g=2),
                                                          in1=Tc[dd][:].unsqueeze(1).to_broadcast([128, 2, 128]), op=ALU.mult), reads=[pCB, Tc[dd]], writes=[CBm])
                    for g in range(2):
                        eng = "dve" if g == 0 else "pool"
                        tr.op(eng, lambda e, g=g: e.tensor_tensor(out=M[:, g * 8:(g + 1) * 8, :], in0=E[:, g * 8:(g + 1) * 8, :],
                                                                  in1=CBm[:, g:g + 1, :].to_broadcast([128, 8, 128]), op=ALU.mult), reads=[E, CBm], writes=[M])
                    for h in range(16):
                        py = pY[h // 8]
                        cs = (h % 8) * 64
                        tr.op("pe", lambda e, h=h, py=py, cs=cs: e.matmul(out=py[:, cs:cs + 64], lhsT=M[:, h, :], rhs=xdt[:, h * 64:(h + 1) * 64], start=True, stop=True),
                              reads=[M, xdt], writes=[py])
                    for g in range(2):
                        tr.op("pe", lambda e, g=g: e.matmul(out=pD[g][:], lhsT=CT[:, g, tok0:tok0 + 128], rhs=STbf[:, g, :], start=True, stop=True),
                              reads=[CT, STbf], writes=[pD[g]])
                        tr.op("dve", lambda e, g=g: e.tensor_tensor(out=tmp[:, g * 512:(g + 1) * 512].rearrange("p (h q) -> p h q", h=8), in0=pD[g][:].rearrange("p (h q) -> p h q", h=8),
                                                                    in1=ea[:, g * 8:(g + 1) * 8].unsqueeze(2).to_broadcast([128, 8, 64]), op=ALU.mult), reads=[pD[g], ea], writes=[tmp])
                        tr.op("dve", lambda e, g=g: e.tensor_tensor(out=y_sb[:, g * 512:(g + 1) * 512], in0=pY[g][:], in1=tmp[:, g * 512:(g + 1) * 512], op=ALU.add),
                              reads=[pY[g], tmp], writes=[y_sb])
                tr.op("pool", lambda e: e.tensor_tensor(out=xdd[:].rearrange("p (h q) -> p h q", h=16), in0=xdt[:].rearrange("p (h q) -> p h q", h=16),
                                                       in1=dend[:].unsqueeze(2).to_broadcast([128, 16, 64]), op=ALU.mult), reads=[xdt, dend], writes=[xdd])
                for g in range(2):
                    tr.op("pe", lambda e, g=g: e.matmul(out=pD[2 + g][:], lhsT=b_t[:, g * 128:(g + 1) * 128], rhs=xdd[:, g * 512:(g + 1) * 512], start=True, stop=True),
                          reads=[b_t, xdd], writes=[pD[2 + g]])
                    tr.op("pool", lambda e, g=g: e.tensor_tensor(out=STd[:, g, :].rearrange("p (h q) -> p h q", h=8), in0=STd[:, g, :].rearrange("p (h q) -> p h q", h=8),
                                                                 in1=dtot[:, g * 8:(g + 1) * 8].unsqueeze(2).to_broadcast([128, 8, 64]), op=ALU.mult), reads=[STd, dtot], writes=[STd])
                    tr.op("dve", lambda e, g=g: e.tensor_tensor(out=STd[:, g, :], in0=pD[2 + g][:], in1=STd[:, g, :], op=ALU.add), reads=[pD[2 + g], STd], writes=[STd])
                if not full:
                    return
                if not sweepA:
                    ys = yst[own_idx % 2]
                    tr.op("act", lambda e: e.activation(out=ys[:], in_=y_sb[:], func=AF.Copy), reads=[y_sb], writes=[ys])
                    tr.dma("sp", ysts[own_idx % 2], out=yB_v[own_idx], in_=ys[:], reads=[ys], writes=[])
                    return
                yb = yB_sb[own_idx % 2]
                tr.dma("sp", yBs[own_idx % 2], out=yb[:], in_=yB_v[own_idx], writes=[yb])
                tr.op("dve", lambda e: e.tensor_tensor(out=y_sb[:], in0=y_sb[:], in1=yb[:], op=ALU.add), reads=[y_sb, yb], writes=[y_sb])
                tr.op("pool", lambda e: e.tensor_tensor(out=tmp[:].rearrange("p (h q) -> p h q", h=16), in0=x_t[:].rearrange("p (h q) -> p h q", h=16),
                                                       in1=Dsk[:].unsqueeze(2).to_broadcast([128, 16, 64]), op=ALU.mult), reads=[x_t, Dsk], writes=[tmp])
                tr.op("dve", lambda e: e.tensor_tensor(out=y_sb[:], in0=y_sb[:], in1=tmp[:], op=ALU.add), reads=[y_sb, tmp], writes=[y_sb])
                tr.op("dve", lambda e: e.tensor_tensor(out=y_sb[:], in0=y_sb[:], in1=silz[:], op=ALU.mult), reads=[y_sb, silz], writes=[y_sb])
                for g in range(2):
                    tr.op("act", lambda e, g=g: e.activation(out=junk[:], in_=y_sb[:, g * 512:(g + 1) * 512], func=AF.Square, accum_out=ss2[:, g:g + 1]), reads=[y_sb], writes=[junk, ss2])
                tr.op("act", lambda e: e.activation(out=rs2[:], in_=ss2[:], func=AF.Ln, scale=1.0 / 512.0, bias=c_eps[:]), reads=[ss2, c_eps], writes=[rs2])
                tr.op("act", lambda e: e.activation(out=rs2[:], in_=rs2[:], func=AF.Exp, scale=-0.5), reads=[rs2], writes=[rs2])
                tr.op("dve", lambda e: e.tensor_tensor(out=y_sb[:].rearrange("p (g q) -> p g q", g=2), in0=y_sb[:].rearrange("p (g q) -> p g q", g=2),
                                                      in1=rs2[:].unsqueeze(2).to_broadcast([128, 2, 512]), op=ALU.mult), reads=[y_sb, rs2], writes=[y_sb])
                yo = yxs[own_idx % 2]
                tr.op("pool", lambda e: e.tensor_tensor(out=yo[:], in0=y_sb[:], in1=snb[:], op=ALU.mult), reads=[y_sb, snb], writes=[yo])
                tr.dma("sp", yxss[own_idx % 2], out=yx_v[own_idx][:, 1024:2048], in_=yo[:], reads=[yo], writes=[])

            for t in (1, 0):
                ssd_tile(t, 1, False, False, None)
            self.tap("sS_B", ST[1][:].rearrange("p a b -> p (a b)"), [128, 1024], F32, [ST[1]])
            for t in range(NT - 1, T_OTH0 - 1, -1):
                ssd_tile(t, 1, False, False, None)
            for t in range(T_OTH0 - 1, T_OWN0 - 1, -1):
                ssd_tile(t, 1, True, False, t - T_OWN0)
            for e in Tracker.ENG:
                tr.wait_all(e, yst)
            for t in (0, 1):
                ssd_tile(t, 0, False, True, None)
            self.tap("sS_A", ST[0][:].rearrange("p a b -> p (a b)"), [128, 1024], F32, [ST[0]])
            for t in range(T_OWN0, T_OTH0):
                ssd_tile(t, 0, True, True, t - T_OWN0)
            for e in Tracker.ENG:
                tr.wait_all(e, yxs)
            self.barrier_release(rel)

    def stage_post(self, st):
        tr, c, I = self.tr, self.c, self.I
        self.fence()
        c["h_lat"] = self.sb(st, "h_lat", [128, 16, D], F32)
        c["h_r"] = [Res("h_lat%d" % i) for i in range(16)]
        c["h2T"] = self.sb(st, "h2T", [128, 8, NOWN], BF16)
        c["h2_r"] = [Res("h2T%d" % i) for i in range(16)]
        c["comb"] = self.sb(st, "comb", [128, 16, 32], F32)
        yx_v = c["yx"].t.rearrange("(n p) c -> n p c", p=128)
        h_lat, h2T = c["h_lat"], c["h2T"]
        with ExitStack() as s2:
            wo = self.sb(s2, "wo", [128, 16, D], BF16)
            wr = self.sb(s2, "wr", [128, 8, 36], F32)
            brr = self.sb(s2, "brr", [1, 36], F32)
            c_eps = self.sb(s2, "c_eps3", [128, 1], F32)
            lg = self.sb(s2, "lg", [128, 16, 36], F32)
            yxt = [self.sb(s2, "yxt%d" % i, [128, 2048], BF16) for i in range(2)]
            yxs = [self.dsem() for _ in range(2)]
            xr = [self.sb(s2, "xr2_%d" % i, [128, D], F32) for i in range(2)]
            xrs = [self.dsem() for _ in range(2)]
            yxT = self.sb(s2, "yxT", [128, 16, 128], BF16)
            tmp = self.sb(s2, "ptmp", [128, D], F32)
            hn = self.sb(s2, "hn", [128, D], F32)
            h2f = self.sb(s2, "h2f", [128, 8, 128], F32)
            ss = self.sb(s2, "pss", [128, 1], F32)
            rs = self.sb(s2, "prs", [128, 1], F32)
            junk = self.sb(s2, "pjunk", [128, D], BF16)
            pT = [self.ps(s2, "ppT%d" % i, [128, 1024], BF16) for i in range(2)]
            pO = [self.ps(s2, "ppO%d" % i) for i in range(2)]
            pF = [self.ps(s2, "ppF%d" % i) for i in range(2)]
            pR = self.ps(s2, "ppR")
            rel = [wo, wr, brr, c_eps, lg, yxT, tmp, hn, h2f, ss, rs, junk, pR] + yxt + xr + pT + pO + pF
            import os
            if os.environ.get("BISECT3") == "2":
                self.tap("g1x", c["g1_bc"][:], [128, D], F32, [c["g1_bc"]])
                return
            w_out_v = I["w_out"].t.rearrange("(kc p) n -> p kc n", p=128)
            d0 = self.dsem(2)
            wst = self.sb(s2, "wst", [128, 4, D], F32)
            rel.append(wst)
            wsts = self.dsem()
            for q in range(4):
                tr.dma("sp", wsts, out=wst[:], in_=w_out_v[:, q * 4:(q + 1) * 4, :], writes=[wst])
                tr.op("pool", lambda e, q=q: e.tensor_copy(out=wo[:, q * 4:(q + 1) * 4, :], in_=wst[:]), reads=[wst], writes=[wo])
            tr.dma("sp", d0, out=wr[:].rearrange("p a b -> p (a b)"), in_=I["w_router"].t, writes=[wr])
            tr.dma("sp", d0, out=brr[:], in_=I["b_router"].t, writes=[brr])
            tr.op("pool", lambda e: e.memset(c_eps[:], EPS), writes=[c_eps])
            import os
            B3 = os.environ.get("BISECT3", "")
            for i in range(16 if B3 != "1" else 0):
                y_t, x_t = yxt[i % 2], xr[i % 2]
                tr.dma("sp", yxs[i % 2], out=y_t[:], in_=yx_v[i], writes=[y_t])
                tr.dma("sp", xrs[i % 2], out=x_t[:], in_=I["xs"].t[NCTX + i * 128: NCTX + (i + 1) * 128, :], writes=[x_t])
                for kc in range(16):
                    tr.op("pe", lambda e, kc=kc: e.transpose(out=pT[kc // 8][:, (kc % 8) * 128:(kc % 8 + 1) * 128], in_=y_t[:, kc * 128:(kc + 1) * 128], identity=c["ident_b"][:]),
                          reads=[y_t, c["ident_b"]], writes=[pT[kc // 8]])
                tr.op("act", lambda e: e.activation(out=yxT[:, 0:8, :].rearrange("p a b -> p (a b)"), in_=pT[0][:], func=AF.Copy), reads=[pT[0]], writes=[yxT])
                tr.op("dve", lambda e: e.tensor_copy(out=yxT[:, 8:16, :].rearrange("p a b -> p (a b)"), in_=pT[1][:]), reads=[pT[1]], writes=[yxT])
                hl = h_lat[:, i, :]
                for hh in range(2):
                    for kc in range(16):
                        tr.op("pe", lambda e, kc=kc, hh=hh: e.matmul(out=pO[hh][:], lhsT=yxT[:, kc, :], rhs=wo[:, kc, hh * 512:(hh + 1) * 512], start=(kc == 0), stop=(kc == 15)),
                              reads=[yxT, wo], writes=[pO[hh]])
                    tr.op("dve", lambda e, hh=hh: e.tensor_tensor(out=tmp[:, hh * 512:(hh + 1) * 512], in0=pO[hh][:], in1=c["g1_bc"][:, hh * 512:(hh + 1) * 512], op=ALU.mult),
                          reads=[pO[hh], c["g1_bc"]], writes=[tmp])
                tr.op("pool", lambda e: e.tensor_tensor(out=hl, in0=tmp[:], in1=x_t[:], op=ALU.add), reads=[tmp, x_t], writes=[c["h_r"][i]])
                import os
                if os.environ.get("BISECT2") == "b":
                    continue
                tr.op("act", lambda e: e.activation(out=junk[:], in_=hl, func=AF.Square, accum_out=ss[:]), reads=[c["h_r"][i]], writes=[junk, ss])
                tr.op("act", lambda e: e.activation(out=rs[:], in_=ss[:], func=AF.Ln, scale=1.0 / D, bias=c_eps[:]), reads=[ss, c_eps], writes=[rs])
                tr.op("act", lambda e: e.activation(out=rs[:], in_=rs[:], func=AF.Exp, scale=-0.5), reads=[rs], writes=[rs])
                tr.op("dve", lambda e: e.tensor_scalar(out=hn[:], in0=hl, scalar1=rs[:], scalar2=None, op0=ALU.mult), reads=[c["h_r"][i], rs], writes=[hn])
                for kc in range(8):
                    tr.op("pe", lambda e, kc=kc: e.transpose(out=pF[kc // 4][:, (kc % 4) * 128:(kc % 4 + 1) * 128], in_=hn[:, kc * 128:(kc + 1) * 128], identity=c["ident_f"][:]),
                          reads=[hn, c["ident_f"]], writes=[pF[kc // 4]])
                for q in range(2):
                    tr.op("dve", lambda e, q=q: e.tensor_tensor(out=h2f[:, q * 4:(q + 1) * 4, :], in0=pF[q][:].rearrange("p (k t) -> p k t", k=4),
                                                               in1=c["s2"][:, q * 4:(q + 1) * 4].unsqueeze(2).to_broadcast([128, 4, 128]), op=ALU.mult), reads=[pF[q], c["s2"]], writes=[h2f])
                tr.op("pool", lambda e: e.tensor_tensor(out=h2f[:], in0=h2f[:], in1=c["b2"][:].unsqueeze(2).to_broadcast([128, 8, 128]), op=ALU.add), reads=[h2f, c["b2"]], writes=[h2f])
                tr.op("act", lambda e: e.activation(out=h2T[:, :, i * 128:(i + 1) * 128], in_=h2f[:], func=AF.Copy), reads=[h2f], writes=[c["h2_r"][i]])
                if os.environ.get("BISECT2") == "c":
                    continue
                for kc in range(8):
                    tr.op("pe", lambda e, kc=kc: e.matmul(out=pR[:, 0:36], lhsT=h2f[:, kc, :], rhs=wr[:, kc, :], start=(kc == 0), stop=False), reads=[h2f, wr], writes=[pR])
                tr.op("pe", lambda e: e.matmul(out=pR[:, 0:36], lhsT=c["ones_f"][0:1, :], rhs=brr[0:1, :], start=False, stop=True), reads=[c["ones_f"], brr], writes=[pR])
                tr.op("dve", lambda e: e.tensor_copy(out=lg[:, i, :], in_=pR[:, 0:36]), reads=[pR], writes=[lg])
            self.tap("lg", lg[:].rearrange("p a b -> p (a b)"), [128, 16 * 36], F32, [lg])
            self.tap("h_lat", h_lat[:].rearrange("p a b -> p (a b)"), [128, 16 * D], F32, c["h_r"])
            import os
            if os.environ.get("BISECT") == "a":
                self.tap("wo", wo[:, 0, :], [128, D], BF16, [wo])
                self.tap("wrx", wr[:].rearrange("p a b -> p (a b)"), [128, 288], F32, [wr])
                self.barrier_release(rel)
                return
            def T(name, shape):
                t_ = self.sb(s2, name, shape, F32)
                rel.append(t_)
                return t_
            gmax = T("gmax", [128, 16]); mg = T("mg", [128, 16, 4]); eg = T("eg", [128, 16, 4]); gsum = T("gsum", [128, 16]); pg = T("pg", [128, 16])
            t48 = T("t48", [128, 16, 4, 8]); ein = T("ein", [128, 16, 8]); m1 = T("m1", [128, 16]); k1 = T("k1", [128, 16, 8]); e2 = T("e2", [128, 16, 8])
            m2 = T("m2", [128, 16]); k2 = T("k2", [128, 16, 8]); dd_ = T("dd_", [128, 16]); w1 = T("w1", [128, 16]); w2 = T("w2", [128, 16]); cw8 = T("cw8", [128, 16, 8])
            gl = lg[:, :, 0:4]
            el = lg[:, :, 4:36].rearrange("p t (g x) -> p t g x", g=4)
            V = lambda fn, r, w: tr.op("dve", fn, reads=r, writes=w)
            V(lambda e: e.tensor_reduce(out=gmax[:], in_=gl, axis=AX.X, op=ALU.max), [lg], [gmax])
            V(lambda e: e.tensor_tensor(out=mg[:], in0=gl, in1=gmax[:].unsqueeze(2).to_broadcast([128, 16, 4]), op=ALU.is_equal), [lg, gmax], [mg])
            V(lambda e: e.tensor_tensor(out=eg[:], in0=gl, in1=gmax[:].unsqueeze(2).to_broadcast([128, 16, 4]), op=ALU.subtract), [lg, gmax], [eg])
            tr.op("act", lambda e: e.activation(out=eg[:], in_=eg[:], func=AF.Exp), reads=[eg], writes=[eg])
            V(lambda e: e.tensor_reduce(out=gsum[:], in_=eg[:], axis=AX.X, op=ALU.add), [eg], [gsum])
            V(lambda e: e.reciprocal(out=pg[:], in_=gsum[:]), [gsum], [pg])
            V(lambda e: e.tensor_tensor(out=t48[:], in0=el, in1=mg[:].unsqueeze(3).to_broadcast([128, 16, 4, 8]), op=ALU.mult), [lg, mg], [t48])
            V(lambda e: e.tensor_reduce(out=ein[:], in_=t48[:].rearrange("p t g x -> p t x g"), axis=AX.X, op=ALU.add), [t48], [ein])
            V(lambda e: e.tensor_reduce(out=m1[:], in_=ein[:], axis=AX.X, op=ALU.max), [ein], [m1])
            V(lambda e: e.tensor_tensor(out=k1[:], in0=ein[:], in1=m1[:].unsqueeze(2).to_broadcast([128, 16, 8]), op=ALU.is_equal), [ein, m1], [k1])
            V(lambda e: e.scalar_tensor_tensor(out=e2[:], in0=k1[:], scalar=-1.0e30, in1=ein[:], op0=ALU.mult, op1=ALU.add), [k1, ein], [e2])
            V(lambda e: e.tensor_reduce(out=m2[:], in_=e2[:], axis=AX.X, op=ALU.max), [e2], [m2])
            V(lambda e: e.tensor_tensor(out=k2[:], in0=e2[:], in1=m2[:].unsqueeze(2).to_broadcast([128, 16, 8]), op=ALU.is_equal), [e2, m2], [k2])
            V(lambda e: e.tensor_tensor(out=dd_[:], in0=m2[:], in1=m1[:], op=ALU.subtract), [m1, m2], [dd_])
            tr.op("act", lambda e: e.activation(out=dd_[:], in_=dd_[:], func=AF.Exp), reads=[dd_], writes=[dd_])
            V(lambda e: e.tensor_scalar(out=w1[:], in0=dd_[:], scalar1=1.0, scalar2=None, op0=ALU.add), [dd_], [w1])
            V(lambda e: e.reciprocal(out=w1[:], in_=w1[:]), [w1], [w1])
            V(lambda e: e.tensor_tensor(out=w2[:], in0=dd_[:], in1=w1[:], op=ALU.mult), [dd_, w1], [w2])
            V(lambda e: e.tensor_tensor(out=w1[:], in0=w1[:], in1=pg[:], op=ALU.mult), [w1, pg], [w1])
            V(lambda e: e.tensor_tensor(out=w2[:], in0=w2[:], in1=pg[:], op=ALU.mult), [w2, pg], [w2])
            V(lambda e: e.tensor_tensor(out=k1[:], in0=k1[:], in1=w1[:].unsqueeze(2).to_broadcast([128, 16, 8]), op=ALU.mult), [k1, w1], [k1])
            V(lambda e: e.tensor_tensor(out=k2[:], in0=k2[:], in1=w2[:].unsqueeze(2).to_broadcast([128, 16, 8]), op=ALU.mult), [k2, w2], [k2])
            V(lambda e: e.tensor_tensor(out=cw8[:], in0=k1[:], in1=k2[:], op=ALU.add), [k1, k2], [cw8])
            V(lambda e: e.tensor_tensor(out=c["comb"][:].rearrange("p t (g x) -> p t g x", g=4), in0=mg[:].unsqueeze(3).to_broadcast([128, 16, 4, 8]),
                                        in1=cw8[:].unsqueeze(2).to_broadcast([128, 16, 4, 8]), op=ALU.mult), [mg, cw8], [c["comb"]])
            self.tap("comb", c["comb"][:].rearrange("p a b -> p (a b)"), [128, 512], F32, [c["comb"]])
            self.barrier_release(rel)

    def stage_moe(self, st):
        tr, c, I = self.tr, self.c, self.I
        self.fence()
        h_lat, h2T, comb = c["h_lat"], c["h2T"], c["comb"]
        with ExitStack() as s2:
            wgt = [self.sb(s2, "mwg%d" % i, [128, 8, DFF], BF16) for i in range(2)]
            wut = [self.sb(s2, "mwu%d" % i, [128, 8, DFF], BF16) for i in range(2)]
            wdt = [self.sb(s2, "mwd%d" % i, [128, 4, D], BF16) for i in range(2)]
            stg = [self.sb(s2, "mstg%d" % i, [128, 8, DFF], F32) for i in range(2)]
            stgs = [self.dsem() for _ in range(2)]
            ns = [0]
            sg_ = [self.sb(s2, "msg%d" % i, [128, 512], F32) for i in range(2)]
            heT = [self.sb(s2, "heT%d" % i, [128, 4, 512], BF16) for i in range(2)]
            pG = [self.ps(s2, "mpG%d" % i) for i in range(2)]
            pU = [self.ps(s2, "mpU%d" % i) for i in range(2)]
            pDn = [self.ps(s2, "mpD%d" % i) for i in range(4)]
            rel = wgt + wut + wdt + sg_ + heT + pG + pU + pDn + stg
            nb = 0
            for ex in range(NEXP):
                k = ex % 2
                wg_e, wu_e, wd_e = wgt[k], wut[k], wdt[k]
                for (dst, src) in ((wg_e, I["w_gate"].t[ex].rearrange("(kc p) n -> p kc n", p=128)), (wu_e, I["w_up"].t[ex].rearrange("(kc p) n -> p kc n", p=128))):
                    sg_t, sg_s = stg[ns[0] % 2], stgs[ns[0] % 2]
                    ns[0] += 1
                    tr.dma("sp", sg_s, out=sg_t[:], in_=src, writes=[sg_t])
                    tr.op("pool", lambda e, dst=dst, sg_t=sg_t: e.tensor_copy(out=dst[:], in_=sg_t[:]), reads=[sg_t], writes=[dst])
                sg_t, sg_s = stg[ns[0] % 2], stgs[ns[0] % 2]
                ns[0] += 1
                sv = sg_t[:].rearrange("p a b -> p (a b)").rearrange("p (f n) -> p f n", f=4)
                tr.dma("sp", sg_s, out=sv, in_=I["w_down"].t[ex].rearrange("(fc p) n -> p fc n", p=128), writes=[sg_t])
                tr.op("pool", lambda e, wd_e=wd_e, sv=sv: e.tensor_tensor(out=wd_e[:], in0=sv, in1=c["g2_bc"][:].unsqueeze(1).to_broadcast([128, 4, D]), op=ALU.mult),
                      reads=[sg_t, c["g2_bc"]], writes=[wd_e])
                for j in range(4):
                    he = heT[nb % 2]
                    nb += 1
                    hres = [c["h2_r"][j * 4 + q] for q in range(4)]
                    for fc in range(4):
                        g_p, u_p, sg = pG[fc % 2], pU[fc % 2], sg_[fc % 2]
                        for kc in range(8):
                            tr.op("pe", lambda e, kc=kc, fc=fc, g_p=g_p: e.matmul(out=g_p[:], lhsT=wg_e[:, kc, fc * 128:(fc + 1) * 128], rhs=h2T[:, kc, j * 512:(j + 1) * 512],
                                                                               start=(kc == 0), stop=(kc == 7)), reads=[wg_e] + hres, writes=[g_p])
                        for kc in range(8):
                            tr.op("pe", lambda e, kc=kc, fc=fc, u_p=u_p: e.matmul(out=u_p[:], lhsT=wu_e[:, kc, fc * 128:(fc + 1) * 128], rhs=h2T[:, kc, j * 512:(j + 1) * 512],
                                                                               start=(kc == 0), stop=(kc == 7)), reads=[wu_e] + hres, writes=[u_p])
                        tr.op("act", lambda e, g_p=g_p, sg=sg: e.activation(out=sg[:], in_=g_p[:], func=AF.Silu), reads=[g_p], writes=[sg])
                        tr.op("dve", lambda e, u_p=u_p, sg=sg, fc=fc, he=he: e.tensor_tensor(out=he[:, fc, :], in0=u_p[:], in1=sg[:], op=ALU.mult), reads=[u_p, sg], writes=[he])
                    for tt in range(4):
                        ti = j * 4 + tt
                        for hh in range(2):
                            d_p = pDn[(tt * 2 + hh) % 4]
                            for fc in range(4):
                                tr.op("pe", lambda e, fc=fc, d_p=d_p, tt=tt, hh=hh, he=he: e.matmul(out=d_p[:], lhsT=he[:, fc, tt * 128:(tt + 1) * 128], rhs=wd_e[:, fc, hh * 512:(hh + 1) * 512],
                                                                                              start=(fc == 0), stop=(fc == 3)), reads=[he, wd_e], writes=[d_p])
                            tr.op("dve", lambda e, d_p=d_p, ti=ti, hh=hh, ex=ex: e.scalar_tensor_tensor(
                                out=h_lat[:, ti, hh * 512:(hh + 1) * 512], in0=d_p[:], scalar=comb[:, ti, ex:ex + 1], in1=h_lat[:, ti, hh * 512:(hh + 1) * 512], op0=ALU.mult, op1=ALU.add),
                                reads=[d_p, comb, c["h_r"][ti]], writes=[c["h_r"][ti]])
            self.tap("h_fin", h_lat[:].rearrange("p a b -> p (a b)"), [128, 16 * D], F32, c["h_r"])
            self.barrier_release(rel)

    def stage_final(self, st):
        tr, c, I = self.tr, self.c, self.I
        self.fence()
        h_lat = c["h_lat"]
        out_v = self.out.t.rearrange("(n p) c -> n p c", p=128)
        with ExitStack() as s2:
            fn = self.sb(s2, "fn_bc", [128, D], F32)
            c_eps = self.sb(s2, "c_eps4", [128, 1], F32)
            junk = self.sb(s2, "fjunk", [128, D], BF16)
            ss = [self.sb(s2, "fss%d" % i, [128, 1], F32) for i in range(2)]
            rs = [self.sb(s2, "frs%d" % i, [128, 1], F32) for i in range(2)]
            ob = [self.sb(s2, "fob%d" % i, [128, D], F32) for i in range(2)]
            obs = [self.dsem() for _ in range(2)]
            tr.dma("sp", self.dsem(), out=fn[:], in_=I["final_norm"].t.partition_broadcast(128), writes=[fn])
            tr.op("pool", lambda e: e.memset(c_eps[:], EPS), writes=[c_eps])
            for i in range(16):
                hl = h_lat[:, i, :]
                s_, r_, o_ = ss[i % 2], rs[i % 2], ob[i % 2]
                tr.op("act", lambda e, s_=s_, hl=hl: e.activation(out=junk[:], in_=hl, func=AF.Square, accum_out=s_[:]), reads=[c["h_r"][i]], writes=[junk, s_])
                tr.op("act", lambda e, s_=s_, r_=r_: e.activation(out=r_[:], in_=s_[:], func=AF.Ln, scale=1.0 / D, bias=c_eps[:]), reads=[s_, c_eps], writes=[r_])
                tr.op("act", lambda e, r_=r_: e.activation(out=r_[:], in_=r_[:], func=AF.Exp, scale=-0.5), reads=[r_], writes=[r_])
                tr.op("dve", lambda e, r_=r_, o_=o_, hl=hl: e.scalar_tensor_tensor(out=o_[:], in0=hl, scalar=r_[:], in1=fn[:], op0=ALU.mult, op1=ALU.mult),
                      reads=[c["h_r"][i], r_, fn], writes=[o_])
                tr.dma("sp", obs[i % 2], out=out_v[i], in_=o_[:], reads=[o_], writes=[])
            self.final += ob


def prep_core(inp, b, hf):
    L = 0
    rev = hf == 1
    x, ctx = inp["x"][b], inp["ctx"][b]
    if not rev:
        ctx_a, own, oth = ctx, x[0:2048], x[2048:4096]
        dA, dB = 0, 1
    else:
        ctx_a, own, oth = ctx[::-1], x[2048:4096][::-1], x[0:2048][::-1]
        dA, dB = 1, 0
    m = {}
    m["xs"] = np.ascontiguousarray(np.concatenate([ctx_a, own, oth], axis=0), dtype=np.float32)
    cT = np.stack([inp["c"][b].reshape(8, 128).T, inp["c_ctx"].reshape(8, 128).T], axis=2).reshape(128, 16)
    m["cT"] = np.ascontiguousarray(cT, dtype=np.float32)
    m["w_ada"] = np.ascontiguousarray(inp["w_ada"][L])
    m["b_ada"] = np.ascontiguousarray(inp["b_ada"][L].reshape(1, -1))
    m["norm_mix_fm"] = np.ascontiguousarray(inp["norm_mix"][L].reshape(8, 128).T)
    m["norm_ffn_fm"] = np.ascontiguousarray(inp["norm_ffn"][L].reshape(8, 128).T)
    w_in = inp["w_in"][L]
    if rev:
        w_in = np.concatenate([w_in[:, :OFF_G], w_in[:, OFF_G + 16:OFF_G + 32], w_in[:, OFF_G:OFF_G + 16],
                               w_in[:, OFF_Z:OFF_DT], w_in[:, OFF_DT + 16:OFF_DT + 32], w_in[:, OFF_DT:OFF_DT + 16]], axis=1)
    m["w_in"] = np.ascontiguousarray(w_in)
    wu, gb = inp["gla_w_up"][L], inp["gla_b"][L]
    m["w_up_aug"] = np.ascontiguousarray(np.stack([np.concatenate([wu[dA], gb[dA][None, :]], axis=0),
                                                   np.concatenate([wu[dB], gb[dB][None, :]], axis=0)], axis=0))
    m["gla_norm"] = np.ascontiguousarray(inp["gla_norm"][L].reshape(1, -1))
    m["ssd_norm"] = np.ascontiguousarray(inp["ssd_norm"][L].reshape(1, -1))
    m["final_norm"] = np.ascontiguousarray(inp["final_norm"].reshape(1, -1))
    cw = inp["ssd_conv_w"][L]
    if rev:
        cw = cw[::-1, ::-1, :]
    m["conv_w_fm"] = np.ascontiguousarray(cw.reshape(9, 12, 128).transpose(2, 1, 0))
    m["conv_b_fm"] = np.ascontiguousarray(inp["ssd_conv_b"][L].reshape(12, 128).T)
    m["dt_bias"] = np.ascontiguousarray(np.concatenate([inp["ssd_dt_bias"][L][dA], inp["ssd_dt_bias"][L][dB]]).reshape(1, 32))
    m["a_log"] = np.ascontiguousarray(np.concatenate([inp["ssd_a_log"][L][dA], inp["ssd_a_log"][L][dB]]).reshape(1, 32))
    m["ssd_d"] = np.ascontiguousarray(inp["ssd_d"][L].reshape(1, 16))
    m["w_out"] = np.ascontiguousarray(inp["w_out"][L])
    wrt = np.concatenate([inp["router_group_w"][L], inp["router_expert_w"][L]], axis=1)
    m["w_router"] = np.ascontiguousarray(wrt.reshape(8, 128, 36).transpose(1, 0, 2).reshape(128, 8 * 36))
    m["b_router"] = np.ascontiguousarray(np.concatenate([inp["router_group_b"][L], inp["router_expert_b"][L]]).reshape(1, 36))
    m["w_gate"] = np.ascontiguousarray(inp["expert_w_gate"][L])
    m["w_up"] = np.ascontiguousarray(inp["expert_w_up"][L])
    m["w_down"] = np.ascontiguousarray(inp["expert_w_down"][L])
    return {k: np.asarray(v, dtype=np.float32) for k, v in m.items()}


def run(inputs, debug=None, stop_after=None, cores=8):
    bld = Builder(debug=debug, stop_after=stop_after)
    nc = bld.build()
    in_maps = [prep_core(inputs, i // 2, i % 2) for i in range(cores)]
    res = run_bass_kernel_spmd(nc, in_maps, core_ids=list(range(cores)))
    return res, bld


def kernel(**inputs):
    inputs = {k: np.asarray(v) for k, v in inputs.items()}
    res, _ = run(inputs)
    out = np.empty((4, 4096, D), dtype=np.float32)
    for i in range(8):
        b, hf = i // 2, i % 2
        o = np.asarray(res.results[i]["out"], dtype=np.float32)
        if hf == 0:
            out[b, 0:2048] = o
        else:
            out[b, 2048:4096] = o[::-1]
    return out
```

```python
import math
from contextlib import ExitStack

import numpy as np
import concourse.bass as bass
import concourse.mybir as mybir
from concourse.bass_utils import run_bass_kernel_spmd

F32 = mybir.dt.float32
BF16 = mybir.dt.bfloat16
AF = mybir.ActivationFunctionType
ALU = mybir.AluOpType
AX = mybir.AxisListType

D = 1024
NCTX, NOWN, NOTH = 256, 2048, 2048
TOK = NCTX + NOWN + NOTH
NT = TOK // 128
T_CTX0, T_OWN0, T_OTH0 = 0, 2, 18
EPS = 1e-6
IN_W = 5696
OFF_K, OFF_V, OFF_R, OFF_G, OFF_Z, OFF_XBC, OFF_DT = 512, 1024, 2048, 3072, 3104, 4128, 5664
NEXP, DFF = 32, 512


class Res:
    __slots__ = ("name", "lw", "rd")

    def __init__(self, name=""):
        self.name = name
        self.lw = None
        self.rd = {}


class Tile:
    def __init__(self, t, name):
        self.t = t
        self.r = Res(name)

    def __getitem__(self, idx):
        return self.t[idx]


class Tracker:
    ENG = ("pe", "act", "dve", "pool", "sp")
    CH = 2000

    def __init__(self, nc, sems, dma_sems, same_engine_sync=True):
        self.nc = nc
        self.eng = {"pe": nc.tensor, "act": nc.scalar, "dve": nc.vector, "pool": nc.gpsimd, "sp": nc.sync}
        self.cnt = {e: 0 for e in self.ENG}
        self.waited = {e: {} for e in self.ENG}
        self.sems = {e: [sems[e]] for e in sems}
        self.free_dma = list(dma_sems)
        self.same = same_engine_sync
        self.ninst = 0

    def new_dma_sem(self, group=0):
        d = self.free_dma.pop()
        self._uid = getattr(self, "_uid", 0) + 1
        d = d if isinstance(d, list) else [d, 0, 0, 0, "dma%d" % self._uid]
        if group:
            d[2] = group
            d[3] = d[1] + 16 * group
        return d

    @staticmethod
    def regroup(d, n):
        assert d[2] == 0
        d[2] = n
        d[3] = d[1] + 16 * n

    def release_dma_sem(self, d):
        self.free_dma.append(d)

    def _wait(self, e, ev):
        if ev is None:
            return
        if ev[0] == "dma":
            _, s, v, key = ev
            if self.waited[e].get(key, 0) >= v:
                return
            self.waited[e][key] = v
            self.eng[e].wait_ge(s, v)
        else:
            pe, n = ev
            if pe == e and (not self.same or e in ("pe", "sp")):
                return
            if self.waited[e].get(pe, 0) >= n:
                return
            self.waited[e][pe] = n
            self.eng[e].wait_ge(self.sems[pe][(n - 1) // self.CH], (n - 1) % self.CH + 1)

    def _deps(self, e, reads, writes):
        for r in reads:
            self._wait(e, r.lw)
        for w in writes:
            self._wait(e, w.lw)
            for ev in w.rd.values():
                self._wait(e, ev)

    @staticmethod
    def _note_read(r, ev):
        key = ev[3] if ev[0] == "dma" else ev[0]
        old = r.rd.get(key)
        if old is None or (old[2] if old[0] == "dma" else old[1]) < (ev[2] if ev[0] == "dma" else ev[1]):
            r.rd[key] = ev

    def op(self, e, fn, reads=(), writes=()):
        reads = [x.r if isinstance(x, Tile) else x for x in reads]
        writes = [x.r if isinstance(x, Tile) else x for x in writes]
        self._deps(e, reads, writes)
        self.cnt[e] += 1
        ev = (e, self.cnt[e])
        k = (self.cnt[e] - 1) // self.CH
        if k >= len(self.sems[e]):
            self.sems[e].append(self.free_dma.pop(0))
        fn(self.eng[e]).then_inc(self.sems[e][k], 1)
        self.ninst += 1
        for r in reads:
            self._note_read(r, ev)
        for w in writes:
            w.lw = ev
            w.rd = {}
        return ev

    def dma(self, e, dsem, out, in_, reads=(), writes=()):
        reads = [x.r if isinstance(x, Tile) else x for x in reads]
        writes = [x.r if isinstance(x, Tile) else x for x in writes]
        self._deps(e, reads, writes)
        if dsem[2] == 0:
            dsem[2] = 1
            dsem[3] = dsem[1] + 16
        dsem[1] += 16
        dsem[2] -= 1
        ev = ("dma", dsem[0], dsem[3], dsem[4])
        self.eng[e].dma_start(out=out, in_=in_).then_inc(dsem[0], 16)
        self.ninst += 1
        for r in reads:
            self._note_read(r, ev)
        for w in writes:
            w.lw = ev
            w.rd = {}
        return ev

    def wait_all(self, e, resources):
        for r in resources:
            r = r.r if isinstance(r, Tile) else r
            self._wait(e, r.lw)
            for ev in r.rd.values():
                self._wait(e, ev)


class Builder:
    def __init__(self, debug=None, stop_after=None):
        self.debug = debug or ()
        self.stop_after = stop_after
        self.nc = bass.Bass("TRN2", target_bir_lowering=False)
        self.dbg_out = {}

    def sb(self, st, name, shape, dt):
        self._uid = getattr(self, "_uid", 0) + 1
        return Tile(st.enter_context(self.nc.sbuf_tensor("sb%d_%s" % (self._uid, name), list(shape), dt)), name)

    def ps(self, st, name, shape=(128, 512), dt=F32):
        self._uid = getattr(self, "_uid", 0) + 1
        return Tile(st.enter_context(self.nc.psum_tensor("ps%d_%s" % (self._uid, name), list(shape), dt)), name)

    def dram_in(self, name, shape, dt=F32):
        return Tile(self.nc.dram_tensor(name, list(shape), dt, kind="ExternalInput").ap(), name)

    def dram_out(self, name, shape, dt=F32):
        return Tile(self.nc.dram_tensor(name, list(shape), dt, kind="ExternalOutput").ap(), name)

    def dram_scr(self, name, shape, dt):
        return Tile(self.nc.dram_tensor(name, list(shape), dt, kind="Internal").ap(), name)

    def dsem(self, group=0):
        return self.tr.new_dma_sem(group)

    def build(self):
        nc = self.nc
        I = {}
        I["xs"] = self.dram_in("xs", [TOK, D])
        I["cT"] = self.dram_in("cT", [128, 16])
        I["w_ada"] = self.dram_in("w_ada", [D, 6 * D])
        I["b_ada"] = self.dram_in("b_ada", [1, 6 * D])
        I["norm_mix_fm"] = self.dram_in("norm_mix_fm", [128, 8])
        I["norm_ffn_fm"] = self.dram_in("norm_ffn_fm", [128, 8])
        I["w_in"] = self.dram_in("w_in", [D, IN_W])
        I["w_up_aug"] = self.dram_in("w_up_aug", [2, 17, 512])
        I["gla_norm"] = self.dram_in("gla_norm", [1, 256])
        I["ssd_norm"] = self.dram_in("ssd_norm", [1, 1024])
        I["final_norm"] = self.dram_in("final_norm", [1, 1024])
        I["conv_w_fm"] = self.dram_in("conv_w_fm", [128, 12, 9])
        I["conv_b_fm"] = self.dram_in("conv_b_fm", [128, 12])
        I["dt_bias"] = self.dram_in("dt_bias", [1, 32])
        I["a_log"] = self.dram_in("a_log", [1, 32])
        I["ssd_d"] = self.dram_in("ssd_d", [1, 16])
        I["w_out"] = self.dram_in("w_out", [2048, D])
        I["w_router"] = self.dram_in("w_router", [128, 8 * 36])
        I["b_router"] = self.dram_in("b_router", [1, 36])
        I["w_gate"] = self.dram_in("w_gate", [NEXP, D, DFF])
        I["w_up"] = self.dram_in("w_up", [NEXP, D, DFF])
        I["w_down"] = self.dram_in("w_down", [NEXP, DFF, D])
        self.I = I
        self.out = self.dram_out("out", [NOWN, D])

        with ExitStack() as st:
            sems = {e: st.enter_context(nc.semaphore("s_" + e)) for e in Tracker.ENG}
            dsems = [st.enter_context(nc.semaphore("d%d" % i)) for i in range(90)]
            self.tr = Tracker(nc, sems, dsems)
            self.program(st)
        return nc

    def tap(self, name, tile_ap, shape, dt, reads):
        if name not in self.debug:
            return
        o = self.dram_out("dbg_" + name, shape, dt)
        self.dbg_out[name] = o
        n = shape[1]
        step = 2048
        d = self.dsem(len(range(0, n, step)))
        for c0 in range(0, n, step):
            c1 = min(n, c0 + step)
            self.tr.dma("sp", d, out=o.t[:, c0:c1], in_=tile_ap[:, c0:c1], reads=reads, writes=[o])
        self.final.append(o)

    def program(self, st):
        tr = self.tr
        self.final = []
        self.consts(st)
        self.stage_adaln(st)
        with ExitStack() as mst:
            self.stage_hT(mst)
            if self.stop_after == "hT":
                return self.finish()
            self.stage_gla(mst)
            if self.stop_after == "gla":
                return self.finish()
            self.stage_conv(mst)
            if self.stop_after == "conv":
                return self.finish()
            self.stage_ssd(mst)
            if self.stop_after == "ssd":
                return self.finish()
            self.barrier_release([self.c["hT"], self.c["BT"], self.c["CT"]] + self.c["hT_r"])
        self.stage_post(st)
        if self.stop_after == "post":
            return self.finish()
        self.stage_moe(st)
        if self.stop_after == "moe":
            return self.finish()
        self.stage_final(st)
        return self.finish()

    def finish(self):
        self.tr.wait_all("sp", self.final)

    def consts(self, st):
        tr = self.tr
        c = {}
        self.c = c
        c["ident_f"] = self.sb(st, "ident_f", [128, 128], F32)
        c["ident_b"] = self.sb(st, "ident_b", [128, 128], BF16)
        c["ones_f"] = self.sb(st, "ones_f", [128, 128], F32)
        for nm in ("tri_le", "tri_ge", "tri_gt", "tri_lt"):
            c[nm] = self.sb(st, nm, [128, 128], F32)
        idf = c["ident_f"]
        tr.op("pool", lambda e: e.memset(idf[:], 0.0), writes=[idf])
        tr.op("pool", lambda e: e.affine_select(out=idf[:], in_=idf[:], pattern=[[-1, 128]], compare_op=ALU.not_equal,
                                               fill=1.0, base=0, channel_multiplier=1), reads=[idf], writes=[idf])
        tr.op("pool", lambda e: e.tensor_copy(out=c["ident_b"][:], in_=idf[:]), reads=[idf], writes=[c["ident_b"]])
        tr.op("pool", lambda e: e.memset(c["ones_f"][:], 1.0), writes=[c["ones_f"]])
        specs = {"tri_le": (ALU.is_gt, 0), "tri_ge": (ALU.is_gt, 0), "tri_gt": (ALU.is_gt, 0), "tri_lt": (ALU.is_gt, 0)}
        t = c["tri_le"]
        tr.op("pool", lambda e: e.memset(t[:], 1.0), writes=[t])
        tr.op("pool", lambda e: e.affine_select(out=t[:], in_=t[:], pattern=[[1, 128]], compare_op=ALU.is_ge,
                                               fill=0.0, base=0, channel_multiplier=-1), reads=[t], writes=[t])
        t2 = c["tri_ge"]
        tr.op("pool", lambda e: e.memset(t2[:], 1.0), writes=[t2])
        tr.op("pool", lambda e: e.affine_select(out=t2[:], in_=t2[:], pattern=[[-1, 128]], compare_op=ALU.is_ge,
                                               fill=0.0, base=0, channel_multiplier=1), reads=[t2], writes=[t2])
        t3 = c["tri_gt"]
        tr.op("pool", lambda e: e.memset(t3[:], 1.0), writes=[t3])
        tr.op("pool", lambda e: e.affine_select(out=t3[:], in_=t3[:], pattern=[[-1, 128]], compare_op=ALU.is_gt,
                                               fill=0.0, base=0, channel_multiplier=1), reads=[t3], writes=[t3])
        t4 = c["tri_lt"]
        tr.op("pool", lambda e: e.memset(t4[:], 1.0), writes=[t4])
        tr.op("pool", lambda e: e.affine_select(out=t4[:], in_=t4[:], pattern=[[1, 128]], compare_op=ALU.is_gt,
                                               fill=0.0, base=0, channel_multiplier=-1), reads=[t4], writes=[t4])
        self.tap("tri_le", c["tri_le"][:], [128, 128], F32, [c["tri_le"]])
        self.tap("tri_gt", c["tri_gt"][:], [128, 128], F32, [c["tri_gt"]])

    def stage_adaln(self, st):
        tr, c, I = self.tr, self.c, self.I
        c["mod_fm"] = self.sb(st, "mod_fm", [128, 6, 8, 2], F32)
        c["g1_bc"] = self.sb(st, "g1_bc", [128, D], F32)
        c["g2_bc"] = self.sb(st, "g2_bc", [128, D], F32)
        c["s1"] = self.sb(st, "s1", [128, 8], F32)
        c["s1c"] = self.sb(st, "s1c", [128, 8], F32)
        c["b1"] = self.sb(st, "b1", [128, 8], F32)
        c["b1c"] = self.sb(st, "b1c", [128, 8], F32)
        c["s2"] = self.sb(st, "s2", [128, 8], F32)
        c["b2"] = self.sb(st, "b2", [128, 8], F32)
        with ExitStack() as s2:
            cT = self.sb(s2, "cT", [128, 16], F32)
            scT = self.sb(s2, "scT", [128, 16], F32)
            sc_rep = self.sb(s2, "sc_rep", [128, 8, 128], F32)
            brow = self.sb(s2, "brow", [1, 6 * D], F32)
            nm = self.sb(s2, "nm", [128, 8], F32)
            nf = self.sb(s2, "nf", [128, 8], F32)
            wblk = [self.sb(s2, "wblk%d" % i, [128, 8, D], F32) for i in range(2)]
            wsem = [self.dsem() for _ in range(2)]
            modps = self.ps(s2, "modps", [128, 512], F32)
            gps = [self.ps(s2, "gps%d" % i, [128, 512], F32) for i in range(2)]
            d = self.dsem(4)
            tr.dma("sp", d, out=cT[:], in_=I["cT"].t, writes=[cT])
            tr.dma("sp", d, out=brow[:], in_=I["b_ada"].t, writes=[brow])
            tr.dma("sp", d, out=nm[:], in_=I["norm_mix_fm"].t, writes=[nm])
            tr.dma("sp", d, out=nf[:], in_=I["norm_ffn_fm"].t, writes=[nf])
            tr.op("act", lambda e: e.activation(out=scT[:], in_=cT[:], func=AF.Silu), reads=[cT], writes=[scT])
            tr.op("dve", lambda e: e.tensor_copy(out=sc_rep[:], in_=scT[:].rearrange("p (k j) -> p k j", j=2)[:, :, 0:1].to_broadcast([128, 8, 128])),
                  reads=[scT], writes=[sc_rep])
            w_ada = I["w_ada"].t.rearrange("(kc p) n -> p kc n", p=128)
            mview = modps[:, 0:96].rearrange("p (b f t) -> p b f t", b=6, f=8)
            for blk in range(6):
                wb = wblk[blk % 2]
                tr.dma("sp", wsem[blk % 2], out=wb[:], in_=w_ada[:, :, blk * D:(blk + 1) * D], writes=[wb])
                if blk in (0, 1, 3, 4):
                    for fc in range(8):
                        for kc in range(8):
                            tr.op("pe", lambda e, fc=fc, kc=kc, wb=wb, blk=blk: e.matmul(
                                out=mview[:, blk, fc, :], lhsT=wb[:, kc, fc * 128:(fc + 1) * 128],
                                rhs=scT[:, 2 * kc:2 * kc + 2], start=(kc == 0), stop=False),
                                reads=[wb, scT], writes=[modps])
                        tr.op("pe", lambda e, fc=fc, blk=blk: e.matmul(
                            out=mview[:, blk, fc, :], lhsT=brow[0:1, blk * D + fc * 128: blk * D + (fc + 1) * 128],
                            rhs=c["ones_f"][0:1, 0:2], start=False, stop=True),
                            reads=[brow, c["ones_f"]], writes=[modps])
                else:
                    gdst = c["g1_bc"] if blk == 2 else c["g2_bc"]
                    for hh in range(2):
                        for kc in range(8):
                            tr.op("pe", lambda e, hh=hh, kc=kc, wb=wb: e.matmul(
                                out=gps[hh][:], lhsT=sc_rep[:, kc, :], rhs=wb[:, kc, hh * 512:(hh + 1) * 512],
                                start=(kc == 0), stop=False), reads=[wb, sc_rep], writes=[gps[hh]])
                        tr.op("pe", lambda e, hh=hh, blk=blk: e.matmul(
                            out=gps[hh][:], lhsT=c["ones_f"][0:1, :], rhs=brow[0:1, blk * D + hh * 512: blk * D + (hh + 1) * 512],
                            start=False, stop=True), reads=[brow, c["ones_f"]], writes=[gps[hh]])
                        tr.op("act", lambda e, hh=hh, gdst=gdst: e.activation(out=gdst[:, hh * 512:(hh + 1) * 512], in_=gps[hh][:], func=AF.Copy),
                              reads=[gps[hh]], writes=[gdst])
            mf = c["mod_fm"]
            mflat = mf[:].rearrange("p b f t -> p (b f t)")
            tr.op("dve", lambda e: e.tensor_copy(out=mflat[:, 0:32], in_=modps[:, 0:32]), reads=[modps], writes=[mf])
            tr.op("dve", lambda e: e.tensor_copy(out=mflat[:, 48:80], in_=modps[:, 48:80]), reads=[modps], writes=[mf])
            tr.op("dve", lambda e: e.scalar_tensor_tensor(out=c["s1"][:], in0=mf[:, 1, :, 0], scalar=1.0, in1=nm[:], op0=ALU.add, op1=ALU.mult),
                  reads=[mf, nm], writes=[c["s1"]])
            tr.op("dve", lambda e: e.scalar_tensor_tensor(out=c["s1c"][:], in0=mf[:, 1, :, 1], scalar=1.0, in1=nm[:], op0=ALU.add, op1=ALU.mult),
                  reads=[mf, nm], writes=[c["s1c"]])
            tr.op("dve", lambda e: e.scalar_tensor_tensor(out=c["s2"][:], in0=mf[:, 4, :, 0], scalar=1.0, in1=nf[:], op0=ALU.add, op1=ALU.mult),
                  reads=[mf, nf], writes=[c["s2"]])
            tr.op("dve", lambda e: e.tensor_copy(out=c["b1"][:], in_=mf[:, 0, :, 0]), reads=[mf], writes=[c["b1"]])
            tr.op("dve", lambda e: e.tensor_copy(out=c["b1c"][:], in_=mf[:, 0, :, 1]), reads=[mf], writes=[c["b1c"]])
            tr.op("dve", lambda e: e.tensor_copy(out=c["b2"][:], in_=mf[:, 3, :, 0]), reads=[mf], writes=[c["b2"]])
            self.tap("mod_fm", mf[:].rearrange("p b f t -> p (b f t)"), [128, 96], F32, [mf])
            self.tap("g1_bc", c["g1_bc"][:], [128, D], F32, [c["g1_bc"]])
            self.barrier_release([cT, scT, sc_rep, brow, nm, nf, wblk[0], wblk[1], modps, gps[0], gps[1]])

    def barrier_release(self, tiles):
        self.pending = getattr(self, "pending", [])
        for t in tiles:
            self.pending.append(t.r if isinstance(t, Tile) else t)

    def fence(self):
        pend = getattr(self, "pending", [])
        for e in Tracker.ENG:
            self.tr.wait_all(e, pend)
        self.pending = []

    def stage_hT(self, st):
        tr, c, I = self.tr, self.c, self.I
        self.fence()
        c["hT"] = self.sb(st, "hT", [128, 8, TOK], BF16)
        c["hT_r"] = [Res("hT%d" % t) for t in range(NT)]
        with ExitStack() as s2:
            xr = [self.sb(s2, "xr%d" % i, [128, D], F32) for i in range(3)]
            xsem = [self.dsem() for _ in range(3)]
            junk = self.sb(s2, "junk", [128, D], BF16)
            ss = [self.sb(s2, "ss%d" % i, [128, 1], F32) for i in range(3)]
            rstd = [self.sb(s2, "rstd%d" % i, [128, 1], F32) for i in range(3)]
            xn = [self.sb(s2, "xn%d" % i, [128, D], BF16) for i in range(2)]
            tmp = [self.sb(s2, "tmp%d" % i, [128, 8, 128], F32) for i in range(2)]
            tps = [self.ps(s2, "tps%d" % i, [128, 1024], BF16) for i in range(2)]
            epst = self.sb(s2, "epst", [128, 1], F32)
            tr.op("pool", lambda e: e.memset(epst[:], EPS), writes=[epst])
            rel = xr + ss + rstd + xn + tmp + tps + [junk, epst]
            for t in range(NT):
                x_t, ss_t, rs_t, xn_t, tmp_t, ps_t = xr[t % 3], ss[t % 3], rstd[t % 3], xn[t % 2], tmp[t % 2], tps[t % 2]
                hT_ap = c["hT"][:, :, t * 128:(t + 1) * 128]
                hT_r = c["hT_r"][t]
                isctx = t < T_OWN0
                sc, sh = (c["s1c"], c["b1c"]) if isctx else (c["s1"], c["b1"])
                tr.dma("sp", xsem[t % 3], out=x_t[:], in_=I["xs"].t[t * 128:(t + 1) * 128, :], writes=[x_t])
                tr.op("act", lambda e, x_t=x_t, ss_t=ss_t: e.activation(out=junk[:], in_=x_t[:], func=AF.Square, accum_out=ss_t[:]),
                      reads=[x_t], writes=[junk, ss_t])
                tr.op("act", lambda e, ss_t=ss_t, rs_t=rs_t: e.activation(out=rs_t[:], in_=ss_t[:], func=AF.Ln, scale=1.0 / D, bias=epst[:]),
                      reads=[ss_t, epst], writes=[rs_t])
                tr.op("act", lambda e, rs_t=rs_t: e.activation(out=rs_t[:], in_=rs_t[:], func=AF.Exp, scale=-0.5),
                      reads=[rs_t], writes=[rs_t])
                tr.op("dve", lambda e, x_t=x_t, rs_t=rs_t, xn_t=xn_t: e.tensor_scalar(out=xn_t[:], in0=x_t[:], scalar1=rs_t[:], scalar2=None, op0=ALU.mult),
                      reads=[x_t, rs_t], writes=[xn_t])
                for kc in range(8):
                    tr.op("pe", lambda e, kc=kc, xn_t=xn_t, ps_t=ps_t: e.transpose(out=ps_t[:, kc * 128:(kc + 1) * 128], in_=xn_t[:, kc * 128:(kc + 1) * 128], identity=c["ident_b"][:]),
                          reads=[xn_t, c["ident_b"]], writes=[ps_t])
                tr.op("dve", lambda e, ps_t=ps_t, tmp_t=tmp_t, sc=sc: e.tensor_tensor(
                    out=tmp_t[:], in0=ps_t[:].rearrange("p (k t) -> p k t", k=8), in1=sc[:].unsqueeze(2).to_broadcast([128, 8, 128]), op=ALU.mult),
                    reads=[ps_t, sc], writes=[tmp_t])
                tr.op("pool", lambda e, tmp_t=tmp_t, hT_ap=hT_ap, sh=sh: e.tensor_tensor(
                    out=hT_ap, in0=tmp_t[:], in1=sh[:].unsqueeze(2).to_broadcast([128, 8, 128]), op=ALU.add),
                    reads=[tmp_t, sh], writes=[hT_r])
            for t in (0, 2, 17, 33):
                if ("hT%d" % t) in self.debug:
                    o = self.dram_out("dbg_hT%d" % t, [128, 8, 128], BF16)
                    tr.dma("sp", self.dsem(), out=o.t, in_=c["hT"][:, :, t * 128:(t + 1) * 128], reads=[c["hT_r"][t]], writes=[o])
                    self.final.append(o)
            self.barrier_release(rel)

    def scratch(self, name, shape, dt):
        if name in self.debug:
            o = self.dram_out("dbg_" + name, shape, dt)
            self.final.append(o)
            return o
        return self.dram_scr(name, shape, dt)

    def stage_conv(self, st):
        tr, c, I = self.tr, self.c, self.I
        self.fence()
        c["x_tok"] = self.scratch("x_tok", [TOK, 1024], BF16)
        c["B_tok"] = self.scratch("B_tok", [TOK, 256], BF16)
        c["BT"] = self.sb(st, "BT", [128, 2, NOWN], BF16)
        c["CT"] = self.sb(st, "CT", [128, 2, NOWN], BF16)
        xtok_v = c["x_tok"].t.rearrange("(n p) c -> p n c", p=128)
        btok_v = c["B_tok"].t.rearrange("(n p) c -> p n c", p=128)
        w_in_v = I["w_in"].t.rearrange("(kc p) n -> p kc n", p=128)
        with ExitStack() as s2:
            wx = [self.sb(s2, "wx%d" % i, [128, 8, 128], BF16) for i in range(2)]
            wxs = [self.dsem() for _ in range(2)]
            cw = self.sb(s2, "cw", [128, 12, 9], F32)
            cb = self.sb(s2, "cb", [128, 12], F32)
            diag = [self.sb(s2, "diag%d" % i, [128, 9, 128], BF16) for i in range(2)]
            pre = [self.sb(s2, "pre%d" % i, [128, 66, 66], BF16) for i in range(2)]
            prec = [self.sb(s2, "prec%d" % i, [128, 258], BF16) for i in range(2)]
            post = [self.sb(s2, "post%d" % i, [128, 512], BF16) for i in range(3)]
            tst = [self.sb(s2, "tst%d" % i, [128, 4, 128], BF16) for i in range(3)]
            tsem = [self.dsem() for _ in range(3)]
            pp = [self.ps(s2, "pp%d" % i) for i in range(2)]
            pc = [self.ps(s2, "pc%d" % i) for i in range(2)]
            pt = [self.ps(s2, "pt%d" % i, [128, 1024], BF16) for i in range(2)]
            rel = wx + diag + pre + prec + post + tst + pp + pc + pt + [cw, cb]
            d0 = self.dsem(2)
            tr.dma("sp", d0, out=cw[:], in_=I["conv_w_fm"].t, writes=[cw])
            tr.dma("sp", d0, out=cb[:], in_=I["conv_b_fm"].t, writes=[cb])
            for i in range(2):
                tr.op("pool", lambda e, i=i: e.memset(pre[i][:], 0.0), writes=[pre[i]])
                tr.op("pool", lambda e, i=i: e.memset(prec[i][:], 0.0), writes=[prec[i]])
            nev = 0
            npost = 0
            for ct in range(12):
                w, dg, pr, prc = wx[ct % 2], diag[ct % 2], pre[ct % 2], prec[ct % 2]
                tr.dma("pool", wxs[ct % 2], out=w[:], in_=w_in_v[:, :, OFF_XBC + ct * 128: OFF_XBC + (ct + 1) * 128], writes=[w])
                for tap in range(9):
                    tr.op("pool", lambda e, tap=tap, dg=dg, ct=ct: e.tensor_scalar(out=dg[:, tap, :], in0=c["ident_f"][:], scalar1=cw[:, ct, tap:tap + 1], scalar2=None, op0=ALU.mult),
                          reads=[c["ident_f"], cw], writes=[dg])
                for blk in range(9):
                    p_t = pp[nev % 2]
                    if blk == 0:
                        n, tok0, trs = 256, 0, [0, 1]
                    else:
                        n, tok0 = 512, NCTX + (blk - 1) * 512
                        trs = list(range(T_OWN0 + (blk - 1) * 4, T_OWN0 + blk * 4))
                    for kc in range(8):
                        tr.op("pe", lambda e, kc=kc, p_t=p_t, w=w, n=n, tok0=tok0: e.matmul(
                            out=p_t[:, 0:n], lhsT=w[:, kc, :], rhs=c["hT"][:, kc, tok0:tok0 + n], start=(kc == 0), stop=(kc == 7)),
                            reads=[w] + [c["hT_r"][t] for t in trs], writes=[p_t])
                    if blk == 0:
                        dst = prc[:, 1:257]
                        src = p_t[:, 0:256]
                        wr = prc
                    else:
                        r0 = (blk - 1) * 8
                        dst = pr[:, r0 + 1:r0 + 9, 1:65]
                        src = p_t[:, 0:512].rearrange("p (r q) -> p r q", q=64)
                        wr = pr
                    eng = "act" if nev % 2 == 0 else "dve"
                    if eng == "act":
                        tr.op("act", lambda e, dst=dst, src=src: e.activation(out=dst, in_=src, func=AF.Copy), reads=[p_t], writes=[wr])
                    else:
                        tr.op("dve", lambda e, dst=dst, src=src: e.tensor_copy(out=dst, in_=src), reads=[p_t], writes=[wr])
                    nev += 1
                for blk in range(9):
                    if ct >= 10 and (blk == 0 or blk >= 5):
                        continue
                    c_t = pc[blk % 2]
                    if blk == 0:
                        n = 256
                        for kw in range(3):
                            tr.op("pe", lambda e, kw=kw, c_t=c_t, dg=dg, prc=prc: e.matmul(
                                out=c_t[:, 0:256], lhsT=dg[:, 3 + kw, :], rhs=prc[:, kw:kw + 256], start=(kw == 0), stop=(kw == 2)),
                                reads=[dg, prc], writes=[c_t])
                    else:
                        n = 512
                        r0 = (blk - 1) * 8
                        for tap in range(9):
                            kh, kw = tap // 3, tap % 3
                            tr.op("pe", lambda e, tap=tap, kh=kh, kw=kw, c_t=c_t, dg=dg, pr=pr, r0=r0: e.matmul(
                                out=c_t[:, 0:512], lhsT=dg[:, tap, :], rhs=pr[:, r0 + kh:r0 + kh + 8, kw:kw + 64], start=(tap == 0), stop=(tap == 8)),
                                reads=[dg, pr], writes=[c_t])
                    own_blk = 1 <= blk <= 4
                    if ct >= 8 and own_blk:
                        g = (ct - 8) % 2
                        dstT = (c["BT"] if ct < 10 else c["CT"])
                        o0 = (blk - 1) * 512
                        tr.op("act", lambda e, dstT=dstT, g=g, o0=o0, c_t=c_t, ct=ct: e.activation(
                            out=dstT[:, g, o0:o0 + 512], in_=c_t[:, 0:512], func=AF.Silu, bias=cb[:, ct:ct + 1]),
                            reads=[c_t, cb], writes=[dstT])
                        if ct >= 10:
                            continue
                        src_post, src_r = dstT[:, g, o0:o0 + 512], dstT
                    else:
                        po = post[npost % 3]
                        tr.op("act", lambda e, po=po, c_t=c_t, ct=ct, n=n: e.activation(
                            out=po[:, 0:n], in_=c_t[:, 0:n], func=AF.Silu, bias=cb[:, ct:ct + 1]),
                            reads=[c_t, cb], writes=[po])
                        src_post, src_r = po[:, 0:n], po
                    ntl = n // 128
                    t_t = pt[npost % 2]
                    ts_t = tst[npost % 3]
                    for i in range(ntl):
                        tr.op("pe", lambda e, i=i, t_t=t_t, src_post=src_post: e.transpose(
                            out=t_t[:, i * 128:(i + 1) * 128], in_=src_post[:, i * 128:(i + 1) * 128], identity=c["ident_b"][:]),
                            reads=[src_r, c["ident_b"]], writes=[t_t])
                    tr.op("dve", lambda e, t_t=t_t, ts_t=ts_t, ntl=ntl: e.tensor_copy(
                        out=ts_t[:, 0:ntl, :], in_=t_t[:, 0:ntl * 128].rearrange("p (a b) -> p a b", b=128)),
                        reads=[t_t], writes=[ts_t])
                    tile0 = 0 if blk == 0 else T_OWN0 + (blk - 1) * 4
                    if ct < 8:
                        dst_d, dst_r = xtok_v[:, tile0:tile0 + ntl, ct * 128:(ct + 1) * 128], c["x_tok"]
                    else:
                        dst_d, dst_r = btok_v[:, tile0:tile0 + ntl, (ct - 8) * 128:(ct - 7) * 128], c["B_tok"]
                    tr.dma("sp", tsem[npost % 3], out=dst_d, in_=ts_t[:, 0:ntl, :], reads=[ts_t], writes=[])
                    c.setdefault("scr_ev", []).append(ts_t)
                    npost += 1
            self.conv_store_tiles = tst
            self.tap("BT", c["BT"][:].rearrange("p g t -> p (g t)"), [128, 2 * NOWN], BF16, [c["BT"]])
            self.tap("CT", c["CT"][:].rearrange("p g t -> p (g t)"), [128, 2 * NOWN], BF16, [c["CT"]])
            for e in Tracker.ENG:
                tr.wait_all(e, tst)
            self.barrier_release(rel)

    def stage_gla(self, st):
        tr, c, I = self.tr, self.c, self.I
        self.fence()
        c["oB"] = self.scratch("oB", [NOWN, 1024], F32)
        c["yx"] = self.scratch("yx", [NOWN, 2048], BF16)
        oB_v = c["oB"].t.rearrange("(n p) c -> n p c", p=128)
        yx_v = c["yx"].t.rearrange("(n p) c -> n p c", p=128)
        w_in_v = I["w_in"].t.rearrange("(kc p) n -> p kc n", p=128)
        LNQ = math.log(128.0 ** -0.5)
        with ExitStack() as s2:
            wg = self.sb(s2, "wgla", [128, 8, 3072], BF16)
            wgs = [Res("wgla%d" % i) for i in range(6)]
            wgg = self.sb(s2, "wgg", [128, 8, 32], BF16)
            wup = self.sb(s2, "wup", [17, 2, 512], F32)
            gn = self.sb(s2, "gn_bc", [128, 256], F32)
            c_one = self.sb(s2, "c_one", [128, 1], F32)
            c_lnq = self.sb(s2, "c_lnq", [128, 1], F32)
            c_eps = self.sb(s2, "c_eps", [128, 1], F32)
            negcol = self.sb(s2, "negcol", [128, 2], F32)
            Tm = [self.sb(s2, "TmA", [128, 128], F32), self.sb(s2, "TmB", [128, 128], F32)]
            S = [self.sb(s2, "S_A", [128, 4, 256], F32), self.sb(s2, "S_B", [128, 4, 256], F32)]
            Sbf = self.sb(s2, "Sbf", [128, 4, 256], BF16)
            g_aug = self.sb(s2, "g_aug", [32, 128], F32)
            v_bf = self.sb(s2, "v_bf", [128, 1024], BF16)
            lap = self.sb(s2, "lap", [128, 512], F32)
            e1 = lap
            Einv = self.sb(s2, "Einv", [128, 512], F32)
            Eq = self.sb(s2, "Eq", [128, 512], F32)
            kt_ = self.sb(s2, "kt_", [128, 512], BF16)
            qt_ = self.sb(s2, "qt_", [128, 512], BF16)
            kqT = self.sb(s2, "kqT", [128, 8, 128], BF16)
            PT = self.sb(s2, "PT", [128, 4, 128], BF16)
            dcol = self.sb(s2, "dcol", [128, 4], F32)
            silr = self.sb(s2, "silr", [128, 1024], F32)
            o_sb = self.sb(s2, "o_sb", [128, 1024], F32)
            oB_sb = [self.sb(s2, "oB_sb%d" % i, [128, 1024], F32) for i in range(2)]
            oBs = [self.dsem() for _ in range(2)]
            ost = [self.sb(s2, "ost%d" % i, [128, 1024], F32) for i in range(2)]
            osts = [self.dsem() for _ in range(2)]
            yst = [self.sb(s2, "yst%d" % i, [128, 1024], BF16) for i in range(2)]
            ysts = [self.dsem() for _ in range(2)]
            ss4 = self.sb(s2, "ss4", [128, 4], F32)
            rs4 = self.sb(s2, "rs4", [128, 4], F32)
            junk = self.sb(s2, "junkg", [128, 256], BF16)
            t1 = o_sb
            pK = self.ps(s2, "pK"); pQ = self.ps(s2, "pQ")
            pV = [self.ps(s2, "pV0"), self.ps(s2, "pV1")]
            pO = [self.ps(s2, "pO0"), self.ps(s2, "pO1")]
            pL = self.ps(s2, "pL")
            pT = self.ps(s2, "pT", [128, 1024], BF16)
            rel = [wg, wgg, wup, gn, c_one, c_lnq, c_eps, negcol, Tm[0], Tm[1], S[0], S[1], Sbf, g_aug, v_bf, lap, Einv, Eq, kt_, qt_,
                   kqT, PT, dcol, silr, o_sb, ss4, rs4, junk, pK, pQ, pL, pT] + pV + pO + oB_sb + ost + yst + wgs
            d0 = self.dsem(9)
            for i in range(6):
                tr.dma("pool", d0, out=wg[:, :, i * 512:(i + 1) * 512], in_=w_in_v[:, :, i * 512:(i + 1) * 512], writes=[wgs[i]])
            tr.dma("pool", d0, out=wgg[:], in_=w_in_v[:, :, OFF_G:OFF_G + 32], writes=[wgg])
            tr.dma("sp", d0, out=wup[:], in_=I["w_up_aug"].t.rearrange("d k n -> k d n"), writes=[wup])
            tr.dma("sp", d0, out=gn[:], in_=I["gla_norm"].t.partition_broadcast(128), writes=[gn])
            tr.op("pool", lambda e: e.memset(c_one[:], 1.0), writes=[c_one])
            tr.op("pool", lambda e: e.memset(c_lnq[:], LNQ), writes=[c_lnq])
            tr.op("pool", lambda e: e.memset(c_eps[:], EPS), writes=[c_eps])
            tr.op("pool", lambda e: e.memset(negcol[:], -1.0 / 16.0), writes=[negcol])
            tr.op("pool", lambda e: e.tensor_scalar(out=Tm[0][:], in0=c["tri_le"][:], scalar1=-1.0 / 16.0, scalar2=None, op0=ALU.mult), reads=[c["tri_le"]], writes=[Tm[0]])
            tr.op("pool", lambda e: e.tensor_scalar(out=Tm[1][:], in0=c["tri_ge"][:], scalar1=-1.0 / 16.0, scalar2=None, op0=ALU.mult), reads=[c["tri_ge"]], writes=[Tm[1]])
            tr.op("pool", lambda e: e.memset(g_aug[:], 1.0), writes=[g_aug])
            for dd in range(2):
                tr.op("pool", lambda e, dd=dd: e.memset(S[dd][:], 0.0), writes=[S[dd]])
            masks = [c["tri_le"], c["tri_ge"]]

            def mm_tok(ps_t, t, c0, n, wres):
                for kc in range(8):
                    tr.op("pe", lambda e, kc=kc: e.matmul(out=ps_t[:, 0:n], lhsT=c["hT"][:, kc, t * 128:(t + 1) * 128], rhs=wg[:, kc, c0:c0 + n],
                                                         start=(kc == 0), stop=(kc == 7)), reads=[c["hT_r"][t]] + wres, writes=[ps_t])

            def gla_tile(t, dd, full, sweepA, own_idx):
                Sd = S[dd]
                mm_tok(pK, t, 512, 512, [wgs[1]])
                mm_tok(pV[0], t, 1024, 512, [wgs[2]])
                mm_tok(pV[1], t, 1536, 512, [wgs[3]])
                for kc in range(8):
                    tr.op("pe", lambda e, kc=kc: e.matmul(out=pT[0:16, 0:256].bitcast(F32) if False else pL[0:16, 0:128], lhsT=wgg[:, kc, dd * 16:(dd + 1) * 16],
                                                         rhs=c["hT"][:, kc, t * 128:(t + 1) * 128], start=(kc == 0), stop=(kc == 7)),
                          reads=[c["hT_r"][t], wgg], writes=[pL])
                tr.op("act", lambda e: e.activation(out=g_aug[0:16, :], in_=pL[0:16, 0:128], func=AF.Copy), reads=[pL], writes=[g_aug])
                tr.op("act", lambda e: e.activation(out=v_bf[:, 0:512], in_=pV[0][:], func=AF.Copy), reads=[pV[0]], writes=[v_bf])
                tr.op("act", lambda e: e.activation(out=v_bf[:, 512:1024], in_=pV[1][:], func=AF.Copy), reads=[pV[1]], writes=[v_bf])
                if full:
                    mm_tok(pQ, t, 0, 512, [wgs[0]])
                tr.op("pe", lambda e: e.matmul(out=pL[:, 0:512], lhsT=g_aug[0:17, :], rhs=wup[:, dd, :], start=True, stop=True), reads=[g_aug, wup], writes=[pL])
                tr.op("act", lambda e: e.activation(out=e1[:], in_=pL[:, 0:512], func=AF.Exp, scale=-1.0), reads=[pL], writes=[e1])
                tr.op("act", lambda e: e.activation(out=lap[:], in_=e1[:], func=AF.Ln, bias=c_one[:]), reads=[e1, c_one], writes=[lap])
                tr.op("pe", lambda e: e.matmul(out=pL[:, 0:512], lhsT=Tm[dd][:], rhs=lap[:], start=True, stop=True), reads=[Tm[dd], lap], writes=[pL])
                tr.op("act", lambda e: e.activation(out=Einv[:], in_=pL[:, 0:512], func=AF.Exp, scale=-1.0), reads=[pL], writes=[Einv])
                if full:
                    tr.op("act", lambda e: e.activation(out=Eq[:], in_=pL[:, 0:512], func=AF.Exp, bias=c_lnq[:]), reads=[pL, c_lnq], writes=[Eq])
                tr.op("dve", lambda e: e.tensor_tensor(out=kt_[:], in0=pK[:], in1=Einv[:], op=ALU.mult), reads=[pK, Einv], writes=[kt_])
                for h in range(4):
                    tr.op("pe", lambda e, h=h: e.matmul(out=pL[:, 2 * h:2 * h + 2], lhsT=lap[:, h * 128:(h + 1) * 128], rhs=negcol[:], start=True, stop=True),
                          reads=[lap, negcol], writes=[pL])
                tr.op("act", lambda e: e.activation(out=dcol[:], in_=pL[:, 0:8:2], func=AF.Exp), reads=[pL], writes=[dcol])
                if full:
                    tr.op("dve", lambda e: e.tensor_tensor(out=qt_[:], in0=pQ[:], in1=Eq[:], op=ALU.mult), reads=[pQ, Eq], writes=[qt_])
                    for h in range(4):
                        tr.op("pe", lambda e, h=h: e.transpose(out=pT[:, h * 128:(h + 1) * 128], in_=kt_[:, h * 128:(h + 1) * 128], identity=c["ident_b"][:]),
                              reads=[kt_, c["ident_b"]], writes=[pT])
                    for h in range(4):
                        tr.op("pe", lambda e, h=h: e.transpose(out=pT[:, (4 + h) * 128:(5 + h) * 128], in_=qt_[:, h * 128:(h + 1) * 128], identity=c["ident_b"][:]),
                              reads=[qt_, c["ident_b"]], writes=[pT])
                    tr.op("act", lambda e: e.activation(out=kqT[:].rearrange("p a b -> p (a b)"), in_=pT[:], func=AF.Copy), reads=[pT], writes=[kqT])
                    for h in range(4):
                        tr.op("pe", lambda e, h=h: e.matmul(out=pK[:, h * 128:(h + 1) * 128], lhsT=kqT[:, h, :], rhs=kqT[:, 4 + h, :], start=True, stop=True),
                              reads=[kqT], writes=[pK])
                    tr.op("dve", lambda e: e.tensor_tensor(out=PT[:], in0=pK[:].rearrange("p (h i) -> p h i", h=4),
                                                          in1=masks[dd][:].unsqueeze(1).to_broadcast([128, 4, 128]), op=ALU.mult),
                          reads=[pK, masks[dd]], writes=[PT])
                    tr.op("act", lambda e: e.activation(out=Sbf[:].rearrange("p a b -> p (a b)"), in_=Sd[:].rearrange("p a b -> p (a b)"), func=AF.Copy), reads=[Sd], writes=[Sbf])
                    for h in range(4):
                        po = pO[h // 2]
                        cs = (h % 2) * 256
                        tr.op("pe", lambda e, h=h, po=po, cs=cs: e.matmul(out=po[:, cs:cs + 256], lhsT=PT[:, h, :], rhs=v_bf[:, h * 256:(h + 1) * 256], start=True, stop=False),
                              reads=[PT, v_bf], writes=[po])
                        tr.op("pe", lambda e, h=h, po=po, cs=cs: e.matmul(out=po[:, cs:cs + 256], lhsT=kqT[:, 4 + h, :], rhs=Sbf[:, h, :], start=False, stop=True),
                              reads=[kqT, Sbf], writes=[po])
                for h in range(4):
                    pv = pV[h // 2]
                    cs = (h % 2) * 256
                    tr.op("pe", lambda e, h=h, pv=pv, cs=cs: e.matmul(out=pv[:, cs:cs + 256], lhsT=kt_[:, h * 128:(h + 1) * 128], rhs=v_bf[:, h * 256:(h + 1) * 256], start=True, stop=True),
                          reads=[kt_, v_bf], writes=[pv])
                for h in range(4):
                    pv = pV[h // 2]
                    cs = (h % 2) * 256
                    tr.op("pool", lambda e, h=h: e.tensor_scalar(out=Sd[:, h, :], in0=Sd[:, h, :], scalar1=dcol[:, h:h + 1], scalar2=None, op0=ALU.mult), reads=[Sd, dcol], writes=[Sd])
                    tr.op("dve", lambda e, h=h, pv=pv, cs=cs: e.scalar_tensor_tensor(out=Sd[:, h, :], in0=pv[:, cs:cs + 256], scalar=dcol[:, h:h + 1], in1=Sd[:, h, :], op0=ALU.mult, op1=ALU.add),
                          reads=[pv, dcol, Sd], writes=[Sd])
                if not full:
                    return
                if not sweepA:
                    os_ = ost[own_idx % 2]
                    tr.op("act", lambda e: e.activation(out=os_[:, 0:512], in_=pO[0][:], func=AF.Copy), reads=[pO[0]], writes=[os_])
                    tr.op("dve", lambda e: e.tensor_copy(out=os_[:, 512:1024], in_=pO[1][:]), reads=[pO[1]], writes=[os_])
                    tr.dma("sp", osts[own_idx % 2], out=oB_v[own_idx], in_=os_[:], reads=[os_], writes=[])
                    return
                ob = oB_sb[own_idx % 2]
                tr.dma("sp", oBs[own_idx % 2], out=ob[:], in_=oB_v[own_idx], writes=[ob])
                mm_tok(pQ, t, 2048, 512, [wgs[4]])
                tr.op("act", lambda e: e.activation(out=silr[:, 0:512], in_=pQ[:], func=AF.Silu), reads=[pQ], writes=[silr])
                mm_tok(pQ, t, 2560, 512, [wgs[5]])
                tr.op("act", lambda e: e.activation(out=silr[:, 512:1024], in_=pQ[:], func=AF.Silu), reads=[pQ], writes=[silr])
                tr.op("pool", lambda e: e.tensor_tensor(out=silr[:].rearrange("p (h v) -> p h v", h=4), in0=silr[:].rearrange("p (h v) -> p h v", h=4),
                                                       in1=gn[:].unsqueeze(1).to_broadcast([128, 4, 256]), op=ALU.mult), reads=[silr, gn], writes=[silr])
                for hh in range(2):
                    tr.op("dve", lambda e, hh=hh: e.tensor_tensor(out=o_sb[:, hh * 512:(hh + 1) * 512], in0=pO[hh][:], in1=ob[:, hh * 512:(hh + 1) * 512], op=ALU.add),
                          reads=[pO[hh], ob], writes=[o_sb])
                for h in range(4):
                    tr.op("act", lambda e, h=h: e.activation(out=junk[:], in_=o_sb[:, h * 256:(h + 1) * 256], func=AF.Square, accum_out=ss4[:, h:h + 1]),
                          reads=[o_sb], writes=[junk, ss4])
                tr.op("act", lambda e: e.activation(out=rs4[:], in_=ss4[:], func=AF.Ln, scale=1.0 / 256.0, bias=c_eps[:]), reads=[ss4, c_eps], writes=[rs4])
                tr.op("act", lambda e: e.activation(out=rs4[:], in_=rs4[:], func=AF.Exp, scale=-0.5), reads=[rs4], writes=[rs4])
                tr.op("dve", lambda e: e.tensor_tensor(out=t1[:].rearrange("p (h v) -> p h v", h=4), in0=o_sb[:].rearrange("p (h v) -> p h v", h=4),
                                                      in1=rs4[:].unsqueeze(2).to_broadcast([128, 4, 256]), op=ALU.mult), reads=[o_sb, rs4], writes=[t1])
                ys = yst[own_idx % 2]
                tr.op("pool", lambda e: e.tensor_tensor(out=ys[:], in0=t1[:], in1=silr[:], op=ALU.mult), reads=[t1, silr], writes=[ys])
                tr.dma("sp", ysts[own_idx % 2], out=yx_v[own_idx][:, 0:1024], in_=ys[:], reads=[ys], writes=[])

            for t in (1, 0):
                gla_tile(t, 1, False, False, None)
            self.tap("gS_B", S[1][:].rearrange("p a b -> p (a b)"), [128, 1024], F32, [S[1]])
            for t in range(NT - 1, T_OTH0 - 1, -1):
                gla_tile(t, 1, False, False, None)
            for t in range(T_OTH0 - 1, T_OWN0 - 1, -1):
                gla_tile(t, 1, True, False, t - T_OWN0)
            for e in Tracker.ENG:
                tr.wait_all(e, ost)
            for t in (0, 1):
                gla_tile(t, 0, False, True, None)
            self.tap("gS_A", S[0][:].rearrange("p a b -> p (a b)"), [128, 1024], F32, [S[0]])
            for t in range(T_OWN0, T_OTH0):
                gla_tile(t, 0, True, True, t - T_OWN0)
            for e in Tracker.ENG:
                tr.wait_all(e, yst)
            self.barrier_release(rel)

    def stage_ssd(self, st):
        tr, c, I = self.tr, self.c, self.I
        self.fence()
        c["yB"] = self.scratch("yB", [NOWN, 1024], F32)
        yB_v = c["yB"].t.rearrange("(n p) c -> n p c", p=128)
        yx_v = c["yx"].t.rearrange("(n p) c -> n p c", p=128)
        xtok_v = c["x_tok"].t.rearrange("(n p) c -> n p c", p=128)
        btok_v = c["B_tok"].t.rearrange("(n p) c -> n p c", p=128)
        w_in_v = I["w_in"].t.rearrange("(kc p) n -> p kc n", p=128)
        BT, CT = c["BT"], c["CT"]
        with ExitStack() as s2:
            wz = self.sb(s2, "wz", [128, 8, 1024], BF16)
            wdt = self.sb(s2, "wdt", [128, 8, 32], BF16)
            Abc = self.sb(s2, "Abc", [128, 32], F32)
            dtb = self.sb(s2, "dtb", [128, 32], F32)
            Dsk = self.sb(s2, "Dsk", [128, 16], F32)
            snb = self.sb(s2, "snb", [128, 1024], F32)
            c_one = self.sb(s2, "c_one2", [128, 1], F32)
            c_eps = self.sb(s2, "c_eps2", [128, 1], F32)
            ST = [self.sb(s2, "ST_A", [128, 2, 512], F32), self.sb(s2, "ST_B", [128, 2, 512], F32)]
            STbf = self.sb(s2, "STbf", [128, 2, 512], BF16)
            xt = [self.sb(s2, "xt%d" % i, [128, 1024], BF16) for i in range(2)]
            bt = [self.sb(s2, "bt%d" % i, [128, 256], BF16) for i in range(2)]
            xts = [self.dsem() for _ in range(2)]
            dt_ = self.sb(s2, "dt_", [128, 16], F32)
            dtA = self.sb(s2, "dtA", [128, 16], F32)
            acs = self.sb(s2, "acs", [128, 16], F32)
            ea = self.sb(s2, "ea", [128, 16], F32)
            dend = self.sb(s2, "dend", [128, 16], F32)
            dtot = self.sb(s2, "dtot", [128, 16], F32)
            R1 = self.sb(s2, "R1", [128, 16, 128], F32)
            E = self.sb(s2, "E", [128, 16, 128], BF16)
            M = self.sb(s2, "M", [128, 16, 128], BF16)
            CBm = self.sb(s2, "CBm", [128, 2, 128], F32)
            xdt = self.sb(s2, "xdt", [128, 1024], BF16)
            xdd = self.sb(s2, "xdd", [128, 1024], BF16)
            silz = self.sb(s2, "silz", [128, 1024], F32)
            y_sb = self.sb(s2, "y_sb", [128, 1024], F32)
            tmp = self.sb(s2, "ytmp", [128, 1024], F32)
            yB_sb = [self.sb(s2, "yB_sb%d" % i, [128, 1024], F32) for i in range(2)]
            yBs = [self.dsem() for _ in range(2)]
            yst = [self.sb(s2, "ysst%d" % i, [128, 1024], F32) for i in range(2)]
            ysts = [self.dsem() for _ in range(2)]
            yxs = [self.sb(s2, "yxs%d" % i, [128, 1024], BF16) for i in range(2)]
            yxss = [self.dsem() for _ in range(2)]
            ss2 = self.sb(s2, "ss2", [128, 2], F32)
            rs2 = self.sb(s2, "rs2", [128, 2], F32)
            junk = self.sb(s2, "junks", [128, 512], BF16)
            pS = self.ps(s2, "pS")
            pD = [self.ps(s2, "pD%d" % i) for i in range(4)]
            pCB = self.ps(s2, "pCB")
            pY = [self.ps(s2, "pY%d" % i) for i in range(2)]
            rel = [wz, wdt, Abc, dtb, Dsk, snb, c_one, c_eps, ST[0], ST[1], STbf, dt_, dtA, acs, ea, dend, dtot, R1, E, M, CBm, xdt, xdd,
                   silz, y_sb, tmp, ss2, rs2, junk, pS, pCB] + xt + bt + yB_sb + yst + yxs + pD + pY
            d0 = self.dsem(6)
            tr.dma("pool", d0, out=wz[:], in_=w_in_v[:, :, OFF_Z:OFF_Z + 1024], writes=[wz])
            tr.dma("pool", d0, out=wdt[:], in_=w_in_v[:, :, OFF_DT:OFF_DT + 32], writes=[wdt])
            tr.dma("sp", d0, out=Abc[:], in_=I["a_log"].t.partition_broadcast(128), writes=[Abc])
            tr.dma("sp", d0, out=dtb[:], in_=I["dt_bias"].t.partition_broadcast(128), writes=[dtb])
            tr.dma("sp", d0, out=Dsk[:], in_=I["ssd_d"].t.partition_broadcast(128), writes=[Dsk])
            tr.dma("sp", d0, out=snb[:], in_=I["ssd_norm"].t.partition_broadcast(128), writes=[snb])
            tr.op("pool", lambda e: e.memset(c_one[:], 1.0), writes=[c_one])
            tr.op("pool", lambda e: e.memset(c_eps[:], EPS), writes=[c_eps])
            tr.op("act", lambda e: e.activation(out=Abc[:], in_=Abc[:], func=AF.Exp), reads=[Abc], writes=[Abc])
            tr.op("dve", lambda e: e.tensor_scalar(out=Abc[:], in0=Abc[:], scalar1=-1.0, scalar2=None, op0=ALU.mult), reads=[Abc], writes=[Abc])
            for dd in range(2):
                tr.op("pool", lambda e, dd=dd: e.memset(ST[dd][:], 0.0), writes=[ST[dd]])
            Lm = [c["tri_gt"], c["tri_lt"]]
            Tc = [c["tri_le"], c["tri_ge"]]
            cnt = [0]

            def ssd_tile(t, dd, full, sweepA, own_idx):
                STd = ST[dd]
                k = cnt[0] % 2
                cnt[0] += 1
                x_t, b_t = xt[k], bt[k]
                tr.regroup(xts[k], 2)
                tr.dma("sp", xts[k], out=x_t[:], in_=xtok_v[t], writes=[x_t])
                tr.dma("sp", xts[k], out=b_t[:], in_=btok_v[t], writes=[b_t])
                hres = [c["hT_r"][t]]
                lhs = lambda kc: c["hT"][:, kc, t * 128:(t + 1) * 128]
                if full and sweepA:
                    for hh in range(2):
                        for kc in range(8):
                            tr.op("pe", lambda e, kc=kc, hh=hh: e.matmul(out=pD[2 + hh][:], lhsT=lhs(kc), rhs=wz[:, kc, hh * 512:(hh + 1) * 512], start=(kc == 0), stop=(kc == 7)),
                                  reads=hres + [wz], writes=[pD[2 + hh]])
                        tr.op("act", lambda e, hh=hh: e.activation(out=silz[:, hh * 512:(hh + 1) * 512], in_=pD[2 + hh][:], func=AF.Silu), reads=[pD[2 + hh]], writes=[silz])
                for kc in range(8):
                    tr.op("pe", lambda e, kc=kc: e.matmul(out=pS[:, 0:16], lhsT=lhs(kc), rhs=wdt[:, kc, dd * 16:(dd + 1) * 16], start=(kc == 0), stop=(kc == 7)),
                          reads=hres + [wdt], writes=[pS])
                tr.op("dve", lambda e: e.tensor_tensor(out=dt_[:], in0=pS[:, 0:16], in1=dtb[:, dd * 16:(dd + 1) * 16], op=ALU.add), reads=[pS, dtb], writes=[dt_])
                tr.op("act", lambda e: e.activation(out=dt_[:], in_=dt_[:], func=AF.Exp), reads=[dt_], writes=[dt_])
                tr.op("act", lambda e: e.activation(out=dt_[:], in_=dt_[:], func=AF.Ln, bias=c_one[:]), reads=[dt_, c_one], writes=[dt_])
                tr.op("dve", lambda e: e.tensor_tensor(out=dtA[:], in0=dt_[:], in1=Abc[:, dd * 16:(dd + 1) * 16], op=ALU.mult), reads=[dt_, Abc], writes=[dtA])
                tr.op("pe", lambda e: e.matmul(out=pS[:, 16:32], lhsT=Tc[dd][:], rhs=dtA[:], start=True, stop=True), reads=[Tc[dd], dtA], writes=[pS])
                tr.op("pe", lambda e: e.matmul(out=pS[:, 32:48], lhsT=c["ones_f"][:], rhs=dtA[:], start=True, stop=True), reads=[c["ones_f"], dtA], writes=[pS])
                tr.op("dve", lambda e: e.tensor_copy(out=acs[:], in_=pS[:, 16:32]), reads=[pS], writes=[acs])
                tr.op("dve", lambda e: e.tensor_tensor(out=dend[:], in0=pS[:, 32:48], in1=acs[:], op=ALU.subtract), reads=[pS, acs], writes=[dend])
                tr.op("act", lambda e: e.activation(out=dend[:], in_=dend[:], func=AF.Exp), reads=[dend], writes=[dend])
                tr.op("act", lambda e: e.activation(out=dtot[:], in_=pS[:, 32:48], func=AF.Exp), reads=[pS], writes=[dtot])
                tr.op("dve", lambda e: e.tensor_tensor(out=xdt[:].rearrange("p (h q) -> p h q", h=16), in0=x_t[:].rearrange("p (h q) -> p h q", h=16),
                                                      in1=dt_[:].unsqueeze(2).to_broadcast([128, 16, 64]), op=ALU.mult), reads=[x_t, dt_], writes=[xdt])
                if full:
                    tok0 = (t - T_OWN0) * 128
                    tr.op("act", lambda e: e.activation(out=ea[:], in_=acs[:], func=AF.Exp), reads=[acs], writes=[ea])
                    tr.op("act", lambda e: e.activation(out=STbf[:].rearrange("p a b -> p (a b)"), in_=STd[:].rearrange("p a b -> p (a b)"), func=AF.Copy), reads=[STd], writes=[STbf])
                    tr.op("dve", lambda e: e.tensor_tensor(out=R1[:], in0=Tc[dd][:].unsqueeze(1).to_broadcast([128, 16, 128]),
                                                          in1=dtA[:].unsqueeze(2).to_broadcast([128, 16, 128]), op=ALU.mult), reads=[Tc[dd], dtA], writes=[R1])
                    for b4 in range(4):
                        tr.op("pe", lambda e, b4=b4: e.matmul(out=pD[b4][:], lhsT=Lm[dd][:], rhs=R1[:, 4 * b4:4 * b4 + 4, :], start=True, stop=True),
                              reads=[Lm[dd], R1], writes=[pD[b4]])
                        tr.op("act", lambda e, b4=b4: e.activation(out=E[:, 4 * b4:4 * b4 + 4, :], in_=pD[b4][:].rearrange("p (h i) -> p h i", h=4), func=AF.Exp), reads=[pD[b4]], writes=[E])
                    for g in range(2):
                        tr.op("pe", lambda e, g=g: e.matmul(out=pCB[:, g * 128:(g + 1) * 128], lhsT=BT[:, g, tok0:tok0 + 128], rhs=CT[:, g, tok0:tok0 + 128], start=True, stop=True),
                              reads=[BT, CT], writes=[pCB])
                    tr.op("dve", lambda e: e.tensor_tensor(out=CBm[:], in0=pCB[:, 0:256].rearrange("p (g i) -> p g i", g=2),
                                                          in1=Tc[dd][:].unsqueeze(1).to_broadcast([128, 2, 128]), op=ALU.mult), reads=[pCB, Tc[dd]], writes=[CBm])
                    for g in range(2):
                        eng = "dve" if g == 0 else "pool"
                        tr.op(eng, lambda e, g=g: e.tensor_tensor(out=M[:, g * 8:(g + 1) * 8, :], in0=E[:, g * 8:(g + 1) * 8, :],
                                                                  in1=CBm[:, g:g + 1, :].to_broadcast([128, 8, 128]), op=ALU.mult), reads=[E, CBm], writes=[M])
                    for h in range(16):
                        py = pY[h // 8]
                        cs = (h % 8) * 64
                        tr.op("pe", lambda e, h=h, py=py, cs=cs: e.matmul(out=py[:, cs:cs + 64], lhsT=M[:, h, :], rhs=xdt[:, h * 64:(h + 1) * 64], start=True, stop=True),
                              reads=[M, xdt], writes=[py])
                    for g in range(2):
                        tr.op("pe", lambda e, g=g: e.matmul(out=pD[g][:], lhsT=CT[:, g, tok0:tok0 + 128], rhs=STbf[:, g, :], start=True, stop=True),
                              reads=[CT, STbf], writes=[pD[g]])
                        tr.op("dve", lambda e, g=g: e.tensor_tensor(out=tmp[:, g * 512:(g + 1) * 512].rearrange("p (h q) -> p h q", h=8), in0=pD[g][:].rearrange("p (h q) -> p h q", h=8),
                                                                    in1=ea[:, g * 8:(g + 1) * 8].unsqueeze(2).to_broadcast([128, 8, 64]), op=ALU.mult), reads=[pD[g], ea], writes=[tmp])
                        tr.op("dve", lambda e, g=g: e.tensor_tensor(out=y_sb[:, g * 512:(g + 1) * 512], in0=pY[g][:], in1=tmp[:, g * 512:(g + 1) * 512], op=ALU.add),
                              reads=[pY[g], tmp], writes=[y_sb])
                tr.op("pool", lambda e: e.tensor_tensor(out=xdd[:].rearrange("p (h q) -> p h q", h=16), in0=xdt[:].rearrange("p (h q) -> p h q", h=16),
                                                       in1=dend[:].unsqueeze(2).to_broadcast([128, 16, 64]), op=ALU.mult), reads=[xdt, dend], writes=[xdd])
                for g in range(2):
                    tr.op("pe", lambda e, g=g: e.matmul(out=pD[2 + g][:], lhsT=b_t[:, g * 128:(g + 1) * 128], rhs=xdd[:, g * 512:(g + 1) * 512], start=True, stop=True),
                          reads=[b_t, xdd], writes=[pD[2 + g]])
                    tr.op("pool", lambda e, g=g: e.tensor_tensor(out=STd[:, g, :].rearrange("p (h q) -> p h q", h=8), in0=STd[:, g, :].rearrange("p (h q) -> p h q", h=8),
                                                                 in1=dtot[:, g * 8:(g + 1) * 8].unsqueeze(2).to_broadcast([128, 8, 64]), op=ALU.mult), reads=[STd, dtot], writes=[STd])
                    tr.op("dve", lambda e, g=g: e.tensor_tensor(out=STd[:, g, :], in0=pD[2 + g][:], in1=STd[:, g, :], op=ALU.add), reads=[pD[2 + g], STd], writes=[STd])
                if not full:
                    return
                if not sweepA:
                    ys = yst[own_idx % 2]
                    tr.op("act", lambda e: e.activation(out=ys[:], in_=y_sb[:], func=AF.Copy), reads=[y_sb], writes=[ys])
                    tr.dma("sp", ysts[own_idx % 2], out=yB_v[own_idx], in_=ys[:], reads=[ys], writes=[])
                    return
                yb = yB_sb[own_idx % 2]
                tr.dma("sp", yBs[own_idx % 2], out=yb[:], in_=yB_v[own_idx], writes=[yb])
                tr.op("dve", lambda e: e.tensor_tensor(out=y_sb[:], in0=y_sb[:], in1=yb[:], op=ALU.add), reads=[y_sb, yb], writes=[y_sb])
                tr.op("pool", lambda e: e.tensor_tensor(out=tmp[:].rearrange("p (h q) -> p h q", h=16), in0=x_t[:].rearrange("p (h q) -> p h q", h=16),
                                                       in1=Dsk[:].unsqueeze(2).to_broadcast([128, 16, 64]), op=ALU.mult), reads=[x_t, Dsk], writes=[tmp])
                tr.op("dve", lambda e: e.tensor_tensor(out=y_sb[:], in0=y_sb[:], in1=tmp[:], op=ALU.add), reads=[y_sb, tmp], writes=[y_sb])
                tr.op("dve", lambda e: e.tensor_tensor(out=y_sb[:], in0=y_sb[:], in1=silz[:], op=ALU.mult), reads=[y_sb, silz], writes=[y_sb])
                for g in range(2):
                    tr.op("act", lambda e, g=g: e.activation(out=junk[:], in_=y_sb[:, g * 512:(g + 1) * 512], func=AF.Square, accum_out=ss2[:, g:g + 1]), reads=[y_sb], writes=[junk, ss2])
                tr.op("act", lambda e: e.activation(out=rs2[:], in_=ss2[:], func=AF.Ln, scale=1.0 / 512.0, bias=c_eps[:]), reads=[ss2, c_eps], writes=[rs2])
                tr.op("act", lambda e: e.activation(out=rs2[:], in_=rs2[:], func=AF.Exp, scale=-0.5), reads=[rs2], writes=[rs2])
                tr.op("dve", lambda e: e.tensor_tensor(out=y_sb[:].rearrange("p (g q) -> p g q", g=2), in0=y_sb[:].rearrange("p (g q) -> p g q", g=2),
                                                      in1=rs2[:].unsqueeze(2).to_broadcast([128, 2, 512]), op=ALU.mult), reads=[y_sb, rs2], writes=[y_sb])
                yo = yxs[own_idx % 2]
                tr.op("pool", lambda e: e.tensor_tensor(out=yo[:], in0=y_sb[:], in1=snb[:], op=ALU.mult), reads=[y_sb, snb], writes=[yo])
                tr.dma("sp", yxss[own_idx % 2], out=yx_v[own_idx][:, 1024:2048], in_=yo[:], reads=[yo], writes=[])

            for t in (1, 0):
                ssd_tile(t, 1, False, False, None)
            self.tap("sS_B", ST[1][:].rearrange("p a b -> p (a b)"), [128, 1024], F32, [ST[1]])
            for t in range(NT - 1, T_OTH0 - 1, -1):
                ssd_tile(t, 1, False, False, None)
            for t in range(T_OTH0 - 1, T_OWN0 - 1, -1):
                ssd_tile(t, 1, True, False, t - T_OWN0)
            for e in Tracker.ENG:
                tr.wait_all(e, yst)
            for t in (0, 1):
                ssd_tile(t, 0, False, True, None)
            self.tap("sS_A", ST[0][:].rearrange("p a b -> p (a b)"), [128, 1024], F32, [ST[0]])
            for t in range(T_OWN0, T_OTH0):
                ssd_tile(t, 0, True, True, t - T_OWN0)
            for e in Tracker.ENG:
                tr.wait_all(e, yxs)
            self.barrier_release(rel)

    def stage_post(self, st):
        tr, c, I = self.tr, self.c, self.I
        self.fence()
        c["h_lat"] = self.sb(st, "h_lat", [128, 16, D], F32)
        c["h_r"] = [Res("h_lat%d" % i) for i in range(16)]
        c["h2T"] = self.sb(st, "h2T", [128, 8, NOWN], BF16)
        c["h2_r"] = [Res("h2T%d" % i) for i in range(16)]
        c["comb"] = self.sb(st, "comb", [128, 16, 32], F32)
        yx_v = c["yx"].t.rearrange("(n p) c -> n p c", p=128)
        h_lat, h2T = c["h_lat"], c["h2T"]
        with ExitStack() as s2:
            wo = self.sb(s2, "wo", [128, 16, D], BF16)
            wr = self.sb(s2, "wr", [128, 8, 36], F32)
            brr = self.sb(s2, "brr", [1, 36], F32)
            c_eps = self.sb(s2, "c_eps3", [128, 1], F32)
            lg = self.sb(s2, "lg", [128, 16, 36], F32)
            yxt = [self.sb(s2, "yxt%d" % i, [128, 2048], BF16) for i in range(2)]
            yxs = [self.dsem() for _ in range(2)]
            xr = [self.sb(s2, "xr2_%d" % i, [128, D], F32) for i in range(2)]
            xrs = [self.dsem() for _ in range(2)]
            yxT = self.sb(s2, "yxT", [128, 16, 128], BF16)
            tmp = self.sb(s2, "ptmp", [128, D], F32)
            hn = self.sb(s2, "hn", [128, D], F32)
            h2f = self.sb(s2, "h2f", [128, 8, 128], F32)
            ss = self.sb(s2, "pss", [128, 1], F32)
            rs = self.sb(s2, "prs", [128, 1], F32)
            junk = self.sb(s2, "pjunk", [128, D], BF16)
            pT = [self.ps(s2, "ppT%d" % i, [128, 1024], BF16) for i in range(2)]
            pO = [self.ps(s2, "ppO%d" % i) for i in range(2)]
            pF = [self.ps(s2, "ppF%d" % i) for i in range(2)]
            pR = self.ps(s2, "ppR")
            rel = [wo, wr, brr, c_eps, lg, yxT, tmp, hn, h2f, ss, rs, junk, pR] + yxt + xr + pT + pO + pF
            import os
            if os.environ.get("BISECT3") == "2":
                self.tap("g1x", c["g1_bc"][:], [128, D], F32, [c["g1_bc"]])
                return
            w_out_v = I["w_out"].t.rearrange("(kc p) n -> p kc n", p=128)
            d0 = self.dsem(2)
            wst = self.sb(s2, "wst", [128, 4, D], F32)
            rel.append(wst)
            wsts = self.dsem()
            for q in range(4):
                tr.dma("sp", wsts, out=wst[:], in_=w_out_v[:, q * 4:(q + 1) * 4, :], writes=[wst])
                tr.op("pool", lambda e, q=q: e.tensor_copy(out=wo[:, q * 4:(q + 1) * 4, :], in_=wst[:]), reads=[wst], writes=[wo])
            tr.dma("sp", d0, out=wr[:].rearrange("p a b -> p (a b)"), in_=I["w_router"].t, writes=[wr])
            tr.dma("sp", d0, out=brr[:], in_=I["b_router"].t, writes=[brr])
            tr.op("pool", lambda e: e.memset(c_eps[:], EPS), writes=[c_eps])
            import os
            B3 = os.environ.get("BISECT3", "")
            for i in range(16 if B3 != "1" else 0):
                y_t, x_t = yxt[i % 2], xr[i % 2]
                tr.dma("sp", yxs[i % 2], out=y_t[:], in_=yx_v[i], writes=[y_t])
                tr.dma("sp", xrs[i % 2], out=x_t[:], in_=I["xs"].t[NCTX + i * 128: NCTX + (i + 1) * 128, :], writes=[x_t])
                for kc in range(16):
                    tr.op("pe", lambda e, kc=kc: e.transpose(out=pT[kc // 8][:, (kc % 8) * 128:(kc % 8 + 1) * 128], in_=y_t[:, kc * 128:(kc + 1) * 128], identity=c["ident_b"][:]),
                          reads=[y_t, c["ident_b"]], writes=[pT[kc // 8]])
                tr.op("act", lambda e: e.activation(out=yxT[:, 0:8, :].rearrange("p a b -> p (a b)"), in_=pT[0][:], func=AF.Copy), reads=[pT[0]], writes=[yxT])
                tr.op("dve", lambda e: e.tensor_copy(out=yxT[:, 8:16, :].rearrange("p a b -> p (a b)"), in_=pT[1][:]), reads=[pT[1]], writes=[yxT])
                hl = h_lat[:, i, :]
                for hh in range(2):
                    for kc in range(16):
                        tr.op("pe", lambda e, kc=kc, hh=hh: e.matmul(out=pO[hh][:], lhsT=yxT[:, kc, :], rhs=wo[:, kc, hh * 512:(hh + 1) * 512], start=(kc == 0), stop=(kc == 15)),
                              reads=[yxT, wo], writes=[pO[hh]])
                    tr.op("dve", lambda e, hh=hh: e.tensor_tensor(out=tmp[:, hh * 512:(hh + 1) * 512], in0=pO[hh][:], in1=c["g1_bc"][:, hh * 512:(hh + 1) * 512], op=ALU.mult),
                          reads=[pO[hh], c["g1_bc"]], writes=[tmp])
                tr.op("pool", lambda e: e.tensor_tensor(out=hl, in0=tmp[:], in1=x_t[:], op=ALU.add), reads=[tmp, x_t], writes=[c["h_r"][i]])
                import os
                if os.environ.get("BISECT2") == "b":
                    continue
                tr.op("act", lambda e: e.activation(out=junk[:], in_=hl, func=AF.Square, accum_out=ss[:]), reads=[c["h_r"][i]], writes=[junk, ss])
                tr.op("act", lambda e: e.activation(out=rs[:], in_=ss[:], func=AF.Ln, scale=1.0 / D, bias=c_eps[:]), reads=[ss, c_eps], writes=[rs])
                tr.op("act", lambda e: e.activation(out=rs[:], in_=rs[:], func=AF.Exp, scale=-0.5), reads=[rs], writes=[rs])
                tr.op("dve", lambda e: e.tensor_scalar(out=hn[:], in0=hl, scalar1=rs[:], scalar2=None, op0=ALU.mult), reads=[c["h_r"][i], rs], writes=[hn])
                for kc in range(8):
                    tr.op("pe", lambda e, kc=kc: e.transpose(out=pF[kc // 4][:, (kc % 4) * 128:(kc % 4 + 1) * 128], in_=hn[:, kc * 128:(kc + 1) * 128], identity=c["ident_f"][:]),
                          reads=[hn, c["ident_f"]], writes=[pF[kc // 4]])
                for q in range(2):
                    tr.op("dve", lambda e, q=q: e.tensor_tensor(out=h2f[:, q * 4:(q + 1) * 4, :], in0=pF[q][:].rearrange("p (k t) -> p k t", k=4),
                                                               in1=c["s2"][:, q * 4:(q + 1) * 4].unsqueeze(2).to_broadcast([128, 4, 128]), op=ALU.mult), reads=[pF[q], c["s2"]], writes=[h2f])
                tr.op("pool", lambda e: e.tensor_tensor(out=h2f[:], in0=h2f[:], in1=c["b2"][:].unsqueeze(2).to_broadcast([128, 8, 128]), op=ALU.add), reads=[h2f, c["b2"]], writes=[h2f])
                tr.op("act", lambda e: e.activation(out=h2T[:, :, i * 128:(i + 1) * 128], in_=h2f[:], func=AF.Copy), reads=[h2f], writes=[c["h2_r"][i]])
                if os.environ.get("BISECT2") == "c":
                    continue
                for kc in range(8):
                    tr.op("pe", lambda e, kc=kc: e.matmul(out=pR[:, 0:36], lhsT=h2f[:, kc, :], rhs=wr[:, kc, :], start=(kc == 0), stop=False), reads=[h2f, wr], writes=[pR])
                tr.op("pe", lambda e: e.matmul(out=pR[:, 0:36], lhsT=c["ones_f"][0:1, :], rhs=brr[0:1, :], start=False, stop=True), reads=[c["ones_f"], brr], writes=[pR])
                tr.op("dve", lambda e: e.tensor_copy(out=lg[:, i, :], in_=pR[:, 0:36]), reads=[pR], writes=[lg])
            self.tap("lg", lg[:].rearrange("p a b -> p (a b)"), [128, 16 * 36], F32, [lg])
            self.tap("h_lat", h_lat[:].rearrange("p a b -> p (a b)"), [128, 16 * D], F32, c["h_r"])
            import os
            if os.environ.get("BISECT") == "a":
                self.tap("wo", wo[:, 0, :], [128, D], BF16, [wo])
                self.tap("wrx", wr[:].rearrange("p a b -> p (a b)"), [128, 288], F32, [wr])
                self.barrier_release(rel)
                return
            def T(name, shape):
                t_ = self.sb(s2, name, shape, F32)
                rel.append(t_)
                return t_
            gmax = T("gmax", [128, 16]); mg = T("mg", [128, 16, 4]); eg = T("eg", [128, 16, 4]); gsum = T("gsum", [128, 16]); pg = T("pg", [128, 16])
            t48 = T("t48", [128, 16, 4, 8]); ein = T("ein", [128, 16, 8]); m1 = T("m1", [128, 16]); k1 = T("k1", [128, 16, 8]); e2 = T("e2", [128, 16, 8])
            m2 = T("m2", [128, 16]); k2 = T("k2", [128, 16, 8]); dd_ = T("dd_", [128, 16]); w1 = T("w1", [128, 16]); w2 = T("w2", [128, 16]); cw8 = T("cw8", [128, 16, 8])
            gl = lg[:, :, 0:4]
            el = lg[:, :, 4:36].rearrange("p t (g x) -> p t g x", g=4)
            V = lambda fn, r, w: tr.op("dve", fn, reads=r, writes=w)
            V(lambda e: e.tensor_reduce(out=gmax[:], in_=gl, axis=AX.X, op=ALU.max), [lg], [gmax])
            V(lambda e: e.tensor_tensor(out=mg[:], in0=gl, in1=gmax[:].unsqueeze(2).to_broadcast([128, 16, 4]), op=ALU.is_equal), [lg, gmax], [mg])
            V(lambda e: e.tensor_tensor(out=eg[:], in0=gl, in1=gmax[:].unsqueeze(2).to_broadcast([128, 16, 4]), op=ALU.subtract), [lg, gmax], [eg])
            tr.op("act", lambda e: e.activation(out=eg[:], in_=eg[:], func=AF.Exp), reads=[eg], writes=[eg])
            V(lambda e: e.tensor_reduce(out=gsum[:], in_=eg[:], axis=AX.X, op=ALU.add), [eg], [gsum])
            V(lambda e: e.reciprocal(out=pg[:], in_=gsum[:]), [gsum], [pg])
            V(lambda e: e.tensor_tensor(out=t48[:], in0=el, in1=mg[:].unsqueeze(3).to_broadcast([128, 16, 4, 8]), op=ALU.mult), [lg, mg], [t48])
            V(lambda e: e.tensor_reduce(out=ein[:], in_=t48[:].rearrange("p t g x -> p t x g"), axis=AX.X, op=ALU.add), [t48], [ein])
            V(lambda e: e.tensor_reduce(out=m1[:], in_=ein[:], axis=AX.X, op=ALU.max), [ein], [m1])
            V(lambda e: e.tensor_tensor(out=k1[:], in0=ein[:], in1=m1[:].unsqueeze(2).to_broadcast([128, 16, 8]), op=ALU.is_equal), [ein, m1], [k1])
            V(lambda e: e.scalar_tensor_tensor(out=e2[:], in0=k1[:], scalar=-1.0e30, in1=ein[:], op0=ALU.mult, op1=ALU.add), [k1, ein], [e2])
            V(lambda e: e.tensor_reduce(out=m2[:], in_=e2[:], axis=AX.X, op=ALU.max), [e2], [m2])
            V(lambda e: e.tensor_tensor(out=k2[:], in0=e2[:], in1=m2[:].unsqueeze(2).to_broadcast([128, 16, 8]), op=ALU.is_equal), [e2, m2], [k2])
            V(lambda e: e.tensor_tensor(out=dd_[:], in0=m2[:], in1=m1[:], op=ALU.subtract), [m1, m2], [dd_])
            tr.op("act", lambda e: e.activation(out=dd_[:], in_=dd_[:], func=AF.Exp), reads=[dd_], writes=[dd_])
            V(lambda e: e.tensor_scalar(out=w1[:], in0=dd_[:], scalar1=1.0, scalar2=None, op0=ALU.add), [dd_], [w1])
            V(lambda e: e.reciprocal(out=w1[:], in_=w1[:]), [w1], [w1])
            V(lambda e: e.tensor_tensor(out=w2[:], in0=dd_[:], in1=w1[:], op=ALU.mult), [dd_, w1], [w2])
            V(lambda e: e.tensor_tensor(out=w1[:], in0=w1[:], in1=pg[:], op=ALU.mult), [w1, pg], [w1])
            V(lambda e: e.tensor_tensor(out=w2[:], in0=w2[:], in1=pg[:], op=ALU.mult), [w2, pg], [w2])
            V(lambda e: e.tensor_tensor(out=k1[:], in0=k1[:], in1=w1[:].unsqueeze(2).to_broadcast([128, 16, 8]), op=ALU.mult), [k1, w1], [k1])
            V(lambda e: e.tensor_tensor(out=k2[:], in0=k2[:], in1=w2[:].unsqueeze(2).to_broadcast([128, 16, 8]), op=ALU.mult), [k2, w2], [k2])
            V(lambda e: e.tensor_tensor(out=cw8[:], in0=k1[:], in1=k2[:], op=ALU.add), [k1, k2], [cw8])
            V(lambda e: e.tensor_tensor(out=c["comb"][:].rearrange("p t (g x) -> p t g x", g=4), in0=mg[:].unsqueeze(3).to_broadcast([128, 16, 4, 8]),
                                        in1=cw8[:].unsqueeze(2).to_broadcast([128, 16, 4, 8]), op=ALU.mult), [mg, cw8], [c["comb"]])
            self.tap("comb", c["comb"][:].rearrange("p a b -> p (a b)"), [128, 512], F32, [c["comb"]])
            self.barrier_release(rel)

    def stage_moe(self, st):
        tr, c, I = self.tr, self.c, self.I
        self.fence()
        h_lat, h2T, comb = c["h_lat"], c["h2T"], c["comb"]
        with ExitStack() as s2:
            wgt = [self.sb(s2, "mwg%d" % i, [128, 8, DFF], BF16) for i in range(2)]
            wut = [self.sb(s2, "mwu%d" % i, [128, 8, DFF], BF16) for i in range(2)]
            wdt = [self.sb(s2, "mwd%d" % i, [128, 4, D], BF16) for i in range(2)]
            stg = [self.sb(s2, "mstg%d" % i, [128, 8, DFF], F32) for i in range(2)]
            stgs = [self.dsem() for _ in range(2)]
            ns = [0]
            sg_ = [self.sb(s2, "msg%d" % i, [128, 512], F32) for i in range(2)]
            heT = [self.sb(s2, "heT%d" % i, [128, 4, 512], BF16) for i in range(2)]
            pG = [self.ps(s2, "mpG%d" % i) for i in range(2)]
            pU = [self.ps(s2, "mpU%d" % i) for i in range(2)]
            pDn = [self.ps(s2, "mpD%d" % i) for i in range(4)]
            rel = wgt + wut + wdt + sg_ + heT + pG + pU + pDn + stg
            nb = 0
            for ex in range(NEXP):
                k = ex % 2
                wg_e, wu_e, wd_e = wgt[k], wut[k], wdt[k]
                for (dst, src) in ((wg_e, I["w_gate"].t[ex].rearrange("(kc p) n -> p kc n", p=128)), (wu_e, I["w_up"].t[ex].rearrange("(kc p) n -> p kc n", p=128))):
                    sg_t, sg_s = stg[ns[0] % 2], stgs[ns[0] % 2]
                    ns[0] += 1
                    tr.dma("sp", sg_s, out=sg_t[:], in_=src, writes=[sg_t])
                    tr.op("pool", lambda e, dst=dst, sg_t=sg_t: e.tensor_copy(out=dst[:], in_=sg_t[:]), reads=[sg_t], writes=[dst])
                sg_t, sg_s = stg[ns[0] % 2], stgs[ns[0] % 2]
                ns[0] += 1
                sv = sg_t[:].rearrange("p a b -> p (a b)").rearrange("p (f n) -> p f n", f=4)
                tr.dma("sp", sg_s, out=sv, in_=I["w_down"].t[ex].rearrange("(fc p) n -> p fc n", p=128), writes=[sg_t])
                tr.op("pool", lambda e, wd_e=wd_e, sv=sv: e.tensor_tensor(out=wd_e[:], in0=sv, in1=c["g2_bc"][:].unsqueeze(1).to_broadcast([128, 4, D]), op=ALU.mult),
                      reads=[sg_t, c["g2_bc"]], writes=[wd_e])
                for j in range(4):
                    he = heT[nb % 2]
                    nb += 1
                    hres = [c["h2_r"][j * 4 + q] for q in range(4)]
                    for fc in range(4):
                        g_p, u_p, sg = pG[fc % 2], pU[fc % 2], sg_[fc % 2]
                        for kc in range(8):
                            tr.op("pe", lambda e, kc=kc, fc=fc, g_p=g_p: e.matmul(out=g_p[:], lhsT=wg_e[:, kc, fc * 128:(fc + 1) * 128], rhs=h2T[:, kc, j * 512:(j + 1) * 512],
                                                                               start=(kc == 0), stop=(kc == 7)), reads=[wg_e] + hres, writes=[g_p])
                        for kc in range(8):
                            tr.op("pe", lambda e, kc=kc, fc=fc, u_p=u_p: e.matmul(out=u_p[:], lhsT=wu_e[:, kc, fc * 128:(fc + 1) * 128], rhs=h2T[:, kc, j * 512:(j + 1) * 512],
                                                                               start=(kc == 0), stop=(kc == 7)), reads=[wu_e] + hres, writes=[u_p])
                        tr.op("act", lambda e, g_p=g_p, sg=sg: e.activation(out=sg[:], in_=g_p[:], func=AF.Silu), reads=[g_p], writes=[sg])
                        tr.op("dve", lambda e, u_p=u_p, sg=sg, fc=fc, he=he: e.tensor_tensor(out=he[:, fc, :], in0=u_p[:], in1=sg[:], op=ALU.mult), reads=[u_p, sg], writes=[he])
                    for tt in range(4):
                        ti = j * 4 + tt
                        for hh in range(2):
                            d_p = pDn[(tt * 2 + hh) % 4]
                            for fc in range(4):
                                tr.op("pe", lambda e, fc=fc, d_p=d_p, tt=tt, hh=hh, he=he: e.matmul(out=d_p[:], lhsT=he[:, fc, tt * 128:(tt + 1) * 128], rhs=wd_e[:, fc, hh * 512:(hh + 1) * 512],
                                                                                              start=(fc == 0), stop=(fc == 3)), reads=[he, wd_e], writes=[d_p])
                            tr.op("dve", lambda e, d_p=d_p, ti=ti, hh=hh, ex=ex: e.scalar_tensor_tensor(
                                out=h_lat[:, ti, hh * 512:(hh + 1) * 512], in0=d_p[:], scalar=comb[:, ti, ex:ex + 1], in1=h_lat[:, ti, hh * 512:(hh + 1) * 512], op0=ALU.mult, op1=ALU.add),
                                reads=[d_p, comb, c["h_r"][ti]], writes=[c["h_r"][ti]])
            self.tap("h_fin", h_lat[:].rearrange("p a b -> p (a b)"), [128, 16 * D], F32, c["h_r"])
            self.barrier_release(rel)

    def stage_final(self, st):
        tr, c, I = self.tr, self.c, self.I
        self.fence()
        h_lat = c["h_lat"]
        out_v = self.out.t.rearrange("(n p) c -> n p c", p=128)
        with ExitStack() as s2:
            fn = self.sb(s2, "fn_bc", [128, D], F32)
            c_eps = self.sb(s2, "c_eps4", [128, 1], F32)
            junk = self.sb(s2, "fjunk", [128, D], BF16)
            ss = [self.sb(s2, "fss%d" % i, [128, 1], F32) for i in range(2)]
            rs = [self.sb(s2, "frs%d" % i, [128, 1], F32) for i in range(2)]
            ob = [self.sb(s2, "fob%d" % i, [128, D], F32) for i in range(2)]
            obs = [self.dsem() for _ in range(2)]
            tr.dma("sp", self.dsem(), out=fn[:], in_=I["final_norm"].t.partition_broadcast(128), writes=[fn])
            tr.op("pool", lambda e: e.memset(c_eps[:], EPS), writes=[c_eps])
            for i in range(16):
                hl = h_lat[:, i, :]
                s_, r_, o_ = ss[i % 2], rs[i % 2], ob[i % 2]
                tr.op("act", lambda e, s_=s_, hl=hl: e.activation(out=junk[:], in_=hl, func=AF.Square, accum_out=s_[:]), reads=[c["h_r"][i]], writes=[junk, s_])
                tr.op("act", lambda e, s_=s_, r_=r_: e.activation(out=r_[:], in_=s_[:], func=AF.Ln, scale=1.0 / D, bias=c_eps[:]), reads=[s_, c_eps], writes=[r_])
                tr.op("act", lambda e, r_=r_: e.activation(out=r_[:], in_=r_[:], func=AF.Exp, scale=-0.5), reads=[r_], writes=[r_])
                tr.op("dve", lambda e, r_=r_, o_=o_, hl=hl: e.scalar_tensor_tensor(out=o_[:], in0=hl, scalar=r_[:], in1=fn[:], op0=ALU.mult, op1=ALU.mult),
                      reads=[c["h_r"][i], r_, fn], writes=[o_])
                tr.dma("sp", obs[i % 2], out=out_v[i], in_=o_[:], reads=[o_], writes=[])
            self.final += ob


def prep_core(inp, b, hf):
    L = 0
    rev = hf == 1
    x, ctx = inp["x"][b], inp["ctx"][b]
    if not rev:
        ctx_a, own, oth = ctx, x[0:2048], x[2048:4096]
        dA, dB = 0, 1
    else:
        ctx_a, own, oth = ctx[::-1], x[2048:4096][::-1], x[0:2048][::-1]
        dA, dB = 1, 0
    m = {}
    m["xs"] = np.ascontiguousarray(np.concatenate([ctx_a, own, oth], axis=0), dtype=np.float32)
    cT = np.stack([inp["c"][b].reshape(8, 128).T, inp["c_ctx"].reshape(8, 128).T], axis=2).reshape(128, 16)
    m["cT"] = np.ascontiguousarray(cT, dtype=np.float32)
    m["w_ada"] = np.ascontiguousarray(inp["w_ada"][L])
    m["b_ada"] = np.ascontiguousarray(inp["b_ada"][L].reshape(1, -1))
    m["norm_mix_fm"] = np.ascontiguousarray(inp["norm_mix"][L].reshape(8, 128).T)
    m["norm_ffn_fm"] = np.ascontiguousarray(inp["norm_ffn"][L].reshape(8, 128).T)
    w_in = inp["w_in"][L]
    if rev:
        w_in = np.concatenate([w_in[:, :OFF_G], w_in[:, OFF_G + 16:OFF_G + 32], w_in[:, OFF_G:OFF_G + 16],
                               w_in[:, OFF_Z:OFF_DT], w_in[:, OFF_DT + 16:OFF_DT + 32], w_in[:, OFF_DT:OFF_DT + 16]], axis=1)
    m["w_in"] = np.ascontiguousarray(w_in)
    wu, gb = inp["gla_w_up"][L], inp["gla_b"][L]
    m["w_up_aug"] = np.ascontiguousarray(np.stack([np.concatenate([wu[dA], gb[dA][None, :]], axis=0),
                                                   np.concatenate([wu[dB], gb[dB][None, :]], axis=0)], axis=0))
    m["gla_norm"] = np.ascontiguousarray(inp["gla_norm"][L].reshape(1, -1))
    m["ssd_norm"] = np.ascontiguousarray(inp["ssd_norm"][L].reshape(1, -1))
    m["final_norm"] = np.ascontiguousarray(inp["final_norm"].reshape(1, -1))
    cw = inp["ssd_conv_w"][L]
    if rev:
        cw = cw[::-1, ::-1, :]
    m["conv_w_fm"] = np.ascontiguousarray(cw.reshape(9, 12, 128).transpose(2, 1, 0))
    m["conv_b_fm"] = np.ascontiguousarray(inp["ssd_conv_b"][L].reshape(12, 128).T)
    m["dt_bias"] = np.ascontiguousarray(np.concatenate([inp["ssd_dt_bias"][L][dA], inp["ssd_dt_bias"][L][dB]]).reshape(1, 32))
    m["a_log"] = np.ascontiguousarray(np.concatenate([inp["ssd_a_log"][L][dA], inp["ssd_a_log"][L][dB]]).reshape(1, 32))
    m["ssd_d"] = np.ascontiguousarray(inp["ssd_d"][L].reshape(1, 16))
    m["w_out"] = np.ascontiguousarray(inp["w_out"][L])
    wrt = np.concatenate([inp["router_group_w"][L], inp["router_expert_w"][L]], axis=1)
    m["w_router"] = np.ascontiguousarray(wrt.reshape(8, 128, 36).transpose(1, 0, 2).reshape(128, 8 * 36))
    m["b_router"] = np.ascontiguousarray(np.concatenate([inp["router_group_b"][L], inp["router_expert_b"][L]]).reshape(1, 36))
    m["w_gate"] = np.ascontiguousarray(inp["expert_w_gate"][L])
    m["w_up"] = np.ascontiguousarray(inp["expert_w_up"][L])
    m["w_down"] = np.ascontiguousarray(inp["expert_w_down"][L])
    return {k: np.asarray(v, dtype=np.float32) for k, v in m.items()}


def run(inputs, debug=None, stop_after=None, cores=8):
    bld = Builder(debug=debug, stop_after=stop_after)
    nc = bld.build()
    in_maps = [prep_core(inputs, i // 2, i % 2) for i in range(cores)]
    res = run_bass_kernel_spmd(nc, in_maps, core_ids=list(range(cores)))
    return res, bld


def kernel(**inputs):
    inputs = {k: np.asarray(v) for k, v in inputs.items()}
    res, _ = run(inputs)
    out = np.empty((4, 4096, D), dtype=np.float32)
    for i in range(8):
        b, hf = i // 2, i % 2
        o = np.asarray(res.results[i]["out"], dtype=np.float32)
        if hf == 0:
            out[b, 0:2048] = o
        else:
            out[b, 2048:4096] = o[::-1]
    return out
```

```python
import math
from contextlib import ExitStack

import numpy as np
import concourse.bass as bass
import concourse.mybir as mybir
from concourse.bass_utils import run_bass_kernel_spmd

F32 = mybir.dt.float32
BF16 = mybir.dt.bfloat16
AF = mybir.ActivationFunctionType
ALU = mybir.AluOpType
AX = mybir.AxisListType

D = 1024
NCTX, NOWN, NOTH = 256, 2048, 2048
TOK = NCTX + NOWN + NOTH
NT = TOK // 128
T_CTX0, T_OWN0, T_OTH0 = 0, 2, 18
EPS = 1e-6
IN_W = 5696
OFF_K, OFF_V, OFF_R, OFF_G, OFF_Z, OFF_XBC, OFF_DT = 512, 1024, 2048, 3072, 3104, 4128, 5664
NEXP, DFF = 32, 512


class Res:
    __slots__ = ("name", "lw", "rd")

    def __init__(self, name=""):
        self.name = name
        self.lw = None
        self.rd = {}


class Tile:
    def __init__(self, t, name):
        self.t = t
        self.r = Res(name)

    def __getitem__(self, idx):
        return self.t[idx]


class Tracker:
    ENG = ("pe", "act", "dve", "pool", "sp")
    CH = 2000

    def __init__(self, nc, sems, dma_sems, same_engine_sync=True):
        self.nc = nc
        self.eng = {"pe": nc.tensor, "act": nc.scalar, "dve": nc.vector, "pool": nc.gpsimd, "sp": nc.sync}
        self.cnt = {e: 0 for e in self.ENG}
        self.waited = {e: {} for e in self.ENG}
        self.sems = {e: [sems[e]] for e in sems}
        self.free_dma = list(dma_sems)
        self.same = same_engine_sync
        self.ninst = 0
        self.rec = None

    def new_dma_sem(self, group=0):
        d = self.free_dma.pop()
        self._uid = getattr(self, "_uid", 0) + 1
        d = d if isinstance(d, list) else [d, 0, 0, 0, "dma%d" % self._uid]
        if group:
            d[2] = group
            d[3] = d[1] + 16 * group
        return d

    def regroup(self, d, n):
        if self.rec is not None:
            self.rec.append(("call", lambda: self.regroup(d, n)))
            return
        assert d[2] == 0
        d[2] = n
        d[3] = d[1] + 16 * n

    def begin_record(self):
        self.rec = []

    def end_record(self):
        r, self.rec = self.rec, None
        return r

    def mark(self, name):
        self.rec.append(("mark", name))

    def _emit_item(self, it):
        if it[0] == "op":
            self.op(*it[1:])
        elif it[0] == "dma":
            self.dma(*it[1:])
        elif it[0] == "call":
            it[1]()

    def run_pipelined(self, records, depth=2, serial_fronts=False):
        assert self.rec is None
        active = []
        nxt = 0
        finished = -1
        sdone = -1
        while active or nxt < len(records):
            while len(active) < depth and nxt < len(records):
                if serial_fronts and active and not active[-1][3]:
                    break
                active.append([records[nxt], 0, nxt, False])
                nxt += 1
            progressed = False
            for a in list(active):
                lst, pos, idx, _ = a
                if pos >= len(lst):
                    a[3] = True
                    active.remove(a)
                    finished = max(finished, idx)
                    sdone = max(sdone, idx)
                    progressed = True
                    continue
                it = lst[pos]
                if it[0] == "mark":
                    if it[1] == "need_state":
                        a[3] = True
                        if sdone < idx - 1:
                            continue
                    if it[1] == "state_done":
                        sdone = max(sdone, idx)
                    a[1] += 1
                    progressed = True
                    continue
                self._emit_item(it)
                a[1] += 1
                progressed = True
            assert progressed

    def release_dma_sem(self, d):
        self.free_dma.append(d)

    def _wait(self, e, ev):
        if ev is None:
            return
        if ev[0] == "dma":
            _, s, v, key = ev
            if self.waited[e].get(key, 0) >= v:
                return
            self.waited[e][key] = v
            self.eng[e].wait_ge(s, v)
        else:
            pe, n = ev
            if pe == e and (not self.same or e in ("pe", "sp")):
                return
            if self.waited[e].get(pe, 0) >= n:
                return
            self.waited[e][pe] = n
            self.eng[e].wait_ge(self.sems[pe][(n - 1) // self.CH], (n - 1) % self.CH + 1)

    def _deps(self, e, reads, writes):
        for r in reads:
            self._wait(e, r.lw)
        for w in writes:
            self._wait(e, w.lw)
            for ev in w.rd.values():
                self._wait(e, ev)

    @staticmethod
    def _note_read(r, ev):
        key = ev[3] if ev[0] == "dma" else ev[0]
        old = r.rd.get(key)
        if old is None or (old[2] if old[0] == "dma" else old[1]) < (ev[2] if ev[0] == "dma" else ev[1]):
            r.rd[key] = ev

    def op(self, e, fn, reads=(), writes=()):
        if self.rec is not None:
            self.rec.append(("op", e, fn, list(reads), list(writes)))
            return None
        reads = [x.r if isinstance(x, Tile) else x for x in reads]
        writes = [x.r if isinstance(x, Tile) else x for x in writes]
        self._deps(e, reads, writes)
        self.cnt[e] += 1
        ev = (e, self.cnt[e])
        k = (self.cnt[e] - 1) // self.CH
        if k >= len(self.sems[e]):
            self.sems[e].append(self.free_dma.pop(0))
        fn(self.eng[e]).then_inc(self.sems[e][k], 1)
        self.ninst += 1
        for r in reads:
            self._note_read(r, ev)
        for w in writes:
            w.lw = ev
            w.rd = {}
        return ev

    def dma(self, e, dsem, out, in_, reads=(), writes=()):
        if self.rec is not None:
            self.rec.append(("dma", e, dsem, out, in_, list(reads), list(writes)))
            return None
        reads = [x.r if isinstance(x, Tile) else x for x in reads]
        writes = [x.r if isinstance(x, Tile) else x for x in writes]
        self._deps(e, reads, writes)
        if dsem[2] == 0:
            dsem[2] = 1
            dsem[3] = dsem[1] + 16
        dsem[1] += 16
        dsem[2] -= 1
        ev = ("dma", dsem[0], dsem[3], dsem[4])
        self.eng[e].dma_start(out=out, in_=in_).then_inc(dsem[0], 16)
        self.ninst += 1
        for r in reads:
            self._note_read(r, ev)
        for w in writes:
            w.lw = ev
            w.rd = {}
        return ev

    def wait_all(self, e, resources):
        for r in resources:
            r = r.r if isinstance(r, Tile) else r
            self._wait(e, r.lw)
            for ev in r.rd.values():
                self._wait(e, ev)


class Builder:
    def __init__(self, debug=None, stop_after=None):
        self.debug = debug or ()
        self.stop_after = stop_after
        self.nc = bass.Bass("TRN2", target_bir_lowering=False)
        self.dbg_out = {}

    def sb(self, st, name, shape, dt):
        self._uid = getattr(self, "_uid", 0) + 1
        return Tile(st.enter_context(self.nc.sbuf_tensor("sb%d_%s" % (self._uid, name), list(shape), dt)), name)

    def ps(self, st, name, shape=(128, 512), dt=F32):
        self._uid = getattr(self, "_uid", 0) + 1
        return Tile(st.enter_context(self.nc.psum_tensor("ps%d_%s" % (self._uid, name), list(shape), dt)), name)

    def dram_in(self, name, shape, dt=F32):
        return Tile(self.nc.dram_tensor(name, list(shape), dt, kind="ExternalInput").ap(), name)

    def dram_out(self, name, shape, dt=F32):
        return Tile(self.nc.dram_tensor(name, list(shape), dt, kind="ExternalOutput").ap(), name)

    def dram_scr(self, name, shape, dt):
        return Tile(self.nc.dram_tensor(name, list(shape), dt, kind="Internal").ap(), name)

    def dsem(self, group=0):
        return self.tr.new_dma_sem(group)

    def build(self):
        nc = self.nc
        I = {}
        I["xs"] = self.dram_in("xs", [TOK, D])
        I["cT"] = self.dram_in("cT", [128, 16])
        I["w_ada"] = self.dram_in("w_ada", [D, 6 * D])
        I["b_ada"] = self.dram_in("b_ada", [1, 6 * D])
        I["norm_mix_fm"] = self.dram_in("norm_mix_fm", [128, 8])
        I["norm_ffn_fm"] = self.dram_in("norm_ffn_fm", [128, 8])
        I["w_in"] = self.dram_in("w_in", [D, IN_W])
        I["w_up_aug"] = self.dram_in("w_up_aug", [2, 17, 512])
        I["gla_norm"] = self.dram_in("gla_norm", [1, 256])
        I["ssd_norm"] = self.dram_in("ssd_norm", [1, 1024])
        I["final_norm"] = self.dram_in("final_norm", [1, 1024])
        I["conv_w_fm"] = self.dram_in("conv_w_fm", [128, 12, 9])
        I["conv_b_fm"] = self.dram_in("conv_b_fm", [128, 12])
        I["dt_bias"] = self.dram_in("dt_bias", [1, 32])
        I["a_log"] = self.dram_in("a_log", [1, 32])
        I["ssd_d"] = self.dram_in("ssd_d", [1, 16])
        I["w_out"] = self.dram_in("w_out", [2048, D])
        I["w_router"] = self.dram_in("w_router", [128, 8 * 36])
        I["b_router"] = self.dram_in("b_router", [1, 36])
        I["w_gate"] = self.dram_in("w_gate", [NEXP, D, DFF])
        I["w_up"] = self.dram_in("w_up", [NEXP, D, DFF])
        I["w_down"] = self.dram_in("w_down", [NEXP, DFF, D])
        self.I = I
        self.out = self.dram_out("out", [NOWN, D])

        with ExitStack() as st:
            sems = {e: st.enter_context(nc.semaphore("s_" + e)) for e in Tracker.ENG}
            dsems = [st.enter_context(nc.semaphore("d%d" % i)) for i in range(90)]
            self.tr = Tracker(nc, sems, dsems)
            self.program(st)
        return nc

    def tap(self, name, tile_ap, shape, dt, reads):
        if name not in self.debug:
            return
        o = self.dram_out("dbg_" + name, shape, dt)
        self.dbg_out[name] = o
        n = shape[1]
        step = 2048
        d = self.dsem(len(range(0, n, step)))
        for c0 in range(0, n, step):
            c1 = min(n, c0 + step)
            self.tr.dma("sp", d, out=o.t[:, c0:c1], in_=tile_ap[:, c0:c1], reads=reads, writes=[o])
        self.final.append(o)

    def program(self, st):
        tr = self.tr
        self.final = []
        self.consts(st)
        self.stage_adaln(st)
        with ExitStack() as mst:
            self.stage_hT(mst)
            if self.stop_after == "hT":
                return self.finish()
            self.stage_gla(mst)
            if self.stop_after == "gla":
                return self.finish()
            self.stage_conv(mst)
            if self.stop_after == "conv":
                return self.finish()
            self.stage_ssd(mst)
            if self.stop_after == "ssd":
                return self.finish()
            self.barrier_release([self.c["hT"], self.c["BT"], self.c["CT"]] + self.c["hT_r"])
        self.stage_post(st)
        if self.stop_after == "post":
            return self.finish()
        self.stage_moe(st)
        if self.stop_after == "moe":
            return self.finish()
        self.stage_final(st)
        return self.finish()

    def finish(self):
        self.tr.wait_all("sp", self.final)

    def consts(self, st):
        tr = self.tr
        c = {}
        self.c = c
        c["ident_f"] = self.sb(st, "ident_f", [128, 128], F32)
        c["ident_b"] = self.sb(st, "ident_b", [128, 128], BF16)
        c["ones_f"] = self.sb(st, "ones_f", [128, 128], F32)
        for nm in ("tri_le", "tri_ge", "tri_gt", "tri_lt"):
            c[nm] = self.sb(st, nm, [128, 128], F32)
        idf = c["ident_f"]
        tr.op("pool", lambda e: e.memset(idf[:], 0.0), writes=[idf])
        tr.op("pool", lambda e: e.affine_select(out=idf[:], in_=idf[:], pattern=[[-1, 128]], compare_op=ALU.not_equal,
                                               fill=1.0, base=0, channel_multiplier=1), reads=[idf], writes=[idf])
        tr.op("pool", lambda e: e.tensor_copy(out=c["ident_b"][:], in_=idf[:]), reads=[idf], writes=[c["ident_b"]])
        tr.op("pool", lambda e: e.memset(c["ones_f"][:], 1.0), writes=[c["ones_f"]])
        specs = {"tri_le": (ALU.is_gt, 0), "tri_ge": (ALU.is_gt, 0), "tri_gt": (ALU.is_gt, 0), "tri_lt": (ALU.is_gt, 0)}
        t = c["tri_le"]
        tr.op("pool", lambda e: e.memset(t[:], 1.0), writes=[t])
        tr.op("pool", lambda e: e.affine_select(out=t[:], in_=t[:], pattern=[[1, 128]], compare_op=ALU.is_ge,
                                               fill=0.0, base=0, channel_multiplier=-1), reads=[t], writes=[t])
        t2 = c["tri_ge"]
        tr.op("pool", lambda e: e.memset(t2[:], 1.0), writes=[t2])
        tr.op("pool", lambda e: e.affine_select(out=t2[:], in_=t2[:], pattern=[[-1, 128]], compare_op=ALU.is_ge,
                                               fill=0.0, base=0, channel_multiplier=1), reads=[t2], writes=[t2])
        t3 = c["tri_gt"]
        tr.op("pool", lambda e: e.memset(t3[:], 1.0), writes=[t3])
        tr.op("pool", lambda e: e.affine_select(out=t3[:], in_=t3[:], pattern=[[-1, 128]], compare_op=ALU.is_gt,
                                               fill=0.0, base=0, channel_multiplier=1), reads=[t3], writes=[t3])
        t4 = c["tri_lt"]
        tr.op("pool", lambda e: e.memset(t4[:], 1.0), writes=[t4])
        tr.op("pool", lambda e: e.affine_select(out=t4[:], in_=t4[:], pattern=[[1, 128]], compare_op=ALU.is_gt,
                                               fill=0.0, base=0, channel_multiplier=-1), reads=[t4], writes=[t4])
        self.tap("tri_le", c["tri_le"][:], [128, 128], F32, [c["tri_le"]])
        self.tap("tri_gt", c["tri_gt"][:], [128, 128], F32, [c["tri_gt"]])

    def stage_adaln(self, st):
        tr, c, I = self.tr, self.c, self.I
        c["mod_fm"] = self.sb(st, "mod_fm", [128, 6, 8, 2], F32)
        c["g1_bc"] = self.sb(st, "g1_bc", [128, D], F32)
        c["g2_bc"] = self.sb(st, "g2_bc", [128, D], F32)
        c["s1"] = self.sb(st, "s1", [128, 8], F32)
        c["s1c"] = self.sb(st, "s1c", [128, 8], F32)
        c["b1"] = self.sb(st, "b1", [128, 8], F32)
        c["b1c"] = self.sb(st, "b1c", [128, 8], F32)
        c["s2"] = self.sb(st, "s2", [128, 8], F32)
        c["b2"] = self.sb(st, "b2", [128, 8], F32)
        with ExitStack() as s2:
            cT = self.sb(s2, "cT", [128, 16], F32)
            scT = self.sb(s2, "scT", [128, 16], F32)
            sc_rep = self.sb(s2, "sc_rep", [128, 8, 128], F32)
            brow = self.sb(s2, "brow", [1, 6 * D], F32)
            nm = self.sb(s2, "nm", [128, 8], F32)
            nf = self.sb(s2, "nf", [128, 8], F32)
            wblk = [self.sb(s2, "wblk%d" % i, [128, 8, D], F32) for i in range(2)]
            wsem = [self.dsem() for _ in range(2)]
            modps = self.ps(s2, "modps", [128, 512], F32)
            gps = [self.ps(s2, "gps%d" % i, [128, 512], F32) for i in range(2)]
            d = self.dsem(4)
            tr.dma("sp", d, out=cT[:], in_=I["cT"].t, writes=[cT])
            tr.dma("sp", d, out=brow[:], in_=I["b_ada"].t, writes=[brow])
            tr.dma("sp", d, out=nm[:], in_=I["norm_mix_fm"].t, writes=[nm])
            tr.dma("sp", d, out=nf[:], in_=I["norm_ffn_fm"].t, writes=[nf])
            tr.op("act", lambda e: e.activation(out=scT[:], in_=cT[:], func=AF.Silu), reads=[cT], writes=[scT])
            tr.op("dve", lambda e: e.tensor_copy(out=sc_rep[:], in_=scT[:].rearrange("p (k j) -> p k j", j=2)[:, :, 0:1].to_broadcast([128, 8, 128])),
                  reads=[scT], writes=[sc_rep])
            w_ada = I["w_ada"].t.rearrange("(kc p) n -> p kc n", p=128)
            mview = modps[:, 0:96].rearrange("p (b f t) -> p b f t", b=6, f=8)
            for blk in range(6):
                wb = wblk[blk % 2]
                tr.dma("sp", wsem[blk % 2], out=wb[:], in_=w_ada[:, :, blk * D:(blk + 1) * D], writes=[wb])
                if blk in (0, 1, 3, 4):
                    for fc in range(8):
                        for kc in range(8):
                            tr.op("pe", lambda e, fc=fc, kc=kc, wb=wb, blk=blk: e.matmul(
                                out=mview[:, blk, fc, :], lhsT=wb[:, kc, fc * 128:(fc + 1) * 128],
                                rhs=scT[:, 2 * kc:2 * kc + 2], start=(kc == 0), stop=False),
                                reads=[wb, scT], writes=[modps])
                        tr.op("pe", lambda e, fc=fc, blk=blk: e.matmul(
                            out=mview[:, blk, fc, :], lhsT=brow[0:1, blk * D + fc * 128: blk * D + (fc + 1) * 128],
                            rhs=c["ones_f"][0:1, 0:2], start=False, stop=True),
                            reads=[brow, c["ones_f"]], writes=[modps])
                else:
                    gdst = c["g1_bc"] if blk == 2 else c["g2_bc"]
                    for hh in range(2):
                        for kc in range(8):
                            tr.op("pe", lambda e, hh=hh, kc=kc, wb=wb: e.matmul(
                                out=gps[hh][:], lhsT=sc_rep[:, kc, :], rhs=wb[:, kc, hh * 512:(hh + 1) * 512],
                                start=(kc == 0), stop=False), reads=[wb, sc_rep], writes=[gps[hh]])
                        tr.op("pe", lambda e, hh=hh, blk=blk: e.matmul(
                            out=gps[hh][:], lhsT=c["ones_f"][0:1, :], rhs=brow[0:1, blk * D + hh * 512: blk * D + (hh + 1) * 512],
                            start=False, stop=True), reads=[brow, c["ones_f"]], writes=[gps[hh]])
                        tr.op("act", lambda e, hh=hh, gdst=gdst: e.activation(out=gdst[:, hh * 512:(hh + 1) * 512], in_=gps[hh][:], func=AF.Copy),
                              reads=[gps[hh]], writes=[gdst])
            mf = c["mod_fm"]
            mflat = mf[:].rearrange("p b f t -> p (b f t)")
            tr.op("dve", lambda e: e.tensor_copy(out=mflat[:, 0:32], in_=modps[:, 0:32]), reads=[modps], writes=[mf])
            tr.op("dve", lambda e: e.tensor_copy(out=mflat[:, 48:80], in_=modps[:, 48:80]), reads=[modps], writes=[mf])
            tr.op("dve", lambda e: e.scalar_tensor_tensor(out=c["s1"][:], in0=mf[:, 1, :, 0], scalar=1.0, in1=nm[:], op0=ALU.add, op1=ALU.mult),
                  reads=[mf, nm], writes=[c["s1"]])
            tr.op("dve", lambda e: e.scalar_tensor_tensor(out=c["s1c"][:], in0=mf[:, 1, :, 1], scalar=1.0, in1=nm[:], op0=ALU.add, op1=ALU.mult),
                  reads=[mf, nm], writes=[c["s1c"]])
            tr.op("dve", lambda e: e.scalar_tensor_tensor(out=c["s2"][:], in0=mf[:, 4, :, 0], scalar=1.0, in1=nf[:], op0=ALU.add, op1=ALU.mult),
                  reads=[mf, nf], writes=[c["s2"]])
            tr.op("dve", lambda e: e.tensor_copy(out=c["b1"][:], in_=mf[:, 0, :, 0]), reads=[mf], writes=[c["b1"]])
            tr.op("dve", lambda e: e.tensor_copy(out=c["b1c"][:], in_=mf[:, 0, :, 1]), reads=[mf], writes=[c["b1c"]])
            tr.op("dve", lambda e: e.tensor_copy(out=c["b2"][:], in_=mf[:, 3, :, 0]), reads=[mf], writes=[c["b2"]])
            self.tap("mod_fm", mf[:].rearrange("p b f t -> p (b f t)"), [128, 96], F32, [mf])
            self.tap("g1_bc", c["g1_bc"][:], [128, D], F32, [c["g1_bc"]])
            self.barrier_release([cT, scT, sc_rep, brow, nm, nf, wblk[0], wblk[1], modps, gps[0], gps[1]])

    def barrier_release(self, tiles):
        self.pending = getattr(self, "pending", [])
        for t in tiles:
            self.pending.append(t.r if isinstance(t, Tile) else t)

    def fence(self):
        pend = getattr(self, "pending", [])
        for e in Tracker.ENG:
            self.tr.wait_all(e, pend)
        self.pending = []

    def stage_hT(self, st):
        tr, c, I = self.tr, self.c, self.I
        self.fence()
        c["hT"] = self.sb(st, "hT", [128, 8, TOK], BF16)
        c["hT_r"] = [Res("hT%d" % t) for t in range(NT)]
        with ExitStack() as s2:
            xr = [self.sb(s2, "xr%d" % i, [128, D], F32) for i in range(3)]
            xsem = [self.dsem() for _ in range(3)]
            junk = self.sb(s2, "junk", [128, D], BF16)
            ss = [self.sb(s2, "ss%d" % i, [128, 1], F32) for i in range(3)]
            rstd = [self.sb(s2, "rstd%d" % i, [128, 1], F32) for i in range(3)]
            xn = [self.sb(s2, "xn%d" % i, [128, D], BF16) for i in range(2)]
            tmp = [self.sb(s2, "tmp%d" % i, [128, 8, 128], F32) for i in range(2)]
            tps = [self.ps(s2, "tps%d" % i, [128, 1024], BF16) for i in range(2)]
            epst = self.sb(s2, "epst", [128, 1], F32)
            tr.op("pool", lambda e: e.memset(epst[:], EPS), writes=[epst])
            rel = xr + ss + rstd + xn + tmp + tps + [junk, epst]
            for t in range(NT):
                x_t, ss_t, rs_t, xn_t, tmp_t, ps_t = xr[t % 3], ss[t % 3], rstd[t % 3], xn[t % 2], tmp[t % 2], tps[t % 2]
                hT_ap = c["hT"][:, :, t * 128:(t + 1) * 128]
                hT_r = c["hT_r"][t]
                isctx = t < T_OWN0
                sc, sh = (c["s1c"], c["b1c"]) if isctx else (c["s1"], c["b1"])
                tr.dma("sp", xsem[t % 3], out=x_t[:], in_=I["xs"].t[t * 128:(t + 1) * 128, :], writes=[x_t])
                tr.op("act", lambda e, x_t=x_t, ss_t=ss_t: e.activation(out=junk[:], in_=x_t[:], func=AF.Square, accum_out=ss_t[:]),
                      reads=[x_t], writes=[junk, ss_t])
                tr.op("act", lambda e, ss_t=ss_t, rs_t=rs_t: e.activation(out=rs_t[:], in_=ss_t[:], func=AF.Ln, scale=1.0 / D, bias=epst[:]),
                      reads=[ss_t, epst], writes=[rs_t])
                tr.op("act", lambda e, rs_t=rs_t: e.activation(out=rs_t[:], in_=rs_t[:], func=AF.Exp, scale=-0.5),
                      reads=[rs_t], writes=[rs_t])
                tr.op("dve", lambda e, x_t=x_t, rs_t=rs_t, xn_t=xn_t: e.tensor_scalar(out=xn_t[:], in0=x_t[:], scalar1=rs_t[:], scalar2=None, op0=ALU.mult),
                      reads=[x_t, rs_t], writes=[xn_t])
                for kc in range(8):
                    tr.op("pe", lambda e, kc=kc, xn_t=xn_t, ps_t=ps_t: e.transpose(out=ps_t[:, kc * 128:(kc + 1) * 128], in_=xn_t[:, kc * 128:(kc + 1) * 128], identity=c["ident_b"][:]),
                          reads=[xn_t, c["ident_b"]], writes=[ps_t])
                tr.op("dve", lambda e, ps_t=ps_t, tmp_t=tmp_t, sc=sc: e.tensor_tensor(
                    out=tmp_t[:], in0=ps_t[:].rearrange("p (k t) -> p k t", k=8), in1=sc[:].unsqueeze(2).to_broadcast([128, 8, 128]), op=ALU.mult),
                    reads=[ps_t, sc], writes=[tmp_t])
                tr.op("pool", lambda e, tmp_t=tmp_t, hT_ap=hT_ap, sh=sh: e.tensor_tensor(
                    out=hT_ap, in0=tmp_t[:], in1=sh[:].unsqueeze(2).to_broadcast([128, 8, 128]), op=ALU.add),
                    reads=[tmp_t, sh], writes=[hT_r])
            for t in (0, 2, 17, 33):
                if ("hT%d" % t) in self.debug:
                    o = self.dram_out("dbg_hT%d" % t, [128, 8, 128], BF16)
                    tr.dma("sp", self.dsem(), out=o.t, in_=c["hT"][:, :, t * 128:(t + 1) * 128], reads=[c["hT_r"][t]], writes=[o])
                    self.final.append(o)
            self.barrier_release(rel)

    def scratch(self, name, shape, dt):
        if name in self.debug:
            o = self.dram_out("dbg_" + name, shape, dt)
            self.final.append(o)
            return o
        return self.dram_scr(name, shape, dt)

    def stage_conv(self, st):
        tr, c, I = self.tr, self.c, self.I
        self.fence()
        c["x_tok"] = self.scratch("x_tok", [TOK, 1024], BF16)
        c["B_tok"] = self.scratch("B_tok", [TOK, 256], BF16)
        c["BT"] = self.sb(st, "BT", [128, 2, NOWN], BF16)
        c["CT"] = self.sb(st, "CT", [128, 2, NOWN], BF16)
        xtok_v = c["x_tok"].t.rearrange("(n p) c -> p n c", p=128)
        btok_v = c["B_tok"].t.rearrange("(n p) c -> p n c", p=128)
        w_in_v = I["w_in"].t.rearrange("(kc p) n -> p kc n", p=128)
        with ExitStack() as s2:
            wx = [self.sb(s2, "wx%d" % i, [128, 8, 128], BF16) for i in range(2)]
            wxs = [self.dsem() for _ in range(2)]
            cw = self.sb(s2, "cw", [128, 12, 9], F32)
            cb = self.sb(s2, "cb", [128, 12], F32)
            diag = [self.sb(s2, "diag%d" % i, [128, 9, 128], BF16) for i in range(2)]
            pre = [self.sb(s2, "pre%d" % i, [128, 66, 66], BF16) for i in range(2)]
            prec = [self.sb(s2, "prec%d" % i, [128, 258], BF16) for i in range(2)]
            post = [self.sb(s2, "post%d" % i, [128, 512], BF16) for i in range(3)]
            tst = [self.sb(s2, "tst%d" % i, [128, 4, 128], BF16) for i in range(3)]
            tsem = [self.dsem() for _ in range(3)]
            pp = [self.ps(s2, "pp%d" % i) for i in range(2)]
            pc = [self.ps(s2, "pc%d" % i) for i in range(2)]
            pt = [self.ps(s2, "pt%d" % i, [128, 1024], BF16) for i in range(2)]
            rel = wx + diag + pre + prec + post + tst + pp + pc + pt + [cw, cb]
            d0 = self.dsem(2)
            tr.dma("sp", d0, out=cw[:], in_=I["conv_w_fm"].t, writes=[cw])
            tr.dma("sp", d0, out=cb[:], in_=I["conv_b_fm"].t, writes=[cb])
            for i in range(2):
                tr.op("pool", lambda e, i=i: e.memset(pre[i][:], 0.0), writes=[pre[i]])
                tr.op("pool", lambda e, i=i: e.memset(prec[i][:], 0.0), writes=[prec[i]])
            nev = 0
            npost = 0
            for ct in range(12):
                w, dg, pr, prc = wx[ct % 2], diag[ct % 2], pre[ct % 2], prec[ct % 2]
                tr.dma("pool", wxs[ct % 2], out=w[:], in_=w_in_v[:, :, OFF_XBC + ct * 128: OFF_XBC + (ct + 1) * 128], writes=[w])
                tr.op("pool", lambda e, dg=dg, ct=ct: e.tensor_tensor(out=dg[:], in0=c["ident_f"][:].unsqueeze(1).to_broadcast([128, 9, 128]),
                                                                  in1=cw[:, ct, :].unsqueeze(2).to_broadcast([128, 9, 128]), op=ALU.mult),
                      reads=[c["ident_f"], cw], writes=[dg])
                for blk in range(9):
                    p_t = pp[nev % 2]
                    if blk == 0:
                        n, tok0, trs = 256, 0, [0, 1]
                    else:
                        n, tok0 = 512, NCTX + (blk - 1) * 512
                        trs = list(range(T_OWN0 + (blk - 1) * 4, T_OWN0 + blk * 4))
                    for kc in range(8):
                        tr.op("pe", lambda e, kc=kc, p_t=p_t, w=w, n=n, tok0=tok0: e.matmul(
                            out=p_t[:, 0:n], lhsT=w[:, kc, :], rhs=c["hT"][:, kc, tok0:tok0 + n], start=(kc == 0), stop=(kc == 7)),
                            reads=[w] + [c["hT_r"][t] for t in trs], writes=[p_t])
                    if blk == 0:
                        dst = prc[:, 1:257]
                        src = p_t[:, 0:256]
                        wr = prc
                    else:
                        r0 = (blk - 1) * 8
                        dst = pr[:, r0 + 1:r0 + 9, 1:65]
                        src = p_t[:, 0:512].rearrange("p (r q) -> p r q", q=64)
                        wr = pr
                    eng = "act" if nev % 2 == 0 else "dve"
                    if eng == "act":
                        tr.op("act", lambda e, dst=dst, src=src: e.activation(out=dst, in_=src, func=AF.Copy), reads=[p_t], writes=[wr])
                    else:
                        tr.op("dve", lambda e, dst=dst, src=src: e.tensor_copy(out=dst, in_=src), reads=[p_t], writes=[wr])
                    nev += 1
                for blk in range(9):
                    if ct >= 10 and (blk == 0 or blk >= 5):
                        continue
                    c_t = pc[blk % 2]
                    if blk == 0:
                        n = 256
                        for kw in range(3):
                            tr.op("pe", lambda e, kw=kw, c_t=c_t, dg=dg, prc=prc: e.matmul(
                                out=c_t[:, 0:256], lhsT=dg[:, 3 + kw, :], rhs=prc[:, kw:kw + 256], start=(kw == 0), stop=(kw == 2)),
                                reads=[dg, prc], writes=[c_t])
                    else:
                        n = 512
                        r0 = (blk - 1) * 8
                        for tap in range(9):
                            kh, kw = tap // 3, tap % 3
                            tr.op("pe", lambda e, tap=tap, kh=kh, kw=kw, c_t=c_t, dg=dg, pr=pr, r0=r0: e.matmul(
                                out=c_t[:, 0:512], lhsT=dg[:, tap, :], rhs=pr[:, r0 + kh:r0 + kh + 8, kw:kw + 64], start=(tap == 0), stop=(tap == 8)),
                                reads=[dg, pr], writes=[c_t])
                    own_blk = 1 <= blk <= 4
                    if ct >= 8 and own_blk:
                        g = (ct - 8) % 2
                        dstT = (c["BT"] if ct < 10 else c["CT"])
                        o0 = (blk - 1) * 512
                        tr.op("act", lambda e, dstT=dstT, g=g, o0=o0, c_t=c_t, ct=ct: e.activation(
                            out=dstT[:, g, o0:o0 + 512], in_=c_t[:, 0:512], func=AF.Silu, bias=cb[:, ct:ct + 1]),
                            reads=[c_t, cb], writes=[dstT])
                        if ct >= 10:
                            continue
                        src_post, src_r = dstT[:, g, o0:o0 + 512], dstT
                    else:
                        po = post[npost % 3]
                        tr.op("act", lambda e, po=po, c_t=c_t, ct=ct, n=n: e.activation(
                            out=po[:, 0:n], in_=c_t[:, 0:n], func=AF.Silu, bias=cb[:, ct:ct + 1]),
                            reads=[c_t, cb], writes=[po])
                        src_post, src_r = po[:, 0:n], po
                    ntl = n // 128
                    t_t = pt[npost % 2]
                    ts_t = tst[npost % 3]
                    for i in range(ntl):
                        tr.op("pe", lambda e, i=i, t_t=t_t, src_post=src_post: e.transpose(
                            out=t_t[:, i * 128:(i + 1) * 128], in_=src_post[:, i * 128:(i + 1) * 128], identity=c["ident_b"][:]),
                            reads=[src_r, c["ident_b"]], writes=[t_t])
                    tr.op("dve", lambda e, t_t=t_t, ts_t=ts_t, ntl=ntl: e.tensor_copy(
                        out=ts_t[:, 0:ntl, :], in_=t_t[:, 0:ntl * 128].rearrange("p (a b) -> p a b", b=128)),
                        reads=[t_t], writes=[ts_t])
                    tile0 = 0 if blk == 0 else T_OWN0 + (blk - 1) * 4
                    if ct < 8:
                        dst_d, dst_r = xtok_v[:, tile0:tile0 + ntl, ct * 128:(ct + 1) * 128], c["x_tok"]
                    else:
                        dst_d, dst_r = btok_v[:, tile0:tile0 + ntl, (ct - 8) * 128:(ct - 7) * 128], c["B_tok"]
                    tr.dma("sp", tsem[npost % 3], out=dst_d, in_=ts_t[:, 0:ntl, :], reads=[ts_t], writes=[])
                    c.setdefault("scr_ev", []).append(ts_t)
                    npost += 1
            self.conv_store_tiles = tst
            self.tap("BT", c["BT"][:].rearrange("p g t -> p (g t)"), [128, 2 * NOWN], BF16, [c["BT"]])
            self.tap("CT", c["CT"][:].rearrange("p g t -> p (g t)"), [128, 2 * NOWN], BF16, [c["CT"]])
            for e in Tracker.ENG:
                tr.wait_all(e, tst)
            self.barrier_release(rel)

    def stage_gla(self, st):
        tr, c, I = self.tr, self.c, self.I
        self.fence()
        c["oB"] = self.scratch("oB", [NOWN, 1024], F32)
        c["yx"] = self.scratch("yx", [NOWN, 2048], BF16)
        oB_v = c["oB"].t.rearrange("(n p) c -> n p c", p=128)
        yx_v = c["yx"].t.rearrange("(n p) c -> n p c", p=128)
        w_in_v = I["w_in"].t.rearrange("(kc p) n -> p kc n", p=128)
        LNQ = math.log(128.0 ** -0.5)
        with ExitStack() as s2:
            wg = self.sb(s2, "wgla", [128, 8, 3072], BF16)
            wgs = [Res("wgla%d" % i) for i in range(6)]
            wgg = self.sb(s2, "wgg", [128, 8, 32], BF16)
            wup = self.sb(s2, "wup", [17, 2, 512], F32)
            gn = self.sb(s2, "gn_bc", [128, 256], F32)
            c_one = self.sb(s2, "c_one", [128, 1], F32)
            c_lnq = self.sb(s2, "c_lnq", [128, 1], F32)
            c_eps = self.sb(s2, "c_eps", [128, 1], F32)
            negcol = self.sb(s2, "negcol", [128, 2], F32)
            Tm = [self.sb(s2, "TmA", [128, 128], F32), self.sb(s2, "TmB", [128, 128], F32)]
            S = [self.sb(s2, "S_A", [128, 4, 256], F32), self.sb(s2, "S_B", [128, 4, 256], F32)]
            Sbf = self.sb(s2, "Sbf", [128, 4, 256], BF16)
            FS = []
            for par in range(2):
                f = {}
                f["g_aug"] = self.sb(s2, "g_aug%d" % par, [32, 128], F32)
                f["v_bf"] = self.sb(s2, "v_bf%d" % par, [128, 1024], BF16)
                f["lap"] = self.sb(s2, "lap%d" % par, [128, 512], F32)
                f["Einv"] = self.sb(s2, "Einv%d" % par, [128, 512], F32)
                f["Eq"] = self.sb(s2, "Eq%d" % par, [128, 512], F32)
                f["kt_"] = self.sb(s2, "kt_%d" % par, [128, 512], BF16)
                f["qt_"] = self.sb(s2, "qt_%d" % par, [128, 512], BF16)
                f["kqT"] = self.sb(s2, "kqT%d" % par, [128, 8, 128], BF16)
                f["PT"] = self.sb(s2, "PT%d" % par, [128, 4, 128], BF16)
                f["dcol"] = self.sb(s2, "dcol%d" % par, [128, 4], F32)
                f["P"] = [self.ps(s2, "gP%d_%d" % (par, i)) for i in range(4)]
                FS.append(f)
            silr = self.sb(s2, "silr", [128, 1024], F32)
            o_sb = self.sb(s2, "o_sb", [128, 1024], F32)
            oB_sb = [self.sb(s2, "oB_sb%d" % i, [128, 1024], F32) for i in range(2)]
            oBs = [self.dsem() for _ in range(2)]
            ost = [self.sb(s2, "ost%d" % i, [128, 1024], F32) for i in range(2)]
            osts = [self.dsem() for _ in range(2)]
            yst = [self.sb(s2, "yst%d" % i, [128, 1024], BF16) for i in range(2)]
            ysts = [self.dsem() for _ in range(2)]
            ss4 = self.sb(s2, "ss4", [128, 4], F32)
            rs4 = self.sb(s2, "rs4", [128, 4], F32)
            junk = self.sb(s2, "junkg", [128, 256], BF16)
            rel = [wg, wgg, wup, gn, c_one, c_lnq, c_eps, negcol, Tm[0], Tm[1], S[0], S[1], Sbf, silr, o_sb, ss4, rs4, junk] + oB_sb + ost + yst + wgs
            for f in FS:
                rel += [f[k] for k in ("g_aug", "v_bf", "lap", "Einv", "Eq", "kt_", "qt_", "kqT", "PT", "dcol")] + f["P"]
            d0 = self.dsem(9)
            for i in range(6):
                tr.dma("pool", d0, out=wg[:, :, i * 512:(i + 1) * 512], in_=w_in_v[:, :, i * 512:(i + 1) * 512], writes=[wgs[i]])
            tr.dma("pool", d0, out=wgg[:], in_=w_in_v[:, :, OFF_G:OFF_G + 32], writes=[wgg])
            tr.dma("sp", d0, out=wup[:], in_=I["w_up_aug"].t.rearrange("d k n -> k d n"), writes=[wup])
            tr.dma("sp", d0, out=gn[:], in_=I["gla_norm"].t.partition_broadcast(128), writes=[gn])
            tr.op("pool", lambda e: e.memset(c_one[:], 1.0), writes=[c_one])
            tr.op("pool", lambda e: e.memset(c_lnq[:], LNQ), writes=[c_lnq])
            tr.op("pool", lambda e: e.memset(c_eps[:], EPS), writes=[c_eps])
            tr.op("pool", lambda e: e.memset(negcol[:], -1.0 / 16.0), writes=[negcol])
            tr.op("pool", lambda e: e.tensor_scalar(out=Tm[0][:], in0=c["tri_le"][:], scalar1=-1.0 / 16.0, scalar2=None, op0=ALU.mult), reads=[c["tri_le"]], writes=[Tm[0]])
            tr.op("pool", lambda e: e.tensor_scalar(out=Tm[1][:], in0=c["tri_ge"][:], scalar1=-1.0 / 16.0, scalar2=None, op0=ALU.mult), reads=[c["tri_ge"]], writes=[Tm[1]])
            for f in FS:
                tr.op("pool", lambda e, f=f: e.memset(f["g_aug"][:], 1.0), writes=[f["g_aug"]])
            for dd in range(2):
                tr.op("pool", lambda e, dd=dd: e.memset(S[dd][:], 0.0), writes=[S[dd]])
            masks = [c["tri_le"], c["tri_ge"]]

            def gla_tile(t, dd, full, sweepA, own_idx, seq):
                f = FS[seq % 2]
                P = f["P"]
                g_aug, v_bf, lap, Einv, Eq, kt_, qt_, kqT, PT, dcol = (f[k] for k in ("g_aug", "v_bf", "lap", "Einv", "Eq", "kt_", "qt_", "kqT", "PT", "dcol"))
                Sd = S[dd]
                hres = [c["hT_r"][t]]
                lhs = lambda kc: c["hT"][:, kc, t * 128:(t + 1) * 128]

                def mm_tok(ps_t, c0, n, wres):
                    for kc in range(8):
                        tr.op("pe", lambda e, kc=kc: e.matmul(out=ps_t[:, 0:n], lhsT=lhs(kc), rhs=wg[:, kc, c0:c0 + n], start=(kc == 0), stop=(kc == 7)),
                              reads=hres + wres, writes=[ps_t])
                for kc in range(8):
                    tr.op("pe", lambda e, kc=kc: e.matmul(out=P[3][0:16, 0:128], lhsT=wgg[:, kc, dd * 16:(dd + 1) * 16], rhs=lhs(kc), start=(kc == 0), stop=(kc == 7)),
                          reads=hres + [wgg], writes=[P[3]])
                tr.op("act", lambda e: e.activation(out=g_aug[0:16, :], in_=P[3][0:16, 0:128], func=AF.Copy), reads=[P[3]], writes=[g_aug])
                mm_tok(P[0], 512, 512, [wgs[1]])
                mm_tok(P[1], 1024, 512, [wgs[2]])
                mm_tok(P[2], 1536, 512, [wgs[3]])
                tr.op("pe", lambda e: e.matmul(out=P[3][:, 0:512], lhsT=g_aug[0:17, :], rhs=wup[:, dd, :], start=True, stop=True), reads=[g_aug, wup], writes=[P[3]])
                tr.op("act", lambda e: e.activation(out=lap[:], in_=P[3][:, 0:512], func=AF.Exp, scale=-1.0), reads=[P[3]], writes=[lap])
                tr.op("act", lambda e: e.activation(out=lap[:], in_=lap[:], func=AF.Ln, bias=c_one[:]), reads=[lap, c_one], writes=[lap])
                tr.op("act", lambda e: e.activation(out=v_bf[:, 0:512], in_=P[1][:], func=AF.Copy), reads=[P[1]], writes=[v_bf])
                tr.op("dve", lambda e: e.tensor_copy(out=v_bf[:, 512:1024], in_=P[2][:]), reads=[P[2]], writes=[v_bf])
                tr.op("pe", lambda e: e.matmul(out=P[3][:, 0:512], lhsT=Tm[dd][:], rhs=lap[:], start=True, stop=True), reads=[Tm[dd], lap], writes=[P[3]])
                if full:
                    mm_tok(P[1], 0, 512, [wgs[0]])
                tr.op("act", lambda e: e.activation(out=Einv[:], in_=P[3][:, 0:512], func=AF.Exp, scale=-1.0), reads=[P[3]], writes=[Einv])
                if full:
                    tr.op("act", lambda e: e.activation(out=Eq[:], in_=P[3][:, 0:512], func=AF.Exp, bias=c_lnq[:]), reads=[P[3], c_lnq], writes=[Eq])
                tr.op("dve", lambda e: e.tensor_tensor(out=kt_[:], in0=P[0][:], in1=Einv[:], op=ALU.mult), reads=[P[0], Einv], writes=[kt_])
                for h in range(4):
                    tr.op("pe", lambda e, h=h: e.matmul(out=P[3][:, 2 * h:2 * h + 2], lhsT=lap[:, h * 128:(h + 1) * 128], rhs=negcol[:], start=True, stop=True),
                          reads=[lap, negcol], writes=[P[3]])
                tr.op("act", lambda e: e.activation(out=dcol[:], in_=P[3][:, 0:8:2], func=AF.Exp), reads=[P[3]], writes=[dcol])
                if full:
                    pT = P[2][:].bitcast(BF16)
                    tr.op("dve", lambda e: e.tensor_tensor(out=qt_[:], in0=P[1][:], in1=Eq[:], op=ALU.mult), reads=[P[1], Eq], writes=[qt_])
                    for h in range(4):
                        tr.op("pe", lambda e, h=h: e.transpose(out=pT[:, h * 128:(h + 1) * 128], in_=kt_[:, h * 128:(h + 1) * 128], identity=c["ident_b"][:]),
                              reads=[kt_, c["ident_b"]], writes=[P[2]])
                    for h in range(4):
                        tr.op("pe", lambda e, h=h: e.transpose(out=pT[:, (4 + h) * 128:(5 + h) * 128], in_=qt_[:, h * 128:(h + 1) * 128], identity=c["ident_b"][:]),
                              reads=[qt_, c["ident_b"]], writes=[P[2]])
                    tr.op("act", lambda e: e.activation(out=kqT[:].rearrange("p a b -> p (a b)"), in_=pT, func=AF.Copy), reads=[P[2]], writes=[kqT])
                    for h in range(4):
                        tr.op("pe", lambda e, h=h: e.matmul(out=P[0][:, h * 128:(h + 1) * 128], lhsT=kqT[:, h, :], rhs=kqT[:, 4 + h, :], start=True, stop=True),
                              reads=[kqT], writes=[P[0]])
                    tr.op("dve", lambda e: e.tensor_tensor(out=PT[:], in0=P[0][:].rearrange("p (h i) -> p h i", h=4),
                                                          in1=masks[dd][:].unsqueeze(1).to_broadcast([128, 4, 128]), op=ALU.mult),
                          reads=[P[0], masks[dd]], writes=[PT])
                kvb = [P[2], P[2], P[0], P[0]]
                for h in range(4):
                    cs = (h % 2) * 256
                    tr.op("pe", lambda e, h=h, cs=cs: e.matmul(out=kvb[h][:, cs:cs + 256], lhsT=kt_[:, h * 128:(h + 1) * 128], rhs=v_bf[:, h * 256:(h + 1) * 256], start=True, stop=True),
                          reads=[kt_, v_bf], writes=[kvb[h]])
                tr.mark("need_state")
                if full:
                    ob_ = [P[1], P[1], P[3], P[3]]
                    tr.op("act", lambda e: e.activation(out=Sbf[:].rearrange("p a b -> p (a b)"), in_=Sd[:].rearrange("p a b -> p (a b)"), func=AF.Copy), reads=[Sd], writes=[Sbf])
                    for h in range(4):
                        cs = (h % 2) * 256
                        tr.op("pe", lambda e, h=h, cs=cs: e.matmul(out=ob_[h][:, cs:cs + 256], lhsT=PT[:, h, :], rhs=v_bf[:, h * 256:(h + 1) * 256], start=True, stop=False),
                              reads=[PT, v_bf], writes=[ob_[h]])
                        tr.op("pe", lambda e, h=h, cs=cs: e.matmul(out=ob_[h][:, cs:cs + 256], lhsT=kqT[:, 4 + h, :], rhs=Sbf[:, h, :], start=False, stop=True),
                              reads=[kqT, Sbf], writes=[ob_[h]])
                tr.op("dve", lambda e: e.tensor_tensor(out=Sd[:, 0:2, :].rearrange("p a b -> p (a b)"), in0=P[2][:], in1=Sd[:, 0:2, :].rearrange("p a b -> p (a b)"), op=ALU.add),
                      reads=[P[2], Sd], writes=[Sd])
                tr.op("dve", lambda e: e.tensor_tensor(out=Sd[:, 2:4, :].rearrange("p a b -> p (a b)"), in0=P[0][:], in1=Sd[:, 2:4, :].rearrange("p a b -> p (a b)"), op=ALU.add),
                      reads=[P[0], Sd], writes=[Sd])
                tr.op("dve", lambda e: e.tensor_tensor(out=Sd[:], in0=Sd[:], in1=dcol[:].unsqueeze(2).to_broadcast([128, 4, 256]), op=ALU.mult),
                      reads=[Sd, dcol], writes=[Sd])
                if not full:
                    return
                if not sweepA:
                    os_ = ost[own_idx % 2]
                    tr.op("act", lambda e: e.activation(out=os_[:, 0:512], in_=P[1][:], func=AF.Copy), reads=[P[1]], writes=[os_])
                    tr.op("dve", lambda e: e.tensor_copy(out=os_[:, 512:1024], in_=P[3][:]), reads=[P[3]], writes=[os_])
                    tr.dma("sp", osts[own_idx % 2], out=oB_v[own_idx], in_=os_[:], reads=[os_], writes=[])
                    return
                ob = oB_sb[own_idx % 2]
                tr.dma("sp", oBs[own_idx % 2], out=ob[:], in_=oB_v[own_idx], writes=[ob])
                mm_tok(P[2], 2048, 512, [wgs[4]])
                tr.op("act", lambda e: e.activation(out=silr[:, 0:512], in_=P[2][:], func=AF.Silu), reads=[P[2]], writes=[silr])
                mm_tok(P[0], 2560, 512, [wgs[5]])
                tr.op("act", lambda e: e.activation(out=silr[:, 512:1024], in_=P[0][:], func=AF.Silu), reads=[P[0]], writes=[silr])
                tr.op("dve", lambda e: e.tensor_tensor(out=silr[:].rearrange("p (h v) -> p h v", h=4), in0=silr[:].rearrange("p (h v) -> p h v", h=4),
                                                      in1=gn[:].unsqueeze(1).to_broadcast([128, 4, 256]), op=ALU.mult), reads=[silr, gn], writes=[silr])
                for hh, pb in enumerate((P[1], P[3])):
                    tr.op("dve", lambda e, hh=hh, pb=pb: e.tensor_tensor(out=o_sb[:, hh * 512:(hh + 1) * 512], in0=pb[:], in1=ob[:, hh * 512:(hh + 1) * 512], op=ALU.add),
                          reads=[pb, ob], writes=[o_sb])
                for h in range(4):
                    tr.op("act", lambda e, h=h: e.activation(out=junk[:], in_=o_sb[:, h * 256:(h + 1) * 256], func=AF.Square, accum_out=ss4[:, h:h + 1]),
                          reads=[o_sb], writes=[junk, ss4])
                tr.op("act", lambda e: e.activation(out=rs4[:], in_=ss4[:], func=AF.Ln, scale=1.0 / 256.0, bias=c_eps[:]), reads=[ss4, c_eps], writes=[rs4])
                tr.op("act", lambda e: e.activation(out=rs4[:], in_=rs4[:], func=AF.Exp, scale=-0.5), reads=[rs4], writes=[rs4])
                tr.op("dve", lambda e: e.tensor_tensor(out=o_sb[:].rearrange("p (h v) -> p h v", h=4), in0=o_sb[:].rearrange("p (h v) -> p h v", h=4),
                                                      in1=rs4[:].unsqueeze(2).to_broadcast([128, 4, 256]), op=ALU.mult), reads=[o_sb, rs4], writes=[o_sb])
                ys = yst[own_idx % 2]
                tr.op("dve", lambda e: e.tensor_tensor(out=ys[:], in0=o_sb[:], in1=silr[:], op=ALU.mult), reads=[o_sb, silr], writes=[ys])
                tr.dma("sp", ysts[own_idx % 2], out=yx_v[own_idx][:, 0:1024], in_=ys[:], reads=[ys], writes=[])

            def sweep(tiles):
                recs = []
                for seq, (t, dd, full, sweepA, own_idx) in enumerate(tiles):
                    tr.begin_record()
                    gla_tile(t, dd, full, sweepA, own_idx, seq)
                    recs.append(tr.end_record())
                tr.run_pipelined(recs, depth=2)

            sweep([(t, 1, False, False, None) for t in (1, 0)])
            self.tap("gS_B", S[1][:].rearrange("p a b -> p (a b)"), [128, 1024], F32, [S[1]])
            sweep([(t, 1, False, False, None) for t in range(NT - 1, T_OTH0 - 1, -1)] +
                  [(t, 1, True, False, t - T_OWN0) for t in range(T_OTH0 - 1, T_OWN0 - 1, -1)])
            for e in Tracker.ENG:
                tr.wait_all(e, ost)
            sweep([(t, 0, False, True, None) for t in (0, 1)])
            self.tap("gS_A", S[0][:].rearrange("p a b -> p (a b)"), [128, 1024], F32, [S[0]])
            sweep([(t, 0, True, True, t - T_OWN0) for t in range(T_OWN0, T_OTH0)])
            for e in Tracker.ENG:
                tr.wait_all(e, yst)
            self.barrier_release(rel)

    def stage_ssd(self, st):
        tr, c, I = self.tr, self.c, self.I
        self.fence()
        c["yB"] = self.scratch("yB", [NOWN, 1024], F32)
        yB_v = c["yB"].t.rearrange("(n p) c -> n p c", p=128)
        yx_v = c["yx"].t.rearrange("(n p) c -> n p c", p=128)
        xtok_v = c["x_tok"].t.rearrange("(n p) c -> n p c", p=128)
        btok_v = c["B_tok"].t.rearrange("(n p) c -> n p c", p=128)
        w_in_v = I["w_in"].t.rearrange("(kc p) n -> p kc n", p=128)
        BT, CT = c["BT"], c["CT"]
        with ExitStack() as s2:
            wz = self.sb(s2, "wz", [128, 8, 1024], BF16)
            wdt = self.sb(s2, "wdt", [128, 8, 32], BF16)
            Abc = self.sb(s2, "Abc", [128, 32], F32)
            dtb = self.sb(s2, "dtb", [128, 32], F32)
            Dsk = self.sb(s2, "Dsk", [128, 16], F32)
            snb = self.sb(s2, "snb", [128, 1024], F32)
            c_one = self.sb(s2, "c_one2", [128, 1], F32)
            c_eps = self.sb(s2, "c_eps2", [128, 1], F32)
            ST = [self.sb(s2, "ST_A", [128, 2, 512], F32), self.sb(s2, "ST_B", [128, 2, 512], F32)]
            STbf = self.sb(s2, "STbf", [128, 2, 512], BF16)
            xt = [self.sb(s2, "xt%d" % i, [128, 1024], BF16) for i in range(2)]
            bt = [self.sb(s2, "bt%d" % i, [128, 256], BF16) for i in range(2)]
            xts = [self.dsem() for _ in range(2)]
            dt_ = self.sb(s2, "dt_", [128, 16], F32)
            dtA = self.sb(s2, "dtA", [128, 16], F32)
            acs = self.sb(s2, "acs", [128, 16], F32)
            ea = self.sb(s2, "ea", [128, 16], F32)
            dend = self.sb(s2, "dend", [128, 16], F32)
            dtot = self.sb(s2, "dtot", [128, 16], F32)
            R1 = self.sb(s2, "R1", [128, 16, 128], F32)
            E = self.sb(s2, "E", [128, 16, 128], BF16)
            M = self.sb(s2, "M", [128, 16, 128], BF16)
            CBm = self.sb(s2, "CBm", [128, 2, 128], F32)
            xdt = self.sb(s2, "xdt", [128, 1024], BF16)
            xdd = self.sb(s2, "xdd", [128, 1024], BF16)
            silz = self.sb(s2, "silz", [128, 1024], F32)
            y_sb = self.sb(s2, "y_sb", [128, 1024], F32)
            tmp = self.sb(s2, "ytmp", [128, 1024], F32)
            yB_sb = [self.sb(s2, "yB_sb%d" % i, [128, 1024], F32) for i in range(2)]
            yBs = [self.dsem() for _ in range(2)]
            yst = [self.sb(s2, "ysst%d" % i, [128, 1024], F32) for i in range(2)]
            ysts = [self.dsem() for _ in range(2)]
            yxs = [self.sb(s2, "yxs%d" % i, [128, 1024], BF16) for i in range(2)]
            yxss = [self.dsem() for _ in range(2)]
            ss2 = self.sb(s2, "ss2", [128, 2], F32)
            rs2 = self.sb(s2, "rs2", [128, 2], F32)
            junk = self.sb(s2, "junks", [128, 512], BF16)
            pS = self.ps(s2, "pS")
            pD = [self.ps(s2, "pD%d" % i) for i in range(4)]
            pCB = self.ps(s2, "pCB")
            pY = [self.ps(s2, "pY%d" % i) for i in range(2)]
            rel = [wz, wdt, Abc, dtb, Dsk, snb, c_one, c_eps, ST[0], ST[1], STbf, dt_, dtA, acs, ea, dend, dtot, R1, E, M, CBm, xdt, xdd,
                   silz, y_sb, tmp, ss2, rs2, junk, pS, pCB] + xt + bt + yB_sb + yst + yxs + pD + pY
            d0 = self.dsem(6)
            tr.dma("pool", d0, out=wz[:], in_=w_in_v[:, :, OFF_Z:OFF_Z + 1024], writes=[wz])
            tr.dma("pool", d0, out=wdt[:], in_=w_in_v[:, :, OFF_DT:OFF_DT + 32], writes=[wdt])
            tr.dma("sp", d0, out=Abc[:], in_=I["a_log"].t.partition_broadcast(128), writes=[Abc])
            tr.dma("sp", d0, out=dtb[:], in_=I["dt_bias"].t.partition_broadcast(128), writes=[dtb])
            tr.dma("sp", d0, out=Dsk[:], in_=I["ssd_d"].t.partition_broadcast(128), writes=[Dsk])
            tr.dma("sp", d0, out=snb[:], in_=I["ssd_norm"].t.partition_broadcast(128), writes=[snb])
            tr.op("pool", lambda e: e.memset(c_one[:], 1.0), writes=[c_one])
            tr.op("pool", lambda e: e.memset(c_eps[:], EPS), writes=[c_eps])
            tr.op("act", lambda e: e.activation(out=Abc[:], in_=Abc[:], func=AF.Exp), reads=[Abc], writes=[Abc])
            tr.op("dve", lambda e: e.tensor_scalar(out=Abc[:], in0=Abc[:], scalar1=-1.0, scalar2=None, op0=ALU.mult), reads=[Abc], writes=[Abc])
            for dd in range(2):
                tr.op("pool", lambda e, dd=dd: e.memset(ST[dd][:], 0.0), writes=[ST[dd]])
            Lm = [c["tri_gt"], c["tri_lt"]]
            Tc = [c["tri_le"], c["tri_ge"]]
            cnt = [0]

            QS = [[pS, pD[0], pD[1], pD[2]], [pD[3], pCB, pY[0], pY[1]]]
            ea2 = [ea, self.sb(s2, "ea_b", [128, 16], F32)]
            dtot2 = [dtot, self.sb(s2, "dtot_b", [128, 16], F32)]
            xdd2 = [xdd, self.sb(s2, "xdd_b", [128, 1024], BF16)]
            silz2 = [silz, self.sb(s2, "silz_b", [128, 1024], F32)]
            ysb2 = [y_sb, self.sb(s2, "y_sb_b", [128, 1024], F32)]
            tmp2 = [tmp, self.sb(s2, "ytmp_b", [128, 1024], F32)]
            ss22 = [ss2, self.sb(s2, "ss2_b", [128, 2], F32)]
            rs22 = [rs2, self.sb(s2, "rs2_b", [128, 2], F32)]
            junk2 = [junk, self.sb(s2, "junks_b", [128, 512], BF16)]
            rel += [ea2[1], dtot2[1], xdd2[1], silz2[1], ysb2[1], tmp2[1], ss22[1], rs22[1], junk2[1]]

            def ssd_tile(t, dd, full, sweepA, own_idx, seq):
                STd = ST[dd]
                k = seq % 2
                Q = QS[k]
                ea_, dtot_, xdd_, silz_ = ea2[k], dtot2[k], xdd2[k], silz2[k]
                y_sb, tmp, ss2, rs2, junk = ysb2[k], tmp2[k], ss22[k], rs22[k], junk2[k]
                x_t, b_t = xt[k], bt[k]
                tr.regroup(xts[k], 2)
                tr.dma("sp", xts[k], out=x_t[:], in_=xtok_v[t], writes=[x_t])
                tr.dma("sp", xts[k], out=b_t[:], in_=btok_v[t], writes=[b_t])
                hres = [c["hT_r"][t]]
                lhs = lambda kc: c["hT"][:, kc, t * 128:(t + 1) * 128]
                if full and sweepA:
                    for hh in range(2):
                        for kc in range(8):
                            tr.op("pe", lambda e, kc=kc, hh=hh: e.matmul(out=Q[1 + hh][:], lhsT=lhs(kc), rhs=wz[:, kc, hh * 512:(hh + 1) * 512], start=(kc == 0), stop=(kc == 7)),
                                  reads=hres + [wz], writes=[Q[1 + hh]])
                        tr.op("act", lambda e, hh=hh: e.activation(out=silz_[:, hh * 512:(hh + 1) * 512], in_=Q[1 + hh][:], func=AF.Silu), reads=[Q[1 + hh]], writes=[silz_])
                for kc in range(8):
                    tr.op("pe", lambda e, kc=kc: e.matmul(out=Q[0][:, 0:16], lhsT=lhs(kc), rhs=wdt[:, kc, dd * 16:(dd + 1) * 16], start=(kc == 0), stop=(kc == 7)),
                          reads=hres + [wdt], writes=[Q[0]])
                tr.op("dve", lambda e: e.tensor_tensor(out=dt_[:], in0=Q[0][:, 0:16], in1=dtb[:, dd * 16:(dd + 1) * 16], op=ALU.add), reads=[Q[0], dtb], writes=[dt_])
                tr.op("act", lambda e: e.activation(out=dt_[:], in_=dt_[:], func=AF.Exp), reads=[dt_], writes=[dt_])
                tr.op("act", lambda e: e.activation(out=dt_[:], in_=dt_[:], func=AF.Ln, bias=c_one[:]), reads=[dt_, c_one], writes=[dt_])
                tr.op("dve", lambda e: e.tensor_tensor(out=dtA[:], in0=dt_[:], in1=Abc[:, dd * 16:(dd + 1) * 16], op=ALU.mult), reads=[dt_, Abc], writes=[dtA])
                tr.op("pe", lambda e: e.matmul(out=Q[0][:, 16:32], lhsT=Tc[dd][:], rhs=dtA[:], start=True, stop=True), reads=[Tc[dd], dtA], writes=[Q[0]])
                tr.op("pe", lambda e: e.matmul(out=Q[0][:, 32:48], lhsT=c["ones_f"][:], rhs=dtA[:], start=True, stop=True), reads=[c["ones_f"], dtA], writes=[Q[0]])
                tr.op("dve", lambda e: e.tensor_copy(out=acs[:], in_=Q[0][:, 16:32]), reads=[Q[0]], writes=[acs])
                tr.op("dve", lambda e: e.tensor_tensor(out=dend[:], in0=Q[0][:, 32:48], in1=acs[:], op=ALU.subtract), reads=[Q[0], acs], writes=[dend])
                tr.op("act", lambda e: e.activation(out=dend[:], in_=dend[:], func=AF.Exp), reads=[dend], writes=[dend])
                tr.op("act", lambda e: e.activation(out=dtot_[:], in_=Q[0][:, 32:48], func=AF.Exp), reads=[Q[0]], writes=[dtot_])
                tr.op("dve", lambda e: e.tensor_tensor(out=xdt[:].rearrange("p (h q) -> p h q", h=16), in0=x_t[:].rearrange("p (h q) -> p h q", h=16),
                                                      in1=dt_[:].unsqueeze(2).to_broadcast([128, 16, 64]), op=ALU.mult), reads=[x_t, dt_], writes=[xdt])
                tr.op("pool", lambda e: e.tensor_tensor(out=xdd_[:].rearrange("p (h q) -> p h q", h=16), in0=xdt[:].rearrange("p (h q) -> p h q", h=16),
                                                       in1=dend[:].unsqueeze(2).to_broadcast([128, 16, 64]), op=ALU.mult), reads=[xdt, dend], writes=[xdd_])
                if full:
                    tok0 = (t - T_OWN0) * 128
                    Dbank = [Q[1], Q[2], Q[3], Q[1]]
                    tr.op("act", lambda e: e.activation(out=ea_[:], in_=acs[:], func=AF.Exp), reads=[acs], writes=[ea_])
                    tr.op("dve", lambda e: e.tensor_tensor(out=R1[:], in0=Tc[dd][:].unsqueeze(1).to_broadcast([128, 16, 128]),
                                                          in1=dtA[:].unsqueeze(2).to_broadcast([128, 16, 128]), op=ALU.mult), reads=[Tc[dd], dtA], writes=[R1])
                    for b4 in range(4):
                        tr.op("pe", lambda e, b4=b4: e.matmul(out=Dbank[b4][:], lhsT=Lm[dd][:], rhs=R1[:, 4 * b4:4 * b4 + 4, :], start=True, stop=True),
                              reads=[Lm[dd], R1], writes=[Dbank[b4]])
                        tr.op("act", lambda e, b4=b4: e.activation(out=E[:, 4 * b4:4 * b4 + 4, :], in_=Dbank[b4][:].rearrange("p (h i) -> p h i", h=4), func=AF.Exp), reads=[Dbank[b4]], writes=[E])
                    for g in range(2):
                        tr.op("pe", lambda e, g=g: e.matmul(out=Q[2][:, g * 128:(g + 1) * 128], lhsT=BT[:, g, tok0:tok0 + 128], rhs=CT[:, g, tok0:tok0 + 128], start=True, stop=True),
                              reads=[BT, CT], writes=[Q[2]])
                    tr.op("dve", lambda e: e.tensor_tensor(out=CBm[:], in0=Q[2][:, 0:256].rearrange("p (g i) -> p g i", g=2),
                                                          in1=Tc[dd][:].unsqueeze(1).to_broadcast([128, 2, 128]), op=ALU.mult), reads=[Q[2], Tc[dd]], writes=[CBm])
                    for g in range(2):
                        eng = "dve" if g == 0 else "pool"
                        tr.op(eng, lambda e, g=g: e.tensor_tensor(out=M[:, g * 8:(g + 1) * 8, :], in0=E[:, g * 8:(g + 1) * 8, :],
                                                                  in1=CBm[:, g:g + 1, :].to_broadcast([128, 8, 128]), op=ALU.mult), reads=[E, CBm], writes=[M])
                    Yb = [Q[3], Q[1]]
                    for h in range(16):
                        py = Yb[h // 8]
                        cs = (h % 8) * 64
                        tr.op("pe", lambda e, h=h, py=py, cs=cs: e.matmul(out=py[:, cs:cs + 64], lhsT=M[:, h, :], rhs=xdt[:, h * 64:(h + 1) * 64], start=True, stop=True),
                              reads=[M, xdt], writes=[py])
                tr.mark("need_state")
                Ob = [Q[2], Q[0]]
                if full:
                    tr.op("act", lambda e: e.activation(out=STbf[:].rearrange("p a b -> p (a b)"), in_=STd[:].rearrange("p a b -> p (a b)"), func=AF.Copy), reads=[STd], writes=[STbf])
                    for g in range(2):
                        tr.op("pe", lambda e, g=g: e.matmul(out=Ob[g][:], lhsT=CT[:, g, tok0:tok0 + 128], rhs=STbf[:, g, :], start=True, stop=True),
                              reads=[CT, STbf], writes=[Ob[g]])
                        tr.op("dve", lambda e, g=g: e.tensor_tensor(out=tmp[:, g * 512:(g + 1) * 512].rearrange("p (h q) -> p h q", h=8), in0=Ob[g][:].rearrange("p (h q) -> p h q", h=8),
                                                                    in1=ea_[:, g * 8:(g + 1) * 8].unsqueeze(2).to_broadcast([128, 8, 64]), op=ALU.mult), reads=[Ob[g], ea_], writes=[tmp])
                        tr.op("dve", lambda e, g=g: e.tensor_tensor(out=y_sb[:, g * 512:(g + 1) * 512], in0=Yb[g][:], in1=tmp[:, g * 512:(g + 1) * 512], op=ALU.add),
                              reads=[Yb[g], tmp], writes=[y_sb])
                for g in range(2):
                    tr.op("pe", lambda e, g=g: e.matmul(out=Ob[g][:], lhsT=b_t[:, g * 128:(g + 1) * 128], rhs=xdd_[:, g * 512:(g + 1) * 512], start=True, stop=True),
                          reads=[b_t, xdd_], writes=[Ob[g]])
                    tr.op("dve", lambda e, g=g: e.tensor_tensor(out=STd[:, g, :].rearrange("p (h q) -> p h q", h=8), in0=STd[:, g, :].rearrange("p (h q) -> p h q", h=8),
                                                                in1=dtot_[:, g * 8:(g + 1) * 8].unsqueeze(2).to_broadcast([128, 8, 64]), op=ALU.mult), reads=[STd, dtot_], writes=[STd])
                    tr.op("dve", lambda e, g=g: e.tensor_tensor(out=STd[:, g, :], in0=Ob[g][:], in1=STd[:, g, :], op=ALU.add), reads=[Ob[g], STd], writes=[STd])
                tr.mark("state_done")
                if not full:
                    return
                if not sweepA:
                    ys = yst[own_idx % 2]
                    tr.op("act", lambda e: e.activation(out=ys[:], in_=y_sb[:], func=AF.Copy), reads=[y_sb], writes=[ys])
                    tr.dma("sp", ysts[own_idx % 2], out=yB_v[own_idx], in_=ys[:], reads=[ys], writes=[])
                    return
                yb = yB_sb[own_idx % 2]
                tr.dma("sp", yBs[own_idx % 2], out=yb[:], in_=yB_v[own_idx], writes=[yb])
                tr.op("dve", lambda e: e.tensor_tensor(out=y_sb[:], in0=y_sb[:], in1=yb[:], op=ALU.add), reads=[y_sb, yb], writes=[y_sb])
                tr.op("pool", lambda e: e.tensor_tensor(out=tmp[:].rearrange("p (h q) -> p h q", h=16), in0=x_t[:].rearrange("p (h q) -> p h q", h=16),
                                                       in1=Dsk[:].unsqueeze(2).to_broadcast([128, 16, 64]), op=ALU.mult), reads=[x_t, Dsk], writes=[tmp])
                tr.op("dve", lambda e: e.tensor_tensor(out=y_sb[:], in0=y_sb[:], in1=tmp[:], op=ALU.add), reads=[y_sb, tmp], writes=[y_sb])
                tr.op("dve", lambda e: e.tensor_tensor(out=y_sb[:], in0=y_sb[:], in1=silz_[:], op=ALU.mult), reads=[y_sb, silz_], writes=[y_sb])
                for g in range(2):
                    tr.op("act", lambda e, g=g: e.activation(out=junk[:], in_=y_sb[:, g * 512:(g + 1) * 512], func=AF.Square, accum_out=ss2[:, g:g + 1]), reads=[y_sb], writes=[junk, ss2])
                tr.op("act", lambda e: e.activation(out=rs2[:], in_=ss2[:], func=AF.Ln, scale=1.0 / 512.0, bias=c_eps[:]), reads=[ss2, c_eps], writes=[rs2])
                tr.op("act", lambda e: e.activation(out=rs2[:], in_=rs2[:], func=AF.Exp, scale=-0.5), reads=[rs2], writes=[rs2])
                tr.op("dve", lambda e: e.tensor_tensor(out=y_sb[:].rearrange("p (g q) -> p g q", g=2), in0=y_sb[:].rearrange("p (g q) -> p g q", g=2),
                                                      in1=rs2[:].unsqueeze(2).to_broadcast([128, 2, 512]), op=ALU.mult), reads=[y_sb, rs2], writes=[y_sb])
                yo = yxs[own_idx % 2]
                tr.op("pool", lambda e: e.tensor_tensor(out=yo[:], in0=y_sb[:], in1=snb[:], op=ALU.mult), reads=[y_sb, snb], writes=[yo])
                tr.dma("sp", yxss[own_idx % 2], out=yx_v[own_idx][:, 1024:2048], in_=yo[:], reads=[yo], writes=[])

            def sweep(tiles):
                recs = []
                for seq, (t, dd, full, sweepA, own_idx) in enumerate(tiles):
                    tr.begin_record()
                    ssd_tile(t, dd, full, sweepA, own_idx, seq)
                    recs.append(tr.end_record())
                tr.run_pipelined(recs, depth=2, serial_fronts=True)

            sweep([(t, 1, False, False, None) for t in (1, 0)])
            self.tap("sS_B", ST[1][:].rearrange("p a b -> p (a b)"), [128, 1024], F32, [ST[1]])
            sweep([(t, 1, False, False, None) for t in range(NT - 1, T_OTH0 - 1, -1)] +
                  [(t, 1, True, False, t - T_OWN0) for t in range(T_OTH0 - 1, T_OWN0 - 1, -1)])
            for e in Tracker.ENG:
                tr.wait_all(e, yst)
            sweep([(t, 0, False, True, None) for t in (0, 1)])
            self.tap("sS_A", ST[0][:].rearrange("p a b -> p (a b)"), [128, 1024], F32, [ST[0]])
            sweep([(t, 0, True, True, t - T_OWN0) for t in range(T_OWN0, T_OTH0)])
            for e in Tracker.ENG:
                tr.wait_all(e, yxs)
            self.barrier_release(rel)

    def stage_post(self, st):
        tr, c, I = self.tr, self.c, self.I
        self.fence()
        c["h_lat"] = self.sb(st, "h_lat", [128, 16, D], F32)
        c["h_r"] = [Res("h_lat%d" % i) for i in range(16)]
        c["h2T"] = self.sb(st, "h2T", [128, 8, NOWN], BF16)
        c["h2_r"] = [Res("h2T%d" % i) for i in range(16)]
        c["comb"] = self.sb(st, "comb", [128, 16, 32], F32)
        yx_v = c["yx"].t.rearrange("(n p) c -> n p c", p=128)
        h_lat, h2T = c["h_lat"], c["h2T"]
        with ExitStack() as s2:
            wo = self.sb(s2, "wo", [128, 16, D], BF16)
            wr = self.sb(s2, "wr", [128, 8, 36], F32)
            brr = self.sb(s2, "brr", [1, 36], F32)
            c_eps = self.sb(s2, "c_eps3", [128, 1], F32)
            lg = self.sb(s2, "lg", [128, 16, 36], F32)
            yxt = [self.sb(s2, "yxt%d" % i, [128, 2048], BF16) for i in range(2)]
            yxs = [self.dsem() for _ in range(2)]
            xr = [self.sb(s2, "xr2_%d" % i, [128, D], F32) for i in range(2)]
            xrs = [self.dsem() for _ in range(2)]
            yxT = self.sb(s2, "yxT", [128, 16, 128], BF16)
            tmp = self.sb(s2, "ptmp", [128, D], F32)
            hn = self.sb(s2, "hn", [128, D], F32)
            h2f = self.sb(s2, "h2f", [128, 8, 128], F32)
            ss = self.sb(s2, "pss", [128, 1], F32)
            rs = self.sb(s2, "prs", [128, 1], F32)
            junk = self.sb(s2, "pjunk", [128, D], BF16)
            pT = [self.ps(s2, "ppT%d" % i, [128, 1024], BF16) for i in range(2)]
            pO = [self.ps(s2, "ppO%d" % i) for i in range(2)]
            pF = [self.ps(s2, "ppF%d" % i) for i in range(2)]
            pR = self.ps(s2, "ppR")
            rel = [wo, wr, brr, c_eps, lg, yxT, tmp, hn, h2f, ss, rs, junk, pR] + yxt + xr + pT + pO + pF
            import os
            if os.environ.get("BISECT3") == "2":
                self.tap("g1x", c["g1_bc"][:], [128, D], F32, [c["g1_bc"]])
                return
            w_out_v = I["w_out"].t.rearrange("(kc p) n -> p kc n", p=128)
            d0 = self.dsem(2)
            wst = self.sb(s2, "wst", [128, 4, D], F32)
            rel.append(wst)
            wsts = self.dsem()
            for q in range(4):
                tr.dma("sp", wsts, out=wst[:], in_=w_out_v[:, q * 4:(q + 1) * 4, :], writes=[wst])
                tr.op("pool", lambda e, q=q: e.tensor_copy(out=wo[:, q * 4:(q + 1) * 4, :], in_=wst[:]), reads=[wst], writes=[wo])
            tr.dma("sp", d0, out=wr[:].rearrange("p a b -> p (a b)"), in_=I["w_router"].t, writes=[wr])
            tr.dma("sp", d0, out=brr[:], in_=I["b_router"].t, writes=[brr])
            tr.op("pool", lambda e: e.memset(c_eps[:], EPS), writes=[c_eps])
            import os
            B3 = os.environ.get("BISECT3", "")
            for i in range(16 if B3 != "1" else 0):
                y_t, x_t = yxt[i % 2], xr[i % 2]
                tr.dma("sp", yxs[i % 2], out=y_t[:], in_=yx_v[i], writes=[y_t])
                tr.dma("sp", xrs[i % 2], out=x_t[:], in_=I["xs"].t[NCTX + i * 128: NCTX + (i + 1) * 128, :], writes=[x_t])
                for kc in range(16):
                    tr.op("pe", lambda e, kc=kc: e.transpose(out=pT[kc // 8][:, (kc % 8) * 128:(kc % 8 + 1) * 128], in_=y_t[:, kc * 128:(kc + 1) * 128], identity=c["ident_b"][:]),
                          reads=[y_t, c["ident_b"]], writes=[pT[kc // 8]])
                tr.op("act", lambda e: e.activation(out=yxT[:, 0:8, :].rearrange("p a b -> p (a b)"), in_=pT[0][:], func=AF.Copy), reads=[pT[0]], writes=[yxT])
                tr.op("dve", lambda e: e.tensor_copy(out=yxT[:, 8:16, :].rearrange("p a b -> p (a b)"), in_=pT[1][:]), reads=[pT[1]], writes=[yxT])
                hl = h_lat[:, i, :]
                for hh in range(2):
                    for kc in range(16):
                        tr.op("pe", lambda e, kc=kc, hh=hh: e.matmul(out=pO[hh][:], lhsT=yxT[:, kc, :], rhs=wo[:, kc, hh * 512:(hh + 1) * 512], start=(kc == 0), stop=(kc == 15)),
                              reads=[yxT, wo], writes=[pO[hh]])
                    tr.op("dve", lambda e, hh=hh: e.tensor_tensor(out=tmp[:, hh * 512:(hh + 1) * 512], in0=pO[hh][:], in1=c["g1_bc"][:, hh * 512:(hh + 1) * 512], op=ALU.mult),
                          reads=[pO[hh], c["g1_bc"]], writes=[tmp])
                tr.op("pool", lambda e: e.tensor_tensor(out=hl, in0=tmp[:], in1=x_t[:], op=ALU.add), reads=[tmp, x_t], writes=[c["h_r"][i]])
                import os
                if os.environ.get("BISECT2") == "b":
                    continue
                tr.op("act", lambda e: e.activation(out=junk[:], in_=hl, func=AF.Square, accum_out=ss[:]), reads=[c["h_r"][i]], writes=[junk, ss])
                tr.op("act", lambda e: e.activation(out=rs[:], in_=ss[:], func=AF.Ln, scale=1.0 / D, bias=c_eps[:]), reads=[ss, c_eps], writes=[rs])
                tr.op("act", lambda e: e.activation(out=rs[:], in_=rs[:], func=AF.Exp, scale=-0.5), reads=[rs], writes=[rs])
                tr.op("dve", lambda e: e.tensor_scalar(out=hn[:], in0=hl, scalar1=rs[:], scalar2=None, op0=ALU.mult), reads=[c["h_r"][i], rs], writes=[hn])
                for kc in range(8):
                    tr.op("pe", lambda e, kc=kc: e.transpose(out=pF[kc // 4][:, (kc % 4) * 128:(kc % 4 + 1) * 128], in_=hn[:, kc * 128:(kc + 1) * 128], identity=c["ident_f"][:]),
                          reads=[hn, c["ident_f"]], writes=[pF[kc // 4]])
                for q in range(2):
                    tr.op("dve", lambda e, q=q: e.tensor_tensor(out=h2f[:, q * 4:(q + 1) * 4, :], in0=pF[q][:].rearrange("p (k t) -> p k t", k=4),
                                                               in1=c["s2"][:, q * 4:(q + 1) * 4].unsqueeze(2).to_broadcast([128, 4, 128]), op=ALU.mult), reads=[pF[q], c["s2"]], writes=[h2f])
                tr.op("pool", lambda e: e.tensor_tensor(out=h2f[:], in0=h2f[:], in1=c["b2"][:].unsqueeze(2).to_broadcast([128, 8, 128]), op=ALU.add), reads=[h2f, c["b2"]], writes=[h2f])
                tr.op("act", lambda e: e.activation(out=h2T[:, :, i * 128:(i + 1) * 128], in_=h2f[:], func=AF.Copy), reads=[h2f], writes=[c["h2_r"][i]])
                if os.environ.get("BISECT2") == "c":
                    continue
                for kc in range(8):
                    tr.op("pe", lambda e, kc=kc: e.matmul(out=pR[:, 0:36], lhsT=h2f[:, kc, :], rhs=wr[:, kc, :], start=(kc == 0), stop=False), reads=[h2f, wr], writes=[pR])
                tr.op("pe", lambda e: e.matmul(out=pR[:, 0:36], lhsT=c["ones_f"][0:1, :], rhs=brr[0:1, :], start=False, stop=True), reads=[c["ones_f"], brr], writes=[pR])
                tr.op("dve", lambda e: e.tensor_copy(out=lg[:, i, :], in_=pR[:, 0:36]), reads=[pR], writes=[lg])
            self.tap("lg", lg[:].rearrange("p a b -> p (a b)"), [128, 16 * 36], F32, [lg])
            self.tap("h_lat", h_lat[:].rearrange("p a b -> p (a b)"), [128, 16 * D], F32, c["h_r"])
            import os
            if os.environ.get("BISECT") == "a":
                self.tap("wo", wo[:, 0, :], [128, D], BF16, [wo])
                self.tap("wrx", wr[:].rearrange("p a b -> p (a b)"), [128, 288], F32, [wr])
                self.barrier_release(rel)
                return
            def T(name, shape):
                t_ = self.sb(s2, name, shape, F32)
                rel.append(t_)
                return t_
            gmax = T("gmax", [128, 16]); mg = T("mg", [128, 16, 4]); eg = T("eg", [128, 16, 4]); gsum = T("gsum", [128, 16]); pg = T("pg", [128, 16])
            t48 = T("t48", [128, 16, 4, 8]); ein = T("ein", [128, 16, 8]); m1 = T("m1", [128, 16]); k1 = T("k1", [128, 16, 8]); e2 = T("e2", [128, 16, 8])
            m2 = T("m2", [128, 16]); k2 = T("k2", [128, 16, 8]); dd_ = T("dd_", [128, 16]); w1 = T("w1", [128, 16]); w2 = T("w2", [128, 16]); cw8 = T("cw8", [128, 16, 8])
            gl = lg[:, :, 0:4]
            el = lg[:, :, 4:36].rearrange("p t (g x) -> p t g x", g=4)
            V = lambda fn, r, w: tr.op("dve", fn, reads=r, writes=w)
            V(lambda e: e.tensor_reduce(out=gmax[:], in_=gl, axis=AX.X, op=ALU.max), [lg], [gmax])
            V(lambda e: e.tensor_tensor(out=mg[:], in0=gl, in1=gmax[:].unsqueeze(2).to_broadcast([128, 16, 4]), op=ALU.is_equal), [lg, gmax], [mg])
            V(lambda e: e.tensor_tensor(out=eg[:], in0=gl, in1=gmax[:].unsqueeze(2).to_broadcast([128, 16, 4]), op=ALU.subtract), [lg, gmax], [eg])
            tr.op("act", lambda e: e.activation(out=eg[:], in_=eg[:], func=AF.Exp), reads=[eg], writes=[eg])
            V(lambda e: e.tensor_reduce(out=gsum[:], in_=eg[:], axis=AX.X, op=ALU.add), [eg], [gsum])
            V(lambda e: e.reciprocal(out=pg[:], in_=gsum[:]), [gsum], [pg])
            V(lambda e: e.tensor_tensor(out=t48[:], in0=el, in1=mg[:].unsqueeze(3).to_broadcast([128, 16, 4, 8]), op=ALU.mult), [lg, mg], [t48])
            V(lambda e: e.tensor_reduce(out=ein[:], in_=t48[:].rearrange("p t g x -> p t x g"), axis=AX.X, op=ALU.add), [t48], [ein])
            V(lambda e: e.tensor_reduce(out=m1[:], in_=ein[:], axis=AX.X, op=ALU.max), [ein], [m1])
            V(lambda e: e.tensor_tensor(out=k1[:], in0=ein[:], in1=m1[:].unsqueeze(2).to_broadcast([128, 16, 8]), op=ALU.is_equal), [ein, m1], [k1])
            V(lambda e: e.scalar_tensor_tensor(out=e2[:], in0=k1[:], scalar=-1.0e30, in1=ein[:], op0=ALU.mult, op1=ALU.add), [k1, ein], [e2])
            V(lambda e: e.tensor_reduce(out=m2[:], in_=e2[:], axis=AX.X, op=ALU.max), [e2], [m2])
            V(lambda e: e.tensor_tensor(out=k2[:], in0=e2[:], in1=m2[:].unsqueeze(2).to_broadcast([128, 16, 8]), op=ALU.is_equal), [e2, m2], [k2])
            V(lambda e: e.tensor_tensor(out=dd_[:], in0=m2[:], in1=m1[:], op=ALU.subtract), [m1, m2], [dd_])
            tr.op("act", lambda e: e.activation(out=dd_[:], in_=dd_[:], func=AF.Exp), reads=[dd_], writes=[dd_])
            V(lambda e: e.tensor_scalar(out=w1[:], in0=dd_[:], scalar1=1.0, scalar2=None, op0=ALU.add), [dd_], [w1])
            V(lambda e: e.reciprocal(out=w1[:], in_=w1[:]), [w1], [w1])
            V(lambda e: e.tensor_tensor(out=w2[:], in0=dd_[:], in1=w1[:], op=ALU.mult), [dd_, w1], [w2])
            V(lambda e: e.tensor_tensor(out=w1[:], in0=w1[:], in1=pg[:], op=ALU.mult), [w1, pg], [w1])
            V(lambda e: e.tensor_tensor(out=w2[:], in0=w2[:], in1=pg[:], op=ALU.mult), [w2, pg], [w2])
            V(lambda e: e.tensor_tensor(out=k1[:], in0=k1[:], in1=w1[:].unsqueeze(2).to_broadcast([128, 16, 8]), op=ALU.mult), [k1, w1], [k1])
            V(lambda e: e.tensor_tensor(out=k2[:], in0=k2[:], in1=w2[:].unsqueeze(2).to_broadcast([128, 16, 8]), op=ALU.mult), [k2, w2], [k2])
            V(lambda e: e.tensor_tensor(out=cw8[:], in0=k1[:], in1=k2[:], op=ALU.add), [k1, k2], [cw8])
            V(lambda e: e.tensor_tensor(out=c["comb"][:].rearrange("p t (g x) -> p t g x", g=4), in0=mg[:].unsqueeze(3).to_broadcast([128, 16, 4, 8]),
                                        in1=cw8[:].unsqueeze(2).to_broadcast([128, 16, 4, 8]), op=ALU.mult), [mg, cw8], [c["comb"]])
            self.tap("comb", c["comb"][:].rearrange("p a b -> p (a b)"), [128, 512], F32, [c["comb"]])
            self.barrier_release(rel)

    def stage_moe(self, st):
        tr, c, I = self.tr, self.c, self.I
        self.fence()
        h_lat, h2T, comb = c["h_lat"], c["h2T"], c["comb"]
        with ExitStack() as s2:
            wgt = [self.sb(s2, "mwg%d" % i, [128, 8, DFF], BF16) for i in range(2)]
            wut = [self.sb(s2, "mwu%d" % i, [128, 8, DFF], BF16) for i in range(2)]
            wdt = [self.sb(s2, "mwd%d" % i, [128, 4, D], BF16) for i in range(2)]
            stg = [self.sb(s2, "mstg%d" % i, [128, 8, DFF], F32) for i in range(2)]
            stgs = [self.dsem() for _ in range(2)]
            ns = [0]
            sg_ = [self.sb(s2, "msg%d" % i, [128, 512], F32) for i in range(2)]
            heT = [self.sb(s2, "heT%d" % i, [128, 4, 512], BF16) for i in range(2)]
            pG = [self.ps(s2, "mpG%d" % i) for i in range(2)]
            pU = [self.ps(s2, "mpU%d" % i) for i in range(2)]
            pDn = [self.ps(s2, "mpD%d" % i) for i in range(4)]
            rel = wgt + wut + wdt + sg_ + heT + pG + pU + pDn + stg
            nb = 0
            for ex in range(NEXP):
                k = ex % 2
                wg_e, wu_e, wd_e = wgt[k], wut[k], wdt[k]
                for (dst, src) in ((wg_e, I["w_gate"].t[ex].rearrange("(kc p) n -> p kc n", p=128)), (wu_e, I["w_up"].t[ex].rearrange("(kc p) n -> p kc n", p=128))):
                    sg_t, sg_s = stg[ns[0] % 2], stgs[ns[0] % 2]
                    ns[0] += 1
                    tr.dma("sp", sg_s, out=sg_t[:], in_=src, writes=[sg_t])
                    tr.op("pool", lambda e, dst=dst, sg_t=sg_t: e.tensor_copy(out=dst[:], in_=sg_t[:]), reads=[sg_t], writes=[dst])
                sg_t, sg_s = stg[ns[0] % 2], stgs[ns[0] % 2]
                ns[0] += 1
                sv = sg_t[:].rearrange("p a b -> p (a b)").rearrange("p (f n) -> p f n", f=4)
                tr.dma("sp", sg_s, out=sv, in_=I["w_down"].t[ex].rearrange("(fc p) n -> p fc n", p=128), writes=[sg_t])
                tr.op("pool", lambda e, wd_e=wd_e, sv=sv: e.tensor_tensor(out=wd_e[:], in0=sv, in1=c["g2_bc"][:].unsqueeze(1).to_broadcast([128, 4, D]), op=ALU.mult),
                      reads=[sg_t, c["g2_bc"]], writes=[wd_e])
                for j in range(4):
                    he = heT[nb % 2]
                    nb += 1
                    hres = [c["h2_r"][j * 4 + q] for q in range(4)]
                    for fc in range(4):
                        g_p, u_p, sg = pG[fc % 2], pU[fc % 2], sg_[fc % 2]
                        for kc in range(8):
                            tr.op("pe", lambda e, kc=kc, fc=fc, g_p=g_p: e.matmul(out=g_p[:], lhsT=wg_e[:, kc, fc * 128:(fc + 1) * 128], rhs=h2T[:, kc, j * 512:(j + 1) * 512],
                                                                               start=(kc == 0), stop=(kc == 7)), reads=[wg_e] + hres, writes=[g_p])
                        for kc in range(8):
                            tr.op("pe", lambda e, kc=kc, fc=fc, u_p=u_p: e.matmul(out=u_p[:], lhsT=wu_e[:, kc, fc * 128:(fc + 1) * 128], rhs=h2T[:, kc, j * 512:(j + 1) * 512],
                                                                               start=(kc == 0), stop=(kc == 7)), reads=[wu_e] + hres, writes=[u_p])
                        tr.op("act", lambda e, g_p=g_p, sg=sg: e.activation(out=sg[:], in_=g_p[:], func=AF.Silu), reads=[g_p], writes=[sg])
                        tr.op("dve", lambda e, u_p=u_p, sg=sg, fc=fc, he=he: e.tensor_tensor(out=he[:, fc, :], in0=u_p[:], in1=sg[:], op=ALU.mult), reads=[u_p, sg], writes=[he])
                    for tt in range(4):
                        ti = j * 4 + tt
                        for hh in range(2):
                            d_p = pDn[(tt * 2 + hh) % 4]
                            for fc in range(4):
                                tr.op("pe", lambda e, fc=fc, d_p=d_p, tt=tt, hh=hh, he=he: e.matmul(out=d_p[:], lhsT=he[:, fc, tt * 128:(tt + 1) * 128], rhs=wd_e[:, fc, hh * 512:(hh + 1) * 512],
                                                                                              start=(fc == 0), stop=(fc == 3)), reads=[he, wd_e], writes=[d_p])
                            tr.op("dve", lambda e, d_p=d_p, ti=ti, hh=hh, ex=ex: e.scalar_tensor_tensor(
                                out=h_lat[:, ti, hh * 512:(hh + 1) * 512], in0=d_p[:], scalar=comb[:, ti, ex:ex + 1], in1=h_lat[:, ti, hh * 512:(hh + 1) * 512], op0=ALU.mult, op1=ALU.add),
                                reads=[d_p, comb, c["h_r"][ti]], writes=[c["h_r"][ti]])
            self.tap("h_fin", h_lat[:].rearrange("p a b -> p (a b)"), [128, 16 * D], F32, c["h_r"])
            self.barrier_release(rel)

    def stage_final(self, st):
        tr, c, I = self.tr, self.c, self.I
        self.fence()
        h_lat = c["h_lat"]
        out_v = self.out.t.rearrange("(n p) c -> n p c", p=128)
        with ExitStack() as s2:
            fn = self.sb(s2, "fn_bc", [128, D], F32)
            c_eps = self.sb(s2, "c_eps4", [128, 1], F32)
            junk = self.sb(s2, "fjunk", [128, D], BF16)
            ss = [self.sb(s2, "fss%d" % i, [128, 1], F32) for i in range(2)]
            rs = [self.sb(s2, "frs%d" % i, [128, 1], F32) for i in range(2)]
            ob = [self.sb(s2, "fob%d" % i, [128, D], F32) for i in range(2)]
            obs = [self.dsem() for _ in range(2)]
            tr.dma("sp", self.dsem(), out=fn[:], in_=I["final_norm"].t.partition_broadcast(128), writes=[fn])
            tr.op("pool", lambda e: e.memset(c_eps[:], EPS), writes=[c_eps])
            for i in range(16):
                hl = h_lat[:, i, :]
                s_, r_, o_ = ss[i % 2], rs[i % 2], ob[i % 2]
                tr.op("act", lambda e, s_=s_, hl=hl: e.activation(out=junk[:], in_=hl, func=AF.Square, accum_out=s_[:]), reads=[c["h_r"][i]], writes=[junk, s_])
                tr.op("act", lambda e, s_=s_, r_=r_: e.activation(out=r_[:], in_=s_[:], func=AF.Ln, scale=1.0 / D, bias=c_eps[:]), reads=[s_, c_eps], writes=[r_])
                tr.op("act", lambda e, r_=r_: e.activation(out=r_[:], in_=r_[:], func=AF.Exp, scale=-0.5), reads=[r_], writes=[r_])
                tr.op("dve", lambda e, r_=r_, o_=o_, hl=hl: e.scalar_tensor_tensor(out=o_[:], in0=hl, scalar=r_[:], in1=fn[:], op0=ALU.mult, op1=ALU.mult),
                      reads=[c["h_r"][i], r_, fn], writes=[o_])
                tr.dma("sp", obs[i % 2], out=out_v[i], in_=o_[:], reads=[o_], writes=[])
            self.final += ob


def prep_core(inp, b, hf):
    L = 0
    rev = hf == 1
    x, ctx = inp["x"][b], inp["ctx"][b]
    if not rev:
        ctx_a, own, oth = ctx, x[0:2048], x[2048:4096]
        dA, dB = 0, 1
    else:
        ctx_a, own, oth = ctx[::-1], x[2048:4096][::-1], x[0:2048][::-1]
        dA, dB = 1, 0
    m = {}
    m["xs"] = np.ascontiguousarray(np.concatenate([ctx_a, own, oth], axis=0), dtype=np.float32)
    cT = np.stack([inp["c"][b].reshape(8, 128).T, inp["c_ctx"].reshape(8, 128).T], axis=2).reshape(128, 16)
    m["cT"] = np.ascontiguousarray(cT, dtype=np.float32)
    m["w_ada"] = np.ascontiguousarray(inp["w_ada"][L])
    m["b_ada"] = np.ascontiguousarray(inp["b_ada"][L].reshape(1, -1))
    m["norm_mix_fm"] = np.ascontiguousarray(inp["norm_mix"][L].reshape(8, 128).T)
    m["norm_ffn_fm"] = np.ascontiguousarray(inp["norm_ffn"][L].reshape(8, 128).T)
    w_in = inp["w_in"][L]
    if rev:
        w_in = np.concatenate([w_in[:, :OFF_G], w_in[:, OFF_G + 16:OFF_G + 32], w_in[:, OFF_G:OFF_G + 16],
                               w_in[:, OFF_Z:OFF_DT], w_in[:, OFF_DT + 16:OFF_DT + 32], w_in[:, OFF_DT:OFF_DT + 16]], axis=1)
    m["w_in"] = np.ascontiguousarray(w_in)
    wu, gb = inp["gla_w_up"][L], inp["gla_b"][L]
    m["w_up_aug"] = np.ascontiguousarray(np.stack([np.concatenate([wu[dA], gb[dA][None, :]], axis=0),
                                                   np.concatenate([wu[dB], gb[dB][None, :]], axis=0)], axis=0))
    m["gla_norm"] = np.ascontiguousarray(inp["gla_norm"][L].reshape(1, -1))
    m["ssd_norm"] = np.ascontiguousarray(inp["ssd_norm"][L].reshape(1, -1))
    m["final_norm"] = np.ascontiguousarray(inp["final_norm"].reshape(1, -1))
    cw = inp["ssd_conv_w"][L]
    if rev:
        cw = cw[::-1, ::-1, :]
    m["conv_w_fm"] = np.ascontiguousarray(cw.reshape(9, 12, 128).transpose(2, 1, 0))
    m["conv_b_fm"] = np.ascontiguousarray(inp["ssd_conv_b"][L].reshape(12, 128).T)
    m["dt_bias"] = np.ascontiguousarray(np.concatenate([inp["ssd_dt_bias"][L][dA], inp["ssd_dt_bias"][L][dB]]).reshape(1, 32))
    m["a_log"] = np.ascontiguousarray(np.concatenate([inp["ssd_a_log"][L][dA], inp["ssd_a_log"][L][dB]]).reshape(1, 32))
    m["ssd_d"] = np.ascontiguousarray(inp["ssd_d"][L].reshape(1, 16))
    m["w_out"] = np.ascontiguousarray(inp["w_out"][L])
    wrt = np.concatenate([inp["router_group_w"][L], inp["router_expert_w"][L]], axis=1)
    m["w_router"] = np.ascontiguousarray(wrt.reshape(8, 128, 36).transpose(1, 0, 2).reshape(128, 8 * 36))
    m["b_router"] = np.ascontiguousarray(np.concatenate([inp["router_group_b"][L], inp["router_expert_b"][L]]).reshape(1, 36))
    m["w_gate"] = np.ascontiguousarray(inp["expert_w_gate"][L])
    m["w_up"] = np.ascontiguousarray(inp["expert_w_up"][L])
    m["w_down"] = np.ascontiguousarray(inp["expert_w_down"][L])
    return {k: np.asarray(v, dtype=np.float32) for k, v in m.items()}


def run(inputs, debug=None, stop_after=None, cores=8):
    bld = Builder(debug=debug, stop_after=stop_after)
    nc = bld.build()
    in_maps = [prep_core(inputs, i // 2, i % 2) for i in range(cores)]
    res = run_bass_kernel_spmd(nc, in_maps, core_ids=list(range(cores)))
    return res, bld


def kernel(**inputs):
    inputs = {k: np.asarray(v) for k, v in inputs.items()}
    res, _ = run(inputs)
    out = np.empty((4, 4096, D), dtype=np.float32)
    for i in range(8):
        b, hf = i // 2, i % 2
        o = np.asarray(res.results[i]["out"], dtype=np.float32)
        if hf == 0:
            out[b, 0:2048] = o
        else:
            out[b, 2048:4096] = o[::-1]
    return out
```

```python
import math
from contextlib import ExitStack

import numpy as np
import concourse.bass as bass
import concourse.mybir as mybir
from concourse.bass_utils import run_bass_kernel_spmd

F32 = mybir.dt.float32
BF16 = mybir.dt.bfloat16
AF = mybir.ActivationFunctionType
ALU = mybir.AluOpType
AX = mybir.AxisListType

D = 1024
NCTX, NOWN, NOTH = 256, 2048, 2048
TOK = NCTX + NOWN + NOTH
NT = TOK // 128
T_CTX0, T_OWN0, T_OTH0 = 0, 2, 18
EPS = 1e-6
IN_W = 5696
OFF_K, OFF_V, OFF_R, OFF_G, OFF_Z, OFF_XBC, OFF_DT = 512, 1024, 2048, 3072, 3104, 4128, 5664
NEXP, DFF = 32, 512


class Res:
    __slots__ = ("name", "lw", "rd")

    def __init__(self, name=""):
        self.name = name
        self.lw = None
        self.rd = {}


class Tile:
    def __init__(self, t, name):
        self.t = t
        self.r = Res(name)

    def __getitem__(self, idx):
        return self.t[idx]


class Tracker:
    ENG = ("pe", "act", "dve", "pool", "sp")
    CH = 2000

    def __init__(self, nc, sems, dma_sems, same_engine_sync=True):
        self.nc = nc
        self.eng = {"pe": nc.tensor, "act": nc.scalar, "dve": nc.vector, "pool": nc.gpsimd, "sp": nc.sync}
        self.cnt = {e: 0 for e in self.ENG}
        self.waited = {e: {} for e in self.ENG}
        self.sems = {e: [sems[e]] for e in sems}
        self.free_dma = list(dma_sems)
        self.same = same_engine_sync
        self.ninst = 0
        self.rec = None

    def new_dma_sem(self, group=0):
        d = self.free_dma.pop()
        self._uid = getattr(self, "_uid", 0) + 1
        d = d if isinstance(d, list) else [d, 0, 0, 0, "dma%d" % self._uid]
        if group:
            d[2] = group
            d[3] = d[1] + 16 * group
        return d

    def regroup(self, d, n):
        if self.rec is not None:
            self.rec.append(("call", lambda: self.regroup(d, n)))
            return
        assert d[2] == 0
        d[2] = n
        d[3] = d[1] + 16 * n

    def begin_record(self):
        self.rec = []

    def end_record(self):
        r, self.rec = self.rec, None
        return r

    def mark(self, name):
        self.rec.append(("mark", name))

    def _emit_item(self, it):
        if it[0] == "op":
            self.op(*it[1:])
        elif it[0] == "dma":
            self.dma(*it[1:])
        elif it[0] == "call":
            it[1]()

    def run_pipelined(self, records, depth=2, serial_fronts=False):
        assert self.rec is None
        active = []
        nxt = 0
        finished = -1
        sdone = -1
        while active or nxt < len(records):
            while len(active) < depth and nxt < len(records):
                if serial_fronts and active and not active[-1][3]:
                    break
                active.append([records[nxt], 0, nxt, False])
                nxt += 1
            progressed = False
            for a in list(active):
                lst, pos, idx, _ = a
                if pos >= len(lst):
                    a[3] = True
                    active.remove(a)
                    finished = max(finished, idx)
                    sdone = max(sdone, idx)
                    progressed = True
                    continue
                it = lst[pos]
                if it[0] == "mark":
                    if it[1] == "need_state":
                        a[3] = True
                        if sdone < idx - 1:
                            continue
                    if it[1] == "state_done":
                        sdone = max(sdone, idx)
                    a[1] += 1
                    progressed = True
                    continue
                self._emit_item(it)
                a[1] += 1
                progressed = True
            assert progressed

    def release_dma_sem(self, d):
        self.free_dma.append(d)

    def _wait(self, e, ev):
        if ev is None:
            return
        if ev[0] == "dma":
            _, s, v, key = ev
            if self.waited[e].get(key, 0) >= v:
                return
            self.waited[e][key] = v
            self.eng[e].wait_ge(s, v)
        else:
            pe, n = ev
            if pe == e and (not self.same or e in ("pe", "sp")):
                return
            if self.waited[e].get(pe, 0) >= n:
                return
            self.waited[e][pe] = n
            self.eng[e].wait_ge(self.sems[pe][(n - 1) // self.CH], (n - 1) % self.CH + 1)

    def _deps(self, e, reads, writes):
        for r in reads:
            self._wait(e, r.lw)
        for w in writes:
            self._wait(e, w.lw)
            for ev in w.rd.values():
                self._wait(e, ev)

    @staticmethod
    def _note_read(r, ev):
        key = ev[3] if ev[0] == "dma" else ev[0]
        old = r.rd.get(key)
        if old is None or (old[2] if old[0] == "dma" else old[1]) < (ev[2] if ev[0] == "dma" else ev[1]):
            r.rd[key] = ev

    def op(self, e, fn, reads=(), writes=()):
        if self.rec is not None:
            self.rec.append(("op", e, fn, list(reads), list(writes)))
            return None
        reads = [x.r if isinstance(x, Tile) else x for x in reads]
        writes = [x.r if isinstance(x, Tile) else x for x in writes]
        self._deps(e, reads, writes)
        self.cnt[e] += 1
        ev = (e, self.cnt[e])
        k = (self.cnt[e] - 1) // self.CH
        if k >= len(self.sems[e]):
            self.sems[e].append(self.free_dma.pop(0))
        fn(self.eng[e]).then_inc(self.sems[e][k], 1)
        self.ninst += 1
        for r in reads:
            self._note_read(r, ev)
        for w in writes:
            w.lw = ev
            w.rd = {}
        return ev

    def dma(self, e, dsem, out, in_, reads=(), writes=()):
        if self.rec is not None:
            self.rec.append(("dma", e, dsem, out, in_, list(reads), list(writes)))
            return None
        reads = [x.r if isinstance(x, Tile) else x for x in reads]
        writes = [x.r if isinstance(x, Tile) else x for x in writes]
        self._deps(e, reads, writes)
        if dsem[2] == 0:
            dsem[2] = 1
            dsem[3] = dsem[1] + 16
        dsem[1] += 16
        dsem[2] -= 1
        ev = ("dma", dsem[0], dsem[3], dsem[4])
        self.eng[e].dma_start(out=out, in_=in_).then_inc(dsem[0], 16)
        self.ninst += 1
        for r in reads:
            self._note_read(r, ev)
        for w in writes:
            w.lw = ev
            w.rd = {}
        return ev

    def wait_all(self, e, resources):
        for r in resources:
            r = r.r if isinstance(r, Tile) else r
            self._wait(e, r.lw)
            for ev in r.rd.values():
                self._wait(e, ev)


class Builder:
    def __init__(self, debug=None, stop_after=None):
        self.debug = debug or ()
        self.stop_after = stop_after
        self.nc = bass.Bass("TRN2", target_bir_lowering=False)
        self.dbg_out = {}

    def sb(self, st, name, shape, dt):
        self._uid = getattr(self, "_uid", 0) + 1
        return Tile(st.enter_context(self.nc.sbuf_tensor("sb%d_%s" % (self._uid, name), list(shape), dt)), name)

    def ps(self, st, name, shape=(128, 512), dt=F32):
        self._uid = getattr(self, "_uid", 0) + 1
        return Tile(st.enter_context(self.nc.psum_tensor("ps%d_%s" % (self._uid, name), list(shape), dt)), name)

    def dram_in(self, name, shape, dt=F32):
        return Tile(self.nc.dram_tensor(name, list(shape), dt, kind="ExternalInput").ap(), name)

    def dram_out(self, name, shape, dt=F32):
        return Tile(self.nc.dram_tensor(name, list(shape), dt, kind="ExternalOutput").ap(), name)

    def dram_scr(self, name, shape, dt):
        return Tile(self.nc.dram_tensor(name, list(shape), dt, kind="Internal").ap(), name)

    def dsem(self, group=0):
        return self.tr.new_dma_sem(group)

    def build(self):
        nc = self.nc
        I = {}
        I["xs"] = self.dram_in("xs", [TOK, D])
        I["cT"] = self.dram_in("cT", [128, 16])
        I["w_ada"] = self.dram_in("w_ada", [D, 6 * D])
        I["b_ada"] = self.dram_in("b_ada", [1, 6 * D])
        I["norm_mix_fm"] = self.dram_in("norm_mix_fm", [128, 8])
        I["norm_ffn_fm"] = self.dram_in("norm_ffn_fm", [128, 8])
        I["w_in"] = self.dram_in("w_in", [D, IN_W])
        I["w_up_aug"] = self.dram_in("w_up_aug", [2, 17, 512])
        I["gla_norm"] = self.dram_in("gla_norm", [1, 256])
        I["ssd_norm"] = self.dram_in("ssd_norm", [1, 1024])
        I["final_norm"] = self.dram_in("final_norm", [1, 1024])
        I["conv_w_fm"] = self.dram_in("conv_w_fm", [128, 12, 9])
        I["conv_b_fm"] = self.dram_in("conv_b_fm", [128, 12])
        I["dt_bias"] = self.dram_in("dt_bias", [1, 32])
        I["a_log"] = self.dram_in("a_log", [1, 32])
        I["ssd_d"] = self.dram_in("ssd_d", [1, 16])
        I["w_out"] = self.dram_in("w_out", [2048, D])
        I["w_router"] = self.dram_in("w_router", [128, 8 * 36])
        I["b_router"] = self.dram_in("b_router", [1, 36])
        I["w_gate"] = self.dram_in("w_gate", [NEXP, D, DFF])
        I["w_up"] = self.dram_in("w_up", [NEXP, D, DFF])
        I["w_down"] = self.dram_in("w_down", [NEXP, DFF, D])
        self.I = I
        self.out = self.dram_out("out", [NOWN, D])

        with ExitStack() as st:
            sems = {e: st.enter_context(nc.semaphore("s_" + e)) for e in Tracker.ENG}
            dsems = [st.enter_context(nc.semaphore("d%d" % i)) for i in range(90)]
            self.tr = Tracker(nc, sems, dsems)
            self.program(st)
        return nc

    def tap(self, name, tile_ap, shape, dt, reads):
        if name not in self.debug:
            return
        o = self.dram_out("dbg_" + name, shape, dt)
        self.dbg_out[name] = o
        n = shape[1]
        step = 2048
        d = self.dsem(len(range(0, n, step)))
        for c0 in range(0, n, step):
            c1 = min(n, c0 + step)
            self.tr.dma("sp", d, out=o.t[:, c0:c1], in_=tile_ap[:, c0:c1], reads=reads, writes=[o])
        self.final.append(o)

    def program(self, st):
        tr = self.tr
        self.final = []
        self.consts(st)
        self.stage_adaln(st)
        with ExitStack() as mst:
            self.stage_hT(mst)
            if self.stop_after == "hT":
                return self.finish()
            self.stage_gla(mst)
            if self.stop_after == "gla":
                return self.finish()
            self.stage_conv(mst)
            if self.stop_after == "conv":
                return self.finish()
            self.stage_ssd(mst)
            if self.stop_after == "ssd":
                return self.finish()
            self.barrier_release([self.c["hT"], self.c["BT"], self.c["CT"]] + self.c["hT_r"])
        self.stage_post(st)
        if self.stop_after == "post":
            return self.finish()
        self.stage_moe(st)
        if self.stop_after == "moe":
            return self.finish()
        self.stage_final(st)
        return self.finish()

    def finish(self):
        self.tr.wait_all("sp", self.final)

    def consts(self, st):
        tr = self.tr
        c = {}
        self.c = c
        c["ident_f"] = self.sb(st, "ident_f", [128, 128], F32)
        c["ident_b"] = self.sb(st, "ident_b", [128, 128], BF16)
        c["ones_f"] = self.sb(st, "ones_f", [128, 128], F32)
        for nm in ("tri_le", "tri_ge", "tri_gt", "tri_lt"):
            c[nm] = self.sb(st, nm, [128, 128], F32)
        idf = c["ident_f"]
        tr.op("pool", lambda e: e.memset(idf[:], 0.0), writes=[idf])
        tr.op("pool", lambda e: e.affine_select(out=idf[:], in_=idf[:], pattern=[[-1, 128]], compare_op=ALU.not_equal,
                                               fill=1.0, base=0, channel_multiplier=1), reads=[idf], writes=[idf])
        tr.op("pool", lambda e: e.tensor_copy(out=c["ident_b"][:], in_=idf[:]), reads=[idf], writes=[c["ident_b"]])
        tr.op("pool", lambda e: e.memset(c["ones_f"][:], 1.0), writes=[c["ones_f"]])
        specs = {"tri_le": (ALU.is_gt, 0), "tri_ge": (ALU.is_gt, 0), "tri_gt": (ALU.is_gt, 0), "tri_lt": (ALU.is_gt, 0)}
        t = c["tri_le"]
        tr.op("pool", lambda e: e.memset(t[:], 1.0), writes=[t])
        tr.op("pool", lambda e: e.affine_select(out=t[:], in_=t[:], pattern=[[1, 128]], compare_op=ALU.is_ge,
                                               fill=0.0, base=0, channel_multiplier=-1), reads=[t], writes=[t])
        t2 = c["tri_ge"]
        tr.op("pool", lambda e: e.memset(t2[:], 1.0), writes=[t2])
        tr.op("pool", lambda e: e.affine_select(out=t2[:], in_=t2[:], pattern=[[-1, 128]], compare_op=ALU.is_ge,
                                               fill=0.0, base=0, channel_multiplier=1), reads=[t2], writes=[t2])
        t3 = c["tri_gt"]
        tr.op("pool", lambda e: e.memset(t3[:], 1.0), writes=[t3])
        tr.op("pool", lambda e: e.affine_select(out=t3[:], in_=t3[:], pattern=[[-1, 128]], compare_op=ALU.is_gt,
                                               fill=0.0, base=0, channel_multiplier=1), reads=[t3], writes=[t3])
        t4 = c["tri_lt"]
        tr.op("pool", lambda e: e.memset(t4[:], 1.0), writes=[t4])
        tr.op("pool", lambda e: e.affine_select(out=t4[:], in_=t4[:], pattern=[[1, 128]], compare_op=ALU.is_gt,
                                               fill=0.0, base=0, channel_multiplier=-1), reads=[t4], writes=[t4])
        self.tap("tri_le", c["tri_le"][:], [128, 128], F32, [c["tri_le"]])
        self.tap("tri_gt", c["tri_gt"][:], [128, 128], F32, [c["tri_gt"]])

    def stage_adaln(self, st):
        tr, c, I = self.tr, self.c, self.I
        c["mod_fm"] = self.sb(st, "mod_fm", [128, 6, 8, 2], F32)
        c["g1_bc"] = self.sb(st, "g1_bc", [128, D], F32)
        c["g2_bc"] = self.sb(st, "g2_bc", [128, D], F32)
        c["s1"] = self.sb(st, "s1", [128, 8], F32)
        c["s1c"] = self.sb(st, "s1c", [128, 8], F32)
        c["b1"] = self.sb(st, "b1", [128, 8], F32)
        c["b1c"] = self.sb(st, "b1c", [128, 8], F32)
        c["s2"] = self.sb(st, "s2", [128, 8], F32)
        c["b2"] = self.sb(st, "b2", [128, 8], F32)
        with ExitStack() as s2:
            cT = self.sb(s2, "cT", [128, 16], F32)
            scT = self.sb(s2, "scT", [128, 16], F32)
            sc_rep = self.sb(s2, "sc_rep", [128, 8, 128], F32)
            brow = self.sb(s2, "brow", [1, 6 * D], F32)
            nm = self.sb(s2, "nm", [128, 8], F32)
            nf = self.sb(s2, "nf", [128, 8], F32)
            wblk = [self.sb(s2, "wblk%d" % i, [128, 8, D], F32) for i in range(2)]
            wsem = [self.dsem() for _ in range(2)]
            modps = self.ps(s2, "modps", [128, 512], F32)
            gps = [self.ps(s2, "gps%d" % i, [128, 512], F32) for i in range(2)]
            d = self.dsem(4)
            tr.dma("sp", d, out=cT[:], in_=I["cT"].t, writes=[cT])
            tr.dma("sp", d, out=brow[:], in_=I["b_ada"].t, writes=[brow])
            tr.dma("sp", d, out=nm[:], in_=I["norm_mix_fm"].t, writes=[nm])
            tr.dma("sp", d, out=nf[:], in_=I["norm_ffn_fm"].t, writes=[nf])
            tr.op("act", lambda e: e.activation(out=scT[:], in_=cT[:], func=AF.Silu), reads=[cT], writes=[scT])
            tr.op("dve", lambda e: e.tensor_copy(out=sc_rep[:], in_=scT[:].rearrange("p (k j) -> p k j", j=2)[:, :, 0:1].to_broadcast([128, 8, 128])),
                  reads=[scT], writes=[sc_rep])
            w_ada = I["w_ada"].t.rearrange("(kc p) n -> p kc n", p=128)
            mview = modps[:, 0:96].rearrange("p (b f t) -> p b f t", b=6, f=8)
            for blk in range(6):
                wb = wblk[blk % 2]
                tr.dma("sp", wsem[blk % 2], out=wb[:], in_=w_ada[:, :, blk * D:(blk + 1) * D], writes=[wb])
                if blk in (0, 1, 3, 4):
                    for fc in range(8):
                        for kc in range(8):
                            tr.op("pe", lambda e, fc=fc, kc=kc, wb=wb, blk=blk: e.matmul(
                                out=mview[:, blk, fc, :], lhsT=wb[:, kc, fc * 128:(fc + 1) * 128],
                                rhs=scT[:, 2 * kc:2 * kc + 2], start=(kc == 0), stop=False),
                                reads=[wb, scT], writes=[modps])
                        tr.op("pe", lambda e, fc=fc, blk=blk: e.matmul(
                            out=mview[:, blk, fc, :], lhsT=brow[0:1, blk * D + fc * 128: blk * D + (fc + 1) * 128],
                            rhs=c["ones_f"][0:1, 0:2], start=False, stop=True),
                            reads=[brow, c["ones_f"]], writes=[modps])
                else:
                    gdst = c["g1_bc"] if blk == 2 else c["g2_bc"]
                    for hh in range(2):
                        for kc in range(8):
                            tr.op("pe", lambda e, hh=hh, kc=kc, wb=wb: e.matmul(
                                out=gps[hh][:], lhsT=sc_rep[:, kc, :], rhs=wb[:, kc, hh * 512:(hh + 1) * 512],
                                start=(kc == 0), stop=False), reads=[wb, sc_rep], writes=[gps[hh]])
                        tr.op("pe", lambda e, hh=hh, blk=blk: e.matmul(
                            out=gps[hh][:], lhsT=c["ones_f"][0:1, :], rhs=brow[0:1, blk * D + hh * 512: blk * D + (hh + 1) * 512],
                            start=False, stop=True), reads=[brow, c["ones_f"]], writes=[gps[hh]])
                        tr.op("act", lambda e, hh=hh, gdst=gdst: e.activation(out=gdst[:, hh * 512:(hh + 1) * 512], in_=gps[hh][:], func=AF.Copy),
                              reads=[gps[hh]], writes=[gdst])
            mf = c["mod_fm"]
            mflat = mf[:].rearrange("p b f t -> p (b f t)")
            tr.op("dve", lambda e: e.tensor_copy(out=mflat[:, 0:32], in_=modps[:, 0:32]), reads=[modps], writes=[mf])
            tr.op("dve", lambda e: e.tensor_copy(out=mflat[:, 48:80], in_=modps[:, 48:80]), reads=[modps], writes=[mf])
            tr.op("dve", lambda e: e.scalar_tensor_tensor(out=c["s1"][:], in0=mf[:, 1, :, 0], scalar=1.0, in1=nm[:], op0=ALU.add, op1=ALU.mult),
                  reads=[mf, nm], writes=[c["s1"]])
            tr.op("dve", lambda e: e.scalar_tensor_tensor(out=c["s1c"][:], in0=mf[:, 1, :, 1], scalar=1.0, in1=nm[:], op0=ALU.add, op1=ALU.mult),
                  reads=[mf, nm], writes=[c["s1c"]])
            tr.op("dve", lambda e: e.scalar_tensor_tensor(out=c["s2"][:], in0=mf[:, 4, :, 0], scalar=1.0, in1=nf[:], op0=ALU.add, op1=ALU.mult),
                  reads=[mf, nf], writes=[c["s2"]])
            tr.op("dve", lambda e: e.tensor_copy(out=c["b1"][:], in_=mf[:, 0, :, 0]), reads=[mf], writes=[c["b1"]])
            tr.op("dve", lambda e: e.tensor_copy(out=c["b1c"][:], in_=mf[:, 0, :, 1]), reads=[mf], writes=[c["b1c"]])
            tr.op("dve", lambda e: e.tensor_copy(out=c["b2"][:], in_=mf[:, 3, :, 0]), reads=[mf], writes=[c["b2"]])
            self.tap("mod_fm", mf[:].rearrange("p b f t -> p (b f t)"), [128, 96], F32, [mf])
            self.tap("g1_bc", c["g1_bc"][:], [128, D], F32, [c["g1_bc"]])
            self.barrier_release([cT, scT, sc_rep, brow, nm, nf, wblk[0], wblk[1], modps, gps[0], gps[1]])

    def barrier_release(self, tiles):
        self.pending = getattr(self, "pending", [])
        for t in tiles:
            self.pending.append(t.r if isinstance(t, Tile) else t)

    def fence(self):
        pend = getattr(self, "pending", [])
        for e in Tracker.ENG:
            self.tr.wait_all(e, pend)
        self.pending = []

    def stage_hT(self, st):
        tr, c, I = self.tr, self.c, self.I
        self.fence()
        c["hT"] = self.sb(st, "hT", [128, 8, TOK], BF16)
        c["hT_r"] = [Res("hT%d" % t) for t in range(NT)]
        with ExitStack() as s2:
            xr = [self.sb(s2, "xr%d" % i, [128, D], F32) for i in range(3)]
            xsem = [self.dsem() for _ in range(3)]
            junk = self.sb(s2, "junk", [128, D], BF16)
            ss = [self.sb(s2, "ss%d" % i, [128, 1], F32) for i in range(3)]
            rstd = [self.sb(s2, "rstd%d" % i, [128, 1], F32) for i in range(3)]
            xn = [self.sb(s2, "xn%d" % i, [128, D], BF16) for i in range(3)]
            tmp = [self.sb(s2, "tmp%d" % i, [128, 8, 128], F32) for i in range(3)]
            tps = [self.ps(s2, "tps%d" % i, [128, 1024], BF16) for i in range(3)]
            epst = self.sb(s2, "epst", [128, 1], F32)
            tr.op("pool", lambda e: e.memset(epst[:], EPS), writes=[epst])
            rel = xr + ss + rstd + xn + tmp + tps + [junk, epst]
            recs = []
            for t in range(NT):
                tr.begin_record()
                x_t, ss_t, rs_t, xn_t, tmp_t, ps_t = xr[t % 3], ss[t % 3], rstd[t % 3], xn[t % 3], tmp[t % 3], tps[t % 3]
                hT_ap = c["hT"][:, :, t * 128:(t + 1) * 128]
                hT_r = c["hT_r"][t]
                isctx = t < T_OWN0
                sc, sh = (c["s1c"], c["b1c"]) if isctx else (c["s1"], c["b1"])
                tr.dma("sp", xsem[t % 3], out=x_t[:], in_=I["xs"].t[t * 128:(t + 1) * 128, :], writes=[x_t])
                tr.op("act", lambda e, x_t=x_t, ss_t=ss_t: e.activation(out=junk[:], in_=x_t[:], func=AF.Square, accum_out=ss_t[:]),
                      reads=[x_t], writes=[junk, ss_t])
                tr.op("act", lambda e, ss_t=ss_t, rs_t=rs_t: e.activation(out=rs_t[:], in_=ss_t[:], func=AF.Ln, scale=1.0 / D, bias=epst[:]),
                      reads=[ss_t, epst], writes=[rs_t])
                tr.op("act", lambda e, rs_t=rs_t: e.activation(out=rs_t[:], in_=rs_t[:], func=AF.Exp, scale=-0.5),
                      reads=[rs_t], writes=[rs_t])
                tr.op("dve", lambda e, x_t=x_t, rs_t=rs_t, xn_t=xn_t: e.tensor_scalar(out=xn_t[:], in0=x_t[:], scalar1=rs_t[:], scalar2=None, op0=ALU.mult),
                      reads=[x_t, rs_t], writes=[xn_t])
                for kc in range(8):
                    tr.op("pe", lambda e, kc=kc, xn_t=xn_t, ps_t=ps_t: e.transpose(out=ps_t[:, kc * 128:(kc + 1) * 128], in_=xn_t[:, kc * 128:(kc + 1) * 128], identity=c["ident_b"][:]),
                          reads=[xn_t, c["ident_b"]], writes=[ps_t])
                tr.op("dve", lambda e, ps_t=ps_t, tmp_t=tmp_t, sc=sc: e.tensor_tensor(
                    out=tmp_t[:], in0=ps_t[:].rearrange("p (k t) -> p k t", k=8), in1=sc[:].unsqueeze(2).to_broadcast([128, 8, 128]), op=ALU.mult),
                    reads=[ps_t, sc], writes=[tmp_t])
                tr.op("pool", lambda e, tmp_t=tmp_t, hT_ap=hT_ap, sh=sh: e.tensor_tensor(
                    out=hT_ap, in0=tmp_t[:], in1=sh[:].unsqueeze(2).to_broadcast([128, 8, 128]), op=ALU.add),
                    reads=[tmp_t, sh], writes=[hT_r])
                recs.append(tr.end_record())
            tr.run_pipelined(recs, depth=3)
            for t in (0, 2, 17, 33):
                if ("hT%d" % t) in self.debug:
                    o = self.dram_out("dbg_hT%d" % t, [128, 8, 128], BF16)
                    tr.dma("sp", self.dsem(), out=o.t, in_=c["hT"][:, :, t * 128:(t + 1) * 128], reads=[c["hT_r"][t]], writes=[o])
                    self.final.append(o)
            self.barrier_release(rel)

    def scratch(self, name, shape, dt):
        if name in self.debug:
            o = self.dram_out("dbg_" + name, shape, dt)
            self.final.append(o)
            return o
        return self.dram_scr(name, shape, dt)

    def stage_conv(self, st):
        tr, c, I = self.tr, self.c, self.I
        self.fence()
        c["x_tok"] = self.scratch("x_tok", [TOK, 1024], BF16)
        c["B_tok"] = self.scratch("B_tok", [TOK, 256], BF16)
        c["BT"] = self.sb(st, "BT", [128, 2, NOWN], BF16)
        c["CT"] = self.sb(st, "CT", [128, 2, NOWN], BF16)
        xtok_v = c["x_tok"].t.rearrange("(n p) c -> p n c", p=128)
        btok_v = c["B_tok"].t.rearrange("(n p) c -> p n c", p=128)
        w_in_v = I["w_in"].t.rearrange("(kc p) n -> p kc n", p=128)
        with ExitStack() as s2:
            wx = [self.sb(s2, "wx%d" % i, [128, 8, 128], BF16) for i in range(2)]
            wxs = [self.dsem() for _ in range(2)]
            cw = self.sb(s2, "cw", [128, 12, 9], F32)
            cb = self.sb(s2, "cb", [128, 12], F32)
            diag = [self.sb(s2, "diag%d" % i, [128, 9, 128], BF16) for i in range(2)]
            pre = [self.sb(s2, "pre%d" % i, [128, 66, 66], BF16) for i in range(2)]
            prec = [self.sb(s2, "prec%d" % i, [128, 258], BF16) for i in range(2)]
            post = [self.sb(s2, "post%d" % i, [128, 512], BF16) for i in range(3)]
            tst = [self.sb(s2, "tst%d" % i, [128, 4, 128], BF16) for i in range(3)]
            tsem = [self.dsem() for _ in range(3)]
            pp = [self.ps(s2, "pp%d" % i) for i in range(2)]
            pc = [self.ps(s2, "pc%d" % i) for i in range(2)]
            pt = [self.ps(s2, "pt%d" % i, [128, 1024], BF16) for i in range(2)]
            rel = wx + diag + pre + prec + post + tst + pp + pc + pt + [cw, cb]
            d0 = self.dsem(2)
            tr.dma("sp", d0, out=cw[:], in_=I["conv_w_fm"].t, writes=[cw])
            tr.dma("sp", d0, out=cb[:], in_=I["conv_b_fm"].t, writes=[cb])
            for i in range(2):
                tr.op("pool", lambda e, i=i: e.memset(pre[i][:], 0.0), writes=[pre[i]])
                tr.op("pool", lambda e, i=i: e.memset(prec[i][:], 0.0), writes=[prec[i]])
            nev = 0
            npost = 0
            for ct in range(12):
                w, dg, pr, prc = wx[ct % 2], diag[ct % 2], pre[ct % 2], prec[ct % 2]
                tr.dma("pool", wxs[ct % 2], out=w[:], in_=w_in_v[:, :, OFF_XBC + ct * 128: OFF_XBC + (ct + 1) * 128], writes=[w])
                tr.op("pool", lambda e, dg=dg, ct=ct: e.tensor_tensor(out=dg[:], in0=c["ident_f"][:].unsqueeze(1).to_broadcast([128, 9, 128]),
                                                                  in1=cw[:, ct, :].unsqueeze(2).to_broadcast([128, 9, 128]), op=ALU.mult),
                      reads=[c["ident_f"], cw], writes=[dg])
                for blk in range(9):
                    p_t = pp[nev % 2]
                    if blk == 0:
                        n, tok0, trs = 256, 0, [0, 1]
                    else:
                        n, tok0 = 512, NCTX + (blk - 1) * 512
                        trs = list(range(T_OWN0 + (blk - 1) * 4, T_OWN0 + blk * 4))
                    for kc in range(8):
                        tr.op("pe", lambda e, kc=kc, p_t=p_t, w=w, n=n, tok0=tok0: e.matmul(
                            out=p_t[:, 0:n], lhsT=w[:, kc, :], rhs=c["hT"][:, kc, tok0:tok0 + n], start=(kc == 0), stop=(kc == 7)),
                            reads=[w] + [c["hT_r"][t] for t in trs], writes=[p_t])
                    if blk == 0:
                        dst = prc[:, 1:257]
                        src = p_t[:, 0:256]
                        wr = prc
                    else:
                        r0 = (blk - 1) * 8
                        dst = pr[:, r0 + 1:r0 + 9, 1:65]
                        src = p_t[:, 0:512].rearrange("p (r q) -> p r q", q=64)
                        wr = pr
                    eng = "act" if nev % 2 == 0 else "dve"
                    if eng == "act":
                        tr.op("act", lambda e, dst=dst, src=src: e.activation(out=dst, in_=src, func=AF.Copy), reads=[p_t], writes=[wr])
                    else:
                        tr.op("dve", lambda e, dst=dst, src=src: e.tensor_copy(out=dst, in_=src), reads=[p_t], writes=[wr])
                    nev += 1
                for blk in range(9):
                    if ct >= 10 and (blk == 0 or blk >= 5):
                        continue
                    c_t = pc[blk % 2]
                    if blk == 0:
                        n = 256
                        for kw in range(3):
                            tr.op("pe", lambda e, kw=kw, c_t=c_t, dg=dg, prc=prc: e.matmul(
                                out=c_t[:, 0:256], lhsT=dg[:, 3 + kw, :], rhs=prc[:, kw:kw + 256], start=(kw == 0), stop=(kw == 2)),
                                reads=[dg, prc], writes=[c_t])
                    else:
                        n = 512
                        r0 = (blk - 1) * 8
                        for tap in range(9):
                            kh, kw = tap // 3, tap % 3
                            tr.op("pe", lambda e, tap=tap, kh=kh, kw=kw, c_t=c_t, dg=dg, pr=pr, r0=r0: e.matmul(
                                out=c_t[:, 0:512], lhsT=dg[:, tap, :], rhs=pr[:, r0 + kh:r0 + kh + 8, kw:kw + 64], start=(tap == 0), stop=(tap == 8)),
                                reads=[dg, pr], writes=[c_t])
                    own_blk = 1 <= blk <= 4
                    if ct >= 8 and own_blk:
                        g = (ct - 8) % 2
                        dstT = (c["BT"] if ct < 10 else c["CT"])
                        o0 = (blk - 1) * 512
                        tr.op("act", lambda e, dstT=dstT, g=g, o0=o0, c_t=c_t, ct=ct: e.activation(
                            out=dstT[:, g, o0:o0 + 512], in_=c_t[:, 0:512], func=AF.Silu, bias=cb[:, ct:ct + 1]),
                            reads=[c_t, cb], writes=[dstT])
                        if ct >= 10:
                            continue
                        src_post, src_r = dstT[:, g, o0:o0 + 512], dstT
                    else:
                        po = post[npost % 3]
                        tr.op("act", lambda e, po=po, c_t=c_t, ct=ct, n=n: e.activation(
                            out=po[:, 0:n], in_=c_t[:, 0:n], func=AF.Silu, bias=cb[:, ct:ct + 1]),
                            reads=[c_t, cb], writes=[po])
                        src_post, src_r = po[:, 0:n], po
                    ntl = n // 128
                    t_t = pt[npost % 2]
                    ts_t = tst[npost % 3]
                    for i in range(ntl):
                        tr.op("pe", lambda e, i=i, t_t=t_t, src_post=src_post: e.transpose(
                            out=t_t[:, i * 128:(i + 1) * 128], in_=src_post[:, i * 128:(i + 1) * 128], identity=c["ident_b"][:]),
                            reads=[src_r, c["ident_b"]], writes=[t_t])
                    tr.op("dve", lambda e, t_t=t_t, ts_t=ts_t, ntl=ntl: e.tensor_copy(
                        out=ts_t[:, 0:ntl, :], in_=t_t[:, 0:ntl * 128].rearrange("p (a b) -> p a b", b=128)),
                        reads=[t_t], writes=[ts_t])
                    tile0 = 0 if blk == 0 else T_OWN0 + (blk - 1) * 4
                    if ct < 8:
                        dst_d, dst_r = xtok_v[:, tile0:tile0 + ntl, ct * 128:(ct + 1) * 128], c["x_tok"]
                    else:
                        dst_d, dst_r = btok_v[:, tile0:tile0 + ntl, (ct - 8) * 128:(ct - 7) * 128], c["B_tok"]
                    tr.dma("sp", tsem[npost % 3], out=dst_d, in_=ts_t[:, 0:ntl, :], reads=[ts_t], writes=[])
                    c.setdefault("scr_ev", []).append(ts_t)
                    npost += 1
            self.conv_store_tiles = tst
            self.tap("BT", c["BT"][:].rearrange("p g t -> p (g t)"), [128, 2 * NOWN], BF16, [c["BT"]])
            self.tap("CT", c["CT"][:].rearrange("p g t -> p (g t)"), [128, 2 * NOWN], BF16, [c["CT"]])
            for e in Tracker.ENG:
                tr.wait_all(e, tst)
            self.barrier_release(rel)

    def stage_gla(self, st):
        tr, c, I = self.tr, self.c, self.I
        self.fence()
        c["oB"] = self.scratch("oB", [NOWN, 1024], F32)
        c["yx"] = self.scratch("yx", [NOWN, 2048], BF16)
        oB_v = c["oB"].t.rearrange("(n p) c -> n p c", p=128)
        yx_v = c["yx"].t.rearrange("(n p) c -> n p c", p=128)
        w_in_v = I["w_in"].t.rearrange("(kc p) n -> p kc n", p=128)
        LNQ = math.log(128.0 ** -0.5)
        with ExitStack() as s2:
            wg = self.sb(s2, "wgla", [128, 8, 3072], BF16)
            wgs = [Res("wgla%d" % i) for i in range(6)]
            wgg = self.sb(s2, "wgg", [128, 8, 32], BF16)
            wup = self.sb(s2, "wup", [17, 2, 512], F32)
            gn = self.sb(s2, "gn_bc", [128, 256], F32)
            c_one = self.sb(s2, "c_one", [128, 1], F32)
            c_lnq = self.sb(s2, "c_lnq", [128, 1], F32)
            c_eps = self.sb(s2, "c_eps", [128, 1], F32)
            negcol = self.sb(s2, "negcol", [128, 2], F32)
            Tm = [self.sb(s2, "TmA", [128, 128], F32), self.sb(s2, "TmB", [128, 128], F32)]
            S = [self.sb(s2, "S_A", [128, 4, 256], F32), self.sb(s2, "S_B", [128, 4, 256], F32)]
            Sbf = self.sb(s2, "Sbf", [128, 4, 256], BF16)
            FS = []
            for par in range(2):
                f = {}
                f["g_aug"] = self.sb(s2, "g_aug%d" % par, [32, 128], F32)
                f["v_bf"] = self.sb(s2, "v_bf%d" % par, [128, 1024], BF16)
                f["lap"] = self.sb(s2, "lap%d" % par, [128, 512], F32)
                f["Einv"] = self.sb(s2, "Einv%d" % par, [128, 512], F32)
                f["Eq"] = self.sb(s2, "Eq%d" % par, [128, 512], F32)
                f["kt_"] = self.sb(s2, "kt_%d" % par, [128, 512], BF16)
                f["qt_"] = self.sb(s2, "qt_%d" % par, [128, 512], BF16)
                f["kqT"] = self.sb(s2, "kqT%d" % par, [128, 8, 128], BF16)
                f["PT"] = self.sb(s2, "PT%d" % par, [128, 4, 128], BF16)
                f["dcol"] = self.sb(s2, "dcol%d" % par, [128, 4], F32)
                f["P"] = [self.ps(s2, "gP%d_%d" % (par, i)) for i in range(4)]
                FS.append(f)
            silr = self.sb(s2, "silr", [128, 1024], F32)
            o_sb = self.sb(s2, "o_sb", [128, 1024], F32)
            oB_sb = [self.sb(s2, "oB_sb%d" % i, [128, 1024], F32) for i in range(2)]
            oBs = [self.dsem() for _ in range(2)]
            ost = [self.sb(s2, "ost%d" % i, [128, 1024], F32) for i in range(2)]
            osts = [self.dsem() for _ in range(2)]
            yst = [self.sb(s2, "yst%d" % i, [128, 1024], BF16) for i in range(2)]
            ysts = [self.dsem() for _ in range(2)]
            ss4 = self.sb(s2, "ss4", [128, 4], F32)
            rs4 = self.sb(s2, "rs4", [128, 4], F32)
            junk = self.sb(s2, "junkg", [128, 256], BF16)
            rel = [wg, wgg, wup, gn, c_one, c_lnq, c_eps, negcol, Tm[0], Tm[1], S[0], S[1], Sbf, silr, o_sb, ss4, rs4, junk] + oB_sb + ost + yst + wgs
            for f in FS:
                rel += [f[k] for k in ("g_aug", "v_bf", "lap", "Einv", "Eq", "kt_", "qt_", "kqT", "PT", "dcol")] + f["P"]
            d0 = self.dsem(9)
            for i in range(6):
                tr.dma("pool", d0, out=wg[:, :, i * 512:(i + 1) * 512], in_=w_in_v[:, :, i * 512:(i + 1) * 512], writes=[wgs[i]])
            tr.dma("pool", d0, out=wgg[:], in_=w_in_v[:, :, OFF_G:OFF_G + 32], writes=[wgg])
            tr.dma("sp", d0, out=wup[:], in_=I["w_up_aug"].t.rearrange("d k n -> k d n"), writes=[wup])
            tr.dma("sp", d0, out=gn[:], in_=I["gla_norm"].t.partition_broadcast(128), writes=[gn])
            tr.op("pool", lambda e: e.memset(c_one[:], 1.0), writes=[c_one])
            tr.op("pool", lambda e: e.memset(c_lnq[:], LNQ), writes=[c_lnq])
            tr.op("pool", lambda e: e.memset(c_eps[:], EPS), writes=[c_eps])
            tr.op("pool", lambda e: e.memset(negcol[:], -1.0 / 16.0), writes=[negcol])
            tr.op("pool", lambda e: e.tensor_scalar(out=Tm[0][:], in0=c["tri_le"][:], scalar1=-1.0 / 16.0, scalar2=None, op0=ALU.mult), reads=[c["tri_le"]], writes=[Tm[0]])
            tr.op("pool", lambda e: e.tensor_scalar(out=Tm[1][:], in0=c["tri_ge"][:], scalar1=-1.0 / 16.0, scalar2=None, op0=ALU.mult), reads=[c["tri_ge"]], writes=[Tm[1]])
            for f in FS:
                tr.op("pool", lambda e, f=f: e.memset(f["g_aug"][:], 1.0), writes=[f["g_aug"]])
            for dd in range(2):
                tr.op("pool", lambda e, dd=dd: e.memset(S[dd][:], 0.0), writes=[S[dd]])
            masks = [c["tri_le"], c["tri_ge"]]

            def gla_tile(t, dd, full, sweepA, own_idx, seq):
                f = FS[seq % 2]
                P = f["P"]
                g_aug, v_bf, lap, Einv, Eq, kt_, qt_, kqT, PT, dcol = (f[k] for k in ("g_aug", "v_bf", "lap", "Einv", "Eq", "kt_", "qt_", "kqT", "PT", "dcol"))
                Sd = S[dd]
                hres = [c["hT_r"][t]]
                lhs = lambda kc: c["hT"][:, kc, t * 128:(t + 1) * 128]

                def mm_tok(ps_t, c0, n, wres):
                    for kc in range(8):
                        tr.op("pe", lambda e, kc=kc: e.matmul(out=ps_t[:, 0:n], lhsT=lhs(kc), rhs=wg[:, kc, c0:c0 + n], start=(kc == 0), stop=(kc == 7)),
                              reads=hres + wres, writes=[ps_t])
                for kc in range(8):
                    tr.op("pe", lambda e, kc=kc: e.matmul(out=P[3][0:16, 0:128], lhsT=wgg[:, kc, dd * 16:(dd + 1) * 16], rhs=lhs(kc), start=(kc == 0), stop=(kc == 7)),
                          reads=hres + [wgg], writes=[P[3]])
                tr.op("act", lambda e: e.activation(out=g_aug[0:16, :], in_=P[3][0:16, 0:128], func=AF.Copy), reads=[P[3]], writes=[g_aug])
                mm_tok(P[0], 512, 512, [wgs[1]])
                mm_tok(P[1], 1024, 512, [wgs[2]])
                mm_tok(P[2], 1536, 512, [wgs[3]])
                tr.op("pe", lambda e: e.matmul(out=P[3][:, 0:512], lhsT=g_aug[0:17, :], rhs=wup[:, dd, :], start=True, stop=True), reads=[g_aug, wup], writes=[P[3]])
                tr.op("act", lambda e: e.activation(out=lap[:], in_=P[3][:, 0:512], func=AF.Exp, scale=-1.0), reads=[P[3]], writes=[lap])
                tr.op("act", lambda e: e.activation(out=lap[:], in_=lap[:], func=AF.Ln, bias=c_one[:]), reads=[lap, c_one], writes=[lap])
                tr.op("act", lambda e: e.activation(out=v_bf[:, 0:512], in_=P[1][:], func=AF.Copy), reads=[P[1]], writes=[v_bf])
                tr.op("dve", lambda e: e.tensor_copy(out=v_bf[:, 512:1024], in_=P[2][:]), reads=[P[2]], writes=[v_bf])
                tr.op("pe", lambda e: e.matmul(out=P[3][:, 0:512], lhsT=Tm[dd][:], rhs=lap[:], start=True, stop=True), reads=[Tm[dd], lap], writes=[P[3]])
                if full:
                    mm_tok(P[1], 0, 512, [wgs[0]])
                tr.op("act", lambda e: e.activation(out=Einv[:], in_=P[3][:, 0:512], func=AF.Exp, scale=-1.0), reads=[P[3]], writes=[Einv])
                if full:
                    tr.op("act", lambda e: e.activation(out=Eq[:], in_=P[3][:, 0:512], func=AF.Exp, bias=c_lnq[:]), reads=[P[3], c_lnq], writes=[Eq])
                tr.op("dve", lambda e: e.tensor_tensor(out=kt_[:], in0=P[0][:], in1=Einv[:], op=ALU.mult), reads=[P[0], Einv], writes=[kt_])
                for h in range(4):
                    tr.op("pe", lambda e, h=h: e.matmul(out=P[3][:, 2 * h:2 * h + 2], lhsT=lap[:, h * 128:(h + 1) * 128], rhs=negcol[:], start=True, stop=True),
                          reads=[lap, negcol], writes=[P[3]])
                tr.op("act", lambda e: e.activation(out=dcol[:], in_=P[3][:, 0:8:2], func=AF.Exp), reads=[P[3]], writes=[dcol])
                if full:
                    pT = P[2][:].bitcast(BF16)
                    tr.op("dve", lambda e: e.tensor_tensor(out=qt_[:], in0=P[1][:], in1=Eq[:], op=ALU.mult), reads=[P[1], Eq], writes=[qt_])
                    for h in range(4):
                        tr.op("pe", lambda e, h=h: e.transpose(out=pT[:, h * 128:(h + 1) * 128], in_=kt_[:, h * 128:(h + 1) * 128], identity=c["ident_b"][:]),
                              reads=[kt_, c["ident_b"]], writes=[P[2]])
                    for h in range(4):
                        tr.op("pe", lambda e, h=h: e.transpose(out=pT[:, (4 + h) * 128:(5 + h) * 128], in_=qt_[:, h * 128:(h + 1) * 128], identity=c["ident_b"][:]),
                              reads=[qt_, c["ident_b"]], writes=[P[2]])
                    tr.op("act", lambda e: e.activation(out=kqT[:].rearrange("p a b -> p (a b)"), in_=pT, func=AF.Copy), reads=[P[2]], writes=[kqT])
                    for h in range(4):
                        tr.op("pe", lambda e, h=h: e.matmul(out=P[0][:, h * 128:(h + 1) * 128], lhsT=kqT[:, h, :], rhs=kqT[:, 4 + h, :], start=True, stop=True),
                              reads=[kqT], writes=[P[0]])
                    tr.op("dve", lambda e: e.tensor_tensor(out=PT[:], in0=P[0][:].rearrange("p (h i) -> p h i", h=4),
                                                          in1=masks[dd][:].unsqueeze(1).to_broadcast([128, 4, 128]), op=ALU.mult),
                          reads=[P[0], masks[dd]], writes=[PT])
                kvb = [P[2], P[2], P[0], P[0]]
                for h in range(4):
                    cs = (h % 2) * 256
                    tr.op("pe", lambda e, h=h, cs=cs: e.matmul(out=kvb[h][:, cs:cs + 256], lhsT=kt_[:, h * 128:(h + 1) * 128], rhs=v_bf[:, h * 256:(h + 1) * 256], start=True, stop=True),
                          reads=[kt_, v_bf], writes=[kvb[h]])
                tr.mark("need_state")
                if full:
                    ob_ = [P[1], P[1], P[3], P[3]]
                    tr.op("act", lambda e: e.activation(out=Sbf[:].rearrange("p a b -> p (a b)"), in_=Sd[:].rearrange("p a b -> p (a b)"), func=AF.Copy), reads=[Sd], writes=[Sbf])
                    for h in range(4):
                        cs = (h % 2) * 256
                        tr.op("pe", lambda e, h=h, cs=cs: e.matmul(out=ob_[h][:, cs:cs + 256], lhsT=PT[:, h, :], rhs=v_bf[:, h * 256:(h + 1) * 256], start=True, stop=False),
                              reads=[PT, v_bf], writes=[ob_[h]])
                        tr.op("pe", lambda e, h=h, cs=cs: e.matmul(out=ob_[h][:, cs:cs + 256], lhsT=kqT[:, 4 + h, :], rhs=Sbf[:, h, :], start=False, stop=True),
                              reads=[kqT, Sbf], writes=[ob_[h]])
                tr.op("dve", lambda e: e.tensor_tensor(out=Sd[:, 0:2, :].rearrange("p a b -> p (a b)"), in0=P[2][:], in1=Sd[:, 0:2, :].rearrange("p a b -> p (a b)"), op=ALU.add),
                      reads=[P[2], Sd], writes=[Sd])
                tr.op("dve", lambda e: e.tensor_tensor(out=Sd[:, 2:4, :].rearrange("p a b -> p (a b)"), in0=P[0][:], in1=Sd[:, 2:4, :].rearrange("p a b -> p (a b)"), op=ALU.add),
                      reads=[P[0], Sd], writes=[Sd])
                tr.op("dve", lambda e: e.tensor_tensor(out=Sd[:], in0=Sd[:], in1=dcol[:].unsqueeze(2).to_broadcast([128, 4, 256]), op=ALU.mult),
                      reads=[Sd, dcol], writes=[Sd])
                if not full:
                    return
                if not sweepA:
                    os_ = ost[own_idx % 2]
                    tr.op("act", lambda e: e.activation(out=os_[:, 0:512], in_=P[1][:], func=AF.Copy), reads=[P[1]], writes=[os_])
                    tr.op("dve", lambda e: e.tensor_copy(out=os_[:, 512:1024], in_=P[3][:]), reads=[P[3]], writes=[os_])
                    tr.dma("sp", osts[own_idx % 2], out=oB_v[own_idx], in_=os_[:], reads=[os_], writes=[])
                    return
                ob = oB_sb[own_idx % 2]
                tr.dma("sp", oBs[own_idx % 2], out=ob[:], in_=oB_v[own_idx], writes=[ob])
                mm_tok(P[2], 2048, 512, [wgs[4]])
                tr.op("act", lambda e: e.activation(out=silr[:, 0:512], in_=P[2][:], func=AF.Silu), reads=[P[2]], writes=[silr])
                mm_tok(P[0], 2560, 512, [wgs[5]])
                tr.op("act", lambda e: e.activation(out=silr[:, 512:1024], in_=P[0][:], func=AF.Silu), reads=[P[0]], writes=[silr])
                tr.op("dve", lambda e: e.tensor_tensor(out=silr[:].rearrange("p (h v) -> p h v", h=4), in0=silr[:].rearrange("p (h v) -> p h v", h=4),
                                                      in1=gn[:].unsqueeze(1).to_broadcast([128, 4, 256]), op=ALU.mult), reads=[silr, gn], writes=[silr])
                for hh, pb in enumerate((P[1], P[3])):
                    tr.op("dve", lambda e, hh=hh, pb=pb: e.tensor_tensor(out=o_sb[:, hh * 512:(hh + 1) * 512], in0=pb[:], in1=ob[:, hh * 512:(hh + 1) * 512], op=ALU.add),
                          reads=[pb, ob], writes=[o_sb])
                for h in range(4):
                    tr.op("act", lambda e, h=h: e.activation(out=junk[:], in_=o_sb[:, h * 256:(h + 1) * 256], func=AF.Square, accum_out=ss4[:, h:h + 1]),
                          reads=[o_sb], writes=[junk, ss4])
                tr.op("act", lambda e: e.activation(out=rs4[:], in_=ss4[:], func=AF.Ln, scale=1.0 / 256.0, bias=c_eps[:]), reads=[ss4, c_eps], writes=[rs4])
                tr.op("act", lambda e: e.activation(out=rs4[:], in_=rs4[:], func=AF.Exp, scale=-0.5), reads=[rs4], writes=[rs4])
                tr.op("dve", lambda e: e.tensor_tensor(out=o_sb[:].rearrange("p (h v) -> p h v", h=4), in0=o_sb[:].rearrange("p (h v) -> p h v", h=4),
                                                      in1=rs4[:].unsqueeze(2).to_broadcast([128, 4, 256]), op=ALU.mult), reads=[o_sb, rs4], writes=[o_sb])
                ys = yst[own_idx % 2]
                tr.op("dve", lambda e: e.tensor_tensor(out=ys[:], in0=o_sb[:], in1=silr[:], op=ALU.mult), reads=[o_sb, silr], writes=[ys])
                tr.dma("sp", ysts[own_idx % 2], out=yx_v[own_idx][:, 0:1024], in_=ys[:], reads=[ys], writes=[])

            def sweep(tiles):
                recs = []
                for seq, (t, dd, full, sweepA, own_idx) in enumerate(tiles):
                    tr.begin_record()
                    gla_tile(t, dd, full, sweepA, own_idx, seq)
                    recs.append(tr.end_record())
                tr.run_pipelined(recs, depth=2)

            sweep([(t, 1, False, False, None) for t in (1, 0)])
            self.tap("gS_B", S[1][:].rearrange("p a b -> p (a b)"), [128, 1024], F32, [S[1]])
            sweep([(t, 1, False, False, None) for t in range(NT - 1, T_OTH0 - 1, -1)] +
                  [(t, 1, True, False, t - T_OWN0) for t in range(T_OTH0 - 1, T_OWN0 - 1, -1)])
            for e in Tracker.ENG:
                tr.wait_all(e, ost)
            sweep([(t, 0, False, True, None) for t in (0, 1)])
            self.tap("gS_A", S[0][:].rearrange("p a b -> p (a b)"), [128, 1024], F32, [S[0]])
            sweep([(t, 0, True, True, t - T_OWN0) for t in range(T_OWN0, T_OTH0)])
            for e in Tracker.ENG:
                tr.wait_all(e, yst)
            self.barrier_release(rel)

    def stage_ssd(self, st):
        tr, c, I = self.tr, self.c, self.I
        self.fence()
        c["yB"] = self.scratch("yB", [NOWN, 1024], F32)
        yB_v = c["yB"].t.rearrange("(n p) c -> n p c", p=128)
        yx_v = c["yx"].t.rearrange("(n p) c -> n p c", p=128)
        xtok_v = c["x_tok"].t.rearrange("(n p) c -> n p c", p=128)
        btok_v = c["B_tok"].t.rearrange("(n p) c -> n p c", p=128)
        w_in_v = I["w_in"].t.rearrange("(kc p) n -> p kc n", p=128)
        BT, CT = c["BT"], c["CT"]
        with ExitStack() as s2:
            wz = self.sb(s2, "wz", [128, 8, 1024], BF16)
            wdt = self.sb(s2, "wdt", [128, 8, 32], BF16)
            Abc = self.sb(s2, "Abc", [128, 32], F32)
            dtb = self.sb(s2, "dtb", [128, 32], F32)
            Dsk = self.sb(s2, "Dsk", [128, 16], F32)
            snb = self.sb(s2, "snb", [128, 1024], F32)
            c_one = self.sb(s2, "c_one2", [128, 1], F32)
            c_eps = self.sb(s2, "c_eps2", [128, 1], F32)
            ST = [self.sb(s2, "ST_A", [128, 2, 512], F32), self.sb(s2, "ST_B", [128, 2, 512], F32)]
            STbf = self.sb(s2, "STbf", [128, 2, 512], BF16)
            xt = [self.sb(s2, "xt%d" % i, [128, 1024], BF16) for i in range(2)]
            bt = [self.sb(s2, "bt%d" % i, [128, 256], BF16) for i in range(2)]
            xts = [self.dsem() for _ in range(2)]
            dt_ = self.sb(s2, "dt_", [128, 16], F32)
            dtA = self.sb(s2, "dtA", [128, 16], F32)
            acs = self.sb(s2, "acs", [128, 16], F32)
            ea = self.sb(s2, "ea", [128, 16], F32)
            dend = self.sb(s2, "dend", [128, 16], F32)
            dtot = self.sb(s2, "dtot", [128, 16], F32)
            R1 = self.sb(s2, "R1", [128, 16, 128], F32)
            E = self.sb(s2, "E", [128, 16, 128], BF16)
            M = self.sb(s2, "M", [128, 16, 128], BF16)
            CBm = self.sb(s2, "CBm", [128, 2, 128], F32)
            xdt = self.sb(s2, "xdt", [128, 1024], BF16)
            xdd = self.sb(s2, "xdd", [128, 1024], BF16)
            silz = self.sb(s2, "silz", [128, 1024], F32)
            y_sb = self.sb(s2, "y_sb", [128, 1024], F32)
            tmp = self.sb(s2, "ytmp", [128, 1024], F32)
            yB_sb = [self.sb(s2, "yB_sb%d" % i, [128, 1024], F32) for i in range(2)]
            yBs = [self.dsem() for _ in range(2)]
            yst = [self.sb(s2, "ysst%d" % i, [128, 1024], F32) for i in range(2)]
            ysts = [self.dsem() for _ in range(2)]
            yxs = [self.sb(s2, "yxs%d" % i, [128, 1024], BF16) for i in range(2)]
            yxss = [self.dsem() for _ in range(2)]
            ss2 = self.sb(s2, "ss2", [128, 2], F32)
            rs2 = self.sb(s2, "rs2", [128, 2], F32)
            junk = self.sb(s2, "junks", [128, 512], BF16)
            pS = self.ps(s2, "pS")
            pD = [self.ps(s2, "pD%d" % i) for i in range(4)]
            pCB = self.ps(s2, "pCB")
            pY = [self.ps(s2, "pY%d" % i) for i in range(2)]
            rel = [wz, wdt, Abc, dtb, Dsk, snb, c_one, c_eps, ST[0], ST[1], STbf, dt_, dtA, acs, ea, dend, dtot, R1, E, M, CBm, xdt, xdd,
                   silz, y_sb, tmp, ss2, rs2, junk, pS, pCB] + xt + bt + yB_sb + yst + yxs + pD + pY
            d0 = self.dsem(6)
            tr.dma("pool", d0, out=wz[:], in_=w_in_v[:, :, OFF_Z:OFF_Z + 1024], writes=[wz])
            tr.dma("pool", d0, out=wdt[:], in_=w_in_v[:, :, OFF_DT:OFF_DT + 32], writes=[wdt])
            tr.dma("sp", d0, out=Abc[:], in_=I["a_log"].t.partition_broadcast(128), writes=[Abc])
            tr.dma("sp", d0, out=dtb[:], in_=I["dt_bias"].t.partition_broadcast(128), writes=[dtb])
            tr.dma("sp", d0, out=Dsk[:], in_=I["ssd_d"].t.partition_broadcast(128), writes=[Dsk])
            tr.dma("sp", d0, out=snb[:], in_=I["ssd_norm"].t.partition_broadcast(128), writes=[snb])
            tr.op("pool", lambda e: e.memset(c_one[:], 1.0), writes=[c_one])
            tr.op("pool", lambda e: e.memset(c_eps[:], EPS), writes=[c_eps])
            tr.op("act", lambda e: e.activation(out=Abc[:], in_=Abc[:], func=AF.Exp), reads=[Abc], writes=[Abc])
            tr.op("dve", lambda e: e.tensor_scalar(out=Abc[:], in0=Abc[:], scalar1=-1.0, scalar2=None, op0=ALU.mult), reads=[Abc], writes=[Abc])
            for dd in range(2):
                tr.op("pool", lambda e, dd=dd: e.memset(ST[dd][:], 0.0), writes=[ST[dd]])
            Lm = [c["tri_gt"], c["tri_lt"]]
            Tc = [c["tri_le"], c["tri_ge"]]
            cnt = [0]

            QS = [[pS, pD[0], pD[1], pD[2]], [pD[3], pCB, pY[0], pY[1]]]
            ea2 = [ea, self.sb(s2, "ea_b", [128, 16], F32)]
            dtot2 = [dtot, self.sb(s2, "dtot_b", [128, 16], F32)]
            xdd2 = [xdd, self.sb(s2, "xdd_b", [128, 1024], BF16)]
            silz2 = [silz, self.sb(s2, "silz_b", [128, 1024], F32)]
            ysb2 = [y_sb, self.sb(s2, "y_sb_b", [128, 1024], F32)]
            tmp2 = [tmp, self.sb(s2, "ytmp_b", [128, 1024], F32)]
            ss22 = [ss2, self.sb(s2, "ss2_b", [128, 2], F32)]
            rs22 = [rs2, self.sb(s2, "rs2_b", [128, 2], F32)]
            junk2 = [junk, self.sb(s2, "junks_b", [128, 512], BF16)]
            rel += [ea2[1], dtot2[1], xdd2[1], silz2[1], ysb2[1], tmp2[1], ss22[1], rs22[1], junk2[1]]

            def ssd_tile(t, dd, full, sweepA, own_idx, seq):
                STd = ST[dd]
                k = seq % 2
                Q = QS[k]
                ea_, dtot_, xdd_, silz_ = ea2[k], dtot2[k], xdd2[k], silz2[k]
                y_sb, tmp, ss2, rs2, junk = ysb2[k], tmp2[k], ss22[k], rs22[k], junk2[k]
                x_t, b_t = xt[k], bt[k]
                tr.regroup(xts[k], 2)
                tr.dma("sp", xts[k], out=x_t[:], in_=xtok_v[t], writes=[x_t])
                tr.dma("sp", xts[k], out=b_t[:], in_=btok_v[t], writes=[b_t])
                hres = [c["hT_r"][t]]
                lhs = lambda kc: c["hT"][:, kc, t * 128:(t + 1) * 128]
                if full and sweepA:
                    for hh in range(2):
                        for kc in range(8):
                            tr.op("pe", lambda e, kc=kc, hh=hh: e.matmul(out=Q[1 + hh][:], lhsT=lhs(kc), rhs=wz[:, kc, hh * 512:(hh + 1) * 512], start=(kc == 0), stop=(kc == 7)),
                                  reads=hres + [wz], writes=[Q[1 + hh]])
                        tr.op("act", lambda e, hh=hh: e.activation(out=silz_[:, hh * 512:(hh + 1) * 512], in_=Q[1 + hh][:], func=AF.Silu), reads=[Q[1 + hh]], writes=[silz_])
                for kc in range(8):
                    tr.op("pe", lambda e, kc=kc: e.matmul(out=Q[0][:, 0:16], lhsT=lhs(kc), rhs=wdt[:, kc, dd * 16:(dd + 1) * 16], start=(kc == 0), stop=(kc == 7)),
                          reads=hres + [wdt], writes=[Q[0]])
                tr.op("dve", lambda e: e.tensor_tensor(out=dt_[:], in0=Q[0][:, 0:16], in1=dtb[:, dd * 16:(dd + 1) * 16], op=ALU.add), reads=[Q[0], dtb], writes=[dt_])
                tr.op("act", lambda e: e.activation(out=dt_[:], in_=dt_[:], func=AF.Exp), reads=[dt_], writes=[dt_])
                tr.op("act", lambda e: e.activation(out=dt_[:], in_=dt_[:], func=AF.Ln, bias=c_one[:]), reads=[dt_, c_one], writes=[dt_])
                tr.op("dve", lambda e: e.tensor_tensor(out=dtA[:], in0=dt_[:], in1=Abc[:, dd * 16:(dd + 1) * 16], op=ALU.mult), reads=[dt_, Abc], writes=[dtA])
                tr.op("pe", lambda e: e.matmul(out=Q[0][:, 16:32], lhsT=Tc[dd][:], rhs=dtA[:], start=True, stop=True), reads=[Tc[dd], dtA], writes=[Q[0]])
                tr.op("pe", lambda e: e.matmul(out=Q[0][:, 32:48], lhsT=c["ones_f"][:], rhs=dtA[:], start=True, stop=True), reads=[c["ones_f"], dtA], writes=[Q[0]])
                tr.op("dve", lambda e: e.tensor_copy(out=acs[:], in_=Q[0][:, 16:32]), reads=[Q[0]], writes=[acs])
                tr.op("dve", lambda e: e.tensor_tensor(out=dend[:], in0=Q[0][:, 32:48], in1=acs[:], op=ALU.subtract), reads=[Q[0], acs], writes=[dend])
                tr.op("act", lambda e: e.activation(out=dend[:], in_=dend[:], func=AF.Exp), reads=[dend], writes=[dend])
                tr.op("act", lambda e: e.activation(out=dtot_[:], in_=Q[0][:, 32:48], func=AF.Exp), reads=[Q[0]], writes=[dtot_])
                tr.op("dve", lambda e: e.tensor_tensor(out=xdt[:].rearrange("p (h q) -> p h q", h=16), in0=x_t[:].rearrange("p (h q) -> p h q", h=16),
                                                      in1=dt_[:].unsqueeze(2).to_broadcast([128, 16, 64]), op=ALU.mult), reads=[x_t, dt_], writes=[xdt])
                tr.op("pool", lambda e: e.tensor_tensor(out=xdd_[:].rearrange("p (h q) -> p h q", h=16), in0=xdt[:].rearrange("p (h q) -> p h q", h=16),
                                                       in1=dend[:].unsqueeze(2).to_broadcast([128, 16, 64]), op=ALU.mult), reads=[xdt, dend], writes=[xdd_])
                if full:
                    tok0 = (t - T_OWN0) * 128
                    Dbank = [Q[1], Q[2], Q[3], Q[1]]
                    tr.op("act", lambda e: e.activation(out=ea_[:], in_=acs[:], func=AF.Exp), reads=[acs], writes=[ea_])
                    tr.op("dve", lambda e: e.tensor_tensor(out=R1[:], in0=Tc[dd][:].unsqueeze(1).to_broadcast([128, 16, 128]),
                                                          in1=dtA[:].unsqueeze(2).to_broadcast([128, 16, 128]), op=ALU.mult), reads=[Tc[dd], dtA], writes=[R1])
                    for b4 in range(4):
                        tr.op("pe", lambda e, b4=b4: e.matmul(out=Dbank[b4][:], lhsT=Lm[dd][:], rhs=R1[:, 4 * b4:4 * b4 + 4, :], start=True, stop=True),
                              reads=[Lm[dd], R1], writes=[Dbank[b4]])
                        tr.op("act", lambda e, b4=b4: e.activation(out=E[:, 4 * b4:4 * b4 + 4, :], in_=Dbank[b4][:].rearrange("p (h i) -> p h i", h=4), func=AF.Exp), reads=[Dbank[b4]], writes=[E])
                    for g in range(2):
                        tr.op("pe", lambda e, g=g: e.matmul(out=Q[2][:, g * 128:(g + 1) * 128], lhsT=BT[:, g, tok0:tok0 + 128], rhs=CT[:, g, tok0:tok0 + 128], start=True, stop=True),
                              reads=[BT, CT], writes=[Q[2]])
                    tr.op("dve", lambda e: e.tensor_tensor(out=CBm[:], in0=Q[2][:, 0:256].rearrange("p (g i) -> p g i", g=2),
                                                          in1=Tc[dd][:].unsqueeze(1).to_broadcast([128, 2, 128]), op=ALU.mult), reads=[Q[2], Tc[dd]], writes=[CBm])
                    for g in range(2):
                        eng = "dve" if g == 0 else "pool"
                        tr.op(eng, lambda e, g=g: e.tensor_tensor(out=M[:, g * 8:(g + 1) * 8, :], in0=E[:, g * 8:(g + 1) * 8, :],
                                                                  in1=CBm[:, g:g + 1, :].to_broadcast([128, 8, 128]), op=ALU.mult), reads=[E, CBm], writes=[M])
                    Yb = [Q[3], Q[1]]
                    for h in range(16):
                        py = Yb[h // 8]
                        cs = (h % 8) * 64
                        tr.op("pe", lambda e, h=h, py=py, cs=cs: e.matmul(out=py[:, cs:cs + 64], lhsT=M[:, h, :], rhs=xdt[:, h * 64:(h + 1) * 64], start=True, stop=True),
                              reads=[M, xdt], writes=[py])
                tr.mark("need_state")
                Ob = [Q[2], Q[0]]
                if full:
                    tr.op("act", lambda e: e.activation(out=STbf[:].rearrange("p a b -> p (a b)"), in_=STd[:].rearrange("p a b -> p (a b)"), func=AF.Copy), reads=[STd], writes=[STbf])
                    for g in range(2):
                        tr.op("pe", lambda e, g=g: e.matmul(out=Ob[g][:], lhsT=CT[:, g, tok0:tok0 + 128], rhs=STbf[:, g, :], start=True, stop=True),
                              reads=[CT, STbf], writes=[Ob[g]])
                        tr.op("dve", lambda e, g=g: e.tensor_tensor(out=tmp[:, g * 512:(g + 1) * 512].rearrange("p (h q) -> p h q", h=8), in0=Ob[g][:].rearrange("p (h q) -> p h q", h=8),
                                                                    in1=ea_[:, g * 8:(g + 1) * 8].unsqueeze(2).to_broadcast([128, 8, 64]), op=ALU.mult), reads=[Ob[g], ea_], writes=[tmp])
                        tr.op("dve", lambda e, g=g: e.tensor_tensor(out=y_sb[:, g * 512:(g + 1) * 512], in0=Yb[g][:], in1=tmp[:, g * 512:(g + 1) * 512], op=ALU.add),
                              reads=[Yb[g], tmp], writes=[y_sb])
                for g in range(2):
                    tr.op("pe", lambda e, g=g: e.matmul(out=Ob[g][:], lhsT=b_t[:, g * 128:(g + 1) * 128], rhs=xdd_[:, g * 512:(g + 1) * 512], start=True, stop=True),
                          reads=[b_t, xdd_], writes=[Ob[g]])
                    tr.op("dve", lambda e, g=g: e.tensor_tensor(out=STd[:, g, :].rearrange("p (h q) -> p h q", h=8), in0=STd[:, g, :].rearrange("p (h q) -> p h q", h=8),
                                                                in1=dtot_[:, g * 8:(g + 1) * 8].unsqueeze(2).to_broadcast([128, 8, 64]), op=ALU.mult), reads=[STd, dtot_], writes=[STd])
                    tr.op("dve", lambda e, g=g: e.tensor_tensor(out=STd[:, g, :], in0=Ob[g][:], in1=STd[:, g, :], op=ALU.add), reads=[Ob[g], STd], writes=[STd])
                tr.mark("state_done")
                if not full:
                    return
                if not sweepA:
                    ys = yst[own_idx % 2]
                    tr.op("act", lambda e: e.activation(out=ys[:], in_=y_sb[:], func=AF.Copy), reads=[y_sb], writes=[ys])
                    tr.dma("sp", ysts[own_idx % 2], out=yB_v[own_idx], in_=ys[:], reads=[ys], writes=[])
                    return
                yb = yB_sb[own_idx % 2]
                tr.dma("sp", yBs[own_idx % 2], out=yb[:], in_=yB_v[own_idx], writes=[yb])
                tr.op("dve", lambda e: e.tensor_tensor(out=y_sb[:], in0=y_sb[:], in1=yb[:], op=ALU.add), reads=[y_sb, yb], writes=[y_sb])
                tr.op("pool", lambda e: e.tensor_tensor(out=tmp[:].rearrange("p (h q) -> p h q", h=16), in0=x_t[:].rearrange("p (h q) -> p h q", h=16),
                                                       in1=Dsk[:].unsqueeze(2).to_broadcast([128, 16, 64]), op=ALU.mult), reads=[x_t, Dsk], writes=[tmp])
                tr.op("dve", lambda e: e.tensor_tensor(out=y_sb[:], in0=y_sb[:], in1=tmp[:], op=ALU.add), reads=[y_sb, tmp], writes=[y_sb])
                tr.op("dve", lambda e: e.tensor_tensor(out=y_sb[:], in0=y_sb[:], in1=silz_[:], op=ALU.mult), reads=[y_sb, silz_], writes=[y_sb])
                for g in range(2):
                    tr.op("act", lambda e, g=g: e.activation(out=junk[:], in_=y_sb[:, g * 512:(g + 1) * 512], func=AF.Square, accum_out=ss2[:, g:g + 1]), reads=[y_sb], writes=[junk, ss2])
                tr.op("act", lambda e: e.activation(out=rs2[:], in_=ss2[:], func=AF.Ln, scale=1.0 / 512.0, bias=c_eps[:]), reads=[ss2, c_eps], writes=[rs2])
                tr.op("act", lambda e: e.activation(out=rs2[:], in_=rs2[:], func=AF.Exp, scale=-0.5), reads=[rs2], writes=[rs2])
                tr.op("dve", lambda e: e.tensor_tensor(out=y_sb[:].rearrange("p (g q) -> p g q", g=2), in0=y_sb[:].rearrange("p (g q) -> p g q", g=2),
                                                      in1=rs2[:].unsqueeze(2).to_broadcast([128, 2, 512]), op=ALU.mult), reads=[y_sb, rs2], writes=[y_sb])
                yo = yxs[own_idx % 2]
                tr.op("pool", lambda e: e.tensor_tensor(out=yo[:], in0=y_sb[:], in1=snb[:], op=ALU.mult), reads=[y_sb, snb], writes=[yo])
                tr.dma("sp", yxss[own_idx % 2], out=yx_v[own_idx][:, 1024:2048], in_=yo[:], reads=[yo], writes=[])

            def sweep(tiles):
                recs = []
                for seq, (t, dd, full, sweepA, own_idx) in enumerate(tiles):
                    tr.begin_record()
                    ssd_tile(t, dd, full, sweepA, own_idx, seq)
                    recs.append(tr.end_record())
                tr.run_pipelined(recs, depth=2, serial_fronts=True)

            sweep([(t, 1, False, False, None) for t in (1, 0)])
            self.tap("sS_B", ST[1][:].rearrange("p a b -> p (a b)"), [128, 1024], F32, [ST[1]])
            sweep([(t, 1, False, False, None) for t in range(NT - 1, T_OTH0 - 1, -1)] +
                  [(t, 1, True, False, t - T_OWN0) for t in range(T_OTH0 - 1, T_OWN0 - 1, -1)])
            for e in Tracker.ENG:
                tr.wait_all(e, yst)
            sweep([(t, 0, False, True, None) for t in (0, 1)])
            self.tap("sS_A", ST[0][:].rearrange("p a b -> p (a b)"), [128, 1024], F32, [ST[0]])
            sweep([(t, 0, True, True, t - T_OWN0) for t in range(T_OWN0, T_OTH0)])
            for e in Tracker.ENG:
                tr.wait_all(e, yxs)
            self.barrier_release(rel)

    def stage_post(self, st):
        tr, c, I = self.tr, self.c, self.I
        self.fence()
        c["h_lat"] = self.sb(st, "h_lat", [128, 16, D], F32)
        c["h_r"] = [Res("h_lat%d" % i) for i in range(16)]
        c["h2T"] = self.sb(st, "h2T", [128, 8, NOWN], BF16)
        c["h2_r"] = [Res("h2T%d" % i) for i in range(16)]
        c["comb"] = self.sb(st, "comb", [128, 16, 32], F32)
        yx_v = c["yx"].t.rearrange("(n p) c -> n p c", p=128)
        h_lat, h2T = c["h_lat"], c["h2T"]
        with ExitStack() as s2:
            wo = self.sb(s2, "wo", [128, 16, D], BF16)
            wr = self.sb(s2, "wr", [128, 8, 36], F32)
            brr = self.sb(s2, "brr", [1, 36], F32)
            c_eps = self.sb(s2, "c_eps3", [128, 1], F32)
            lg = self.sb(s2, "lg", [128, 16, 36], F32)
            yxt = [self.sb(s2, "yxt%d" % i, [128, 2048], BF16) for i in range(2)]
            yxs = [self.dsem() for _ in range(2)]
            xr = [self.sb(s2, "xr2_%d" % i, [128, D], F32) for i in range(2)]
            xrs = [self.dsem() for _ in range(2)]
            junk = self.sb(s2, "pjunk", [128, D], BF16)
            PS = []
            for par in range(2):
                PS.append({"yxT": self.sb(s2, "yxT%d" % par, [128, 16, 128], BF16), "tmp": self.sb(s2, "ptmp%d" % par, [128, D], F32),
                           "h2f": self.sb(s2, "h2f%d" % par, [128, 8, 128], F32), "ss": self.sb(s2, "pss%d" % par, [128, 1], F32),
                           "rs": self.sb(s2, "prs%d" % par, [128, 1], F32), "B": [self.ps(s2, "ppB%d_%d" % (par, i)) for i in range(4)]})
            rel = [wo, wr, brr, c_eps, lg, junk] + yxt + xr
            for p_ in PS:
                rel += [p_["yxT"], p_["tmp"], p_["h2f"], p_["ss"], p_["rs"]] + p_["B"]
            w_out_v = I["w_out"].t.rearrange("(kc p) n -> p kc n", p=128)
            d0 = self.dsem(2)
            wst = [self.sb(s2, "wst%d" % i, [128, 1, D], F32) for i in range(2)]
            rel += wst
            wsts = [self.dsem() for _ in range(2)]
            for q in range(16):
                tr.dma("sp", wsts[q % 2], out=wst[q % 2][:], in_=w_out_v[:, q:q + 1, :], writes=[wst[q % 2]])
                tr.op("pool", lambda e, q=q: e.tensor_copy(out=wo[:, q:q + 1, :], in_=wst[q % 2][:]), reads=[wst[q % 2]], writes=[wo])
            tr.dma("sp", d0, out=wr[:].rearrange("p a b -> p (a b)"), in_=I["w_router"].t, writes=[wr])
            tr.dma("sp", d0, out=brr[:], in_=I["b_router"].t, writes=[brr])
            tr.op("pool", lambda e: e.memset(c_eps[:], EPS), writes=[c_eps])
            def post_tile(i):
                y_t, x_t = yxt[i % 2], xr[i % 2]
                p_ = PS[i % 2]
                yxT, tmp, h2f, ss, rs, B = p_["yxT"], p_["tmp"], p_["h2f"], p_["ss"], p_["rs"], p_["B"]
                hn = tmp
                pT = [B[0][:].bitcast(BF16), B[1][:].bitcast(BF16)]
                tr.dma("sp", yxs[i % 2], out=y_t[:], in_=yx_v[i], writes=[y_t])
                tr.dma("sp", xrs[i % 2], out=x_t[:], in_=I["xs"].t[NCTX + i * 128: NCTX + (i + 1) * 128, :], writes=[x_t])
                for kc in range(16):
                    tr.op("pe", lambda e, kc=kc: e.transpose(out=pT[kc // 8][:, (kc % 8) * 128:(kc % 8 + 1) * 128], in_=y_t[:, kc * 128:(kc + 1) * 128], identity=c["ident_b"][:]),
                          reads=[y_t, c["ident_b"]], writes=[B[kc // 8]])
                tr.op("act", lambda e: e.activation(out=yxT[:, 0:8, :].rearrange("p a b -> p (a b)"), in_=pT[0], func=AF.Copy), reads=[B[0]], writes=[yxT])
                tr.op("dve", lambda e: e.tensor_copy(out=yxT[:, 8:16, :].rearrange("p a b -> p (a b)"), in_=pT[1]), reads=[B[1]], writes=[yxT])
                hl = h_lat[:, i, :]
                for hh in range(2):
                    for kc in range(16):
                        tr.op("pe", lambda e, kc=kc, hh=hh: e.matmul(out=B[2 + hh][:], lhsT=yxT[:, kc, :], rhs=wo[:, kc, hh * 512:(hh + 1) * 512], start=(kc == 0), stop=(kc == 15)),
                              reads=[yxT, wo], writes=[B[2 + hh]])
                    tr.op("dve", lambda e, hh=hh: e.tensor_tensor(out=tmp[:, hh * 512:(hh + 1) * 512], in0=B[2 + hh][:], in1=c["g1_bc"][:, hh * 512:(hh + 1) * 512], op=ALU.mult),
                          reads=[B[2 + hh], c["g1_bc"]], writes=[tmp])
                tr.op("pool", lambda e: e.tensor_tensor(out=hl, in0=tmp[:], in1=x_t[:], op=ALU.add), reads=[tmp, x_t], writes=[c["h_r"][i]])
                tr.op("act", lambda e: e.activation(out=junk[:], in_=hl, func=AF.Square, accum_out=ss[:]), reads=[c["h_r"][i]], writes=[junk, ss])
                tr.op("act", lambda e: e.activation(out=rs[:], in_=ss[:], func=AF.Ln, scale=1.0 / D, bias=c_eps[:]), reads=[ss, c_eps], writes=[rs])
                tr.op("act", lambda e: e.activation(out=rs[:], in_=rs[:], func=AF.Exp, scale=-0.5), reads=[rs], writes=[rs])
                tr.op("dve", lambda e: e.tensor_scalar(out=hn[:], in0=hl, scalar1=rs[:], scalar2=None, op0=ALU.mult), reads=[c["h_r"][i], rs], writes=[hn])
                for kc in range(8):
                    tr.op("pe", lambda e, kc=kc: e.transpose(out=B[kc // 4][:, (kc % 4) * 128:(kc % 4 + 1) * 128], in_=hn[:, kc * 128:(kc + 1) * 128], identity=c["ident_f"][:]),
                          reads=[hn, c["ident_f"]], writes=[B[kc // 4]])
                for q in range(2):
                    tr.op("dve", lambda e, q=q: e.tensor_tensor(out=h2f[:, q * 4:(q + 1) * 4, :], in0=B[q][:].rearrange("p (k t) -> p k t", k=4),
                                                               in1=c["s2"][:, q * 4:(q + 1) * 4].unsqueeze(2).to_broadcast([128, 4, 128]), op=ALU.mult), reads=[B[q], c["s2"]], writes=[h2f])
                tr.op("pool", lambda e: e.tensor_tensor(out=h2f[:], in0=h2f[:], in1=c["b2"][:].unsqueeze(2).to_broadcast([128, 8, 128]), op=ALU.add), reads=[h2f, c["b2"]], writes=[h2f])
                tr.op("act", lambda e: e.activation(out=h2T[:, :, i * 128:(i + 1) * 128], in_=h2f[:], func=AF.Copy), reads=[h2f], writes=[c["h2_r"][i]])
                for kc in range(8):
                    tr.op("pe", lambda e, kc=kc: e.matmul(out=B[2][:, 0:36], lhsT=h2f[:, kc, :], rhs=wr[:, kc, :], start=(kc == 0), stop=False), reads=[h2f, wr], writes=[B[2]])
                tr.op("pe", lambda e: e.matmul(out=B[2][:, 0:36], lhsT=c["ones_f"][0:1, :], rhs=brr[0:1, :], start=False, stop=True), reads=[c["ones_f"], brr], writes=[B[2]])
                tr.op("dve", lambda e: e.tensor_copy(out=lg[:, i, :], in_=B[2][:, 0:36]), reads=[B[2]], writes=[lg])

            recs = []
            for i in range(16):
                tr.begin_record()
                post_tile(i)
                recs.append(tr.end_record())
            tr.run_pipelined(recs, depth=2)
            self.tap("lg", lg[:].rearrange("p a b -> p (a b)"), [128, 16 * 36], F32, [lg])
            self.tap("h_lat", h_lat[:].rearrange("p a b -> p (a b)"), [128, 16 * D], F32, c["h_r"])
            def T(name, shape):
                t_ = self.sb(s2, name, shape, F32)
                rel.append(t_)
                return t_
            gmax = T("gmax", [128, 16]); mg = T("mg", [128, 16, 4]); eg = T("eg", [128, 16, 4]); gsum = T("gsum", [128, 16]); pg = T("pg", [128, 16])
            t48 = T("t48", [128, 16, 4, 8]); ein = T("ein", [128, 16, 8]); m1 = T("m1", [128, 16]); k1 = T("k1", [128, 16, 8]); e2 = T("e2", [128, 16, 8])
            m2 = T("m2", [128, 16]); k2 = T("k2", [128, 16, 8]); dd_ = T("dd_", [128, 16]); w1 = T("w1", [128, 16]); w2 = T("w2", [128, 16]); cw8 = T("cw8", [128, 16, 8])
            gl = lg[:, :, 0:4]
            el = lg[:, :, 4:36].rearrange("p t (g x) -> p t g x", g=4)
            V = lambda fn, r, w: tr.op("dve", fn, reads=r, writes=w)
            V(lambda e: e.tensor_reduce(out=gmax[:], in_=gl, axis=AX.X, op=ALU.max), [lg], [gmax])
            V(lambda e: e.tensor_tensor(out=mg[:], in0=gl, in1=gmax[:].unsqueeze(2).to_broadcast([128, 16, 4]), op=ALU.is_equal), [lg, gmax], [mg])
            V(lambda e: e.tensor_tensor(out=eg[:], in0=gl, in1=gmax[:].unsqueeze(2).to_broadcast([128, 16, 4]), op=ALU.subtract), [lg, gmax], [eg])
            tr.op("act", lambda e: e.activation(out=eg[:], in_=eg[:], func=AF.Exp), reads=[eg], writes=[eg])
            V(lambda e: e.tensor_reduce(out=gsum[:], in_=eg[:], axis=AX.X, op=ALU.add), [eg], [gsum])
            V(lambda e: e.reciprocal(out=pg[:], in_=gsum[:]), [gsum], [pg])
            V(lambda e: e.tensor_tensor(out=t48[:], in0=el, in1=mg[:].unsqueeze(3).to_broadcast([128, 16, 4, 8]), op=ALU.mult), [lg, mg], [t48])
            V(lambda e: e.tensor_reduce(out=ein[:], in_=t48[:].rearrange("p t g x -> p t x g"), axis=AX.X, op=ALU.add), [t48], [ein])
            V(lambda e: e.tensor_reduce(out=m1[:], in_=ein[:], axis=AX.X, op=ALU.max), [ein], [m1])
            V(lambda e: e.tensor_tensor(out=k1[:], in0=ein[:], in1=m1[:].unsqueeze(2).to_broadcast([128, 16, 8]), op=ALU.is_equal), [ein, m1], [k1])
            V(lambda e: e.scalar_tensor_tensor(out=e2[:], in0=k1[:], scalar=-1.0e30, in1=ein[:], op0=ALU.mult, op1=ALU.add), [k1, ein], [e2])
            V(lambda e: e.tensor_reduce(out=m2[:], in_=e2[:], axis=AX.X, op=ALU.max), [e2], [m2])
            V(lambda e: e.tensor_tensor(out=k2[:], in0=e2[:], in1=m2[:].unsqueeze(2).to_broadcast([128, 16, 8]), op=ALU.is_equal), [e2, m2], [k2])
            V(lambda e: e.tensor_tensor(out=dd_[:], in0=m2[:], in1=m1[:], op=ALU.subtract), [m1, m2], [dd_])
            tr.op("act", lambda e: e.activation(out=dd_[:], in_=dd_[:], func=AF.Exp), reads=[dd_], writes=[dd_])
            V(lambda e: e.tensor_scalar(out=w1[:], in0=dd_[:], scalar1=1.0, scalar2=None, op0=ALU.add), [dd_], [w1])
            V(lambda e: e.reciprocal(out=w1[:], in_=w1[:]), [w1], [w1])
            V(lambda e: e.tensor_tensor(out=w2[:], in0=dd_[:], in1=w1[:], op=ALU.mult), [dd_, w1], [w2])
            V(lambda e: e.tensor_tensor(out=w1[:], in0=w1[:], in1=pg[:], op=ALU.mult), [w1, pg], [w1])
            V(lambda e: e.tensor_tensor(out=w2[:], in0=w2[:], in1=pg[:], op=ALU.mult), [w2, pg], [w2])
            V(lambda e: e.tensor_tensor(out=k1[:], in0=k1[:], in1=w1[:].unsqueeze(2).to_broadcast([128, 16, 8]), op=ALU.mult), [k1, w1], [k1])
            V(lambda e: e.tensor_tensor(out=k2[:], in0=k2[:], in1=w2[:].unsqueeze(2).to_broadcast([128, 16, 8]), op=ALU.mult), [k2, w2], [k2])
            V(lambda e: e.tensor_tensor(out=cw8[:], in0=k1[:], in1=k2[:], op=ALU.add), [k1, k2], [cw8])
            V(lambda e: e.tensor_tensor(out=c["comb"][:].rearrange("p t (g x) -> p t g x", g=4), in0=mg[:].unsqueeze(3).to_broadcast([128, 16, 4, 8]),
                                        in1=cw8[:].unsqueeze(2).to_broadcast([128, 16, 4, 8]), op=ALU.mult), [mg, cw8], [c["comb"]])
            self.tap("comb", c["comb"][:].rearrange("p a b -> p (a b)"), [128, 512], F32, [c["comb"]])
            self.barrier_release(rel)

    def stage_moe(self, st):
        tr, c, I = self.tr, self.c, self.I
        self.fence()
        h_lat, h2T, comb = c["h_lat"], c["h2T"], c["comb"]
        with ExitStack() as s2:
            wgt = [self.sb(s2, "mwg%d" % i, [128, 8, DFF], BF16) for i in range(2)]
            wut = [self.sb(s2, "mwu%d" % i, [128, 8, DFF], BF16) for i in range(2)]
            wdt = [self.sb(s2, "mwd%d" % i, [128, 4, D], BF16) for i in range(2)]
            stg = [self.sb(s2, "mstg%d" % i, [128, 8, DFF], F32) for i in range(2)]
            stgs = [self.dsem() for _ in range(2)]
            ns = [0]
            sg_ = [self.sb(s2, "msg%d" % i, [128, 512], F32) for i in range(2)]
            heT = [self.sb(s2, "heT%d" % i, [128, 4, 512], BF16) for i in range(2)]
            pG = [self.ps(s2, "mpG%d" % i) for i in range(2)]
            pU = [self.ps(s2, "mpU%d" % i) for i in range(2)]
            pDn = [self.ps(s2, "mpD%d" % i) for i in range(4)]
            rel = wgt + wut + wdt + sg_ + heT + pG + pU + pDn + stg
            nb = 0
            for ex in range(NEXP):
                k = ex % 2
                wg_e, wu_e, wd_e = wgt[k], wut[k], wdt[k]
                for (dst, src) in ((wg_e, I["w_gate"].t[ex].rearrange("(kc p) n -> p kc n", p=128)), (wu_e, I["w_up"].t[ex].rearrange("(kc p) n -> p kc n", p=128))):
                    sg_t, sg_s = stg[ns[0] % 2], stgs[ns[0] % 2]
                    ns[0] += 1
                    tr.dma("sp", sg_s, out=sg_t[:], in_=src, writes=[sg_t])
                    tr.op("pool", lambda e, dst=dst, sg_t=sg_t: e.tensor_copy(out=dst[:], in_=sg_t[:]), reads=[sg_t], writes=[dst])
                sg_t, sg_s = stg[ns[0] % 2], stgs[ns[0] % 2]
                ns[0] += 1
                sv = sg_t[:].rearrange("p a b -> p (a b)").rearrange("p (f n) -> p f n", f=4)
                tr.dma("sp", sg_s, out=sv, in_=I["w_down"].t[ex].rearrange("(fc p) n -> p fc n", p=128), writes=[sg_t])
                tr.op("pool", lambda e, wd_e=wd_e, sv=sv: e.tensor_tensor(out=wd_e[:], in0=sv, in1=c["g2_bc"][:].unsqueeze(1).to_broadcast([128, 4, D]), op=ALU.mult),
                      reads=[sg_t, c["g2_bc"]], writes=[wd_e])
                for j in range(4):
                    he = heT[nb % 2]
                    nb += 1
                    hres = [c["h2_r"][j * 4 + q] for q in range(4)]
                    for fc in range(4):
                        g_p, u_p, sg = pG[fc % 2], pU[fc % 2], sg_[fc % 2]
                        for kc in range(8):
                            tr.op("pe", lambda e, kc=kc, fc=fc, g_p=g_p: e.matmul(out=g_p[:], lhsT=wg_e[:, kc, fc * 128:(fc + 1) * 128], rhs=h2T[:, kc, j * 512:(j + 1) * 512],
                                                                               start=(kc == 0), stop=(kc == 7)), reads=[wg_e] + hres, writes=[g_p])
                        for kc in range(8):
                            tr.op("pe", lambda e, kc=kc, fc=fc, u_p=u_p: e.matmul(out=u_p[:], lhsT=wu_e[:, kc, fc * 128:(fc + 1) * 128], rhs=h2T[:, kc, j * 512:(j + 1) * 512],
                                                                               start=(kc == 0), stop=(kc == 7)), reads=[wu_e] + hres, writes=[u_p])
                        tr.op("act", lambda e, g_p=g_p, sg=sg: e.activation(out=sg[:], in_=g_p[:], func=AF.Silu), reads=[g_p], writes=[sg])
                        tr.op("dve", lambda e, u_p=u_p, sg=sg, fc=fc, he=he: e.tensor_tensor(out=he[:, fc, :], in0=u_p[:], in1=sg[:], op=ALU.mult), reads=[u_p, sg], writes=[he])
                    for tt in range(4):
                        ti = j * 4 + tt
                        for hh in range(2):
                            d_p = pDn[(tt * 2 + hh) % 4]
                            for fc in range(4):
                                tr.op("pe", lambda e, fc=fc, d_p=d_p, tt=tt, hh=hh, he=he: e.matmul(out=d_p[:], lhsT=he[:, fc, tt * 128:(tt + 1) * 128], rhs=wd_e[:, fc, hh * 512:(hh + 1) * 512],
                                                                                              start=(fc == 0), stop=(fc == 3)), reads=[he, wd_e], writes=[d_p])
                            tr.op("dve", lambda e, d_p=d_p, ti=ti, hh=hh, ex=ex: e.scalar_tensor_tensor(
                                out=h_lat[:, ti, hh * 512:(hh + 1) * 512], in0=d_p[:], scalar=comb[:, ti, ex:ex + 1], in1=h_lat[:, ti, hh * 512:(hh + 1) * 512], op0=ALU.mult, op1=ALU.add),
                                reads=[d_p, comb, c["h_r"][ti]], writes=[c["h_r"][ti]])
            self.tap("h_fin", h_lat[:].rearrange("p a b -> p (a b)"), [128, 16 * D], F32, c["h_r"])
            self.barrier_release(rel)

    def stage_final(self, st):
        tr, c, I = self.tr, self.c, self.I
        self.fence()
        h_lat = c["h_lat"]
        out_v = self.out.t.rearrange("(n p) c -> n p c", p=128)
        with ExitStack() as s2:
            fn = self.sb(s2, "fn_bc", [128, D], F32)
            c_eps = self.sb(s2, "c_eps4", [128, 1], F32)
            junk = self.sb(s2, "fjunk", [128, D], BF16)
            ss = [self.sb(s2, "fss%d" % i, [128, 1], F32) for i in range(2)]
            rs = [self.sb(s2, "frs%d" % i, [128, 1], F32) for i in range(2)]
            ob = [self.sb(s2, "fob%d" % i, [128, D], F32) for i in range(2)]
            obs = [self.dsem() for _ in range(2)]
            tr.dma("sp", self.dsem(), out=fn[:], in_=I["final_norm"].t.partition_broadcast(128), writes=[fn])
            tr.op("pool", lambda e: e.memset(c_eps[:], EPS), writes=[c_eps])
            for i in range(16):
                hl = h_lat[:, i, :]
                s_, r_, o_ = ss[i % 2], rs[i % 2], ob[i % 2]
                tr.op("act", lambda e, s_=s_, hl=hl: e.activation(out=junk[:], in_=hl, func=AF.Square, accum_out=s_[:]), reads=[c["h_r"][i]], writes=[junk, s_])
                tr.op("act", lambda e, s_=s_, r_=r_: e.activation(out=r_[:], in_=s_[:], func=AF.Ln, scale=1.0 / D, bias=c_eps[:]), reads=[s_, c_eps], writes=[r_])
                tr.op("act", lambda e, r_=r_: e.activation(out=r_[:], in_=r_[:], func=AF.Exp, scale=-0.5), reads=[r_], writes=[r_])
                tr.op("dve", lambda e, r_=r_, o_=o_, hl=hl: e.scalar_tensor_tensor(out=o_[:], in0=hl, scalar=r_[:], in1=fn[:], op0=ALU.mult, op1=ALU.mult),
                      reads=[c["h_r"][i], r_, fn], writes=[o_])
                tr.dma("sp", obs[i % 2], out=out_v[i], in_=o_[:], reads=[o_], writes=[])
            self.final += ob


def prep_core(inp, b, hf):
    L = 0
    rev = hf == 1
    x, ctx = inp["x"][b], inp["ctx"][b]
    if not rev:
        ctx_a, own, oth = ctx, x[0:2048], x[2048:4096]
        dA, dB = 0, 1
    else:
        ctx_a, own, oth = ctx[::-1], x[2048:4096][::-1], x[0:2048][::-1]
        dA, dB = 1, 0
    m = {}
    m["xs"] = np.ascontiguousarray(np.concatenate([ctx_a, own, oth], axis=0), dtype=np.float32)
    cT = np.stack([inp["c"][b].reshape(8, 128).T, inp["c_ctx"].reshape(8, 128).T], axis=2).reshape(128, 16)
    m["cT"] = np.ascontiguousarray(cT, dtype=np.float32)
    m["w_ada"] = np.ascontiguousarray(inp["w_ada"][L])
    m["b_ada"] = np.ascontiguousarray(inp["b_ada"][L].reshape(1, -1))
    m["norm_mix_fm"] = np.ascontiguousarray(inp["norm_mix"][L].reshape(8, 128).T)
    m["norm_ffn_fm"] = np.ascontiguousarray(inp["norm_ffn"][L].reshape(8, 128).T)
    w_in = inp["w_in"][L]
    if rev:
        w_in = np.concatenate([w_in[:, :OFF_G], w_in[:, OFF_G + 16:OFF_G + 32], w_in[:, OFF_G:OFF_G + 16],
                               w_in[:, OFF_Z:OFF_DT], w_in[:, OFF_DT + 16:OFF_DT + 32], w_in[:, OFF_DT:OFF_DT + 16]], axis=1)
    m["w_in"] = np.ascontiguousarray(w_in)
    wu, gb = inp["gla_w_up"][L], inp["gla_b"][L]
    m["w_up_aug"] = np.ascontiguousarray(np.stack([np.concatenate([wu[dA], gb[dA][None, :]], axis=0),
                                                   np.concatenate([wu[dB], gb[dB][None, :]], axis=0)], axis=0))
    m["gla_norm"] = np.ascontiguousarray(inp["gla_norm"][L].reshape(1, -1))
    m["ssd_norm"] = np.ascontiguousarray(inp["ssd_norm"][L].reshape(1, -1))
    m["final_norm"] = np.ascontiguousarray(inp["final_norm"].reshape(1, -1))
    cw = inp["ssd_conv_w"][L]
    if rev:
        cw = cw[::-1, ::-1, :]
    m["conv_w_fm"] = np.ascontiguousarray(cw.reshape(9, 12, 128).transpose(2, 1, 0))
    m["conv_b_fm"] = np.ascontiguousarray(inp["ssd_conv_b"][L].reshape(12, 128).T)
    m["dt_bias"] = np.ascontiguousarray(np.concatenate([inp["ssd_dt_bias"][L][dA], inp["ssd_dt_bias"][L][dB]]).reshape(1, 32))
    m["a_log"] = np.ascontiguousarray(np.concatenate([inp["ssd_a_log"][L][dA], inp["ssd_a_log"][L][dB]]).reshape(1, 32))
    m["ssd_d"] = np.ascontiguousarray(inp["ssd_d"][L].reshape(1, 16))
    m["w_out"] = np.ascontiguousarray(inp["w_out"][L])
    wrt = np.concatenate([inp["router_group_w"][L], inp["router_expert_w"][L]], axis=1)
    m["w_router"] = np.ascontiguousarray(wrt.reshape(8, 128, 36).transpose(1, 0, 2).reshape(128, 8 * 36))
    m["b_router"] = np.ascontiguousarray(np.concatenate([inp["router_group_b"][L], inp["router_expert_b"][L]]).reshape(1, 36))
    m["w_gate"] = np.ascontiguousarray(inp["expert_w_gate"][L])
    m["w_up"] = np.ascontiguousarray(inp["expert_w_up"][L])
    m["w_down"] = np.ascontiguousarray(inp["expert_w_down"][L])
    return {k: np.asarray(v, dtype=np.float32) for k, v in m.items()}


def run(inputs, debug=None, stop_after=None, cores=8):
    bld = Builder(debug=debug, stop_after=stop_after)
    nc = bld.build()
    in_maps = [prep_core(inputs, i // 2, i % 2) for i in range(cores)]
    res = run_bass_kernel_spmd(nc, in_maps, core_ids=list(range(cores)))
    return res, bld


def kernel(**inputs):
    inputs = {k: np.asarray(v) for k, v in inputs.items()}
    res, _ = run(inputs)
    out = np.empty((4, 4096, D), dtype=np.float32)
    for i in range(8):
        b, hf = i // 2, i % 2
        o = np.asarray(res.results[i]["out"], dtype=np.float32)
        if hf == 0:
            out[b, 0:2048] = o
        else:
            out[b, 2048:4096] = o[::-1]
    return out
```

```python
import math
from contextlib import ExitStack

import numpy as np
import concourse.bass as bass
import concourse.mybir as mybir
from concourse.bass_utils import run_bass_kernel_spmd

F32 = mybir.dt.float32
BF16 = mybir.dt.bfloat16
AF = mybir.ActivationFunctionType
ALU = mybir.AluOpType
AX = mybir.AxisListType

D = 1024
NCTX, NOWN, NOTH = 256, 2048, 2048
TOK = NCTX + NOWN + NOTH
NT = TOK // 128
T_CTX0, T_OWN0, T_OTH0 = 0, 2, 18
EPS = 1e-6
IN_W = 5696
OFF_K, OFF_V, OFF_R, OFF_G, OFF_Z, OFF_XBC, OFF_DT = 512, 1024, 2048, 3072, 3104, 4128, 5664
NEXP, DFF = 32, 512


class Res:
    __slots__ = ("name", "lw", "rd")

    def __init__(self, name=""):
        self.name = name
        self.lw = None
        self.rd = {}


class Tile:
    def __init__(self, t, name):
        self.t = t
        self.r = Res(name)

    def __getitem__(self, idx):
        return self.t[idx]


class Tracker:
    ENG = ("pe", "act", "dve", "pool", "sp")
    CH = 2000

    def __init__(self, nc, sems, dma_sems, same_engine_sync=True):
        self.nc = nc
        self.eng = {"pe": nc.tensor, "act": nc.scalar, "dve": nc.vector, "pool": nc.gpsimd, "sp": nc.sync}
        self.cnt = {e: 0 for e in self.ENG}
        self.waited = {e: {} for e in self.ENG}
        self.sems = {e: [sems[e]] for e in sems}
        self.free_dma = list(dma_sems)
        self.same = same_engine_sync
        self.ninst = 0
        self.rec = None

    def new_dma_sem(self, group=0):
        d = self.free_dma.pop()
        self._uid = getattr(self, "_uid", 0) + 1
        d = d if isinstance(d, list) else [d, 0, 0, 0, "dma%d" % self._uid]
        if group:
            d[2] = group
            d[3] = d[1] + 16 * group
        return d

    def regroup(self, d, n):
        if self.rec is not None:
            self.rec.append(("call", lambda: self.regroup(d, n)))
            return
        assert d[2] == 0
        d[2] = n
        d[3] = d[1] + 16 * n

    def begin_record(self):
        self.rec = []

    def end_record(self):
        r, self.rec = self.rec, None
        return r

    def mark(self, name):
        self.rec.append(("mark", name))

    def _emit_item(self, it):
        if it[0] == "op":
            self.op(*it[1:])
        elif it[0] == "dma":
            self.dma(*it[1:])
        elif it[0] == "call":
            it[1]()

    def run_pipelined(self, records, depth=2, serial_fronts=False):
        assert self.rec is None
        active = []
        nxt = 0
        done = {}
        fin = -1

        def released(name, idx):
            return max(done.get(name, -1), fin) >= idx - 1 or idx == 0

        while active or nxt < len(records):
            while len(active) < depth and nxt < len(records):
                if serial_fronts and active and not active[-1][3]:
                    break
                if active and min(x[2] for x in active) <= nxt - depth:
                    break
                active.append([records[nxt], 0, nxt, False])
                nxt += 1
            progressed = False
            for a in list(active):
                lst, pos, idx, _ = a
                if pos >= len(lst):
                    a[3] = True
                    active.remove(a)
                    fin = max(fin, idx) if all(x[2] > idx for x in active) else fin
                    for nm in list(done.keys()):
                        done[nm] = max(done[nm], idx) if done[nm] >= idx - 1 else done[nm]
                    progressed = True
                    continue
                it = lst[pos]
                if it[0] == "mark":
                    nm = it[1]
                    if nm.startswith("need_"):
                        sec = nm[5:]
                        if sec == "state":
                            a[3] = True
                        if not released(sec, idx):
                            continue
                    elif nm.endswith("_done"):
                        sec = nm[:-5]
                        done[sec] = max(done.get(sec, -1), idx)
                    a[1] += 1
                    progressed = True
                    continue
                self._emit_item(it)
                a[1] += 1
                progressed = True
            if not progressed:
                for a in active:
                    it = a[0][a[1]]
                    if it[0] == "mark" and it[1].startswith("need_"):
                        done[it[1][5:]] = max(done.get(it[1][5:], -1), a[2] - 1)
                        progressed = True
                assert progressed

    def release_dma_sem(self, d):
        self.free_dma.append(d)

    def _wait(self, e, ev):
        if ev is None:
            return
        if ev[0] == "dma":
            _, s, v, key = ev
            if self.waited[e].get(key, 0) >= v:
                return
            self.waited[e][key] = v
            self.eng[e].wait_ge(s, v)
        else:
            pe, n = ev
            if pe == e and (not self.same or e in ("pe", "sp")):
                return
            if self.waited[e].get(pe, 0) >= n:
                return
            self.waited[e][pe] = n
            self.eng[e].wait_ge(self.sems[pe][(n - 1) // self.CH], (n - 1) % self.CH + 1)

    def _deps(self, e, reads, writes):
        for r in reads:
            self._wait(e, r.lw)
        for w in writes:
            self._wait(e, w.lw)
            for ev in w.rd.values():
                self._wait(e, ev)

    @staticmethod
    def _note_read(r, ev):
        key = ev[3] if ev[0] == "dma" else ev[0]
        old = r.rd.get(key)
        if old is None or (old[2] if old[0] == "dma" else old[1]) < (ev[2] if ev[0] == "dma" else ev[1]):
            r.rd[key] = ev

    def op(self, e, fn, reads=(), writes=()):
        if self.rec is not None:
            self.rec.append(("op", e, fn, list(reads), list(writes)))
            return None
        reads = [x.r if isinstance(x, Tile) else x for x in reads]
        writes = [x.r if isinstance(x, Tile) else x for x in writes]
        self._deps(e, reads, writes)
        self.cnt[e] += 1
        ev = (e, self.cnt[e])
        k = (self.cnt[e] - 1) // self.CH
        if k >= len(self.sems[e]):
            self.sems[e].append(self.free_dma.pop(0))
        fn(self.eng[e]).then_inc(self.sems[e][k], 1)
        self.ninst += 1
        for r in reads:
            self._note_read(r, ev)
        for w in writes:
            w.lw = ev
            w.rd = {}
        return ev

    def dma(self, e, dsem, out, in_, reads=(), writes=()):
        if self.rec is not None:
            self.rec.append(("dma", e, dsem, out, in_, list(reads), list(writes)))
            return None
        reads = [x.r if isinstance(x, Tile) else x for x in reads]
        writes = [x.r if isinstance(x, Tile) else x for x in writes]
        self._deps(e, reads, writes)
        if dsem[2] == 0:
            dsem[2] = 1
            dsem[3] = dsem[1] + 16
        dsem[1] += 16
        dsem[2] -= 1
        ev = ("dma", dsem[0], dsem[3], dsem[4])
        self.eng[e].dma_start(out=out, in_=in_).then_inc(dsem[0], 16)
        self.ninst += 1
        for r in reads:
            self._note_read(r, ev)
        for w in writes:
            w.lw = ev
            w.rd = {}
        return ev

    def wait_all(self, e, resources):
        for r in resources:
            r = r.r if isinstance(r, Tile) else r
            self._wait(e, r.lw)
            for ev in r.rd.values():
                self._wait(e, ev)


class Builder:
    def __init__(self, debug=None, stop_after=None):
        self.debug = debug or ()
        self.stop_after = stop_after
        self.nc = bass.Bass("TRN2", target_bir_lowering=False)
        self.dbg_out = {}

    def sb(self, st, name, shape, dt):
        self._uid = getattr(self, "_uid", 0) + 1
        return Tile(st.enter_context(self.nc.sbuf_tensor("sb%d_%s" % (self._uid, name), list(shape), dt)), name)

    def ps(self, st, name, shape=(128, 512), dt=F32):
        self._uid = getattr(self, "_uid", 0) + 1
        return Tile(st.enter_context(self.nc.psum_tensor("ps%d_%s" % (self._uid, name), list(shape), dt)), name)

    def dram_in(self, name, shape, dt=F32):
        return Tile(self.nc.dram_tensor(name, list(shape), dt, kind="ExternalInput").ap(), name)

    def dram_out(self, name, shape, dt=F32):
        return Tile(self.nc.dram_tensor(name, list(shape), dt, kind="ExternalOutput").ap(), name)

    def dram_scr(self, name, shape, dt):
        return Tile(self.nc.dram_tensor(name, list(shape), dt, kind="Internal").ap(), name)

    def dsem(self, group=0):
        return self.tr.new_dma_sem(group)

    def build(self):
        nc = self.nc
        I = {}
        I["xs"] = self.dram_in("xs", [TOK, D])
        I["cT"] = self.dram_in("cT", [128, 16])
        I["w_ada"] = self.dram_in("w_ada", [D, 6 * D])
        I["b_ada"] = self.dram_in("b_ada", [1, 6 * D])
        I["norm_mix_fm"] = self.dram_in("norm_mix_fm", [128, 8])
        I["norm_ffn_fm"] = self.dram_in("norm_ffn_fm", [128, 8])
        I["w_in"] = self.dram_in("w_in", [D, IN_W])
        I["w_up_aug"] = self.dram_in("w_up_aug", [2, 17, 512])
        I["gla_norm"] = self.dram_in("gla_norm", [1, 256])
        I["ssd_norm"] = self.dram_in("ssd_norm", [1, 1024])
        I["final_norm"] = self.dram_in("final_norm", [1, 1024])
        I["conv_w_fm"] = self.dram_in("conv_w_fm", [128, 12, 9])
        I["conv_b_fm"] = self.dram_in("conv_b_fm", [128, 12])
        I["dt_bias"] = self.dram_in("dt_bias", [1, 32])
        I["a_log"] = self.dram_in("a_log", [1, 32])
        I["ssd_d"] = self.dram_in("ssd_d", [1, 16])
        I["w_out"] = self.dram_in("w_out", [2048, D])
        I["w_router"] = self.dram_in("w_router", [128, 8 * 36])
        I["b_router"] = self.dram_in("b_router", [1, 36])
        I["w_gate"] = self.dram_in("w_gate", [NEXP, D, DFF])
        I["w_up"] = self.dram_in("w_up", [NEXP, D, DFF])
        I["w_down"] = self.dram_in("w_down", [NEXP, DFF, D])
        self.I = I
        self.out = self.dram_out("out", [NOWN, D])

        with ExitStack() as st:
            sems = {e: st.enter_context(nc.semaphore("s_" + e)) for e in Tracker.ENG}
            dsems = [st.enter_context(nc.semaphore("d%d" % i)) for i in range(90)]
            self.tr = Tracker(nc, sems, dsems)
            self.program(st)
        return nc

    def tap(self, name, tile_ap, shape, dt, reads):
        if name not in self.debug:
            return
        o = self.dram_out("dbg_" + name, shape, dt)
        self.dbg_out[name] = o
        n = shape[1]
        step = 2048
        d = self.dsem(len(range(0, n, step)))
        for c0 in range(0, n, step):
            c1 = min(n, c0 + step)
            self.tr.dma("sp", d, out=o.t[:, c0:c1], in_=tile_ap[:, c0:c1], reads=reads, writes=[o])
        self.final.append(o)

    def program(self, st):
        tr = self.tr
        self.final = []
        self.consts(st)
        self.stage_adaln(st)
        with ExitStack() as mst:
            self.stage_hT(mst)
            if self.stop_after == "hT":
                return self.finish()
            self.stage_gla(mst)
            if self.stop_after == "gla":
                return self.finish()
            self.stage_conv(mst)
            if self.stop_after == "conv":
                return self.finish()
            self.stage_ssd(mst)
            if self.stop_after == "ssd":
                return self.finish()
            self.barrier_release([self.c["hT"], self.c["BT"], self.c["CT"]] + self.c["hT_r"])
        self.stage_post(st)
        if self.stop_after == "post":
            return self.finish()
        self.stage_moe(st)
        if self.stop_after == "moe":
            return self.finish()
        self.stage_final(st)
        return self.finish()

    def finish(self):
        self.tr.wait_all("sp", self.final)

    def consts(self, st):
        tr = self.tr
        c = {}
        self.c = c
        c["ident_f"] = self.sb(st, "ident_f", [128, 128], F32)
        c["ident_b"] = self.sb(st, "ident_b", [128, 128], BF16)
        c["ones_f"] = self.sb(st, "ones_f", [128, 128], F32)
        for nm in ("tri_le", "tri_ge", "tri_gt", "tri_lt"):
            c[nm] = self.sb(st, nm, [128, 128], F32)
        idf = c["ident_f"]
        tr.op("pool", lambda e: e.memset(idf[:], 0.0), writes=[idf])
        tr.op("pool", lambda e: e.affine_select(out=idf[:], in_=idf[:], pattern=[[-1, 128]], compare_op=ALU.not_equal,
                                               fill=1.0, base=0, channel_multiplier=1), reads=[idf], writes=[idf])
        tr.op("pool", lambda e: e.tensor_copy(out=c["ident_b"][:], in_=idf[:]), reads=[idf], writes=[c["ident_b"]])
        tr.op("pool", lambda e: e.memset(c["ones_f"][:], 1.0), writes=[c["ones_f"]])
        specs = {"tri_le": (ALU.is_gt, 0), "tri_ge": (ALU.is_gt, 0), "tri_gt": (ALU.is_gt, 0), "tri_lt": (ALU.is_gt, 0)}
        t = c["tri_le"]
        tr.op("pool", lambda e: e.memset(t[:], 1.0), writes=[t])
        tr.op("pool", lambda e: e.affine_select(out=t[:], in_=t[:], pattern=[[1, 128]], compare_op=ALU.is_ge,
                                               fill=0.0, base=0, channel_multiplier=-1), reads=[t], writes=[t])
        t2 = c["tri_ge"]
        tr.op("pool", lambda e: e.memset(t2[:], 1.0), writes=[t2])
        tr.op("pool", lambda e: e.affine_select(out=t2[:], in_=t2[:], pattern=[[-1, 128]], compare_op=ALU.is_ge,
                                               fill=0.0, base=0, channel_multiplier=1), reads=[t2], writes=[t2])
        t3 = c["tri_gt"]
        tr.op("pool", lambda e: e.memset(t3[:], 1.0), writes=[t3])
        tr.op("pool", lambda e: e.affine_select(out=t3[:], in_=t3[:], pattern=[[-1, 128]], compare_op=ALU.is_gt,
                                               fill=0.0, base=0, channel_multiplier=1), reads=[t3], writes=[t3])
        t4 = c["tri_lt"]
        tr.op("pool", lambda e: e.memset(t4[:], 1.0), writes=[t4])
        tr.op("pool", lambda e: e.affine_select(out=t4[:], in_=t4[:], pattern=[[1, 128]], compare_op=ALU.is_gt,
                                               fill=0.0, base=0, channel_multiplier=-1), reads=[t4], writes=[t4])
        self.tap("tri_le", c["tri_le"][:], [128, 128], F32, [c["tri_le"]])
        self.tap("tri_gt", c["tri_gt"][:], [128, 128], F32, [c["tri_gt"]])

    def stage_adaln(self, st):
        tr, c, I = self.tr, self.c, self.I
        c["mod_fm"] = self.sb(st, "mod_fm", [128, 6, 8, 2], F32)
        c["g1_bc"] = self.sb(st, "g1_bc", [128, D], F32)
        c["g2_bc"] = self.sb(st, "g2_bc", [128, D], F32)
        c["s1"] = self.sb(st, "s1", [128, 8], F32)
        c["s1c"] = self.sb(st, "s1c", [128, 8], F32)
        c["b1"] = self.sb(st, "b1", [128, 8], F32)
        c["b1c"] = self.sb(st, "b1c", [128, 8], F32)
        c["s2"] = self.sb(st, "s2", [128, 8], F32)
        c["b2"] = self.sb(st, "b2", [128, 8], F32)
        with ExitStack() as s2:
            cT = self.sb(s2, "cT", [128, 16], F32)
            scT = self.sb(s2, "scT", [128, 16], F32)
            sc_rep = self.sb(s2, "sc_rep", [128, 8, 128], F32)
            brow = self.sb(s2, "brow", [1, 6 * D], F32)
            nm = self.sb(s2, "nm", [128, 8], F32)
            nf = self.sb(s2, "nf", [128, 8], F32)
            wblk = [self.sb(s2, "wblk%d" % i, [128, 8, D], F32) for i in range(2)]
            wsem = [self.dsem() for _ in range(2)]
            modps = self.ps(s2, "modps", [128, 512], F32)
            gps = [self.ps(s2, "gps%d" % i, [128, 512], F32) for i in range(2)]
            d = self.dsem(4)
            tr.dma("sp", d, out=cT[:], in_=I["cT"].t, writes=[cT])
            tr.dma("sp", d, out=brow[:], in_=I["b_ada"].t, writes=[brow])
            tr.dma("sp", d, out=nm[:], in_=I["norm_mix_fm"].t, writes=[nm])
            tr.dma("sp", d, out=nf[:], in_=I["norm_ffn_fm"].t, writes=[nf])
            tr.op("act", lambda e: e.activation(out=scT[:], in_=cT[:], func=AF.Silu), reads=[cT], writes=[scT])
            tr.op("dve", lambda e: e.tensor_copy(out=sc_rep[:], in_=scT[:].rearrange("p (k j) -> p k j", j=2)[:, :, 0:1].to_broadcast([128, 8, 128])),
                  reads=[scT], writes=[sc_rep])
            w_ada = I["w_ada"].t.rearrange("(kc p) n -> p kc n", p=128)
            mview = modps[:, 0:96].rearrange("p (b f t) -> p b f t", b=6, f=8)
            for blk in range(6):
                wb = wblk[blk % 2]
                tr.dma("sp", wsem[blk % 2], out=wb[:], in_=w_ada[:, :, blk * D:(blk + 1) * D], writes=[wb])
                if blk in (0, 1, 3, 4):
                    for fc in range(8):
                        for kc in range(8):
                            tr.op("pe", lambda e, fc=fc, kc=kc, wb=wb, blk=blk: e.matmul(
                                out=mview[:, blk, fc, :], lhsT=wb[:, kc, fc * 128:(fc + 1) * 128],
                                rhs=scT[:, 2 * kc:2 * kc + 2], start=(kc == 0), stop=False),
                                reads=[wb, scT], writes=[modps])
                        tr.op("pe", lambda e, fc=fc, blk=blk: e.matmul(
                            out=mview[:, blk, fc, :], lhsT=brow[0:1, blk * D + fc * 128: blk * D + (fc + 1) * 128],
                            rhs=c["ones_f"][0:1, 0:2], start=False, stop=True),
                            reads=[brow, c["ones_f"]], writes=[modps])
                else:
                    gdst = c["g1_bc"] if blk == 2 else c["g2_bc"]
                    for hh in range(2):
                        for kc in range(8):
                            tr.op("pe", lambda e, hh=hh, kc=kc, wb=wb: e.matmul(
                                out=gps[hh][:], lhsT=sc_rep[:, kc, :], rhs=wb[:, kc, hh * 512:(hh + 1) * 512],
                                start=(kc == 0), stop=False), reads=[wb, sc_rep], writes=[gps[hh]])
                        tr.op("pe", lambda e, hh=hh, blk=blk: e.matmul(
                            out=gps[hh][:], lhsT=c["ones_f"][0:1, :], rhs=brow[0:1, blk * D + hh * 512: blk * D + (hh + 1) * 512],
                            start=False, stop=True), reads=[brow, c["ones_f"]], writes=[gps[hh]])
                        tr.op("act", lambda e, hh=hh, gdst=gdst: e.activation(out=gdst[:, hh * 512:(hh + 1) * 512], in_=gps[hh][:], func=AF.Copy),
                              reads=[gps[hh]], writes=[gdst])
            mf = c["mod_fm"]
            mflat = mf[:].rearrange("p b f t -> p (b f t)")
            tr.op("dve", lambda e: e.tensor_copy(out=mflat[:, 0:32], in_=modps[:, 0:32]), reads=[modps], writes=[mf])
            tr.op("dve", lambda e: e.tensor_copy(out=mflat[:, 48:80], in_=modps[:, 48:80]), reads=[modps], writes=[mf])
            tr.op("dve", lambda e: e.scalar_tensor_tensor(out=c["s1"][:], in0=mf[:, 1, :, 0], scalar=1.0, in1=nm[:], op0=ALU.add, op1=ALU.mult),
                  reads=[mf, nm], writes=[c["s1"]])
            tr.op("dve", lambda e: e.scalar_tensor_tensor(out=c["s1c"][:], in0=mf[:, 1, :, 1], scalar=1.0, in1=nm[:], op0=ALU.add, op1=ALU.mult),
                  reads=[mf, nm], writes=[c["s1c"]])
            tr.op("dve", lambda e: e.scalar_tensor_tensor(out=c["s2"][:], in0=mf[:, 4, :, 0], scalar=1.0, in1=nf[:], op0=ALU.add, op1=ALU.mult),
                  reads=[mf, nf], writes=[c["s2"]])
            tr.op("dve", lambda e: e.tensor_copy(out=c["b1"][:], in_=mf[:, 0, :, 0]), reads=[mf], writes=[c["b1"]])
            tr.op("dve", lambda e: e.tensor_copy(out=c["b1c"][:], in_=mf[:, 0, :, 1]), reads=[mf], writes=[c["b1c"]])
            tr.op("dve", lambda e: e.tensor_copy(out=c["b2"][:], in_=mf[:, 3, :, 0]), reads=[mf], writes=[c["b2"]])
            self.tap("mod_fm", mf[:].rearrange("p b f t -> p (b f t)"), [128, 96], F32, [mf])
            self.tap("g1_bc", c["g1_bc"][:], [128, D], F32, [c["g1_bc"]])
            self.barrier_release([cT, scT, sc_rep, brow, nm, nf, wblk[0], wblk[1], modps, gps[0], gps[1]])

    def barrier_release(self, tiles):
        self.pending = getattr(self, "pending", [])
        for t in tiles:
            self.pending.append(t.r if isinstance(t, Tile) else t)

    def fence(self):
        pend = getattr(self, "pending", [])
        for e in Tracker.ENG:
            self.tr.wait_all(e, pend)
        self.pending = []

    def stage_hT(self, st):
        tr, c, I = self.tr, self.c, self.I
        self.fence()
        c["hT"] = self.sb(st, "hT", [128, 8, TOK], BF16)
        c["hT_r"] = [Res("hT%d" % t) for t in range(NT)]
        with ExitStack() as s2:
            xr = [self.sb(s2, "xr%d" % i, [128, D], F32) for i in range(3)]
            xsem = [self.dsem() for _ in range(3)]
            junk = self.sb(s2, "junk", [128, D], BF16)
            ss = [self.sb(s2, "ss%d" % i, [128, 1], F32) for i in range(3)]
            rstd = [self.sb(s2, "rstd%d" % i, [128, 1], F32) for i in range(3)]
            xn = [self.sb(s2, "xn%d" % i, [128, D], BF16) for i in range(3)]
            tmp = [self.sb(s2, "tmp%d" % i, [128, 8, 128], F32) for i in range(3)]
            tps = [self.ps(s2, "tps%d" % i, [128, 1024], BF16) for i in range(3)]
            epst = self.sb(s2, "epst", [128, 1], F32)
            tr.op("pool", lambda e: e.memset(epst[:], EPS), writes=[epst])
            rel = xr + ss + rstd + xn + tmp + tps + [junk, epst]
            recs = []
            for t in range(NT):
                tr.begin_record()
                x_t, ss_t, rs_t, xn_t, tmp_t, ps_t = xr[t % 3], ss[t % 3], rstd[t % 3], xn[t % 3], tmp[t % 3], tps[t % 3]
                hT_ap = c["hT"][:, :, t * 128:(t + 1) * 128]
                hT_r = c["hT_r"][t]
                isctx = t < T_OWN0
                sc, sh = (c["s1c"], c["b1c"]) if isctx else (c["s1"], c["b1"])
                tr.dma("sp", xsem[t % 3], out=x_t[:], in_=I["xs"].t[t * 128:(t + 1) * 128, :], writes=[x_t])
                tr.op("act", lambda e, x_t=x_t, ss_t=ss_t: e.activation(out=junk[:], in_=x_t[:], func=AF.Square, accum_out=ss_t[:]),
                      reads=[x_t], writes=[junk, ss_t])
                tr.op("act", lambda e, ss_t=ss_t, rs_t=rs_t: e.activation(out=rs_t[:], in_=ss_t[:], func=AF.Ln, scale=1.0 / D, bias=epst[:]),
                      reads=[ss_t, epst], writes=[rs_t])
                tr.op("act", lambda e, rs_t=rs_t: e.activation(out=rs_t[:], in_=rs_t[:], func=AF.Exp, scale=-0.5),
                      reads=[rs_t], writes=[rs_t])
                tr.op("dve", lambda e, x_t=x_t, rs_t=rs_t, xn_t=xn_t: e.tensor_scalar(out=xn_t[:], in0=x_t[:], scalar1=rs_t[:], scalar2=None, op0=ALU.mult),
                      reads=[x_t, rs_t], writes=[xn_t])
                for kc in range(8):
                    tr.op("pe", lambda e, kc=kc, xn_t=xn_t, ps_t=ps_t: e.transpose(out=ps_t[:, kc * 128:(kc + 1) * 128], in_=xn_t[:, kc * 128:(kc + 1) * 128], identity=c["ident_b"][:]),
                          reads=[xn_t, c["ident_b"]], writes=[ps_t])
                tr.op("dve", lambda e, ps_t=ps_t, tmp_t=tmp_t, sc=sc: e.tensor_tensor(
                    out=tmp_t[:], in0=ps_t[:].rearrange("p (k t) -> p k t", k=8), in1=sc[:].unsqueeze(2).to_broadcast([128, 8, 128]), op=ALU.mult),
                    reads=[ps_t, sc], writes=[tmp_t])
                tr.op("pool", lambda e, tmp_t=tmp_t, hT_ap=hT_ap, sh=sh: e.tensor_tensor(
                    out=hT_ap, in0=tmp_t[:], in1=sh[:].unsqueeze(2).to_broadcast([128, 8, 128]), op=ALU.add),
                    reads=[tmp_t, sh], writes=[hT_r])
                recs.append(tr.end_record())
            tr.run_pipelined(recs, depth=3)
            for t in (0, 2, 17, 33):
                if ("hT%d" % t) in self.debug:
                    o = self.dram_out("dbg_hT%d" % t, [128, 8, 128], BF16)
                    tr.dma("sp", self.dsem(), out=o.t, in_=c["hT"][:, :, t * 128:(t + 1) * 128], reads=[c["hT_r"][t]], writes=[o])
                    self.final.append(o)
            self.barrier_release(rel)

    def scratch(self, name, shape, dt):
        if name in self.debug:
            o = self.dram_out("dbg_" + name, shape, dt)
            self.final.append(o)
            return o
        return self.dram_scr(name, shape, dt)

    def stage_conv(self, st):
        tr, c, I = self.tr, self.c, self.I
        self.fence()
        c["x_tok"] = self.scratch("x_tok", [TOK, 1024], BF16)
        c["B_tok"] = self.scratch("B_tok", [TOK, 256], BF16)
        c["BT"] = self.sb(st, "BT", [128, 2, NOWN], BF16)
        c["CT"] = self.sb(st, "CT", [128, 2, NOWN], BF16)
        xtok_v = c["x_tok"].t.rearrange("(n p) c -> p n c", p=128)
        btok_v = c["B_tok"].t.rearrange("(n p) c -> p n c", p=128)
        w_in_v = I["w_in"].t.rearrange("(kc p) n -> p kc n", p=128)
        with ExitStack() as s2:
            wx = [self.sb(s2, "wx%d" % i, [128, 8, 128], BF16) for i in range(2)]
            wxs = [self.dsem() for _ in range(2)]
            cw = self.sb(s2, "cw", [128, 12, 9], F32)
            cb = self.sb(s2, "cb", [128, 12], F32)
            diag = [self.sb(s2, "diag%d" % i, [128, 9, 128], BF16) for i in range(2)]
            pre = [self.sb(s2, "pre%d" % i, [128, 66, 66], BF16) for i in range(2)]
            prec = [self.sb(s2, "prec%d" % i, [128, 258], BF16) for i in range(2)]
            post = [self.sb(s2, "post%d" % i, [128, 512], BF16) for i in range(3)]
            tst = [self.sb(s2, "tst%d" % i, [128, 4, 128], BF16) for i in range(3)]
            tsem = [self.dsem() for _ in range(3)]
            pp = [self.ps(s2, "pp%d" % i) for i in range(2)]
            pc = [self.ps(s2, "pc%d" % i) for i in range(2)]
            pt = [self.ps(s2, "pt%d" % i, [128, 1024], BF16) for i in range(2)]
            rel = wx + diag + pre + prec + post + tst + pp + pc + pt + [cw, cb]
            d0 = self.dsem(2)
            tr.dma("sp", d0, out=cw[:], in_=I["conv_w_fm"].t, writes=[cw])
            tr.dma("sp", d0, out=cb[:], in_=I["conv_b_fm"].t, writes=[cb])
            for i in range(2):
                tr.op("pool", lambda e, i=i: e.memset(pre[i][:], 0.0), writes=[pre[i]])
                tr.op("pool", lambda e, i=i: e.memset(prec[i][:], 0.0), writes=[prec[i]])
            nev = 0
            npost = 0
            for ct in range(12):
                w, dg, pr, prc = wx[ct % 2], diag[ct % 2], pre[ct % 2], prec[ct % 2]
                tr.dma("pool", wxs[ct % 2], out=w[:], in_=w_in_v[:, :, OFF_XBC + ct * 128: OFF_XBC + (ct + 1) * 128], writes=[w])
                tr.op("pool", lambda e, dg=dg, ct=ct: e.tensor_tensor(out=dg[:], in0=c["ident_f"][:].unsqueeze(1).to_broadcast([128, 9, 128]),
                                                                  in1=cw[:, ct, :].unsqueeze(2).to_broadcast([128, 9, 128]), op=ALU.mult),
                      reads=[c["ident_f"], cw], writes=[dg])
                for blk in range(9):
                    p_t = pp[nev % 2]
                    if blk == 0:
                        n, tok0, trs = 256, 0, [0, 1]
                    else:
                        n, tok0 = 512, NCTX + (blk - 1) * 512
                        trs = list(range(T_OWN0 + (blk - 1) * 4, T_OWN0 + blk * 4))
                    for kc in range(8):
                        tr.op("pe", lambda e, kc=kc, p_t=p_t, w=w, n=n, tok0=tok0: e.matmul(
                            out=p_t[:, 0:n], lhsT=w[:, kc, :], rhs=c["hT"][:, kc, tok0:tok0 + n], start=(kc == 0), stop=(kc == 7)),
                            reads=[w] + [c["hT_r"][t] for t in trs], writes=[p_t])
                    if blk == 0:
                        dst = prc[:, 1:257]
                        src = p_t[:, 0:256]
                        wr = prc
                    else:
                        r0 = (blk - 1) * 8
                        dst = pr[:, r0 + 1:r0 + 9, 1:65]
                        src = p_t[:, 0:512].rearrange("p (r q) -> p r q", q=64)
                        wr = pr
                    eng = "act" if nev % 2 == 0 else "dve"
                    if eng == "act":
                        tr.op("act", lambda e, dst=dst, src=src: e.activation(out=dst, in_=src, func=AF.Copy), reads=[p_t], writes=[wr])
                    else:
                        tr.op("dve", lambda e, dst=dst, src=src: e.tensor_copy(out=dst, in_=src), reads=[p_t], writes=[wr])
                    nev += 1
                for blk in range(9):
                    if ct >= 10 and (blk == 0 or blk >= 5):
                        continue
                    c_t = pc[blk % 2]
                    if blk == 0:
                        n = 256
                        for kw in range(3):
                            tr.op("pe", lambda e, kw=kw, c_t=c_t, dg=dg, prc=prc: e.matmul(
                                out=c_t[:, 0:256], lhsT=dg[:, 3 + kw, :], rhs=prc[:, kw:kw + 256], start=(kw == 0), stop=(kw == 2)),
                                reads=[dg, prc], writes=[c_t])
                    else:
                        n = 512
                        r0 = (blk - 1) * 8
                        for tap in range(9):
                            kh, kw = tap // 3, tap % 3
                            tr.op("pe", lambda e, tap=tap, kh=kh, kw=kw, c_t=c_t, dg=dg, pr=pr, r0=r0: e.matmul(
                                out=c_t[:, 0:512], lhsT=dg[:, tap, :], rhs=pr[:, r0 + kh:r0 + kh + 8, kw:kw + 64], start=(tap == 0), stop=(tap == 8)),
                                reads=[dg, pr], writes=[c_t])
                    own_blk = 1 <= blk <= 4
                    if ct >= 8 and own_blk:
                        g = (ct - 8) % 2
                        dstT = (c["BT"] if ct < 10 else c["CT"])
                        o0 = (blk - 1) * 512
                        tr.op("act", lambda e, dstT=dstT, g=g, o0=o0, c_t=c_t, ct=ct: e.activation(
                            out=dstT[:, g, o0:o0 + 512], in_=c_t[:, 0:512], func=AF.Silu, bias=cb[:, ct:ct + 1]),
                            reads=[c_t, cb], writes=[dstT])
                        if ct >= 10:
                            continue
                        src_post, src_r = dstT[:, g, o0:o0 + 512], dstT
                    else:
                        po = post[npost % 3]
                        tr.op("act", lambda e, po=po, c_t=c_t, ct=ct, n=n: e.activation(
                            out=po[:, 0:n], in_=c_t[:, 0:n], func=AF.Silu, bias=cb[:, ct:ct + 1]),
                            reads=[c_t, cb], writes=[po])
                        src_post, src_r = po[:, 0:n], po
                    ntl = n // 128
                    t_t = pt[npost % 2]
                    ts_t = tst[npost % 3]
                    for i in range(ntl):
                        tr.op("pe", lambda e, i=i, t_t=t_t, src_post=src_post: e.transpose(
                            out=t_t[:, i * 128:(i + 1) * 128], in_=src_post[:, i * 128:(i + 1) * 128], identity=c["ident_b"][:]),
                            reads=[src_r, c["ident_b"]], writes=[t_t])
                    tr.op("dve", lambda e, t_t=t_t, ts_t=ts_t, ntl=ntl: e.tensor_copy(
                        out=ts_t[:, 0:ntl, :], in_=t_t[:, 0:ntl * 128].rearrange("p (a b) -> p a b", b=128)),
                        reads=[t_t], writes=[ts_t])
                    tile0 = 0 if blk == 0 else T_OWN0 + (blk - 1) * 4
                    if ct < 8:
                        dst_d, dst_r = xtok_v[:, tile0:tile0 + ntl, ct * 128:(ct + 1) * 128], c["x_tok"]
                    else:
                        dst_d, dst_r = btok_v[:, tile0:tile0 + ntl, (ct - 8) * 128:(ct - 7) * 128], c["B_tok"]
                    tr.dma("sp", tsem[npost % 3], out=dst_d, in_=ts_t[:, 0:ntl, :], reads=[ts_t], writes=[])
                    c.setdefault("scr_ev", []).append(ts_t)
                    npost += 1
            self.conv_store_tiles = tst
            self.tap("BT", c["BT"][:].rearrange("p g t -> p (g t)"), [128, 2 * NOWN], BF16, [c["BT"]])
            self.tap("CT", c["CT"][:].rearrange("p g t -> p (g t)"), [128, 2 * NOWN], BF16, [c["CT"]])
            for e in Tracker.ENG:
                tr.wait_all(e, tst)
            self.barrier_release(rel)

    def stage_gla(self, st):
        tr, c, I = self.tr, self.c, self.I
        self.fence()
        c["oB"] = self.scratch("oB", [NOWN, 1024], F32)
        c["yx"] = self.scratch("yx", [NOWN, 2048], BF16)
        oB_v = c["oB"].t.rearrange("(n p) c -> n p c", p=128)
        yx_v = c["yx"].t.rearrange("(n p) c -> n p c", p=128)
        w_in_v = I["w_in"].t.rearrange("(kc p) n -> p kc n", p=128)
        LNQ = math.log(128.0 ** -0.5)
        with ExitStack() as s2:
            wg = self.sb(s2, "wgla", [128, 8, 3072], BF16)
            wgs = [Res("wgla%d" % i) for i in range(6)]
            wgg = self.sb(s2, "wgg", [128, 8, 32], BF16)
            wup = self.sb(s2, "wup", [17, 2, 512], F32)
            gn = self.sb(s2, "gn_bc", [128, 256], F32)
            c_one = self.sb(s2, "c_one", [128, 1], F32)
            c_lnq = self.sb(s2, "c_lnq", [128, 1], F32)
            c_eps = self.sb(s2, "c_eps", [128, 1], F32)
            negcol = self.sb(s2, "negcol", [128, 2], F32)
            Tm = [self.sb(s2, "TmA", [128, 128], F32), self.sb(s2, "TmB", [128, 128], F32)]
            S = [self.sb(s2, "S_A", [128, 4, 256], F32), self.sb(s2, "S_B", [128, 4, 256], F32)]
            Sbf = self.sb(s2, "Sbf", [128, 4, 256], BF16)
            FS = []
            for par in range(2):
                f = {}
                f["g_aug"] = self.sb(s2, "g_aug%d" % par, [32, 128], F32)
                f["v_bf"] = self.sb(s2, "v_bf%d" % par, [128, 1024], BF16)
                f["lap"] = self.sb(s2, "lap%d" % par, [128, 512], F32)
                f["Einv"] = self.sb(s2, "Einv%d" % par, [128, 512], F32)
                f["Eq"] = self.sb(s2, "Eq%d" % par, [128, 512], F32)
                f["kt_"] = self.sb(s2, "kt_%d" % par, [128, 512], BF16)
                f["qt_"] = self.sb(s2, "qt_%d" % par, [128, 512], BF16)
                f["kqT"] = self.sb(s2, "kqT%d" % par, [128, 8, 128], BF16)
                f["PT"] = self.sb(s2, "PT%d" % par, [128, 4, 128], BF16)
                f["dcol"] = self.sb(s2, "dcol%d" % par, [128, 4], F32)
                f["P"] = [self.ps(s2, "gP%d_%d" % (par, i)) for i in range(4)]
                FS.append(f)
            silr = self.sb(s2, "silr", [128, 1024], F32)
            o_sb = self.sb(s2, "o_sb", [128, 1024], F32)
            oB_sb = [self.sb(s2, "oB_sb%d" % i, [128, 1024], F32) for i in range(2)]
            oBs = [self.dsem() for _ in range(2)]
            ost = [self.sb(s2, "ost%d" % i, [128, 1024], F32) for i in range(2)]
            osts = [self.dsem() for _ in range(2)]
            yst = [self.sb(s2, "yst%d" % i, [128, 1024], BF16) for i in range(2)]
            ysts = [self.dsem() for _ in range(2)]
            ss4 = self.sb(s2, "ss4", [128, 4], F32)
            rs4 = self.sb(s2, "rs4", [128, 4], F32)
            junk = self.sb(s2, "junkg", [128, 256], BF16)
            rel = [wg, wgg, wup, gn, c_one, c_lnq, c_eps, negcol, Tm[0], Tm[1], S[0], S[1], Sbf, silr, o_sb, ss4, rs4, junk] + oB_sb + ost + yst + wgs
            for f in FS:
                rel += [f[k] for k in ("g_aug", "v_bf", "lap", "Einv", "Eq", "kt_", "qt_", "kqT", "PT", "dcol")] + f["P"]
            d0 = self.dsem(9)
            for i in range(6):
                tr.dma("pool", d0, out=wg[:, :, i * 512:(i + 1) * 512], in_=w_in_v[:, :, i * 512:(i + 1) * 512], writes=[wgs[i]])
            tr.dma("pool", d0, out=wgg[:], in_=w_in_v[:, :, OFF_G:OFF_G + 32], writes=[wgg])
            tr.dma("sp", d0, out=wup[:], in_=I["w_up_aug"].t.rearrange("d k n -> k d n"), writes=[wup])
            tr.dma("sp", d0, out=gn[:], in_=I["gla_norm"].t.partition_broadcast(128), writes=[gn])
            tr.op("pool", lambda e: e.memset(c_one[:], 1.0), writes=[c_one])
            tr.op("pool", lambda e: e.memset(c_lnq[:], LNQ), writes=[c_lnq])
            tr.op("pool", lambda e: e.memset(c_eps[:], EPS), writes=[c_eps])
            tr.op("pool", lambda e: e.memset(negcol[:], -1.0 / 16.0), writes=[negcol])
            tr.op("pool", lambda e: e.tensor_scalar(out=Tm[0][:], in0=c["tri_le"][:], scalar1=-1.0 / 16.0, scalar2=None, op0=ALU.mult), reads=[c["tri_le"]], writes=[Tm[0]])
            tr.op("pool", lambda e: e.tensor_scalar(out=Tm[1][:], in0=c["tri_ge"][:], scalar1=-1.0 / 16.0, scalar2=None, op0=ALU.mult), reads=[c["tri_ge"]], writes=[Tm[1]])
            for f in FS:
                tr.op("pool", lambda e, f=f: e.memset(f["g_aug"][:], 1.0), writes=[f["g_aug"]])
            for dd in range(2):
                tr.op("pool", lambda e, dd=dd: e.memset(S[dd][:], 0.0), writes=[S[dd]])
            masks = [c["tri_le"], c["tri_ge"]]

            def gla_tile(t, dd, full, sweepA, own_idx, seq):
                f = FS[seq % 2]
                P = f["P"]
                g_aug, v_bf, lap, Einv, Eq, kt_, qt_, kqT, PT, dcol = (f[k] for k in ("g_aug", "v_bf", "lap", "Einv", "Eq", "kt_", "qt_", "kqT", "PT", "dcol"))
                Sd = S[dd]
                hres = [c["hT_r"][t]]
                lhs = lambda kc: c["hT"][:, kc, t * 128:(t + 1) * 128]

                def mm_tok(ps_t, c0, n, wres):
                    for kc in range(8):
                        tr.op("pe", lambda e, kc=kc: e.matmul(out=ps_t[:, 0:n], lhsT=lhs(kc), rhs=wg[:, kc, c0:c0 + n], start=(kc == 0), stop=(kc == 7)),
                              reads=hres + wres, writes=[ps_t])
                for kc in range(8):
                    tr.op("pe", lambda e, kc=kc: e.matmul(out=P[3][0:16, 0:128], lhsT=wgg[:, kc, dd * 16:(dd + 1) * 16], rhs=lhs(kc), start=(kc == 0), stop=(kc == 7)),
                          reads=hres + [wgg], writes=[P[3]])
                tr.op("act", lambda e: e.activation(out=g_aug[0:16, :], in_=P[3][0:16, 0:128], func=AF.Copy), reads=[P[3]], writes=[g_aug])
                mm_tok(P[0], 512, 512, [wgs[1]])
                mm_tok(P[1], 1024, 512, [wgs[2]])
                mm_tok(P[2], 1536, 512, [wgs[3]])
                tr.op("pe", lambda e: e.matmul(out=P[3][:, 0:512], lhsT=g_aug[0:17, :], rhs=wup[:, dd, :], start=True, stop=True), reads=[g_aug, wup], writes=[P[3]])
                tr.op("act", lambda e: e.activation(out=lap[:], in_=P[3][:, 0:512], func=AF.Exp, scale=-1.0), reads=[P[3]], writes=[lap])
                tr.op("act", lambda e: e.activation(out=lap[:], in_=lap[:], func=AF.Ln, bias=c_one[:]), reads=[lap, c_one], writes=[lap])
                tr.op("act", lambda e: e.activation(out=v_bf[:, 0:512], in_=P[1][:], func=AF.Copy), reads=[P[1]], writes=[v_bf])
                tr.op("dve", lambda e: e.tensor_copy(out=v_bf[:, 512:1024], in_=P[2][:]), reads=[P[2]], writes=[v_bf])
                tr.op("pe", lambda e: e.matmul(out=P[3][:, 0:512], lhsT=Tm[dd][:], rhs=lap[:], start=True, stop=True), reads=[Tm[dd], lap], writes=[P[3]])
                if full:
                    mm_tok(P[1], 0, 512, [wgs[0]])
                tr.op("act", lambda e: e.activation(out=Einv[:], in_=P[3][:, 0:512], func=AF.Exp, scale=-1.0), reads=[P[3]], writes=[Einv])
                if full:
                    tr.op("act", lambda e: e.activation(out=Eq[:], in_=P[3][:, 0:512], func=AF.Exp, bias=c_lnq[:]), reads=[P[3], c_lnq], writes=[Eq])
                tr.op("dve", lambda e: e.tensor_tensor(out=kt_[:], in0=P[0][:], in1=Einv[:], op=ALU.mult), reads=[P[0], Einv], writes=[kt_])
                for h in range(4):
                    tr.op("pe", lambda e, h=h: e.matmul(out=P[3][:, 2 * h:2 * h + 2], lhsT=lap[:, h * 128:(h + 1) * 128], rhs=negcol[:], start=True, stop=True),
                          reads=[lap, negcol], writes=[P[3]])
                tr.op("act", lambda e: e.activation(out=dcol[:], in_=P[3][:, 0:8:2], func=AF.Exp), reads=[P[3]], writes=[dcol])
                if full:
                    pT = P[2][:].bitcast(BF16)
                    tr.op("dve", lambda e: e.tensor_tensor(out=qt_[:], in0=P[1][:], in1=Eq[:], op=ALU.mult), reads=[P[1], Eq], writes=[qt_])
                    for h in range(4):
                        tr.op("pe", lambda e, h=h: e.transpose(out=pT[:, h * 128:(h + 1) * 128], in_=kt_[:, h * 128:(h + 1) * 128], identity=c["ident_b"][:]),
                              reads=[kt_, c["ident_b"]], writes=[P[2]])
                    for h in range(4):
                        tr.op("pe", lambda e, h=h: e.transpose(out=pT[:, (4 + h) * 128:(5 + h) * 128], in_=qt_[:, h * 128:(h + 1) * 128], identity=c["ident_b"][:]),
                              reads=[qt_, c["ident_b"]], writes=[P[2]])
                    tr.op("act", lambda e: e.activation(out=kqT[:].rearrange("p a b -> p (a b)"), in_=pT, func=AF.Copy), reads=[P[2]], writes=[kqT])
                    for h in range(4):
                        tr.op("pe", lambda e, h=h: e.matmul(out=P[0][:, h * 128:(h + 1) * 128], lhsT=kqT[:, h, :], rhs=kqT[:, 4 + h, :], start=True, stop=True),
                              reads=[kqT], writes=[P[0]])
                    tr.op("dve", lambda e: e.tensor_tensor(out=PT[:], in0=P[0][:].rearrange("p (h i) -> p h i", h=4),
                                                          in1=masks[dd][:].unsqueeze(1).to_broadcast([128, 4, 128]), op=ALU.mult),
                          reads=[P[0], masks[dd]], writes=[PT])
                kvb = [P[2], P[2], P[0], P[0]]
                for h in range(4):
                    cs = (h % 2) * 256
                    tr.op("pe", lambda e, h=h, cs=cs: e.matmul(out=kvb[h][:, cs:cs + 256], lhsT=kt_[:, h * 128:(h + 1) * 128], rhs=v_bf[:, h * 256:(h + 1) * 256], start=True, stop=True),
                          reads=[kt_, v_bf], writes=[kvb[h]])
                tr.mark("need_state")
                if full:
                    ob_ = [P[1], P[1], P[3], P[3]]
                    tr.op("act", lambda e: e.activation(out=Sbf[:].rearrange("p a b -> p (a b)"), in_=Sd[:].rearrange("p a b -> p (a b)"), func=AF.Copy), reads=[Sd], writes=[Sbf])
                    for h in range(4):
                        cs = (h % 2) * 256
                        tr.op("pe", lambda e, h=h, cs=cs: e.matmul(out=ob_[h][:, cs:cs + 256], lhsT=PT[:, h, :], rhs=v_bf[:, h * 256:(h + 1) * 256], start=True, stop=False),
                              reads=[PT, v_bf], writes=[ob_[h]])
                        tr.op("pe", lambda e, h=h, cs=cs: e.matmul(out=ob_[h][:, cs:cs + 256], lhsT=kqT[:, 4 + h, :], rhs=Sbf[:, h, :], start=False, stop=True),
                              reads=[kqT, Sbf], writes=[ob_[h]])
                tr.op("dve", lambda e: e.tensor_tensor(out=Sd[:, 0:2, :].rearrange("p a b -> p (a b)"), in0=P[2][:], in1=Sd[:, 0:2, :].rearrange("p a b -> p (a b)"), op=ALU.add),
                      reads=[P[2], Sd], writes=[Sd])
                tr.op("dve", lambda e: e.tensor_tensor(out=Sd[:, 2:4, :].rearrange("p a b -> p (a b)"), in0=P[0][:], in1=Sd[:, 2:4, :].rearrange("p a b -> p (a b)"), op=ALU.add),
                      reads=[P[0], Sd], writes=[Sd])
                tr.op("dve", lambda e: e.tensor_tensor(out=Sd[:], in0=Sd[:], in1=dcol[:].unsqueeze(2).to_broadcast([128, 4, 256]), op=ALU.mult),
                      reads=[Sd, dcol], writes=[Sd])
                if not full:
                    return
                if not sweepA:
                    os_ = ost[own_idx % 2]
                    tr.op("act", lambda e: e.activation(out=os_[:, 0:512], in_=P[1][:], func=AF.Copy), reads=[P[1]], writes=[os_])
                    tr.op("dve", lambda e: e.tensor_copy(out=os_[:, 512:1024], in_=P[3][:]), reads=[P[3]], writes=[os_])
                    tr.dma("sp", osts[own_idx % 2], out=oB_v[own_idx], in_=os_[:], reads=[os_], writes=[])
                    return
                ob = oB_sb[own_idx % 2]
                tr.dma("sp", oBs[own_idx % 2], out=ob[:], in_=oB_v[own_idx], writes=[ob])
                mm_tok(P[2], 2048, 512, [wgs[4]])
                tr.op("act", lambda e: e.activation(out=silr[:, 0:512], in_=P[2][:], func=AF.Silu), reads=[P[2]], writes=[silr])
                mm_tok(P[0], 2560, 512, [wgs[5]])
                tr.op("act", lambda e: e.activation(out=silr[:, 512:1024], in_=P[0][:], func=AF.Silu), reads=[P[0]], writes=[silr])
                tr.op("dve", lambda e: e.tensor_tensor(out=silr[:].rearrange("p (h v) -> p h v", h=4), in0=silr[:].rearrange("p (h v) -> p h v", h=4),
                                                      in1=gn[:].unsqueeze(1).to_broadcast([128, 4, 256]), op=ALU.mult), reads=[silr, gn], writes=[silr])
                for hh, pb in enumerate((P[1], P[3])):
                    tr.op("dve", lambda e, hh=hh, pb=pb: e.tensor_tensor(out=o_sb[:, hh * 512:(hh + 1) * 512], in0=pb[:], in1=ob[:, hh * 512:(hh + 1) * 512], op=ALU.add),
                          reads=[pb, ob], writes=[o_sb])
                for h in range(4):
                    tr.op("act", lambda e, h=h: e.activation(out=junk[:], in_=o_sb[:, h * 256:(h + 1) * 256], func=AF.Square, accum_out=ss4[:, h:h + 1]),
                          reads=[o_sb], writes=[junk, ss4])
                tr.op("act", lambda e: e.activation(out=rs4[:], in_=ss4[:], func=AF.Ln, scale=1.0 / 256.0, bias=c_eps[:]), reads=[ss4, c_eps], writes=[rs4])
                tr.op("act", lambda e: e.activation(out=rs4[:], in_=rs4[:], func=AF.Exp, scale=-0.5), reads=[rs4], writes=[rs4])
                tr.op("dve", lambda e: e.tensor_tensor(out=o_sb[:].rearrange("p (h v) -> p h v", h=4), in0=o_sb[:].rearrange("p (h v) -> p h v", h=4),
                                                      in1=rs4[:].unsqueeze(2).to_broadcast([128, 4, 256]), op=ALU.mult), reads=[o_sb, rs4], writes=[o_sb])
                ys = yst[own_idx % 2]
                tr.op("dve", lambda e: e.tensor_tensor(out=ys[:], in0=o_sb[:], in1=silr[:], op=ALU.mult), reads=[o_sb, silr], writes=[ys])
                tr.dma("sp", ysts[own_idx % 2], out=yx_v[own_idx][:, 0:1024], in_=ys[:], reads=[ys], writes=[])

            def sweep(tiles):
                recs = []
                for seq, (t, dd, full, sweepA, own_idx) in enumerate(tiles):
                    tr.begin_record()
                    gla_tile(t, dd, full, sweepA, own_idx, seq)
                    recs.append(tr.end_record())
                tr.run_pipelined(recs, depth=2)

            sweep([(t, 1, False, False, None) for t in (1, 0)])
            self.tap("gS_B", S[1][:].rearrange("p a b -> p (a b)"), [128, 1024], F32, [S[1]])
            sweep([(t, 1, False, False, None) for t in range(NT - 1, T_OTH0 - 1, -1)] +
                  [(t, 1, True, False, t - T_OWN0) for t in range(T_OTH0 - 1, T_OWN0 - 1, -1)])
            for e in Tracker.ENG:
                tr.wait_all(e, ost)
            sweep([(t, 0, False, True, None) for t in (0, 1)])
            self.tap("gS_A", S[0][:].rearrange("p a b -> p (a b)"), [128, 1024], F32, [S[0]])
            sweep([(t, 0, True, True, t - T_OWN0) for t in range(T_OWN0, T_OTH0)])
            for e in Tracker.ENG:
                tr.wait_all(e, yst)
            self.barrier_release(rel)

    def stage_ssd(self, st):
        tr, c, I = self.tr, self.c, self.I
        self.fence()
        c["yB"] = self.scratch("yB", [NOWN, 1024], F32)
        yB_v = c["yB"].t.rearrange("(n p) c -> n p c", p=128)
        yx_v = c["yx"].t.rearrange("(n p) c -> n p c", p=128)
        xtok_v = c["x_tok"].t.rearrange("(n p) c -> n p c", p=128)
        btok_v = c["B_tok"].t.rearrange("(n p) c -> n p c", p=128)
        w_in_v = I["w_in"].t.rearrange("(kc p) n -> p kc n", p=128)
        BT, CT = c["BT"], c["CT"]
        with ExitStack() as s2:
            wz = self.sb(s2, "wz", [128, 8, 1024], BF16)
            wdt = self.sb(s2, "wdt", [128, 8, 32], BF16)
            Abc = self.sb(s2, "Abc", [128, 32], F32)
            dtb = self.sb(s2, "dtb", [128, 32], F32)
            Dsk = self.sb(s2, "Dsk", [128, 16], F32)
            snb = self.sb(s2, "snb", [128, 1024], F32)
            c_one = self.sb(s2, "c_one2", [128, 1], F32)
            c_eps = self.sb(s2, "c_eps2", [128, 1], F32)
            ST = [self.sb(s2, "ST_A", [128, 2, 512], F32), self.sb(s2, "ST_B", [128, 2, 512], F32)]
            STbf = self.sb(s2, "STbf", [128, 2, 512], BF16)
            xt = [self.sb(s2, "xt%d" % i, [128, 1024], BF16) for i in range(2)]
            bt = [self.sb(s2, "bt%d" % i, [128, 256], BF16) for i in range(2)]
            xts = [self.dsem() for _ in range(2)]
            dt_ = self.sb(s2, "dt_", [128, 16], F32)
            dtA = self.sb(s2, "dtA", [128, 16], F32)
            acs = self.sb(s2, "acs", [128, 16], F32)
            ea = self.sb(s2, "ea", [128, 16], F32)
            dend = self.sb(s2, "dend", [128, 16], F32)
            dtot = self.sb(s2, "dtot", [128, 16], F32)
            R1 = self.sb(s2, "R1", [128, 16, 128], BF16)
            E = self.sb(s2, "E", [128, 16, 128], BF16)
            M = self.sb(s2, "M", [128, 16, 128], BF16)
            CBm = self.sb(s2, "CBm", [128, 2, 128], F32)
            xdt = self.sb(s2, "xdt", [128, 1024], BF16)
            xdd = self.sb(s2, "xdd", [128, 1024], BF16)
            silz = self.sb(s2, "silz", [128, 1024], F32)
            y_sb = self.sb(s2, "y_sb", [128, 1024], F32)
            tmp = self.sb(s2, "ytmp", [128, 1024], F32)
            yB_sb = [self.sb(s2, "yB_sb%d" % i, [128, 1024], F32) for i in range(2)]
            yBs = [self.dsem() for _ in range(2)]
            yst = [self.sb(s2, "ysst%d" % i, [128, 1024], F32) for i in range(2)]
            ysts = [self.dsem() for _ in range(2)]
            yxs = [self.sb(s2, "yxs%d" % i, [128, 1024], BF16) for i in range(2)]
            yxss = [self.dsem() for _ in range(2)]
            ss2 = self.sb(s2, "ss2", [128, 2], F32)
            rs2 = self.sb(s2, "rs2", [128, 2], F32)
            junk = self.sb(s2, "junks", [128, 512], BF16)
            pS = self.ps(s2, "pS")
            pD = [self.ps(s2, "pD%d" % i) for i in range(4)]
            pCB = self.ps(s2, "pCB")
            pY = [self.ps(s2, "pY%d" % i) for i in range(2)]
            rel = [wz, wdt, Abc, dtb, Dsk, snb, c_one, c_eps, ST[0], ST[1], STbf, dt_, dtA, acs, ea, dend, dtot, R1, E, M, CBm, xdt, xdd,
                   silz, y_sb, tmp, ss2, rs2, junk, pS, pCB] + xt + bt + yB_sb + yst + yxs + pD + pY
            d0 = self.dsem(6)
            tr.dma("pool", d0, out=wz[:], in_=w_in_v[:, :, OFF_Z:OFF_Z + 1024], writes=[wz])
            tr.dma("pool", d0, out=wdt[:], in_=w_in_v[:, :, OFF_DT:OFF_DT + 32], writes=[wdt])
            tr.dma("sp", d0, out=Abc[:], in_=I["a_log"].t.partition_broadcast(128), writes=[Abc])
            tr.dma("sp", d0, out=dtb[:], in_=I["dt_bias"].t.partition_broadcast(128), writes=[dtb])
            tr.dma("sp", d0, out=Dsk[:], in_=I["ssd_d"].t.partition_broadcast(128), writes=[Dsk])
            tr.dma("sp", d0, out=snb[:], in_=I["ssd_norm"].t.partition_broadcast(128), writes=[snb])
            tr.op("pool", lambda e: e.memset(c_one[:], 1.0), writes=[c_one])
            tr.op("pool", lambda e: e.memset(c_eps[:], EPS), writes=[c_eps])
            tr.op("act", lambda e: e.activation(out=Abc[:], in_=Abc[:], func=AF.Exp), reads=[Abc], writes=[Abc])
            tr.op("dve", lambda e: e.tensor_scalar(out=Abc[:], in0=Abc[:], scalar1=-1.0, scalar2=None, op0=ALU.mult), reads=[Abc], writes=[Abc])
            for dd in range(2):
                tr.op("pool", lambda e, dd=dd: e.memset(ST[dd][:], 0.0), writes=[ST[dd]])
            Lm = [c["tri_gt"], c["tri_lt"]]
            Tc = [c["tri_le"], c["tri_ge"]]
            cnt = [0]

            QS = [[pS, pD[0], pD[1], pD[2]], [pD[3], pCB, pY[0], pY[1]]]
            ea2 = [ea, self.sb(s2, "ea_b", [128, 16], F32)]
            dtot2 = [dtot, self.sb(s2, "dtot_b", [128, 16], F32)]
            xdd2 = [xdd, self.sb(s2, "xdd_b", [128, 1024], BF16)]
            silz2 = [silz, self.sb(s2, "silz_b", [128, 1024], F32)]
            ysb2 = [y_sb, self.sb(s2, "y_sb_b", [128, 1024], F32)]
            tmp2 = [tmp, self.sb(s2, "ytmp_b", [128, 1024], F32)]
            ss22 = [ss2, self.sb(s2, "ss2_b", [128, 2], F32)]
            rs22 = [rs2, self.sb(s2, "rs2_b", [128, 2], F32)]
            junk2 = [junk, self.sb(s2, "junks_b", [128, 512], BF16)]
            dt2 = [dt_, self.sb(s2, "dt_b", [128, 16], F32)]
            dtA2 = [dtA, self.sb(s2, "dtA_b", [128, 16], F32)]
            acs2 = [acs, self.sb(s2, "acs_b", [128, 16], F32)]
            dend2 = [dend, self.sb(s2, "dend_b", [128, 16], F32)]
            xdt2 = [xdt, self.sb(s2, "xdt_b", [128, 1024], BF16)]
            Lmb = [self.sb(s2, "Lmb%d" % i, [128, 128], BF16) for i in range(2)]
            for i in range(2):
                tr.op("pool", lambda e, i=i: e.tensor_copy(out=Lmb[i][:], in_=Lm[i][:]), reads=[Lm[i]], writes=[Lmb[i]])
            rel += [dt2[1], dtA2[1], acs2[1], dend2[1], xdt2[1]] + Lmb
            rel += [ea2[1], dtot2[1], xdd2[1], silz2[1], ysb2[1], tmp2[1], ss22[1], rs22[1], junk2[1]]

            def ssd_tile(t, dd, full, sweepA, own_idx, seq):
                STd = ST[dd]
                k = seq % 2
                Q = QS[k]
                ea_, dtot_, xdd_, silz_ = ea2[k], dtot2[k], xdd2[k], silz2[k]
                y_sb, tmp, ss2, rs2, junk = ysb2[k], tmp2[k], ss22[k], rs22[k], junk2[k]
                dt_, dtA, acs, dend, xdt = dt2[k], dtA2[k], acs2[k], dend2[k], xdt2[k]
                x_t, b_t = xt[k], bt[k]
                tr.regroup(xts[k], 2)
                tr.dma("sp", xts[k], out=x_t[:], in_=xtok_v[t], writes=[x_t])
                tr.dma("sp", xts[k], out=b_t[:], in_=btok_v[t], writes=[b_t])
                hres = [c["hT_r"][t]]
                lhs = lambda kc: c["hT"][:, kc, t * 128:(t + 1) * 128]
                if full and sweepA:
                    for hh in range(2):
                        for kc in range(8):
                            tr.op("pe", lambda e, kc=kc, hh=hh: e.matmul(out=Q[1 + hh][:], lhsT=lhs(kc), rhs=wz[:, kc, hh * 512:(hh + 1) * 512], start=(kc == 0), stop=(kc == 7)),
                                  reads=hres + [wz], writes=[Q[1 + hh]])
                        tr.op("act", lambda e, hh=hh: e.activation(out=silz_[:, hh * 512:(hh + 1) * 512], in_=Q[1 + hh][:], func=AF.Silu), reads=[Q[1 + hh]], writes=[silz_])
                for kc in range(8):
                    tr.op("pe", lambda e, kc=kc: e.matmul(out=Q[0][:, 0:16], lhsT=lhs(kc), rhs=wdt[:, kc, dd * 16:(dd + 1) * 16], start=(kc == 0), stop=(kc == 7)),
                          reads=hres + [wdt], writes=[Q[0]])
                tr.op("dve", lambda e: e.tensor_tensor(out=dt_[:], in0=Q[0][:, 0:16], in1=dtb[:, dd * 16:(dd + 1) * 16], op=ALU.add), reads=[Q[0], dtb], writes=[dt_])
                tr.op("act", lambda e: e.activation(out=dt_[:], in_=dt_[:], func=AF.Exp), reads=[dt_], writes=[dt_])
                tr.op("act", lambda e: e.activation(out=dt_[:], in_=dt_[:], func=AF.Ln, bias=c_one[:]), reads=[dt_, c_one], writes=[dt_])
                tr.op("dve", lambda e: e.tensor_tensor(out=dtA[:], in0=dt_[:], in1=Abc[:, dd * 16:(dd + 1) * 16], op=ALU.mult), reads=[dt_, Abc], writes=[dtA])
                tr.op("pe", lambda e: e.matmul(out=Q[0][:, 16:32], lhsT=Tc[dd][:], rhs=dtA[:], start=True, stop=True), reads=[Tc[dd], dtA], writes=[Q[0]])
                tr.op("pe", lambda e: e.matmul(out=Q[0][:, 32:48], lhsT=c["ones_f"][:], rhs=dtA[:], start=True, stop=True), reads=[c["ones_f"], dtA], writes=[Q[0]])
                tr.op("dve", lambda e: e.tensor_copy(out=acs[:], in_=Q[0][:, 16:32]), reads=[Q[0]], writes=[acs])
                tr.op("dve", lambda e: e.tensor_tensor(out=dend[:], in0=Q[0][:, 32:48], in1=acs[:], op=ALU.subtract), reads=[Q[0], acs], writes=[dend])
                tr.op("act", lambda e: e.activation(out=dend[:], in_=dend[:], func=AF.Exp), reads=[dend], writes=[dend])
                tr.op("act", lambda e: e.activation(out=dtot_[:], in_=Q[0][:, 32:48], func=AF.Exp), reads=[Q[0]], writes=[dtot_])
                tr.op("dve", lambda e: e.tensor_tensor(out=xdt[:].rearrange("p (h q) -> p h q", h=16), in0=x_t[:].rearrange("p (h q) -> p h q", h=16),
                                                      in1=dt_[:].unsqueeze(2).to_broadcast([128, 16, 64]), op=ALU.mult), reads=[x_t, dt_], writes=[xdt])
                tr.op("pool", lambda e: e.tensor_tensor(out=xdd_[:].rearrange("p (h q) -> p h q", h=16), in0=xdt[:].rearrange("p (h q) -> p h q", h=16),
                                                       in1=dend[:].unsqueeze(2).to_broadcast([128, 16, 64]), op=ALU.mult), reads=[xdt, dend], writes=[xdd_])
                if full:
                    tok0 = (t - T_OWN0) * 128
                    Dbank = [Q[1], Q[2], Q[3], Q[1]]
                    tr.op("act", lambda e: e.activation(out=ea_[:], in_=acs[:], func=AF.Exp), reads=[acs], writes=[ea_])
                    tr.mark("need_r1")
                    tr.op("dve", lambda e: e.tensor_tensor(out=R1[:], in0=Tc[dd][:].unsqueeze(1).to_broadcast([128, 16, 128]),
                                                          in1=dtA[:].unsqueeze(2).to_broadcast([128, 16, 128]), op=ALU.mult), reads=[Tc[dd], dtA], writes=[R1])
                    for b4 in range(4):
                        tr.op("pe", lambda e, b4=b4: e.matmul(out=Dbank[b4][:], lhsT=Lmb[dd][:], rhs=R1[:, 4 * b4:4 * b4 + 4, :], start=True, stop=True),
                              reads=[Lmb[dd], R1], writes=[Dbank[b4]])
                        tr.op("act", lambda e, b4=b4: e.activation(out=E[:, 4 * b4:4 * b4 + 4, :], in_=Dbank[b4][:].rearrange("p (h i) -> p h i", h=4), func=AF.Exp), reads=[Dbank[b4]], writes=[E])
                    for g in range(2):
                        tr.op("pe", lambda e, g=g: e.matmul(out=Q[2][:, g * 128:(g + 1) * 128], lhsT=BT[:, g, tok0:tok0 + 128], rhs=CT[:, g, tok0:tok0 + 128], start=True, stop=True),
                              reads=[BT, CT], writes=[Q[2]])
                    tr.op("dve", lambda e: e.tensor_tensor(out=CBm[:], in0=Q[2][:, 0:256].rearrange("p (g i) -> p g i", g=2),
                                                          in1=Tc[dd][:].unsqueeze(1).to_broadcast([128, 2, 128]), op=ALU.mult), reads=[Q[2], Tc[dd]], writes=[CBm])
                    for g in range(2):
                        eng = "dve" if g == 0 else "pool"
                        tr.op(eng, lambda e, g=g: e.tensor_tensor(out=M[:, g * 8:(g + 1) * 8, :], in0=E[:, g * 8:(g + 1) * 8, :],
                                                                  in1=CBm[:, g:g + 1, :].to_broadcast([128, 8, 128]), op=ALU.mult), reads=[E, CBm], writes=[M])
                    Yb = [Q[3], Q[1]]
                    for h in range(16):
                        py = Yb[h // 8]
                        cs = (h % 8) * 64
                        tr.op("pe", lambda e, h=h, py=py, cs=cs: e.matmul(out=py[:, cs:cs + 64], lhsT=M[:, h, :], rhs=xdt[:, h * 64:(h + 1) * 64], start=True, stop=True),
                              reads=[M, xdt], writes=[py])
                    tr.mark("r1_done")
                tr.mark("need_state")
                Ob = [Q[2], Q[0]]
                if full:
                    tr.op("act", lambda e: e.activation(out=STbf[:].rearrange("p a b -> p (a b)"), in_=STd[:].rearrange("p a b -> p (a b)"), func=AF.Copy), reads=[STd], writes=[STbf])
                    for g in range(2):
                        tr.op("pe", lambda e, g=g: e.matmul(out=Ob[g][:], lhsT=CT[:, g, tok0:tok0 + 128], rhs=STbf[:, g, :], start=True, stop=True),
                              reads=[CT, STbf], writes=[Ob[g]])
                        tr.op("dve", lambda e, g=g: e.tensor_tensor(out=tmp[:, g * 512:(g + 1) * 512].rearrange("p (h q) -> p h q", h=8), in0=Ob[g][:].rearrange("p (h q) -> p h q", h=8),
                                                                    in1=ea_[:, g * 8:(g + 1) * 8].unsqueeze(2).to_broadcast([128, 8, 64]), op=ALU.mult), reads=[Ob[g], ea_], writes=[tmp])
                        tr.op("dve", lambda e, g=g: e.tensor_tensor(out=y_sb[:, g * 512:(g + 1) * 512], in0=Yb[g][:], in1=tmp[:, g * 512:(g + 1) * 512], op=ALU.add),
                              reads=[Yb[g], tmp], writes=[y_sb])
                for g in range(2):
                    tr.op("pe", lambda e, g=g: e.matmul(out=Ob[g][:], lhsT=b_t[:, g * 128:(g + 1) * 128], rhs=xdd_[:, g * 512:(g + 1) * 512], start=True, stop=True),
                          reads=[b_t, xdd_], writes=[Ob[g]])
                    tr.op("dve", lambda e, g=g: e.tensor_tensor(out=STd[:, g, :].rearrange("p (h q) -> p h q", h=8), in0=STd[:, g, :].rearrange("p (h q) -> p h q", h=8),
                                                                in1=dtot_[:, g * 8:(g + 1) * 8].unsqueeze(2).to_broadcast([128, 8, 64]), op=ALU.mult), reads=[STd, dtot_], writes=[STd])
                    tr.op("dve", lambda e, g=g: e.tensor_tensor(out=STd[:, g, :], in0=Ob[g][:], in1=STd[:, g, :], op=ALU.add), reads=[Ob[g], STd], writes=[STd])
                tr.mark("state_done")
                if not full:
                    return
                if not sweepA:
                    ys = yst[own_idx % 2]
                    tr.op("act", lambda e: e.activation(out=ys[:], in_=y_sb[:], func=AF.Copy), reads=[y_sb], writes=[ys])
                    tr.dma("sp", ysts[own_idx % 2], out=yB_v[own_idx], in_=ys[:], reads=[ys], writes=[])
                    return
                yb = yB_sb[own_idx % 2]
                tr.dma("sp", yBs[own_idx % 2], out=yb[:], in_=yB_v[own_idx], writes=[yb])
                tr.op("dve", lambda e: e.tensor_tensor(out=y_sb[:], in0=y_sb[:], in1=yb[:], op=ALU.add), reads=[y_sb, yb], writes=[y_sb])
                tr.op("pool", lambda e: e.tensor_tensor(out=tmp[:].rearrange("p (h q) -> p h q", h=16), in0=x_t[:].rearrange("p (h q) -> p h q", h=16),
                                                       in1=Dsk[:].unsqueeze(2).to_broadcast([128, 16, 64]), op=ALU.mult), reads=[x_t, Dsk], writes=[tmp])
                tr.op("dve", lambda e: e.tensor_tensor(out=y_sb[:], in0=y_sb[:], in1=tmp[:], op=ALU.add), reads=[y_sb, tmp], writes=[y_sb])
                tr.op("dve", lambda e: e.tensor_tensor(out=y_sb[:], in0=y_sb[:], in1=silz_[:], op=ALU.mult), reads=[y_sb, silz_], writes=[y_sb])
                for g in range(2):
                    tr.op("act", lambda e, g=g: e.activation(out=junk[:], in_=y_sb[:, g * 512:(g + 1) * 512], func=AF.Square, accum_out=ss2[:, g:g + 1]), reads=[y_sb], writes=[junk, ss2])
                tr.op("act", lambda e: e.activation(out=rs2[:], in_=ss2[:], func=AF.Ln, scale=1.0 / 512.0, bias=c_eps[:]), reads=[ss2, c_eps], writes=[rs2])
                tr.op("act", lambda e: e.activation(out=rs2[:], in_=rs2[:], func=AF.Exp, scale=-0.5), reads=[rs2], writes=[rs2])
                tr.op("dve", lambda e: e.tensor_tensor(out=y_sb[:].rearrange("p (g q) -> p g q", g=2), in0=y_sb[:].rearrange("p (g q) -> p g q", g=2),
                                                      in1=rs2[:].unsqueeze(2).to_broadcast([128, 2, 512]), op=ALU.mult), reads=[y_sb, rs2], writes=[y_sb])
                yo = yxs[own_idx % 2]
                tr.op("pool", lambda e: e.tensor_tensor(out=yo[:], in0=y_sb[:], in1=snb[:], op=ALU.mult), reads=[y_sb, snb], writes=[yo])
                tr.dma("sp", yxss[own_idx % 2], out=yx_v[own_idx][:, 1024:2048], in_=yo[:], reads=[yo], writes=[])

            def sweep(tiles):
                recs = []
                for seq, (t, dd, full, sweepA, own_idx) in enumerate(tiles):
                    tr.begin_record()
                    ssd_tile(t, dd, full, sweepA, own_idx, seq)
                    recs.append(tr.end_record())
                tr.run_pipelined(recs, depth=2)

            sweep([(t, 1, False, False, None) for t in (1, 0)])
            self.tap("sS_B", ST[1][:].rearrange("p a b -> p (a b)"), [128, 1024], F32, [ST[1]])
            sweep([(t, 1, False, False, None) for t in range(NT - 1, T_OTH0 - 1, -1)] +
                  [(t, 1, True, False, t - T_OWN0) for t in range(T_OTH0 - 1, T_OWN0 - 1, -1)])
            for e in Tracker.ENG:
                tr.wait_all(e, yst)
            sweep([(t, 0, False, True, None) for t in (0, 1)])
            self.tap("sS_A", ST[0][:].rearrange("p a b -> p (a b)"), [128, 1024], F32, [ST[0]])
            sweep([(t, 0, True, True, t - T_OWN0) for t in range(T_OWN0, T_OTH0)])
            for e in Tracker.ENG:
                tr.wait_all(e, yxs)
            self.barrier_release(rel)

    def stage_post(self, st):
        tr, c, I = self.tr, self.c, self.I
        self.fence()
        c["h_lat"] = self.sb(st, "h_lat", [128, 16, D], F32)
        c["h_r"] = [Res("h_lat%d" % i) for i in range(16)]
        c["h2T"] = self.sb(st, "h2T", [128, 8, NOWN], BF16)
        c["h2_r"] = [Res("h2T%d" % i) for i in range(16)]
        c["comb"] = self.sb(st, "comb", [128, 16, 32], F32)
        yx_v = c["yx"].t.rearrange("(n p) c -> n p c", p=128)
        h_lat, h2T = c["h_lat"], c["h2T"]
        with ExitStack() as s2:
            wo = self.sb(s2, "wo", [128, 16, D], BF16)
            wr = self.sb(s2, "wr", [128, 8, 36], F32)
            brr = self.sb(s2, "brr", [1, 36], F32)
            c_eps = self.sb(s2, "c_eps3", [128, 1], F32)
            lg = self.sb(s2, "lg", [128, 16, 36], F32)
            yxt = [self.sb(s2, "yxt%d" % i, [128, 2048], BF16) for i in range(2)]
            yxs = [self.dsem() for _ in range(2)]
            xr = [self.sb(s2, "xr2_%d" % i, [128, D], F32) for i in range(2)]
            xrs = [self.dsem() for _ in range(2)]
            junk = self.sb(s2, "pjunk", [128, D], BF16)
            PS = []
            for par in range(2):
                PS.append({"yxT": self.sb(s2, "yxT%d" % par, [128, 16, 128], BF16), "tmp": self.sb(s2, "ptmp%d" % par, [128, D], F32),
                           "h2f": self.sb(s2, "h2f%d" % par, [128, 8, 128], F32), "ss": self.sb(s2, "pss%d" % par, [128, 1], F32),
                           "rs": self.sb(s2, "prs%d" % par, [128, 1], F32), "B": [self.ps(s2, "ppB%d_%d" % (par, i)) for i in range(4)]})
            rel = [wo, wr, brr, c_eps, lg, junk] + yxt + xr
            for p_ in PS:
                rel += [p_["yxT"], p_["tmp"], p_["h2f"], p_["ss"], p_["rs"]] + p_["B"]
            w_out_v = I["w_out"].t.rearrange("(kc p) n -> p kc n", p=128)
            d0 = self.dsem(2)
            wst = [self.sb(s2, "wst%d" % i, [128, 1, D], F32) for i in range(2)]
            rel += wst
            wsts = [self.dsem() for _ in range(2)]
            for q in range(16):
                tr.dma("sp", wsts[q % 2], out=wst[q % 2][:], in_=w_out_v[:, q:q + 1, :], writes=[wst[q % 2]])
                tr.op("pool", lambda e, q=q: e.tensor_copy(out=wo[:, q:q + 1, :], in_=wst[q % 2][:]), reads=[wst[q % 2]], writes=[wo])
            tr.dma("sp", d0, out=wr[:].rearrange("p a b -> p (a b)"), in_=I["w_router"].t, writes=[wr])
            tr.dma("sp", d0, out=brr[:], in_=I["b_router"].t, writes=[brr])
            tr.op("pool", lambda e: e.memset(c_eps[:], EPS), writes=[c_eps])
            def post_tile(i):
                y_t, x_t = yxt[i % 2], xr[i % 2]
                p_ = PS[i % 2]
                yxT, tmp, h2f, ss, rs, B = p_["yxT"], p_["tmp"], p_["h2f"], p_["ss"], p_["rs"], p_["B"]
                hn = tmp
                pT = [B[0][:].bitcast(BF16), B[1][:].bitcast(BF16)]
                tr.dma("sp", yxs[i % 2], out=y_t[:], in_=yx_v[i], writes=[y_t])
                tr.dma("sp", xrs[i % 2], out=x_t[:], in_=I["xs"].t[NCTX + i * 128: NCTX + (i + 1) * 128, :], writes=[x_t])
                for kc in range(16):
                    tr.op("pe", lambda e, kc=kc: e.transpose(out=pT[kc // 8][:, (kc % 8) * 128:(kc % 8 + 1) * 128], in_=y_t[:, kc * 128:(kc + 1) * 128], identity=c["ident_b"][:]),
                          reads=[y_t, c["ident_b"]], writes=[B[kc // 8]])
                tr.op("act", lambda e: e.activation(out=yxT[:, 0:8, :].rearrange("p a b -> p (a b)"), in_=pT[0], func=AF.Copy), reads=[B[0]], writes=[yxT])
                tr.op("dve", lambda e: e.tensor_copy(out=yxT[:, 8:16, :].rearrange("p a b -> p (a b)"), in_=pT[1]), reads=[B[1]], writes=[yxT])
                hl = h_lat[:, i, :]
                for hh in range(2):
                    for kc in range(16):
                        tr.op("pe", lambda e, kc=kc, hh=hh: e.matmul(out=B[2 + hh][:], lhsT=yxT[:, kc, :], rhs=wo[:, kc, hh * 512:(hh + 1) * 512], start=(kc == 0), stop=(kc == 15)),
                              reads=[yxT, wo], writes=[B[2 + hh]])
                    tr.op("dve", lambda e, hh=hh: e.tensor_tensor(out=tmp[:, hh * 512:(hh + 1) * 512], in0=B[2 + hh][:], in1=c["g1_bc"][:, hh * 512:(hh + 1) * 512], op=ALU.mult),
                          reads=[B[2 + hh], c["g1_bc"]], writes=[tmp])
                tr.op("pool", lambda e: e.tensor_tensor(out=hl, in0=tmp[:], in1=x_t[:], op=ALU.add), reads=[tmp, x_t], writes=[c["h_r"][i]])
                tr.op("act", lambda e: e.activation(out=junk[:], in_=hl, func=AF.Square, accum_out=ss[:]), reads=[c["h_r"][i]], writes=[junk, ss])
                tr.op("act", lambda e: e.activation(out=rs[:], in_=ss[:], func=AF.Ln, scale=1.0 / D, bias=c_eps[:]), reads=[ss, c_eps], writes=[rs])
                tr.op("act", lambda e: e.activation(out=rs[:], in_=rs[:], func=AF.Exp, scale=-0.5), reads=[rs], writes=[rs])
                tr.op("dve", lambda e: e.tensor_scalar(out=hn[:], in0=hl, scalar1=rs[:], scalar2=None, op0=ALU.mult), reads=[c["h_r"][i], rs], writes=[hn])
                for kc in range(8):
                    tr.op("pe", lambda e, kc=kc: e.transpose(out=B[kc // 4][:, (kc % 4) * 128:(kc % 4 + 1) * 128], in_=hn[:, kc * 128:(kc + 1) * 128], identity=c["ident_f"][:]),
                          reads=[hn, c["ident_f"]], writes=[B[kc // 4]])
                for q in range(2):
                    tr.op("dve", lambda e, q=q: e.tensor_tensor(out=h2f[:, q * 4:(q + 1) * 4, :], in0=B[q][:].rearrange("p (k t) -> p k t", k=4),
                                                               in1=c["s2"][:, q * 4:(q + 1) * 4].unsqueeze(2).to_broadcast([128, 4, 128]), op=ALU.mult), reads=[B[q], c["s2"]], writes=[h2f])
                tr.op("pool", lambda e: e.tensor_tensor(out=h2f[:], in0=h2f[:], in1=c["b2"][:].unsqueeze(2).to_broadcast([128, 8, 128]), op=ALU.add), reads=[h2f, c["b2"]], writes=[h2f])
                tr.op("act", lambda e: e.activation(out=h2T[:, :, i * 128:(i + 1) * 128], in_=h2f[:], func=AF.Copy), reads=[h2f], writes=[c["h2_r"][i]])
                for kc in range(8):
                    tr.op("pe", lambda e, kc=kc: e.matmul(out=B[2][:, 0:36], lhsT=h2f[:, kc, :], rhs=wr[:, kc, :], start=(kc == 0), stop=False), reads=[h2f, wr], writes=[B[2]])
                tr.op("pe", lambda e: e.matmul(out=B[2][:, 0:36], lhsT=c["ones_f"][0:1, :], rhs=brr[0:1, :], start=False, stop=True), reads=[c["ones_f"], brr], writes=[B[2]])
                tr.op("dve", lambda e: e.tensor_copy(out=lg[:, i, :], in_=B[2][:, 0:36]), reads=[B[2]], writes=[lg])

            recs = []
            for i in range(16):
                tr.begin_record()
                post_tile(i)
                recs.append(tr.end_record())
            tr.run_pipelined(recs, depth=2)
            self.tap("lg", lg[:].rearrange("p a b -> p (a b)"), [128, 16 * 36], F32, [lg])
            self.tap("h_lat", h_lat[:].rearrange("p a b -> p (a b)"), [128, 16 * D], F32, c["h_r"])
            def T(name, shape):
                t_ = self.sb(s2, name, shape, F32)
                rel.append(t_)
                return t_
            gmax = T("gmax", [128, 16]); mg = T("mg", [128, 16, 4]); eg = T("eg", [128, 16, 4]); gsum = T("gsum", [128, 16]); pg = T("pg", [128, 16])
            t48 = T("t48", [128, 16, 4, 8]); ein = T("ein", [128, 16, 8]); m1 = T("m1", [128, 16]); k1 = T("k1", [128, 16, 8]); e2 = T("e2", [128, 16, 8])
            m2 = T("m2", [128, 16]); k2 = T("k2", [128, 16, 8]); dd_ = T("dd_", [128, 16]); w1 = T("w1", [128, 16]); w2 = T("w2", [128, 16]); cw8 = T("cw8", [128, 16, 8])
            gl = lg[:, :, 0:4]
            el = lg[:, :, 4:36].rearrange("p t (g x) -> p t g x", g=4)
            V = lambda fn, r, w: tr.op("dve", fn, reads=r, writes=w)
            V(lambda e: e.tensor_reduce(out=gmax[:], in_=gl, axis=AX.X, op=ALU.max), [lg], [gmax])
            V(lambda e: e.tensor_tensor(out=mg[:], in0=gl, in1=gmax[:].unsqueeze(2).to_broadcast([128, 16, 4]), op=ALU.is_equal), [lg, gmax], [mg])
            V(lambda e: e.tensor_tensor(out=eg[:], in0=gl, in1=gmax[:].unsqueeze(2).to_broadcast([128, 16, 4]), op=ALU.subtract), [lg, gmax], [eg])
            tr.op("act", lambda e: e.activation(out=eg[:], in_=eg[:], func=AF.Exp), reads=[eg], writes=[eg])
            V(lambda e: e.tensor_reduce(out=gsum[:], in_=eg[:], axis=AX.X, op=ALU.add), [eg], [gsum])
            V(lambda e: e.reciprocal(out=pg[:], in_=gsum[:]), [gsum], [pg])
            V(lambda e: e.tensor_tensor(out=t48[:], in0=el, in1=mg[:].unsqueeze(3).to_broadcast([128, 16, 4, 8]), op=ALU.mult), [lg, mg], [t48])
            V(lambda e: e.tensor_reduce(out=ein[:], in_=t48[:].rearrange("p t g x -> p t x g"), axis=AX.X, op=ALU.add), [t48], [ein])
            V(lambda e: e.tensor_reduce(out=m1[:], in_=ein[:], axis=AX.X, op=ALU.max), [ein], [m1])
            V(lambda e: e.tensor_tensor(out=k1[:], in0=ein[:], in1=m1[:].unsqueeze(2).to_broadcast([128, 16, 8]), op=ALU.is_equal), [ein, m1], [k1])
            V(lambda e: e.scalar_tensor_tensor(out=e2[:], in0=k1[:], scalar=-1.0e30, in1=ein[:], op0=ALU.mult, op1=ALU.add), [k1, ein], [e2])
            V(lambda e: e.tensor_reduce(out=m2[:], in_=e2[:], axis=AX.X, op=ALU.max), [e2], [m2])
            V(lambda e: e.tensor_tensor(out=k2[:], in0=e2[:], in1=m2[:].unsqueeze(2).to_broadcast([128, 16, 8]), op=ALU.is_equal), [e2, m2], [k2])
            V(lambda e: e.tensor_tensor(out=dd_[:], in0=m2[:], in1=m1[:], op=ALU.subtract), [m1, m2], [dd_])
            tr.op("act", lambda e: e.activation(out=dd_[:], in_=dd_[:], func=AF.Exp), reads=[dd_], writes=[dd_])
            V(lambda e: e.tensor_scalar(out=w1[:], in0=dd_[:], scalar1=1.0, scalar2=None, op0=ALU.add), [dd_], [w1])
            V(lambda e: e.reciprocal(out=w1[:], in_=w1[:]), [w1], [w1])
            V(lambda e: e.tensor_tensor(out=w2[:], in0=dd_[:], in1=w1[:], op=ALU.mult), [dd_, w1], [w2])
            V(lambda e: e.tensor_tensor(out=w1[:], in0=w1[:], in1=pg[:], op=ALU.mult), [w1, pg], [w1])
            V(lambda e: e.tensor_tensor(out=w2[:], in0=w2[:], in1=pg[:], op=ALU.mult), [w2, pg], [w2])
            V(lambda e: e.tensor_tensor(out=k1[:], in0=k1[:], in1=w1[:].unsqueeze(2).to_broadcast([128, 16, 8]), op=ALU.mult), [k1, w1], [k1])
            V(lambda e: e.tensor_tensor(out=k2[:], in0=k2[:], in1=w2[:].unsqueeze(2).to_broadcast([128, 16, 8]), op=ALU.mult), [k2, w2], [k2])
            V(lambda e: e.tensor_tensor(out=cw8[:], in0=k1[:], in1=k2[:], op=ALU.add), [k1, k2], [cw8])
            V(lambda e: e.tensor_tensor(out=c["comb"][:].rearrange("p t (g x) -> p t g x", g=4), in0=mg[:].unsqueeze(3).to_broadcast([128, 16, 4, 8]),
                                        in1=cw8[:].unsqueeze(2).to_broadcast([128, 16, 4, 8]), op=ALU.mult), [mg, cw8], [c["comb"]])
            self.tap("comb", c["comb"][:].rearrange("p a b -> p (a b)"), [128, 512], F32, [c["comb"]])
            self.barrier_release(rel)

    def stage_moe(self, st):
        tr, c, I = self.tr, self.c, self.I
        self.fence()
        h_lat, h2T, comb = c["h_lat"], c["h2T"], c["comb"]
        with ExitStack() as s2:
            wgt = [self.sb(s2, "mwg%d" % i, [128, 8, DFF], BF16) for i in range(2)]
            wut = [self.sb(s2, "mwu%d" % i, [128, 8, DFF], BF16) for i in range(2)]
            wdt = [self.sb(s2, "mwd%d" % i, [128, 4, D], BF16) for i in range(2)]
            stg = [self.sb(s2, "mstg%d" % i, [128, 8, DFF], F32) for i in range(2)]
            stgs = [self.dsem() for _ in range(2)]
            ns = [0]
            sg_ = [self.sb(s2, "msg%d" % i, [128, 512], F32) for i in range(2)]
            heT = [self.sb(s2, "heT%d" % i, [128, 4, 512], BF16) for i in range(2)]
            pG = [self.ps(s2, "mpG%d" % i) for i in range(2)]
            pU = [self.ps(s2, "mpU%d" % i) for i in range(2)]
            pDn = [self.ps(s2, "mpD%d" % i) for i in range(4)]
            rel = wgt + wut + wdt + sg_ + heT + pG + pU + pDn + stg
            nb = 0
            for ex in range(NEXP):
                k = ex % 2
                wg_e, wu_e, wd_e = wgt[k], wut[k], wdt[k]
                for (dst, src) in ((wg_e, I["w_gate"].t[ex].rearrange("(kc p) n -> p kc n", p=128)), (wu_e, I["w_up"].t[ex].rearrange("(kc p) n -> p kc n", p=128))):
                    sg_t, sg_s = stg[ns[0] % 2], stgs[ns[0] % 2]
                    ns[0] += 1
                    tr.dma("sp", sg_s, out=sg_t[:], in_=src, writes=[sg_t])
                    tr.op("pool", lambda e, dst=dst, sg_t=sg_t: e.tensor_copy(out=dst[:], in_=sg_t[:]), reads=[sg_t], writes=[dst])
                sg_t, sg_s = stg[ns[0] % 2], stgs[ns[0] % 2]
                ns[0] += 1
                sv = sg_t[:].rearrange("p a b -> p (a b)").rearrange("p (f n) -> p f n", f=4)
                tr.dma("sp", sg_s, out=sv, in_=I["w_down"].t[ex].rearrange("(fc p) n -> p fc n", p=128), writes=[sg_t])
                tr.op("pool", lambda e, wd_e=wd_e, sv=sv: e.tensor_tensor(out=wd_e[:], in0=sv, in1=c["g2_bc"][:].unsqueeze(1).to_broadcast([128, 4, D]), op=ALU.mult),
                      reads=[sg_t, c["g2_bc"]], writes=[wd_e])
                for j in range(4):
                    he = heT[nb % 2]
                    nb += 1
                    hres = [c["h2_r"][j * 4 + q] for q in range(4)]
                    for fc in range(4):
                        g_p, u_p, sg = pG[fc % 2], pU[fc % 2], sg_[fc % 2]
                        for kc in range(8):
                            tr.op("pe", lambda e, kc=kc, fc=fc, g_p=g_p: e.matmul(out=g_p[:], lhsT=wg_e[:, kc, fc * 128:(fc + 1) * 128], rhs=h2T[:, kc, j * 512:(j + 1) * 512],
                                                                               start=(kc == 0), stop=(kc == 7)), reads=[wg_e] + hres, writes=[g_p])
                        for kc in range(8):
                            tr.op("pe", lambda e, kc=kc, fc=fc, u_p=u_p: e.matmul(out=u_p[:], lhsT=wu_e[:, kc, fc * 128:(fc + 1) * 128], rhs=h2T[:, kc, j * 512:(j + 1) * 512],
                                                                               start=(kc == 0), stop=(kc == 7)), reads=[wu_e] + hres, writes=[u_p])
                        tr.op("act", lambda e, g_p=g_p, sg=sg: e.activation(out=sg[:], in_=g_p[:], func=AF.Silu), reads=[g_p], writes=[sg])
                        tr.op("dve", lambda e, u_p=u_p, sg=sg, fc=fc, he=he: e.tensor_tensor(out=he[:, fc, :], in0=u_p[:], in1=sg[:], op=ALU.mult), reads=[u_p, sg], writes=[he])
                    for tt in range(4):
                        ti = j * 4 + tt
                        for hh in range(2):
                            d_p = pDn[(tt * 2 + hh) % 4]
                            for fc in range(4):
                                tr.op("pe", lambda e, fc=fc, d_p=d_p, tt=tt, hh=hh, he=he: e.matmul(out=d_p[:], lhsT=he[:, fc, tt * 128:(tt + 1) * 128], rhs=wd_e[:, fc, hh * 512:(hh + 1) * 512],
                                                                                              start=(fc == 0), stop=(fc == 3)), reads=[he, wd_e], writes=[d_p])
                            tr.op("dve", lambda e, d_p=d_p, ti=ti, hh=hh, ex=ex: e.scalar_tensor_tensor(
                                out=h_lat[:, ti, hh * 512:(hh + 1) * 512], in0=d_p[:], scalar=comb[:, ti, ex:ex + 1], in1=h_lat[:, ti, hh * 512:(hh + 1) * 512], op0=ALU.mult, op1=ALU.add),
                                reads=[d_p, comb, c["h_r"][ti]], writes=[c["h_r"][ti]])
            self.tap("h_fin", h_lat[:].rearrange("p a b -> p (a b)"), [128, 16 * D], F32, c["h_r"])
            self.barrier_release(rel)

    def stage_final(self, st):
        tr, c, I = self.tr, self.c, self.I
        self.fence()
        h_lat = c["h_lat"]
        out_v = self.out.t.rearrange("(n p) c -> n p c", p=128)
        with ExitStack() as s2:
            fn = self.sb(s2, "fn_bc", [128, D], F32)
            c_eps = self.sb(s2, "c_eps4", [128, 1], F32)
            junk = self.sb(s2, "fjunk", [128, D], BF16)
            ss = [self.sb(s2, "fss%d" % i, [128, 1], F32) for i in range(2)]
            rs = [self.sb(s2, "frs%d" % i, [128, 1], F32) for i in range(2)]
            ob = [self.sb(s2, "fob%d" % i, [128, D], F32) for i in range(2)]
            obs = [self.dsem() for _ in range(2)]
            tr.dma("sp", self.dsem(), out=fn[:], in_=I["final_norm"].t.partition_broadcast(128), writes=[fn])
            tr.op("pool", lambda e: e.memset(c_eps[:], EPS), writes=[c_eps])
            for i in range(16):
                hl = h_lat[:, i, :]
                s_, r_, o_ = ss[i % 2], rs[i % 2], ob[i % 2]
                tr.op("act", lambda e, s_=s_, hl=hl: e.activation(out=junk[:], in_=hl, func=AF.Square, accum_out=s_[:]), reads=[c["h_r"][i]], writes=[junk, s_])
                tr.op("act", lambda e, s_=s_, r_=r_: e.activation(out=r_[:], in_=s_[:], func=AF.Ln, scale=1.0 / D, bias=c_eps[:]), reads=[s_, c_eps], writes=[r_])
                tr.op("act", lambda e, r_=r_: e.activation(out=r_[:], in_=r_[:], func=AF.Exp, scale=-0.5), reads=[r_], writes=[r_])
                tr.op("dve", lambda e, r_=r_, o_=o_, hl=hl: e.scalar_tensor_tensor(out=o_[:], in0=hl, scalar=r_[:], in1=fn[:], op0=ALU.mult, op1=ALU.mult),
                      reads=[c["h_r"][i], r_, fn], writes=[o_])
                tr.dma("sp", obs[i % 2], out=out_v[i], in_=o_[:], reads=[o_], writes=[])
            self.final += ob


def prep_core(inp, b, hf):
    L = 0
    rev = hf == 1
    x, ctx = inp["x"][b], inp["ctx"][b]
    if not rev:
        ctx_a, own, oth = ctx, x[0:2048], x[2048:4096]
        dA, dB = 0, 1
    else:
        ctx_a, own, oth = ctx[::-1], x[2048:4096][::-1], x[0:2048][::-1]
        dA, dB = 1, 0
    m = {}
    m["xs"] = np.ascontiguousarray(np.concatenate([ctx_a, own, oth], axis=0), dtype=np.float32)
    cT = np.stack([inp["c"][b].reshape(8, 128).T, inp["c_ctx"].reshape(8, 128).T], axis=2).reshape(128, 16)
    m["cT"] = np.ascontiguousarray(cT, dtype=np.float32)
    m["w_ada"] = np.ascontiguousarray(inp["w_ada"][L])
    m["b_ada"] = np.ascontiguousarray(inp["b_ada"][L].reshape(1, -1))
    m["norm_mix_fm"] = np.ascontiguousarray(inp["norm_mix"][L].reshape(8, 128).T)
    m["norm_ffn_fm"] = np.ascontiguousarray(inp["norm_ffn"][L].reshape(8, 128).T)
    w_in = inp["w_in"][L]
    if rev:
        w_in = np.concatenate([w_in[:, :OFF_G], w_in[:, OFF_G + 16:OFF_G + 32], w_in[:, OFF_G:OFF_G + 16],
                               w_in[:, OFF_Z:OFF_DT], w_in[:, OFF_DT + 16:OFF_DT + 32], w_in[:, OFF_DT:OFF_DT + 16]], axis=1)
    m["w_in"] = np.ascontiguousarray(w_in)
    wu, gb = inp["gla_w_up"][L], inp["gla_b"][L]
    m["w_up_aug"] = np.ascontiguousarray(np.stack([np.concatenate([wu[dA], gb[dA][None, :]], axis=0),
                                                   np.concatenate([wu[dB], gb[dB][None, :]], axis=0)], axis=0))
    m["gla_norm"] = np.ascontiguousarray(inp["gla_norm"][L].reshape(1, -1))
    m["ssd_norm"] = np.ascontiguousarray(inp["ssd_norm"][L].reshape(1, -1))
    m["final_norm"] = np.ascontiguousarray(inp["final_norm"].reshape(1, -1))
    cw = inp["ssd_conv_w"][L]
    if rev:
        cw = cw[::-1, ::-1, :]
    m["conv_w_fm"] = np.ascontiguousarray(cw.reshape(9, 12, 128).transpose(2, 1, 0))
    m["conv_b_fm"] = np.ascontiguousarray(inp["ssd_conv_b"][L].reshape(12, 128).T)
    m["dt_bias"] = np.ascontiguousarray(np.concatenate([inp["ssd_dt_bias"][L][dA], inp["ssd_dt_bias"][L][dB]]).reshape(1, 32))
    m["a_log"] = np.ascontiguousarray(np.concatenate([inp["ssd_a_log"][L][dA], inp["ssd_a_log"][L][dB]]).reshape(1, 32))
    m["ssd_d"] = np.ascontiguousarray(inp["ssd_d"][L].reshape(1, 16))
    m["w_out"] = np.ascontiguousarray(inp["w_out"][L])
    wrt = np.concatenate([inp["router_group_w"][L], inp["router_expert_w"][L]], axis=1)
    m["w_router"] = np.ascontiguousarray(wrt.reshape(8, 128, 36).transpose(1, 0, 2).reshape(128, 8 * 36))
    m["b_router"] = np.ascontiguousarray(np.concatenate([inp["router_group_b"][L], inp["router_expert_b"][L]]).reshape(1, 36))
    m["w_gate"] = np.ascontiguousarray(inp["expert_w_gate"][L])
    m["w_up"] = np.ascontiguousarray(inp["expert_w_up"][L])
    m["w_down"] = np.ascontiguousarray(inp["expert_w_down"][L])
    return {k: np.asarray(v, dtype=np.float32) for k, v in m.items()}


def run(inputs, debug=None, stop_after=None, cores=8):
    bld = Builder(debug=debug, stop_after=stop_after)
    nc = bld.build()
    in_maps = [prep_core(inputs, i // 2, i % 2) for i in range(cores)]
    res = run_bass_kernel_spmd(nc, in_maps, core_ids=list(range(cores)))
    return res, bld


def kernel(**inputs):
    inputs = {k: np.asarray(v) for k, v in inputs.items()}
    res, _ = run(inputs)
    out = np.empty((4, 4096, D), dtype=np.float32)
    for i in range(8):
        b, hf = i // 2, i % 2
        o = np.asarray(res.results[i]["out"], dtype=np.float32)
        if hf == 0:
            out[b, 0:2048] = o
        else:
            out[b, 2048:4096] = o[::-1]
    return out
```

```python
import math
from contextlib import ExitStack

import numpy as np
import concourse.bass as bass
import concourse.mybir as mybir
from concourse.bass_utils import run_bass_kernel_spmd

F32 = mybir.dt.float32
BF16 = mybir.dt.bfloat16
AF = mybir.ActivationFunctionType
ALU = mybir.AluOpType
AX = mybir.AxisListType

D = 1024
NCTX, NOWN, NOTH = 256, 2048, 2048
TOK = NCTX + NOWN + NOTH
NT = TOK // 128
T_CTX0, T_OWN0, T_OTH0 = 0, 2, 18
EPS = 1e-6
IN_W = 5696
OFF_K, OFF_V, OFF_R, OFF_G, OFF_Z, OFF_XBC, OFF_DT = 512, 1024, 2048, 3072, 3104, 4128, 5664
NEXP, DFF = 32, 512


class Res:
    __slots__ = ("name", "lw", "rd")

    def __init__(self, name=""):
        self.name = name
        self.lw = None
        self.rd = {}


class Tile:
    def __init__(self, t, name):
        self.t = t
        self.r = Res(name)

    def __getitem__(self, idx):
        return self.t[idx]


class Tracker:
    ENG = ("pe", "act", "dve", "pool", "sp")
    CH = 2000

    def __init__(self, nc, sems, dma_sems, same_engine_sync=True):
        self.nc = nc
        self.eng = {"pe": nc.tensor, "act": nc.scalar, "dve": nc.vector, "pool": nc.gpsimd, "sp": nc.sync}
        self.cnt = {e: 0 for e in self.ENG}
        self.waited = {e: {} for e in self.ENG}
        self.sems = {e: [sems[e]] for e in sems}
        self.free_dma = list(dma_sems)
        self.same = same_engine_sync
        self.ninst = 0
        self.rec = None

    def new_dma_sem(self, group=0):
        d = self.free_dma.pop()
        self._uid = getattr(self, "_uid", 0) + 1
        d = d if isinstance(d, list) else [d, 0, 0, 0, "dma%d" % self._uid]
        if group:
            d[2] = group
            d[3] = d[1] + 16 * group
        return d

    def regroup(self, d, n):
        if self.rec is not None:
            self.rec.append(("call", lambda: self.regroup(d, n)))
            return
        assert d[2] == 0
        d[2] = n
        d[3] = d[1] + 16 * n

    def begin_record(self):
        self.rec = []

    def end_record(self):
        r, self.rec = self.rec, None
        return r

    def mark(self, name):
        self.rec.append(("mark", name))

    def _emit_item(self, it):
        if it[0] == "op":
            self.op(*it[1:])
        elif it[0] == "dma":
            self.dma(*it[1:])
        elif it[0] == "call":
            it[1]()

    def run_pipelined(self, records, depth=2, serial_fronts=False):
        assert self.rec is None
        active = []
        nxt = 0
        done = {}
        fin = -1

        def released(name, idx):
            return max(done.get(name, -1), fin) >= idx - 1 or idx == 0

        while active or nxt < len(records):
            while len(active) < depth and nxt < len(records):
                if serial_fronts and active and not active[-1][3]:
                    break
                if active and min(x[2] for x in active) <= nxt - depth:
                    break
                active.append([records[nxt], 0, nxt, False])
                nxt += 1
            progressed = False
            for a in list(active):
                lst, pos, idx, _ = a
                if pos >= len(lst):
                    a[3] = True
                    active.remove(a)
                    fin = max(fin, idx) if all(x[2] > idx for x in active) else fin
                    for nm in list(done.keys()):
                        done[nm] = max(done[nm], idx) if done[nm] >= idx - 1 else done[nm]
                    progressed = True
                    continue
                it = lst[pos]
                if it[0] == "mark":
                    nm = it[1]
                    if nm.startswith("need_"):
                        sec = nm[5:]
                        if sec == "state":
                            a[3] = True
                        if not released(sec, idx):
                            continue
                    elif nm.endswith("_done"):
                        sec = nm[:-5]
                        done[sec] = max(done.get(sec, -1), idx)
                    a[1] += 1
                    progressed = True
                    continue
                self._emit_item(it)
                a[1] += 1
                progressed = True
            if not progressed:
                for a in active:
                    it = a[0][a[1]]
                    if it[0] == "mark" and it[1].startswith("need_"):
                        done[it[1][5:]] = max(done.get(it[1][5:], -1), a[2] - 1)
                        progressed = True
                assert progressed

    def release_dma_sem(self, d):
        self.free_dma.append(d)

    def _wait(self, e, ev):
        if ev is None:
            return
        if ev[0] == "dma":
            _, s, v, key = ev
            if self.waited[e].get(key, 0) >= v:
                return
            self.waited[e][key] = v
            self.eng[e].wait_ge(s, v)
        else:
            pe, n = ev
            if pe == e and (not self.same or e in ("pe", "sp")):
                return
            if self.waited[e].get(pe, 0) >= n:
                return
            self.waited[e][pe] = n
            self.eng[e].wait_ge(self.sems[pe][(n - 1) // self.CH], (n - 1) % self.CH + 1)

    def _deps(self, e, reads, writes):
        for r in reads:
            self._wait(e, r.lw)
        for w in writes:
            self._wait(e, w.lw)
            for ev in w.rd.values():
                self._wait(e, ev)

    @staticmethod
    def _note_read(r, ev):
        key = ev[3] if ev[0] == "dma" else ev[0]
        old = r.rd.get(key)
        if old is None or (old[2] if old[0] == "dma" else old[1]) < (ev[2] if ev[0] == "dma" else ev[1]):
            r.rd[key] = ev

    def op(self, e, fn, reads=(), writes=()):
        if self.rec is not None:
            self.rec.append(("op", e, fn, list(reads), list(writes)))
            return None
        reads = [x.r if isinstance(x, Tile) else x for x in reads]
        writes = [x.r if isinstance(x, Tile) else x for x in writes]
        self._deps(e, reads, writes)
        self.cnt[e] += 1
        ev = (e, self.cnt[e])
        k = (self.cnt[e] - 1) // self.CH
        if k >= len(self.sems[e]):
            self.sems[e].append(self.free_dma.pop(0))
        fn(self.eng[e]).then_inc(self.sems[e][k], 1)
        self.ninst += 1
        for r in reads:
            self._note_read(r, ev)
        for w in writes:
            w.lw = ev
            w.rd = {}
        return ev

    def dma(self, e, dsem, out, in_, reads=(), writes=()):
        if self.rec is not None:
            self.rec.append(("dma", e, dsem, out, in_, list(reads), list(writes)))
            return None
        reads = [x.r if isinstance(x, Tile) else x for x in reads]
        writes = [x.r if isinstance(x, Tile) else x for x in writes]
        self._deps(e, reads, writes)
        if dsem[2] == 0:
            dsem[2] = 1
            dsem[3] = dsem[1] + 16
        dsem[1] += 16
        dsem[2] -= 1
        ev = ("dma", dsem[0], dsem[3], dsem[4])
        self.eng[e].dma_start(out=out, in_=in_).then_inc(dsem[0], 16)
        self.ninst += 1
        for r in reads:
            self._note_read(r, ev)
        for w in writes:
            w.lw = ev
            w.rd = {}
        return ev

    def wait_all(self, e, resources):
        for r in resources:
            r = r.r if isinstance(r, Tile) else r
            self._wait(e, r.lw)
            for ev in r.rd.values():
                self._wait(e, ev)


class Builder:
    def __init__(self, debug=None, stop_after=None):
        self.debug = debug or ()
        self.stop_after = stop_after
        self.nc = bass.Bass("TRN2", target_bir_lowering=False)
        self.dbg_out = {}

    def sb(self, st, name, shape, dt):
        self._uid = getattr(self, "_uid", 0) + 1
        return Tile(st.enter_context(self.nc.sbuf_tensor("sb%d_%s" % (self._uid, name), list(shape), dt)), name)

    def ps(self, st, name, shape=(128, 512), dt=F32):
        self._uid = getattr(self, "_uid", 0) + 1
        return Tile(st.enter_context(self.nc.psum_tensor("ps%d_%s" % (self._uid, name), list(shape), dt)), name)

    def dram_in(self, name, shape, dt=F32):
        return Tile(self.nc.dram_tensor(name, list(shape), dt, kind="ExternalInput").ap(), name)

    def dram_out(self, name, shape, dt=F32):
        return Tile(self.nc.dram_tensor(name, list(shape), dt, kind="ExternalOutput").ap(), name)

    def dram_scr(self, name, shape, dt):
        return Tile(self.nc.dram_tensor(name, list(shape), dt, kind="Internal").ap(), name)

    def dsem(self, group=0):
        return self.tr.new_dma_sem(group)

    def build(self):
        nc = self.nc
        I = {}
        I["xs"] = self.dram_in("xs", [TOK, D])
        I["cT"] = self.dram_in("cT", [128, 16])
        I["w_ada"] = self.dram_in("w_ada", [D, 6 * D])
        I["b_ada"] = self.dram_in("b_ada", [1, 6 * D])
        I["norm_mix_fm"] = self.dram_in("norm_mix_fm", [128, 8])
        I["norm_ffn_fm"] = self.dram_in("norm_ffn_fm", [128, 8])
        I["w_in"] = self.dram_in("w_in", [D, IN_W])
        I["w_up_aug"] = self.dram_in("w_up_aug", [2, 17, 512])
        I["gla_norm"] = self.dram_in("gla_norm", [1, 256])
        I["ssd_norm"] = self.dram_in("ssd_norm", [1, 1024])
        I["final_norm"] = self.dram_in("final_norm", [1, 1024])
        I["conv_w_fm"] = self.dram_in("conv_w_fm", [128, 12, 9])
        I["conv_b_fm"] = self.dram_in("conv_b_fm", [128, 12])
        I["dt_bias"] = self.dram_in("dt_bias", [1, 32])
        I["a_log"] = self.dram_in("a_log", [1, 32])
        I["ssd_d"] = self.dram_in("ssd_d", [1, 16])
        I["w_out"] = self.dram_in("w_out", [2048, D])
        I["w_router"] = self.dram_in("w_router", [128, 8 * 36])
        I["b_router"] = self.dram_in("b_router", [1, 36])
        I["w_gate"] = self.dram_in("w_gate", [NEXP, D, DFF])
        I["w_up"] = self.dram_in("w_up", [NEXP, D, DFF])
        I["w_down"] = self.dram_in("w_down", [NEXP, DFF, D])
        self.I = I
        self.out = self.dram_out("out", [NOWN, D])

        with ExitStack() as st:
            sems = {e: st.enter_context(nc.semaphore("s_" + e)) for e in Tracker.ENG}
            dsems = [st.enter_context(nc.semaphore("d%d" % i)) for i in range(90)]
            self.tr = Tracker(nc, sems, dsems)
            self.program(st)
        return nc

    def tap(self, name, tile_ap, shape, dt, reads):
        if name not in self.debug:
            return
        o = self.dram_out("dbg_" + name, shape, dt)
        self.dbg_out[name] = o
        n = shape[1]
        step = 2048
        d = self.dsem(len(range(0, n, step)))
        for c0 in range(0, n, step):
            c1 = min(n, c0 + step)
            self.tr.dma("sp", d, out=o.t[:, c0:c1], in_=tile_ap[:, c0:c1], reads=reads, writes=[o])
        self.final.append(o)

    def program(self, st):
        tr = self.tr
        self.final = []
        self.consts(st)
        self.stage_adaln(st)
        with ExitStack() as mst:
            self.stage_hT(mst)
            if self.stop_after == "hT":
                return self.finish()
            self.stage_gla(mst)
            if self.stop_after == "gla":
                return self.finish()
            self.stage_conv(mst)
            if self.stop_after == "conv":
                return self.finish()
            self.stage_ssd(mst)
            if self.stop_after == "ssd":
                return self.finish()
            self.barrier_release([self.c["hT"], self.c["BT"], self.c["CT"]] + self.c["hT_r"])
        self.stage_post(st)
        if self.stop_after == "post":
            return self.finish()
        self.stage_moe(st)
        if self.stop_after == "moe":
            return self.finish()
        self.stage_final(st)
        return self.finish()

    def finish(self):
        self.tr.wait_all("sp", self.final)

    def consts(self, st):
        tr = self.tr
        c = {}
        self.c = c
        c["ident_f"] = self.sb(st, "ident_f", [128, 128], F32)
        c["ident_b"] = self.sb(st, "ident_b", [128, 128], BF16)
        c["ones_f"] = self.sb(st, "ones_f", [128, 128], F32)
        for nm in ("tri_le", "tri_ge", "tri_gt", "tri_lt"):
            c[nm] = self.sb(st, nm, [128, 128], F32)
        idf = c["ident_f"]
        tr.op("pool", lambda e: e.memset(idf[:], 0.0), writes=[idf])
        tr.op("pool", lambda e: e.affine_select(out=idf[:], in_=idf[:], pattern=[[-1, 128]], compare_op=ALU.not_equal,
                                               fill=1.0, base=0, channel_multiplier=1), reads=[idf], writes=[idf])
        tr.op("pool", lambda e: e.tensor_copy(out=c["ident_b"][:], in_=idf[:]), reads=[idf], writes=[c["ident_b"]])
        tr.op("pool", lambda e: e.memset(c["ones_f"][:], 1.0), writes=[c["ones_f"]])
        specs = {"tri_le": (ALU.is_gt, 0), "tri_ge": (ALU.is_gt, 0), "tri_gt": (ALU.is_gt, 0), "tri_lt": (ALU.is_gt, 0)}
        t = c["tri_le"]
        tr.op("pool", lambda e: e.memset(t[:], 1.0), writes=[t])
        tr.op("pool", lambda e: e.affine_select(out=t[:], in_=t[:], pattern=[[1, 128]], compare_op=ALU.is_ge,
                                               fill=0.0, base=0, channel_multiplier=-1), reads=[t], writes=[t])
        t2 = c["tri_ge"]
        tr.op("pool", lambda e: e.memset(t2[:], 1.0), writes=[t2])
        tr.op("pool", lambda e: e.affine_select(out=t2[:], in_=t2[:], pattern=[[-1, 128]], compare_op=ALU.is_ge,
                                               fill=0.0, base=0, channel_multiplier=1), reads=[t2], writes=[t2])
        t3 = c["tri_gt"]
        tr.op("pool", lambda e: e.memset(t3[:], 1.0), writes=[t3])
        tr.op("pool", lambda e: e.affine_select(out=t3[:], in_=t3[:], pattern=[[-1, 128]], compare_op=ALU.is_gt,
                                               fill=0.0, base=0, channel_multiplier=1), reads=[t3], writes=[t3])
        t4 = c["tri_lt"]
        tr.op("pool", lambda e: e.memset(t4[:], 1.0), writes=[t4])
        tr.op("pool", lambda e: e.affine_select(out=t4[:], in_=t4[:], pattern=[[1, 128]], compare_op=ALU.is_gt,
                                               fill=0.0, base=0, channel_multiplier=-1), reads=[t4], writes=[t4])
        self.tap("tri_le", c["tri_le"][:], [128, 128], F32, [c["tri_le"]])
        self.tap("tri_gt", c["tri_gt"][:], [128, 128], F32, [c["tri_gt"]])

    def stage_adaln(self, st):
        tr, c, I = self.tr, self.c, self.I
        c["mod_fm"] = self.sb(st, "mod_fm", [128, 6, 8, 2], F32)
        c["g1_bc"] = self.sb(st, "g1_bc", [128, D], F32)
        c["g2_bc"] = self.sb(st, "g2_bc", [128, D], F32)
        c["s1"] = self.sb(st, "s1", [128, 8], F32)
        c["s1c"] = self.sb(st, "s1c", [128, 8], F32)
        c["b1"] = self.sb(st, "b1", [128, 8], F32)
        c["b1c"] = self.sb(st, "b1c", [128, 8], F32)
        c["s2"] = self.sb(st, "s2", [128, 8], F32)
        c["b2"] = self.sb(st, "b2", [128, 8], F32)
        with ExitStack() as s2:
            cT = self.sb(s2, "cT", [128, 16], F32)
            scT = self.sb(s2, "scT", [128, 16], F32)
            sc_rep = self.sb(s2, "sc_rep", [128, 8, 128], F32)
            brow = self.sb(s2, "brow", [1, 6 * D], F32)
            nm = self.sb(s2, "nm", [128, 8], F32)
            nf = self.sb(s2, "nf", [128, 8], F32)
            wblk = [self.sb(s2, "wblk%d" % i, [128, 8, D], F32) for i in range(2)]
            wsem = [self.dsem() for _ in range(2)]
            modps = self.ps(s2, "modps", [128, 512], F32)
            gps = [self.ps(s2, "gps%d" % i, [128, 512], F32) for i in range(2)]
            d = self.dsem(4)
            tr.dma("sp", d, out=cT[:], in_=I["cT"].t, writes=[cT])
            tr.dma("sp", d, out=brow[:], in_=I["b_ada"].t, writes=[brow])
            tr.dma("sp", d, out=nm[:], in_=I["norm_mix_fm"].t, writes=[nm])
            tr.dma("sp", d, out=nf[:], in_=I["norm_ffn_fm"].t, writes=[nf])
            tr.op("act", lambda e: e.activation(out=scT[:], in_=cT[:], func=AF.Silu), reads=[cT], writes=[scT])
            tr.op("dve", lambda e: e.tensor_copy(out=sc_rep[:], in_=scT[:].rearrange("p (k j) -> p k j", j=2)[:, :, 0:1].to_broadcast([128, 8, 128])),
                  reads=[scT], writes=[sc_rep])
            w_ada = I["w_ada"].t.rearrange("(kc p) n -> p kc n", p=128)
            mview = modps[:, 0:96].rearrange("p (b f t) -> p b f t", b=6, f=8)
            for blk in range(6):
                wb = wblk[blk % 2]
                tr.dma("sp", wsem[blk % 2], out=wb[:], in_=w_ada[:, :, blk * D:(blk + 1) * D], writes=[wb])
                if blk in (0, 1, 3, 4):
                    for fc in range(8):
                        for kc in range(8):
                            tr.op("pe", lambda e, fc=fc, kc=kc, wb=wb, blk=blk: e.matmul(
                                out=mview[:, blk, fc, :], lhsT=wb[:, kc, fc * 128:(fc + 1) * 128],
                                rhs=scT[:, 2 * kc:2 * kc + 2], start=(kc == 0), stop=False),
                                reads=[wb, scT], writes=[modps])
                        tr.op("pe", lambda e, fc=fc, blk=blk: e.matmul(
                            out=mview[:, blk, fc, :], lhsT=brow[0:1, blk * D + fc * 128: blk * D + (fc + 1) * 128],
                            rhs=c["ones_f"][0:1, 0:2], start=False, stop=True),
                            reads=[brow, c["ones_f"]], writes=[modps])
                else:
                    gdst = c["g1_bc"] if blk == 2 else c["g2_bc"]
                    for hh in range(2):
                        for kc in range(8):
                            tr.op("pe", lambda e, hh=hh, kc=kc, wb=wb: e.matmul(
                                out=gps[hh][:], lhsT=sc_rep[:, kc, :], rhs=wb[:, kc, hh * 512:(hh + 1) * 512],
                                start=(kc == 0), stop=False), reads=[wb, sc_rep], writes=[gps[hh]])
                        tr.op("pe", lambda e, hh=hh, blk=blk: e.matmul(
                            out=gps[hh][:], lhsT=c["ones_f"][0:1, :], rhs=brow[0:1, blk * D + hh * 512: blk * D + (hh + 1) * 512],
                            start=False, stop=True), reads=[brow, c["ones_f"]], writes=[gps[hh]])
                        tr.op("act", lambda e, hh=hh, gdst=gdst: e.activation(out=gdst[:, hh * 512:(hh + 1) * 512], in_=gps[hh][:], func=AF.Copy),
                              reads=[gps[hh]], writes=[gdst])
            mf = c["mod_fm"]
            mflat = mf[:].rearrange("p b f t -> p (b f t)")
            tr.op("dve", lambda e: e.tensor_copy(out=mflat[:, 0:32], in_=modps[:, 0:32]), reads=[modps], writes=[mf])
            tr.op("dve", lambda e: e.tensor_copy(out=mflat[:, 48:80], in_=modps[:, 48:80]), reads=[modps], writes=[mf])
            tr.op("dve", lambda e: e.scalar_tensor_tensor(out=c["s1"][:], in0=mf[:, 1, :, 0], scalar=1.0, in1=nm[:], op0=ALU.add, op1=ALU.mult),
                  reads=[mf, nm], writes=[c["s1"]])
            tr.op("dve", lambda e: e.scalar_tensor_tensor(out=c["s1c"][:], in0=mf[:, 1, :, 1], scalar=1.0, in1=nm[:], op0=ALU.add, op1=ALU.mult),
                  reads=[mf, nm], writes=[c["s1c"]])
            tr.op("dve", lambda e: e.scalar_tensor_tensor(out=c["s2"][:], in0=mf[:, 4, :, 0], scalar=1.0, in1=nf[:], op0=ALU.add, op1=ALU.mult),
                  reads=[mf, nf], writes=[c["s2"]])
            tr.op("dve", lambda e: e.tensor_copy(out=c["b1"][:], in_=mf[:, 0, :, 0]), reads=[mf], writes=[c["b1"]])
            tr.op("dve", lambda e: e.tensor_copy(out=c["b1c"][:], in_=mf[:, 0, :, 1]), reads=[mf], writes=[c["b1c"]])
            tr.op("dve", lambda e: e.tensor_copy(out=c["b2"][:], in_=mf[:, 3, :, 0]), reads=[mf], writes=[c["b2"]])
            self.tap("mod_fm", mf[:].rearrange("p b f t -> p (b f t)"), [128, 96], F32, [mf])
            self.tap("g1_bc", c["g1_bc"][:], [128, D], F32, [c["g1_bc"]])
            self.barrier_release([cT, scT, sc_rep, brow, nm, nf, wblk[0], wblk[1], modps, gps[0], gps[1]])

    def barrier_release(self, tiles):
        self.pending = getattr(self, "pending", [])
        for t in tiles:
            self.pending.append(t.r if isinstance(t, Tile) else t)

    def fence(self):
        pend = getattr(self, "pending", [])
        for e in Tracker.ENG:
            self.tr.wait_all(e, pend)
        self.pending = []

    def stage_hT(self, st):
        tr, c, I = self.tr, self.c, self.I
        self.fence()
        c["hT"] = self.sb(st, "hT", [128, 8, TOK], BF16)
        c["hT_r"] = [Res("hT%d" % t) for t in range(NT)]
        with ExitStack() as s2:
            xr = [self.sb(s2, "xr%d" % i, [128, D], F32) for i in range(3)]
            xsem = [self.dsem() for _ in range(3)]
            junk = self.sb(s2, "junk", [128, D], BF16)
            ss = [self.sb(s2, "ss%d" % i, [128, 1], F32) for i in range(3)]
            rstd = [self.sb(s2, "rstd%d" % i, [128, 1], F32) for i in range(3)]
            xn = [self.sb(s2, "xn%d" % i, [128, D], BF16) for i in range(3)]
            tmp = [self.sb(s2, "tmp%d" % i, [128, 8, 128], F32) for i in range(3)]
            tps = [self.ps(s2, "tps%d" % i, [128, 1024], BF16) for i in range(3)]
            epst = self.sb(s2, "epst", [128, 1], F32)
            tr.op("pool", lambda e: e.memset(epst[:], EPS), writes=[epst])
            rel = xr + ss + rstd + xn + tmp + tps + [junk, epst]
            recs = []
            for t in range(NT):
                tr.begin_record()
                x_t, ss_t, rs_t, xn_t, tmp_t, ps_t = xr[t % 3], ss[t % 3], rstd[t % 3], xn[t % 3], tmp[t % 3], tps[t % 3]
                hT_ap = c["hT"][:, :, t * 128:(t + 1) * 128]
                hT_r = c["hT_r"][t]
                isctx = t < T_OWN0
                sc, sh = (c["s1c"], c["b1c"]) if isctx else (c["s1"], c["b1"])
                tr.dma("sp", xsem[t % 3], out=x_t[:], in_=I["xs"].t[t * 128:(t + 1) * 128, :], writes=[x_t])
                tr.op("act", lambda e, x_t=x_t, ss_t=ss_t: e.activation(out=junk[:], in_=x_t[:], func=AF.Square, accum_out=ss_t[:]),
                      reads=[x_t], writes=[junk, ss_t])
                tr.op("act", lambda e, ss_t=ss_t, rs_t=rs_t: e.activation(out=rs_t[:], in_=ss_t[:], func=AF.Ln, scale=1.0 / D, bias=epst[:]),
                      reads=[ss_t, epst], writes=[rs_t])
                tr.op("act", lambda e, rs_t=rs_t: e.activation(out=rs_t[:], in_=rs_t[:], func=AF.Exp, scale=-0.5),
                      reads=[rs_t], writes=[rs_t])
                tr.op("dve", lambda e, x_t=x_t, rs_t=rs_t, xn_t=xn_t: e.tensor_scalar(out=xn_t[:], in0=x_t[:], scalar1=rs_t[:], scalar2=None, op0=ALU.mult),
                      reads=[x_t, rs_t], writes=[xn_t])
                for kc in range(8):
                    tr.op("pe", lambda e, kc=kc, xn_t=xn_t, ps_t=ps_t: e.transpose(out=ps_t[:, kc * 128:(kc + 1) * 128], in_=xn_t[:, kc * 128:(kc + 1) * 128], identity=c["ident_b"][:]),
                          reads=[xn_t, c["ident_b"]], writes=[ps_t])
                tr.op("dve", lambda e, ps_t=ps_t, tmp_t=tmp_t, sc=sc: e.tensor_tensor(
                    out=tmp_t[:], in0=ps_t[:].rearrange("p (k t) -> p k t", k=8), in1=sc[:].unsqueeze(2).to_broadcast([128, 8, 128]), op=ALU.mult),
                    reads=[ps_t, sc], writes=[tmp_t])
                tr.op("pool", lambda e, tmp_t=tmp_t, hT_ap=hT_ap, sh=sh: e.tensor_tensor(
                    out=hT_ap, in0=tmp_t[:], in1=sh[:].unsqueeze(2).to_broadcast([128, 8, 128]), op=ALU.add),
                    reads=[tmp_t, sh], writes=[hT_r])
                recs.append(tr.end_record())
            tr.run_pipelined(recs, depth=3)
            for t in (0, 2, 17, 33):
                if ("hT%d" % t) in self.debug:
                    o = self.dram_out("dbg_hT%d" % t, [128, 8, 128], BF16)
                    tr.dma("sp", self.dsem(), out=o.t, in_=c["hT"][:, :, t * 128:(t + 1) * 128], reads=[c["hT_r"][t]], writes=[o])
                    self.final.append(o)
            self.barrier_release(rel)

    def scratch(self, name, shape, dt):
        if name in self.debug:
            o = self.dram_out("dbg_" + name, shape, dt)
            self.final.append(o)
            return o
        return self.dram_scr(name, shape, dt)

    def stage_conv(self, st):
        tr, c, I = self.tr, self.c, self.I
        self.fence()
        c["x_tok"] = self.scratch("x_tok", [TOK, 1024], BF16)
        c["B_tok"] = self.scratch("B_tok", [TOK, 256], BF16)
        c["BT"] = self.sb(st, "BT", [128, 2, NOWN], BF16)
        c["CT"] = self.sb(st, "CT", [128, 2, NOWN], BF16)
        xtok_v = c["x_tok"].t.rearrange("(n p) c -> p n c", p=128)
        btok_v = c["B_tok"].t.rearrange("(n p) c -> p n c", p=128)
        w_in_v = I["w_in"].t.rearrange("(kc p) n -> p kc n", p=128)
        with ExitStack() as s2:
            wx = [self.sb(s2, "wx%d" % i, [128, 8, 128], BF16) for i in range(2)]
            wxs = [self.dsem() for _ in range(2)]
            cw = self.sb(s2, "cw", [128, 12, 9], F32)
            cb = self.sb(s2, "cb", [128, 12], F32)
            diag = [self.sb(s2, "diag%d" % i, [128, 9, 128], BF16) for i in range(2)]
            pre = [self.sb(s2, "pre%d" % i, [128, 66, 66], BF16) for i in range(2)]
            prec = [self.sb(s2, "prec%d" % i, [128, 258], BF16) for i in range(2)]
            post = [self.sb(s2, "post%d" % i, [128, 512], BF16) for i in range(3)]
            tst = [self.sb(s2, "tst%d" % i, [128, 4, 128], BF16) for i in range(3)]
            tsem = [self.dsem() for _ in range(3)]
            pp = [self.ps(s2, "pp%d" % i) for i in range(2)]
            pc = [self.ps(s2, "pc%d" % i) for i in range(2)]
            pt = [self.ps(s2, "pt%d" % i, [128, 1024], BF16) for i in range(2)]
            rel = wx + diag + pre + prec + post + tst + pp + pc + pt + [cw, cb]
            d0 = self.dsem(2)
            tr.dma("sp", d0, out=cw[:], in_=I["conv_w_fm"].t, writes=[cw])
            tr.dma("sp", d0, out=cb[:], in_=I["conv_b_fm"].t, writes=[cb])
            for i in range(2):
                tr.op("pool", lambda e, i=i: e.memset(pre[i][:], 0.0), writes=[pre[i]])
                tr.op("pool", lambda e, i=i: e.memset(prec[i][:], 0.0), writes=[prec[i]])
            nev = 0
            npost = 0
            for ct in range(12):
                w, dg, pr, prc = wx[ct % 2], diag[ct % 2], pre[ct % 2], prec[ct % 2]
                tr.dma("pool", wxs[ct % 2], out=w[:], in_=w_in_v[:, :, OFF_XBC + ct * 128: OFF_XBC + (ct + 1) * 128], writes=[w])
                tr.op("pool", lambda e, dg=dg, ct=ct: e.tensor_tensor(out=dg[:], in0=c["ident_f"][:].unsqueeze(1).to_broadcast([128, 9, 128]),
                                                                  in1=cw[:, ct, :].unsqueeze(2).to_broadcast([128, 9, 128]), op=ALU.mult),
                      reads=[c["ident_f"], cw], writes=[dg])
                for blk in range(9):
                    p_t = pp[nev % 2]
                    if blk == 0:
                        n, tok0, trs = 256, 0, [0, 1]
                    else:
                        n, tok0 = 512, NCTX + (blk - 1) * 512
                        trs = list(range(T_OWN0 + (blk - 1) * 4, T_OWN0 + blk * 4))
                    for kc in range(8):
                        tr.op("pe", lambda e, kc=kc, p_t=p_t, w=w, n=n, tok0=tok0: e.matmul(
                            out=p_t[:, 0:n], lhsT=w[:, kc, :], rhs=c["hT"][:, kc, tok0:tok0 + n], start=(kc == 0), stop=(kc == 7)),
                            reads=[w] + [c["hT_r"][t] for t in trs], writes=[p_t])
                    if blk == 0:
                        dst = prc[:, 1:257]
                        src = p_t[:, 0:256]
                        wr = prc
                    else:
                        r0 = (blk - 1) * 8
                        dst = pr[:, r0 + 1:r0 + 9, 1:65]
                        src = p_t[:, 0:512].rearrange("p (r q) -> p r q", q=64)
                        wr = pr
                    eng = "act" if nev % 2 == 0 else "dve"
                    if eng == "act":
                        tr.op("act", lambda e, dst=dst, src=src: e.activation(out=dst, in_=src, func=AF.Copy), reads=[p_t], writes=[wr])
                    else:
                        tr.op("dve", lambda e, dst=dst, src=src: e.tensor_copy(out=dst, in_=src), reads=[p_t], writes=[wr])
                    nev += 1
                for blk in range(9):
                    if ct >= 10 and (blk == 0 or blk >= 5):
                        continue
                    c_t = pc[blk % 2]
                    if blk == 0:
                        n = 256
                        for kw in range(3):
                            tr.op("pe", lambda e, kw=kw, c_t=c_t, dg=dg, prc=prc: e.matmul(
                                out=c_t[:, 0:256], lhsT=dg[:, 3 + kw, :], rhs=prc[:, kw:kw + 256], start=(kw == 0), stop=(kw == 2)),
                                reads=[dg, prc], writes=[c_t])
                    else:
                        n = 512
                        r0 = (blk - 1) * 8
                        for tap in range(9):
                            kh, kw = tap // 3, tap % 3
                            tr.op("pe", lambda e, tap=tap, kh=kh, kw=kw, c_t=c_t, dg=dg, pr=pr, r0=r0: e.matmul(
                                out=c_t[:, 0:512], lhsT=dg[:, tap, :], rhs=pr[:, r0 + kh:r0 + kh + 8, kw:kw + 64], start=(tap == 0), stop=(tap == 8)),
                                reads=[dg, pr], writes=[c_t])
                    own_blk = 1 <= blk <= 4
                    if ct >= 8 and own_blk:
                        g = (ct - 8) % 2
                        dstT = (c["BT"] if ct < 10 else c["CT"])
                        o0 = (blk - 1) * 512
                        tr.op("act", lambda e, dstT=dstT, g=g, o0=o0, c_t=c_t, ct=ct: e.activation(
                            out=dstT[:, g, o0:o0 + 512], in_=c_t[:, 0:512], func=AF.Silu, bias=cb[:, ct:ct + 1]),
                            reads=[c_t, cb], writes=[dstT])
                        if ct >= 10:
                            continue
                        src_post, src_r = dstT[:, g, o0:o0 + 512], dstT
                    else:
                        po = post[npost % 3]
                        tr.op("act", lambda e, po=po, c_t=c_t, ct=ct, n=n: e.activation(
                            out=po[:, 0:n], in_=c_t[:, 0:n], func=AF.Silu, bias=cb[:, ct:ct + 1]),
                            reads=[c_t, cb], writes=[po])
                        src_post, src_r = po[:, 0:n], po
                    ntl = n // 128
                    t_t = pt[npost % 2]
                    ts_t = tst[npost % 3]
                    for i in range(ntl):
                        tr.op("pe", lambda e, i=i, t_t=t_t, src_post=src_post: e.transpose(
                            out=t_t[:, i * 128:(i + 1) * 128], in_=src_post[:, i * 128:(i + 1) * 128], identity=c["ident_b"][:]),
                            reads=[src_r, c["ident_b"]], writes=[t_t])
                    tr.op("dve", lambda e, t_t=t_t, ts_t=ts_t, ntl=ntl: e.tensor_copy(
                        out=ts_t[:, 0:ntl, :], in_=t_t[:, 0:ntl * 128].rearrange("p (a b) -> p a b", b=128)),
                        reads=[t_t], writes=[ts_t])
                    tile0 = 0 if blk == 0 else T_OWN0 + (blk - 1) * 4
                    if ct < 8:
                        dst_d, dst_r = xtok_v[:, tile0:tile0 + ntl, ct * 128:(ct + 1) * 128], c["x_tok"]
                    else:
                        dst_d, dst_r = btok_v[:, tile0:tile0 + ntl, (ct - 8) * 128:(ct - 7) * 128], c["B_tok"]
                    tr.dma("sp", tsem[npost % 3], out=dst_d, in_=ts_t[:, 0:ntl, :], reads=[ts_t], writes=[])
                    c.setdefault("scr_ev", []).append(ts_t)
                    npost += 1
            self.conv_store_tiles = tst
            self.tap("BT", c["BT"][:].rearrange("p g t -> p (g t)"), [128, 2 * NOWN], BF16, [c["BT"]])
            self.tap("CT", c["CT"][:].rearrange("p g t -> p (g t)"), [128, 2 * NOWN], BF16, [c["CT"]])
            for e in Tracker.ENG:
                tr.wait_all(e, tst)
            self.barrier_release(rel)

    def stage_gla(self, st):
        tr, c, I = self.tr, self.c, self.I
        self.fence()
        c["oB"] = self.scratch("oB", [NOWN, 1024], F32)
        c["yx"] = self.scratch("yx", [NOWN, 2048], BF16)
        oB_v = c["oB"].t.rearrange("(n p) c -> n p c", p=128)
        yx_v = c["yx"].t.rearrange("(n p) c -> n p c", p=128)
        w_in_v = I["w_in"].t.rearrange("(kc p) n -> p kc n", p=128)
        LNQ = math.log(128.0 ** -0.5)
        with ExitStack() as s2:
            wg = self.sb(s2, "wgla", [128, 8, 3072], BF16)
            wgs = [Res("wgla%d" % i) for i in range(6)]
            wgg = self.sb(s2, "wgg", [128, 8, 32], BF16)
            wup = self.sb(s2, "wup", [17, 2, 512], F32)
            gn = self.sb(s2, "gn_bc", [128, 256], F32)
            c_one = self.sb(s2, "c_one", [128, 1], F32)
            c_lnq = self.sb(s2, "c_lnq", [128, 1], F32)
            c_eps = self.sb(s2, "c_eps", [128, 1], F32)
            negcol = self.sb(s2, "negcol", [128, 2], F32)
            Tm = [self.sb(s2, "TmA", [128, 128], F32), self.sb(s2, "TmB", [128, 128], F32)]
            S = [self.sb(s2, "S_A", [128, 4, 256], F32), self.sb(s2, "S_B", [128, 4, 256], F32)]
            Sbf = self.sb(s2, "Sbf", [128, 4, 256], BF16)
            FS = []
            for par in range(2):
                f = {}
                f["g_aug"] = self.sb(s2, "g_aug%d" % par, [32, 128], F32)
                f["v_bf"] = self.sb(s2, "v_bf%d" % par, [128, 1024], BF16)
                f["lap"] = self.sb(s2, "lap%d" % par, [128, 512], F32)
                f["Einv"] = self.sb(s2, "Einv%d" % par, [128, 512], F32)
                f["Eq"] = self.sb(s2, "Eq%d" % par, [128, 512], F32)
                f["kt_"] = self.sb(s2, "kt_%d" % par, [128, 512], BF16)
                f["qt_"] = self.sb(s2, "qt_%d" % par, [128, 512], BF16)
                f["kqT"] = self.sb(s2, "kqT%d" % par, [128, 8, 128], BF16)
                f["PT"] = self.sb(s2, "PT%d" % par, [128, 4, 128], BF16)
                f["dcol"] = self.sb(s2, "dcol%d" % par, [128, 4], F32)
                f["P"] = [self.ps(s2, "gP%d_%d" % (par, i)) for i in range(4)]
                FS.append(f)
            silr = self.sb(s2, "silr", [128, 1024], F32)
            o_sb = self.sb(s2, "o_sb", [128, 1024], F32)
            oB_sb = [self.sb(s2, "oB_sb%d" % i, [128, 1024], F32) for i in range(2)]
            oBs = [self.dsem() for _ in range(2)]
            ost = [self.sb(s2, "ost%d" % i, [128, 1024], F32) for i in range(2)]
            osts = [self.dsem() for _ in range(2)]
            yst = [self.sb(s2, "yst%d" % i, [128, 1024], BF16) for i in range(2)]
            ysts = [self.dsem() for _ in range(2)]
            ss4 = self.sb(s2, "ss4", [128, 4], F32)
            rs4 = self.sb(s2, "rs4", [128, 4], F32)
            junk = self.sb(s2, "junkg", [128, 256], BF16)
            rel = [wg, wgg, wup, gn, c_one, c_lnq, c_eps, negcol, Tm[0], Tm[1], S[0], S[1], Sbf, silr, o_sb, ss4, rs4, junk] + oB_sb + ost + yst + wgs
            for f in FS:
                rel += [f[k] for k in ("g_aug", "v_bf", "lap", "Einv", "Eq", "kt_", "qt_", "kqT", "PT", "dcol")] + f["P"]
            d0 = self.dsem(9)
            for i in range(6):
                tr.dma("pool", d0, out=wg[:, :, i * 512:(i + 1) * 512], in_=w_in_v[:, :, i * 512:(i + 1) * 512], writes=[wgs[i]])
            tr.dma("pool", d0, out=wgg[:], in_=w_in_v[:, :, OFF_G:OFF_G + 32], writes=[wgg])
            tr.dma("sp", d0, out=wup[:], in_=I["w_up_aug"].t.rearrange("d k n -> k d n"), writes=[wup])
            tr.dma("sp", d0, out=gn[:], in_=I["gla_norm"].t.partition_broadcast(128), writes=[gn])
            tr.op("pool", lambda e: e.memset(c_one[:], 1.0), writes=[c_one])
            tr.op("pool", lambda e: e.memset(c_lnq[:], LNQ), writes=[c_lnq])
            tr.op("pool", lambda e: e.memset(c_eps[:], EPS), writes=[c_eps])
            tr.op("pool", lambda e: e.memset(negcol[:], -1.0 / 16.0), writes=[negcol])
            tr.op("pool", lambda e: e.tensor_scalar(out=Tm[0][:], in0=c["tri_le"][:], scalar1=-1.0 / 16.0, scalar2=None, op0=ALU.mult), reads=[c["tri_le"]], writes=[Tm[0]])
            tr.op("pool", lambda e: e.tensor_scalar(out=Tm[1][:], in0=c["tri_ge"][:], scalar1=-1.0 / 16.0, scalar2=None, op0=ALU.mult), reads=[c["tri_ge"]], writes=[Tm[1]])
            for f in FS:
                tr.op("pool", lambda e, f=f: e.memset(f["g_aug"][:], 1.0), writes=[f["g_aug"]])
            for dd in range(2):
                tr.op("pool", lambda e, dd=dd: e.memset(S[dd][:], 0.0), writes=[S[dd]])
            masks = [c["tri_le"], c["tri_ge"]]

            def gla_tile(t, dd, full, sweepA, own_idx, seq):
                f = FS[seq % 2]
                P = f["P"]
                g_aug, v_bf, lap, Einv, Eq, kt_, qt_, kqT, PT, dcol = (f[k] for k in ("g_aug", "v_bf", "lap", "Einv", "Eq", "kt_", "qt_", "kqT", "PT", "dcol"))
                Sd = S[dd]
                hres = [c["hT_r"][t]]
                lhs = lambda kc: c["hT"][:, kc, t * 128:(t + 1) * 128]

                def mm_tok(ps_t, c0, n, wres):
                    for kc in range(8):
                        tr.op("pe", lambda e, kc=kc: e.matmul(out=ps_t[:, 0:n], lhsT=lhs(kc), rhs=wg[:, kc, c0:c0 + n], start=(kc == 0), stop=(kc == 7)),
                              reads=hres + wres, writes=[ps_t])
                for kc in range(8):
                    tr.op("pe", lambda e, kc=kc: e.matmul(out=P[3][0:16, 0:128], lhsT=wgg[:, kc, dd * 16:(dd + 1) * 16], rhs=lhs(kc), start=(kc == 0), stop=(kc == 7)),
                          reads=hres + [wgg], writes=[P[3]])
                tr.op("act", lambda e: e.activation(out=g_aug[0:16, :], in_=P[3][0:16, 0:128], func=AF.Copy), reads=[P[3]], writes=[g_aug])
                mm_tok(P[0], 512, 512, [wgs[1]])
                mm_tok(P[1], 1024, 512, [wgs[2]])
                mm_tok(P[2], 1536, 512, [wgs[3]])
                tr.op("pe", lambda e: e.matmul(out=P[3][:, 0:512], lhsT=g_aug[0:17, :], rhs=wup[:, dd, :], start=True, stop=True), reads=[g_aug, wup], writes=[P[3]])
                tr.op("act", lambda e: e.activation(out=lap[:], in_=P[3][:, 0:512], func=AF.Exp, scale=-1.0), reads=[P[3]], writes=[lap])
                tr.op("act", lambda e: e.activation(out=lap[:], in_=lap[:], func=AF.Ln, bias=c_one[:]), reads=[lap, c_one], writes=[lap])
                tr.op("act", lambda e: e.activation(out=v_bf[:, 0:512], in_=P[1][:], func=AF.Copy), reads=[P[1]], writes=[v_bf])
                tr.op("dve", lambda e: e.tensor_copy(out=v_bf[:, 512:1024], in_=P[2][:]), reads=[P[2]], writes=[v_bf])
                tr.op("pe", lambda e: e.matmul(out=P[3][:, 0:512], lhsT=Tm[dd][:], rhs=lap[:], start=True, stop=True), reads=[Tm[dd], lap], writes=[P[3]])
                if full:
                    mm_tok(P[1], 0, 512, [wgs[0]])
                tr.op("act", lambda e: e.activation(out=Einv[:], in_=P[3][:, 0:512], func=AF.Exp, scale=-1.0), reads=[P[3]], writes=[Einv])
                if full:
                    tr.op("act", lambda e: e.activation(out=Eq[:], in_=P[3][:, 0:512], func=AF.Exp, bias=c_lnq[:]), reads=[P[3], c_lnq], writes=[Eq])
                tr.op("dve", lambda e: e.tensor_tensor(out=kt_[:], in0=P[0][:], in1=Einv[:], op=ALU.mult), reads=[P[0], Einv], writes=[kt_])
                for h in range(4):
                    tr.op("pe", lambda e, h=h: e.matmul(out=P[3][:, 2 * h:2 * h + 2], lhsT=lap[:, h * 128:(h + 1) * 128], rhs=negcol[:], start=True, stop=True),
                          reads=[lap, negcol], writes=[P[3]])
                tr.op("act", lambda e: e.activation(out=dcol[:], in_=P[3][:, 0:8:2], func=AF.Exp), reads=[P[3]], writes=[dcol])
                if full:
                    pT = P[2][:].bitcast(BF16)
                    tr.op("dve", lambda e: e.tensor_tensor(out=qt_[:], in0=P[1][:], in1=Eq[:], op=ALU.mult), reads=[P[1], Eq], writes=[qt_])
                    for h in range(4):
                        tr.op("pe", lambda e, h=h: e.transpose(out=pT[:, h * 128:(h + 1) * 128], in_=kt_[:, h * 128:(h + 1) * 128], identity=c["ident_b"][:]),
                              reads=[kt_, c["ident_b"]], writes=[P[2]])
                    for h in range(4):
                        tr.op("pe", lambda e, h=h: e.transpose(out=pT[:, (4 + h) * 128:(5 + h) * 128], in_=qt_[:, h * 128:(h + 1) * 128], identity=c["ident_b"][:]),
                              reads=[qt_, c["ident_b"]], writes=[P[2]])
                    tr.op("act", lambda e: e.activation(out=kqT[:].rearrange("p a b -> p (a b)"), in_=pT, func=AF.Copy), reads=[P[2]], writes=[kqT])
                    for h in range(4):
                        tr.op("pe", lambda e, h=h: e.matmul(out=P[0][:, h * 128:(h + 1) * 128], lhsT=kqT[:, h, :], rhs=kqT[:, 4 + h, :], start=True, stop=True),
                              reads=[kqT], writes=[P[0]])
                    tr.op("dve", lambda e: e.tensor_tensor(out=PT[:], in0=P[0][:].rearrange("p (h i) -> p h i", h=4),
                                                          in1=masks[dd][:].unsqueeze(1).to_broadcast([128, 4, 128]), op=ALU.mult),
                          reads=[P[0], masks[dd]], writes=[PT])
                kvb = [P[2], P[2], P[0], P[0]]
                for h in range(4):
                    cs = (h % 2) * 256
                    tr.op("pe", lambda e, h=h, cs=cs: e.matmul(out=kvb[h][:, cs:cs + 256], lhsT=kt_[:, h * 128:(h + 1) * 128], rhs=v_bf[:, h * 256:(h + 1) * 256], start=True, stop=True),
                          reads=[kt_, v_bf], writes=[kvb[h]])
                tr.mark("need_state")
                if full:
                    ob_ = [P[1], P[1], P[3], P[3]]
                    tr.op("act", lambda e: e.activation(out=Sbf[:].rearrange("p a b -> p (a b)"), in_=Sd[:].rearrange("p a b -> p (a b)"), func=AF.Copy), reads=[Sd], writes=[Sbf])
                    for h in range(4):
                        cs = (h % 2) * 256
                        tr.op("pe", lambda e, h=h, cs=cs: e.matmul(out=ob_[h][:, cs:cs + 256], lhsT=PT[:, h, :], rhs=v_bf[:, h * 256:(h + 1) * 256], start=True, stop=False),
                              reads=[PT, v_bf], writes=[ob_[h]])
                        tr.op("pe", lambda e, h=h, cs=cs: e.matmul(out=ob_[h][:, cs:cs + 256], lhsT=kqT[:, 4 + h, :], rhs=Sbf[:, h, :], start=False, stop=True),
                              reads=[kqT, Sbf], writes=[ob_[h]])
                tr.op("dve", lambda e: e.tensor_tensor(out=Sd[:, 0:2, :].rearrange("p a b -> p (a b)"), in0=P[2][:], in1=Sd[:, 0:2, :].rearrange("p a b -> p (a b)"), op=ALU.add),
                      reads=[P[2], Sd], writes=[Sd])
                tr.op("dve", lambda e: e.tensor_tensor(out=Sd[:, 2:4, :].rearrange("p a b -> p (a b)"), in0=P[0][:], in1=Sd[:, 2:4, :].rearrange("p a b -> p (a b)"), op=ALU.add),
                      reads=[P[0], Sd], writes=[Sd])
                tr.op("dve", lambda e: e.tensor_tensor(out=Sd[:], in0=Sd[:], in1=dcol[:].unsqueeze(2).to_broadcast([128, 4, 256]), op=ALU.mult),
                      reads=[Sd, dcol], writes=[Sd])
                if not full:
                    return
                if not sweepA:
                    os_ = ost[own_idx % 2]
                    tr.op("act", lambda e: e.activation(out=os_[:, 0:512], in_=P[1][:], func=AF.Copy), reads=[P[1]], writes=[os_])
                    tr.op("dve", lambda e: e.tensor_copy(out=os_[:, 512:1024], in_=P[3][:]), reads=[P[3]], writes=[os_])
                    tr.dma("sp", osts[own_idx % 2], out=oB_v[own_idx], in_=os_[:], reads=[os_], writes=[])
                    return
                ob = oB_sb[own_idx % 2]
                tr.dma("sp", oBs[own_idx % 2], out=ob[:], in_=oB_v[own_idx], writes=[ob])
                mm_tok(P[2], 2048, 512, [wgs[4]])
                tr.op("act", lambda e: e.activation(out=silr[:, 0:512], in_=P[2][:], func=AF.Silu), reads=[P[2]], writes=[silr])
                mm_tok(P[0], 2560, 512, [wgs[5]])
                tr.op("act", lambda e: e.activation(out=silr[:, 512:1024], in_=P[0][:], func=AF.Silu), reads=[P[0]], writes=[silr])
                tr.op("dve", lambda e: e.tensor_tensor(out=silr[:].rearrange("p (h v) -> p h v", h=4), in0=silr[:].rearrange("p (h v) -> p h v", h=4),
                                                      in1=gn[:].unsqueeze(1).to_broadcast([128, 4, 256]), op=ALU.mult), reads=[silr, gn], writes=[silr])
                for hh, pb in enumerate((P[1], P[3])):
                    tr.op("dve", lambda e, hh=hh, pb=pb: e.tensor_tensor(out=o_sb[:, hh * 512:(hh + 1) * 512], in0=pb[:], in1=ob[:, hh * 512:(hh + 1) * 512], op=ALU.add),
                          reads=[pb, ob], writes=[o_sb])
                for h in range(4):
                    tr.op("act", lambda e, h=h: e.activation(out=junk[:], in_=o_sb[:, h * 256:(h + 1) * 256], func=AF.Square, accum_out=ss4[:, h:h + 1]),
                          reads=[o_sb], writes=[junk, ss4])
                tr.op("act", lambda e: e.activation(out=rs4[:], in_=ss4[:], func=AF.Ln, scale=1.0 / 256.0, bias=c_eps[:]), reads=[ss4, c_eps], writes=[rs4])
                tr.op("act", lambda e: e.activation(out=rs4[:], in_=rs4[:], func=AF.Exp, scale=-0.5), reads=[rs4], writes=[rs4])
                tr.op("dve", lambda e: e.tensor_tensor(out=o_sb[:].rearrange("p (h v) -> p h v", h=4), in0=o_sb[:].rearrange("p (h v) -> p h v", h=4),
                                                      in1=rs4[:].unsqueeze(2).to_broadcast([128, 4, 256]), op=ALU.mult), reads=[o_sb, rs4], writes=[o_sb])
                ys = yst[own_idx % 2]
                tr.op("dve", lambda e: e.tensor_tensor(out=ys[:], in0=o_sb[:], in1=silr[:], op=ALU.mult), reads=[o_sb, silr], writes=[ys])
                tr.dma("sp", ysts[own_idx % 2], out=yx_v[own_idx][:, 0:1024], in_=ys[:], reads=[ys], writes=[])

            def sweep(tiles):
                recs = []
                for seq, (t, dd, full, sweepA, own_idx) in enumerate(tiles):
                    tr.begin_record()
                    gla_tile(t, dd, full, sweepA, own_idx, seq)
                    recs.append(tr.end_record())
                tr.run_pipelined(recs, depth=2)

            sweep([(t, 1, False, False, None) for t in (1, 0)])
            self.tap("gS_B", S[1][:].rearrange("p a b -> p (a b)"), [128, 1024], F32, [S[1]])
            sweep([(t, 1, False, False, None) for t in range(NT - 1, T_OTH0 - 1, -1)] +
                  [(t, 1, True, False, t - T_OWN0) for t in range(T_OTH0 - 1, T_OWN0 - 1, -1)])
            for e in Tracker.ENG:
                tr.wait_all(e, ost)
            sweep([(t, 0, False, True, None) for t in (0, 1)])
            self.tap("gS_A", S[0][:].rearrange("p a b -> p (a b)"), [128, 1024], F32, [S[0]])
            sweep([(t, 0, True, True, t - T_OWN0) for t in range(T_OWN0, T_OTH0)])
            for e in Tracker.ENG:
                tr.wait_all(e, yst)
            self.barrier_release(rel)

    def stage_ssd(self, st):
        tr, c, I = self.tr, self.c, self.I
        self.fence()
        c["yB"] = self.scratch("yB", [NOWN, 1024], F32)
        yB_v = c["yB"].t.rearrange("(n p) c -> n p c", p=128)
        yx_v = c["yx"].t.rearrange("(n p) c -> n p c", p=128)
        xtok_v = c["x_tok"].t.rearrange("(n p) c -> n p c", p=128)
        btok_v = c["B_tok"].t.rearrange("(n p) c -> n p c", p=128)
        w_in_v = I["w_in"].t.rearrange("(kc p) n -> p kc n", p=128)
        BT, CT = c["BT"], c["CT"]
        with ExitStack() as s2:
            wz = self.sb(s2, "wz", [128, 8, 1024], BF16)
            wdt = self.sb(s2, "wdt", [128, 8, 32], BF16)
            Abc = self.sb(s2, "Abc", [128, 32], F32)
            dtb = self.sb(s2, "dtb", [128, 32], F32)
            Dsk = self.sb(s2, "Dsk", [128, 16], F32)
            snb = self.sb(s2, "snb", [128, 1024], F32)
            c_one = self.sb(s2, "c_one2", [128, 1], F32)
            c_eps = self.sb(s2, "c_eps2", [128, 1], F32)
            ST = [self.sb(s2, "ST_A", [128, 2, 512], F32), self.sb(s2, "ST_B", [128, 2, 512], F32)]
            STbf = self.sb(s2, "STbf", [128, 2, 512], BF16)
            xt = [self.sb(s2, "xt%d" % i, [128, 1024], BF16) for i in range(2)]
            bt = [self.sb(s2, "bt%d" % i, [128, 256], BF16) for i in range(2)]
            xts = [self.dsem() for _ in range(2)]
            dt_ = self.sb(s2, "dt_", [128, 16], F32)
            dtA = self.sb(s2, "dtA", [128, 16], F32)
            acs = self.sb(s2, "acs", [128, 16], F32)
            ea = self.sb(s2, "ea", [128, 16], F32)
            dend = self.sb(s2, "dend", [128, 16], F32)
            dtot = self.sb(s2, "dtot", [128, 16], F32)
            R1 = self.sb(s2, "R1", [128, 16, 128], BF16)
            E = self.sb(s2, "E", [128, 16, 128], BF16)
            M = self.sb(s2, "M", [128, 16, 128], BF16)
            CBm = self.sb(s2, "CBm", [128, 2, 128], F32)
            xdt = self.sb(s2, "xdt", [128, 1024], BF16)
            xdd = self.sb(s2, "xdd", [128, 1024], BF16)
            silz = self.sb(s2, "silz", [128, 1024], F32)
            y_sb = self.sb(s2, "y_sb", [128, 1024], F32)
            tmp = self.sb(s2, "ytmp", [128, 1024], F32)
            yB_sb = [self.sb(s2, "yB_sb%d" % i, [128, 1024], F32) for i in range(2)]
            yBs = [self.dsem() for _ in range(2)]
            yst = [self.sb(s2, "ysst%d" % i, [128, 1024], F32) for i in range(2)]
            ysts = [self.dsem() for _ in range(2)]
            yxs = [self.sb(s2, "yxs%d" % i, [128, 1024], BF16) for i in range(2)]
            yxss = [self.dsem() for _ in range(2)]
            ss2 = self.sb(s2, "ss2", [128, 2], F32)
            rs2 = self.sb(s2, "rs2", [128, 2], F32)
            junk = self.sb(s2, "junks", [128, 512], BF16)
            pS = self.ps(s2, "pS")
            pD = [self.ps(s2, "pD%d" % i) for i in range(4)]
            pCB = self.ps(s2, "pCB")
            pY = [self.ps(s2, "pY%d" % i) for i in range(2)]
            rel = [wz, wdt, Abc, dtb, Dsk, snb, c_one, c_eps, ST[0], ST[1], STbf, dt_, dtA, acs, ea, dend, dtot, R1, E, M, CBm, xdt, xdd,
                   silz, y_sb, tmp, ss2, rs2, junk, pS, pCB] + xt + bt + yB_sb + yst + yxs + pD + pY
            d0 = self.dsem(6)
            tr.dma("pool", d0, out=wz[:], in_=w_in_v[:, :, OFF_Z:OFF_Z + 1024], writes=[wz])
            tr.dma("pool", d0, out=wdt[:], in_=w_in_v[:, :, OFF_DT:OFF_DT + 32], writes=[wdt])
            tr.dma("sp", d0, out=Abc[:], in_=I["a_log"].t.partition_broadcast(128), writes=[Abc])
            tr.dma("sp", d0, out=dtb[:], in_=I["dt_bias"].t.partition_broadcast(128), writes=[dtb])
            tr.dma("sp", d0, out=Dsk[:], in_=I["ssd_d"].t.partition_broadcast(128), writes=[Dsk])
            tr.dma("sp", d0, out=snb[:], in_=I["ssd_norm"].t.partition_broadcast(128), writes=[snb])
            tr.op("pool", lambda e: e.memset(c_one[:], 1.0), writes=[c_one])
            tr.op("pool", lambda e: e.memset(c_eps[:], EPS), writes=[c_eps])
            tr.op("act", lambda e: e.activation(out=Abc[:], in_=Abc[:], func=AF.Exp), reads=[Abc], writes=[Abc])
            tr.op("dve", lambda e: e.tensor_scalar(out=Abc[:], in0=Abc[:], scalar1=-1.0, scalar2=None, op0=ALU.mult), reads=[Abc], writes=[Abc])
            for dd in range(2):
                tr.op("pool", lambda e, dd=dd: e.memset(ST[dd][:], 0.0), writes=[ST[dd]])
            Lm = [c["tri_gt"], c["tri_lt"]]
            Tc = [c["tri_le"], c["tri_ge"]]
            cnt = [0]

            QS = [[pS, pD[0], pD[1], pD[2]], [pD[3], pCB, pY[0], pY[1]]]
            ea2 = [ea, self.sb(s2, "ea_b", [128, 16], F32)]
            dtot2 = [dtot, self.sb(s2, "dtot_b", [128, 16], F32)]
            xdd2 = [xdd, self.sb(s2, "xdd_b", [128, 1024], BF16)]
            silz2 = [silz, self.sb(s2, "silz_b", [128, 1024], F32)]
            ysb2 = [y_sb, self.sb(s2, "y_sb_b", [128, 1024], F32)]
            tmp2 = [tmp, self.sb(s2, "ytmp_b", [128, 1024], F32)]
            ss22 = [ss2, self.sb(s2, "ss2_b", [128, 2], F32)]
            rs22 = [rs2, self.sb(s2, "rs2_b", [128, 2], F32)]
            junk2 = [junk, self.sb(s2, "junks_b", [128, 512], BF16)]
            dt2 = [dt_, self.sb(s2, "dt_b", [128, 16], F32)]
            dtA2 = [dtA, self.sb(s2, "dtA_b", [128, 16], F32)]
            acs2 = [acs, self.sb(s2, "acs_b", [128, 16], F32)]
            dend2 = [dend, self.sb(s2, "dend_b", [128, 16], F32)]
            xdt2 = [xdt, self.sb(s2, "xdt_b", [128, 1024], BF16)]
            Lmb = [self.sb(s2, "Lmb%d" % i, [128, 128], BF16) for i in range(2)]
            for i in range(2):
                tr.op("pool", lambda e, i=i: e.tensor_copy(out=Lmb[i][:], in_=Lm[i][:]), reads=[Lm[i]], writes=[Lmb[i]])
            rel += [dt2[1], dtA2[1], acs2[1], dend2[1], xdt2[1]] + Lmb
            rel += [ea2[1], dtot2[1], xdd2[1], silz2[1], ysb2[1], tmp2[1], ss22[1], rs22[1], junk2[1]]

            def ssd_tile(t, dd, full, sweepA, own_idx, seq):
                STd = ST[dd]
                k = seq % 2
                Q = QS[k]
                ea_, dtot_, xdd_, silz_ = ea2[k], dtot2[k], xdd2[k], silz2[k]
                y_sb, tmp, ss2, rs2, junk = ysb2[k], tmp2[k], ss22[k], rs22[k], junk2[k]
                dt_, dtA, acs, dend, xdt = dt2[k], dtA2[k], acs2[k], dend2[k], xdt2[k]
                x_t, b_t = xt[k], bt[k]
                tr.regroup(xts[k], 2)
                tr.dma("sp", xts[k], out=x_t[:], in_=xtok_v[t], writes=[x_t])
                tr.dma("sp", xts[k], out=b_t[:], in_=btok_v[t], writes=[b_t])
                hres = [c["hT_r"][t]]
                lhs = lambda kc: c["hT"][:, kc, t * 128:(t + 1) * 128]
                if full and sweepA:
                    for hh in range(2):
                        for kc in range(8):
                            tr.op("pe", lambda e, kc=kc, hh=hh: e.matmul(out=Q[1 + hh][:], lhsT=lhs(kc), rhs=wz[:, kc, hh * 512:(hh + 1) * 512], start=(kc == 0), stop=(kc == 7)),
                                  reads=hres + [wz], writes=[Q[1 + hh]])
                        tr.op("act", lambda e, hh=hh: e.activation(out=silz_[:, hh * 512:(hh + 1) * 512], in_=Q[1 + hh][:], func=AF.Silu), reads=[Q[1 + hh]], writes=[silz_])
                for kc in range(8):
                    tr.op("pe", lambda e, kc=kc: e.matmul(out=Q[0][:, 0:16], lhsT=lhs(kc), rhs=wdt[:, kc, dd * 16:(dd + 1) * 16], start=(kc == 0), stop=(kc == 7)),
                          reads=hres + [wdt], writes=[Q[0]])
                tr.op("dve", lambda e: e.tensor_tensor(out=dt_[:], in0=Q[0][:, 0:16], in1=dtb[:, dd * 16:(dd + 1) * 16], op=ALU.add), reads=[Q[0], dtb], writes=[dt_])
                tr.op("act", lambda e: e.activation(out=dt_[:], in_=dt_[:], func=AF.Exp), reads=[dt_], writes=[dt_])
                tr.op("act", lambda e: e.activation(out=dt_[:], in_=dt_[:], func=AF.Ln, bias=c_one[:]), reads=[dt_, c_one], writes=[dt_])
                tr.op("dve", lambda e: e.tensor_tensor(out=dtA[:], in0=dt_[:], in1=Abc[:, dd * 16:(dd + 1) * 16], op=ALU.mult), reads=[dt_, Abc], writes=[dtA])
                tr.op("pe", lambda e: e.matmul(out=Q[0][:, 16:32], lhsT=Tc[dd][:], rhs=dtA[:], start=True, stop=True), reads=[Tc[dd], dtA], writes=[Q[0]])
                tr.op("pe", lambda e: e.matmul(out=Q[0][:, 32:48], lhsT=c["ones_f"][:], rhs=dtA[:], start=True, stop=True), reads=[c["ones_f"], dtA], writes=[Q[0]])
                tr.op("dve", lambda e: e.tensor_copy(out=acs[:], in_=Q[0][:, 16:32]), reads=[Q[0]], writes=[acs])
                tr.op("dve", lambda e: e.tensor_tensor(out=dend[:], in0=Q[0][:, 32:48], in1=acs[:], op=ALU.subtract), reads=[Q[0], acs], writes=[dend])
                tr.op("act", lambda e: e.activation(out=dend[:], in_=dend[:], func=AF.Exp), reads=[dend], writes=[dend])
                tr.op("act", lambda e: e.activation(out=dtot_[:], in_=Q[0][:, 32:48], func=AF.Exp), reads=[Q[0]], writes=[dtot_])
                tr.op("dve", lambda e: e.tensor_tensor(out=xdt[:].rearrange("p (h q) -> p h q", h=16), in0=x_t[:].rearrange("p (h q) -> p h q", h=16),
                                                      in1=dt_[:].unsqueeze(2).to_broadcast([128, 16, 64]), op=ALU.mult), reads=[x_t, dt_], writes=[xdt])
                tr.op("pool", lambda e: e.tensor_tensor(out=xdd_[:].rearrange("p (h q) -> p h q", h=16), in0=xdt[:].rearrange("p (h q) -> p h q", h=16),
                                                       in1=dend[:].unsqueeze(2).to_broadcast([128, 16, 64]), op=ALU.mult), reads=[xdt, dend], writes=[xdd_])
                if full:
                    tok0 = (t - T_OWN0) * 128
                    Dbank = [Q[1], Q[2], Q[3], Q[1]]
                    tr.op("act", lambda e: e.activation(out=ea_[:], in_=acs[:], func=AF.Exp), reads=[acs], writes=[ea_])
                    tr.mark("need_r1")
                    tr.op("dve", lambda e: e.tensor_tensor(out=R1[:], in0=Tc[dd][:].unsqueeze(1).to_broadcast([128, 16, 128]),
                                                          in1=dtA[:].unsqueeze(2).to_broadcast([128, 16, 128]), op=ALU.mult), reads=[Tc[dd], dtA], writes=[R1])
                    for b4 in range(4):
                        tr.op("pe", lambda e, b4=b4: e.matmul(out=Dbank[b4][:], lhsT=Lmb[dd][:], rhs=R1[:, 4 * b4:4 * b4 + 4, :], start=True, stop=True),
                              reads=[Lmb[dd], R1], writes=[Dbank[b4]])
                        tr.op("act", lambda e, b4=b4: e.activation(out=E[:, 4 * b4:4 * b4 + 4, :], in_=Dbank[b4][:].rearrange("p (h i) -> p h i", h=4), func=AF.Exp), reads=[Dbank[b4]], writes=[E])
                    for g in range(2):
                        tr.op("pe", lambda e, g=g: e.matmul(out=Q[2][:, g * 128:(g + 1) * 128], lhsT=BT[:, g, tok0:tok0 + 128], rhs=CT[:, g, tok0:tok0 + 128], start=True, stop=True),
                              reads=[BT, CT], writes=[Q[2]])
                    tr.op("dve", lambda e: e.tensor_tensor(out=CBm[:], in0=Q[2][:, 0:256].rearrange("p (g i) -> p g i", g=2),
                                                          in1=Tc[dd][:].unsqueeze(1).to_broadcast([128, 2, 128]), op=ALU.mult), reads=[Q[2], Tc[dd]], writes=[CBm])
                    for g in range(2):
                        eng = "dve" if g == 0 else "pool"
                        tr.op(eng, lambda e, g=g: e.tensor_tensor(out=M[:, g * 8:(g + 1) * 8, :], in0=E[:, g * 8:(g + 1) * 8, :],
                                                                  in1=CBm[:, g:g + 1, :].to_broadcast([128, 8, 128]), op=ALU.mult), reads=[E, CBm], writes=[M])
                    Yb = [Q[3], Q[1]]
                    for h in range(16):
                        py = Yb[h // 8]
                        cs = (h % 8) * 64
                        tr.op("pe", lambda e, h=h, py=py, cs=cs: e.matmul(out=py[:, cs:cs + 64], lhsT=M[:, h, :], rhs=xdt[:, h * 64:(h + 1) * 64], start=True, stop=True),
                              reads=[M, xdt], writes=[py])
                    tr.mark("r1_done")
                tr.mark("need_state")
                Ob = [Q[2], Q[0]]
                if full:
                    tr.op("act", lambda e: e.activation(out=STbf[:].rearrange("p a b -> p (a b)"), in_=STd[:].rearrange("p a b -> p (a b)"), func=AF.Copy), reads=[STd], writes=[STbf])
                    for g in range(2):
                        tr.op("pe", lambda e, g=g: e.matmul(out=Ob[g][:], lhsT=CT[:, g, tok0:tok0 + 128], rhs=STbf[:, g, :], start=True, stop=True),
                              reads=[CT, STbf], writes=[Ob[g]])
                        tr.op("dve", lambda e, g=g: e.tensor_tensor(out=tmp[:, g * 512:(g + 1) * 512].rearrange("p (h q) -> p h q", h=8), in0=Ob[g][:].rearrange("p (h q) -> p h q", h=8),
                                                                    in1=ea_[:, g * 8:(g + 1) * 8].unsqueeze(2).to_broadcast([128, 8, 64]), op=ALU.mult), reads=[Ob[g], ea_], writes=[tmp])
                        tr.op("dve", lambda e, g=g: e.tensor_tensor(out=y_sb[:, g * 512:(g + 1) * 512], in0=Yb[g][:], in1=tmp[:, g * 512:(g + 1) * 512], op=ALU.add),
                              reads=[Yb[g], tmp], writes=[y_sb])
                for g in range(2):
                    tr.op("pe", lambda e, g=g: e.matmul(out=Ob[g][:], lhsT=b_t[:, g * 128:(g + 1) * 128], rhs=xdd_[:, g * 512:(g + 1) * 512], start=True, stop=True),
                          reads=[b_t, xdd_], writes=[Ob[g]])
                    tr.op("dve", lambda e, g=g: e.tensor_tensor(out=STd[:, g, :].rearrange("p (h q) -> p h q", h=8), in0=STd[:, g, :].rearrange("p (h q) -> p h q", h=8),
                                                                in1=dtot_[:, g * 8:(g + 1) * 8].unsqueeze(2).to_broadcast([128, 8, 64]), op=ALU.mult), reads=[STd, dtot_], writes=[STd])
                    tr.op("dve", lambda e, g=g: e.tensor_tensor(out=STd[:, g, :], in0=Ob[g][:], in1=STd[:, g, :], op=ALU.add), reads=[Ob[g], STd], writes=[STd])
                tr.mark("state_done")
                if not full:
                    return
                if not sweepA:
                    ys = yst[own_idx % 2]
                    tr.op("act", lambda e: e.activation(out=ys[:], in_=y_sb[:], func=AF.Copy), reads=[y_sb], writes=[ys])
                    tr.dma("sp", ysts[own_idx % 2], out=yB_v[own_idx], in_=ys[:], reads=[ys], writes=[])
                    return
                yb = yB_sb[own_idx % 2]
                tr.dma("sp", yBs[own_idx % 2], out=yb[:], in_=yB_v[own_idx], writes=[yb])
                tr.op("dve", lambda e: e.tensor_tensor(out=y_sb[:], in0=y_sb[:], in1=yb[:], op=ALU.add), reads=[y_sb, yb], writes=[y_sb])
                tr.op("pool", lambda e: e.tensor_tensor(out=tmp[:].rearrange("p (h q) -> p h q", h=16), in0=x_t[:].rearrange("p (h q) -> p h q", h=16),
                                                       in1=Dsk[:].unsqueeze(2).to_broadcast([128, 16, 64]), op=ALU.mult), reads=[x_t, Dsk], writes=[tmp])
                tr.op("dve", lambda e: e.tensor_tensor(out=y_sb[:], in0=y_sb[:], in1=tmp[:], op=ALU.add), reads=[y_sb, tmp], writes=[y_sb])
                tr.op("dve", lambda e: e.tensor_tensor(out=y_sb[:], in0=y_sb[:], in1=silz_[:], op=ALU.mult), reads=[y_sb, silz_], writes=[y_sb])
                for g in range(2):
                    tr.op("act", lambda e, g=g: e.activation(out=junk[:], in_=y_sb[:, g * 512:(g + 1) * 512], func=AF.Square, accum_out=ss2[:, g:g + 1]), reads=[y_sb], writes=[junk, ss2])
                tr.op("act", lambda e: e.activation(out=rs2[:], in_=ss2[:], func=AF.Ln, scale=1.0 / 512.0, bias=c_eps[:]), reads=[ss2, c_eps], writes=[rs2])
                tr.op("act", lambda e: e.activation(out=rs2[:], in_=rs2[:], func=AF.Exp, scale=-0.5), reads=[rs2], writes=[rs2])
                tr.op("dve", lambda e: e.tensor_tensor(out=y_sb[:].rearrange("p (g q) -> p g q", g=2), in0=y_sb[:].rearrange("p (g q) -> p g q", g=2),
                                                      in1=rs2[:].unsqueeze(2).to_broadcast([128, 2, 512]), op=ALU.mult), reads=[y_sb, rs2], writes=[y_sb])
                yo = yxs[own_idx % 2]
                tr.op("pool", lambda e: e.tensor_tensor(out=yo[:], in0=y_sb[:], in1=snb[:], op=ALU.mult), reads=[y_sb, snb], writes=[yo])
                tr.dma("sp", yxss[own_idx % 2], out=yx_v[own_idx][:, 1024:2048], in_=yo[:], reads=[yo], writes=[])

            def sweep(tiles):
                recs = []
                for seq, (t, dd, full, sweepA, own_idx) in enumerate(tiles):
                    tr.begin_record()
                    ssd_tile(t, dd, full, sweepA, own_idx, seq)
                    recs.append(tr.end_record())
                tr.run_pipelined(recs, depth=2)

            sweep([(t, 1, False, False, None) for t in (1, 0)])
            self.tap("sS_B", ST[1][:].rearrange("p a b -> p (a b)"), [128, 1024], F32, [ST[1]])
            sweep([(t, 1, False, False, None) for t in range(NT - 1, T_OTH0 - 1, -1)] +
                  [(t, 1, True, False, t - T_OWN0) for t in range(T_OTH0 - 1, T_OWN0 - 1, -1)])
            for e in Tracker.ENG:
                tr.wait_all(e, yst)
            sweep([(t, 0, False, True, None) for t in (0, 1)])
            self.tap("sS_A", ST[0][:].rearrange("p a b -> p (a b)"), [128, 1024], F32, [ST[0]])
            sweep([(t, 0, True, True, t - T_OWN0) for t in range(T_OWN0, T_OTH0)])
            for e in Tracker.ENG:
                tr.wait_all(e, yxs)
            self.barrier_release(rel)

    def stage_post(self, st):
        tr, c, I = self.tr, self.c, self.I
        self.fence()
        c["h_lat"] = self.sb(st, "h_lat", [128, 16, D], F32)
        c["h_r"] = [Res("h_lat%d" % i) for i in range(16)]
        c["h2T"] = self.sb(st, "h2T", [128, 8, NOWN], BF16)
        c["h2_r"] = [Res("h2T%d" % i) for i in range(16)]
        c["comb"] = self.sb(st, "comb", [128, 16, 32], F32)
        yx_v = c["yx"].t.rearrange("(n p) c -> n p c", p=128)
        h_lat, h2T = c["h_lat"], c["h2T"]
        with ExitStack() as s2:
            wo = self.sb(s2, "wo", [128, 16, D], BF16)
            wr = self.sb(s2, "wr", [128, 8, 36], F32)
            brr = self.sb(s2, "brr", [1, 36], F32)
            c_eps = self.sb(s2, "c_eps3", [128, 1], F32)
            lg = self.sb(s2, "lg", [128, 16, 36], F32)
            yxt = [self.sb(s2, "yxt%d" % i, [128, 2048], BF16) for i in range(2)]
            yxs = [self.dsem() for _ in range(2)]
            xr = [self.sb(s2, "xr2_%d" % i, [128, D], F32) for i in range(2)]
            xrs = [self.dsem() for _ in range(2)]
            junk = self.sb(s2, "pjunk", [128, D], BF16)
            PS = []
            for par in range(2):
                PS.append({"yxT": self.sb(s2, "yxT%d" % par, [128, 16, 128], BF16), "tmp": self.sb(s2, "ptmp%d" % par, [128, D], F32),
                           "h2f": self.sb(s2, "h2f%d" % par, [128, 8, 128], F32), "ss": self.sb(s2, "pss%d" % par, [128, 1], F32),
                           "rs": self.sb(s2, "prs%d" % par, [128, 1], F32), "B": [self.ps(s2, "ppB%d_%d" % (par, i)) for i in range(4)]})
            rel = [wo, wr, brr, c_eps, lg, junk] + yxt + xr
            for p_ in PS:
                rel += [p_["yxT"], p_["tmp"], p_["h2f"], p_["ss"], p_["rs"]] + p_["B"]
            w_out_v = I["w_out"].t.rearrange("(kc p) n -> p kc n", p=128)
            d0 = self.dsem(2)
            wst = [self.sb(s2, "wst%d" % i, [128, 1, D], F32) for i in range(2)]
            rel += wst
            wsts = [self.dsem() for _ in range(2)]
            for q in range(16):
                tr.dma("sp", wsts[q % 2], out=wst[q % 2][:], in_=w_out_v[:, q:q + 1, :], writes=[wst[q % 2]])
                tr.op("pool", lambda e, q=q: e.tensor_copy(out=wo[:, q:q + 1, :], in_=wst[q % 2][:]), reads=[wst[q % 2]], writes=[wo])
            tr.dma("sp", d0, out=wr[:].rearrange("p a b -> p (a b)"), in_=I["w_router"].t, writes=[wr])
            tr.dma("sp", d0, out=brr[:], in_=I["b_router"].t, writes=[brr])
            tr.op("pool", lambda e: e.memset(c_eps[:], EPS), writes=[c_eps])
            def post_tile(i):
                y_t, x_t = yxt[i % 2], xr[i % 2]
                p_ = PS[i % 2]
                yxT, tmp, h2f, ss, rs, B = p_["yxT"], p_["tmp"], p_["h2f"], p_["ss"], p_["rs"], p_["B"]
                hn = tmp
                pT = [B[0][:].bitcast(BF16), B[1][:].bitcast(BF16)]
                tr.dma("sp", yxs[i % 2], out=y_t[:], in_=yx_v[i], writes=[y_t])
                tr.dma("sp", xrs[i % 2], out=x_t[:], in_=I["xs"].t[NCTX + i * 128: NCTX + (i + 1) * 128, :], writes=[x_t])
                for kc in range(16):
                    tr.op("pe", lambda e, kc=kc: e.transpose(out=pT[kc // 8][:, (kc % 8) * 128:(kc % 8 + 1) * 128], in_=y_t[:, kc * 128:(kc + 1) * 128], identity=c["ident_b"][:]),
                          reads=[y_t, c["ident_b"]], writes=[B[kc // 8]])
                tr.op("act", lambda e: e.activation(out=yxT[:, 0:8, :].rearrange("p a b -> p (a b)"), in_=pT[0], func=AF.Copy), reads=[B[0]], writes=[yxT])
                tr.op("dve", lambda e: e.tensor_copy(out=yxT[:, 8:16, :].rearrange("p a b -> p (a b)"), in_=pT[1]), reads=[B[1]], writes=[yxT])
                hl = h_lat[:, i, :]
                for hh in range(2):
                    for kc in range(16):
                        tr.op("pe", lambda e, kc=kc, hh=hh: e.matmul(out=B[2 + hh][:], lhsT=yxT[:, kc, :], rhs=wo[:, kc, hh * 512:(hh + 1) * 512], start=(kc == 0), stop=(kc == 15)),
                              reads=[yxT, wo], writes=[B[2 + hh]])
                    tr.op("dve", lambda e, hh=hh: e.tensor_tensor(out=tmp[:, hh * 512:(hh + 1) * 512], in0=B[2 + hh][:], in1=c["g1_bc"][:, hh * 512:(hh + 1) * 512], op=ALU.mult),
                          reads=[B[2 + hh], c["g1_bc"]], writes=[tmp])
                tr.op("pool", lambda e: e.tensor_tensor(out=hl, in0=tmp[:], in1=x_t[:], op=ALU.add), reads=[tmp, x_t], writes=[c["h_r"][i]])
                tr.op("act", lambda e: e.activation(out=junk[:], in_=hl, func=AF.Square, accum_out=ss[:]), reads=[c["h_r"][i]], writes=[junk, ss])
                tr.op("act", lambda e: e.activation(out=rs[:], in_=ss[:], func=AF.Ln, scale=1.0 / D, bias=c_eps[:]), reads=[ss, c_eps], writes=[rs])
                tr.op("act", lambda e: e.activation(out=rs[:], in_=rs[:], func=AF.Exp, scale=-0.5), reads=[rs], writes=[rs])
                tr.op("dve", lambda e: e.tensor_scalar(out=hn[:], in0=hl, scalar1=rs[:], scalar2=None, op0=ALU.mult), reads=[c["h_r"][i], rs], writes=[hn])
                for kc in range(8):
                    tr.op("pe", lambda e, kc=kc: e.transpose(out=B[kc // 4][:, (kc % 4) * 128:(kc % 4 + 1) * 128], in_=hn[:, kc * 128:(kc + 1) * 128], identity=c["ident_f"][:]),
                          reads=[hn, c["ident_f"]], writes=[B[kc // 4]])
                for q in range(2):
                    tr.op("dve", lambda e, q=q: e.tensor_tensor(out=h2f[:, q * 4:(q + 1) * 4, :], in0=B[q][:].rearrange("p (k t) -> p k t", k=4),
                                                               in1=c["s2"][:, q * 4:(q + 1) * 4].unsqueeze(2).to_broadcast([128, 4, 128]), op=ALU.mult), reads=[B[q], c["s2"]], writes=[h2f])
                tr.op("pool", lambda e: e.tensor_tensor(out=h2f[:], in0=h2f[:], in1=c["b2"][:].unsqueeze(2).to_broadcast([128, 8, 128]), op=ALU.add), reads=[h2f, c["b2"]], writes=[h2f])
                tr.op("act", lambda e: e.activation(out=h2T[:, :, i * 128:(i + 1) * 128], in_=h2f[:], func=AF.Copy), reads=[h2f], writes=[c["h2_r"][i]])
                for kc in range(8):
                    tr.op("pe", lambda e, kc=kc: e.matmul(out=B[2][:, 0:36], lhsT=h2f[:, kc, :], rhs=wr[:, kc, :], start=(kc == 0), stop=False), reads=[h2f, wr], writes=[B[2]])
                tr.op("pe", lambda e: e.matmul(out=B[2][:, 0:36], lhsT=c["ones_f"][0:1, :], rhs=brr[0:1, :], start=False, stop=True), reads=[c["ones_f"], brr], writes=[B[2]])
                tr.op("dve", lambda e: e.tensor_copy(out=lg[:, i, :], in_=B[2][:, 0:36]), reads=[B[2]], writes=[lg])

            recs = []
            for i in range(16):
                tr.begin_record()
                post_tile(i)
                recs.append(tr.end_record())
            tr.run_pipelined(recs, depth=2)
            self.tap("lg", lg[:].rearrange("p a b -> p (a b)"), [128, 16 * 36], F32, [lg])
            self.tap("h_lat", h_lat[:].rearrange("p a b -> p (a b)"), [128, 16 * D], F32, c["h_r"])
            def T(name, shape):
                t_ = self.sb(s2, name, shape, F32)
                rel.append(t_)
                return t_
            gmax = T("gmax", [128, 16]); mg = T("mg", [128, 16, 4]); eg = T("eg", [128, 16, 4]); gsum = T("gsum", [128, 16]); pg = T("pg", [128, 16])
            t48 = T("t48", [128, 16, 4, 8]); ein = T("ein", [128, 16, 8]); m1 = T("m1", [128, 16]); k1 = T("k1", [128, 16, 8]); e2 = T("e2", [128, 16, 8])
            m2 = T("m2", [128, 16]); k2 = T("k2", [128, 16, 8]); dd_ = T("dd_", [128, 16]); w1 = T("w1", [128, 16]); w2 = T("w2", [128, 16]); cw8 = T("cw8", [128, 16, 8])
            gl = lg[:, :, 0:4]
            el = lg[:, :, 4:36].rearrange("p t (g x) -> p t g x", g=4)
            V = lambda fn, r, w: tr.op("dve", fn, reads=r, writes=w)
            V(lambda e: e.tensor_reduce(out=gmax[:], in_=gl, axis=AX.X, op=ALU.max), [lg], [gmax])
            V(lambda e: e.tensor_tensor(out=mg[:], in0=gl, in1=gmax[:].unsqueeze(2).to_broadcast([128, 16, 4]), op=ALU.is_equal), [lg, gmax], [mg])
            V(lambda e: e.tensor_tensor(out=eg[:], in0=gl, in1=gmax[:].unsqueeze(2).to_broadcast([128, 16, 4]), op=ALU.subtract), [lg, gmax], [eg])
            tr.op("act", lambda e: e.activation(out=eg[:], in_=eg[:], func=AF.Exp), reads=[eg], writes=[eg])
            V(lambda e: e.tensor_reduce(out=gsum[:], in_=eg[:], axis=AX.X, op=ALU.add), [eg], [gsum])
            V(lambda e: e.reciprocal(out=pg[:], in_=gsum[:]), [gsum], [pg])
            V(lambda e: e.tensor_tensor(out=t48[:], in0=el, in1=mg[:].unsqueeze(3).to_broadcast([128, 16, 4, 8]), op=ALU.mult), [lg, mg], [t48])
            V(lambda e: e.tensor_reduce(out=ein[:], in_=t48[:].rearrange("p t g x -> p t x g"), axis=AX.X, op=ALU.add), [t48], [ein])
            V(lambda e: e.tensor_reduce(out=m1[:], in_=ein[:], axis=AX.X, op=ALU.max), [ein], [m1])
            V(lambda e: e.tensor_tensor(out=k1[:], in0=ein[:], in1=m1[:].unsqueeze(2).to_broadcast([128, 16, 8]), op=ALU.is_equal), [ein, m1], [k1])
            V(lambda e: e.scalar_tensor_tensor(out=e2[:], in0=k1[:], scalar=-1.0e30, in1=ein[:], op0=ALU.mult, op1=ALU.add), [k1, ein], [e2])
            V(lambda e: e.tensor_reduce(out=m2[:], in_=e2[:], axis=AX.X, op=ALU.max), [e2], [m2])
            V(lambda e: e.tensor_tensor(out=k2[:], in0=e2[:], in1=m2[:].unsqueeze(2).to_broadcast([128, 16, 8]), op=ALU.is_equal), [e2, m2], [k2])
            V(lambda e: e.tensor_tensor(out=dd_[:], in0=m2[:], in1=m1[:], op=ALU.subtract), [m1, m2], [dd_])
            tr.op("act", lambda e: e.activation(out=dd_[:], in_=dd_[:], func=AF.Exp), reads=[dd_], writes=[dd_])
            V(lambda e: e.tensor_scalar(out=w1[:], in0=dd_[:], scalar1=1.0, scalar2=None, op0=ALU.add), [dd_], [w1])
            V(lambda e: e.reciprocal(out=w1[:], in_=w1[:]), [w1], [w1])
            V(lambda e: e.tensor_tensor(out=w2[:], in0=dd_[:], in1=w1[:], op=ALU.mult), [dd_, w1], [w2])
            V(lambda e: e.tensor_tensor(out=w1[:], in0=w1[:], in1=pg[:], op=ALU.mult), [w1, pg], [w1])
            V(lambda e: e.tensor_tensor(out=w2[:], in0=w2[:], in1=pg[:], op=ALU.mult), [w2, pg], [w2])
            V(lambda e: e.tensor_tensor(out=k1[:], in0=k1[:], in1=w1[:].unsqueeze(2).to_broadcast([128, 16, 8]), op=ALU.mult), [k1, w1], [k1])
            V(lambda e: e.tensor_tensor(out=k2[:], in0=k2[:], in1=w2[:].unsqueeze(2).to_broadcast([128, 16, 8]), op=ALU.mult), [k2, w2], [k2])
            V(lambda e: e.tensor_tensor(out=cw8[:], in0=k1[:], in1=k2[:], op=ALU.add), [k1, k2], [cw8])
            V(lambda e: e.tensor_tensor(out=c["comb"][:].rearrange("p t (g x) -> p t g x", g=4), in0=mg[:].unsqueeze(3).to_broadcast([128, 16, 4, 8]),
                                        in1=cw8[:].unsqueeze(2).to_broadcast([128, 16, 4, 8]), op=ALU.mult), [mg, cw8], [c["comb"]])
            self.tap("comb", c["comb"][:].rearrange("p a b -> p (a b)"), [128, 512], F32, [c["comb"]])
            self.barrier_release(rel)

    def stage_moe(self, st):
        tr, c, I = self.tr, self.c, self.I
        self.fence()
        h_lat, h2T, comb = c["h_lat"], c["h2T"], c["comb"]
        with ExitStack() as s2:
            wgt = [self.sb(s2, "mwg%d" % i, [128, 8, DFF], BF16) for i in range(2)]
            wut = [self.sb(s2, "mwu%d" % i, [128, 8, DFF], BF16) for i in range(2)]
            wdt = [self.sb(s2, "mwd%d" % i, [128, 4, D], BF16) for i in range(2)]
            stg = [self.sb(s2, "mstg%d" % i, [128, 8, DFF], F32) for i in range(2)]
            stgs = [self.dsem() for _ in range(2)]
            ns = [0]
            sg_ = [self.sb(s2, "msg%d" % i, [128, 512], F32) for i in range(2)]
            heT = [self.sb(s2, "heT%d" % i, [128, 4, 512], BF16) for i in range(2)]
            pG = [self.ps(s2, "mpG%d" % i) for i in range(2)]
            pU = [self.ps(s2, "mpU%d" % i) for i in range(2)]
            pDn = [self.ps(s2, "mpD%d" % i) for i in range(4)]
            rel = wgt + wut + wdt + sg_ + heT + pG + pU + pDn + stg
            nb = 0
            pending = [None]

            def emit_down(he, wd_e, j, ex):
                for tt in range(4):
                    ti = j * 4 + tt
                    for hh in range(2):
                        d_p = pDn[(tt * 2 + hh) % 4]
                        for fc in range(4):
                            tr.op("pe", lambda e, fc=fc: e.matmul(out=d_p[:], lhsT=he[:, fc, tt * 128:(tt + 1) * 128], rhs=wd_e[:, fc, hh * 512:(hh + 1) * 512],
                                                                  start=(fc == 0), stop=(fc == 3)), reads=[he, wd_e], writes=[d_p])
                        tr.op("dve", lambda e: e.scalar_tensor_tensor(
                            out=h_lat[:, ti, hh * 512:(hh + 1) * 512], in0=d_p[:], scalar=comb[:, ti, ex:ex + 1], in1=h_lat[:, ti, hh * 512:(hh + 1) * 512], op0=ALU.mult, op1=ALU.add),
                            reads=[d_p, comb, c["h_r"][ti]], writes=[c["h_r"][ti]])

            for ex in range(NEXP):
                k = ex % 2
                wg_e, wu_e, wd_e = wgt[k], wut[k], wdt[k]
                for (dst, src) in ((wg_e, I["w_gate"].t[ex].rearrange("(kc p) n -> p kc n", p=128)), (wu_e, I["w_up"].t[ex].rearrange("(kc p) n -> p kc n", p=128))):
                    sg_t, sg_s = stg[ns[0] % 2], stgs[ns[0] % 2]
                    ns[0] += 1
                    tr.dma("sp", sg_s, out=sg_t[:], in_=src, writes=[sg_t])
                    tr.op("pool", lambda e, dst=dst, sg_t=sg_t: e.tensor_copy(out=dst[:], in_=sg_t[:]), reads=[sg_t], writes=[dst])
                sg_t, sg_s = stg[ns[0] % 2], stgs[ns[0] % 2]
                ns[0] += 1
                sv = sg_t[:].rearrange("p a b -> p (a b)").rearrange("p (f n) -> p f n", f=4)
                tr.dma("sp", sg_s, out=sv, in_=I["w_down"].t[ex].rearrange("(fc p) n -> p fc n", p=128), writes=[sg_t])
                tr.op("pool", lambda e, wd_e=wd_e, sv=sv: e.tensor_tensor(out=wd_e[:], in0=sv, in1=c["g2_bc"][:].unsqueeze(1).to_broadcast([128, 4, D]), op=ALU.mult),
                      reads=[sg_t, c["g2_bc"]], writes=[wd_e])
                for j in range(4):
                    he = heT[nb % 2]
                    nb += 1
                    hres = [c["h2_r"][j * 4 + q] for q in range(4)]
                    for fc in range(4):
                        g_p, u_p, sg = pG[fc % 2], pU[fc % 2], sg_[fc % 2]
                        for kc in range(8):
                            tr.op("pe", lambda e, kc=kc, fc=fc, g_p=g_p: e.matmul(out=g_p[:], lhsT=wg_e[:, kc, fc * 128:(fc + 1) * 128], rhs=h2T[:, kc, j * 512:(j + 1) * 512],
                                                                               start=(kc == 0), stop=(kc == 7)), reads=[wg_e] + hres, writes=[g_p])
                        for kc in range(8):
                            tr.op("pe", lambda e, kc=kc, fc=fc, u_p=u_p: e.matmul(out=u_p[:], lhsT=wu_e[:, kc, fc * 128:(fc + 1) * 128], rhs=h2T[:, kc, j * 512:(j + 1) * 512],
                                                                               start=(kc == 0), stop=(kc == 7)), reads=[wu_e] + hres, writes=[u_p])
                        tr.op("act", lambda e, g_p=g_p, sg=sg: e.activation(out=sg[:], in_=g_p[:], func=AF.Silu), reads=[g_p], writes=[sg])
                        tr.op("dve", lambda e, u_p=u_p, sg=sg, fc=fc, he=he: e.tensor_tensor(out=he[:, fc, :], in0=u_p[:], in1=sg[:], op=ALU.mult), reads=[u_p, sg], writes=[he])
                    if pending[0] is not None:
                        emit_down(*pending[0])
                    pending[0] = (he, wd_e, j, ex)
            emit_down(*pending[0])
            self.tap("h_fin", h_lat[:].rearrange("p a b -> p (a b)"), [128, 16 * D], F32, c["h_r"])
            self.barrier_release(rel)

    def stage_final(self, st):
        tr, c, I = self.tr, self.c, self.I
        self.fence()
        h_lat = c["h_lat"]
        out_v = self.out.t.rearrange("(n p) c -> n p c", p=128)
        with ExitStack() as s2:
            fn = self.sb(s2, "fn_bc", [128, D], F32)
            c_eps = self.sb(s2, "c_eps4", [128, 1], F32)
            junk = self.sb(s2, "fjunk", [128, D], BF16)
            ss = [self.sb(s2, "fss%d" % i, [128, 1], F32) for i in range(2)]
            rs = [self.sb(s2, "frs%d" % i, [128, 1], F32) for i in range(2)]
            ob = [self.sb(s2, "fob%d" % i, [128, D], F32) for i in range(2)]
            obs = [self.dsem() for _ in range(2)]
            tr.dma("sp", self.dsem(), out=fn[:], in_=I["final_norm"].t.partition_broadcast(128), writes=[fn])
            tr.op("pool", lambda e: e.memset(c_eps[:], EPS), writes=[c_eps])
            for i in range(16):
                hl = h_lat[:, i, :]
                s_, r_, o_ = ss[i % 2], rs[i % 2], ob[i % 2]
                tr.op("act", lambda e, s_=s_, hl=hl: e.activation(out=junk[:], in_=hl, func=AF.Square, accum_out=s_[:]), reads=[c["h_r"][i]], writes=[junk, s_])
                tr.op("act", lambda e, s_=s_, r_=r_: e.activation(out=r_[:], in_=s_[:], func=AF.Ln, scale=1.0 / D, bias=c_eps[:]), reads=[s_, c_eps], writes=[r_])
                tr.op("act", lambda e, r_=r_: e.activation(out=r_[:], in_=r_[:], func=AF.Exp, scale=-0.5), reads=[r_], writes=[r_])
                tr.op("dve", lambda e, r_=r_, o_=o_, hl=hl: e.scalar_tensor_tensor(out=o_[:], in0=hl, scalar=r_[:], in1=fn[:], op0=ALU.mult, op1=ALU.mult),
                      reads=[c["h_r"][i], r_, fn], writes=[o_])
                tr.dma("sp", obs[i % 2], out=out_v[i], in_=o_[:], reads=[o_], writes=[])
            self.final += ob


def prep_core(inp, b, hf):
    L = 0
    rev = hf == 1
    x, ctx = inp["x"][b], inp["ctx"][b]
    if not rev:
        ctx_a, own, oth = ctx, x[0:2048], x[2048:4096]
        dA, dB = 0, 1
    else:
        ctx_a, own, oth = ctx[::-1], x[2048:4096][::-1], x[0:2048][::-1]
        dA, dB = 1, 0
    m = {}
    m["xs"] = np.ascontiguousarray(np.concatenate([ctx_a, own, oth], axis=0), dtype=np.float32)
    cT = np.stack([inp["c"][b].reshape(8, 128).T, inp["c_ctx"].reshape(8, 128).T], axis=2).reshape(128, 16)
    m["cT"] = np.ascontiguousarray(cT, dtype=np.float32)
    m["w_ada"] = np.ascontiguousarray(inp["w_ada"][L])
    m["b_ada"] = np.ascontiguousarray(inp["b_ada"][L].reshape(1, -1))
    m["norm_mix_fm"] = np.ascontiguousarray(inp["norm_mix"][L].reshape(8, 128).T)
    m["norm_ffn_fm"] = np.ascontiguousarray(inp["norm_ffn"][L].reshape(8, 128).T)
    w_in = inp["w_in"][L]
    if rev:
        w_in = np.concatenate([w_in[:, :OFF_G], w_in[:, OFF_G + 16:OFF_G + 32], w_in[:, OFF_G:OFF_G + 16],
                               w_in[:, OFF_Z:OFF_DT], w_in[:, OFF_DT + 16:OFF_DT + 32], w_in[:, OFF_DT:OFF_DT + 16]], axis=1)
    m["w_in"] = np.ascontiguousarray(w_in)
    wu, gb = inp["gla_w_up"][L], inp["gla_b"][L]
    m["w_up_aug"] = np.ascontiguousarray(np.stack([np.concatenate([wu[dA], gb[dA][None, :]], axis=0),
                                                   np.concatenate([wu[dB], gb[dB][None, :]], axis=0)], axis=0))
    m["gla_norm"] = np.ascontiguousarray(inp["gla_norm"][L].reshape(1, -1))
    m["ssd_norm"] = np.ascontiguousarray(inp["ssd_norm"][L].reshape(1, -1))
    m["final_norm"] = np.ascontiguousarray(inp["final_norm"].reshape(1, -1))
    cw = inp["ssd_conv_w"][L]
    if rev:
        cw = cw[::-1, ::-1, :]
    m["conv_w_fm"] = np.ascontiguousarray(cw.reshape(9, 12, 128).transpose(2, 1, 0))
    m["conv_b_fm"] = np.ascontiguousarray(inp["ssd_conv_b"][L].reshape(12, 128).T)
    m["dt_bias"] = np.ascontiguousarray(np.concatenate([inp["ssd_dt_bias"][L][dA], inp["ssd_dt_bias"][L][dB]]).reshape(1, 32))
    m["a_log"] = np.ascontiguousarray(np.concatenate([inp["ssd_a_log"][L][dA], inp["ssd_a_log"][L][dB]]).reshape(1, 32))
    m["ssd_d"] = np.ascontiguousarray(inp["ssd_d"][L].reshape(1, 16))
    m["w_out"] = np.ascontiguousarray(inp["w_out"][L])
    wrt = np.concatenate([inp["router_group_w"][L], inp["router_expert_w"][L]], axis=1)
    m["w_router"] = np.ascontiguousarray(wrt.reshape(8, 128, 36).transpose(1, 0, 2).reshape(128, 8 * 36))
    m["b_router"] = np.ascontiguousarray(np.concatenate([inp["router_group_b"][L], inp["router_expert_b"][L]]).reshape(1, 36))
    m["w_gate"] = np.ascontiguousarray(inp["expert_w_gate"][L])
    m["w_up"] = np.ascontiguousarray(inp["expert_w_up"][L])
    m["w_down"] = np.ascontiguousarray(inp["expert_w_down"][L])
    return {k: np.asarray(v, dtype=np.float32) for k, v in m.items()}


def run(inputs, debug=None, stop_after=None, cores=8):
    bld = Builder(debug=debug, stop_after=stop_after)
    nc = bld.build()
    in_maps = [prep_core(inputs, i // 2, i % 2) for i in range(cores)]
    res = run_bass_kernel_spmd(nc, in_maps, core_ids=list(range(cores)))
    return res, bld


def kernel(**inputs):
    inputs = {k: np.asarray(v) for k, v in inputs.items()}
    res, _ = run(inputs)
    out = np.empty((4, 4096, D), dtype=np.float32)
    for i in range(8):
        b, hf = i // 2, i % 2
        o = np.asarray(res.results[i]["out"], dtype=np.float32)
        if hf == 0:
            out[b, 0:2048] = o
        else:
            out[b, 2048:4096] = o[::-1]
    return out
```

```python
import math
from contextlib import ExitStack

import numpy as np
import concourse.bass as bass
import concourse.mybir as mybir
from concourse.bass_utils import run_bass_kernel_spmd

F32 = mybir.dt.float32
BF16 = mybir.dt.bfloat16
AF = mybir.ActivationFunctionType
ALU = mybir.AluOpType
AX = mybir.AxisListType

D = 1024
NCTX, NOWN, NOTH = 256, 2048, 2048
TOK = NCTX + NOWN + NOTH
NT = TOK // 128
T_CTX0, T_OWN0, T_OTH0 = 0, 2, 18
EPS = 1e-6
IN_W = 5696
OFF_K, OFF_V, OFF_R, OFF_G, OFF_Z, OFF_XBC, OFF_DT = 512, 1024, 2048, 3072, 3104, 4128, 5664
NEXP, DFF = 32, 512


class Res:
    __slots__ = ("name", "lw", "rd")

    def __init__(self, name=""):
        self.name = name
        self.lw = None
        self.rd = {}


class Tile:
    def __init__(self, t, name):
        self.t = t
        self.r = Res(name)

    def __getitem__(self, idx):
        return self.t[idx]


class Tracker:
    ENG = ("pe", "act", "dve", "pool", "sp")
    CH = 2000

    def __init__(self, nc, sems, dma_sems, same_engine_sync=True):
        self.nc = nc
        self.eng = {"pe": nc.tensor, "act": nc.scalar, "dve": nc.vector, "pool": nc.gpsimd, "sp": nc.sync}
        self.cnt = {e: 0 for e in self.ENG}
        self.waited = {e: {} for e in self.ENG}
        self.sems = {e: [sems[e]] for e in sems}
        self.free_dma = list(dma_sems)
        self.same = same_engine_sync
        self.ninst = 0
        self.rec = None

    def new_dma_sem(self, group=0):
        d = self.free_dma.pop()
        self._uid = getattr(self, "_uid", 0) + 1
        d = d if isinstance(d, list) else [d, 0, 0, 0, "dma%d" % self._uid]
        if group:
            d[2] = group
            d[3] = d[1] + 16 * group
        return d

    def regroup(self, d, n):
        if self.rec is not None:
            self.rec.append(("call", lambda: self.regroup(d, n)))
            return
        assert d[2] == 0
        d[2] = n
        d[3] = d[1] + 16 * n

    def begin_record(self):
        self.rec = []

    def end_record(self):
        r, self.rec = self.rec, None
        return r

    def mark(self, name):
        self.rec.append(("mark", name))

    def _emit_item(self, it):
        if it[0] == "op":
            self.op(*it[1:])
        elif it[0] == "dma":
            self.dma(*it[1:])
        elif it[0] == "call":
            it[1]()

    def run_pipelined(self, records, depth=2, serial_fronts=False):
        assert self.rec is None
        active = []
        nxt = 0
        done = {}
        fin = -1

        def released(name, idx):
            return max(done.get(name, -1), fin) >= idx - 1 or idx == 0

        while active or nxt < len(records):
            while len(active) < depth and nxt < len(records):
                if serial_fronts and active and not active[-1][3]:
                    break
                if active and min(x[2] for x in active) <= nxt - depth:
                    break
                active.append([records[nxt], 0, nxt, False])
                nxt += 1
            progressed = False
            for a in list(active):
                lst, pos, idx, _ = a
                if pos >= len(lst):
                    a[3] = True
                    active.remove(a)
                    fin = max(fin, idx) if all(x[2] > idx for x in active) else fin
                    for nm in list(done.keys()):
                        done[nm] = max(done[nm], idx) if done[nm] >= idx - 1 else done[nm]
                    progressed = True
                    continue
                it = lst[pos]
                if it[0] == "mark":
                    nm = it[1]
                    if nm.startswith("need_"):
                        sec = nm[5:]
                        if sec == "state":
                            a[3] = True
                        if not released(sec, idx):
                            continue
                    elif nm.endswith("_done"):
                        sec = nm[:-5]
                        done[sec] = max(done.get(sec, -1), idx)
                    a[1] += 1
                    progressed = True
                    continue
                self._emit_item(it)
                a[1] += 1
                progressed = True
            if not progressed:
                for a in active:
                    it = a[0][a[1]]
                    if it[0] == "mark" and it[1].startswith("need_"):
                        done[it[1][5:]] = max(done.get(it[1][5:], -1), a[2] - 1)
                        progressed = True
                assert progressed

    def release_dma_sem(self, d):
        self.free_dma.append(d)

    def _wait(self, e, ev):
        if ev is None:
            return
        if ev[0] == "dma":
            _, s, v, key = ev
            if self.waited[e].get(key, 0) >= v:
                return
            self.waited[e][key] = v
            self.eng[e].wait_ge(s, v)
        else:
            pe, n = ev
            if pe == e and (not self.same or e in ("pe", "sp")):
                return
            if self.waited[e].get(pe, 0) >= n:
                return
            self.waited[e][pe] = n
            self.eng[e].wait_ge(self.sems[pe][(n - 1) // self.CH], (n - 1) % self.CH + 1)

    def _deps(self, e, reads, writes):
        for r in reads:
            self._wait(e, r.lw)
        for w in writes:
            self._wait(e, w.lw)
            for ev in w.rd.values():
                self._wait(e, ev)

    @staticmethod
    def _note_read(r, ev):
        key = ev[3] if ev[0] == "dma" else ev[0]
        old = r.rd.get(key)
        if old is None or (old[2] if old[0] == "dma" else old[1]) < (ev[2] if ev[0] == "dma" else ev[1]):
            r.rd[key] = ev

    def op(self, e, fn, reads=(), writes=()):
        if self.rec is not None:
            self.rec.append(("op", e, fn, list(reads), list(writes)))
            return None
        reads = [x.r if isinstance(x, Tile) else x for x in reads]
        writes = [x.r if isinstance(x, Tile) else x for x in writes]
        self._deps(e, reads, writes)
        self.cnt[e] += 1
        ev = (e, self.cnt[e])
        k = (self.cnt[e] - 1) // self.CH
        if k >= len(self.sems[e]):
            self.sems[e].append(self.free_dma.pop(0))
        fn(self.eng[e]).then_inc(self.sems[e][k], 1)
        self.ninst += 1
        for r in reads:
            self._note_read(r, ev)
        for w in writes:
            w.lw = ev
            w.rd = {}
        return ev

    def dma(self, e, dsem, out, in_, reads=(), writes=()):
        if self.rec is not None:
            self.rec.append(("dma", e, dsem, out, in_, list(reads), list(writes)))
            return None
        reads = [x.r if isinstance(x, Tile) else x for x in reads]
        writes = [x.r if isinstance(x, Tile) else x for x in writes]
        self._deps(e, reads, writes)
        if dsem[2] == 0:
            dsem[2] = 1
            dsem[3] = dsem[1] + 16
        dsem[1] += 16
        dsem[2] -= 1
        ev = ("dma", dsem[0], dsem[3], dsem[4])
        self.eng[e].dma_start(out=out, in_=in_).then_inc(dsem[0], 16)
        self.ninst += 1
        for r in reads:
            self._note_read(r, ev)
        for w in writes:
            w.lw = ev
            w.rd = {}
        return ev

    def wait_all(self, e, resources):
        for r in resources:
            r = r.r if isinstance(r, Tile) else r
            self._wait(e, r.lw)
            for ev in r.rd.values():
                self._wait(e, ev)


class Builder:
    def __init__(self, debug=None, stop_after=None):
        self.debug = debug or ()
        self.stop_after = stop_after
        self.nc = bass.Bass("TRN2", target_bir_lowering=False)
        self.dbg_out = {}

    def sb(self, st, name, shape, dt):
        self._uid = getattr(self, "_uid", 0) + 1
        return Tile(st.enter_context(self.nc.sbuf_tensor("sb%d_%s" % (self._uid, name), list(shape), dt)), name)

    def ps(self, st, name, shape=(128, 512), dt=F32):
        self._uid = getattr(self, "_uid", 0) + 1
        return Tile(st.enter_context(self.nc.psum_tensor("ps%d_%s" % (self._uid, name), list(shape), dt)), name)

    def dram_in(self, name, shape, dt=F32):
        return Tile(self.nc.dram_tensor(name, list(shape), dt, kind="ExternalInput").ap(), name)

    def dram_out(self, name, shape, dt=F32):
        return Tile(self.nc.dram_tensor(name, list(shape), dt, kind="ExternalOutput").ap(), name)

    def dram_scr(self, name, shape, dt):
        return Tile(self.nc.dram_tensor(name, list(shape), dt, kind="Internal").ap(), name)

    def dsem(self, group=0):
        return self.tr.new_dma_sem(group)

    def build(self):
        nc = self.nc
        I = {}
        I["xs"] = self.dram_in("xs", [TOK, D])
        I["cT"] = self.dram_in("cT", [128, 16])
        I["w_ada"] = self.dram_in("w_ada", [D, 6 * D])
        I["b_ada"] = self.dram_in("b_ada", [1, 6 * D])
        I["norm_mix_fm"] = self.dram_in("norm_mix_fm", [128, 8])
        I["norm_ffn_fm"] = self.dram_in("norm_ffn_fm", [128, 8])
        I["w_in"] = self.dram_in("w_in", [D, IN_W])
        I["w_up_aug"] = self.dram_in("w_up_aug", [2, 17, 512])
        I["gla_norm"] = self.dram_in("gla_norm", [1, 256])
        I["ssd_norm"] = self.dram_in("ssd_norm", [1, 1024])
        I["final_norm"] = self.dram_in("final_norm", [1, 1024])
        I["conv_w_fm"] = self.dram_in("conv_w_fm", [128, 12, 9])
        I["conv_b_fm"] = self.dram_in("conv_b_fm", [128, 12])
        I["dt_bias"] = self.dram_in("dt_bias", [1, 32])
        I["a_log"] = self.dram_in("a_log", [1, 32])
        I["ssd_d"] = self.dram_in("ssd_d", [1, 16])
        I["w_out"] = self.dram_in("w_out", [2048, D])
        I["w_router"] = self.dram_in("w_router", [128, 8 * 36])
        I["b_router"] = self.dram_in("b_router", [1, 36])
        I["w_gate"] = self.dram_in("w_gate", [NEXP, D, DFF])
        I["w_up"] = self.dram_in("w_up", [NEXP, D, DFF])
        I["w_down"] = self.dram_in("w_down", [NEXP, DFF, D])
        self.I = I
        self.out = self.dram_out("out", [NOWN, D])

        with ExitStack() as st:
            sems = {e: st.enter_context(nc.semaphore("s_" + e)) for e in Tracker.ENG}
            dsems = [st.enter_context(nc.semaphore("d%d" % i)) for i in range(90)]
            self.tr = Tracker(nc, sems, dsems)
            self.program(st)
        return nc

    def tap(self, name, tile_ap, shape, dt, reads):
        if name not in self.debug:
            return
        o = self.dram_out("dbg_" + name, shape, dt)
        self.dbg_out[name] = o
        n = shape[1]
        step = 2048
        d = self.dsem(len(range(0, n, step)))
        for c0 in range(0, n, step):
            c1 = min(n, c0 + step)
            self.tr.dma("sp", d, out=o.t[:, c0:c1], in_=tile_ap[:, c0:c1], reads=reads, writes=[o])
        self.final.append(o)

    def program(self, st):
        tr = self.tr
        self.final = []
        self.consts(st)
        self.stage_adaln(st)
        with ExitStack() as mst:
            self.stage_hT(mst)
            if self.stop_after == "hT":
                return self.finish()
            self.stage_gla(mst)
            if self.stop_after == "gla":
                return self.finish()
            self.stage_conv(mst)
            if self.stop_after == "conv":
                return self.finish()
            self.stage_ssd(mst)
            if self.stop_after == "ssd":
                return self.finish()
            self.barrier_release([self.c["hT"], self.c["BT"], self.c["CT"]] + self.c["hT_r"])
        self.stage_post(st)
        if self.stop_after == "post":
            return self.finish()
        self.stage_moe(st)
        if self.stop_after == "moe":
            return self.finish()
        self.stage_final(st)
        return self.finish()

    def finish(self):
        self.tr.wait_all("sp", self.final)

    def consts(self, st):
        tr = self.tr
        c = {}
        self.c = c
        c["ident_f"] = self.sb(st, "ident_f", [128, 128], F32)
        c["ident_b"] = self.sb(st, "ident_b", [128, 128], BF16)
        c["ones_f"] = self.sb(st, "ones_f", [128, 128], F32)
        for nm in ("tri_le", "tri_ge", "tri_gt", "tri_lt"):
            c[nm] = self.sb(st, nm, [128, 128], F32)
        idf = c["ident_f"]
        tr.op("pool", lambda e: e.memset(idf[:], 0.0), writes=[idf])
        tr.op("pool", lambda e: e.affine_select(out=idf[:], in_=idf[:], pattern=[[-1, 128]], compare_op=ALU.not_equal,
                                               fill=1.0, base=0, channel_multiplier=1), reads=[idf], writes=[idf])
        tr.op("pool", lambda e: e.tensor_copy(out=c["ident_b"][:], in_=idf[:]), reads=[idf], writes=[c["ident_b"]])
        tr.op("pool", lambda e: e.memset(c["ones_f"][:], 1.0), writes=[c["ones_f"]])
        specs = {"tri_le": (ALU.is_gt, 0), "tri_ge": (ALU.is_gt, 0), "tri_gt": (ALU.is_gt, 0), "tri_lt": (ALU.is_gt, 0)}
        t = c["tri_le"]
        tr.op("pool", lambda e: e.memset(t[:], 1.0), writes=[t])
        tr.op("pool", lambda e: e.affine_select(out=t[:], in_=t[:], pattern=[[1, 128]], compare_op=ALU.is_ge,
                                               fill=0.0, base=0, channel_multiplier=-1), reads=[t], writes=[t])
        t2 = c["tri_ge"]
        tr.op("pool", lambda e: e.memset(t2[:], 1.0), writes=[t2])
        tr.op("pool", lambda e: e.affine_select(out=t2[:], in_=t2[:], pattern=[[-1, 128]], compare_op=ALU.is_ge,
                                               fill=0.0, base=0, channel_multiplier=1), reads=[t2], writes=[t2])
        t3 = c["tri_gt"]
        tr.op("pool", lambda e: e.memset(t3[:], 1.0), writes=[t3])
        tr.op("pool", lambda e: e.affine_select(out=t3[:], in_=t3[:], pattern=[[-1, 128]], compare_op=ALU.is_gt,
                                               fill=0.0, base=0, channel_multiplier=1), reads=[t3], writes=[t3])
        t4 = c["tri_lt"]
        tr.op("pool", lambda e: e.memset(t4[:], 1.0), writes=[t4])
        tr.op("pool", lambda e: e.affine_select(out=t4[:], in_=t4[:], pattern=[[1, 128]], compare_op=ALU.is_gt,
                                               fill=0.0, base=0, channel_multiplier=-1), reads=[t4], writes=[t4])
        self.tap("tri_le", c["tri_le"][:], [128, 128], F32, [c["tri_le"]])
        self.tap("tri_gt", c["tri_gt"][:], [128, 128], F32, [c["tri_gt"]])

    def stage_adaln(self, st):
        tr, c, I = self.tr, self.c, self.I
        c["mod_fm"] = self.sb(st, "mod_fm", [128, 6, 8, 2], F32)
        c["g1_bc"] = self.sb(st, "g1_bc", [128, D], F32)
        c["g2_bc"] = self.sb(st, "g2_bc", [128, D], F32)
        c["s1"] = self.sb(st, "s1", [128, 8], F32)
        c["s1c"] = self.sb(st, "s1c", [128, 8], F32)
        c["b1"] = self.sb(st, "b1", [128, 8], F32)
        c["b1c"] = self.sb(st, "b1c", [128, 8], F32)
        c["s2"] = self.sb(st, "s2", [128, 8], F32)
        c["b2"] = self.sb(st, "b2", [128, 8], F32)
        with ExitStack() as s2:
            cT = self.sb(s2, "cT", [128, 16], F32)
            scT = self.sb(s2, "scT", [128, 16], F32)
            sc_rep = self.sb(s2, "sc_rep", [128, 8, 128], F32)
            brow = self.sb(s2, "brow", [1, 6 * D], F32)
            nm = self.sb(s2, "nm", [128, 8], F32)
            nf = self.sb(s2, "nf", [128, 8], F32)
            wblk = [self.sb(s2, "wblk%d" % i, [128, 8, D], F32) for i in range(2)]
            wsem = [self.dsem() for _ in range(2)]
            modps = self.ps(s2, "modps", [128, 512], F32)
            gps = [self.ps(s2, "gps%d" % i, [128, 512], F32) for i in range(2)]
            d = self.dsem(4)
            tr.dma("sp", d, out=cT[:], in_=I["cT"].t, writes=[cT])
            tr.dma("sp", d, out=brow[:], in_=I["b_ada"].t, writes=[brow])
            tr.dma("sp", d, out=nm[:], in_=I["norm_mix_fm"].t, writes=[nm])
            tr.dma("sp", d, out=nf[:], in_=I["norm_ffn_fm"].t, writes=[nf])
            tr.op("act", lambda e: e.activation(out=scT[:], in_=cT[:], func=AF.Silu), reads=[cT], writes=[scT])
            tr.op("dve", lambda e: e.tensor_copy(out=sc_rep[:], in_=scT[:].rearrange("p (k j) -> p k j", j=2)[:, :, 0:1].to_broadcast([128, 8, 128])),
                  reads=[scT], writes=[sc_rep])
            w_ada = I["w_ada"].t.rearrange("(kc p) n -> p kc n", p=128)
            mview = modps[:, 0:96].rearrange("p (b f t) -> p b f t", b=6, f=8)
            for blk in range(6):
                wb = wblk[blk % 2]
                tr.dma("sp", wsem[blk % 2], out=wb[:], in_=w_ada[:, :, blk * D:(blk + 1) * D], writes=[wb])
                if blk in (0, 1, 3, 4):
                    for fc in range(8):
                        for kc in range(8):
                            tr.op("pe", lambda e, fc=fc, kc=kc, wb=wb, blk=blk: e.matmul(
                                out=mview[:, blk, fc, :], lhsT=wb[:, kc, fc * 128:(fc + 1) * 128],
                                rhs=scT[:, 2 * kc:2 * kc + 2], start=(kc == 0), stop=False),
                                reads=[wb, scT], writes=[modps])
                        tr.op("pe", lambda e, fc=fc, blk=blk: e.matmul(
                            out=mview[:, blk, fc, :], lhsT=brow[0:1, blk * D + fc * 128: blk * D + (fc + 1) * 128],
                            rhs=c["ones_f"][0:1, 0:2], start=False, stop=True),
                            reads=[brow, c["ones_f"]], writes=[modps])
                else:
                    gdst = c["g1_bc"] if blk == 2 else c["g2_bc"]
                    for hh in range(2):
                        for kc in range(8):
                            tr.op("pe", lambda e, hh=hh, kc=kc, wb=wb: e.matmul(
                                out=gps[hh][:], lhsT=sc_rep[:, kc, :], rhs=wb[:, kc, hh * 512:(hh + 1) * 512],
                                start=(kc == 0), stop=False), reads=[wb, sc_rep], writes=[gps[hh]])
                        tr.op("pe", lambda e, hh=hh, blk=blk: e.matmul(
                            out=gps[hh][:], lhsT=c["ones_f"][0:1, :], rhs=brow[0:1, blk * D + hh * 512: blk * D + (hh + 1) * 512],
                            start=False, stop=True), reads=[brow, c["ones_f"]], writes=[gps[hh]])
                        tr.op("act", lambda e, hh=hh, gdst=gdst: e.activation(out=gdst[:, hh * 512:(hh + 1) * 512], in_=gps[hh][:], func=AF.Copy),
                              reads=[gps[hh]], writes=[gdst])
            mf = c["mod_fm"]
            mflat = mf[:].rearrange("p b f t -> p (b f t)")
            tr.op("dve", lambda e: e.tensor_copy(out=mflat[:, 0:32], in_=modps[:, 0:32]), reads=[modps], writes=[mf])
            tr.op("dve", lambda e: e.tensor_copy(out=mflat[:, 48:80], in_=modps[:, 48:80]), reads=[modps], writes=[mf])
            tr.op("dve", lambda e: e.scalar_tensor_tensor(out=c["s1"][:], in0=mf[:, 1, :, 0], scalar=1.0, in1=nm[:], op0=ALU.add, op1=ALU.mult),
                  reads=[mf, nm], writes=[c["s1"]])
            tr.op("dve", lambda e: e.scalar_tensor_tensor(out=c["s1c"][:], in0=mf[:, 1, :, 1], scalar=1.0, in1=nm[:], op0=ALU.add, op1=ALU.mult),
                  reads=[mf, nm], writes=[c["s1c"]])
            tr.op("dve", lambda e: e.scalar_tensor_tensor(out=c["s2"][:], in0=mf[:, 4, :, 0], scalar=1.0, in1=nf[:], op0=ALU.add, op1=ALU.mult),
                  reads=[mf, nf], writes=[c["s2"]])
            tr.op("dve", lambda e: e.tensor_copy(out=c["b1"][:], in_=mf[:, 0, :, 0]), reads=[mf], writes=[c["b1"]])
            tr.op("dve", lambda e: e.tensor_copy(out=c["b1c"][:], in_=mf[:, 0, :, 1]), reads=[mf], writes=[c["b1c"]])
            tr.op("dve", lambda e: e.tensor_copy(out=c["b2"][:], in_=mf[:, 3, :, 0]), reads=[mf], writes=[c["b2"]])
            self.tap("mod_fm", mf[:].rearrange("p b f t -> p (b f t)"), [128, 96], F32, [mf])
            self.tap("g1_bc", c["g1_bc"][:], [128, D], F32, [c["g1_bc"]])
            self.barrier_release([cT, scT, sc_rep, brow, nm, nf, wblk[0], wblk[1], modps, gps[0], gps[1]])

    def barrier_release(self, tiles):
        self.pending = getattr(self, "pending", [])
        for t in tiles:
            self.pending.append(t.r if isinstance(t, Tile) else t)

    def fence(self):
        pend = getattr(self, "pending", [])
        for e in Tracker.ENG:
            self.tr.wait_all(e, pend)
        self.pending = []

    def stage_hT(self, st):
        tr, c, I = self.tr, self.c, self.I
        self.fence()
        c["hT"] = self.sb(st, "hT", [128, 8, TOK], BF16)
        c["hT_r"] = [Res("hT%d" % t) for t in range(NT)]
        with ExitStack() as s2:
            xr = [self.sb(s2, "xr%d" % i, [128, D], F32) for i in range(3)]
            xsem = [self.dsem() for _ in range(3)]
            junk = self.sb(s2, "junk", [128, D], BF16)
            ss = [self.sb(s2, "ss%d" % i, [128, 1], F32) for i in range(3)]
            rstd = [self.sb(s2, "rstd%d" % i, [128, 1], F32) for i in range(3)]
            xn = [self.sb(s2, "xn%d" % i, [128, D], BF16) for i in range(3)]
            tmp = [self.sb(s2, "tmp%d" % i, [128, 8, 128], F32) for i in range(3)]
            tps = [self.ps(s2, "tps%d" % i, [128, 1024], BF16) for i in range(3)]
            epst = self.sb(s2, "epst", [128, 1], F32)
            tr.op("pool", lambda e: e.memset(epst[:], EPS), writes=[epst])
            rel = xr + ss + rstd + xn + tmp + tps + [junk, epst]
            recs = []
            for t in range(NT):
                tr.begin_record()
                x_t, ss_t, rs_t, xn_t, tmp_t, ps_t = xr[t % 3], ss[t % 3], rstd[t % 3], xn[t % 3], tmp[t % 3], tps[t % 3]
                hT_ap = c["hT"][:, :, t * 128:(t + 1) * 128]
                hT_r = c["hT_r"][t]
                isctx = t < T_OWN0
                sc, sh = (c["s1c"], c["b1c"]) if isctx else (c["s1"], c["b1"])
                tr.dma("sp", xsem[t % 3], out=x_t[:], in_=I["xs"].t[t * 128:(t + 1) * 128, :], writes=[x_t])
                tr.op("act", lambda e, x_t=x_t, ss_t=ss_t: e.activation(out=junk[:], in_=x_t[:], func=AF.Square, accum_out=ss_t[:]),
                      reads=[x_t], writes=[junk, ss_t])
                tr.op("act", lambda e, ss_t=ss_t, rs_t=rs_t: e.activation(out=rs_t[:], in_=ss_t[:], func=AF.Ln, scale=1.0 / D, bias=epst[:]),
                      reads=[ss_t, epst], writes=[rs_t])
                tr.op("act", lambda e, rs_t=rs_t: e.activation(out=rs_t[:], in_=rs_t[:], func=AF.Exp, scale=-0.5),
                      reads=[rs_t], writes=[rs_t])
                tr.op("dve", lambda e, x_t=x_t, rs_t=rs_t, xn_t=xn_t: e.tensor_scalar(out=xn_t[:], in0=x_t[:], scalar1=rs_t[:], scalar2=None, op0=ALU.mult),
                      reads=[x_t, rs_t], writes=[xn_t])
                for kc in range(8):
                    tr.op("pe", lambda e, kc=kc, xn_t=xn_t, ps_t=ps_t: e.transpose(out=ps_t[:, kc * 128:(kc + 1) * 128], in_=xn_t[:, kc * 128:(kc + 1) * 128], identity=c["ident_b"][:]),
                          reads=[xn_t, c["ident_b"]], writes=[ps_t])
                tr.op("dve", lambda e, ps_t=ps_t, tmp_t=tmp_t, sc=sc: e.tensor_tensor(
                    out=tmp_t[:], in0=ps_t[:].rearrange("p (k t) -> p k t", k=8), in1=sc[:].unsqueeze(2).to_broadcast([128, 8, 128]), op=ALU.mult),
                    reads=[ps_t, sc], writes=[tmp_t])
                tr.op("pool", lambda e, tmp_t=tmp_t, hT_ap=hT_ap, sh=sh: e.tensor_tensor(
                    out=hT_ap, in0=tmp_t[:], in1=sh[:].unsqueeze(2).to_broadcast([128, 8, 128]), op=ALU.add),
                    reads=[tmp_t, sh], writes=[hT_r])
                recs.append(tr.end_record())
            tr.run_pipelined(recs, depth=3)
            for t in (0, 2, 17, 33):
                if ("hT%d" % t) in self.debug:
                    o = self.dram_out("dbg_hT%d" % t, [128, 8, 128], BF16)
                    tr.dma("sp", self.dsem(), out=o.t, in_=c["hT"][:, :, t * 128:(t + 1) * 128], reads=[c["hT_r"][t]], writes=[o])
                    self.final.append(o)
            self.barrier_release(rel)

    def scratch(self, name, shape, dt):
        if name in self.debug:
            o = self.dram_out("dbg_" + name, shape, dt)
            self.final.append(o)
            return o
        return self.dram_scr(name, shape, dt)

    def stage_conv(self, st):
        tr, c, I = self.tr, self.c, self.I
        self.fence()
        c["x_tok"] = self.scratch("x_tok", [TOK, 1024], BF16)
        c["B_tok"] = self.scratch("B_tok", [TOK, 256], BF16)
        c["BT"] = self.sb(st, "BT", [128, 2, NOWN], BF16)
        c["CT"] = self.sb(st, "CT", [128, 2, NOWN], BF16)
        xtok_v = c["x_tok"].t.rearrange("(n p) c -> p n c", p=128)
        btok_v = c["B_tok"].t.rearrange("(n p) c -> p n c", p=128)
        w_in_v = I["w_in"].t.rearrange("(kc p) n -> p kc n", p=128)
        with ExitStack() as s2:
            wx = [self.sb(s2, "wx%d" % i, [128, 8, 128], BF16) for i in range(2)]
            wxs = [self.dsem() for _ in range(2)]
            cw = self.sb(s2, "cw", [128, 12, 9], F32)
            cb = self.sb(s2, "cb", [128, 12], F32)
            diag = [self.sb(s2, "diag%d" % i, [128, 9, 128], BF16) for i in range(2)]
            pre = [self.sb(s2, "pre%d" % i, [128, 66, 66], BF16) for i in range(2)]
            prec = [self.sb(s2, "prec%d" % i, [128, 258], BF16) for i in range(2)]
            post = [self.sb(s2, "post%d" % i, [128, 512], BF16) for i in range(3)]
            tst = [self.sb(s2, "tst%d" % i, [128, 4, 128], BF16) for i in range(3)]
            tsem = [self.dsem() for _ in range(3)]
            pp = [self.ps(s2, "pp%d" % i) for i in range(2)]
            pc = [self.ps(s2, "pc%d" % i) for i in range(2)]
            pt = [self.ps(s2, "pt%d" % i, [128, 1024], BF16) for i in range(2)]
            rel = wx + diag + pre + prec + post + tst + pp + pc + pt + [cw, cb]
            d0 = self.dsem(2)
            tr.dma("sp", d0, out=cw[:], in_=I["conv_w_fm"].t, writes=[cw])
            tr.dma("sp", d0, out=cb[:], in_=I["conv_b_fm"].t, writes=[cb])
            for i in range(2):
                tr.op("pool", lambda e, i=i: e.memset(pre[i][:], 0.0), writes=[pre[i]])
                tr.op("pool", lambda e, i=i: e.memset(prec[i][:], 0.0), writes=[prec[i]])
            nev = 0
            npost = 0
            for ct in range(12):
                w, dg, pr, prc = wx[ct % 2], diag[ct % 2], pre[ct % 2], prec[ct % 2]
                tr.dma("pool", wxs[ct % 2], out=w[:], in_=w_in_v[:, :, OFF_XBC + ct * 128: OFF_XBC + (ct + 1) * 128], writes=[w])
                tr.op("pool", lambda e, dg=dg, ct=ct: e.tensor_tensor(out=dg[:], in0=c["ident_f"][:].unsqueeze(1).to_broadcast([128, 9, 128]),
                                                                  in1=cw[:, ct, :].unsqueeze(2).to_broadcast([128, 9, 128]), op=ALU.mult),
                      reads=[c["ident_f"], cw], writes=[dg])
                for blk in range(9):
                    p_t = pp[nev % 2]
                    if blk == 0:
                        n, tok0, trs = 256, 0, [0, 1]
                    else:
                        n, tok0 = 512, NCTX + (blk - 1) * 512
                        trs = list(range(T_OWN0 + (blk - 1) * 4, T_OWN0 + blk * 4))
                    for kc in range(8):
                        tr.op("pe", lambda e, kc=kc, p_t=p_t, w=w, n=n, tok0=tok0: e.matmul(
                            out=p_t[:, 0:n], lhsT=w[:, kc, :], rhs=c["hT"][:, kc, tok0:tok0 + n], start=(kc == 0), stop=(kc == 7)),
                            reads=[w] + [c["hT_r"][t] for t in trs], writes=[p_t])
                    if blk == 0:
                        dst = prc[:, 1:257]
                        src = p_t[:, 0:256]
                        wr = prc
                    else:
                        r0 = (blk - 1) * 8
                        dst = pr[:, r0 + 1:r0 + 9, 1:65]
                        src = p_t[:, 0:512].rearrange("p (r q) -> p r q", q=64)
                        wr = pr
                    eng = "act" if nev % 2 == 0 else "dve"
                    if eng == "act":
                        tr.op("act", lambda e, dst=dst, src=src: e.activation(out=dst, in_=src, func=AF.Copy), reads=[p_t], writes=[wr])
                    else:
                        tr.op("dve", lambda e, dst=dst, src=src: e.tensor_copy(out=dst, in_=src), reads=[p_t], writes=[wr])
                    nev += 1
                for blk in range(9):
                    if ct >= 10 and (blk == 0 or blk >= 5):
                        continue
                    c_t = pc[blk % 2]
                    if blk == 0:
                        n = 256
                        for kw in range(3):
                            tr.op("pe", lambda e, kw=kw, c_t=c_t, dg=dg, prc=prc: e.matmul(
                                out=c_t[:, 0:256], lhsT=dg[:, 3 + kw, :], rhs=prc[:, kw:kw + 256], start=(kw == 0), stop=(kw == 2)),
                                reads=[dg, prc], writes=[c_t])
                    else:
                        n = 512
                        r0 = (blk - 1) * 8
                        for tap in range(9):
                            kh, kw = tap // 3, tap % 3
                            tr.op("pe", lambda e, tap=tap, kh=kh, kw=kw, c_t=c_t, dg=dg, pr=pr, r0=r0: e.matmul(
                                out=c_t[:, 0:512], lhsT=dg[:, tap, :], rhs=pr[:, r0 + kh:r0 + kh + 8, kw:kw + 64], start=(tap == 0), stop=(tap == 8)),
                                reads=[dg, pr], writes=[c_t])
                    own_blk = 1 <= blk <= 4
                    if ct >= 8 and own_blk:
                        g = (ct - 8) % 2
                        dstT = (c["BT"] if ct < 10 else c["CT"])
                        o0 = (blk - 1) * 512
                        tr.op("act", lambda e, dstT=dstT, g=g, o0=o0, c_t=c_t, ct=ct: e.activation(
                            out=dstT[:, g, o0:o0 + 512], in_=c_t[:, 0:512], func=AF.Silu, bias=cb[:, ct:ct + 1]),
                            reads=[c_t, cb], writes=[dstT])
                        if ct >= 10:
                            continue
                        src_post, src_r = dstT[:, g, o0:o0 + 512], dstT
                    else:
                        po = post[npost % 3]
                        tr.op("act", lambda e, po=po, c_t=c_t, ct=ct, n=n: e.activation(
                            out=po[:, 0:n], in_=c_t[:, 0:n], func=AF.Silu, bias=cb[:, ct:ct + 1]),
                            reads=[c_t, cb], writes=[po])
                        src_post, src_r = po[:, 0:n], po
                    ntl = n // 128
                    t_t = pt[npost % 2]
                    ts_t = tst[npost % 3]
                    for i in range(ntl):
                        tr.op("pe", lambda e, i=i, t_t=t_t, src_post=src_post: e.transpose(
                            out=t_t[:, i * 128:(i + 1) * 128], in_=src_post[:, i * 128:(i + 1) * 128], identity=c["ident_b"][:]),
                            reads=[src_r, c["ident_b"]], writes=[t_t])
                    tr.op("dve", lambda e, t_t=t_t, ts_t=ts_t, ntl=ntl: e.tensor_copy(
                        out=ts_t[:, 0:ntl, :], in_=t_t[:, 0:ntl * 128].rearrange("p (a b) -> p a b", b=128)),
                        reads=[t_t], writes=[ts_t])
                    tile0 = 0 if blk == 0 else T_OWN0 + (blk - 1) * 4
                    if ct < 8:
                        dst_d, dst_r = xtok_v[:, tile0:tile0 + ntl, ct * 128:(ct + 1) * 128], c["x_tok"]
                    else:
                        dst_d, dst_r = btok_v[:, tile0:tile0 + ntl, (ct - 8) * 128:(ct - 7) * 128], c["B_tok"]
                    tr.dma("sp", tsem[npost % 3], out=dst_d, in_=ts_t[:, 0:ntl, :], reads=[ts_t], writes=[])
                    c.setdefault("scr_ev", []).append(ts_t)
                    npost += 1
            self.conv_store_tiles = tst
            self.tap("BT", c["BT"][:].rearrange("p g t -> p (g t)"), [128, 2 * NOWN], BF16, [c["BT"]])
            self.tap("CT", c["CT"][:].rearrange("p g t -> p (g t)"), [128, 2 * NOWN], BF16, [c["CT"]])
            for e in Tracker.ENG:
                tr.wait_all(e, tst)
            self.barrier_release(rel)

    def stage_gla(self, st):
        tr, c, I = self.tr, self.c, self.I
        self.fence()
        c["oB"] = self.scratch("oB", [NOWN, 1024], F32)
        c["yx"] = self.scratch("yx", [NOWN, 2048], BF16)
        oB_v = c["oB"].t.rearrange("(n p) c -> n p c", p=128)
        yx_v = c["yx"].t.rearrange("(n p) c -> n p c", p=128)
        w_in_v = I["w_in"].t.rearrange("(kc p) n -> p kc n", p=128)
        LNQ = math.log(128.0 ** -0.5)
        with ExitStack() as s2:
            wg = self.sb(s2, "wgla", [128, 8, 3072], BF16)
            wgs = [Res("wgla%d" % i) for i in range(6)]
            wgg = self.sb(s2, "wgg", [128, 8, 32], BF16)
            wup = self.sb(s2, "wup", [17, 2, 512], F32)
            gn = self.sb(s2, "gn_bc", [128, 256], F32)
            c_one = self.sb(s2, "c_one", [128, 1], F32)
            c_lnq = self.sb(s2, "c_lnq", [128, 1], F32)
            c_eps = self.sb(s2, "c_eps", [128, 1], F32)
            negcol = self.sb(s2, "negcol", [128, 2], F32)
            Tm = [self.sb(s2, "TmA", [128, 128], F32), self.sb(s2, "TmB", [128, 128], F32)]
            S = [self.sb(s2, "S_A", [128, 4, 256], F32), self.sb(s2, "S_B", [128, 4, 256], F32)]
            Sbf = self.sb(s2, "Sbf", [128, 4, 256], BF16)
            FS = []
            for par in range(2):
                f = {}
                f["g_aug"] = self.sb(s2, "g_aug%d" % par, [32, 128], F32)
                f["v_bf"] = self.sb(s2, "v_bf%d" % par, [128, 1024], BF16)
                f["lap"] = self.sb(s2, "lap%d" % par, [128, 512], F32)
                f["Einv"] = self.sb(s2, "Einv%d" % par, [128, 512], F32)
                f["Eq"] = self.sb(s2, "Eq%d" % par, [128, 512], F32)
                f["kt_"] = self.sb(s2, "kt_%d" % par, [128, 512], BF16)
                f["qt_"] = self.sb(s2, "qt_%d" % par, [128, 512], BF16)
                f["kqT"] = self.sb(s2, "kqT%d" % par, [128, 8, 128], BF16)
                f["PT"] = self.sb(s2, "PT%d" % par, [128, 4, 128], BF16)
                f["dcol"] = self.sb(s2, "dcol%d" % par, [128, 4], F32)
                f["P"] = [self.ps(s2, "gP%d_%d" % (par, i)) for i in range(4)]
                FS.append(f)
            silr2 = [self.sb(s2, "silr%d" % i, [128, 1024], F32) for i in range(2)]
            o_sb2 = [self.sb(s2, "o_sb%d" % i, [128, 1024], F32) for i in range(2)]
            ost = [self.sb(s2, "ost%d" % i, [128, 1024], F32) for i in range(2)]
            osts = [self.dsem() for _ in range(2)]
            oB_sb = ost
            oBs = [self.dsem() for _ in range(2)]
            yst = [self.sb(s2, "yst%d" % i, [128, 1024], BF16) for i in range(2)]
            ysts = [self.dsem() for _ in range(2)]
            ss42 = [self.sb(s2, "ss4_%d" % i, [128, 4], F32) for i in range(2)]
            rs42 = [self.sb(s2, "rs4_%d" % i, [128, 4], F32) for i in range(2)]
            junk2 = [self.sb(s2, "junkg%d" % i, [128, 256], BF16) for i in range(2)]
            rel = [wg, wgg, wup, gn, c_one, c_lnq, c_eps, negcol, Tm[0], Tm[1], S[0], S[1], Sbf] + silr2 + o_sb2 + ss42 + rs42 + junk2 + ost + yst + wgs
            for f in FS:
                rel += [f[k] for k in ("g_aug", "v_bf", "lap", "Einv", "Eq", "kt_", "qt_", "kqT", "PT", "dcol")] + f["P"]
            d0 = self.dsem(9)
            for i in range(6):
                tr.dma("pool", d0, out=wg[:, :, i * 512:(i + 1) * 512], in_=w_in_v[:, :, i * 512:(i + 1) * 512], writes=[wgs[i]])
            tr.dma("pool", d0, out=wgg[:], in_=w_in_v[:, :, OFF_G:OFF_G + 32], writes=[wgg])
            tr.dma("sp", d0, out=wup[:], in_=I["w_up_aug"].t.rearrange("d k n -> k d n"), writes=[wup])
            tr.dma("sp", d0, out=gn[:], in_=I["gla_norm"].t.partition_broadcast(128), writes=[gn])
            tr.op("pool", lambda e: e.memset(c_one[:], 1.0), writes=[c_one])
            tr.op("pool", lambda e: e.memset(c_lnq[:], LNQ), writes=[c_lnq])
            tr.op("pool", lambda e: e.memset(c_eps[:], EPS), writes=[c_eps])
            tr.op("pool", lambda e: e.memset(negcol[:], -1.0 / 16.0), writes=[negcol])
            tr.op("pool", lambda e: e.tensor_scalar(out=Tm[0][:], in0=c["tri_le"][:], scalar1=-1.0 / 16.0, scalar2=None, op0=ALU.mult), reads=[c["tri_le"]], writes=[Tm[0]])
            tr.op("pool", lambda e: e.tensor_scalar(out=Tm[1][:], in0=c["tri_ge"][:], scalar1=-1.0 / 16.0, scalar2=None, op0=ALU.mult), reads=[c["tri_ge"]], writes=[Tm[1]])
            for f in FS:
                tr.op("pool", lambda e, f=f: e.memset(f["g_aug"][:], 1.0), writes=[f["g_aug"]])
            for dd in range(2):
                tr.op("pool", lambda e, dd=dd: e.memset(S[dd][:], 0.0), writes=[S[dd]])
            masks = [c["tri_le"], c["tri_ge"]]

            def gla_tile(t, dd, full, sweepA, own_idx, seq):
                f = FS[seq % 2]
                P = f["P"]
                g_aug, v_bf, lap, Einv, Eq, kt_, qt_, kqT, PT, dcol = (f[k] for k in ("g_aug", "v_bf", "lap", "Einv", "Eq", "kt_", "qt_", "kqT", "PT", "dcol"))
                Sd = S[dd]
                silr, o_sb, ss4, rs4, junk = silr2[seq % 2], o_sb2[seq % 2], ss42[seq % 2], rs42[seq % 2], junk2[seq % 2]
                hres = [c["hT_r"][t]]
                lhs = lambda kc: c["hT"][:, kc, t * 128:(t + 1) * 128]

                def mm_tok(ps_t, c0, n, wres):
                    for kc in range(8):
                        tr.op("pe", lambda e, kc=kc: e.matmul(out=ps_t[:, 0:n], lhsT=lhs(kc), rhs=wg[:, kc, c0:c0 + n], start=(kc == 0), stop=(kc == 7)),
                              reads=hres + wres, writes=[ps_t])
                for kc in range(8):
                    tr.op("pe", lambda e, kc=kc: e.matmul(out=P[3][0:16, 0:128], lhsT=wgg[:, kc, dd * 16:(dd + 1) * 16], rhs=lhs(kc), start=(kc == 0), stop=(kc == 7)),
                          reads=hres + [wgg], writes=[P[3]])
                tr.op("act", lambda e: e.activation(out=g_aug[0:16, :], in_=P[3][0:16, 0:128], func=AF.Copy), reads=[P[3]], writes=[g_aug])
                mm_tok(P[0], 512, 512, [wgs[1]])
                mm_tok(P[1], 1024, 512, [wgs[2]])
                mm_tok(P[2], 1536, 512, [wgs[3]])
                tr.op("pe", lambda e: e.matmul(out=P[3][:, 0:512], lhsT=g_aug[0:17, :], rhs=wup[:, dd, :], start=True, stop=True), reads=[g_aug, wup], writes=[P[3]])
                tr.op("act", lambda e: e.activation(out=lap[:], in_=P[3][:, 0:512], func=AF.Exp, scale=-1.0), reads=[P[3]], writes=[lap])
                tr.op("act", lambda e: e.activation(out=lap[:], in_=lap[:], func=AF.Ln, bias=c_one[:]), reads=[lap, c_one], writes=[lap])
                tr.op("act", lambda e: e.activation(out=v_bf[:, 0:512], in_=P[1][:], func=AF.Copy), reads=[P[1]], writes=[v_bf])
                tr.op("dve", lambda e: e.tensor_copy(out=v_bf[:, 512:1024], in_=P[2][:]), reads=[P[2]], writes=[v_bf])
                tr.op("pe", lambda e: e.matmul(out=P[3][:, 0:512], lhsT=Tm[dd][:], rhs=lap[:], start=True, stop=True), reads=[Tm[dd], lap], writes=[P[3]])
                if full:
                    mm_tok(P[1], 0, 512, [wgs[0]])
                tr.op("act", lambda e: e.activation(out=Einv[:], in_=P[3][:, 0:512], func=AF.Exp, scale=-1.0), reads=[P[3]], writes=[Einv])
                if full:
                    tr.op("act", lambda e: e.activation(out=Eq[:], in_=P[3][:, 0:512], func=AF.Exp, bias=c_lnq[:]), reads=[P[3], c_lnq], writes=[Eq])
                tr.op("dve", lambda e: e.tensor_tensor(out=kt_[:], in0=P[0][:], in1=Einv[:], op=ALU.mult), reads=[P[0], Einv], writes=[kt_])
                for h in range(4):
                    tr.op("pe", lambda e, h=h: e.matmul(out=P[3][:, 2 * h:2 * h + 2], lhsT=lap[:, h * 128:(h + 1) * 128], rhs=negcol[:], start=True, stop=True),
                          reads=[lap, negcol], writes=[P[3]])
                tr.op("act", lambda e: e.activation(out=dcol[:], in_=P[3][:, 0:8:2], func=AF.Exp), reads=[P[3]], writes=[dcol])
                if full:
                    pT = P[2][:].bitcast(BF16)
                    tr.op("dve", lambda e: e.tensor_tensor(out=qt_[:], in0=P[1][:], in1=Eq[:], op=ALU.mult), reads=[P[1], Eq], writes=[qt_])
                    for h in range(4):
                        tr.op("pe", lambda e, h=h: e.transpose(out=pT[:, h * 128:(h + 1) * 128], in_=kt_[:, h * 128:(h + 1) * 128], identity=c["ident_b"][:]),
                              reads=[kt_, c["ident_b"]], writes=[P[2]])
                    for h in range(4):
                        tr.op("pe", lambda e, h=h: e.transpose(out=pT[:, (4 + h) * 128:(5 + h) * 128], in_=qt_[:, h * 128:(h + 1) * 128], identity=c["ident_b"][:]),
                              reads=[qt_, c["ident_b"]], writes=[P[2]])
                    tr.op("act", lambda e: e.activation(out=kqT[:].rearrange("p a b -> p (a b)"), in_=pT, func=AF.Copy), reads=[P[2]], writes=[kqT])
                    for h in range(4):
                        tr.op("pe", lambda e, h=h: e.matmul(out=P[0][:, h * 128:(h + 1) * 128], lhsT=kqT[:, h, :], rhs=kqT[:, 4 + h, :], start=True, stop=True),
                              reads=[kqT], writes=[P[0]])
                    tr.op("dve", lambda e: e.tensor_tensor(out=PT[:], in0=P[0][:].rearrange("p (h i) -> p h i", h=4),
                                                          in1=masks[dd][:].unsqueeze(1).to_broadcast([128, 4, 128]), op=ALU.mult),
                          reads=[P[0], masks[dd]], writes=[PT])
                kvb = [P[2], P[2], P[0], P[0]]
                for h in range(4):
                    cs = (h % 2) * 256
                    tr.op("pe", lambda e, h=h, cs=cs: e.matmul(out=kvb[h][:, cs:cs + 256], lhsT=kt_[:, h * 128:(h + 1) * 128], rhs=v_bf[:, h * 256:(h + 1) * 256], start=True, stop=True),
                          reads=[kt_, v_bf], writes=[kvb[h]])
                tr.mark("need_state")
                if full:
                    ob_ = [P[1], P[1], P[3], P[3]]
                    tr.op("act", lambda e: e.activation(out=Sbf[:].rearrange("p a b -> p (a b)"), in_=Sd[:].rearrange("p a b -> p (a b)"), func=AF.Copy), reads=[Sd], writes=[Sbf])
                    for h in range(4):
                        cs = (h % 2) * 256
                        tr.op("pe", lambda e, h=h, cs=cs: e.matmul(out=ob_[h][:, cs:cs + 256], lhsT=PT[:, h, :], rhs=v_bf[:, h * 256:(h + 1) * 256], start=True, stop=False),
                              reads=[PT, v_bf], writes=[ob_[h]])
                        tr.op("pe", lambda e, h=h, cs=cs: e.matmul(out=ob_[h][:, cs:cs + 256], lhsT=kqT[:, 4 + h, :], rhs=Sbf[:, h, :], start=False, stop=True),
                              reads=[kqT, Sbf], writes=[ob_[h]])
                tr.op("dve", lambda e: e.tensor_tensor(out=Sd[:, 0:2, :].rearrange("p a b -> p (a b)"), in0=P[2][:], in1=Sd[:, 0:2, :].rearrange("p a b -> p (a b)"), op=ALU.add),
                      reads=[P[2], Sd], writes=[Sd])
                tr.op("dve", lambda e: e.tensor_tensor(out=Sd[:, 2:4, :].rearrange("p a b -> p (a b)"), in0=P[0][:], in1=Sd[:, 2:4, :].rearrange("p a b -> p (a b)"), op=ALU.add),
                      reads=[P[0], Sd], writes=[Sd])
                tr.op("dve", lambda e: e.tensor_tensor(out=Sd[:], in0=Sd[:], in1=dcol[:].unsqueeze(2).to_broadcast([128, 4, 256]), op=ALU.mult),
                      reads=[Sd, dcol], writes=[Sd])
                tr.mark("state_done")
                if not full:
                    return
                if not sweepA:
                    os_ = ost[own_idx % 2]
                    tr.op("act", lambda e: e.activation(out=os_[:, 0:512], in_=P[1][:], func=AF.Copy), reads=[P[1]], writes=[os_])
                    tr.op("dve", lambda e: e.tensor_copy(out=os_[:, 512:1024], in_=P[3][:]), reads=[P[3]], writes=[os_])
                    tr.dma("sp", osts[own_idx % 2], out=oB_v[own_idx], in_=os_[:], reads=[os_], writes=[])
                    return
                ob = oB_sb[own_idx % 2]
                tr.dma("sp", oBs[own_idx % 2], out=ob[:], in_=oB_v[own_idx], writes=[ob])
                mm_tok(P[2], 2048, 512, [wgs[4]])
                tr.op("act", lambda e: e.activation(out=silr[:, 0:512], in_=P[2][:], func=AF.Silu), reads=[P[2]], writes=[silr])
                mm_tok(P[0], 2560, 512, [wgs[5]])
                tr.op("act", lambda e: e.activation(out=silr[:, 512:1024], in_=P[0][:], func=AF.Silu), reads=[P[0]], writes=[silr])
                tr.op("dve", lambda e: e.tensor_tensor(out=silr[:].rearrange("p (h v) -> p h v", h=4), in0=silr[:].rearrange("p (h v) -> p h v", h=4),
                                                      in1=gn[:].unsqueeze(1).to_broadcast([128, 4, 256]), op=ALU.mult), reads=[silr, gn], writes=[silr])
                for hh, pb in enumerate((P[1], P[3])):
                    tr.op("dve", lambda e, hh=hh, pb=pb: e.tensor_tensor(out=o_sb[:, hh * 512:(hh + 1) * 512], in0=pb[:], in1=ob[:, hh * 512:(hh + 1) * 512], op=ALU.add),
                          reads=[pb, ob], writes=[o_sb])
                for h in range(4):
                    tr.op("act", lambda e, h=h: e.activation(out=junk[:], in_=o_sb[:, h * 256:(h + 1) * 256], func=AF.Square, accum_out=ss4[:, h:h + 1]),
                          reads=[o_sb], writes=[junk, ss4])
                tr.op("act", lambda e: e.activation(out=rs4[:], in_=ss4[:], func=AF.Ln, scale=1.0 / 256.0, bias=c_eps[:]), reads=[ss4, c_eps], writes=[rs4])
                tr.op("act", lambda e: e.activation(out=rs4[:], in_=rs4[:], func=AF.Exp, scale=-0.5), reads=[rs4], writes=[rs4])
                tr.op("dve", lambda e: e.tensor_tensor(out=o_sb[:].rearrange("p (h v) -> p h v", h=4), in0=o_sb[:].rearrange("p (h v) -> p h v", h=4),
                                                      in1=rs4[:].unsqueeze(2).to_broadcast([128, 4, 256]), op=ALU.mult), reads=[o_sb, rs4], writes=[o_sb])
                ys = yst[own_idx % 2]
                tr.op("dve", lambda e: e.tensor_tensor(out=ys[:], in0=o_sb[:], in1=silr[:], op=ALU.mult), reads=[o_sb, silr], writes=[ys])
                tr.dma("sp", ysts[own_idx % 2], out=yx_v[own_idx][:, 0:1024], in_=ys[:], reads=[ys], writes=[])

            def sweep(tiles):
                recs = []
                for seq, (t, dd, full, sweepA, own_idx) in enumerate(tiles):
                    tr.begin_record()
                    gla_tile(t, dd, full, sweepA, own_idx, seq)
                    recs.append(tr.end_record())
                tr.run_pipelined(recs, depth=2)

            sweep([(t, 1, False, False, None) for t in (1, 0)])
            self.tap("gS_B", S[1][:].rearrange("p a b -> p (a b)"), [128, 1024], F32, [S[1]])
            sweep([(t, 1, False, False, None) for t in range(NT - 1, T_OTH0 - 1, -1)] +
                  [(t, 1, True, False, t - T_OWN0) for t in range(T_OTH0 - 1, T_OWN0 - 1, -1)])
            for e in Tracker.ENG:
                tr.wait_all(e, ost)
            sweep([(t, 0, False, True, None) for t in (0, 1)])
            self.tap("gS_A", S[0][:].rearrange("p a b -> p (a b)"), [128, 1024], F32, [S[0]])
            sweep([(t, 0, True, True, t - T_OWN0) for t in range(T_OWN0, T_OTH0)])
            for e in Tracker.ENG:
                tr.wait_all(e, yst)
            self.barrier_release(rel)

    def stage_ssd(self, st):
        tr, c, I = self.tr, self.c, self.I
        self.fence()
        c["yB"] = self.scratch("yB", [NOWN, 1024], F32)
        yB_v = c["yB"].t.rearrange("(n p) c -> n p c", p=128)
        yx_v = c["yx"].t.rearrange("(n p) c -> n p c", p=128)
        xtok_v = c["x_tok"].t.rearrange("(n p) c -> n p c", p=128)
        btok_v = c["B_tok"].t.rearrange("(n p) c -> n p c", p=128)
        w_in_v = I["w_in"].t.rearrange("(kc p) n -> p kc n", p=128)
        BT, CT = c["BT"], c["CT"]
        with ExitStack() as s2:
            wz = self.sb(s2, "wz", [128, 8, 1024], BF16)
            wdt = self.sb(s2, "wdt", [128, 8, 32], BF16)
            Abc = self.sb(s2, "Abc", [128, 32], F32)
            dtb = self.sb(s2, "dtb", [128, 32], F32)
            Dsk = self.sb(s2, "Dsk", [128, 16], F32)
            snb = self.sb(s2, "snb", [128, 1024], F32)
            c_one = self.sb(s2, "c_one2", [128, 1], F32)
            c_eps = self.sb(s2, "c_eps2", [128, 1], F32)
            ST = [self.sb(s2, "ST_A", [128, 2, 512], F32), self.sb(s2, "ST_B", [128, 2, 512], F32)]
            STbf = self.sb(s2, "STbf", [128, 2, 512], BF16)
            xt = [self.sb(s2, "xt%d" % i, [128, 1024], BF16) for i in range(2)]
            bt = [self.sb(s2, "bt%d" % i, [128, 256], BF16) for i in range(2)]
            xts = [self.dsem() for _ in range(2)]
            dt_ = self.sb(s2, "dt_", [128, 16], F32)
            dtA = self.sb(s2, "dtA", [128, 16], F32)
            acs = self.sb(s2, "acs", [128, 16], F32)
            ea = self.sb(s2, "ea", [128, 16], F32)
            dend = self.sb(s2, "dend", [128, 16], F32)
            dtot = self.sb(s2, "dtot", [128, 16], F32)
            R1 = self.sb(s2, "R1", [128, 16, 128], BF16)
            E = self.sb(s2, "E", [128, 16, 128], BF16)
            M = self.sb(s2, "M", [128, 16, 128], BF16)
            CBm = self.sb(s2, "CBm", [128, 2, 128], F32)
            xdt = self.sb(s2, "xdt", [128, 1024], BF16)
            xdd = self.sb(s2, "xdd", [128, 1024], BF16)
            silz = self.sb(s2, "silz", [128, 1024], F32)
            y_sb = self.sb(s2, "y_sb", [128, 1024], F32)
            tmp = self.sb(s2, "ytmp", [128, 1024], F32)
            yB_sb = [self.sb(s2, "yB_sb%d" % i, [128, 1024], F32) for i in range(2)]
            yBs = [self.dsem() for _ in range(2)]
            yst = [self.sb(s2, "ysst%d" % i, [128, 1024], F32) for i in range(2)]
            ysts = [self.dsem() for _ in range(2)]
            yxs = [self.sb(s2, "yxs%d" % i, [128, 1024], BF16) for i in range(2)]
            yxss = [self.dsem() for _ in range(2)]
            ss2 = self.sb(s2, "ss2", [128, 2], F32)
            rs2 = self.sb(s2, "rs2", [128, 2], F32)
            junk = self.sb(s2, "junks", [128, 512], BF16)
            pS = self.ps(s2, "pS")
            pD = [self.ps(s2, "pD%d" % i) for i in range(4)]
            pCB = self.ps(s2, "pCB")
            pY = [self.ps(s2, "pY%d" % i) for i in range(2)]
            rel = [wz, wdt, Abc, dtb, Dsk, snb, c_one, c_eps, ST[0], ST[1], STbf, dt_, dtA, acs, ea, dend, dtot, R1, E, M, CBm, xdt, xdd,
                   silz, y_sb, tmp, ss2, rs2, junk, pS, pCB] + xt + bt + yB_sb + yst + yxs + pD + pY
            d0 = self.dsem(6)
            tr.dma("pool", d0, out=wz[:], in_=w_in_v[:, :, OFF_Z:OFF_Z + 1024], writes=[wz])
            tr.dma("pool", d0, out=wdt[:], in_=w_in_v[:, :, OFF_DT:OFF_DT + 32], writes=[wdt])
            tr.dma("sp", d0, out=Abc[:], in_=I["a_log"].t.partition_broadcast(128), writes=[Abc])
            tr.dma("sp", d0, out=dtb[:], in_=I["dt_bias"].t.partition_broadcast(128), writes=[dtb])
            tr.dma("sp", d0, out=Dsk[:], in_=I["ssd_d"].t.partition_broadcast(128), writes=[Dsk])
            tr.dma("sp", d0, out=snb[:], in_=I["ssd_norm"].t.partition_broadcast(128), writes=[snb])
            tr.op("pool", lambda e: e.memset(c_one[:], 1.0), writes=[c_one])
            tr.op("pool", lambda e: e.memset(c_eps[:], EPS), writes=[c_eps])
            tr.op("act", lambda e: e.activation(out=Abc[:], in_=Abc[:], func=AF.Exp), reads=[Abc], writes=[Abc])
            tr.op("dve", lambda e: e.tensor_scalar(out=Abc[:], in0=Abc[:], scalar1=-1.0, scalar2=None, op0=ALU.mult), reads=[Abc], writes=[Abc])
            for dd in range(2):
                tr.op("pool", lambda e, dd=dd: e.memset(ST[dd][:], 0.0), writes=[ST[dd]])
            Lm = [c["tri_gt"], c["tri_lt"]]
            Tc = [c["tri_le"], c["tri_ge"]]
            cnt = [0]

            QS = [[pS, pD[0], pD[1], pD[2]], [pD[3], pCB, pY[0], pY[1]]]
            ea2 = [ea, self.sb(s2, "ea_b", [128, 16], F32)]
            dtot2 = [dtot, self.sb(s2, "dtot_b", [128, 16], F32)]
            xdd2 = [xdd, self.sb(s2, "xdd_b", [128, 1024], BF16)]
            silz2 = [silz, self.sb(s2, "silz_b", [128, 1024], F32)]
            ysb2 = [y_sb, self.sb(s2, "y_sb_b", [128, 1024], F32)]
            tmp2 = [tmp, self.sb(s2, "ytmp_b", [128, 1024], F32)]
            ss22 = [ss2, self.sb(s2, "ss2_b", [128, 2], F32)]
            rs22 = [rs2, self.sb(s2, "rs2_b", [128, 2], F32)]
            junk2 = [junk, self.sb(s2, "junks_b", [128, 512], BF16)]
            dt2 = [dt_, self.sb(s2, "dt_b", [128, 16], F32)]
            dtA2 = [dtA, self.sb(s2, "dtA_b", [128, 16], F32)]
            acs2 = [acs, self.sb(s2, "acs_b", [128, 16], F32)]
            dend2 = [dend, self.sb(s2, "dend_b", [128, 16], F32)]
            xdt2 = [xdt, self.sb(s2, "xdt_b", [128, 1024], BF16)]
            Lmb = [self.sb(s2, "Lmb%d" % i, [128, 128], BF16) for i in range(2)]
            for i in range(2):
                tr.op("pool", lambda e, i=i: e.tensor_copy(out=Lmb[i][:], in_=Lm[i][:]), reads=[Lm[i]], writes=[Lmb[i]])
            rel += [dt2[1], dtA2[1], acs2[1], dend2[1], xdt2[1]] + Lmb
            rel += [ea2[1], dtot2[1], xdd2[1], silz2[1], ysb2[1], tmp2[1], ss22[1], rs22[1], junk2[1]]

            def ssd_tile(t, dd, full, sweepA, own_idx, seq):
                STd = ST[dd]
                k = seq % 2
                Q = QS[k]
                ea_, dtot_, xdd_, silz_ = ea2[k], dtot2[k], xdd2[k], silz2[k]
                y_sb, tmp, ss2, rs2, junk = ysb2[k], tmp2[k], ss22[k], rs22[k], junk2[k]
                dt_, dtA, acs, dend, xdt = dt2[k], dtA2[k], acs2[k], dend2[k], xdt2[k]
                x_t, b_t = xt[k], bt[k]
                tr.regroup(xts[k], 2)
                tr.dma("sp", xts[k], out=x_t[:], in_=xtok_v[t], writes=[x_t])
                tr.dma("sp", xts[k], out=b_t[:], in_=btok_v[t], writes=[b_t])
                hres = [c["hT_r"][t]]
                lhs = lambda kc: c["hT"][:, kc, t * 128:(t + 1) * 128]
                if full and sweepA:
                    for hh in range(2):
                        for kc in range(8):
                            tr.op("pe", lambda e, kc=kc, hh=hh: e.matmul(out=Q[1 + hh][:], lhsT=lhs(kc), rhs=wz[:, kc, hh * 512:(hh + 1) * 512], start=(kc == 0), stop=(kc == 7)),
                                  reads=hres + [wz], writes=[Q[1 + hh]])
                        tr.op("act", lambda e, hh=hh: e.activation(out=silz_[:, hh * 512:(hh + 1) * 512], in_=Q[1 + hh][:], func=AF.Silu), reads=[Q[1 + hh]], writes=[silz_])
                for kc in range(8):
                    tr.op("pe", lambda e, kc=kc: e.matmul(out=Q[0][:, 0:16], lhsT=lhs(kc), rhs=wdt[:, kc, dd * 16:(dd + 1) * 16], start=(kc == 0), stop=(kc == 7)),
                          reads=hres + [wdt], writes=[Q[0]])
                tr.op("dve", lambda e: e.tensor_tensor(out=dt_[:], in0=Q[0][:, 0:16], in1=dtb[:, dd * 16:(dd + 1) * 16], op=ALU.add), reads=[Q[0], dtb], writes=[dt_])
                tr.op("act", lambda e: e.activation(out=dt_[:], in_=dt_[:], func=AF.Exp), reads=[dt_], writes=[dt_])
                tr.op("act", lambda e: e.activation(out=dt_[:], in_=dt_[:], func=AF.Ln, bias=c_one[:]), reads=[dt_, c_one], writes=[dt_])
                tr.op("dve", lambda e: e.tensor_tensor(out=dtA[:], in0=dt_[:], in1=Abc[:, dd * 16:(dd + 1) * 16], op=ALU.mult), reads=[dt_, Abc], writes=[dtA])
                tr.op("pe", lambda e: e.matmul(out=Q[0][:, 16:32], lhsT=Tc[dd][:], rhs=dtA[:], start=True, stop=True), reads=[Tc[dd], dtA], writes=[Q[0]])
                tr.op("pe", lambda e: e.matmul(out=Q[0][:, 32:48], lhsT=c["ones_f"][:], rhs=dtA[:], start=True, stop=True), reads=[c["ones_f"], dtA], writes=[Q[0]])
                tr.op("dve", lambda e: e.tensor_copy(out=acs[:], in_=Q[0][:, 16:32]), reads=[Q[0]], writes=[acs])
                tr.op("dve", lambda e: e.tensor_tensor(out=dend[:], in0=Q[0][:, 32:48], in1=acs[:], op=ALU.subtract), reads=[Q[0], acs], writes=[dend])
                tr.op("act", lambda e: e.activation(out=dend[:], in_=dend[:], func=AF.Exp), reads=[dend], writes=[dend])
                tr.op("act", lambda e: e.activation(out=dtot_[:], in_=Q[0][:, 32:48], func=AF.Exp), reads=[Q[0]], writes=[dtot_])
                tr.op("dve", lambda e: e.tensor_tensor(out=xdt[:].rearrange("p (h q) -> p h q", h=16), in0=x_t[:].rearrange("p (h q) -> p h q", h=16),
                                                      in1=dt_[:].unsqueeze(2).to_broadcast([128, 16, 64]), op=ALU.mult), reads=[x_t, dt_], writes=[xdt])
                tr.op("pool", lambda e: e.tensor_tensor(out=xdd_[:].rearrange("p (h q) -> p h q", h=16), in0=xdt[:].rearrange("p (h q) -> p h q", h=16),
                                                       in1=dend[:].unsqueeze(2).to_broadcast([128, 16, 64]), op=ALU.mult), reads=[xdt, dend], writes=[xdd_])
                if full:
                    tok0 = (t - T_OWN0) * 128
                    Dbank = [Q[1], Q[2], Q[3], Q[1]]
                    tr.op("act", lambda e: e.activation(out=ea_[:], in_=acs[:], func=AF.Exp), reads=[acs], writes=[ea_])
                    tr.mark("need_r1")
                    tr.op("dve", lambda e: e.tensor_tensor(out=R1[:], in0=Tc[dd][:].unsqueeze(1).to_broadcast([128, 16, 128]),
                                                          in1=dtA[:].unsqueeze(2).to_broadcast([128, 16, 128]), op=ALU.mult), reads=[Tc[dd], dtA], writes=[R1])
                    for b4 in range(4):
                        tr.op("pe", lambda e, b4=b4: e.matmul(out=Dbank[b4][:], lhsT=Lmb[dd][:], rhs=R1[:, 4 * b4:4 * b4 + 4, :], start=True, stop=True),
                              reads=[Lmb[dd], R1], writes=[Dbank[b4]])
                        tr.op("act", lambda e, b4=b4: e.activation(out=E[:, 4 * b4:4 * b4 + 4, :], in_=Dbank[b4][:].rearrange("p (h i) -> p h i", h=4), func=AF.Exp), reads=[Dbank[b4]], writes=[E])
                    for g in range(2):
                        tr.op("pe", lambda e, g=g: e.matmul(out=Q[2][:, g * 128:(g + 1) * 128], lhsT=BT[:, g, tok0:tok0 + 128], rhs=CT[:, g, tok0:tok0 + 128], start=True, stop=True),
                              reads=[BT, CT], writes=[Q[2]])
                    tr.op("dve", lambda e: e.tensor_tensor(out=CBm[:], in0=Q[2][:, 0:256].rearrange("p (g i) -> p g i", g=2),
                                                          in1=Tc[dd][:].unsqueeze(1).to_broadcast([128, 2, 128]), op=ALU.mult), reads=[Q[2], Tc[dd]], writes=[CBm])
                    for g in range(2):
                        eng = "dve" if g == 0 else "pool"
                        tr.op(eng, lambda e, g=g: e.tensor_tensor(out=M[:, g * 8:(g + 1) * 8, :], in0=E[:, g * 8:(g + 1) * 8, :],
                                                                  in1=CBm[:, g:g + 1, :].to_broadcast([128, 8, 128]), op=ALU.mult), reads=[E, CBm], writes=[M])
                    Yb = [Q[3], Q[1]]
                    for h in range(16):
                        py = Yb[h // 8]
                        cs = (h % 8) * 64
                        tr.op("pe", lambda e, h=h, py=py, cs=cs: e.matmul(out=py[:, cs:cs + 64], lhsT=M[:, h, :], rhs=xdt[:, h * 64:(h + 1) * 64], start=True, stop=True),
                              reads=[M, xdt], writes=[py])
                    tr.mark("r1_done")
                tr.mark("need_state")
                Ob = [Q[2], Q[0]]
                if full:
                    tr.op("act", lambda e: e.activation(out=STbf[:].rearrange("p a b -> p (a b)"), in_=STd[:].rearrange("p a b -> p (a b)"), func=AF.Copy), reads=[STd], writes=[STbf])
                    for g in range(2):
                        tr.op("pe", lambda e, g=g: e.matmul(out=Ob[g][:], lhsT=CT[:, g, tok0:tok0 + 128], rhs=STbf[:, g, :], start=True, stop=True),
                              reads=[CT, STbf], writes=[Ob[g]])
                        tr.op("dve", lambda e, g=g: e.tensor_tensor(out=tmp[:, g * 512:(g + 1) * 512].rearrange("p (h q) -> p h q", h=8), in0=Ob[g][:].rearrange("p (h q) -> p h q", h=8),
                                                                    in1=ea_[:, g * 8:(g + 1) * 8].unsqueeze(2).to_broadcast([128, 8, 64]), op=ALU.mult), reads=[Ob[g], ea_], writes=[tmp])
                        tr.op("dve", lambda e, g=g: e.tensor_tensor(out=y_sb[:, g * 512:(g + 1) * 512], in0=Yb[g][:], in1=tmp[:, g * 512:(g + 1) * 512], op=ALU.add),
                              reads=[Yb[g], tmp], writes=[y_sb])
                for g in range(2):
                    tr.op("pe", lambda e, g=g: e.matmul(out=Ob[g][:], lhsT=b_t[:, g * 128:(g + 1) * 128], rhs=xdd_[:, g * 512:(g + 1) * 512], start=True, stop=True),
                          reads=[b_t, xdd_], writes=[Ob[g]])
                    tr.op("dve", lambda e, g=g: e.tensor_tensor(out=STd[:, g, :].rearrange("p (h q) -> p h q", h=8), in0=STd[:, g, :].rearrange("p (h q) -> p h q", h=8),
                                                                in1=dtot_[:, g * 8:(g + 1) * 8].unsqueeze(2).to_broadcast([128, 8, 64]), op=ALU.mult), reads=[STd, dtot_], writes=[STd])
                    tr.op("dve", lambda e, g=g: e.tensor_tensor(out=STd[:, g, :], in0=Ob[g][:], in1=STd[:, g, :], op=ALU.add), reads=[Ob[g], STd], writes=[STd])
                tr.mark("state_done")
                if not full:
                    return
                if not sweepA:
                    ys = yst[own_idx % 2]
                    tr.op("act", lambda e: e.activation(out=ys[:], in_=y_sb[:], func=AF.Copy), reads=[y_sb], writes=[ys])
                    tr.dma("sp", ysts[own_idx % 2], out=yB_v[own_idx], in_=ys[:], reads=[ys], writes=[])
                    return
                yb = yB_sb[own_idx % 2]
                tr.dma("sp", yBs[own_idx % 2], out=yb[:], in_=yB_v[own_idx], writes=[yb])
                tr.op("dve", lambda e: e.tensor_tensor(out=y_sb[:], in0=y_sb[:], in1=yb[:], op=ALU.add), reads=[y_sb, yb], writes=[y_sb])
                tr.op("pool", lambda e: e.tensor_tensor(out=tmp[:].rearrange("p (h q) -> p h q", h=16), in0=x_t[:].rearrange("p (h q) -> p h q", h=16),
                                                       in1=Dsk[:].unsqueeze(2).to_broadcast([128, 16, 64]), op=ALU.mult), reads=[x_t, Dsk], writes=[tmp])
                tr.op("dve", lambda e: e.tensor_tensor(out=y_sb[:], in0=y_sb[:], in1=tmp[:], op=ALU.add), reads=[y_sb, tmp], writes=[y_sb])
                tr.op("dve", lambda e: e.tensor_tensor(out=y_sb[:], in0=y_sb[:], in1=silz_[:], op=ALU.mult), reads=[y_sb, silz_], writes=[y_sb])
                for g in range(2):
                    tr.op("act", lambda e, g=g: e.activation(out=junk[:], in_=y_sb[:, g * 512:(g + 1) * 512], func=AF.Square, accum_out=ss2[:, g:g + 1]), reads=[y_sb], writes=[junk, ss2])
                tr.op("act", lambda e: e.activation(out=rs2[:], in_=ss2[:], func=AF.Ln, scale=1.0 / 512.0, bias=c_eps[:]), reads=[ss2, c_eps], writes=[rs2])
                tr.op("act", lambda e: e.activation(out=rs2[:], in_=rs2[:], func=AF.Exp, scale=-0.5), reads=[rs2], writes=[rs2])
                tr.op("dve", lambda e: e.tensor_tensor(out=y_sb[:].rearrange("p (g q) -> p g q", g=2), in0=y_sb[:].rearrange("p (g q) -> p g q", g=2),
                                                      in1=rs2[:].unsqueeze(2).to_broadcast([128, 2, 512]), op=ALU.mult), reads=[y_sb, rs2], writes=[y_sb])
                yo = yxs[own_idx % 2]
                tr.op("pool", lambda e: e.tensor_tensor(out=yo[:], in0=y_sb[:], in1=snb[:], op=ALU.mult), reads=[y_sb, snb], writes=[yo])
                tr.dma("sp", yxss[own_idx % 2], out=yx_v[own_idx][:, 1024:2048], in_=yo[:], reads=[yo], writes=[])

            def sweep(tiles):
                recs = []
                for seq, (t, dd, full, sweepA, own_idx) in enumerate(tiles):
                    tr.begin_record()
                    ssd_tile(t, dd, full, sweepA, own_idx, seq)
                    recs.append(tr.end_record())
                tr.run_pipelined(recs, depth=2)

            sweep([(t, 1, False, False, None) for t in (1, 0)])
            self.tap("sS_B", ST[1][:].rearrange("p a b -> p (a b)"), [128, 1024], F32, [ST[1]])
            sweep([(t, 1, False, False, None) for t in range(NT - 1, T_OTH0 - 1, -1)] +
                  [(t, 1, True, False, t - T_OWN0) for t in range(T_OTH0 - 1, T_OWN0 - 1, -1)])
            for e in Tracker.ENG:
                tr.wait_all(e, yst)
            sweep([(t, 0, False, True, None) for t in (0, 1)])
            self.tap("sS_A", ST[0][:].rearrange("p a b -> p (a b)"), [128, 1024], F32, [ST[0]])
            sweep([(t, 0, True, True, t - T_OWN0) for t in range(T_OWN0, T_OTH0)])
            for e in Tracker.ENG:
                tr.wait_all(e, yxs)
            self.barrier_release(rel)

    def stage_post(self, st):
        tr, c, I = self.tr, self.c, self.I
        self.fence()
        c["h_lat"] = self.sb(st, "h_lat", [128, 16, D], F32)
        c["h_r"] = [Res("h_lat%d" % i) for i in range(16)]
        c["h2T"] = self.sb(st, "h2T", [128, 8, NOWN], BF16)
        c["h2_r"] = [Res("h2T%d" % i) for i in range(16)]
        c["comb"] = self.sb(st, "comb", [128, 16, 32], F32)
        yx_v = c["yx"].t.rearrange("(n p) c -> n p c", p=128)
        h_lat, h2T = c["h_lat"], c["h2T"]
        with ExitStack() as s2:
            wo = self.sb(s2, "wo", [128, 16, D], BF16)
            wr = self.sb(s2, "wr", [128, 8, 36], F32)
            brr = self.sb(s2, "brr", [1, 36], F32)
            c_eps = self.sb(s2, "c_eps3", [128, 1], F32)
            lg = self.sb(s2, "lg", [128, 16, 36], F32)
            yxt = [self.sb(s2, "yxt%d" % i, [128, 2048], BF16) for i in range(2)]
            yxs = [self.dsem() for _ in range(2)]
            xr = [self.sb(s2, "xr2_%d" % i, [128, D], F32) for i in range(2)]
            xrs = [self.dsem() for _ in range(2)]
            junk = self.sb(s2, "pjunk", [128, D], BF16)
            PS = []
            for par in range(2):
                PS.append({"yxT": self.sb(s2, "yxT%d" % par, [128, 16, 128], BF16), "tmp": self.sb(s2, "ptmp%d" % par, [128, D], F32),
                           "h2f": self.sb(s2, "h2f%d" % par, [128, 8, 128], F32), "ss": self.sb(s2, "pss%d" % par, [128, 1], F32),
                           "rs": self.sb(s2, "prs%d" % par, [128, 1], F32), "B": [self.ps(s2, "ppB%d_%d" % (par, i)) for i in range(4)]})
            rel = [wo, wr, brr, c_eps, lg, junk] + yxt + xr
            for p_ in PS:
                rel += [p_["yxT"], p_["tmp"], p_["h2f"], p_["ss"], p_["rs"]] + p_["B"]
            w_out_v = I["w_out"].t.rearrange("(kc p) n -> p kc n", p=128)
            d0 = self.dsem(2)
            wst = [self.sb(s2, "wst%d" % i, [128, 1, D], F32) for i in range(2)]
            rel += wst
            wsts = [self.dsem() for _ in range(2)]
            for q in range(16):
                tr.dma("sp", wsts[q % 2], out=wst[q % 2][:], in_=w_out_v[:, q:q + 1, :], writes=[wst[q % 2]])
                tr.op("pool", lambda e, q=q: e.tensor_copy(out=wo[:, q:q + 1, :], in_=wst[q % 2][:]), reads=[wst[q % 2]], writes=[wo])
            tr.dma("sp", d0, out=wr[:].rearrange("p a b -> p (a b)"), in_=I["w_router"].t, writes=[wr])
            tr.dma("sp", d0, out=brr[:], in_=I["b_router"].t, writes=[brr])
            tr.op("pool", lambda e: e.memset(c_eps[:], EPS), writes=[c_eps])
            def post_tile(i):
                y_t, x_t = yxt[i % 2], xr[i % 2]
                p_ = PS[i % 2]
                yxT, tmp, h2f, ss, rs, B = p_["yxT"], p_["tmp"], p_["h2f"], p_["ss"], p_["rs"], p_["B"]
                hn = tmp
                pT = [B[0][:].bitcast(BF16), B[1][:].bitcast(BF16)]
                tr.dma("sp", yxs[i % 2], out=y_t[:], in_=yx_v[i], writes=[y_t])
                tr.dma("sp", xrs[i % 2], out=x_t[:], in_=I["xs"].t[NCTX + i * 128: NCTX + (i + 1) * 128, :], writes=[x_t])
                for kc in range(16):
                    tr.op("pe", lambda e, kc=kc: e.transpose(out=pT[kc // 8][:, (kc % 8) * 128:(kc % 8 + 1) * 128], in_=y_t[:, kc * 128:(kc + 1) * 128], identity=c["ident_b"][:]),
                          reads=[y_t, c["ident_b"]], writes=[B[kc // 8]])
                tr.op("act", lambda e: e.activation(out=yxT[:, 0:8, :].rearrange("p a b -> p (a b)"), in_=pT[0], func=AF.Copy), reads=[B[0]], writes=[yxT])
                tr.op("dve", lambda e: e.tensor_copy(out=yxT[:, 8:16, :].rearrange("p a b -> p (a b)"), in_=pT[1]), reads=[B[1]], writes=[yxT])
                hl = h_lat[:, i, :]
                for hh in range(2):
                    for kc in range(16):
                        tr.op("pe", lambda e, kc=kc, hh=hh: e.matmul(out=B[2 + hh][:], lhsT=yxT[:, kc, :], rhs=wo[:, kc, hh * 512:(hh + 1) * 512], start=(kc == 0), stop=(kc == 15)),
                              reads=[yxT, wo], writes=[B[2 + hh]])
                    tr.op("dve", lambda e, hh=hh: e.tensor_tensor(out=tmp[:, hh * 512:(hh + 1) * 512], in0=B[2 + hh][:], in1=c["g1_bc"][:, hh * 512:(hh + 1) * 512], op=ALU.mult),
                          reads=[B[2 + hh], c["g1_bc"]], writes=[tmp])
                tr.op("pool", lambda e: e.tensor_tensor(out=hl, in0=tmp[:], in1=x_t[:], op=ALU.add), reads=[tmp, x_t], writes=[c["h_r"][i]])
                tr.op("act", lambda e: e.activation(out=junk[:], in_=hl, func=AF.Square, accum_out=ss[:]), reads=[c["h_r"][i]], writes=[junk, ss])
                tr.op("act", lambda e: e.activation(out=rs[:], in_=ss[:], func=AF.Ln, scale=1.0 / D, bias=c_eps[:]), reads=[ss, c_eps], writes=[rs])
                tr.op("act", lambda e: e.activation(out=rs[:], in_=rs[:], func=AF.Exp, scale=-0.5), reads=[rs], writes=[rs])
                tr.op("dve", lambda e: e.tensor_scalar(out=hn[:], in0=hl, scalar1=rs[:], scalar2=None, op0=ALU.mult), reads=[c["h_r"][i], rs], writes=[hn])
                for kc in range(8):
                    tr.op("pe", lambda e, kc=kc: e.transpose(out=B[kc // 4][:, (kc % 4) * 128:(kc % 4 + 1) * 128], in_=hn[:, kc * 128:(kc + 1) * 128], identity=c["ident_f"][:]),
                          reads=[hn, c["ident_f"]], writes=[B[kc // 4]])
                for q in range(2):
                    tr.op("dve", lambda e, q=q: e.tensor_tensor(out=h2f[:, q * 4:(q + 1) * 4, :], in0=B[q][:].rearrange("p (k t) -> p k t", k=4),
                                                               in1=c["s2"][:, q * 4:(q + 1) * 4].unsqueeze(2).to_broadcast([128, 4, 128]), op=ALU.mult), reads=[B[q], c["s2"]], writes=[h2f])
                tr.op("pool", lambda e: e.tensor_tensor(out=h2f[:], in0=h2f[:], in1=c["b2"][:].unsqueeze(2).to_broadcast([128, 8, 128]), op=ALU.add), reads=[h2f, c["b2"]], writes=[h2f])
                tr.op("act", lambda e: e.activation(out=h2T[:, :, i * 128:(i + 1) * 128], in_=h2f[:], func=AF.Copy), reads=[h2f], writes=[c["h2_r"][i]])
                for kc in range(8):
                    tr.op("pe", lambda e, kc=kc: e.matmul(out=B[2][:, 0:36], lhsT=h2f[:, kc, :], rhs=wr[:, kc, :], start=(kc == 0), stop=False), reads=[h2f, wr], writes=[B[2]])
                tr.op("pe", lambda e: e.matmul(out=B[2][:, 0:36], lhsT=c["ones_f"][0:1, :], rhs=brr[0:1, :], start=False, stop=True), reads=[c["ones_f"], brr], writes=[B[2]])
                tr.op("dve", lambda e: e.tensor_copy(out=lg[:, i, :], in_=B[2][:, 0:36]), reads=[B[2]], writes=[lg])

            recs = []
            for i in range(16):
                tr.begin_record()
                post_tile(i)
                recs.append(tr.end_record())
            tr.run_pipelined(recs, depth=2)
            self.tap("lg", lg[:].rearrange("p a b -> p (a b)"), [128, 16 * 36], F32, [lg])
            self.tap("h_lat", h_lat[:].rearrange("p a b -> p (a b)"), [128, 16 * D], F32, c["h_r"])
            def T(name, shape):
                t_ = self.sb(s2, name, shape, F32)
                rel.append(t_)
                return t_
            gmax = T("gmax", [128, 16]); mg = T("mg", [128, 16, 4]); eg = T("eg", [128, 16, 4]); gsum = T("gsum", [128, 16]); pg = T("pg", [128, 16])
            t48 = T("t48", [128, 16, 4, 8]); ein = T("ein", [128, 16, 8]); m1 = T("m1", [128, 16]); k1 = T("k1", [128, 16, 8]); e2 = T("e2", [128, 16, 8])
            m2 = T("m2", [128, 16]); k2 = T("k2", [128, 16, 8]); dd_ = T("dd_", [128, 16]); w1 = T("w1", [128, 16]); w2 = T("w2", [128, 16]); cw8 = T("cw8", [128, 16, 8])
            gl = lg[:, :, 0:4]
            el = lg[:, :, 4:36].rearrange("p t (g x) -> p t g x", g=4)
            V = lambda fn, r, w: tr.op("dve", fn, reads=r, writes=w)
            V(lambda e: e.tensor_reduce(out=gmax[:], in_=gl, axis=AX.X, op=ALU.max), [lg], [gmax])
            V(lambda e: e.tensor_tensor(out=mg[:], in0=gl, in1=gmax[:].unsqueeze(2).to_broadcast([128, 16, 4]), op=ALU.is_equal), [lg, gmax], [mg])
            V(lambda e: e.tensor_tensor(out=eg[:], in0=gl, in1=gmax[:].unsqueeze(2).to_broadcast([128, 16, 4]), op=ALU.subtract), [lg, gmax], [eg])
            tr.op("act", lambda e: e.activation(out=eg[:], in_=eg[:], func=AF.Exp), reads=[eg], writes=[eg])
            V(lambda e: e.tensor_reduce(out=gsum[:], in_=eg[:], axis=AX.X, op=ALU.add), [eg], [gsum])
            V(lambda e: e.reciprocal(out=pg[:], in_=gsum[:]), [gsum], [pg])
            V(lambda e: e.tensor_tensor(out=t48[:], in0=el, in1=mg[:].unsqueeze(3).to_broadcast([128, 16, 4, 8]), op=ALU.mult), [lg, mg], [t48])
            V(lambda e: e.tensor_reduce(out=ein[:], in_=t48[:].rearrange("p t g x -> p t x g"), axis=AX.X, op=ALU.add), [t48], [ein])
            V(lambda e: e.tensor_reduce(out=m1[:], in_=ein[:], axis=AX.X, op=ALU.max), [ein], [m1])
            V(lambda e: e.tensor_tensor(out=k1[:], in0=ein[:], in1=m1[:].unsqueeze(2).to_broadcast([128, 16, 8]), op=ALU.is_equal), [ein, m1], [k1])
            V(lambda e: e.scalar_tensor_tensor(out=e2[:], in0=k1[:], scalar=-1.0e30, in1=ein[:], op0=ALU.mult, op1=ALU.add), [k1, ein], [e2])
            V(lambda e: e.tensor_reduce(out=m2[:], in_=e2[:], axis=AX.X, op=ALU.max), [e2], [m2])
            V(lambda e: e.tensor_tensor(out=k2[:], in0=e2[:], in1=m2[:].unsqueeze(2).to_broadcast([128, 16, 8]), op=ALU.is_equal), [e2, m2], [k2])
            V(lambda e: e.tensor_tensor(out=dd_[:], in0=m2[:], in1=m1[:], op=ALU.subtract), [m1, m2], [dd_])
            tr.op("act", lambda e: e.activation(out=dd_[:], in_=dd_[:], func=AF.Exp), reads=[dd_], writes=[dd_])
            V(lambda e: e.tensor_scalar(out=w1[:], in0=dd_[:], scalar1=1.0, scalar2=None, op0=ALU.add), [dd_], [w1])
            V(lambda e: e.reciprocal(out=w1[:], in_=w1[:]), [w1], [w1])
            V(lambda e: e.tensor_tensor(out=w2[:], in0=dd_[:], in1=w1[:], op=ALU.mult), [dd_, w1], [w2])
            V(lambda e: e.tensor_tensor(out=w1[:], in0=w1[:], in1=pg[:], op=ALU.mult), [w1, pg], [w1])
            V(lambda e: e.tensor_tensor(out=w2[:], in0=w2[:], in1=pg[:], op=ALU.mult), [w2, pg], [w2])
            V(lambda e: e.tensor_tensor(out=k1[:], in0=k1[:], in1=w1[:].unsqueeze(2).to_broadcast([128, 16, 8]), op=ALU.mult), [k1, w1], [k1])
            V(lambda e: e.tensor_tensor(out=k2[:], in0=k2[:], in1=w2[:].unsqueeze(2).to_broadcast([128, 16, 8]), op=ALU.mult), [k2, w2], [k2])
            V(lambda e: e.tensor_tensor(out=cw8[:], in0=k1[:], in1=k2[:], op=ALU.add), [k1, k2], [cw8])
            V(lambda e: e.tensor_tensor(out=c["comb"][:].rearrange("p t (g x) -> p t g x", g=4), in0=mg[:].unsqueeze(3).to_broadcast([128, 16, 4, 8]),
                                        in1=cw8[:].unsqueeze(2).to_broadcast([128, 16, 4, 8]), op=ALU.mult), [mg, cw8], [c["comb"]])
            self.tap("comb", c["comb"][:].rearrange("p a b -> p (a b)"), [128, 512], F32, [c["comb"]])
            self.barrier_release(rel)

    def stage_moe(self, st):
        tr, c, I = self.tr, self.c, self.I
        self.fence()
        h_lat, h2T, comb = c["h_lat"], c["h2T"], c["comb"]
        with ExitStack() as s2:
            wgt = [self.sb(s2, "mwg%d" % i, [128, 8, DFF], BF16) for i in range(2)]
            wut = [self.sb(s2, "mwu%d" % i, [128, 8, DFF], BF16) for i in range(2)]
            wdt = [self.sb(s2, "mwd%d" % i, [128, 4, D], BF16) for i in range(2)]
            stg = [self.sb(s2, "mstg%d" % i, [128, 8, DFF], F32) for i in range(2)]
            stgs = [self.dsem() for _ in range(2)]
            ns = [0]
            sg_ = [self.sb(s2, "msg%d" % i, [128, 512], F32) for i in range(2)]
            heT = [self.sb(s2, "heT%d" % i, [128, 4, 512], BF16) for i in range(2)]
            pG = [self.ps(s2, "mpG%d" % i) for i in range(2)]
            pU = [self.ps(s2, "mpU%d" % i) for i in range(2)]
            pDn = [self.ps(s2, "mpD%d" % i) for i in range(4)]
            rel = wgt + wut + wdt + sg_ + heT + pG + pU + pDn + stg
            nb = 0
            pending = [None]

            def emit_down(he, wd_e, j, ex):
                for tt in range(4):
                    ti = j * 4 + tt
                    for hh in range(2):
                        d_p = pDn[(tt * 2 + hh) % 4]
                        for fc in range(4):
                            tr.op("pe", lambda e, fc=fc: e.matmul(out=d_p[:], lhsT=he[:, fc, tt * 128:(tt + 1) * 128], rhs=wd_e[:, fc, hh * 512:(hh + 1) * 512],
                                                                  start=(fc == 0), stop=(fc == 3)), reads=[he, wd_e], writes=[d_p])
                        tr.op("dve", lambda e: e.scalar_tensor_tensor(
                            out=h_lat[:, ti, hh * 512:(hh + 1) * 512], in0=d_p[:], scalar=comb[:, ti, ex:ex + 1], in1=h_lat[:, ti, hh * 512:(hh + 1) * 512], op0=ALU.mult, op1=ALU.add),
                            reads=[d_p, comb, c["h_r"][ti]], writes=[c["h_r"][ti]])

            for ex in range(NEXP):
                k = ex % 2
                wg_e, wu_e, wd_e = wgt[k], wut[k], wdt[k]
                for (dst, src) in ((wg_e, I["w_gate"].t[ex].rearrange("(kc p) n -> p kc n", p=128)), (wu_e, I["w_up"].t[ex].rearrange("(kc p) n -> p kc n", p=128))):
                    sg_t, sg_s = stg[ns[0] % 2], stgs[ns[0] % 2]
                    ns[0] += 1
                    tr.dma("sp", sg_s, out=sg_t[:], in_=src, writes=[sg_t])
                    tr.op("pool", lambda e, dst=dst, sg_t=sg_t: e.tensor_copy(out=dst[:], in_=sg_t[:]), reads=[sg_t], writes=[dst])
                sg_t, sg_s = stg[ns[0] % 2], stgs[ns[0] % 2]
                ns[0] += 1
                sv = sg_t[:].rearrange("p a b -> p (a b)").rearrange("p (f n) -> p f n", f=4)
                tr.dma("sp", sg_s, out=sv, in_=I["w_down"].t[ex].rearrange("(fc p) n -> p fc n", p=128), writes=[sg_t])
                tr.op("pool", lambda e, wd_e=wd_e, sv=sv: e.tensor_tensor(out=wd_e[:], in0=sv, in1=c["g2_bc"][:].unsqueeze(1).to_broadcast([128, 4, D]), op=ALU.mult),
                      reads=[sg_t, c["g2_bc"]], writes=[wd_e])
                for j in range(4):
                    he = heT[nb % 2]
                    nb += 1
                    hres = [c["h2_r"][j * 4 + q] for q in range(4)]
                    for fc in range(4):
                        g_p, u_p, sg = pG[fc % 2], pU[fc % 2], sg_[fc % 2]
                        for kc in range(8):
                            tr.op("pe", lambda e, kc=kc, fc=fc, g_p=g_p: e.matmul(out=g_p[:], lhsT=wg_e[:, kc, fc * 128:(fc + 1) * 128], rhs=h2T[:, kc, j * 512:(j + 1) * 512],
                                                                               start=(kc == 0), stop=(kc == 7)), reads=[wg_e] + hres, writes=[g_p])
                        for kc in range(8):
                            tr.op("pe", lambda e, kc=kc, fc=fc, u_p=u_p: e.matmul(out=u_p[:], lhsT=wu_e[:, kc, fc * 128:(fc + 1) * 128], rhs=h2T[:, kc, j * 512:(j + 1) * 512],
                                                                               start=(kc == 0), stop=(kc == 7)), reads=[wu_e] + hres, writes=[u_p])
                        tr.op("act", lambda e, g_p=g_p, sg=sg: e.activation(out=sg[:], in_=g_p[:], func=AF.Silu), reads=[g_p], writes=[sg])
                        tr.op("dve", lambda e, u_p=u_p, sg=sg, fc=fc, he=he: e.tensor_tensor(out=he[:, fc, :], in0=u_p[:], in1=sg[:], op=ALU.mult), reads=[u_p, sg], writes=[he])
                    if pending[0] is not None:
                        emit_down(*pending[0])
                    pending[0] = (he, wd_e, j, ex)
            emit_down(*pending[0])
            self.tap("h_fin", h_lat[:].rearrange("p a b -> p (a b)"), [128, 16 * D], F32, c["h_r"])
            self.barrier_release(rel)

    def stage_final(self, st):
        tr, c, I = self.tr, self.c, self.I
        self.fence()
        h_lat = c["h_lat"]
        out_v = self.out.t.rearrange("(n p) c -> n p c", p=128)
        with ExitStack() as s2:
            fn = self.sb(s2, "fn_bc", [128, D], F32)
            c_eps = self.sb(s2, "c_eps4", [128, 1], F32)
            junk = self.sb(s2, "fjunk", [128, D], BF16)
            ss = [self.sb(s2, "fss%d" % i, [128, 1], F32) for i in range(2)]
            rs = [self.sb(s2, "frs%d" % i, [128, 1], F32) for i in range(2)]
            ob = [self.sb(s2, "fob%d" % i, [128, D], F32) for i in range(2)]
            obs = [self.dsem() for _ in range(2)]
            tr.dma("sp", self.dsem(), out=fn[:], in_=I["final_norm"].t.partition_broadcast(128), writes=[fn])
            tr.op("pool", lambda e: e.memset(c_eps[:], EPS), writes=[c_eps])
            for i in range(16):
                hl = h_lat[:, i, :]
                s_, r_, o_ = ss[i % 2], rs[i % 2], ob[i % 2]
                tr.op("act", lambda e, s_=s_, hl=hl: e.activation(out=junk[:], in_=hl, func=AF.Square, accum_out=s_[:]), reads=[c["h_r"][i]], writes=[junk, s_])
                tr.op("act", lambda e, s_=s_, r_=r_: e.activation(out=r_[:], in_=s_[:], func=AF.Ln, scale=1.0 / D, bias=c_eps[:]), reads=[s_, c_eps], writes=[r_])
                tr.op("act", lambda e, r_=r_: e.activation(out=r_[:], in_=r_[:], func=AF.Exp, scale=-0.5), reads=[r_], writes=[r_])
                tr.op("dve", lambda e, r_=r_, o_=o_, hl=hl: e.scalar_tensor_tensor(out=o_[:], in0=hl, scalar=r_[:], in1=fn[:], op0=ALU.mult, op1=ALU.mult),
                      reads=[c["h_r"][i], r_, fn], writes=[o_])
                tr.dma("sp", obs[i % 2], out=out_v[i], in_=o_[:], reads=[o_], writes=[])
            self.final += ob


def prep_core(inp, b, hf):
    L = 0
    rev = hf == 1
    x, ctx = inp["x"][b], inp["ctx"][b]
    if not rev:
        ctx_a, own, oth = ctx, x[0:2048], x[2048:4096]
        dA, dB = 0, 1
    else:
        ctx_a, own, oth = ctx[::-1], x[2048:4096][::-1], x[0:2048][::-1]
        dA, dB = 1, 0
    m = {}
    m["xs"] = np.ascontiguousarray(np.concatenate([ctx_a, own, oth], axis=0), dtype=np.float32)
    cT = np.stack([inp["c"][b].reshape(8, 128).T, inp["c_ctx"].reshape(8, 128).T], axis=2).reshape(128, 16)
    m["cT"] = np.ascontiguousarray(cT, dtype=np.float32)
    m["w_ada"] = np.ascontiguousarray(inp["w_ada"][L])
    m["b_ada"] = np.ascontiguousarray(inp["b_ada"][L].reshape(1, -1))
    m["norm_mix_fm"] = np.ascontiguousarray(inp["norm_mix"][L].reshape(8, 128).T)
    m["norm_ffn_fm"] = np.ascontiguousarray(inp["norm_ffn"][L].reshape(8, 128).T)
    w_in = inp["w_in"][L]
    if rev:
        w_in = np.concatenate([w_in[:, :OFF_G], w_in[:, OFF_G + 16:OFF_G + 32], w_in[:, OFF_G:OFF_G + 16],
                               w_in[:, OFF_Z:OFF_DT], w_in[:, OFF_DT + 16:OFF_DT + 32], w_in[:, OFF_DT:OFF_DT + 16]], axis=1)
    m["w_in"] = np.ascontiguousarray(w_in)
    wu, gb = inp["gla_w_up"][L], inp["gla_b"][L]
    m["w_up_aug"] = np.ascontiguousarray(np.stack([np.concatenate([wu[dA], gb[dA][None, :]], axis=0),
                                                   np.concatenate([wu[dB], gb[dB][None, :]], axis=0)], axis=0))
    m["gla_norm"] = np.ascontiguousarray(inp["gla_norm"][L].reshape(1, -1))
    m["ssd_norm"] = np.ascontiguousarray(inp["ssd_norm"][L].reshape(1, -1))
    m["final_norm"] = np.ascontiguousarray(inp["final_norm"].reshape(1, -1))
    cw = inp["ssd_conv_w"][L]
    if rev:
        cw = cw[::-1, ::-1, :]
    m["conv_w_fm"] = np.ascontiguousarray(cw.reshape(9, 12, 128).transpose(2, 1, 0))
    m["conv_b_fm"] = np.ascontiguousarray(inp["ssd_conv_b"][L].reshape(12, 128).T)
    m["dt_bias"] = np.ascontiguousarray(np.concatenate([inp["ssd_dt_bias"][L][dA], inp["ssd_dt_bias"][L][dB]]).reshape(1, 32))
    m["a_log"] = np.ascontiguousarray(np.concatenate([inp["ssd_a_log"][L][dA], inp["ssd_a_log"][L][dB]]).reshape(1, 32))
    m["ssd_d"] = np.ascontiguousarray(inp["ssd_d"][L].reshape(1, 16))
    m["w_out"] = np.ascontiguousarray(inp["w_out"][L])
    wrt = np.concatenate([inp["router_group_w"][L], inp["router_expert_w"][L]], axis=1)
    m["w_router"] = np.ascontiguousarray(wrt.reshape(8, 128, 36).transpose(1, 0, 2).reshape(128, 8 * 36))
    m["b_router"] = np.ascontiguousarray(np.concatenate([inp["router_group_b"][L], inp["router_expert_b"][L]]).reshape(1, 36))
    m["w_gate"] = np.ascontiguousarray(inp["expert_w_gate"][L])
    m["w_up"] = np.ascontiguousarray(inp["expert_w_up"][L])
    m["w_down"] = np.ascontiguousarray(inp["expert_w_down"][L])
    return {k: np.asarray(v, dtype=np.float32) for k, v in m.items()}


def run(inputs, debug=None, stop_after=None, cores=8):
    bld = Builder(debug=debug, stop_after=stop_after)
    nc = bld.build()
    in_maps = [prep_core(inputs, i // 2, i % 2) for i in range(cores)]
    res = run_bass_kernel_spmd(nc, in_maps, core_ids=list(range(cores)))
    return res, bld


def kernel(**inputs):
    inputs = {k: np.asarray(v) for k, v in inputs.items()}
    res, _ = run(inputs)
    out = np.empty((4, 4096, D), dtype=np.float32)
    for i in range(8):
        b, hf = i // 2, i % 2
        o = np.asarray(res.results[i]["out"], dtype=np.float32)
        if hf == 0:
            out[b, 0:2048] = o
        else:
            out[b, 2048:4096] = o[::-1]
    return out
```

```python
import math
from contextlib import ExitStack

import numpy as np
import concourse.bass as bass
import concourse.mybir as mybir
from concourse.bass_utils import run_bass_kernel_spmd

F32 = mybir.dt.float32
BF16 = mybir.dt.bfloat16
AF = mybir.ActivationFunctionType
ALU = mybir.AluOpType
AX = mybir.AxisListType

D = 1024
NCTX, NOWN, NOTH = 256, 2048, 2048
TOK = NCTX + NOWN + NOTH
NT = TOK // 128
T_CTX0, T_OWN0, T_OTH0 = 0, 2, 18
EPS = 1e-6
IN_W = 5696
OFF_K, OFF_V, OFF_R, OFF_G, OFF_Z, OFF_XBC, OFF_DT = 512, 1024, 2048, 3072, 3104, 4128, 5664
NEXP, DFF = 32, 512


class Res:
    __slots__ = ("name", "lw", "rd")

    def __init__(self, name=""):
        self.name = name
        self.lw = None
        self.rd = {}


class Tile:
    def __init__(self, t, name):
        self.t = t
        self.r = Res(name)

    def __getitem__(self, idx):
        return self.t[idx]


class Tracker:
    ENG = ("pe", "act", "dve", "pool", "sp")
    CH = 2000

    def __init__(self, nc, sems, dma_sems, same_engine_sync=True):
        self.nc = nc
        self.eng = {"pe": nc.tensor, "act": nc.scalar, "dve": nc.vector, "pool": nc.gpsimd, "sp": nc.sync}
        self.cnt = {e: 0 for e in self.ENG}
        self.waited = {e: {} for e in self.ENG}
        self.sems = {e: [sems[e]] for e in sems}
        self.free_dma = list(dma_sems)
        self.same = same_engine_sync
        self.ninst = 0
        self.rec = None

    def new_dma_sem(self, group=0):
        d = self.free_dma.pop()
        self._uid = getattr(self, "_uid", 0) + 1
        d = d if isinstance(d, list) else [d, 0, 0, 0, "dma%d" % self._uid]
        if group:
            d[2] = group
            d[3] = d[1] + 16 * group
        return d

    def regroup(self, d, n):
        if self.rec is not None:
            self.rec.append(("call", lambda: self.regroup(d, n)))
            return
        assert d[2] == 0
        d[2] = n
        d[3] = d[1] + 16 * n

    def begin_record(self):
        self.rec = []

    def end_record(self):
        r, self.rec = self.rec, None
        return r

    def mark(self, name):
        self.rec.append(("mark", name))

    def _emit_item(self, it):
        if it[0] == "op":
            self.op(*it[1:])
        elif it[0] == "dma":
            self.dma(*it[1:])
        elif it[0] == "call":
            it[1]()

    def run_pipelined(self, records, depth=2, serial_fronts=False):
        assert self.rec is None
        active = []
        nxt = 0
        done = {}
        fin = -1

        def released(name, idx):
            return max(done.get(name, -1), fin) >= idx - 1 or idx == 0

        while active or nxt < len(records):
            while len(active) < depth and nxt < len(records):
                if serial_fronts and active and not active[-1][3]:
                    break
                if active and min(x[2] for x in active) <= nxt - depth:
                    break
                active.append([records[nxt], 0, nxt, False])
                nxt += 1
            progressed = False
            for a in list(active):
                lst, pos, idx, _ = a
                if pos >= len(lst):
                    a[3] = True
                    active.remove(a)
                    fin = max(fin, idx) if all(x[2] > idx for x in active) else fin
                    for nm in list(done.keys()):
                        done[nm] = max(done[nm], idx) if done[nm] >= idx - 1 else done[nm]
                    progressed = True
                    continue
                it = lst[pos]
                if it[0] == "mark":
                    nm = it[1]
                    if nm.startswith("need_"):
                        sec = nm[5:]
                        if sec == "state":
                            a[3] = True
                        if not released(sec, idx):
                            continue
                    elif nm.endswith("_done"):
                        sec = nm[:-5]
                        done[sec] = max(done.get(sec, -1), idx)
                    a[1] += 1
                    progressed = True
                    continue
                self._emit_item(it)
                a[1] += 1
                progressed = True
            if not progressed:
                for a in active:
                    it = a[0][a[1]]
                    if it[0] == "mark" and it[1].startswith("need_"):
                        done[it[1][5:]] = max(done.get(it[1][5:], -1), a[2] - 1)
                        progressed = True
                assert progressed

    def release_dma_sem(self, d):
        self.free_dma.append(d)

    def _wait(self, e, ev):
        if ev is None:
            return
        if ev[0] == "dma":
            _, s, v, key = ev
            if self.waited[e].get(key, 0) >= v:
                return
            self.waited[e][key] = v
            self.eng[e].wait_ge(s, v)
        else:
            pe, n = ev
            if pe == e and (not self.same or e in ("pe", "sp")):
                return
            if self.waited[e].get(pe, 0) >= n:
                return
            self.waited[e][pe] = n
            self.eng[e].wait_ge(self.sems[pe][(n - 1) // self.CH], (n - 1) % self.CH + 1)

    def _deps(self, e, reads, writes):
        for r in reads:
            self._wait(e, r.lw)
        for w in writes:
            self._wait(e, w.lw)
            for ev in w.rd.values():
                self._wait(e, ev)

    @staticmethod
    def _note_read(r, ev):
        key = ev[3] if ev[0] == "dma" else ev[0]
        old = r.rd.get(key)
        if old is None or (old[2] if old[0] == "dma" else old[1]) < (ev[2] if ev[0] == "dma" else ev[1]):
            r.rd[key] = ev

    def op(self, e, fn, reads=(), writes=()):
        if self.rec is not None:
            self.rec.append(("op", e, fn, list(reads), list(writes)))
            return None
        reads = [x.r if isinstance(x, Tile) else x for x in reads]
        writes = [x.r if isinstance(x, Tile) else x for x in writes]
        self._deps(e, reads, writes)
        self.cnt[e] += 1
        ev = (e, self.cnt[e])
        k = (self.cnt[e] - 1) // self.CH
        if k >= len(self.sems[e]):
            self.sems[e].append(self.free_dma.pop(0))
        fn(self.eng[e]).then_inc(self.sems[e][k], 1)
        self.ninst += 1
        for r in reads:
            self._note_read(r, ev)
        for w in writes:
            w.lw = ev
            w.rd = {}
        return ev

    def dma(self, e, dsem, out, in_, reads=(), writes=()):
        if self.rec is not None:
            self.rec.append(("dma", e, dsem, out, in_, list(reads), list(writes)))
            return None
        reads = [x.r if isinstance(x, Tile) else x for x in reads]
        writes = [x.r if isinstance(x, Tile) else x for x in writes]
        self._deps(e, reads, writes)
        if dsem[2] == 0:
            dsem[2] = 1
            dsem[3] = dsem[1] + 16
        dsem[1] += 16
        dsem[2] -= 1
        ev = ("dma", dsem[0], dsem[3], dsem[4])
        self.eng[e].dma_start(out=out, in_=in_).then_inc(dsem[0], 16)
        self.ninst += 1
        for r in reads:
            self._note_read(r, ev)
        for w in writes:
            w.lw = ev
            w.rd = {}
        return ev

    def wait_all(self, e, resources):
        for r in resources:
            r = r.r if isinstance(r, Tile) else r
            self._wait(e, r.lw)
            for ev in r.rd.values():
                self._wait(e, ev)


class Builder:
    def __init__(self, debug=None, stop_after=None):
        self.debug = debug or ()
        self.stop_after = stop_after
        self.nc = bass.Bass("TRN2", target_bir_lowering=False)
        self.dbg_out = {}

    def sb(self, st, name, shape, dt):
        self._uid = getattr(self, "_uid", 0) + 1
        return Tile(st.enter_context(self.nc.sbuf_tensor("sb%d_%s" % (self._uid, name), list(shape), dt)), name)

    def ps(self, st, name, shape=(128, 512), dt=F32):
        self._uid = getattr(self, "_uid", 0) + 1
        return Tile(st.enter_context(self.nc.psum_tensor("ps%d_%s" % (self._uid, name), list(shape), dt)), name)

    def dram_in(self, name, shape, dt=F32):
        return Tile(self.nc.dram_tensor(name, list(shape), dt, kind="ExternalInput").ap(), name)

    def dram_out(self, name, shape, dt=F32):
        return Tile(self.nc.dram_tensor(name, list(shape), dt, kind="ExternalOutput").ap(), name)

    def dram_scr(self, name, shape, dt):
        return Tile(self.nc.dram_tensor(name, list(shape), dt, kind="Internal").ap(), name)

    def dsem(self, group=0):
        return self.tr.new_dma_sem(group)

    def build(self):
        nc = self.nc
        I = {}
        I["xs"] = self.dram_in("xs", [TOK, D])
        I["cT"] = self.dram_in("cT", [128, 16])
        I["w_ada"] = self.dram_in("w_ada", [D, 6 * D])
        I["b_ada"] = self.dram_in("b_ada", [1, 6 * D])
        I["norm_mix_fm"] = self.dram_in("norm_mix_fm", [128, 8])
        I["norm_ffn_fm"] = self.dram_in("norm_ffn_fm", [128, 8])
        I["w_in"] = self.dram_in("w_in", [D, IN_W])
        I["w_up_aug"] = self.dram_in("w_up_aug", [2, 17, 512])
        I["gla_norm"] = self.dram_in("gla_norm", [1, 256])
        I["ssd_norm"] = self.dram_in("ssd_norm", [1, 1024])
        I["final_norm"] = self.dram_in("final_norm", [1, 1024])
        I["conv_w_fm"] = self.dram_in("conv_w_fm", [128, 12, 9])
        I["conv_b_fm"] = self.dram_in("conv_b_fm", [128, 12])
        I["dt_bias"] = self.dram_in("dt_bias", [1, 32])
        I["a_log"] = self.dram_in("a_log", [1, 32])
        I["ssd_d"] = self.dram_in("ssd_d", [1, 16])
        I["w_out"] = self.dram_in("w_out", [2048, D])
        I["w_router"] = self.dram_in("w_router", [128, 8 * 36])
        I["b_router"] = self.dram_in("b_router", [1, 36])
        I["w_gate"] = self.dram_in("w_gate", [NEXP, D, DFF])
        I["w_up"] = self.dram_in("w_up", [NEXP, D, DFF])
        I["w_down"] = self.dram_in("w_down", [NEXP, DFF, D])
        self.I = I
        self.out = self.dram_out("out", [NOWN, D])

        with ExitStack() as st:
            sems = {e: st.enter_context(nc.semaphore("s_" + e)) for e in Tracker.ENG}
            dsems = [st.enter_context(nc.semaphore("d%d" % i)) for i in range(90)]
            self.tr = Tracker(nc, sems, dsems)
            self.program(st)
        return nc

    def tap(self, name, tile_ap, shape, dt, reads):
        if name not in self.debug:
            return
        o = self.dram_out("dbg_" + name, shape, dt)
        self.dbg_out[name] = o
        n = shape[1]
        step = 2048
        d = self.dsem(len(range(0, n, step)))
        for c0 in range(0, n, step):
            c1 = min(n, c0 + step)
            self.tr.dma("sp", d, out=o.t[:, c0:c1], in_=tile_ap[:, c0:c1], reads=reads, writes=[o])
        self.final.append(o)

    def program(self, st):
        tr = self.tr
        self.final = []
        self.consts(st)
        self.stage_adaln(st)
        with ExitStack() as mst:
            self.stage_hT(mst)
            if self.stop_after == "hT":
                return self.finish()
            self.stage_gla(mst)
            if self.stop_after == "gla":
                return self.finish()
            self.stage_conv(mst)
            if self.stop_after == "conv":
                return self.finish()
            self.stage_ssd(mst)
            if self.stop_after == "ssd":
                return self.finish()
            self.barrier_release([self.c["hT"], self.c["BT"], self.c["CT"]] + self.c["hT_r"])
        self.stage_post(st)
        if self.stop_after == "post":
            return self.finish()
        self.stage_moe(st)
        if self.stop_after == "moe":
            return self.finish()
        self.stage_final(st)
        return self.finish()

    def finish(self):
        self.tr.wait_all("sp", self.final)

    def consts(self, st):
        tr = self.tr
        c = {}
        self.c = c
        c["ident_f"] = self.sb(st, "ident_f", [128, 128], F32)
        c["ident_b"] = self.sb(st, "ident_b", [128, 128], BF16)
        c["ones_f"] = self.sb(st, "ones_f", [128, 128], F32)
        for nm in ("tri_le", "tri_ge", "tri_gt", "tri_lt"):
            c[nm] = self.sb(st, nm, [128, 128], F32)
        idf = c["ident_f"]
        tr.op("pool", lambda e: e.memset(idf[:], 0.0), writes=[idf])
        tr.op("pool", lambda e: e.affine_select(out=idf[:], in_=idf[:], pattern=[[-1, 128]], compare_op=ALU.not_equal,
                                               fill=1.0, base=0, channel_multiplier=1), reads=[idf], writes=[idf])
        tr.op("pool", lambda e: e.tensor_copy(out=c["ident_b"][:], in_=idf[:]), reads=[idf], writes=[c["ident_b"]])
        tr.op("pool", lambda e: e.memset(c["ones_f"][:], 1.0), writes=[c["ones_f"]])
        specs = {"tri_le": (ALU.is_gt, 0), "tri_ge": (ALU.is_gt, 0), "tri_gt": (ALU.is_gt, 0), "tri_lt": (ALU.is_gt, 0)}
        t = c["tri_le"]
        tr.op("pool", lambda e: e.memset(t[:], 1.0), writes=[t])
        tr.op("pool", lambda e: e.affine_select(out=t[:], in_=t[:], pattern=[[1, 128]], compare_op=ALU.is_ge,
                                               fill=0.0, base=0, channel_multiplier=-1), reads=[t], writes=[t])
        t2 = c["tri_ge"]
        tr.op("pool", lambda e: e.memset(t2[:], 1.0), writes=[t2])
        tr.op("pool", lambda e: e.affine_select(out=t2[:], in_=t2[:], pattern=[[-1, 128]], compare_op=ALU.is_ge,
                                               fill=0.0, base=0, channel_multiplier=1), reads=[t2], writes=[t2])
        t3 = c["tri_gt"]
        tr.op("pool", lambda e: e.memset(t3[:], 1.0), writes=[t3])
        tr.op("pool", lambda e: e.affine_select(out=t3[:], in_=t3[:], pattern=[[-1, 128]], compare_op=ALU.is_gt,
                                               fill=0.0, base=0, channel_multiplier=1), reads=[t3], writes=[t3])
        t4 = c["tri_lt"]
        tr.op("pool", lambda e: e.memset(t4[:], 1.0), writes=[t4])
        tr.op("pool", lambda e: e.affine_select(out=t4[:], in_=t4[:], pattern=[[1, 128]], compare_op=ALU.is_gt,
                                               fill=0.0, base=0, channel_multiplier=-1), reads=[t4], writes=[t4])
        self.tap("tri_le", c["tri_le"][:], [128, 128], F32, [c["tri_le"]])
        self.tap("tri_gt", c["tri_gt"][:], [128, 128], F32, [c["tri_gt"]])

    def stage_adaln(self, st):
        tr, c, I = self.tr, self.c, self.I
        c["mod_fm"] = self.sb(st, "mod_fm", [128, 6, 8, 2], F32)
        c["g1_bc"] = self.sb(st, "g1_bc", [128, D], F32)
        c["g2_bc"] = self.sb(st, "g2_bc", [128, D], F32)
        c["s1"] = self.sb(st, "s1", [128, 8], F32)
        c["s1c"] = self.sb(st, "s1c", [128, 8], F32)
        c["b1"] = self.sb(st, "b1", [128, 8], F32)
        c["b1c"] = self.sb(st, "b1c", [128, 8], F32)
        c["s2"] = self.sb(st, "s2", [128, 8], F32)
        c["b2"] = self.sb(st, "b2", [128, 8], F32)
        with ExitStack() as s2:
            cT = self.sb(s2, "cT", [128, 16], F32)
            scT = self.sb(s2, "scT", [128, 16], F32)
            sc_rep = self.sb(s2, "sc_rep", [128, 8, 128], F32)
            brow = self.sb(s2, "brow", [1, 6 * D], F32)
            nm = self.sb(s2, "nm", [128, 8], F32)
            nf = self.sb(s2, "nf", [128, 8], F32)
            wblk = [self.sb(s2, "wblk%d" % i, [128, 8, D], F32) for i in range(2)]
            wsem = [self.dsem() for _ in range(2)]
            modps = self.ps(s2, "modps", [128, 512], F32)
            gps = [self.ps(s2, "gps%d" % i, [128, 512], F32) for i in range(2)]
            d = self.dsem(4)
            tr.dma("sp", d, out=cT[:], in_=I["cT"].t, writes=[cT])
            tr.dma("sp", d, out=brow[:], in_=I["b_ada"].t, writes=[brow])
            tr.dma("sp", d, out=nm[:], in_=I["norm_mix_fm"].t, writes=[nm])
            tr.dma("sp", d, out=nf[:], in_=I["norm_ffn_fm"].t, writes=[nf])
            tr.op("act", lambda e: e.activation(out=scT[:], in_=cT[:], func=AF.Silu), reads=[cT], writes=[scT])
            tr.op("dve", lambda e: e.tensor_copy(out=sc_rep[:], in_=scT[:].rearrange("p (k j) -> p k j", j=2)[:, :, 0:1].to_broadcast([128, 8, 128])),
                  reads=[scT], writes=[sc_rep])
            w_ada = I["w_ada"].t.rearrange("(kc p) n -> p kc n", p=128)
            mview = modps[:, 0:96].rearrange("p (b f t) -> p b f t", b=6, f=8)
            for blk in range(6):
                wb = wblk[blk % 2]
                tr.dma("sp", wsem[blk % 2], out=wb[:], in_=w_ada[:, :, blk * D:(blk + 1) * D], writes=[wb])
                if blk in (0, 1, 3, 4):
                    for fc in range(8):
                        for kc in range(8):
                            tr.op("pe", lambda e, fc=fc, kc=kc, wb=wb, blk=blk: e.matmul(
                                out=mview[:, blk, fc, :], lhsT=wb[:, kc, fc * 128:(fc + 1) * 128],
                                rhs=scT[:, 2 * kc:2 * kc + 2], start=(kc == 0), stop=False),
                                reads=[wb, scT], writes=[modps])
                        tr.op("pe", lambda e, fc=fc, blk=blk: e.matmul(
                            out=mview[:, blk, fc, :], lhsT=brow[0:1, blk * D + fc * 128: blk * D + (fc + 1) * 128],
                            rhs=c["ones_f"][0:1, 0:2], start=False, stop=True),
                            reads=[brow, c["ones_f"]], writes=[modps])
                else:
                    gdst = c["g1_bc"] if blk == 2 else c["g2_bc"]
                    for hh in range(2):
                        for kc in range(8):
                            tr.op("pe", lambda e, hh=hh, kc=kc, wb=wb: e.matmul(
                                out=gps[hh][:], lhsT=sc_rep[:, kc, :], rhs=wb[:, kc, hh * 512:(hh + 1) * 512],
                                start=(kc == 0), stop=False), reads=[wb, sc_rep], writes=[gps[hh]])
                        tr.op("pe", lambda e, hh=hh, blk=blk: e.matmul(
                            out=gps[hh][:], lhsT=c["ones_f"][0:1, :], rhs=brow[0:1, blk * D + hh * 512: blk * D + (hh + 1) * 512],
                            start=False, stop=True), reads=[brow, c["ones_f"]], writes=[gps[hh]])
                        tr.op("act", lambda e, hh=hh, gdst=gdst: e.activation(out=gdst[:, hh * 512:(hh + 1) * 512], in_=gps[hh][:], func=AF.Copy),
                              reads=[gps[hh]], writes=[gdst])
            mf = c["mod_fm"]
            mflat = mf[:].rearrange("p b f t -> p (b f t)")
            tr.op("dve", lambda e: e.tensor_copy(out=mflat[:, 0:32], in_=modps[:, 0:32]), reads=[modps], writes=[mf])
            tr.op("dve", lambda e: e.tensor_copy(out=mflat[:, 48:80], in_=modps[:, 48:80]), reads=[modps], writes=[mf])
            tr.op("dve", lambda e: e.scalar_tensor_tensor(out=c["s1"][:], in0=mf[:, 1, :, 0], scalar=1.0, in1=nm[:], op0=ALU.add, op1=ALU.mult),
                  reads=[mf, nm], writes=[c["s1"]])
            tr.op("dve", lambda e: e.scalar_tensor_tensor(out=c["s1c"][:], in0=mf[:, 1, :, 1], scalar=1.0, in1=nm[:], op0=ALU.add, op1=ALU.mult),
                  reads=[mf, nm], writes=[c["s1c"]])
            tr.op("dve", lambda e: e.scalar_tensor_tensor(out=c["s2"][:], in0=mf[:, 4, :, 0], scalar=1.0, in1=nf[:], op0=ALU.add, op1=ALU.mult),
                  reads=[mf, nf], writes=[c["s2"]])
            tr.op("dve", lambda e: e.tensor_copy(out=c["b1"][:], in_=mf[:, 0, :, 0]), reads=[mf], writes=[c["b1"]])
            tr.op("dve", lambda e: e.tensor_copy(out=c["b1c"][:], in_=mf[:, 0, :, 1]), reads=[mf], writes=[c["b1c"]])
            tr.op("dve", lambda e: e.tensor_copy(out=c["b2"][:], in_=mf[:, 3, :, 0]), reads=[mf], writes=[c["b2"]])
            self.tap("mod_fm", mf[:].rearrange("p b f t -> p (b f t)"), [128, 96], F32, [mf])
            self.tap("g1_bc", c["g1_bc"][:], [128, D], F32, [c["g1_bc"]])
            self.barrier_release([cT, scT, sc_rep, brow, nm, nf, wblk[0], wblk[1], modps, gps[0], gps[1]])

    def barrier_release(self, tiles):
        self.pending = getattr(self, "pending", [])
        for t in tiles:
            self.pending.append(t.r if isinstance(t, Tile) else t)

    def fence(self):
        pend = getattr(self, "pending", [])
        for e in Tracker.ENG:
            self.tr.wait_all(e, pend)
        self.pending = []

    def stage_hT(self, st):
        tr, c, I = self.tr, self.c, self.I
        self.fence()
        c["hT"] = self.sb(st, "hT", [128, 8, TOK], BF16)
        c["hT_r"] = [Res("hT%d" % t) for t in range(NT)]
        with ExitStack() as s2:
            xr = [self.sb(s2, "xr%d" % i, [128, D], F32) for i in range(3)]
            xsem = [self.dsem() for _ in range(3)]
            junk = self.sb(s2, "junk", [128, D], BF16)
            ss = [self.sb(s2, "ss%d" % i, [128, 1], F32) for i in range(3)]
            rstd = [self.sb(s2, "rstd%d" % i, [128, 1], F32) for i in range(3)]
            xn = [self.sb(s2, "xn%d" % i, [128, D], BF16) for i in range(3)]
            tmp = [self.sb(s2, "tmp%d" % i, [128, 8, 128], F32) for i in range(3)]
            tps = [self.ps(s2, "tps%d" % i, [128, 1024], BF16) for i in range(3)]
            epst = self.sb(s2, "epst", [128, 1], F32)
            tr.op("pool", lambda e: e.memset(epst[:], EPS), writes=[epst])
            rel = xr + ss + rstd + xn + tmp + tps + [junk, epst]
            recs = []
            for t in range(NT):
                tr.begin_record()
                x_t, ss_t, rs_t, xn_t, tmp_t, ps_t = xr[t % 3], ss[t % 3], rstd[t % 3], xn[t % 3], tmp[t % 3], tps[t % 3]
                hT_ap = c["hT"][:, :, t * 128:(t + 1) * 128]
                hT_r = c["hT_r"][t]
                isctx = t < T_OWN0
                sc, sh = (c["s1c"], c["b1c"]) if isctx else (c["s1"], c["b1"])
                tr.dma("sp", xsem[t % 3], out=x_t[:], in_=I["xs"].t[t * 128:(t + 1) * 128, :], writes=[x_t])
                tr.op("act", lambda e, x_t=x_t, ss_t=ss_t: e.activation(out=junk[:], in_=x_t[:], func=AF.Square, accum_out=ss_t[:]),
                      reads=[x_t], writes=[junk, ss_t])
                tr.op("act", lambda e, ss_t=ss_t, rs_t=rs_t: e.activation(out=rs_t[:], in_=ss_t[:], func=AF.Ln, scale=1.0 / D, bias=epst[:]),
                      reads=[ss_t, epst], writes=[rs_t])
                tr.op("act", lambda e, rs_t=rs_t: e.activation(out=rs_t[:], in_=rs_t[:], func=AF.Exp, scale=-0.5),
                      reads=[rs_t], writes=[rs_t])
                tr.op("dve", lambda e, x_t=x_t, rs_t=rs_t, xn_t=xn_t: e.tensor_scalar(out=xn_t[:], in0=x_t[:], scalar1=rs_t[:], scalar2=None, op0=ALU.mult),
                      reads=[x_t, rs_t], writes=[xn_t])
                for kc in range(8):
                    tr.op("pe", lambda e, kc=kc, xn_t=xn_t, ps_t=ps_t: e.transpose(out=ps_t[:, kc * 128:(kc + 1) * 128], in_=xn_t[:, kc * 128:(kc + 1) * 128], identity=c["ident_b"][:]),
                          reads=[xn_t, c["ident_b"]], writes=[ps_t])
                tr.op("dve", lambda e, ps_t=ps_t, tmp_t=tmp_t, sc=sc: e.tensor_tensor(
                    out=tmp_t[:], in0=ps_t[:].rearrange("p (k t) -> p k t", k=8), in1=sc[:].unsqueeze(2).to_broadcast([128, 8, 128]), op=ALU.mult),
                    reads=[ps_t, sc], writes=[tmp_t])
                tr.op("pool", lambda e, tmp_t=tmp_t, hT_ap=hT_ap, sh=sh: e.tensor_tensor(
                    out=hT_ap, in0=tmp_t[:], in1=sh[:].unsqueeze(2).to_broadcast([128, 8, 128]), op=ALU.add),
                    reads=[tmp_t, sh], writes=[hT_r])
                recs.append(tr.end_record())
            tr.run_pipelined(recs, depth=3)
            for t in (0, 2, 17, 33):
                if ("hT%d" % t) in self.debug:
                    o = self.dram_out("dbg_hT%d" % t, [128, 8, 128], BF16)
                    tr.dma("sp", self.dsem(), out=o.t, in_=c["hT"][:, :, t * 128:(t + 1) * 128], reads=[c["hT_r"][t]], writes=[o])
                    self.final.append(o)
            self.barrier_release(rel)

    def scratch(self, name, shape, dt):
        if name in self.debug:
            o = self.dram_out("dbg_" + name, shape, dt)
            self.final.append(o)
            return o
        return self.dram_scr(name, shape, dt)

    def stage_conv(self, st):
        tr, c, I = self.tr, self.c, self.I
        self.fence()
        c["x_tok"] = self.scratch("x_tok", [TOK, 1024], BF16)
        c["B_tok"] = self.scratch("B_tok", [TOK, 256], BF16)
        c["BT"] = self.sb(st, "BT", [128, 2, NOWN], BF16)
        c["CT"] = self.sb(st, "CT", [128, 2, NOWN], BF16)
        xtok_v = c["x_tok"].t.rearrange("(n p) c -> p n c", p=128)
        btok_v = c["B_tok"].t.rearrange("(n p) c -> p n c", p=128)
        w_in_v = I["w_in"].t.rearrange("(kc p) n -> p kc n", p=128)
        with ExitStack() as s2:
            wx = [self.sb(s2, "wx%d" % i, [128, 8, 128], BF16) for i in range(2)]
            wxs = [self.dsem() for _ in range(2)]
            cw = self.sb(s2, "cw", [128, 12, 9], F32)
            cb = self.sb(s2, "cb", [128, 12], F32)
            diag = [self.sb(s2, "diag%d" % i, [128, 9, 128], BF16) for i in range(2)]
            pre = [self.sb(s2, "pre%d" % i, [128, 66, 66], BF16) for i in range(2)]
            prec = [self.sb(s2, "prec%d" % i, [128, 258], BF16) for i in range(2)]
            post = [self.sb(s2, "post%d" % i, [128, 512], BF16) for i in range(3)]
            tst = [self.sb(s2, "tst%d" % i, [128, 4, 128], BF16) for i in range(3)]
            tsem = [self.dsem() for _ in range(3)]
            pp = [self.ps(s2, "pp%d" % i) for i in range(2)]
            pc = [self.ps(s2, "pc%d" % i) for i in range(2)]
            pt = [self.ps(s2, "pt%d" % i, [128, 1024], BF16) for i in range(2)]
            rel = wx + diag + pre + prec + post + tst + pp + pc + pt + [cw, cb]
            d0 = self.dsem(2)
            tr.dma("sp", d0, out=cw[:], in_=I["conv_w_fm"].t, writes=[cw])
            tr.dma("sp", d0, out=cb[:], in_=I["conv_b_fm"].t, writes=[cb])
            for i in range(2):
                tr.op("pool", lambda e, i=i: e.memset(pre[i][:], 0.0), writes=[pre[i]])
                tr.op("pool", lambda e, i=i: e.memset(prec[i][:], 0.0), writes=[prec[i]])
            nev = 0
            npost = 0
            for ct in range(12):
                w, dg, pr, prc = wx[ct % 2], diag[ct % 2], pre[ct % 2], prec[ct % 2]
                tr.dma("pool", wxs[ct % 2], out=w[:], in_=w_in_v[:, :, OFF_XBC + ct * 128: OFF_XBC + (ct + 1) * 128], writes=[w])
                tr.op("pool", lambda e, dg=dg, ct=ct: e.tensor_tensor(out=dg[:], in0=c["ident_f"][:].unsqueeze(1).to_broadcast([128, 9, 128]),
                                                                  in1=cw[:, ct, :].unsqueeze(2).to_broadcast([128, 9, 128]), op=ALU.mult),
                      reads=[c["ident_f"], cw], writes=[dg])
                for blk in range(9):
                    p_t = pp[nev % 2]
                    if blk == 0:
                        n, tok0, trs = 256, 0, [0, 1]
                    else:
                        n, tok0 = 512, NCTX + (blk - 1) * 512
                        trs = list(range(T_OWN0 + (blk - 1) * 4, T_OWN0 + blk * 4))
                    for kc in range(8):
                        tr.op("pe", lambda e, kc=kc, p_t=p_t, w=w, n=n, tok0=tok0: e.matmul(
                            out=p_t[:, 0:n], lhsT=w[:, kc, :], rhs=c["hT"][:, kc, tok0:tok0 + n], start=(kc == 0), stop=(kc == 7)),
                            reads=[w] + [c["hT_r"][t] for t in trs], writes=[p_t])
                    if blk == 0:
                        dst = prc[:, 1:257]
                        src = p_t[:, 0:256]
                        wr = prc
                    else:
                        r0 = (blk - 1) * 8
                        dst = pr[:, r0 + 1:r0 + 9, 1:65]
                        src = p_t[:, 0:512].rearrange("p (r q) -> p r q", q=64)
                        wr = pr
                    eng = "act" if nev % 2 == 0 else "dve"
                    if eng == "act":
                        tr.op("act", lambda e, dst=dst, src=src: e.activation(out=dst, in_=src, func=AF.Copy), reads=[p_t], writes=[wr])
                    else:
                        tr.op("dve", lambda e, dst=dst, src=src: e.tensor_copy(out=dst, in_=src), reads=[p_t], writes=[wr])
                    nev += 1
                for blk in range(9):
                    if ct >= 10 and (blk == 0 or blk >= 5):
                        continue
                    c_t = pc[blk % 2]
                    if blk == 0:
                        n = 256
                        for kw in range(3):
                            tr.op("pe", lambda e, kw=kw, c_t=c_t, dg=dg, prc=prc: e.matmul(
                                out=c_t[:, 0:256], lhsT=dg[:, 3 + kw, :], rhs=prc[:, kw:kw + 256], start=(kw == 0), stop=(kw == 2)),
                                reads=[dg, prc], writes=[c_t])
                    else:
                        n = 512
                        r0 = (blk - 1) * 8
                        for tap in range(9):
                            kh, kw = tap // 3, tap % 3
                            tr.op("pe", lambda e, tap=tap, kh=kh, kw=kw, c_t=c_t, dg=dg, pr=pr, r0=r0: e.matmul(
                                out=c_t[:, 0:512], lhsT=dg[:, tap, :], rhs=pr[:, r0 + kh:r0 + kh + 8, kw:kw + 64], start=(tap == 0), stop=(tap == 8)),
                                reads=[dg, pr], writes=[c_t])
                    own_blk = 1 <= blk <= 4
                    if ct >= 8 and own_blk:
                        g = (ct - 8) % 2
                        dstT = (c["BT"] if ct < 10 else c["CT"])
                        o0 = (blk - 1) * 512
                        tr.op("act", lambda e, dstT=dstT, g=g, o0=o0, c_t=c_t, ct=ct: e.activation(
                            out=dstT[:, g, o0:o0 + 512], in_=c_t[:, 0:512], func=AF.Silu, bias=cb[:, ct:ct + 1]),
                            reads=[c_t, cb], writes=[dstT])
                        if ct >= 10:
                            continue
                        src_post, src_r = dstT[:, g, o0:o0 + 512], dstT
                    else:
                        po = post[npost % 3]
                        tr.op("act", lambda e, po=po, c_t=c_t, ct=ct, n=n: e.activation(
                            out=po[:, 0:n], in_=c_t[:, 0:n], func=AF.Silu, bias=cb[:, ct:ct + 1]),
                            reads=[c_t, cb], writes=[po])
                        src_post, src_r = po[:, 0:n], po
                    ntl = n // 128
                    t_t = pt[npost % 2]
                    ts_t = tst[npost % 3]
                    for i in range(ntl):
                        tr.op("pe", lambda e, i=i, t_t=t_t, src_post=src_post: e.transpose(
                            out=t_t[:, i * 128:(i + 1) * 128], in_=src_post[:, i * 128:(i + 1) * 128], identity=c["ident_b"][:]),
                            reads=[src_r, c["ident_b"]], writes=[t_t])
                    tr.op("dve", lambda e, t_t=t_t, ts_t=ts_t, ntl=ntl: e.tensor_copy(
                        out=ts_t[:, 0:ntl, :], in_=t_t[:, 0:ntl * 128].rearrange("p (a b) -> p a b", b=128)),
                        reads=[t_t], writes=[ts_t])
                    tile0 = 0 if blk == 0 else T_OWN0 + (blk - 1) * 4
                    if ct < 8:
                        dst_d, dst_r = xtok_v[:, tile0:tile0 + ntl, ct * 128:(ct + 1) * 128], c["x_tok"]
                    else:
                        dst_d, dst_r = btok_v[:, tile0:tile0 + ntl, (ct - 8) * 128:(ct - 7) * 128], c["B_tok"]
                    tr.dma("sp", tsem[npost % 3], out=dst_d, in_=ts_t[:, 0:ntl, :], reads=[ts_t], writes=[])
                    c.setdefault("scr_ev", []).append(ts_t)
                    npost += 1
            self.conv_store_tiles = tst
            self.tap("BT", c["BT"][:].rearrange("p g t -> p (g t)"), [128, 2 * NOWN], BF16, [c["BT"]])
            self.tap("CT", c["CT"][:].rearrange("p g t -> p (g t)"), [128, 2 * NOWN], BF16, [c["CT"]])
            for e in Tracker.ENG:
                tr.wait_all(e, tst)
            self.barrier_release(rel)

    def stage_gla(self, st):
        tr, c, I = self.tr, self.c, self.I
        self.fence()
        c["oB"] = self.scratch("oB", [NOWN, 1024], F32)
        c["yx"] = self.scratch("yx", [NOWN, 2048], BF16)
        oB_v = c["oB"].t.rearrange("(n p) c -> n p c", p=128)
        yx_v = c["yx"].t.rearrange("(n p) c -> n p c", p=128)
        w_in_v = I["w_in"].t.rearrange("(kc p) n -> p kc n", p=128)
        LNQ = math.log(128.0 ** -0.5)
        with ExitStack() as s2:
            wg = self.sb(s2, "wgla", [128, 8, 3072], BF16)
            wgs = [Res("wgla%d" % i) for i in range(6)]
            wgg = self.sb(s2, "wgg", [128, 8, 32], BF16)
            wup = self.sb(s2, "wup", [17, 2, 512], F32)
            gn = self.sb(s2, "gn_bc", [128, 256], F32)
            c_one = self.sb(s2, "c_one", [128, 1], F32)
            c_lnq = self.sb(s2, "c_lnq", [128, 1], F32)
            c_eps = self.sb(s2, "c_eps", [128, 1], F32)
            negcol = self.sb(s2, "negcol", [128, 2], F32)
            Tm = [self.sb(s2, "TmA", [128, 128], F32), self.sb(s2, "TmB", [128, 128], F32)]
            S = [self.sb(s2, "S_A", [128, 4, 256], F32), self.sb(s2, "S_B", [128, 4, 256], F32)]
            Sbf = self.sb(s2, "Sbf", [128, 4, 256], BF16)
            FS = []
            for par in range(2):
                f = {}
                f["g_aug"] = self.sb(s2, "g_aug%d" % par, [32, 128], F32)
                f["v_bf"] = self.sb(s2, "v_bf%d" % par, [128, 1024], BF16)
                f["lap"] = self.sb(s2, "lap%d" % par, [128, 512], F32)
                f["Einv"] = self.sb(s2, "Einv%d" % par, [128, 512], F32)
                f["Eq"] = self.sb(s2, "Eq%d" % par, [128, 512], F32)
                f["kt_"] = self.sb(s2, "kt_%d" % par, [128, 512], BF16)
                f["qt_"] = self.sb(s2, "qt_%d" % par, [128, 512], BF16)
                f["kqT"] = self.sb(s2, "kqT%d" % par, [128, 8, 128], BF16)
                f["PT"] = self.sb(s2, "PT%d" % par, [128, 4, 128], BF16)
                f["dcol"] = self.sb(s2, "dcol%d" % par, [128, 4], F32)
                f["P"] = [self.ps(s2, "gP%d_%d" % (par, i)) for i in range(4)]
                FS.append(f)
            silr2 = [self.sb(s2, "silr%d" % i, [128, 1024], F32) for i in range(2)]
            o_sb2 = [self.sb(s2, "o_sb%d" % i, [128, 1024], F32) for i in range(2)]
            ost = [self.sb(s2, "ost%d" % i, [128, 1024], F32) for i in range(2)]
            osts = [self.dsem() for _ in range(2)]
            oB_sb = ost
            oBs = [self.dsem() for _ in range(2)]
            yst = [self.sb(s2, "yst%d" % i, [128, 1024], BF16) for i in range(2)]
            ysts = [self.dsem() for _ in range(2)]
            ss42 = [self.sb(s2, "ss4_%d" % i, [128, 4], F32) for i in range(2)]
            rs42 = [self.sb(s2, "rs4_%d" % i, [128, 4], F32) for i in range(2)]
            junk2 = [self.sb(s2, "junkg%d" % i, [128, 256], BF16) for i in range(2)]
            rel = [wg, wgg, wup, gn, c_one, c_lnq, c_eps, negcol, Tm[0], Tm[1], S[0], S[1], Sbf] + silr2 + o_sb2 + ss42 + rs42 + junk2 + ost + yst + wgs
            for f in FS:
                rel += [f[k] for k in ("g_aug", "v_bf", "lap", "Einv", "Eq", "kt_", "qt_", "kqT", "PT", "dcol")] + f["P"]
            d0 = self.dsem(9)
            for i in range(6):
                tr.dma("pool", d0, out=wg[:, :, i * 512:(i + 1) * 512], in_=w_in_v[:, :, i * 512:(i + 1) * 512], writes=[wgs[i]])
            tr.dma("pool", d0, out=wgg[:], in_=w_in_v[:, :, OFF_G:OFF_G + 32], writes=[wgg])
            tr.dma("sp", d0, out=wup[:], in_=I["w_up_aug"].t.rearrange("d k n -> k d n"), writes=[wup])
            tr.dma("sp", d0, out=gn[:], in_=I["gla_norm"].t.partition_broadcast(128), writes=[gn])
            tr.op("pool", lambda e: e.memset(c_one[:], 1.0), writes=[c_one])
            tr.op("pool", lambda e: e.memset(c_lnq[:], LNQ), writes=[c_lnq])
            tr.op("pool", lambda e: e.memset(c_eps[:], EPS), writes=[c_eps])
            tr.op("pool", lambda e: e.memset(negcol[:], -1.0 / 16.0), writes=[negcol])
            tr.op("pool", lambda e: e.tensor_scalar(out=Tm[0][:], in0=c["tri_le"][:], scalar1=-1.0 / 16.0, scalar2=None, op0=ALU.mult), reads=[c["tri_le"]], writes=[Tm[0]])
            tr.op("pool", lambda e: e.tensor_scalar(out=Tm[1][:], in0=c["tri_ge"][:], scalar1=-1.0 / 16.0, scalar2=None, op0=ALU.mult), reads=[c["tri_ge"]], writes=[Tm[1]])
            for f in FS:
                tr.op("pool", lambda e, f=f: e.memset(f["g_aug"][:], 1.0), writes=[f["g_aug"]])
            for dd in range(2):
                tr.op("pool", lambda e, dd=dd: e.memset(S[dd][:], 0.0), writes=[S[dd]])
            masks = [c["tri_le"], c["tri_ge"]]

            def gla_tile(t, dd, full, sweepA, own_idx, seq):
                f = FS[seq % 2]
                P = f["P"]
                g_aug, v_bf, lap, Einv, Eq, kt_, qt_, kqT, PT, dcol = (f[k] for k in ("g_aug", "v_bf", "lap", "Einv", "Eq", "kt_", "qt_", "kqT", "PT", "dcol"))
                Sd = S[dd]
                silr, o_sb, ss4, rs4, junk = silr2[seq % 2], o_sb2[seq % 2], ss42[seq % 2], rs42[seq % 2], junk2[seq % 2]
                hres = [c["hT_r"][t]]
                lhs = lambda kc: c["hT"][:, kc, t * 128:(t + 1) * 128]

                def mm_tok(ps_t, c0, n, wres):
                    for kc in range(8):
                        tr.op("pe", lambda e, kc=kc: e.matmul(out=ps_t[:, 0:n], lhsT=lhs(kc), rhs=wg[:, kc, c0:c0 + n], start=(kc == 0), stop=(kc == 7)),
                              reads=hres + wres, writes=[ps_t])
                for kc in range(8):
                    tr.op("pe", lambda e, kc=kc: e.matmul(out=P[3][0:16, 0:128], lhsT=wgg[:, kc, dd * 16:(dd + 1) * 16], rhs=lhs(kc), start=(kc == 0), stop=(kc == 7)),
                          reads=hres + [wgg], writes=[P[3]])
                tr.op("act", lambda e: e.activation(out=g_aug[0:16, :], in_=P[3][0:16, 0:128], func=AF.Copy), reads=[P[3]], writes=[g_aug])
                mm_tok(P[0], 512, 512, [wgs[1]])
                mm_tok(P[1], 1024, 512, [wgs[2]])
                mm_tok(P[2], 1536, 512, [wgs[3]])
                tr.op("pe", lambda e: e.matmul(out=P[3][:, 0:512], lhsT=g_aug[0:17, :], rhs=wup[:, dd, :], start=True, stop=True), reads=[g_aug, wup], writes=[P[3]])
                tr.op("act", lambda e: e.activation(out=lap[:], in_=P[3][:, 0:512], func=AF.Exp, scale=-1.0), reads=[P[3]], writes=[lap])
                tr.op("act", lambda e: e.activation(out=lap[:], in_=lap[:], func=AF.Ln, bias=c_one[:]), reads=[lap, c_one], writes=[lap])
                tr.op("act", lambda e: e.activation(out=v_bf[:, 0:512], in_=P[1][:], func=AF.Copy), reads=[P[1]], writes=[v_bf])
                tr.op("dve", lambda e: e.tensor_copy(out=v_bf[:, 512:1024], in_=P[2][:]), reads=[P[2]], writes=[v_bf])
                tr.op("pe", lambda e: e.matmul(out=P[3][:, 0:512], lhsT=Tm[dd][:], rhs=lap[:], start=True, stop=True), reads=[Tm[dd], lap], writes=[P[3]])
                if full:
                    mm_tok(P[1], 0, 512, [wgs[0]])
                tr.op("act", lambda e: e.activation(out=Einv[:], in_=P[3][:, 0:512], func=AF.Exp, scale=-1.0), reads=[P[3]], writes=[Einv])
                if full:
                    tr.op("act", lambda e: e.activation(out=Eq[:], in_=P[3][:, 0:512], func=AF.Exp, bias=c_lnq[:]), reads=[P[3], c_lnq], writes=[Eq])
                tr.op("dve", lambda e: e.tensor_tensor(out=kt_[:], in0=P[0][:], in1=Einv[:], op=ALU.mult), reads=[P[0], Einv], writes=[kt_])
                for h in range(4):
                    tr.op("pe", lambda e, h=h: e.matmul(out=P[3][:, 2 * h:2 * h + 2], lhsT=lap[:, h * 128:(h + 1) * 128], rhs=negcol[:], start=True, stop=True),
                          reads=[lap, negcol], writes=[P[3]])
                tr.op("act", lambda e: e.activation(out=dcol[:], in_=P[3][:, 0:8:2], func=AF.Exp), reads=[P[3]], writes=[dcol])
                if full:
                    pT = P[2][:].bitcast(BF16)
                    tr.op("dve", lambda e: e.tensor_tensor(out=qt_[:], in0=P[1][:], in1=Eq[:], op=ALU.mult), reads=[P[1], Eq], writes=[qt_])
                    for h in range(4):
                        tr.op("pe", lambda e, h=h: e.transpose(out=pT[:, h * 128:(h + 1) * 128], in_=kt_[:, h * 128:(h + 1) * 128], identity=c["ident_b"][:]),
                              reads=[kt_, c["ident_b"]], writes=[P[2]])
                    for h in range(4):
                        tr.op("pe", lambda e, h=h: e.transpose(out=pT[:, (4 + h) * 128:(5 + h) * 128], in_=qt_[:, h * 128:(h + 1) * 128], identity=c["ident_b"][:]),
                              reads=[qt_, c["ident_b"]], writes=[P[2]])
                    tr.op("act", lambda e: e.activation(out=kqT[:].rearrange("p a b -> p (a b)"), in_=pT, func=AF.Copy), reads=[P[2]], writes=[kqT])
                    for h in range(4):
                        tr.op("pe", lambda e, h=h: e.matmul(out=P[0][:, h * 128:(h + 1) * 128], lhsT=kqT[:, h, :], rhs=kqT[:, 4 + h, :], start=True, stop=True),
                              reads=[kqT], writes=[P[0]])
                    tr.op("dve", lambda e: e.tensor_tensor(out=PT[:], in0=P[0][:].rearrange("p (h i) -> p h i", h=4),
                                                          in1=masks[dd][:].unsqueeze(1).to_broadcast([128, 4, 128]), op=ALU.mult),
                          reads=[P[0], masks[dd]], writes=[PT])
                kvb = [P[2], P[2], P[0], P[0]]
                for h in range(4):
                    cs = (h % 2) * 256
                    tr.op("pe", lambda e, h=h, cs=cs: e.matmul(out=kvb[h][:, cs:cs + 256], lhsT=kt_[:, h * 128:(h + 1) * 128], rhs=v_bf[:, h * 256:(h + 1) * 256], start=True, stop=True),
                          reads=[kt_, v_bf], writes=[kvb[h]])
                tr.mark("need_state")
                if full:
                    ob_ = [P[1], P[1], P[3], P[3]]
                    tr.op("act", lambda e: e.activation(out=Sbf[:].rearrange("p a b -> p (a b)"), in_=Sd[:].rearrange("p a b -> p (a b)"), func=AF.Copy), reads=[Sd], writes=[Sbf])
                    for h in range(4):
                        cs = (h % 2) * 256
                        tr.op("pe", lambda e, h=h, cs=cs: e.matmul(out=ob_[h][:, cs:cs + 256], lhsT=PT[:, h, :], rhs=v_bf[:, h * 256:(h + 1) * 256], start=True, stop=False),
                              reads=[PT, v_bf], writes=[ob_[h]])
                        tr.op("pe", lambda e, h=h, cs=cs: e.matmul(out=ob_[h][:, cs:cs + 256], lhsT=kqT[:, 4 + h, :], rhs=Sbf[:, h, :], start=False, stop=True),
                              reads=[kqT, Sbf], writes=[ob_[h]])
                tr.op("dve", lambda e: e.tensor_tensor(out=Sd[:, 0:2, :].rearrange("p a b -> p (a b)"), in0=P[2][:], in1=Sd[:, 0:2, :].rearrange("p a b -> p (a b)"), op=ALU.add),
                      reads=[P[2], Sd], writes=[Sd])
                tr.op("dve", lambda e: e.tensor_tensor(out=Sd[:, 2:4, :].rearrange("p a b -> p (a b)"), in0=P[0][:], in1=Sd[:, 2:4, :].rearrange("p a b -> p (a b)"), op=ALU.add),
                      reads=[P[0], Sd], writes=[Sd])
                tr.op("dve", lambda e: e.tensor_tensor(out=Sd[:], in0=Sd[:], in1=dcol[:].unsqueeze(2).to_broadcast([128, 4, 256]), op=ALU.mult),
                      reads=[Sd, dcol], writes=[Sd])
                tr.mark("state_done")
                if not full:
                    return
                if not sweepA:
                    os_ = ost[own_idx % 2]
                    tr.op("act", lambda e: e.activation(out=os_[:, 0:512], in_=P[1][:], func=AF.Copy), reads=[P[1]], writes=[os_])
                    tr.op("dve", lambda e: e.tensor_copy(out=os_[:, 512:1024], in_=P[3][:]), reads=[P[3]], writes=[os_])
                    tr.dma("sp", osts[own_idx % 2], out=oB_v[own_idx], in_=os_[:], reads=[os_], writes=[])
                    return
                ob = oB_sb[own_idx % 2]
                tr.dma("sp", oBs[own_idx % 2], out=ob[:], in_=oB_v[own_idx], writes=[ob])
                mm_tok(P[2], 2048, 512, [wgs[4]])
                tr.op("act", lambda e: e.activation(out=silr[:, 0:512], in_=P[2][:], func=AF.Silu), reads=[P[2]], writes=[silr])
                mm_tok(P[0], 2560, 512, [wgs[5]])
                tr.op("act", lambda e: e.activation(out=silr[:, 512:1024], in_=P[0][:], func=AF.Silu), reads=[P[0]], writes=[silr])
                tr.op("dve", lambda e: e.tensor_tensor(out=silr[:].rearrange("p (h v) -> p h v", h=4), in0=silr[:].rearrange("p (h v) -> p h v", h=4),
                                                      in1=gn[:].unsqueeze(1).to_broadcast([128, 4, 256]), op=ALU.mult), reads=[silr, gn], writes=[silr])
                for hh, pb in enumerate((P[1], P[3])):
                    tr.op("dve", lambda e, hh=hh, pb=pb: e.tensor_tensor(out=o_sb[:, hh * 512:(hh + 1) * 512], in0=pb[:], in1=ob[:, hh * 512:(hh + 1) * 512], op=ALU.add),
                          reads=[pb, ob], writes=[o_sb])
                for h in range(4):
                    tr.op("act", lambda e, h=h: e.activation(out=junk[:], in_=o_sb[:, h * 256:(h + 1) * 256], func=AF.Square, accum_out=ss4[:, h:h + 1]),
                          reads=[o_sb], writes=[junk, ss4])
                tr.op("act", lambda e: e.activation(out=rs4[:], in_=ss4[:], func=AF.Ln, scale=1.0 / 256.0, bias=c_eps[:]), reads=[ss4, c_eps], writes=[rs4])
                tr.op("act", lambda e: e.activation(out=rs4[:], in_=rs4[:], func=AF.Exp, scale=-0.5), reads=[rs4], writes=[rs4])
                tr.op("dve", lambda e: e.tensor_tensor(out=o_sb[:].rearrange("p (h v) -> p h v", h=4), in0=o_sb[:].rearrange("p (h v) -> p h v", h=4),
                                                      in1=rs4[:].unsqueeze(2).to_broadcast([128, 4, 256]), op=ALU.mult), reads=[o_sb, rs4], writes=[o_sb])
                ys = yst[own_idx % 2]
                tr.op("dve", lambda e: e.tensor_tensor(out=ys[:], in0=o_sb[:], in1=silr[:], op=ALU.mult), reads=[o_sb, silr], writes=[ys])
                tr.dma("sp", ysts[own_idx % 2], out=yx_v[own_idx][:, 0:1024], in_=ys[:], reads=[ys], writes=[])

            def sweep(tiles):
                recs = []
                for seq, (t, dd, full, sweepA, own_idx) in enumerate(tiles):
                    tr.begin_record()
                    gla_tile(t, dd, full, sweepA, own_idx, seq)
                    recs.append(tr.end_record())
                tr.run_pipelined(recs, depth=2)

            sweep([(t, 1, False, False, None) for t in (1, 0)])
            self.tap("gS_B", S[1][:].rearrange("p a b -> p (a b)"), [128, 1024], F32, [S[1]])
            sweep([(t, 1, False, False, None) for t in range(NT - 1, T_OTH0 - 1, -1)] +
                  [(t, 1, True, False, t - T_OWN0) for t in range(T_OTH0 - 1, T_OWN0 - 1, -1)])
            for e in Tracker.ENG:
                tr.wait_all(e, ost)
            sweep([(t, 0, False, True, None) for t in (0, 1)])
            self.tap("gS_A", S[0][:].rearrange("p a b -> p (a b)"), [128, 1024], F32, [S[0]])
            sweep([(t, 0, True, True, t - T_OWN0) for t in range(T_OWN0, T_OTH0)])
            for e in Tracker.ENG:
                tr.wait_all(e, yst)
            self.barrier_release(rel)

    def stage_ssd(self, st):
        tr, c, I = self.tr, self.c, self.I
        self.fence()
        c["yB"] = self.scratch("yB", [NOWN, 1024], F32)
        yB_v = c["yB"].t.rearrange("(n p) c -> n p c", p=128)
        yx_v = c["yx"].t.rearrange("(n p) c -> n p c", p=128)
        xtok_v = c["x_tok"].t.rearrange("(n p) c -> n p c", p=128)
        btok_v = c["B_tok"].t.rearrange("(n p) c -> n p c", p=128)
        w_in_v = I["w_in"].t.rearrange("(kc p) n -> p kc n", p=128)
        BT, CT = c["BT"], c["CT"]
        with ExitStack() as s2:
            wz = self.sb(s2, "wz", [128, 8, 1024], BF16)
            wdt = self.sb(s2, "wdt", [128, 8, 32], BF16)
            Abc = self.sb(s2, "Abc", [128, 32], F32)
            dtb = self.sb(s2, "dtb", [128, 32], F32)
            Dsk = self.sb(s2, "Dsk", [128, 16], F32)
            snb = self.sb(s2, "snb", [128, 1024], F32)
            c_one = self.sb(s2, "c_one2", [128, 1], F32)
            c_eps = self.sb(s2, "c_eps2", [128, 1], F32)
            ST = [self.sb(s2, "ST_A", [128, 2, 512], F32), self.sb(s2, "ST_B", [128, 2, 512], F32)]
            STbf = self.sb(s2, "STbf", [128, 2, 512], BF16)
            xt = [self.sb(s2, "xt%d" % i, [128, 1024], BF16) for i in range(2)]
            bt = [self.sb(s2, "bt%d" % i, [128, 256], BF16) for i in range(2)]
            xts = [self.dsem() for _ in range(2)]
            dt_ = self.sb(s2, "dt_", [128, 16], F32)
            dtA = self.sb(s2, "dtA", [128, 16], F32)
            acs = self.sb(s2, "acs", [128, 16], F32)
            ea = self.sb(s2, "ea", [128, 16], F32)
            dend = self.sb(s2, "dend", [128, 16], F32)
            dtot = self.sb(s2, "dtot", [128, 16], F32)
            R1 = self.sb(s2, "R1", [128, 16, 128], BF16)
            E = self.sb(s2, "E", [128, 16, 128], BF16)
            M = self.sb(s2, "M", [128, 16, 128], BF16)
            CBm = self.sb(s2, "CBm", [128, 2, 128], F32)
            xdt = self.sb(s2, "xdt", [128, 1024], BF16)
            xdd = self.sb(s2, "xdd", [128, 1024], BF16)
            silz = self.sb(s2, "silz", [128, 1024], F32)
            y_sb = self.sb(s2, "y_sb", [128, 1024], F32)
            tmp = self.sb(s2, "ytmp", [128, 1024], F32)
            yB_sb = [self.sb(s2, "yB_sb%d" % i, [128, 1024], F32) for i in range(2)]
            yBs = [self.dsem() for _ in range(2)]
            yst = [self.sb(s2, "ysst%d" % i, [128, 1024], F32) for i in range(2)]
            ysts = [self.dsem() for _ in range(2)]
            yxs = [self.sb(s2, "yxs%d" % i, [128, 1024], BF16) for i in range(2)]
            yxss = [self.dsem() for _ in range(2)]
            ss2 = self.sb(s2, "ss2", [128, 2], F32)
            rs2 = self.sb(s2, "rs2", [128, 2], F32)
            junk = self.sb(s2, "junks", [128, 512], BF16)
            pS = self.ps(s2, "pS")
            pD = [self.ps(s2, "pD%d" % i) for i in range(4)]
            pCB = self.ps(s2, "pCB")
            pY = [self.ps(s2, "pY%d" % i) for i in range(2)]
            rel = [wz, wdt, Abc, dtb, Dsk, snb, c_one, c_eps, ST[0], ST[1], STbf, dt_, dtA, acs, ea, dend, dtot, R1, E, M, CBm, xdt, xdd,
                   silz, y_sb, tmp, ss2, rs2, junk, pS, pCB] + xt + bt + yB_sb + yst + yxs + pD + pY
            d0 = self.dsem(6)
            tr.dma("pool", d0, out=wz[:], in_=w_in_v[:, :, OFF_Z:OFF_Z + 1024], writes=[wz])
            tr.dma("pool", d0, out=wdt[:], in_=w_in_v[:, :, OFF_DT:OFF_DT + 32], writes=[wdt])
            tr.dma("sp", d0, out=Abc[:], in_=I["a_log"].t.partition_broadcast(128), writes=[Abc])
            tr.dma("sp", d0, out=dtb[:], in_=I["dt_bias"].t.partition_broadcast(128), writes=[dtb])
            tr.dma("sp", d0, out=Dsk[:], in_=I["ssd_d"].t.partition_broadcast(128), writes=[Dsk])
            tr.dma("sp", d0, out=snb[:], in_=I["ssd_norm"].t.partition_broadcast(128), writes=[snb])
            tr.op("pool", lambda e: e.memset(c_one[:], 1.0), writes=[c_one])
            tr.op("pool", lambda e: e.memset(c_eps[:], EPS), writes=[c_eps])
            tr.op("act", lambda e: e.activation(out=Abc[:], in_=Abc[:], func=AF.Exp), reads=[Abc], writes=[Abc])
            tr.op("dve", lambda e: e.tensor_scalar(out=Abc[:], in0=Abc[:], scalar1=-1.0, scalar2=None, op0=ALU.mult), reads=[Abc], writes=[Abc])
            for dd in range(2):
                tr.op("pool", lambda e, dd=dd: e.memset(ST[dd][:], 0.0), writes=[ST[dd]])
            Lm = [c["tri_gt"], c["tri_lt"]]
            Tc = [c["tri_le"], c["tri_ge"]]
            cnt = [0]

            QS = [[pS, pD[0], pD[1], pD[2]], [pD[3], pCB, pY[0], pY[1]]]
            ea2 = [ea, self.sb(s2, "ea_b", [128, 16], F32)]
            dtot2 = [dtot, self.sb(s2, "dtot_b", [128, 16], F32)]
            xdd2 = [xdd, self.sb(s2, "xdd_b", [128, 1024], BF16)]
            silz2 = [silz, self.sb(s2, "silz_b", [128, 1024], F32)]
            ysb2 = [y_sb, self.sb(s2, "y_sb_b", [128, 1024], F32)]
            tmp2 = [tmp, self.sb(s2, "ytmp_b", [128, 1024], F32)]
            ss22 = [ss2, self.sb(s2, "ss2_b", [128, 2], F32)]
            rs22 = [rs2, self.sb(s2, "rs2_b", [128, 2], F32)]
            junk2 = [junk, self.sb(s2, "junks_b", [128, 512], BF16)]
            dt2 = [dt_, self.sb(s2, "dt_b", [128, 16], F32)]
            dtA2 = [dtA, self.sb(s2, "dtA_b", [128, 16], F32)]
            acs2 = [acs, self.sb(s2, "acs_b", [128, 16], F32)]
            dend2 = [dend, self.sb(s2, "dend_b", [128, 16], F32)]
            xdt2 = [xdt, self.sb(s2, "xdt_b", [128, 1024], BF16)]
            R12 = [R1, self.sb(s2, "R1_b", [128, 16, 128], BF16)]
            rel.append(R12[1])
            Lmb = [self.sb(s2, "Lmb%d" % i, [128, 128], BF16) for i in range(2)]
            for i in range(2):
                tr.op("pool", lambda e, i=i: e.tensor_copy(out=Lmb[i][:], in_=Lm[i][:]), reads=[Lm[i]], writes=[Lmb[i]])
            rel += [dt2[1], dtA2[1], acs2[1], dend2[1], xdt2[1]] + Lmb
            rel += [ea2[1], dtot2[1], xdd2[1], silz2[1], ysb2[1], tmp2[1], ss22[1], rs22[1], junk2[1]]

            def ssd_tile(t, dd, full, sweepA, own_idx, seq):
                STd = ST[dd]
                k = seq % 2
                Q = QS[k]
                ea_, dtot_, xdd_, silz_ = ea2[k], dtot2[k], xdd2[k], silz2[k]
                y_sb, tmp, ss2, rs2, junk = ysb2[k], tmp2[k], ss22[k], rs22[k], junk2[k]
                dt_, dtA, acs, dend, xdt = dt2[k], dtA2[k], acs2[k], dend2[k], xdt2[k]
                R1 = R12[k]
                x_t, b_t = xt[k], bt[k]
                tr.regroup(xts[k], 2)
                tr.dma("sp", xts[k], out=x_t[:], in_=xtok_v[t], writes=[x_t])
                tr.dma("sp", xts[k], out=b_t[:], in_=btok_v[t], writes=[b_t])
                hres = [c["hT_r"][t]]
                lhs = lambda kc: c["hT"][:, kc, t * 128:(t + 1) * 128]
                if full and sweepA:
                    for hh in range(2):
                        for kc in range(8):
                            tr.op("pe", lambda e, kc=kc, hh=hh: e.matmul(out=Q[1 + hh][:], lhsT=lhs(kc), rhs=wz[:, kc, hh * 512:(hh + 1) * 512], start=(kc == 0), stop=(kc == 7)),
                                  reads=hres + [wz], writes=[Q[1 + hh]])
                        tr.op("act", lambda e, hh=hh: e.activation(out=silz_[:, hh * 512:(hh + 1) * 512], in_=Q[1 + hh][:], func=AF.Silu), reads=[Q[1 + hh]], writes=[silz_])
                for kc in range(8):
                    tr.op("pe", lambda e, kc=kc: e.matmul(out=Q[0][:, 0:16], lhsT=lhs(kc), rhs=wdt[:, kc, dd * 16:(dd + 1) * 16], start=(kc == 0), stop=(kc == 7)),
                          reads=hres + [wdt], writes=[Q[0]])
                tr.op("dve", lambda e: e.tensor_tensor(out=dt_[:], in0=Q[0][:, 0:16], in1=dtb[:, dd * 16:(dd + 1) * 16], op=ALU.add), reads=[Q[0], dtb], writes=[dt_])
                tr.op("act", lambda e: e.activation(out=dt_[:], in_=dt_[:], func=AF.Exp), reads=[dt_], writes=[dt_])
                tr.op("act", lambda e: e.activation(out=dt_[:], in_=dt_[:], func=AF.Ln, bias=c_one[:]), reads=[dt_, c_one], writes=[dt_])
                tr.op("dve", lambda e: e.tensor_tensor(out=dtA[:], in0=dt_[:], in1=Abc[:, dd * 16:(dd + 1) * 16], op=ALU.mult), reads=[dt_, Abc], writes=[dtA])
                tr.op("pe", lambda e: e.matmul(out=Q[0][:, 16:32], lhsT=Tc[dd][:], rhs=dtA[:], start=True, stop=True), reads=[Tc[dd], dtA], writes=[Q[0]])
                tr.op("pe", lambda e: e.matmul(out=Q[0][:, 32:48], lhsT=c["ones_f"][:], rhs=dtA[:], start=True, stop=True), reads=[c["ones_f"], dtA], writes=[Q[0]])
                tr.op("dve", lambda e: e.tensor_copy(out=acs[:], in_=Q[0][:, 16:32]), reads=[Q[0]], writes=[acs])
                tr.op("dve", lambda e: e.tensor_tensor(out=dend[:], in0=Q[0][:, 32:48], in1=acs[:], op=ALU.subtract), reads=[Q[0], acs], writes=[dend])
                tr.op("act", lambda e: e.activation(out=dend[:], in_=dend[:], func=AF.Exp), reads=[dend], writes=[dend])
                tr.op("act", lambda e: e.activation(out=dtot_[:], in_=Q[0][:, 32:48], func=AF.Exp), reads=[Q[0]], writes=[dtot_])
                tr.op("dve", lambda e: e.tensor_tensor(out=xdt[:].rearrange("p (h q) -> p h q", h=16), in0=x_t[:].rearrange("p (h q) -> p h q", h=16),
                                                      in1=dt_[:].unsqueeze(2).to_broadcast([128, 16, 64]), op=ALU.mult), reads=[x_t, dt_], writes=[xdt])
                tr.op("pool", lambda e: e.tensor_tensor(out=xdd_[:].rearrange("p (h q) -> p h q", h=16), in0=xdt[:].rearrange("p (h q) -> p h q", h=16),
                                                       in1=dend[:].unsqueeze(2).to_broadcast([128, 16, 64]), op=ALU.mult), reads=[xdt, dend], writes=[xdd_])
                if full:
                    tok0 = (t - T_OWN0) * 128
                    Dbank = [Q[1], Q[2], Q[3], Q[1]]
                    tr.op("act", lambda e: e.activation(out=ea_[:], in_=acs[:], func=AF.Exp), reads=[acs], writes=[ea_])
                    tr.op("dve", lambda e: e.tensor_tensor(out=R1[:], in0=Tc[dd][:].unsqueeze(1).to_broadcast([128, 16, 128]),
                                                          in1=dtA[:].unsqueeze(2).to_broadcast([128, 16, 128]), op=ALU.mult), reads=[Tc[dd], dtA], writes=[R1])
                    tr.mark("need_r1")
                    for b4 in range(4):
                        tr.op("pe", lambda e, b4=b4: e.matmul(out=Dbank[b4][:], lhsT=Lmb[dd][:], rhs=R1[:, 4 * b4:4 * b4 + 4, :], start=True, stop=True),
                              reads=[Lmb[dd], R1], writes=[Dbank[b4]])
                        tr.op("act", lambda e, b4=b4: e.activation(out=E[:, 4 * b4:4 * b4 + 4, :], in_=Dbank[b4][:].rearrange("p (h i) -> p h i", h=4), func=AF.Exp), reads=[Dbank[b4]], writes=[E])
                    for g in range(2):
                        tr.op("pe", lambda e, g=g: e.matmul(out=Q[2][:, g * 128:(g + 1) * 128], lhsT=BT[:, g, tok0:tok0 + 128], rhs=CT[:, g, tok0:tok0 + 128], start=True, stop=True),
                              reads=[BT, CT], writes=[Q[2]])
                    tr.op("dve", lambda e: e.tensor_tensor(out=CBm[:], in0=Q[2][:, 0:256].rearrange("p (g i) -> p g i", g=2),
                                                          in1=Tc[dd][:].unsqueeze(1).to_broadcast([128, 2, 128]), op=ALU.mult), reads=[Q[2], Tc[dd]], writes=[CBm])
                    for g in range(2):
                        eng = "dve" if g == 0 else "pool"
                        tr.op(eng, lambda e, g=g: e.tensor_tensor(out=M[:, g * 8:(g + 1) * 8, :], in0=E[:, g * 8:(g + 1) * 8, :],
                                                                  in1=CBm[:, g:g + 1, :].to_broadcast([128, 8, 128]), op=ALU.mult), reads=[E, CBm], writes=[M])
                    Yb = [Q[3], Q[1]]
                    for h in range(16):
                        py = Yb[h // 8]
                        cs = (h % 8) * 64
                        tr.op("pe", lambda e, h=h, py=py, cs=cs: e.matmul(out=py[:, cs:cs + 64], lhsT=M[:, h, :], rhs=xdt[:, h * 64:(h + 1) * 64], start=True, stop=True),
                              reads=[M, xdt], writes=[py])
                    tr.mark("r1_done")
                tr.mark("need_state")
                Ob = [Q[2], Q[0]]
                if full:
                    tr.op("act", lambda e: e.activation(out=STbf[:].rearrange("p a b -> p (a b)"), in_=STd[:].rearrange("p a b -> p (a b)"), func=AF.Copy), reads=[STd], writes=[STbf])
                    for g in range(2):
                        tr.op("pe", lambda e, g=g: e.matmul(out=Ob[g][:], lhsT=CT[:, g, tok0:tok0 + 128], rhs=STbf[:, g, :], start=True, stop=True),
                              reads=[CT, STbf], writes=[Ob[g]])
                        tr.op("dve", lambda e, g=g: e.tensor_tensor(out=tmp[:, g * 512:(g + 1) * 512].rearrange("p (h q) -> p h q", h=8), in0=Ob[g][:].rearrange("p (h q) -> p h q", h=8),
                                                                    in1=ea_[:, g * 8:(g + 1) * 8].unsqueeze(2).to_broadcast([128, 8, 64]), op=ALU.mult), reads=[Ob[g], ea_], writes=[tmp])
                        tr.op("dve", lambda e, g=g: e.tensor_tensor(out=y_sb[:, g * 512:(g + 1) * 512], in0=Yb[g][:], in1=tmp[:, g * 512:(g + 1) * 512], op=ALU.add),
                              reads=[Yb[g], tmp], writes=[y_sb])
                for g in range(2):
                    tr.op("pe", lambda e, g=g: e.matmul(out=Ob[g][:], lhsT=b_t[:, g * 128:(g + 1) * 128], rhs=xdd_[:, g * 512:(g + 1) * 512], start=True, stop=True),
                          reads=[b_t, xdd_], writes=[Ob[g]])
                    tr.op("dve", lambda e, g=g: e.tensor_tensor(out=STd[:, g, :].rearrange("p (h q) -> p h q", h=8), in0=STd[:, g, :].rearrange("p (h q) -> p h q", h=8),
                                                                in1=dtot_[:, g * 8:(g + 1) * 8].unsqueeze(2).to_broadcast([128, 8, 64]), op=ALU.mult), reads=[STd, dtot_], writes=[STd])
                    tr.op("dve", lambda e, g=g: e.tensor_tensor(out=STd[:, g, :], in0=Ob[g][:], in1=STd[:, g, :], op=ALU.add), reads=[Ob[g], STd], writes=[STd])
                tr.mark("state_done")
                if not full:
                    return
                if not sweepA:
                    ys = yst[own_idx % 2]
                    tr.op("act", lambda e: e.activation(out=ys[:], in_=y_sb[:], func=AF.Copy), reads=[y_sb], writes=[ys])
                    tr.dma("sp", ysts[own_idx % 2], out=yB_v[own_idx], in_=ys[:], reads=[ys], writes=[])
                    return
                yb = yB_sb[own_idx % 2]
                tr.dma("sp", yBs[own_idx % 2], out=yb[:], in_=yB_v[own_idx], writes=[yb])
                tr.op("dve", lambda e: e.tensor_tensor(out=y_sb[:], in0=y_sb[:], in1=yb[:], op=ALU.add), reads=[y_sb, yb], writes=[y_sb])
                tr.op("pool", lambda e: e.tensor_tensor(out=tmp[:].rearrange("p (h q) -> p h q", h=16), in0=x_t[:].rearrange("p (h q) -> p h q", h=16),
                                                       in1=Dsk[:].unsqueeze(2).to_broadcast([128, 16, 64]), op=ALU.mult), reads=[x_t, Dsk], writes=[tmp])
                tr.op("dve", lambda e: e.tensor_tensor(out=y_sb[:], in0=y_sb[:], in1=tmp[:], op=ALU.add), reads=[y_sb, tmp], writes=[y_sb])
                tr.op("dve", lambda e: e.tensor_tensor(out=y_sb[:], in0=y_sb[:], in1=silz_[:], op=ALU.mult), reads=[y_sb, silz_], writes=[y_sb])
                for g in range(2):
                    tr.op("act", lambda e, g=g: e.activation(out=junk[:], in_=y_sb[:, g * 512:(g + 1) * 512], func=AF.Square, accum_out=ss2[:, g:g + 1]), reads=[y_sb], writes=[junk, ss2])
                tr.op("act", lambda e: e.activation(out=rs2[:], in_=ss2[:], func=AF.Ln, scale=1.0 / 512.0, bias=c_eps[:]), reads=[ss2, c_eps], writes=[rs2])
                tr.op("act", lambda e: e.activation(out=rs2[:], in_=rs2[:], func=AF.Exp, scale=-0.5), reads=[rs2], writes=[rs2])
                tr.op("dve", lambda e: e.tensor_tensor(out=y_sb[:].rearrange("p (g q) -> p g q", g=2), in0=y_sb[:].rearrange("p (g q) -> p g q", g=2),
                                                      in1=rs2[:].unsqueeze(2).to_broadcast([128, 2, 512]), op=ALU.mult), reads=[y_sb, rs2], writes=[y_sb])
                yo = yxs[own_idx % 2]
                tr.op("pool", lambda e: e.tensor_tensor(out=yo[:], in0=y_sb[:], in1=snb[:], op=ALU.mult), reads=[y_sb, snb], writes=[yo])
                tr.dma("sp", yxss[own_idx % 2], out=yx_v[own_idx][:, 1024:2048], in_=yo[:], reads=[yo], writes=[])

            def sweep(tiles):
                recs = []
                for seq, (t, dd, full, sweepA, own_idx) in enumerate(tiles):
                    tr.begin_record()
                    ssd_tile(t, dd, full, sweepA, own_idx, seq)
                    recs.append(tr.end_record())
                tr.run_pipelined(recs, depth=2)

            sweep([(t, 1, False, False, None) for t in (1, 0)])
            self.tap("sS_B", ST[1][:].rearrange("p a b -> p (a b)"), [128, 1024], F32, [ST[1]])
            sweep([(t, 1, False, False, None) for t in range(NT - 1, T_OTH0 - 1, -1)] +
                  [(t, 1, True, False, t - T_OWN0) for t in range(T_OTH0 - 1, T_OWN0 - 1, -1)])
            for e in Tracker.ENG:
                tr.wait_all(e, yst)
            sweep([(t, 0, False, True, None) for t in (0, 1)])
            self.tap("sS_A", ST[0][:].rearrange("p a b -> p (a b)"), [128, 1024], F32, [ST[0]])
            sweep([(t, 0, True, True, t - T_OWN0) for t in range(T_OWN0, T_OTH0)])
            for e in Tracker.ENG:
                tr.wait_all(e, yxs)
            self.barrier_release(rel)

    def stage_post(self, st):
        tr, c, I = self.tr, self.c, self.I
        self.fence()
        c["h_lat"] = self.sb(st, "h_lat", [128, 16, D], F32)
        c["h_r"] = [Res("h_lat%d" % i) for i in range(16)]
        c["h2T"] = self.sb(st, "h2T", [128, 8, NOWN], BF16)
        c["h2_r"] = [Res("h2T%d" % i) for i in range(16)]
        c["comb"] = self.sb(st, "comb", [128, 16, 32], F32)
        yx_v = c["yx"].t.rearrange("(n p) c -> n p c", p=128)
        h_lat, h2T = c["h_lat"], c["h2T"]
        with ExitStack() as s2:
            wo = self.sb(s2, "wo", [128, 16, D], BF16)
            wr = self.sb(s2, "wr", [128, 8, 36], F32)
            brr = self.sb(s2, "brr", [1, 36], F32)
            c_eps = self.sb(s2, "c_eps3", [128, 1], F32)
            lg = self.sb(s2, "lg", [128, 16, 36], F32)
            yxt = [self.sb(s2, "yxt%d" % i, [128, 2048], BF16) for i in range(2)]
            yxs = [self.dsem() for _ in range(2)]
            xr = [self.sb(s2, "xr2_%d" % i, [128, D], F32) for i in range(2)]
            xrs = [self.dsem() for _ in range(2)]
            junk = self.sb(s2, "pjunk", [128, D], BF16)
            PS = []
            for par in range(2):
                PS.append({"yxT": self.sb(s2, "yxT%d" % par, [128, 16, 128], BF16), "tmp": self.sb(s2, "ptmp%d" % par, [128, D], F32),
                           "h2f": self.sb(s2, "h2f%d" % par, [128, 8, 128], F32), "ss": self.sb(s2, "pss%d" % par, [128, 1], F32),
                           "rs": self.sb(s2, "prs%d" % par, [128, 1], F32), "B": [self.ps(s2, "ppB%d_%d" % (par, i)) for i in range(4)]})
            rel = [wo, wr, brr, c_eps, lg, junk] + yxt + xr
            for p_ in PS:
                rel += [p_["yxT"], p_["tmp"], p_["h2f"], p_["ss"], p_["rs"]] + p_["B"]
            w_out_v = I["w_out"].t.rearrange("(kc p) n -> p kc n", p=128)
            d0 = self.dsem(2)
            wst = [self.sb(s2, "wst%d" % i, [128, 1, D], F32) for i in range(2)]
            rel += wst
            wsts = [self.dsem() for _ in range(2)]
            for q in range(16):
                tr.dma("sp", wsts[q % 2], out=wst[q % 2][:], in_=w_out_v[:, q:q + 1, :], writes=[wst[q % 2]])
                tr.op("pool", lambda e, q=q: e.tensor_copy(out=wo[:, q:q + 1, :], in_=wst[q % 2][:]), reads=[wst[q % 2]], writes=[wo])
            tr.dma("sp", d0, out=wr[:].rearrange("p a b -> p (a b)"), in_=I["w_router"].t, writes=[wr])
            tr.dma("sp", d0, out=brr[:], in_=I["b_router"].t, writes=[brr])
            tr.op("pool", lambda e: e.memset(c_eps[:], EPS), writes=[c_eps])
            def post_tile(i):
                y_t, x_t = yxt[i % 2], xr[i % 2]
                p_ = PS[i % 2]
                yxT, tmp, h2f, ss, rs, B = p_["yxT"], p_["tmp"], p_["h2f"], p_["ss"], p_["rs"], p_["B"]
                hn = tmp
                pT = [B[0][:].bitcast(BF16), B[1][:].bitcast(BF16)]
                tr.dma("sp", yxs[i % 2], out=y_t[:], in_=yx_v[i], writes=[y_t])
                tr.dma("sp", xrs[i % 2], out=x_t[:], in_=I["xs"].t[NCTX + i * 128: NCTX + (i + 1) * 128, :], writes=[x_t])
                for kc in range(16):
                    tr.op("pe", lambda e, kc=kc: e.transpose(out=pT[kc // 8][:, (kc % 8) * 128:(kc % 8 + 1) * 128], in_=y_t[:, kc * 128:(kc + 1) * 128], identity=c["ident_b"][:]),
                          reads=[y_t, c["ident_b"]], writes=[B[kc // 8]])
                tr.op("act", lambda e: e.activation(out=yxT[:, 0:8, :].rearrange("p a b -> p (a b)"), in_=pT[0], func=AF.Copy), reads=[B[0]], writes=[yxT])
                tr.op("dve", lambda e: e.tensor_copy(out=yxT[:, 8:16, :].rearrange("p a b -> p (a b)"), in_=pT[1]), reads=[B[1]], writes=[yxT])
                hl = h_lat[:, i, :]
                for hh in range(2):
                    for kc in range(16):
                        tr.op("pe", lambda e, kc=kc, hh=hh: e.matmul(out=B[2 + hh][:], lhsT=yxT[:, kc, :], rhs=wo[:, kc, hh * 512:(hh + 1) * 512], start=(kc == 0), stop=(kc == 15)),
                              reads=[yxT, wo], writes=[B[2 + hh]])
                    tr.op("dve", lambda e, hh=hh: e.tensor_tensor(out=tmp[:, hh * 512:(hh + 1) * 512], in0=B[2 + hh][:], in1=c["g1_bc"][:, hh * 512:(hh + 1) * 512], op=ALU.mult),
                          reads=[B[2 + hh], c["g1_bc"]], writes=[tmp])
                tr.op("pool", lambda e: e.tensor_tensor(out=hl, in0=tmp[:], in1=x_t[:], op=ALU.add), reads=[tmp, x_t], writes=[c["h_r"][i]])
                tr.op("act", lambda e: e.activation(out=junk[:], in_=hl, func=AF.Square, accum_out=ss[:]), reads=[c["h_r"][i]], writes=[junk, ss])
                tr.op("act", lambda e: e.activation(out=rs[:], in_=ss[:], func=AF.Ln, scale=1.0 / D, bias=c_eps[:]), reads=[ss, c_eps], writes=[rs])
                tr.op("act", lambda e: e.activation(out=rs[:], in_=rs[:], func=AF.Exp, scale=-0.5), reads=[rs], writes=[rs])
                tr.op("dve", lambda e: e.tensor_scalar(out=hn[:], in0=hl, scalar1=rs[:], scalar2=None, op0=ALU.mult), reads=[c["h_r"][i], rs], writes=[hn])
                for kc in range(8):
                    tr.op("pe", lambda e, kc=kc: e.transpose(out=B[kc // 4][:, (kc % 4) * 128:(kc % 4 + 1) * 128], in_=hn[:, kc * 128:(kc + 1) * 128], identity=c["ident_f"][:]),
                          reads=[hn, c["ident_f"]], writes=[B[kc // 4]])
                for q in range(2):
                    tr.op("dve", lambda e, q=q: e.tensor_tensor(out=h2f[:, q * 4:(q + 1) * 4, :], in0=B[q][:].rearrange("p (k t) -> p k t", k=4),
                                                               in1=c["s2"][:, q * 4:(q + 1) * 4].unsqueeze(2).to_broadcast([128, 4, 128]), op=ALU.mult), reads=[B[q], c["s2"]], writes=[h2f])
                tr.op("pool", lambda e: e.tensor_tensor(out=h2f[:], in0=h2f[:], in1=c["b2"][:].unsqueeze(2).to_broadcast([128, 8, 128]), op=ALU.add), reads=[h2f, c["b2"]], writes=[h2f])
                tr.op("act", lambda e: e.activation(out=h2T[:, :, i * 128:(i + 1) * 128], in_=h2f[:], func=AF.Copy), reads=[h2f], writes=[c["h2_r"][i]])
                for kc in range(8):
                    tr.op("pe", lambda e, kc=kc: e.matmul(out=B[2][:, 0:36], lhsT=h2f[:, kc, :], rhs=wr[:, kc, :], start=(kc == 0), stop=False), reads=[h2f, wr], writes=[B[2]])
                tr.op("pe", lambda e: e.matmul(out=B[2][:, 0:36], lhsT=c["ones_f"][0:1, :], rhs=brr[0:1, :], start=False, stop=True), reads=[c["ones_f"], brr], writes=[B[2]])
                tr.op("dve", lambda e: e.tensor_copy(out=lg[:, i, :], in_=B[2][:, 0:36]), reads=[B[2]], writes=[lg])

            recs = []
            for i in range(16):
                tr.begin_record()
                post_tile(i)
                recs.append(tr.end_record())
            tr.run_pipelined(recs, depth=2)
            self.tap("lg", lg[:].rearrange("p a b -> p (a b)"), [128, 16 * 36], F32, [lg])
            self.tap("h_lat", h_lat[:].rearrange("p a b -> p (a b)"), [128, 16 * D], F32, c["h_r"])
            def T(name, shape):
                t_ = self.sb(s2, name, shape, F32)
                rel.append(t_)
                return t_
            gmax = T("gmax", [128, 16]); mg = T("mg", [128, 16, 4]); eg = T("eg", [128, 16, 4]); gsum = T("gsum", [128, 16]); pg = T("pg", [128, 16])
            t48 = T("t48", [128, 16, 4, 8]); ein = T("ein", [128, 16, 8]); m1 = T("m1", [128, 16]); k1 = T("k1", [128, 16, 8]); e2 = T("e2", [128, 16, 8])
            m2 = T("m2", [128, 16]); k2 = T("k2", [128, 16, 8]); dd_ = T("dd_", [128, 16]); w1 = T("w1", [128, 16]); w2 = T("w2", [128, 16]); cw8 = T("cw8", [128, 16, 8])
            gl = lg[:, :, 0:4]
            el = lg[:, :, 4:36].rearrange("p t (g x) -> p t g x", g=4)
            V = lambda fn, r, w: tr.op("dve", fn, reads=r, writes=w)
            V(lambda e: e.tensor_reduce(out=gmax[:], in_=gl, axis=AX.X, op=ALU.max), [lg], [gmax])
            V(lambda e: e.tensor_tensor(out=mg[:], in0=gl, in1=gmax[:].unsqueeze(2).to_broadcast([128, 16, 4]), op=ALU.is_equal), [lg, gmax], [mg])
            V(lambda e: e.tensor_tensor(out=eg[:], in0=gl, in1=gmax[:].unsqueeze(2).to_broadcast([128, 16, 4]), op=ALU.subtract), [lg, gmax], [eg])
            tr.op("act", lambda e: e.activation(out=eg[:], in_=eg[:], func=AF.Exp), reads=[eg], writes=[eg])
            V(lambda e: e.tensor_reduce(out=gsum[:], in_=eg[:], axis=AX.X, op=ALU.add), [eg], [gsum])
            V(lambda e: e.reciprocal(out=pg[:], in_=gsum[:]), [gsum], [pg])
            V(lambda e: e.tensor_tensor(out=t48[:], in0=el, in1=mg[:].unsqueeze(3).to_broadcast([128, 16, 4, 8]), op=ALU.mult), [lg, mg], [t48])
            V(lambda e: e.tensor_reduce(out=ein[:], in_=t48[:].rearrange("p t g x -> p t x g"), axis=AX.X, op=ALU.add), [t48], [ein])
            V(lambda e: e.tensor_reduce(out=m1[:], in_=ein[:], axis=AX.X, op=ALU.max), [ein], [m1])
            V(lambda e: e.tensor_tensor(out=k1[:], in0=ein[:], in1=m1[:].unsqueeze(2).to_broadcast([128, 16, 8]), op=ALU.is_equal), [ein, m1], [k1])
            V(lambda e: e.scalar_tensor_tensor(out=e2[:], in0=k1[:], scalar=-1.0e30, in1=ein[:], op0=ALU.mult, op1=ALU.add), [k1, ein], [e2])
            V(lambda e: e.tensor_reduce(out=m2[:], in_=e2[:], axis=AX.X, op=ALU.max), [e2], [m2])
            V(lambda e: e.tensor_tensor(out=k2[:], in0=e2[:], in1=m2[:].unsqueeze(2).to_broadcast([128, 16, 8]), op=ALU.is_equal), [e2, m2], [k2])
            V(lambda e: e.tensor_tensor(out=dd_[:], in0=m2[:], in1=m1[:], op=ALU.subtract), [m1, m2], [dd_])
            tr.op("act", lambda e: e.activation(out=dd_[:], in_=dd_[:], func=AF.Exp), reads=[dd_], writes=[dd_])
            V(lambda e: e.tensor_scalar(out=w1[:], in0=dd_[:], scalar1=1.0, scalar2=None, op0=ALU.add), [dd_], [w1])
            V(lambda e: e.reciprocal(out=w1[:], in_=w1[:]), [w1], [w1])
            V(lambda e: e.tensor_tensor(out=w2[:], in0=dd_[:], in1=w1[:], op=ALU.mult), [dd_, w1], [w2])
            V(lambda e: e.tensor_tensor(out=w1[:], in0=w1[:], in1=pg[:], op=ALU.mult), [w1, pg], [w1])
            V(lambda e: e.tensor_tensor(out=w2[:], in0=w2[:], in1=pg[:], op=ALU.mult), [w2, pg], [w2])
            V(lambda e: e.tensor_tensor(out=k1[:], in0=k1[:], in1=w1[:].unsqueeze(2).to_broadcast([128, 16, 8]), op=ALU.mult), [k1, w1], [k1])
            V(lambda e: e.tensor_tensor(out=k2[:], in0=k2[:], in1=w2[:].unsqueeze(2).to_broadcast([128, 16, 8]), op=ALU.mult), [k2, w2], [k2])
            V(lambda e: e.tensor_tensor(out=cw8[:], in0=k1[:], in1=k2[:], op=ALU.add), [k1, k2], [cw8])
            V(lambda e: e.tensor_tensor(out=c["comb"][:].rearrange("p t (g x) -> p t g x", g=4), in0=mg[:].unsqueeze(3).to_broadcast([128, 16, 4, 8]),
                                        in1=cw8[:].unsqueeze(2).to_broadcast([128, 16, 4, 8]), op=ALU.mult), [mg, cw8], [c["comb"]])
            self.tap("comb", c["comb"][:].rearrange("p a b -> p (a b)"), [128, 512], F32, [c["comb"]])
            self.barrier_release(rel)

    def stage_moe(self, st):
        tr, c, I = self.tr, self.c, self.I
        self.fence()
        h_lat, h2T, comb = c["h_lat"], c["h2T"], c["comb"]
        with ExitStack() as s2:
            wgt = [self.sb(s2, "mwg%d" % i, [128, 8, DFF], BF16) for i in range(2)]
            wut = [self.sb(s2, "mwu%d" % i, [128, 8, DFF], BF16) for i in range(2)]
            wdt = [self.sb(s2, "mwd%d" % i, [128, 4, D], BF16) for i in range(2)]
            stg = [self.sb(s2, "mstg%d" % i, [128, 8, DFF], F32) for i in range(2)]
            stgs = [self.dsem() for _ in range(2)]
            ns = [0]
            sg_ = [self.sb(s2, "msg%d" % i, [128, 512], F32) for i in range(2)]
            heT = [self.sb(s2, "heT%d" % i, [128, 4, 512], BF16) for i in range(2)]
            pG = [self.ps(s2, "mpG%d" % i) for i in range(2)]
            pU = [self.ps(s2, "mpU%d" % i) for i in range(2)]
            pDn = [self.ps(s2, "mpD%d" % i) for i in range(4)]
            rel = wgt + wut + wdt + sg_ + heT + pG + pU + pDn + stg
            nb = 0
            pending = [None]

            def emit_down(he, wd_e, j, ex):
                for tt in range(4):
                    ti = j * 4 + tt
                    for hh in range(2):
                        d_p = pDn[(tt * 2 + hh) % 4]
                        for fc in range(4):
                            tr.op("pe", lambda e, fc=fc: e.matmul(out=d_p[:], lhsT=he[:, fc, tt * 128:(tt + 1) * 128], rhs=wd_e[:, fc, hh * 512:(hh + 1) * 512],
                                                                  start=(fc == 0), stop=(fc == 3)), reads=[he, wd_e], writes=[d_p])
                        tr.op("dve", lambda e: e.scalar_tensor_tensor(
                            out=h_lat[:, ti, hh * 512:(hh + 1) * 512], in0=d_p[:], scalar=comb[:, ti, ex:ex + 1], in1=h_lat[:, ti, hh * 512:(hh + 1) * 512], op0=ALU.mult, op1=ALU.add),
                            reads=[d_p, comb, c["h_r"][ti]], writes=[c["h_r"][ti]])

            for ex in range(NEXP):
                k = ex % 2
                wg_e, wu_e, wd_e = wgt[k], wut[k], wdt[k]
                for (dst, src) in ((wg_e, I["w_gate"].t[ex].rearrange("(kc p) n -> p kc n", p=128)), (wu_e, I["w_up"].t[ex].rearrange("(kc p) n -> p kc n", p=128))):
                    sg_t, sg_s = stg[ns[0] % 2], stgs[ns[0] % 2]
                    ns[0] += 1
                    tr.dma("sp", sg_s, out=sg_t[:], in_=src, writes=[sg_t])
                    tr.op("pool", lambda e, dst=dst, sg_t=sg_t: e.tensor_copy(out=dst[:], in_=sg_t[:]), reads=[sg_t], writes=[dst])
                sg_t, sg_s = stg[ns[0] % 2], stgs[ns[0] % 2]
                ns[0] += 1
                sv = sg_t[:].rearrange("p a b -> p (a b)").rearrange("p (f n) -> p f n", f=4)
                tr.dma("sp", sg_s, out=sv, in_=I["w_down"].t[ex].rearrange("(fc p) n -> p fc n", p=128), writes=[sg_t])
                tr.op("pool", lambda e, wd_e=wd_e, sv=sv: e.tensor_tensor(out=wd_e[:], in0=sv, in1=c["g2_bc"][:].unsqueeze(1).to_broadcast([128, 4, D]), op=ALU.mult),
                      reads=[sg_t, c["g2_bc"]], writes=[wd_e])
                for j in range(4):
                    he = heT[nb % 2]
                    nb += 1
                    hres = [c["h2_r"][j * 4 + q] for q in range(4)]
                    for fc in range(4):
                        g_p, u_p, sg = pG[fc % 2], pU[fc % 2], sg_[fc % 2]
                        for kc in range(8):
                            tr.op("pe", lambda e, kc=kc, fc=fc, g_p=g_p: e.matmul(out=g_p[:], lhsT=wg_e[:, kc, fc * 128:(fc + 1) * 128], rhs=h2T[:, kc, j * 512:(j + 1) * 512],
                                                                               start=(kc == 0), stop=(kc == 7)), reads=[wg_e] + hres, writes=[g_p])
                        for kc in range(8):
                            tr.op("pe", lambda e, kc=kc, fc=fc, u_p=u_p: e.matmul(out=u_p[:], lhsT=wu_e[:, kc, fc * 128:(fc + 1) * 128], rhs=h2T[:, kc, j * 512:(j + 1) * 512],
                                                                               start=(kc == 0), stop=(kc == 7)), reads=[wu_e] + hres, writes=[u_p])
                        tr.op("act", lambda e, g_p=g_p, sg=sg: e.activation(out=sg[:], in_=g_p[:], func=AF.Silu), reads=[g_p], writes=[sg])
                        tr.op("dve", lambda e, u_p=u_p, sg=sg, fc=fc, he=he: e.tensor_tensor(out=he[:, fc, :], in0=u_p[:], in1=sg[:], op=ALU.mult), reads=[u_p, sg], writes=[he])
                    if pending[0] is not None:
                        emit_down(*pending[0])
                    pending[0] = (he, wd_e, j, ex)
            emit_down(*pending[0])
            self.tap("h_fin", h_lat[:].rearrange("p a b -> p (a b)"), [128, 16 * D], F32, c["h_r"])
            self.barrier_release(rel)

    def stage_final(self, st):
        tr, c, I = self.tr, self.c, self.I
        self.fence()
        h_lat = c["h_lat"]
        out_v = self.out.t.rearrange("(n p) c -> n p c", p=128)
        with ExitStack() as s2:
            fn = self.sb(s2, "fn_bc", [128, D], F32)
            c_eps = self.sb(s2, "c_eps4", [128, 1], F32)
            junk = self.sb(s2, "fjunk", [128, D], BF16)
            ss = [self.sb(s2, "fss%d" % i, [128, 1], F32) for i in range(2)]
            rs = [self.sb(s2, "frs%d" % i, [128, 1], F32) for i in range(2)]
            ob = [self.sb(s2, "fob%d" % i, [128, D], F32) for i in range(2)]
            obs = [self.dsem() for _ in range(2)]
            tr.dma("sp", self.dsem(), out=fn[:], in_=I["final_norm"].t.partition_broadcast(128), writes=[fn])
            tr.op("pool", lambda e: e.memset(c_eps[:], EPS), writes=[c_eps])
            for i in range(16):
                hl = h_lat[:, i, :]
                s_, r_, o_ = ss[i % 2], rs[i % 2], ob[i % 2]
                tr.op("act", lambda e, s_=s_, hl=hl: e.activation(out=junk[:], in_=hl, func=AF.Square, accum_out=s_[:]), reads=[c["h_r"][i]], writes=[junk, s_])
                tr.op("act", lambda e, s_=s_, r_=r_: e.activation(out=r_[:], in_=s_[:], func=AF.Ln, scale=1.0 / D, bias=c_eps[:]), reads=[s_, c_eps], writes=[r_])
                tr.op("act", lambda e, r_=r_: e.activation(out=r_[:], in_=r_[:], func=AF.Exp, scale=-0.5), reads=[r_], writes=[r_])
                tr.op("dve", lambda e, r_=r_, o_=o_, hl=hl: e.scalar_tensor_tensor(out=o_[:], in0=hl, scalar=r_[:], in1=fn[:], op0=ALU.mult, op1=ALU.mult),
                      reads=[c["h_r"][i], r_, fn], writes=[o_])
                tr.dma("sp", obs[i % 2], out=out_v[i], in_=o_[:], reads=[o_], writes=[])
            self.final += ob


def prep_core(inp, b, hf):
    L = 0
    rev = hf == 1
    x, ctx = inp["x"][b], inp["ctx"][b]
    if not rev:
        ctx_a, own, oth = ctx, x[0:2048], x[2048:4096]
        dA, dB = 0, 1
    else:
        ctx_a, own, oth = ctx[::-1], x[2048:4096][::-1], x[0:2048][::-1]
        dA, dB = 1, 0
    m = {}
    m["xs"] = np.ascontiguousarray(np.concatenate([ctx_a, own, oth], axis=0), dtype=np.float32)
    cT = np.stack([inp["c"][b].reshape(8, 128).T, inp["c_ctx"].reshape(8, 128).T], axis=2).reshape(128, 16)
    m["cT"] = np.ascontiguousarray(cT, dtype=np.float32)
    m["w_ada"] = np.ascontiguousarray(inp["w_ada"][L])
    m["b_ada"] = np.ascontiguousarray(inp["b_ada"][L].reshape(1, -1))
    m["norm_mix_fm"] = np.ascontiguousarray(inp["norm_mix"][L].reshape(8, 128).T)
    m["norm_ffn_fm"] = np.ascontiguousarray(inp["norm_ffn"][L].reshape(8, 128).T)
    w_in = inp["w_in"][L]
    if rev:
        w_in = np.concatenate([w_in[:, :OFF_G], w_in[:, OFF_G + 16:OFF_G + 32], w_in[:, OFF_G:OFF_G + 16],
                               w_in[:, OFF_Z:OFF_DT], w_in[:, OFF_DT + 16:OFF_DT + 32], w_in[:, OFF_DT:OFF_DT + 16]], axis=1)
    m["w_in"] = np.ascontiguousarray(w_in)
    wu, gb = inp["gla_w_up"][L], inp["gla_b"][L]
    m["w_up_aug"] = np.ascontiguousarray(np.stack([np.concatenate([wu[dA], gb[dA][None, :]], axis=0),
                                                   np.concatenate([wu[dB], gb[dB][None, :]], axis=0)], axis=0))
    m["gla_norm"] = np.ascontiguousarray(inp["gla_norm"][L].reshape(1, -1))
    m["ssd_norm"] = np.ascontiguousarray(inp["ssd_norm"][L].reshape(1, -1))
    m["final_norm"] = np.ascontiguousarray(inp["final_norm"].reshape(1, -1))
    cw = inp["ssd_conv_w"][L]
    if rev:
        cw = cw[::-1, ::-1, :]
    m["conv_w_fm"] = np.ascontiguousarray(cw.reshape(9, 12, 128).transpose(2, 1, 0))
    m["conv_b_fm"] = np.ascontiguousarray(inp["ssd_conv_b"][L].reshape(12, 128).T)
    m["dt_bias"] = np.ascontiguousarray(np.concatenate([inp["ssd_dt_bias"][L][dA], inp["ssd_dt_bias"][L][dB]]).reshape(1, 32))
    m["a_log"] = np.ascontiguousarray(np.concatenate([inp["ssd_a_log"][L][dA], inp["ssd_a_log"][L][dB]]).reshape(1, 32))
    m["ssd_d"] = np.ascontiguousarray(inp["ssd_d"][L].reshape(1, 16))
    m["w_out"] = np.ascontiguousarray(inp["w_out"][L])
    wrt = np.concatenate([inp["router_group_w"][L], inp["router_expert_w"][L]], axis=1)
    m["w_router"] = np.ascontiguousarray(wrt.reshape(8, 128, 36).transpose(1, 0, 2).reshape(128, 8 * 36))
    m["b_router"] = np.ascontiguousarray(np.concatenate([inp["router_group_b"][L], inp["router_expert_b"][L]]).reshape(1, 36))
    m["w_gate"] = np.ascontiguousarray(inp["expert_w_gate"][L])
    m["w_up"] = np.ascontiguousarray(inp["expert_w_up"][L])
    m["w_down"] = np.ascontiguousarray(inp["expert_w_down"][L])
    return {k: np.asarray(v, dtype=np.float32) for k, v in m.items()}


def run(inputs, debug=None, stop_after=None, cores=8):
    bld = Builder(debug=debug, stop_after=stop_after)
    nc = bld.build()
    in_maps = [prep_core(inputs, i // 2, i % 2) for i in range(cores)]
    res = run_bass_kernel_spmd(nc, in_maps, core_ids=list(range(cores)))
    return res, bld


def kernel(**inputs):
    inputs = {k: np.asarray(v) for k, v in inputs.items()}
    res, _ = run(inputs)
    out = np.empty((4, 4096, D), dtype=np.float32)
    for i in range(8):
        b, hf = i // 2, i % 2
        o = np.asarray(res.results[i]["out"], dtype=np.float32)
        if hf == 0:
            out[b, 0:2048] = o
        else:
            out[b, 2048:4096] = o[::-1]
    return out
```
